# Optimizing a Trainium2 kernel written in Bass

```python
import math
import jax
import jax.numpy as jnp
from jax import lax
import numpy as np

D_MODEL = 1024
BATCH = 2
SEQ = 8192
DEPTH = 2

GRID_W = 64
CTX_LEN = 256
N_BRANCH = 4
BRANCH_W = D_MODEL // 4
FNET_GROUPS = 4
FNET_GROUP_DIM = BRANCH_W // FNET_GROUPS
S5_GROUP_CH = 16
S5_GROUPS = BRANCH_W // S5_GROUP_CH
S5_STATE = 64
RET_HEADS = 4
RET_DIM = BRANCH_W // RET_HEADS
RET_CHUNK = 128
NA_HEADS = 4
NA_DIM = BRANCH_W // NA_HEADS
NA_WIN_ROWS = 8
NA_WIN_COLS = 16
ROPE_BASE = 10000.0
D_FF = -(-(8 * D_MODEL // 3) // 256) * 256
N_EXPERTS = 8
TOP_K = 2
D_FF_EXPERT = 7 * D_MODEL // 2
N_DENSE = (DEPTH + 1) // 2
N_MOE = DEPTH // 2
EPS = 1e-6
IN_WIDTHS = (BRANCH_W,) * 9 + (N_BRANCH * D_MODEL,)
IN_W = sum(IN_WIDTHS)
IN_SPLITS = tuple(sum(IN_WIDTHS[:i + 1]) for i in range(len(IN_WIDTHS) - 1))
F32 = jnp.float32

kernel_name = 'hybrid_gated_mixer_dit_block'


def rms_norm(x, g):
    xf = x.astype(F32)
    y = xf * lax.rsqrt(jnp.mean(xf * xf, axis=-1, keepdims=True) + EPS)
    return (y * g.astype(F32)).astype(x.dtype)


def split_heads(t, n_heads):
    return t.reshape(t.shape[0], t.shape[1], n_heads, t.shape[-1] // n_heads)


def flip_seq(t, rev):
    return jnp.flip(t, axis=1) if rev else t


def axial_rope(n_tokens, head_dim):
    t = jnp.arange(n_tokens)
    row = (t // GRID_W).astype(F32)
    col = (t % GRID_W).astype(F32)
    n_freq = head_dim // 4
    inv_freq = 1.0 / (ROPE_BASE ** (jnp.arange(n_freq, dtype=F32) / n_freq))
    ang = jnp.concatenate([row[:, None] * inv_freq, col[:, None] * inv_freq], axis=-1)
    return jnp.cos(ang), jnp.sin(ang)


def apply_rope(x, cos, sin):
    x1, x2 = jnp.split(x, 2, axis=-1)
    c, s = cos[:, None, :], sin[:, None, :]
    return jnp.concatenate([x1 * c - x2 * s, x1 * s + x2 * c], axis=-1)


def fourier_mix(u):
    b_, l_, _ = u.shape
    ug = u.astype(F32).reshape(b_, l_, FNET_GROUPS, FNET_GROUP_DIM)
    f = jnp.fft.fft2(ug, axes=(1, 3), norm='ortho').real
    return f.reshape(b_, l_, BRANCH_W).astype(u.dtype)


def linear_combine(left, right):
    a_l, b_l = left
    a_r, b_r = right
    return a_l * a_r, a_r * b_l + b_r


def s5_states(u, a_re, a_im, log_dt, b_re, b_im, h0):
    lam = lax.complex(a_re, a_im)
    a_bar = jnp.exp(lam * jnp.exp(log_dt)[:, None])
    b_bar = ((a_bar - 1.0) / lam)[..., None] * lax.complex(b_re, b_im)
    bu = jnp.einsum('blgh,gph->blgp', u.astype(jnp.complex64), b_bar)
    bu = bu.at[:, 0].add(a_bar * h0)
    a = jnp.broadcast_to(a_bar, bu.shape)
    _, h = lax.associative_scan(linear_combine, (a, bu), axis=1)
    return h


def s5_mixer(u_l, u_c, a_re, a_im, log_dt, b_re, b_im, c_re, c_im, d_skip, w_glu, b_glu, need_ctx_out):
    dtype = u_l.dtype
    b_ = u_l.shape[0]

    def groups(u):
        return u.astype(F32).reshape(u.shape[0], u.shape[1], S5_GROUPS, S5_GROUP_CH)

    ul, uc = groups(u_l), groups(u_c)
    d = d_skip.astype(F32).reshape(S5_GROUPS, S5_GROUP_CH)
    y_l = d * ul
    y_c = d * uc if need_ctx_out else None
    for dirn in range(2):
        rev = dirn == 1
        prm = [p[dirn].astype(F32) for p in (a_re, a_im, log_dt, b_re, b_im)]
        c_mat = lax.complex(c_re[dirn].astype(F32), c_im[dirn].astype(F32))
        h0 = jnp.zeros((b_, S5_GROUPS, S5_STATE), jnp.complex64)
        h_c = s5_states(flip_seq(uc, rev), *prm, h0)
        h_l = s5_states(flip_seq(ul, rev), *prm, h_c[:, -1])
        y_l = y_l + flip_seq(jnp.einsum('blgp,ghp->blgh', h_l, c_mat).real, rev)
        if need_ctx_out:
            y_c = y_c + flip_seq(jnp.einsum('blgp,ghp->blgh', h_c, c_mat).real, rev)

    def glu(y):
        z = jax.nn.gelu(y).reshape(y.shape[0], y.shape[1], BRANCH_W).astype(dtype)
        return z * jax.nn.sigmoid(z @ w_glu + b_glu)

    return glu(y_l), (glu(y_c) if need_ctx_out else None)


def retention_scan(q, k, v, log_gamma, s0, with_output):
    b_, l_, h_, dk = k.shape
    dv = v.shape[-1]
    n_ch = l_ // RET_CHUNK
    kc = k.reshape(b_, n_ch, RET_CHUNK, h_, dk)
    vc = v.reshape(b_, n_ch, RET_CHUNK, h_, dv)
    pos = jnp.arange(RET_CHUNK, dtype=F32)
    k_decay = jnp.exp((RET_CHUNK - 1.0 - pos)[:, None] * log_gamma[None, :])
    chunk_decay = jnp.exp(RET_CHUNK * log_gamma)[None, :, None, None]
    kv = jnp.einsum('bnjhd,bnjhe->nbhde', kc * k_decay[:, :, None], vc)

    def step(s, kv_n):
        return chunk_decay * s + kv_n, s

    s_final, s_prev = lax.scan(step, s0, kv)
    if not with_output:
        return None, s_final
    qc = q.reshape(b_, n_ch, RET_CHUNK, h_, dk)
    diff = pos[:, None] - pos[None, :]
    intra = jnp.where(diff >= 0, jnp.exp(jnp.maximum(diff, 0.0)[None] * log_gamma[:, None, None]), 0.0)
    q_decay = jnp.exp((pos + 1.0)[:, None] * log_gamma[None, :])
    scores = jnp.einsum('bnihd,bnjhd->bnhij', qc, kc) * intra
    inner = jnp.einsum('bnhij,bnjhe->bnihe', scores, vc)
    cross = jnp.einsum('bnihd,nbhde->bnihe', qc * q_decay[:, :, None], s_prev)
    return (inner + cross).reshape(b_, l_, h_, dv), s_final


def head_norm(o, g):
    mu = jnp.mean(o, axis=-1, keepdims=True)
    var = jnp.mean(jnp.square(o - mu), axis=-1, keepdims=True)
    y = (o - mu) * lax.rsqrt(var + EPS)
    return y.reshape(o.shape[0], o.shape[1], -1) * g.astype(F32)


def retention_mixer(q_l, k_l, v_l, g_l, q_c, k_c, v_c, g_c, ret_decay, ret_gn, need_ctx_out):
    dtype = g_l.dtype
    log_gamma = jax.nn.log_sigmoid(ret_decay.astype(F32))
    s0 = jnp.zeros((q_l.shape[0], RET_HEADS, RET_DIM, RET_DIM), F32)
    o_l = jnp.zeros_like(v_l)
    o_c = jnp.zeros_like(v_c) if need_ctx_out else None
    for dirn in range(2):
        rev = dirn == 1
        oc_d, s_c = retention_scan(flip_seq(q_c, rev), flip_seq(k_c, rev), flip_seq(v_c, rev),
                                   log_gamma[dirn], s0, need_ctx_out)
        ol_d, _ = retention_scan(flip_seq(q_l, rev), flip_seq(k_l, rev), flip_seq(v_l, rev),
                                 log_gamma[dirn], s_c, True)
        o_l = o_l + flip_seq(ol_d, rev)
        if need_ctx_out:
            o_c = o_c + flip_seq(oc_d, rev)
    out_l = (jax.nn.silu(g_l.astype(F32)) * head_norm(o_l, ret_gn)).astype(dtype)
    out_c = (jax.nn.silu(g_c.astype(F32)) * head_norm(o_c, ret_gn)).astype(dtype) if need_ctx_out else None
    return out_l, out_c


def neighborhood_attention(q_l, k_l, v_l, q_c, k_c, v_c, rpb, need_ctx_out):
    b_, n_, h_, dh = q_l.shape
    rows = n_ // GRID_W
    kr = min(NA_WIN_ROWS, rows)
    kw = NA_WIN_COLS
    scale = dh ** -0.5
    qg = q_l.reshape(b_, rows, GRID_W, h_, dh)
    kg = k_l.reshape(b_, rows, GRID_W, h_, dh)
    vg = v_l.reshape(b_, rows, GRID_W, h_, dh)
    r = jnp.arange(rows)
    row_idx = jnp.clip(r - kr // 2, 0, rows - kr)[:, None] + jnp.arange(kr)[None, :]
    k_band = kg[:, row_idx]
    v_band = vg[:, row_idx]
    col = jnp.arange(GRID_W)
    col_start = jnp.clip(col - kw // 2, 0, GRID_W - kw)
    in_win = (col[None, :] >= col_start[:, None]) & (col[None, :] < col_start[:, None] + kw)
    dr = row_idx - r[:, None] + (NA_WIN_ROWS - 1)
    dc = jnp.clip(col[None, :] - col[:, None], -(kw - 1), kw - 1) + (kw - 1)
    bias = rpb[:, dr[:, None, :, None], dc[None, :, None, :]].astype(F32)
    s_band = jnp.einsum('brqhd,brikhd->bhrqik', qg, k_band).astype(F32) * scale + bias[None]
    s_band = jnp.where(in_win[:, None, :], s_band, -jnp.inf)
    s_ctx = jnp.einsum('brqhd,bchd->bhrqc', qg, k_c).astype(F32) * scale
    n_band = kr * GRID_W
    s_all = jnp.concatenate([s_band.reshape(b_, h_, rows, GRID_W, n_band), s_ctx], axis=-1)
    p = jax.nn.softmax(s_all, axis=-1).astype(v_l.dtype)
    p_band = p[..., :n_band].reshape(b_, h_, rows, GRID_W, kr, GRID_W)
    o = (jnp.einsum('bhrqik,brikhd->brqhd', p_band, v_band)
         + jnp.einsum('bhrqc,bchd->brqhd', p[..., n_band:], v_c))
    out_l = o.reshape(b_, n_, h_ * dh)
    out_c = None
    if need_ctx_out:
        s_cc = jnp.einsum('bqhd,bkhd->bhqk', q_c, k_c).astype(F32) * scale
        p_cc = jax.nn.softmax(s_cc, axis=-1).astype(v_c.dtype)
        out_c = jnp.einsum('bhqk,bkhd->bqhd', p_cc, v_c).reshape(b_, q_c.shape[1], h_ * dh)
    return out_l, out_c


def merge_branches(outs, gate_logits, w_branch, w_out):
    gates = jax.nn.sigmoid(gate_logits.astype(F32)).astype(gate_logits.dtype)
    y = gates[..., :D_MODEL] * (outs[0] @ w_branch[0])
    for b in range(1, N_BRANCH):
        y = y + gates[..., b * D_MODEL:(b + 1) * D_MODEL] * (outs[b] @ w_branch[b])
    return y @ w_out


def token_mixers(h_l, h_c, w_in, s5_a_re, s5_a_im, s5_log_dt, s5_b_re, s5_b_im, s5_c_re, s5_c_im,
                 s5_d, s5_w_glu, s5_b_glu, ret_decay, ret_gn, na_rpb, w_branch, w_out,
                 rope_cos, rope_sin, need_ctx_out):
    f_l, s_l, rq_l, rk_l, rv_l, rg_l, nq_l, nk_l, nv_l, gt_l = jnp.split(h_l @ w_in, IN_SPLITS, axis=-1)
    f_c, s_c, rq_c, rk_c, rv_c, rg_c, nq_c, nk_c, nv_c, gt_c = jnp.split(h_c @ w_in, IN_SPLITS, axis=-1)
    a_l = fourier_mix(f_l)
    b_l, b_c = s5_mixer(s_l, s_c, s5_a_re, s5_a_im, s5_log_dt, s5_b_re, s5_b_im, s5_c_re, s5_c_im,
                        s5_d, s5_w_glu, s5_b_glu, need_ctx_out)
    k_scale = RET_DIM ** -0.5

    def rh(t):
        return split_heads(t.astype(F32), RET_HEADS)

    q_lr = apply_rope(rh(rq_l), rope_cos, rope_sin)
    k_lr = apply_rope(rh(rk_l), rope_cos, rope_sin) * k_scale
    r_l, r_c = retention_mixer(q_lr, k_lr, rh(rv_l), rg_l, rh(rq_c), rh(rk_c) * k_scale, rh(rv_c), rg_c,
                               ret_decay, ret_gn, need_ctx_out)
    n_l, n_c = neighborhood_attention(split_heads(nq_l, NA_HEADS), split_heads(nk_l, NA_HEADS),
                                      split_heads(nv_l, NA_HEADS), split_heads(nq_c, NA_HEADS),
                                      split_heads(nk_c, NA_HEADS), split_heads(nv_c, NA_HEADS),
                                      na_rpb, need_ctx_out)
    y_l = merge_branches((a_l, b_l, r_l, n_l), gt_l, w_branch, w_out)
    y_c = merge_branches((fourier_mix(f_c), b_c, r_c, n_c), gt_c, w_branch, w_out) if need_ctx_out else None
    return y_l, y_c


def swiglu(h, w_gate, w_up, w_down):
    return (jax.nn.silu(h @ w_gate) * (h @ w_up)) @ w_down


def moe_swiglu(h, w_router, b_router, w_gate, w_up, w_down):
    logits = (h @ w_router).astype(F32) + b_router.astype(F32)
    top_val, top_idx = lax.top_k(logits, TOP_K)
    top_w = jax.nn.softmax(top_val, axis=-1)
    combine = jnp.sum(jax.nn.one_hot(top_idx, N_EXPERTS, dtype=F32) * top_w[..., None], axis=-2).astype(h.dtype)
    out = jnp.zeros_like(h)
    for e in range(N_EXPERTS):
        out = out + combine[..., e:e + 1] * swiglu(h, w_gate[e], w_up[e], w_down[e])
    return out


def setup_inputs(seed: int = 0) -> dict:
    key = jax.random.key(seed)
    ks = iter(jax.random.split(key, 40))

    def nrm(shape, scale):
        return jax.random.normal(next(ks), shape, F32) * scale

    L = DEPTH
    s5_shape = (L, 2, S5_GROUPS, S5_STATE)
    ret_init = jnp.asarray(np.log(2.0 ** (5 + np.arange(RET_HEADS)) - 1.0), F32)
    return {
        'x': nrm((BATCH, SEQ, D_MODEL), 1.0),
        'c': nrm((BATCH, D_MODEL), 1.0),
        'ctx': nrm((BATCH, CTX_LEN, D_MODEL), 1.0),
        'c_ctx': nrm((D_MODEL,), 1.0),
        'w_mod': nrm((L, D_MODEL, 6 * D_MODEL), 0.5 * D_MODEL ** -0.5),
        'b_mod': nrm((L, 6 * D_MODEL), 0.01),
        'norm_g': 1.0 + nrm((L, 4, D_MODEL), 0.05),
        'w_in': nrm((L, D_MODEL, IN_W), D_MODEL ** -0.5),
        's5_a_re': -0.5 + nrm(s5_shape, 0.01),
        's5_a_im': math.pi * jnp.arange(S5_STATE, dtype=F32) + nrm(s5_shape, 0.01),
        's5_log_dt': jax.random.uniform(next(ks), (L, 2, S5_GROUPS), F32, math.log(1e-3), math.log(1e-1)),
        's5_b_re': nrm((L, 2, S5_GROUPS, S5_STATE, S5_GROUP_CH), (2 * S5_GROUP_CH) ** -0.5),
        's5_b_im': nrm((L, 2, S5_GROUPS, S5_STATE, S5_GROUP_CH), (2 * S5_GROUP_CH) ** -0.5),
        's5_c_re': nrm((L, 2, S5_GROUPS, S5_GROUP_CH, S5_STATE), S5_STATE ** -0.5),
        's5_c_im': nrm((L, 2, S5_GROUPS, S5_GROUP_CH, S5_STATE), S5_STATE ** -0.5),
        's5_d': nrm((L, BRANCH_W), 1.0),
        's5_w_glu': nrm((L, BRANCH_W, BRANCH_W), BRANCH_W ** -0.5),
        's5_b_glu': nrm((L, BRANCH_W), 0.01),
        'ret_decay': ret_init + nrm((L, 2, RET_HEADS), 0.05),
        'ret_gn': 1.0 + nrm((L, BRANCH_W), 0.05),
        'na_rpb': nrm((L, NA_HEADS, 2 * NA_WIN_ROWS - 1, 2 * NA_WIN_COLS - 1), 0.02),
        'w_branch': nrm((L, N_BRANCH, BRANCH_W, D_MODEL), BRANCH_W ** -0.5),
        'w_out': nrm((L, D_MODEL, D_MODEL), D_MODEL ** -0.5),
        'ffn_w_gate': nrm((N_DENSE, D_MODEL, D_FF), D_MODEL ** -0.5),
        'ffn_w_up': nrm((N_DENSE, D_MODEL, D_FF), D_MODEL ** -0.5),
        'ffn_w_down': nrm((N_DENSE, D_FF, D_MODEL), D_FF ** -0.5),
        'moe_w_router': nrm((N_MOE, D_MODEL, N_EXPERTS), D_MODEL ** -0.5),
        'moe_b_router': nrm((N_MOE, N_EXPERTS), 0.01),
        'moe_w_gate': nrm((N_MOE, N_EXPERTS, D_MODEL, D_FF_EXPERT), D_MODEL ** -0.5),
        'moe_w_up': nrm((N_MOE, N_EXPERTS, D_MODEL, D_FF_EXPERT), D_MODEL ** -0.5),
        'moe_w_down': nrm((N_MOE, N_EXPERTS, D_FF_EXPERT, D_MODEL), D_FF_EXPERT ** -0.5),
    }


def reference(x, c, ctx, c_ctx, w_mod, b_mod, norm_g, w_in, s5_a_re, s5_a_im, s5_log_dt, s5_b_re, s5_b_im,
              s5_c_re, s5_c_im, s5_d, s5_w_glu, s5_b_glu, ret_decay, ret_gn, na_rpb, w_branch, w_out,
              ffn_w_gate, ffn_w_up, ffn_w_down, moe_w_router, moe_b_router, moe_w_gate, moe_w_up, moe_w_down):
    rope_cos, rope_sin = axial_rope(x.shape[1], RET_DIM)
    cond = jnp.concatenate([c, c_ctx[None, :]], axis=0)
    for layer in range(DEPTH):
        last = layer == DEPTH - 1
        mod = jax.nn.silu(cond) @ w_mod[layer] + b_mod[layer]
        sh_a, sc_a, g_a, sh_f, sc_f, g_f = jnp.split(mod[:-1, None, :], 6, axis=-1)
        csh_a, csc_a, cg_a, csh_f, csc_f, cg_f = jnp.split(mod[-1:, None, :], 6, axis=-1)
        h_l = rms_norm(x, norm_g[layer, 0]) * (1 + sc_a) + sh_a
        h_c = rms_norm(ctx, norm_g[layer, 0]) * (1 + csc_a) + csh_a
        y_l, y_c = token_mixers(h_l, h_c, w_in[layer], s5_a_re[layer], s5_a_im[layer], s5_log_dt[layer],
                                s5_b_re[layer], s5_b_im[layer], s5_c_re[layer], s5_c_im[layer], s5_d[layer],
                                s5_w_glu[layer], s5_b_glu[layer], ret_decay[layer], ret_gn[layer],
                                na_rpb[layer], w_branch[layer], w_out[layer], rope_cos, rope_sin, not last)
        x = x + g_a * rms_norm(y_l, norm_g[layer, 1])
        if not last:
            ctx = ctx + cg_a * rms_norm(y_c, norm_g[layer, 1])
        i = layer // 2
        if layer % 2 == 0:
            def ffn(h):
                return swiglu(h, ffn_w_gate[i], ffn_w_up[i], ffn_w_down[i])
        else:
            def ffn(h):
                return moe_swiglu(h, moe_w_router[i], moe_b_router[i], moe_w_gate[i], moe_w_up[i], moe_w_down[i])
        h_l = rms_norm(x, norm_g[layer, 2]) * (1 + sc_f) + sh_f
        x = x + g_f * rms_norm(ffn(h_l), norm_g[layer, 3])
        if not last:
            h_c = rms_norm(ctx, norm_g[layer, 2]) * (1 + csc_f) + csh_f
            ctx = ctx + cg_f * rms_norm(ffn(h_c), norm_g[layer, 3])
    return x
```

```python
import numpy as np
from contextlib import ExitStack
import concourse.bass as bass
import concourse.mybir as mybir
from concourse.bass_utils import run_bass_kernel_spmd

F32 = mybir.dt.float32
BF16 = mybir.dt.bfloat16
I32 = mybir.dt.int32
ALU = mybir.AluOpType
AF = mybir.ActivationFunctionType
AX = mybir.AxisListType

ENGS = ("pe", "act", "dve", "pool", "sp")
NDSEM = 12


class Buf:
    __slots__ = ("name", "lw", "rd", "psum")

    def __init__(self, name="", psum=False):
        self.name = name
        self.psum = psum
        self.lw = None
        self.rd = {}


class Prog:
    def __init__(self, nc):
        self.nc = nc
        self.stack = ExitStack()
        self.ops = {e: [] for e in ENGS}
        self.cnt = {e: 0 for e in ENGS}
        self.seen = {e: {} for e in ENGS}
        self.sems = {}
        for e in ENGS:
            self.sems[e] = self.stack.enter_context(nc.semaphore("s_" + e))
        self.dsem_use = {}
        self.dq_next = {}
        for q in ("sp", "act", "pool"):
            for i in range(NDSEM):
                k = "d_%s%d" % (q, i)
                self.sems[k] = self.stack.enter_context(nc.semaphore(k))
                self.dsem_use[k] = 0
            self.dq_next[q] = 0
        self.nbuf = 0

    def sb(self, name, shape, dt):
        return self.stack.enter_context(self.nc.sbuf_tensor(name, list(shape), dt))

    def ps(self, name, shape, dt=F32):
        return self.stack.enter_context(self.nc.psum_tensor(name, list(shape), dt))

    def buf(self, name=None, psum=None):
        self.nbuf += 1
        name = name or "b%d" % self.nbuf
        if psum is None:
            psum = name.startswith("ps")
        return Buf(name, psum)

    def _deps(self, eng, reads, writes, is_dma):
        w = {}

        def add(t):
            if t is None:
                return
            k, v = t
            if w.get(k, 0) < v:
                w[k] = v
        for b in reads:
            add(b.lw)
            if b.psum:
                for k, v in b.rd.items():
                    if k != eng:
                        add((k, v))
        for b in writes:
            if b.lw is not None:
                if not (eng == "pe" and b.lw[0] == "pe" and not is_dma):
                    add(b.lw)
            for k, v in b.rd.items():
                if k == eng and not is_dma and eng != "pool":
                    continue
                add((k, v))
        seen = self.seen[eng]
        out = []
        for k, v in w.items():
            if seen.get(k, 0) < v:
                seen[k] = v
                out.append((k, v))
        return out

    def _commit(self, ticket, reads, writes):
        for b in writes:
            b.lw = ticket
            b.rd = {}
        for b in reads:
            k, v = ticket
            if b.rd.get(k, 0) < v:
                b.rd[k] = v

    def op(self, eng, fn, reads=(), writes=()):
        waits = self._deps(eng, reads, writes, False)
        self.cnt[eng] += 1
        ticket = (eng, self.cnt[eng])
        self.ops[eng].append((waits, fn, (eng, 1)))
        self._commit(ticket, reads, writes)
        return ticket

    def dma(self, q, fn, reads=(), writes=()):
        i = self.dq_next[q]
        self.dq_next[q] = (i + 1) % NDSEM
        k = "d_%s%d" % (q, i)
        waits = self._deps(q, reads, writes, True)
        prev = self.dsem_use[k]
        if prev > 0 and self.seen[q].get(k, 0) < 16 * prev:
            self.seen[q][k] = 16 * prev
            waits.append((k, 16 * prev))
        self.dsem_use[k] = prev + 1
        ticket = (k, 16 * (prev + 1))
        self.ops[q].append((waits, fn, (k, 16)))
        self._commit(ticket, reads, writes)
        return ticket

    def finish_wait(self, eng, tickets):
        waits = []
        for k, v in tickets:
            if self.seen[eng].get(k, 0) < v:
                self.seen[eng][k] = v
                waits.append((k, v))
        self.ops[eng].append((waits, None, None))

    def emit(self):
        nc = self.nc
        sems = self.sems
        ops = self.ops

        def replay(e, h):
            for waits, fn, inc in ops[e]:
                for k, v in waits:
                    h.wait_ge(sems[k], v)
                if fn is not None:
                    ins = fn(h)
                    ins.then_inc(sems[inc[0]], inc[1])

        with nc.Block() as block:
            @block.sync
            def _(h):
                replay("sp", h)

            @block.scalar
            def _(h):
                replay("act", h)

            @block.vector
            def _(h):
                replay("dve", h)

            @block.gpsimd
            def _(h):
                replay("pool", h)

            @block.tensor
            def _(h):
                replay("pe", h)
        self.stack.close()


def _mm(P, out, lhsT, rhs, start, stop, reads, writes):
    return P.op("pe", lambda h: h.matmul(out, lhsT=lhsT, rhs=rhs, start=start, stop=stop), reads=reads, writes=writes)


def _tr(P, out, in_, ident, reads, writes):
    return P.op("pe", lambda h: h.transpose(out, in_, ident), reads=reads, writes=writes)


def _act(P, out, in_, func, reads, writes, scale=None, bias=None):
    kw = {}
    if scale is not None:
        kw["scale"] = scale
    if bias is not None:
        kw["bias"] = bias
    return P.op("act", lambda h: h.activation(out=out, in_=in_, func=func, **kw), reads=reads, writes=writes)


def _tt(P, eng, out, in0, in1, op, reads, writes):
    return P.op(eng, lambda h: h.tensor_tensor(out=out, in0=in0, in1=in1, op=op), reads=reads, writes=writes)


def _ts(P, eng, out, in0, s1, s2, op0, op1, reads, writes):
    if op1 is None:
        return P.op(eng, lambda h: h.tensor_scalar(out=out, in0=in0, scalar1=s1, scalar2=None, op0=op0), reads=reads, writes=writes)
    return P.op(eng, lambda h: h.tensor_scalar(out=out, in0=in0, scalar1=s1, scalar2=s2, op0=op0, op1=op1), reads=reads, writes=writes)


def _stt(P, out, in0, scalar, in1, op0, op1, reads, writes):
    return P.op("dve", lambda h: h.scalar_tensor_tensor(out=out, in0=in0, scalar=scalar, in1=in1, op0=op0, op1=op1), reads=reads, writes=writes)


def _cp(P, eng, out, in_, reads, writes):
    if eng == "act":
        return P.op("act", lambda h: h.activation(out=out, in_=in_, func=AF.Copy), reads=reads, writes=writes)
    return P.op(eng, lambda h: h.tensor_copy(out=out, in_=in_), reads=reads, writes=writes)


def _ld(P, q, out, in_, writes, reads=()):
    return P.dma(q, lambda h: h.dma_start(out=out, in_=in_), reads=reads, writes=writes)


D = 1024
KC = 8
EPS = 1e-6


def arena_init(P, nbytes=206 * 1024):
    lo, hi = P.nc.bump_sbuf(nbytes)
    P.a_lo, P.a_hi, P.a_cur = lo, hi, lo
    P.a_n = 0


def A(P, shape, dt):
    nb = int(np.prod(shape[1:])) * (4 if dt in (F32, I32) else 2)
    off = (P.a_cur + 31) // 32 * 32
    assert off + nb <= P.a_hi, ("SBUF arena overflow", off + nb - P.a_lo)
    P.a_cur = off + nb
    P.a_n += 1
    return P.nc.alloc_sbuf_tensor_at("t%d" % P.a_n, list(shape), dt, offset=off)


def barrier(P):
    tick = [(e, P.cnt[e]) for e in ENGS if P.cnt[e] > 0]
    tick += [(k, 16 * v) for k, v in P.dsem_use.items() if v > 0]
    for e in ENGS:
        P.finish_wait(e, tick)


def rms_rstd(P, src, srcb, n, sq, sqb, ss_ps, ssb, rstd, rstdb, ones):
    P.op("act", lambda h: h.activation(out=sq[:, :, 0:n], in_=src[:, :, 0:n], func=AF.Square), reads=[srcb], writes=[sqb])
    for k in range(KC):
        P.op("pe", lambda h, k=k: h.matmul(ss_ps[:, 0:n], lhsT=ones[:], rhs=sq[:, k, 0:n], start=(k == 0), stop=(k == KC - 1)),
             reads=[sqb], writes=[ssb])
    P.op("act", lambda h: h.activation(out=rstd[:, 0:n], in_=ss_ps[:, 0:n], func=AF.Ln, scale=1.0 / D, bias=P.eps_t[:, 0:1]), reads=[ssb], writes=[rstdb])
    P.op("act", lambda h: h.activation(out=rstd[:, 0:n], in_=rstd[:, 0:n], func=AF.Exp, scale=-0.5), reads=[rstdb], writes=[rstdb])


def norm_mod(P, src, srcb, n, rstd, rstdb, gm, sh, r, dst, dstb, tmp, tmpb, dst_off=0, dst32=None, dst32b=None):
    for k in range(KC):
        tb = tmpb[k % 2]
        tt = tmp[k % 2]
        P.op("dve", lambda h, k=k, tt=tt: h.tensor_tensor(out=tt[:, 0:n], in0=src[:, k, 0:n], in1=rstd[:, 0:n], op=ALU.mult),
             reads=[srcb, rstdb], writes=[tb])
        P.op("act", lambda h, k=k, tt=tt: h.activation(out=dst[:, k, dst_off:dst_off + n], in_=tt[:, 0:n], func=AF.Identity,
                                                     scale=gm[:, k, r:r + 1], bias=sh[:, k, r:r + 1]),
             reads=[tb, P.modb], writes=[dstb])
        if dst32 is not None:
            P.op("pool", lambda h, k=k, tt=tt: h.tensor_scalar(out=dst32[:, k, 0:n], in0=tt[:, 0:n], scalar1=gm[:, k, r:r + 1],
                                                             scalar2=sh[:, k, r:r + 1], op0=ALU.mult, op1=ALU.add),
                 reads=[tb, P.modb], writes=[dst32b])


def compute_mod(P, dr, which, mod_ps, modpb):
    nc = P.nc
    cs = A(P, [128, KC, 2], F32)
    csb = P.buf()
    P.dma("sp", lambda h: h.dma_start(out=cs[:], in_=dr["condT"][:, :, :]), writes=[csb])
    sig = A(P, [128, KC, 2], F32)
    P.op("act", lambda h: h.activation(out=sig[:], in_=cs[:], func=AF.Sigmoid), reads=[csb], writes=[csb])
    P.op("dve", lambda h: h.tensor_tensor(out=cs[:], in0=cs[:], in1=sig[:], op=ALU.mult), reads=[csb], writes=[csb])
    modT = A(P, [128, 48, 2], F32)
    P.modT = modT
    P.modb = P.buf("mod")
    bm = A(P, [128, 48, 2], F32)
    bmb = P.buf()
    P.dma("sp", lambda h: h.dma_start(out=bm[:], in_=dr["b_modT"][:, :, :]), writes=[bmb])
    ng = A(P, [128, 4, KC, 2], F32)
    P.ng = ng
    P.dma("sp", lambda h: h.dma_start(out=ng[:], in_=dr["norm_gT"][:, :, :, :]), writes=[P.modb])
    mark = P.a_cur
    wm = [A(P, [128, KC, 1024], F32) for _ in range(2)]
    wmb = [P.buf(), P.buf()]
    wsrc = dr["w_mod"].rearrange("(k p) f -> p k f", p=128)
    for i, j in enumerate(which):
        w = wm[i % 2]
        wb = wmb[i % 2]
        for k2 in range(2):
            P.dma("sp", lambda h, w=w, j=j, k2=k2: h.dma_start(out=w[:, 4 * k2:4 * k2 + 4, :], in_=wsrc[:, 4 * k2:4 * k2 + 4, j * 1024:(j + 1) * 1024]), writes=[wb])
        for fc in range(8):
            for k in range(KC):
                P.op("pe", lambda h, w=w, j=j, fc=fc, k=k: h.matmul(mod_ps[:, j * 8 + fc, :], lhsT=w[:, k, fc * 128:(fc + 1) * 128], rhs=cs[:, k, :],
                                                                   start=(k == 0), stop=(k == KC - 1)), reads=[wb, csb], writes=[modpb])
    for j in which:
        P.op("dve", lambda h, j=j: h.tensor_tensor(out=modT[:, j * 8:(j + 1) * 8, :], in0=mod_ps[:, j * 8:(j + 1) * 8, :], in1=bm[:, j * 8:(j + 1) * 8, :], op=ALU.add),
             reads=[modpb, bmb], writes=[P.modb])
    barrier(P)
    P.a_cur = mark


def mod_derived(P, jsc, jg, gi_norm, gi_gate):
    gm = A(P, [128, KC, 2], F32)
    gg = A(P, [128, KC, 2], F32)
    modT, ng = P.modT, P.ng
    P.op("dve", lambda h: h.scalar_tensor_tensor(out=gm[:], in0=modT[:, jsc * 8:(jsc + 1) * 8, :], scalar=1.0, in1=ng[:, gi_norm, :, :],
                                                 op0=ALU.add, op1=ALU.mult), reads=[P.modb], writes=[P.modb])
    if jg is not None:
        P.op("dve", lambda h: h.tensor_tensor(out=gg[:], in0=modT[:, jg * 8:(jg + 1) * 8, :], in1=ng[:, gi_gate, :, :], op=ALU.mult),
             reads=[P.modb], writes=[P.modb])
    return gm, gg


def build_F(blocks, blocksB, n_exp, dff, moe, DBG=False, mode='full'):
    TT = sum(b[1] for b in blocks)
    nc = bass.Bass("TRN2", target_bir_lowering=False)
    dr = {}

    def din(name, shape, dt=F32):
        dr[name] = nc.dram_tensor(name, list(shape), dt, kind="ExternalInput").ap()
    din("xT", [KC, 128, TT])
    din("brT", [KC, 128, TT], BF16)
    din("condT", [128, KC, 2])
    din("w_mod", [D, 6 * D])
    din("b_modT", [128, 48, 2])
    din("norm_gT", [128, 4, KC, 2])
    din("w_in", [D, 6400])
    din("w_br", [KC * 128, D])
    din("w_o", [D, D])
    din("w_glu", [256, 256])
    din("b_gluT", [128, 2])
    if mode == 'full':
        din("w_g", [n_exp, D, dff])
        din("w_u", [n_exp, D, dff])
        din("w_d", [n_exp, dff, D])
    if moe:
        din("w_r", [D, 8])
        din("b_r", [128, 8])
        din("ident", [128, 128])
        din("sel", [8, 8, 128])
    if mode == 'moe_a':
        h2o = nc.dram_tensor("h2o", [KC, 128, TT], BF16, kind="ExternalOutput").ap().rearrange("k p t -> p k t")
        cbo = nc.dram_tensor("cbo", [8, TT], BF16, kind="ExternalOutput").ap()
        mko = nc.dram_tensor("mko", [8, TT], BF16, kind="ExternalOutput").ap()
    xo = nc.dram_tensor("xo", [KC, 128, TT], F32, kind="ExternalOutput").ap()
    xoT = xo.rearrange("k p t -> p k t")
    if DBG: dbg_mod = nc.dram_tensor("dbg_mod", [128, 48, 2], F32, kind="ExternalOutput").ap()
    if DBG: dbg_xm = nc.dram_tensor("dbg_xm", [KC, 128, TT], F32, kind="ExternalOutput").ap().rearrange("k p t -> p k t")
    if DBG: dbg_h = nc.dram_tensor("dbg_h", [KC, 128, TT], BF16, kind="ExternalOutput").ap().rearrange("k p t -> p k t")
    if DBG: dbg_z = nc.dram_tensor("dbg_z", [KC, 128, TT], F32, kind="ExternalOutput").ap().rearrange("k p t -> p k t")
    if DBG: dbg_r = nc.dram_tensor("dbg_r", [128, TT], F32, kind="ExternalOutput").ap()
    if DBG: dbg_sq = nc.dram_tensor("dbg_sq", [KC, 128, TT], BF16, kind="ExternalOutput").ap().rearrange("k p t -> p k t")
    if DBG: dbg_ss = nc.dram_tensor("dbg_ss", [128, TT], F32, kind="ExternalOutput").ap()
    sscp = A(P, [128, 256], F32) if False else None
    if DBG: dbg_y = nc.dram_tensor("dbg_y", [KC, 128, TT], BF16, kind="ExternalOutput").ap().rearrange("k p t -> p k t")
    xT = dr["xT"].rearrange("k p t -> p k t")
    brT = dr["brT"].rearrange("k p t -> p k t")

    P = Prog(nc)
    arena_init(P)
    ps = [P.ps("ps%d" % i, [128, 512], F32) for i in range(8)]
    psb = [P.buf("ps%d" % i) for i in range(8)]
    ones = A(P, [128, 128], BF16)
    onesb = P.buf()
    P.op("dve", lambda h: h.memset(ones[:], 1.0), writes=[onesb])
    P.eps_t = A(P, [128, 1], F32)
    P.op("dve", lambda h: h.memset(P.eps_t[:], EPS), writes=[onesb])

    mod_ps = nc.alloc_psum_tensor
    mod_view = ps[7][:, 0:96].rearrange("p (j r) -> p j r", r=2)
    compute_mod(P, dr, [0, 1, 2, 3, 4, 5], mod_view, psb[7])
    gm_a, gg_a = mod_derived(P, 1, 2, 0, 1)
    gm_f, gg_f = mod_derived(P, 4, 5, 2, 3)
    sh_a = P.modT[:, 0:8, :]
    sh_f = P.modT[:, 24:32, :]
    P.dbgt = []
    if DBG: P.dbgt += [P.dma("sp", lambda h: h.dma_start(out=dbg_mod[:, :, :], in_=P.modT[:]), reads=[P.modb], writes=[P.buf()])]

    h2 = A(P, [128, KC, TT], BF16)
    h2b = P.buf("h2")
    if moe:
        cbT = A(P, [8, TT], BF16)
        cbTb = P.buf("cbT")
        mkT = A(P, [8, TT], BF16)
        mkTb = P.buf("mkT")
        ident = A(P, [128, 128], F32)
        P.dma("sp", lambda h: h.dma_start(out=ident[:], in_=dr["ident"][:, :]), writes=[onesb])
        wr = A(P, [128, KC, 8], F32)
        P.dma("sp", lambda h: h.dma_start(out=wr[:], in_=dr["w_r"].rearrange("(k p) e -> p k e", p=128)), writes=[onesb])
        br_t = A(P, [128, 8], F32)
        P.dma("sp", lambda h: h.dma_start(out=br_t[:], in_=dr["b_r"][:, :]), writes=[onesb])
        sel = A(P, [8, 8, 128], BF16)
        P.dma("pool", lambda h: h.dma_start(out=sel[:], in_=dr["sel"][:, :, :]), writes=[onesb])
    markA = P.a_cur
    wgt = A(P, [128, KC, 4096], BF16)
    wbr = A(P, [128, KC, D], BF16)
    wo = A(P, [128, KC, D], BF16)
    wAb = P.buf("wA")
    w_in_v = dr["w_in"].rearrange("(k p) c -> p k c", p=128)
    for k in range(KC):
        for c4 in range(2):
            P.dma("pool", lambda h, k=k, c4=c4: h.dma_start(out=wgt[:, k, c4 * 2048:(c4 + 1) * 2048], in_=w_in_v[:, k, 2304 + c4 * 2048:2304 + (c4 + 1) * 2048]), writes=[wAb])
    P.dma("pool", lambda h: h.dma_start(out=wbr[:, 0:4, :], in_=dr["w_br"].rearrange("(k p) c -> p k c", p=128)[:, 0:4, :]), writes=[wAb])
    P.dma("pool", lambda h: h.dma_start(out=wbr[:, 4:8, :], in_=dr["w_br"].rearrange("(k p) c -> p k c", p=128)[:, 4:8, :]), writes=[wAb])
    P.dma("pool", lambda h: h.dma_start(out=wo[:, 0:4, :], in_=dr["w_o"].rearrange("(k p) c -> p k c", p=128)[:, 0:4, :]), writes=[wAb])
    P.dma("pool", lambda h: h.dma_start(out=wo[:, 4:8, :], in_=dr["w_o"].rearrange("(k p) c -> p k c", p=128)[:, 4:8, :]), writes=[wAb])

    wglu = A(P, [128, 2, 256], BF16)
    bglu = A(P, [128, 2], F32)
    P.dma("pool", lambda h: h.dma_start(out=wglu[:], in_=dr["w_glu"].rearrange("(k p) c -> p k c", p=128)), writes=[wAb])
    P.dma("sp", lambda h: h.dma_start(out=bglu[:], in_=dr["b_gluT"][:, :]), writes=[wAb])
    glu_t = A(P, [128, 2, 256], BF16)
    glub = P.buf("glu")
    sgl = A(P, [128, 256], F32)
    sglb = P.buf("sgl")
    xb = [A(P, [128, KC, 256], F32) for _ in range(2)]
    xbb = [P.buf(), P.buf()]
    brb_t = [A(P, [128, KC, 256], BF16) for _ in range(2)]
    brbb = [P.buf(), P.buf()]
    sq = A(P, [128, KC, 256], BF16)
    sqb = P.buf()
    rstd = A(P, [128, 256], F32)
    rstdb = P.buf()
    tmp = [A(P, [128, 256], F32) for _ in range(2)]
    tmpb = [P.buf(), P.buf()]
    hb = A(P, [128, KC, 256], BF16)
    hbb = P.buf()
    yb = A(P, [128, KC, 256], BF16)
    ybb = P.buf()
    zb = A(P, [128, KC, 256], F32)
    zbb = P.buf()
    sg = [A(P, [128, 256], F32) for _ in range(2)]
    sgb = [P.buf(), P.buf()]
    tt2 = [A(P, [128, 256], F32) for _ in range(2)]
    tt2b = [P.buf(), P.buf()]
    accA = [A(P, [128, 256], F32) for _ in range(2)]
    accAb = [P.buf(), P.buf()]
    xob = P.buf("xo")
    h2fb = P.buf("h2f")
    if moe:
        h2f = A(P, [128, KC, 256], F32)
        lg = A(P, [128, 8], F32)
        mx8 = A(P, [128, 8], F32)
        msk = A(P, [128, 8], F32)
        ex = A(P, [128, 8], F32)
        den = A(P, [128, 1], F32)
        nmx = A(P, [128, 1], F32)
        rb = P.buf("router")
    cnt = 0
    P.sscp = A(P, [128, 256], F32)
    P.sscpb = P.buf()
    def _ldA(bi_):
        t0_, n_, _r = blocks[bi_]
        xx, xxb = xb[bi_ % 2], xbb[bi_ % 2]
        bb_, bbb = brb_t[bi_ % 2], brbb[bi_ % 2]
        P.dma("sp", lambda h: h.dma_start(out=xx[:, 0:4, 0:n_], in_=xT[:, 0:4, t0_:t0_ + n_]), writes=[xxb])
        P.dma("sp", lambda h: h.dma_start(out=xx[:, 4:8, 0:n_], in_=xT[:, 4:8, t0_:t0_ + n_]), writes=[xxb])
        P.dma("sp", lambda h: h.dma_start(out=bb_[:, :, 0:n_], in_=brT[:, :, t0_:t0_ + n_]), writes=[bbb])
    _ldA(0)
    for bi, (t0, n, r) in enumerate(blocks):
        x_t, x_b = xb[bi % 2], xbb[bi % 2]
        b_t, b_b = brb_t[bi % 2], brbb[bi % 2]
        if bi + 1 < len(blocks):
            _ldA(bi + 1)
        rms_rstd(P, x_t, x_b, n, sq, sqb, ps[6], psb[6], rstd, rstdb, ones)
        norm_mod(P, x_t, x_b, n, rstd, rstdb, gm_a, sh_a, r, hb, hbb, tmp, tmpb)
        for oc in range(2):
            for kc in range(2):
                P.op("pe", lambda h, oc=oc, kc=kc, n=n, b_t=b_t: h.matmul(ps[6][:, 0:n], lhsT=wglu[:, kc, oc * 128:(oc + 1) * 128], rhs=b_t[:, 2 + kc, 0:n],
                                                                      start=(kc == 0), stop=(kc == 1)), reads=[wAb, b_b], writes=[psb[6]])
            P.op("act", lambda h, oc=oc, n=n: h.activation(out=sgl[:, 0:n], in_=ps[6][:, 0:n], func=AF.Sigmoid, bias=bglu[:, oc:oc + 1], scale=1.0), reads=[psb[6], wAb], writes=[sglb])
            P.op("dve", lambda h, oc=oc, n=n, b_t=b_t: h.tensor_tensor(out=glu_t[:, oc, 0:n], in0=sgl[:, 0:n], in1=b_t[:, 2 + oc, 0:n], op=ALU.mult), reads=[sglb, b_b], writes=[glub])
        for fc in range(8):
            ac, acb = accA[fc % 2], accAb[fc % 2]
            for b in range(4):
                gi = cnt % 2
                cnt += 1
                gps, gpb = ps[gi], psb[gi]
                pps, ppb = ps[2 + gi], psb[2 + gi]
                for k in range(KC):
                    P.op("pe", lambda h, gps=gps, k=k, b=b, fc=fc, n=n: h.matmul(gps[:, 0:n], lhsT=wgt[:, k, b * 1024 + fc * 128:b * 1024 + (fc + 1) * 128], rhs=hb[:, k, 0:n],
                                                                                 start=(k == 0), stop=(k == KC - 1)), reads=[wAb, hbb], writes=[gpb])
                for hh in range(2):
                    rhs_ap = glu_t[:, hh, 0:n] if b == 1 else b_t[:, 2 * b + hh, 0:n]
                    P.op("pe", lambda h, pps=pps, hh=hh, b=b, fc=fc, n=n, rhs_ap=rhs_ap: h.matmul(pps[:, 0:n], lhsT=wbr[:, 2 * b + hh, fc * 128:(fc + 1) * 128], rhs=rhs_ap,
                                                                                          start=(hh == 0), stop=(hh == 1)), reads=[wAb, b_b, glub], writes=[ppb])
                s_t, s_b = sg[gi], sgb[gi]
                P.op("act", lambda h, s_t=s_t, gps=gps, n=n: h.activation(out=s_t[:, 0:n], in_=gps[:, 0:n], func=AF.Sigmoid), reads=[gpb], writes=[s_b])
                if b == 0:
                    P.op("dve", lambda h, ac=ac, s_t=s_t, pps=pps, n=n: h.tensor_tensor(out=ac[:, 0:n], in0=s_t[:, 0:n], in1=pps[:, 0:n], op=ALU.mult),
                         reads=[s_b, ppb], writes=[acb])
                else:
                    t_t, t_b = tt2[gi], tt2b[gi]
                    P.op("dve", lambda h, t_t=t_t, s_t=s_t, pps=pps, n=n: h.tensor_tensor(out=t_t[:, 0:n], in0=s_t[:, 0:n], in1=pps[:, 0:n], op=ALU.mult),
                         reads=[s_b, ppb], writes=[t_b])
                    if b < 3:
                        P.op("pool", lambda h, ac=ac, t_t=t_t, n=n: h.tensor_tensor(out=ac[:, 0:n], in0=ac[:, 0:n], in1=t_t[:, 0:n], op=ALU.add),
                             reads=[acb, t_b], writes=[acb])
                    else:
                        P.op("pool", lambda h, ac=ac, t_t=t_t, n=n, fc=fc: h.tensor_tensor(out=yb[:, fc, 0:n], in0=ac[:, 0:n], in1=t_t[:, 0:n], op=ALU.add),
                             reads=[acb, t_b], writes=[ybb])
        for fc in range(8):
            zi = 4 + fc % 2
            for k in range(KC):
                P.op("pe", lambda h, zi=zi, k=k, fc=fc, n=n: h.matmul(ps[zi][:, 0:n], lhsT=wo[:, k, fc * 128:(fc + 1) * 128], rhs=yb[:, k, 0:n], start=(k == 0), stop=(k == KC - 1)),
                     reads=[wAb, ybb], writes=[psb[zi]])
            P.op("act", lambda h, zi=zi, fc=fc, n=n: h.activation(out=zb[:, fc, 0:n], in_=ps[zi][:, 0:n], func=AF.Copy), reads=[psb[zi]], writes=[zbb])
        rms_rstd(P, zb, zbb, n, sq, sqb, ps[6], psb[6], rstd, rstdb, ones)
        if DBG: P.dbgt.append(P.dma("sp", lambda h, t0=t0, n=n: h.dma_start(out=dbg_z[:, :, t0:t0 + n], in_=zb[:, :, 0:n]), reads=[zbb], writes=[P.buf()]))
        if DBG: P.dbgt.append(P.dma("sp", lambda h, t0=t0, n=n: h.dma_start(out=dbg_r[:, t0:t0 + n], in_=rstd[:, 0:n]), reads=[rstdb], writes=[P.buf()]))
        if DBG: P.dbgt.append(P.dma("sp", lambda h, t0=t0, n=n: h.dma_start(out=dbg_sq[:, :, t0:t0 + n], in_=sq[:, :, 0:n]), reads=[sqb], writes=[P.buf()]))
        if DBG: P.op("dve", lambda h, n=n: h.tensor_copy(out=P.sscp[:, 0:n], in_=ps[6][:, 0:n]), reads=[psb[6]], writes=[P.sscpb])
        if DBG: P.dbgt.append(P.dma("sp", lambda h, t0=t0, n=n: h.dma_start(out=dbg_ss[:, t0:t0 + n], in_=P.sscp[:, 0:n]), reads=[P.sscpb], writes=[P.buf()]))
        for k in range(KC):
            tb_, tt_ = tmpb[k % 2], tmp[k % 2]
            P.op("dve", lambda h, k=k, tt_=tt_, n=n: h.tensor_tensor(out=tt_[:, 0:n], in0=zb[:, k, 0:n], in1=rstd[:, 0:n], op=ALU.mult), reads=[zbb, rstdb], writes=[tb_])
            P.op("dve", lambda h, k=k, tt_=tt_, n=n, x_t=x_t, r=r: h.scalar_tensor_tensor(out=x_t[:, k, 0:n], in0=tt_[:, 0:n], scalar=gg_a[:, k, r:r + 1], in1=x_t[:, k, 0:n],
                                                                                    op0=ALU.mult, op1=ALU.add), reads=[tb_, P.modb, x_b], writes=[x_b])
        P.dma("sp", lambda h, x_t=x_t, t0=t0, n=n: h.dma_start(out=xoT[:, :, t0:t0 + n], in_=x_t[:, :, 0:n]), reads=[x_b], writes=[xob])
        if DBG: P.dbgt.append(P.dma("sp", lambda h, x_t=x_t, t0=t0, n=n: h.dma_start(out=dbg_xm[:, :, t0:t0 + n], in_=x_t[:, :, 0:n]), reads=[x_b], writes=[P.buf()]))
        if DBG: P.dbgt.append(P.dma("sp", lambda h, t0=t0, n=n: h.dma_start(out=dbg_h[:, :, t0:t0 + n], in_=hb[:, :, 0:n]), reads=[hbb], writes=[P.buf()]))
        if DBG: P.dbgt.append(P.dma("sp", lambda h, t0=t0, n=n: h.dma_start(out=dbg_y[:, :, t0:t0 + n], in_=yb[:, :, 0:n]), reads=[ybb], writes=[P.buf()]))
        rms_rstd(P, x_t, x_b, n, sq, sqb, ps[6], psb[6], rstd, rstdb, ones)
        norm_mod(P, x_t, x_b, n, rstd, rstdb, gm_f, sh_f, r, h2, h2b, tmp, tmpb, dst_off=t0, dst32=(h2f if moe else None), dst32b=h2fb)
        if moe:
            for tt in range(n // 128):
                for k in range(KC):
                    P.op("pe", lambda h, k=k, tt=tt: h.matmul(ps[7][:, 0:8], lhsT=h2f[:, k, tt * 128:(tt + 1) * 128], rhs=wr[:, k, :], start=(k == 0), stop=(k == KC - 1)),
                         reads=[h2fb, onesb], writes=[psb[7]])
                P.op("dve", lambda h: h.tensor_tensor(out=lg[:], in0=ps[7][:, 0:8], in1=br_t[:], op=ALU.add), reads=[psb[7], onesb], writes=[rb])
                P.op("dve", lambda h: h.max(out=mx8[:], in_=lg[:]), reads=[rb], writes=[rb])
                P.op("dve", lambda h: h.tensor_scalar(out=msk[:], in0=lg[:], scalar1=mx8[:, 1:2], scalar2=None, op0=ALU.is_ge), reads=[rb], writes=[rb])
                P.op("dve", lambda h: h.tensor_scalar(out=nmx[:], in0=mx8[:, 0:1], scalar1=-1.0, scalar2=None, op0=ALU.mult), reads=[rb], writes=[rb])
                P.op("act", lambda h: h.activation(out=ex[:], in_=lg[:], func=AF.Exp, bias=nmx[:, 0:1], scale=1.0), reads=[rb], writes=[rb])
                P.op("dve", lambda h: h.tensor_tensor(out=ex[:], in0=ex[:], in1=msk[:], op=ALU.mult), reads=[rb], writes=[rb])
                P.op("dve", lambda h: h.reduce_sum(out=den[:], in_=ex[:], axis=AX.X), reads=[rb], writes=[rb])
                P.op("dve", lambda h: h.reciprocal(out=den[:], in_=den[:]), reads=[rb], writes=[rb])
                P.op("dve", lambda h: h.tensor_scalar(out=ex[:], in0=ex[:], scalar1=den[:, 0:1], scalar2=None, op0=ALU.mult), reads=[rb], writes=[rb])
                P.op("pe", lambda h: h.transpose(ps[7][0:8, 128:256], ex[:], ident[:]), reads=[rb, onesb], writes=[psb[7]])
                P.op("act", lambda h, t0=t0, tt=tt: h.activation(out=cbT[:, t0 + tt * 128:t0 + (tt + 1) * 128], in_=ps[7][0:8, 128:256], func=AF.Copy), reads=[psb[7]], writes=[cbTb])
                if mode == 'moe_a':
                    P.op("pe", lambda h: h.transpose(ps[7][0:8, 256:384], msk[:], ident[:]), reads=[rb, onesb], writes=[psb[7]])
                    P.op("act", lambda h, t0=t0, tt=tt: h.activation(out=mkT[:, t0 + tt * 128:t0 + (tt + 1) * 128], in_=ps[7][0:8, 256:384], func=AF.Copy), reads=[psb[7]], writes=[mkTb])
    barrier(P)
    P.a_cur = markA
    if mode == 'moe_a':
        fin = [P.dma("sp", lambda h: h.dma_start(out=h2o[:, :, :], in_=h2[:, :, :]), reads=[h2b], writes=[P.buf()]),
               P.dma("sp", lambda h: h.dma_start(out=cbo[:, :], in_=cbT[:, :]), reads=[cbTb], writes=[P.buf()]),
               P.dma("sp", lambda h: h.dma_start(out=mko[:, :], in_=mkT[:, :]), reads=[mkTb], writes=[P.buf()])]
        barrier(P)
        P.finish_wait("sp", fin + P.dbgt)
        P.emit()
        return nc
    blocks = blocksB
    acc = A(P, [128, KC, TT], F32)
    accb = [P.buf() for _ in blocks]
    markB = P.a_cur
    NSL = 4
    wg_s = [A(P, [128, KC, NSL * 128], BF16) for _ in range(2)]
    wu_s = [A(P, [128, KC, NSL * 128], BF16) for _ in range(2)]
    wd_s = [A(P, [128, NSL, D], BF16) for _ in range(2)]
    wsb = [P.buf(), P.buf()]
    hid = [A(P, [128, NSL, 512], BF16) for _ in range(2)]
    hidb = [P.buf(), P.buf()]
    ssb_t = [A(P, [128, 512], F32) for _ in range(2)]
    ssbb = [P.buf(), P.buf()]
    cbe = A(P, [128, 512], BF16)
    cbeb = P.buf()
    ntile = dff // 128
    slices = [(s0, min(NSL, ntile - s0)) for s0 in range(0, ntile, NSL)]
    si = 0
    hcnt = 0
    gcnt = 0
    work = [(e, s0, ns) for e in range(n_exp) for (s0, ns) in slices]

    def _ldW(widx):
        e_, s0_, ns_ = work[widx]
        wi_ = widx % 2
        wgv_ = dr["w_g"][e_].rearrange("(k p) f -> p k f", p=128)
        wuv_ = dr["w_u"][e_].rearrange("(k p) f -> p k f", p=128)
        wdv_ = dr["w_d"][e_].rearrange("(j p) c -> p j c", p=128)
        for k2 in range(2):
            P.dma("pool", lambda h, k2=k2: h.dma_start(out=wg_s[wi_][:, 4 * k2:4 * k2 + 4, 0:ns_ * 128], in_=wgv_[:, 4 * k2:4 * k2 + 4, s0_ * 128:(s0_ + ns_) * 128]), writes=[wsb[wi_]])
            P.dma("pool", lambda h, k2=k2: h.dma_start(out=wu_s[wi_][:, 4 * k2:4 * k2 + 4, 0:ns_ * 128], in_=wuv_[:, 4 * k2:4 * k2 + 4, s0_ * 128:(s0_ + ns_) * 128]), writes=[wsb[wi_]])
        for j in range(ns_):
            P.dma("pool", lambda h, j=j: h.dma_start(out=wd_s[wi_][:, j, :], in_=wdv_[:, s0_ + j, :]), writes=[wsb[wi_]])
    _ldW(0)
    for widx, (e, s0, ns) in enumerate(work):
        if True:
            wi = widx % 2
            if widx + 1 < len(work):
                _ldW(widx + 1)
            for bi, (t0, n, r) in enumerate(blocks):
                hi = hcnt % 2
                hcnt += 1
                if moe:
                    P.op("pe", lambda h, e=e, t0=t0, n=n: h.matmul(ps[7][:, 0:n], lhsT=sel[:, e, :], rhs=cbT[:, t0:t0 + n], start=True, stop=True), reads=[cbTb, onesb], writes=[psb[7]])
                    P.op("act", lambda h, n=n: h.activation(out=cbe[:, 0:n], in_=ps[7][:, 0:n], func=AF.Copy), reads=[psb[7]], writes=[cbeb])
                for j in range(ns):
                    gi = gcnt % 2
                    gcnt += 1
                    for k in range(KC):
                        P.op("pe", lambda h, gi=gi, wi=wi, j=j, k=k, t0=t0, n=n: h.matmul(ps[gi][:, 0:n], lhsT=wg_s[wi][:, k, j * 128:(j + 1) * 128], rhs=h2[:, k, t0:t0 + n], start=(k == 0), stop=(k == KC - 1)),
                             reads=[wsb[wi], h2b], writes=[psb[gi]])
                    for k in range(KC):
                        P.op("pe", lambda h, gi=gi, wi=wi, j=j, k=k, t0=t0, n=n: h.matmul(ps[2 + gi][:, 0:n], lhsT=wu_s[wi][:, k, j * 128:(j + 1) * 128], rhs=h2[:, k, t0:t0 + n], start=(k == 0), stop=(k == KC - 1)),
                             reads=[wsb[wi], h2b], writes=[psb[2 + gi]])
                    P.op("act", lambda h, gi=gi, n=n: h.activation(out=ssb_t[gi][:, 0:n], in_=ps[gi][:, 0:n], func=AF.Silu), reads=[psb[gi]], writes=[ssbb[gi]])
                    if moe:
                        P.op("dve", lambda h, gi=gi, n=n: h.tensor_tensor(out=ssb_t[gi][:, 0:n], in0=ssb_t[gi][:, 0:n], in1=ps[2 + gi][:, 0:n], op=ALU.mult),
                             reads=[ssbb[gi], psb[2 + gi]], writes=[ssbb[gi]])
                        P.op("pool", lambda h, gi=gi, hi=hi, j=j, n=n: h.tensor_tensor(out=hid[hi][:, j, 0:n], in0=ssb_t[gi][:, 0:n], in1=cbe[:, 0:n], op=ALU.mult),
                             reads=[ssbb[gi], cbeb], writes=[hidb[hi]])
                    else:
                        P.op("dve", lambda h, gi=gi, hi=hi, j=j, n=n: h.tensor_tensor(out=hid[hi][:, j, 0:n], in0=ssb_t[gi][:, 0:n], in1=ps[2 + gi][:, 0:n], op=ALU.mult),
                             reads=[ssbb[gi], psb[2 + gi]], writes=[hidb[hi]])
                first = (e == 0 and s0 == 0)
                for fc in range(8):
                    oi = 4 + fc % 2
                    for j in range(ns):
                        P.op("pe", lambda h, oi=oi, wi=wi, j=j, fc=fc, hi=hi, n=n, ns=ns: h.matmul(ps[oi][:, 0:n], lhsT=wd_s[wi][:, j, fc * 128:(fc + 1) * 128], rhs=hid[hi][:, j, 0:n], start=(j == 0), stop=(j == ns - 1)),
                             reads=[wsb[wi], hidb[hi]], writes=[psb[oi]])
                    if first:
                        P.op("act", lambda h, oi=oi, fc=fc, t0=t0, n=n: h.activation(out=acc[:, fc, t0:t0 + n], in_=ps[oi][:, 0:n], func=AF.Copy), reads=[psb[oi]], writes=[accb[bi]])
                    else:
                        P.op("dve", lambda h, oi=oi, fc=fc, t0=t0, n=n: h.tensor_tensor(out=acc[:, fc, t0:t0 + n], in0=acc[:, fc, t0:t0 + n], in1=ps[oi][:, 0:n], op=ALU.add),
                             reads=[psb[oi], accb[bi]], writes=[accb[bi]])
    barrier(P)
    P.a_cur = markB
    xm = [A(P, [128, KC, 512], F32) for _ in range(2)]
    xmb = [P.buf(), P.buf()]
    sqF = A(P, [128, KC, 512], BF16)
    rstdF = A(P, [128, 512], F32)
    tmpF = [A(P, [128, 512], F32) for _ in range(2)]
    outs = []
    for bi, (t0, n, r) in enumerate(blocks):
        x_t, x_b = xm[bi % 2], xmb[bi % 2]
        P.dma("sp", lambda h, x_t=x_t, t0=t0, n=n: h.dma_start(out=x_t[:, :, 0:n], in_=xoT[:, :, t0:t0 + n]), reads=[xob], writes=[x_b])
        accv = acc[:, :, t0:t0 + n]
        P.op("act", lambda h, accv=accv, n=n: h.activation(out=sqF[:, :, 0:n], in_=accv, func=AF.Square), reads=[accb[bi]], writes=[sqb])
        for k in range(KC):
            P.op("pe", lambda h, k=k, n=n: h.matmul(ps[6][:, 0:n], lhsT=ones[:], rhs=sqF[:, k, 0:n], start=(k == 0), stop=(k == KC - 1)), reads=[sqb, onesb], writes=[psb[6]])
        P.op("act", lambda h, n=n: h.activation(out=rstdF[:, 0:n], in_=ps[6][:, 0:n], func=AF.Ln, scale=1.0 / D, bias=P.eps_t[:, 0:1]), reads=[psb[6]], writes=[rstdb])
        P.op("act", lambda h, n=n: h.activation(out=rstdF[:, 0:n], in_=rstdF[:, 0:n], func=AF.Exp, scale=-0.5), reads=[rstdb], writes=[rstdb])
        for k in range(KC):
            tb_, tt_ = tmpb[k % 2], tmpF[k % 2]
            P.op("dve", lambda h, k=k, tt_=tt_, n=n, t0=t0: h.tensor_tensor(out=tt_[:, 0:n], in0=acc[:, k, t0:t0 + n], in1=rstdF[:, 0:n], op=ALU.mult), reads=[accb[bi], rstdb], writes=[tb_])
            P.op("dve", lambda h, k=k, tt_=tt_, n=n, x_t=x_t, r=r: h.scalar_tensor_tensor(out=x_t[:, k, 0:n], in0=tt_[:, 0:n], scalar=gg_f[:, k, r:r + 1], in1=x_t[:, k, 0:n],
                                                                                    op0=ALU.mult, op1=ALU.add), reads=[tb_, P.modb, x_b], writes=[x_b])
        outs.append(P.dma("sp", lambda h, x_t=x_t, t0=t0, n=n: h.dma_start(out=xoT[:, :, t0:t0 + n], in_=x_t[:, :, 0:n]), reads=[x_b], writes=[xob]))
    P.finish_wait("sp", outs + P.dbgt)
    P.emit()
    return nc


def build_E(groups=(4,) * 8, dff=3584):
    nc = bass.Bass("TRN2", target_bir_lowering=False)
    ngrp = len(groups)
    gtok = 512 * max(groups)
    NT = 512 * sum(groups)
    goff = [512 * sum(groups[:g]) for g in range(ngrp)]
    h2d = nc.dram_tensor("h2", [KC, 128, NT], BF16, kind="ExternalInput").ap().rearrange("k p t -> p k t")
    cbd = nc.dram_tensor("cbe", [128, NT], BF16, kind="ExternalInput").ap()
    wgd = nc.dram_tensor("w_g", [D, dff], F32, kind="ExternalInput").ap().rearrange("(k p) f -> p k f", p=128)
    wud = nc.dram_tensor("w_u", [D, dff], F32, kind="ExternalInput").ap().rearrange("(k p) f -> p k f", p=128)
    wdd = nc.dram_tensor("w_d", [dff, D], F32, kind="ExternalInput").ap().rearrange("(j p) c -> p j c", p=128)
    ye = nc.dram_tensor("ye", [KC, 128, NT], F32, kind="ExternalOutput").ap().rearrange("k p t -> p k t")
    P = Prog(nc)
    arena_init(P)
    ps = [P.ps("ps%d" % i, [128, 512], F32) for i in range(8)]
    psb = [P.buf("ps%d" % i) for i in range(8)]
    h2g = [A(P, [128, KC, gtok], BF16) for _ in range(2)]
    h2gb = [P.buf(), P.buf()]
    cbg = [A(P, [128, gtok], BF16) for _ in range(2)]
    acc = A(P, [128, KC, gtok], F32)
    NSL = 4
    wg_s = [A(P, [128, KC, NSL * 128], BF16) for _ in range(2)]
    wu_s = [A(P, [128, KC, NSL * 128], BF16) for _ in range(2)]
    wd_s = [A(P, [128, NSL, D], BF16) for _ in range(2)]
    wsb = [P.buf(), P.buf()]
    hid = [A(P, [128, NSL, 512], BF16) for _ in range(2)]
    hidb = [P.buf(), P.buf()]
    ssb_t = [A(P, [128, 512], F32) for _ in range(2)]
    ssbb = [P.buf(), P.buf()]
    ntile = dff // 128
    slices = [(s0, min(NSL, ntile - s0)) for s0 in range(0, ntile, NSL)]
    accb = [P.buf() for _ in range(max(groups))]
    si = hcnt = gcnt = 0
    outs = []
    work = [(g, sidx, s0, ns) for g in range(ngrp) for sidx, (s0, ns) in enumerate(slices)]

    def _ldG(g_):
        hg_, hgb_ = h2g[g_ % 2], h2gb[g_ % 2]
        gn_ = 512 * groups[g_]
        for k2 in range(2):
            _ldF(P, "sp", hg_[:, 4 * k2:4 * k2 + 4, 0:gn_], h2d[:, 4 * k2:4 * k2 + 4, goff[g_]:goff[g_] + gn_], [hgb_])
        _ldF(P, "sp", cbg[g_ % 2][:, 0:gn_], cbd[:, goff[g_]:goff[g_] + gn_], [hgb_])

    def _ldW(widx):
        _g, _sidx, s0_, ns_ = work[widx]
        wi_ = widx % 2
        for k2 in range(2):
            _ldF(P, "pool", wg_s[wi_][:, 4 * k2:4 * k2 + 4, 0:ns_ * 128], wgd[:, 4 * k2:4 * k2 + 4, s0_ * 128:(s0_ + ns_) * 128], [wsb[wi_]])
            _ldF(P, "pool", wu_s[wi_][:, 4 * k2:4 * k2 + 4, 0:ns_ * 128], wud[:, 4 * k2:4 * k2 + 4, s0_ * 128:(s0_ + ns_) * 128], [wsb[wi_]])
        for j in range(ns_):
            _ldF(P, "pool", wd_s[wi_][:, j, :], wdd[:, s0_ + j, :], [wsb[wi_]])
    _ldG(0)
    _ldW(0)
    for widx, (g, sidx, s0, ns) in enumerate(work):
        hg, hgb = h2g[g % 2], h2gb[g % 2]
        cg = cbg[g % 2]
        nblk = groups[g]
        if sidx == 0 and g + 1 < ngrp:
            _ldG(g + 1)
        if True:
            wi = widx % 2
            if widx + 1 < len(work):
                _ldW(widx + 1)
            for bi in range(nblk):
                t0, n = bi * 512, 512
                hi = hcnt % 2
                hcnt += 1
                for j in range(ns):
                    gi = gcnt % 2
                    gcnt += 1
                    for k in range(KC):
                        _mmF(P, ps[gi][:, 0:n], wg_s[wi][:, k, j * 128:(j + 1) * 128], hg[:, k, t0:t0 + n], k == 0, k == KC - 1, [wsb[wi], hgb], [psb[gi]])
                    for k in range(KC):
                        _mmF(P, ps[2 + gi][:, 0:n], wu_s[wi][:, k, j * 128:(j + 1) * 128], hg[:, k, t0:t0 + n], k == 0, k == KC - 1, [wsb[wi], hgb], [psb[2 + gi]])
                    st_, stb_ = ssb_t[gi], ssbb[gi]
                    P.op("act", lambda h, st_=st_, gi=gi, n=n: h.activation(out=st_[:, 0:n], in_=ps[gi][:, 0:n], func=AF.Silu), reads=[psb[gi]], writes=[stb_])
                    P.op("dve", lambda h, st_=st_, gi=gi, n=n: h.tensor_tensor(out=st_[:, 0:n], in0=st_[:, 0:n], in1=ps[2 + gi][:, 0:n], op=ALU.mult), reads=[stb_, psb[2 + gi]], writes=[stb_])
                    hd = hid[hi]
                    P.op("pool", lambda h, st_=st_, hd=hd, j=j, n=n, cg=cg, t0=t0: h.tensor_tensor(out=hd[:, j, 0:n], in0=st_[:, 0:n], in1=cg[:, t0:t0 + n], op=ALU.mult), reads=[stb_, hgb], writes=[hidb[hi]])
                for fc in range(8):
                    oi = 4 + fc % 2
                    for j in range(ns):
                        _mmF(P, ps[oi][:, 0:n], wd_s[wi][:, j, fc * 128:(fc + 1) * 128], hid[hi][:, j, 0:n], j == 0, j == ns - 1, [wsb[wi], hidb[hi]], [psb[oi]])
                    av = acc[:, fc, t0:t0 + n]
                    pv = ps[oi][:, 0:n]
                    if sidx == 0:
                        P.op("act", lambda h, av=av, pv=pv: h.activation(out=av, in_=pv, func=AF.Copy), reads=[psb[oi]], writes=[accb[bi]])
                    else:
                        P.op("dve", lambda h, av=av, pv=pv: h.tensor_tensor(out=av, in0=av, in1=pv, op=ALU.add), reads=[psb[oi], accb[bi]], writes=[accb[bi]])
        if sidx == len(slices) - 1:
            for bi in range(nblk):
                t0 = bi * 512
                outs.append(_ldF(P, "sp", ye[:, :, goff[g] + t0:goff[g] + t0 + 512], acc[:, :, t0:t0 + 512], [P.buf()], reads=[accb[bi]]))
    P.finish_wait("sp", outs)
    P.emit()
    return nc


def _ldF(P, q, out, in_, writes, reads=()):
    return P.dma(q, lambda h: h.dma_start(out=out, in_=in_), reads=reads, writes=writes)


def _mmF(P, out, lhsT, rhs, start, stop, reads, writes):
    return P.op("pe", lambda h: h.matmul(out, lhsT=lhsT, rhs=rhs, start=start, stop=stop), reads=reads, writes=writes)


def build_Fc(TT=2048, nexp=8):
    nc = bass.Bass("TRN2", target_bir_lowering=False)
    dr = {}

    def din(name, shape, dt=F32):
        dr[name] = nc.dram_tensor(name, list(shape), dt, kind="ExternalInput").ap()
    din("xm", [KC, 128, TT])
    din("yp", [nexp, KC, 128, TT])
    din("condT", [128, KC, 2])
    din("w_mod", [D, 6 * D])
    din("b_modT", [128, 48, 2])
    din("norm_gT", [128, 4, KC, 2])
    xo = nc.dram_tensor("xo", [KC, 128, TT], F32, kind="ExternalOutput").ap().rearrange("k p t -> p k t")
    xm = dr["xm"].rearrange("k p t -> p k t")
    P = Prog(nc)
    arena_init(P)
    ps = [P.ps("ps%d" % i, [128, 512], F32) for i in range(8)]
    psb = [P.buf("ps%d" % i) for i in range(8)]
    ones = A(P, [128, 128], BF16)
    onesb = P.buf()
    P.op("dve", lambda h: h.memset(ones[:], 1.0), writes=[onesb])
    P.eps_t = A(P, [128, 1], F32)
    P.op("dve", lambda h: h.memset(P.eps_t[:], EPS), writes=[onesb])
    mod_view = ps[7][:, 0:96].rearrange("p (j r) -> p j r", r=2)
    compute_mod(P, dr, [5], mod_view, psb[7])
    _, gg_f = mod_derived(P, 4, 5, 2, 3)
    acc = [A(P, [128, KC, 512], F32) for _ in range(2)]
    accb = [P.buf(), P.buf()]
    part = [A(P, [128, KC, 512], F32) for _ in range(3)]
    partb = [P.buf() for _ in range(3)]
    xt = [A(P, [128, KC, 512], F32) for _ in range(2)]
    xtb = [P.buf(), P.buf()]
    sq = A(P, [128, KC, 512], BF16)
    sqb = P.buf()
    rstd = A(P, [128, 512], F32)
    rstdb = P.buf()
    tmp = [A(P, [128, 512], F32) for _ in range(2)]
    tmpb = [P.buf(), P.buf()]
    outs = []
    pc = 0
    for bi in range(TT // 512):
        t0, n = bi * 512, 512
        a_t, a_b = acc[bi % 2], accb[bi % 2]
        x_t, x_b = xt[bi % 2], xtb[bi % 2]
        _ldF(P, "sp", x_t[:, :, :], xm[:, :, t0:t0 + n], [x_b])
        _ldF(P, "sp", a_t[:, :, :], dr["yp"][0].rearrange("k p t -> p k t")[:, :, t0:t0 + n], [a_b])
        for e in range(1, nexp):
            p_t, p_b = part[pc % 3], partb[pc % 3]
            pc += 1
            _ldF(P, "act" if e % 2 else "sp", p_t[:, :, :], dr["yp"][e].rearrange("k p t -> p k t")[:, :, t0:t0 + n], [p_b])
            eng = "dve" if e % 2 else "pool"
            P.op(eng, lambda h, a_t=a_t, p_t=p_t: h.tensor_tensor(out=a_t[:, :, :], in0=a_t[:, :, :], in1=p_t[:, :, :], op=ALU.add), reads=[a_b, p_b], writes=[a_b])
        rms_rstd(P, a_t, a_b, n, sq, sqb, ps[6], psb[6], rstd, rstdb, ones)
        for k in range(KC):
            tb_, tt_ = tmpb[k % 2], tmp[k % 2]
            P.op("dve", lambda h, k=k, tt_=tt_, a_t=a_t: h.tensor_tensor(out=tt_[:, :], in0=a_t[:, k, :], in1=rstd[:, :], op=ALU.mult), reads=[a_b, rstdb], writes=[tb_])
            P.op("dve", lambda h, k=k, tt_=tt_, x_t=x_t: h.scalar_tensor_tensor(out=x_t[:, k, :], in0=tt_[:, :], scalar=gg_f[:, k, 0:1], in1=x_t[:, k, :], op0=ALU.mult, op1=ALU.add),
                 reads=[tb_, P.modb, x_b], writes=[x_b])
        outs.append(_ldF(P, "sp", xo[:, :, t0:t0 + n], x_t[:, :, :], [P.buf()], reads=[x_b]))
    P.finish_wait("sp", outs)
    P.emit()
    return nc


import math, os
RET_STOP = int(os.environ.get('RET_STOP', '99'))
SKIP = os.environ.get('SKIP', '')

NTOK = 8448
NCH = 66
MAGIC = 12582912.0
TWO_PI = 2.0 * math.pi


def pos_of(dd):
    if dd == 0:
        return list(range(NCH))
    order = [1, 0] + list(range(65, 1, -1))
    pos = [0] * NCH
    for p_, c in enumerate(order):
        pos[c] = p_
    return pos


def range_reduce_sincos(P, ph, sn, cs, tmp, shape_ap, b):
    v = shape_ap
    _ts(P, "dve", v(tmp), v(ph), 1.0 / TWO_PI, MAGIC, ALU.mult, ALU.add, [b], [b])
    _ts(P, "dve", v(tmp), v(tmp), -MAGIC, None, ALU.add, None, [b], [b])
    _stt(P, v(ph), v(tmp), -TWO_PI, v(ph), ALU.mult, ALU.add, [b], [b])
    _ts(P, "dve", v(ph), v(ph), -math.pi, math.pi, ALU.max, ALU.min, [b], [b])
    _act(P, v(sn), v(ph), AF.Sin, [b], [b])
    _ts(P, "dve", v(tmp), v(ph), -1.0, None, ALU.mult, None, [b], [b])
    _tt(P, "dve", v(tmp), v(tmp), v(ph), ALU.max, [b], [b])
    _act(P, v(cs), v(tmp), AF.Sin, [b], [b], scale=-1.0, bias=P.halfpi[0:v(tmp).shape[0], 0:1])


def build_M(need_ctx_out, parts=("four", "s5", "ret", "na"), DBG=False):
    nc = bass.Bass("TRN2", target_bir_lowering=False)
    dr = {}

    def din(name, shape, dt=F32):
        dr[name] = nc.dram_tensor(name, list(shape), dt, kind="ExternalInput").ap()
    din("xT", [KC, 128, NTOK])
    din("condT", [128, KC, 2])
    din("w_mod", [D, 6 * D])
    din("b_modT", [128, 48, 2])
    din("norm_gT", [128, 4, KC, 2])
    din("w_fm", [D, 576])
    din("w_tm", [D, 256])
    din("f_CS", [64, 128], BF16); din("f_RP", [64, 128], BF16); din("f_RQ", [64, 128], BF16)
    din("f_CB", [128, 64, 128], BF16); din("f_SB", [128, 64, 128], BF16)
    din("f_C256", [128, 2, 256], BF16); din("f_S256", [128, 2, 256], BF16)
    din("r_cosF", [64, 8192]); din("r_sinF", [64, 8192]); din("r_cosT", [128, 64, 64]); din("r_sinT", [128, 64, 64])
    din("r_jcol", [128, 2]); din("r_dist", [128, 128]); din("r_mask", [2, 128, 128]); din("r_irow", [2, 64, 128])
    din("s_jrow", [128, 129]); din("s_jcol", [128, 1]); din("s_LT", [2, 128, 128], BF16); din("s_mrow", [64, 4]); din("s_msm", [128, 2, 4])
    din("ident_bf", [128, 128], BF16); din("ident_f", [128, 128])
    din("n_mask", [5, 128, 832]); din("n_toep", [15, 64, 64])
    din("s_sm", [128, 2, 2, 3]); din("s_row", [128, 2, 3, 256]); din("s_hs", [64, 2, 3, 64]); din("s_B", [64, 2, 2, 64])
    din("s_C", [128, 2, 2, 2, 16]); din("s_d", [64, 1])
    din("r_dec", [128, 2]); din("r_gn", [64, 1])
    out = nc.dram_tensor("brT_out", [4, 64, NTOK], BF16, kind="ExternalOutput").ap()
    hT = nc.dram_tensor("hT_scr", [KC, 128, NTOK], BF16, kind="Internal").ap().rearrange("k p t -> p k t")
    xT = dr["xT"].rearrange("k p t -> p k t")

    P = Prog(nc)
    arena_init(P)
    ps = [P.ps("ps%d" % i, [128, 512], F32) for i in range(8)]
    psb = [P.buf("ps%d" % i) for i in range(8)]
    cb = P.buf("consts")
    ones = A(P, [128, 128], BF16)
    P.op("dve", lambda h: h.memset(ones[:], 1.0), writes=[cb])
    P.eps_t = A(P, [128, 1], F32)
    P.op("dve", lambda h: h.memset(P.eps_t[:], EPS), writes=[cb])
    P.halfpi = A(P, [128, 1], F32)
    P.op("dve", lambda h: h.memset(P.halfpi[:], math.pi / 2), writes=[cb])
    P.one_t = A(P, [128, 1], F32)
    P.op("dve", lambda h: h.memset(P.one_t[:], 1.0), writes=[cb])
    ident = A(P, [128, 128], BF16)
    _ld(P, "sp", ident[:], dr["ident_bf"][:, :], [cb])
    mod_view = ps[7][:, 0:96].rearrange("p (j r) -> p j r", r=2)
    compute_mod(P, dr, [0, 1], mod_view, psb[7])
    gm_a, _ = mod_derived(P, 1, None, 0, 0)
    sh_a = P.modT[:, 0:8, :]
    wfm = A(P, [128, KC, 576], BF16)
    wtm = A(P, [128, KC, 256], BF16)
    wb = P.buf("w")
    _ld(P, "pool", wfm[:], dr["w_fm"].rearrange("(k p) c -> p k c", p=128), [wb])
    _ld(P, "pool", wtm[:], dr["w_tm"].rearrange("(k p) c -> p k c", p=128), [wb])
    blocks = [(0, 256, 1)] + [(256 + 512 * i, 512, 0) for i in range(16)]
    outs = []
    hTb = P.buf("hT")
    mark0 = P.a_cur

    def fm_proj(hb, hbb, n, g, pst, pstb):
        for k in range(KC):
            _mm(P, pst[0:64, 0:n], wfm[:, k, g * 64:(g + 1) * 64], hb[:, k, 0:n], k == 0, k == KC - 1, [wb, hbb], [pstb])

    sT = A(P, [64, NTOK], BF16)
    markS = P.a_cur
    fT = A(P, [64, NTOK], BF16)
    fTb, sTb = P.buf("fT"), P.buf("sT")
    markA = P.a_cur
    xb = [A(P, [128, KC, 512], F32) for _ in range(2)]
    xbb = [P.buf(), P.buf()]
    sq = A(P, [128, KC, 512], BF16)
    sqb = P.buf()
    rstd = A(P, [128, 512], F32)
    rstdb = P.buf()
    tmp = [A(P, [128, 512], F32) for _ in range(2)]
    tmpb = [P.buf(), P.buf()]
    hbs = [A(P, [128, KC, 512], BF16) for _ in range(2)]
    hbsb = [P.buf(), P.buf()]
    def _ldx(bi_):
        t0_, n_, _r = blocks[bi_]
        _ld(P, "sp", xb[bi_ % 2][:, 0:4, 0:n_], xT[:, 0:4, t0_:t0_ + n_], [xbb[bi_ % 2]])
        _ld(P, "sp", xb[bi_ % 2][:, 4:8, 0:n_], xT[:, 4:8, t0_:t0_ + n_], [xbb[bi_ % 2]])
    _ldx(0)
    for bi, (t0, n, r) in enumerate(blocks):
        x_t, x_b = xb[bi % 2], xbb[bi % 2]
        hb, hbb = hbs[bi % 2], hbsb[bi % 2]
        if bi + 1 < len(blocks):
            _ldx(bi + 1)
        rms_rstd(P, x_t, x_b, n, sq, sqb, ps[6], psb[6], rstd, rstdb, ones)
        norm_mod(P, x_t, x_b, n, rstd, rstdb, gm_a, sh_a, r, hb, hbb, tmp, tmpb)
        _ld(P, "sp", hT[:, :, t0:t0 + n], hb[:, :, 0:n], [hTb], reads=[hbb])
        for gi_, (g, dst, dstb) in enumerate(((0, fT, fTb), (1, sT, sTb))):
            pi_ = (2 * bi + gi_) % 4
            fm_proj(hb, hbb, n, g, ps[pi_], psb[pi_])
            _cp(P, "act" if gi_ == 0 else "dve", dst[:, t0:t0 + n], ps[pi_][0:64, 0:n], [psb[pi_]], [dstb])
    barrier(P)
    P.a_cur = markA

    if "four" in parts:
        markF = P.a_cur
        CS = A(P, [64, 128], BF16); RP = A(P, [64, 128], BF16); RQ = A(P, [64, 128], BF16)
        CB = A(P, [128, 64, 128], BF16); SB = A(P, [128, 64, 128], BF16)
        ftb = P.buf("ftab")
        for t_, nm in ((CS, "f_CS"), (RP, "f_RP"), (RQ, "f_RQ")):
            _ld(P, "sp", t_[:], dr[nm][:, :], [ftb])
        _ld(P, "sp", CB[:], dr["f_CB"][:, :, :], [ftb])
        _ld(P, "sp", SB[:], dr["f_SB"][:, :, :], [ftb])
        PQ = A(P, [64, 128, 128], BF16); PQb = P.buf("PQ")
        UVT = A(P, [128, 64, 128], BF16); UVTb = P.buf("UVT")
        aT = A(P, [64, NTOK], BF16); aTb = P.buf("aT")
        for g4 in range(32):
            pi_ = g4 % 2
            for jj in range(4):
                m2 = g4 * 4 + jj
                _mm(P, ps[pi_][0:64, jj * 128:(jj + 1) * 128], fT[:, 256 + m2:NTOK:128], CS[:, :], True, True, [fTb, ftb], [psb[pi_]])
            _cp(P, "act" if g4 % 2 else "dve", PQ[:, g4 * 4:(g4 + 1) * 4, :], ps[pi_][0:64, 0:512].rearrange("p (a b) -> p a b", b=128), [psb[pi_]], [PQb])
        for g4 in range(16):
            pi_ = 2 + g4 % 2
            for jj in range(4):
                d = g4 * 4 + jj
                _mm(P, ps[pi_][:, jj * 128:(jj + 1) * 128], PQ[:, :, d], RP[:, :], True, False, [PQb, ftb], [psb[pi_]])
                _mm(P, ps[pi_][:, jj * 128:(jj + 1) * 128], PQ[:, :, 64 + d], RQ[:, :], False, True, [PQb, ftb], [psb[pi_]])
            _cp(P, "act" if g4 % 2 else "dve", UVT[:, g4 * 4:(g4 + 1) * 4, :], ps[pi_][:, 0:512].rearrange("p (a b) -> p a b", b=128), [psb[pi_]], [UVTb])
        aT3 = aT[:, 256:NTOK].rearrange("p (a b) -> p a b", b=64)
        for g4 in range(16):
            pi_ = g4 % 2
            for jj in range(4):
                n1 = g4 * 4 + jj
                _mm(P, ps[pi_][0:64, jj * 128:(jj + 1) * 128], UVT[:, :, n1], CB[:, n1, :], True, False, [UVTb, ftb], [psb[pi_]])
                _mm(P, ps[pi_][0:64, jj * 128:(jj + 1) * 128], UVT[:, :, 64 + n1], SB[:, n1, :], False, True, [UVTb, ftb], [psb[pi_]])
            _cp(P, "act" if g4 % 2 else "dve", aT3[:, :, g4 * 4:(g4 + 1) * 4], ps[pi_][0:64, 0:512].rearrange("p (j n) -> p n j", n=128), [psb[pi_]], [aTb])
        if need_ctx_out:
            C256 = A(P, [128, 2, 256], BF16); S256 = A(P, [128, 2, 256], BF16)
            _ld(P, "sp", C256[:], dr["f_C256"][:, :, :], [ftb])
            _ld(P, "sp", S256[:], dr["f_S256"][:, :, :], [ftb])
            PQc = A(P, [128, 2, 128], BF16); PQcb = P.buf()
            for tt in range(2):
                _mm(P, ps[2 + tt][:, 0:128], fT[:, tt * 128:(tt + 1) * 128], CS[:, :], True, True, [fTb, ftb], [psb[2 + tt]])
                _cp(P, "dve", PQc[:, tt, :], ps[2 + tt][:, 0:128], [psb[2 + tt]], [PQcb])
            seq = [(tt, 0) for tt in range(2)] + [(tt, 1) for tt in range(2)]
            for i_, (tt, pq) in enumerate(seq):
                _mm(P, ps[4][0:64, 0:256], PQc[:, tt, pq * 64:(pq + 1) * 64], (C256 if pq == 0 else S256)[:, tt, :], i_ == 0, i_ == 3, [PQcb, ftb], [psb[4]])
            _cp(P, "dve", aT[:, 0:256], ps[4][0:64, 0:256], [psb[4]], [aTb])
        else:
            P.op("dve", lambda h: h.memset(aT[:, 0:256], 0.0), writes=[aTb])
        outs.append(_ld(P, "sp", out[0, :, :], aT[:, :], [P.buf()], reads=[aTb]))
        barrier(P)
    P.a_cur = markS

    if "s5" in parts:
        s5_part(P, dr, ps, psb, sT, sTb, out, outs, need_ctx_out, cb)
    barrier(P)
    P.a_cur = mark0

    if "ret" in parts:
        ret_part(P, dr, ps, psb, hT, hTb, wfm, wtm, wb, blocks, out, outs, need_ctx_out, cb, ones)
        barrier(P)
        P.a_cur = mark0
    if "na" in parts:
        na_part(P, dr, ps, psb, hT, hTb, wfm, wtm, wb, blocks, out, outs, need_ctx_out, cb, ident)
        barrier(P)
    P.finish_wait("sp", outs)
    P.emit()
    return nc


def load_h(P, hT, hTb, hbs, hbsb, bi, t0, n):
    hb, hbb = hbs[bi % 2], hbsb[bi % 2]
    _ld(P, "sp", hb[:, :, 0:n], hT[:, :, t0:t0 + n], [hbb], reads=[hTb])
    return hb, hbb


def na_part(P, dr, ps, psb, hT, hTb, wfm, wtm, wb, blocks, out, outs, need_ctx_out, cb, ident):
    Cn = consts()
    drs, types = Cn["n_drs"], Cn["n_types"]
    nqT = A(P, [64, NTOK], BF16); nkT = A(P, [64, NTOK], BF16); nvT = A(P, [128, NCH, 64], BF16)
    nqb, nkb, nvb = P.buf("nq"), P.buf("nk"), P.buf("nv")
    nT = A(P, [64, NTOK], BF16); nTb = P.buf("nT")
    bias = A(P, [128, 5, 832], F32); biasb = P.buf("bias")
    mask = A(P, [128, 5, 832], F32)
    P.op("pool", lambda h: h.memset(bias[:], 0.0), writes=[biasb])
    maskb = P.buf()
    _ld(P, "sp", mask[:], dr["n_mask"].rearrange("t p c -> p t c"), [maskb])
    for ti in range(5):
        for qr in range(2):
            for i in range(9):
                _ld(P, "sp" if (i % 2) else "act", bias[qr * 64:(qr + 1) * 64, ti, i * 64:(i + 1) * 64], dr["n_toep"][int(drs[ti, qr, i])], [biasb])
    _tt(P, "dve", bias[:], bias[:], mask[:], ALU.add, [biasb, maskb], [biasb])
    mark = P.a_cur
    hbs = [A(P, [128, KC, 512], BF16) for _ in range(2)]
    hbsb = [P.buf(), P.buf()]
    cnt = 0
    for bi, (t0, n, r) in enumerate(blocks):
        hb, hbb = load_h(P, hT, hTb, hbs, hbsb, bi, t0, n)
        for g, dst, dstb in ((7, nqT, nqb), (8, nkT, nkb)):
            pi_ = cnt % 4
            cnt += 1
            for k in range(KC):
                _mm(P, ps[pi_][0:64, 0:n], wfm[:, k, g * 64:(g + 1) * 64], hb[:, k, 0:n], k == 0, k == KC - 1, [wb, hbb], [psb[pi_]])
            _cp(P, "act" if g == 7 else "dve", dst[:, t0:t0 + n], ps[pi_][0:64, 0:n], [psb[pi_]], [dstb])
        for tt in range(n // 128):
            pi_ = 4 + (tt % 2)
            for k in range(KC):
                _mm(P, ps[pi_][:, 0:64], hb[:, k, tt * 128:(tt + 1) * 128], wtm[:, k, 192:256], k == 0, k == KC - 1, [wb, hbb], [psb[pi_]])
            _cp(P, "act", nvT[:, t0 // 128 + tt, :], ps[pi_][:, 0:64], [psb[pi_]], [nvb])
    barrier(P)
    P.a_cur = mark
    NB4 = 4
    s_t = [A(P, [128, 832], F32) for _ in range(NB4)]; s_b = [P.buf() for _ in range(NB4)]
    p_t = [A(P, [128, 832], BF16) for _ in range(NB4)]; p_b = [P.buf() for _ in range(NB4)]
    pT = [A(P, [128, 7, 128], BF16) for _ in range(NB4)]; pTb = [P.buf() for _ in range(NB4)]
    st_ = [A(P, [128, 4], F32) for _ in range(NB4)]; stb = [P.buf() for _ in range(NB4)]
    SC = 0.125

    def softmax_pv(qi, ncols, pv_list, o_ps, o_psb, o_cols, sbi, s4=None):
        if s4 is None:
            s4 = sbi
        s, sb_ = s_t[s4], s_b[s4]
        sm, smb = st_[s4], stb[s4]
        P.op("dve", lambda h: h.reduce_max(out=sm[:, 0:1], in_=s[:, 0:ncols], axis=AX.X), reads=[sb_], writes=[smb])
        _ts(P, "dve", sm[:, 1:2], sm[:, 0:1], -1.0, None, ALU.mult, None, [smb], [smb])
        _act(P, s[:, 0:ncols], s[:, 0:ncols], AF.Exp, [sb_, smb], [sb_], bias=sm[:, 1:2], scale=1.0)
        P.op("dve", lambda h: h.reduce_sum(out=sm[:, 2:3], in_=s[:, 0:ncols], axis=AX.X), reads=[sb_], writes=[smb])
        P.op("dve", lambda h: h.reciprocal(out=sm[:, 3:4], in_=sm[:, 2:3]), reads=[smb], writes=[smb])
        p, pb = p_t[s4], p_b[s4]
        _ts(P, "dve", p[:, 0:ncols], s[:, 0:ncols], sm[:, 3:4], None, ALU.mult, None, [sb_, smb], [pb])
        tp = ps[4 + sbi][:, :].bitcast(BF16)
        for ci, (c0, nk, tile) in enumerate(pv_list):
            _tr(P, tp[0:nk, ci * 128:(ci + 1) * 128], p[:, c0:c0 + nk], ident[:, :], [pb, cb], [psb[4 + sbi]])
        nchk = len(pv_list)
        pt_, ptb = pT[s4], pTb[s4]
        _cp(P, "act", pt_[:, 0:nchk, :], tp[:, 0:nchk * 128].rearrange("p (a b) -> p a b", b=128), [psb[4 + sbi]], [ptb])
        for ci, (c0, nk, tile) in enumerate(pv_list):
            _mm(P, o_ps[0:64, o_cols:o_cols + 128], nvT[0:nk, tile, :], pt_[0:nk, ci, :], ci == 0, ci == nchk - 1, [nvb, ptb], [o_psb])

    for rp in range(64):
        ti = {0: 0, 1: 1, 62: 3, 63: 4}.get(rp, 2)
        r0 = 2 * rp
        if ti == 2:
            R0, nr = r0 - 4, 9
        else:
            R0, nr = types[ti][1], 8
        tq = 256 + 128 * rp
        kb_ = 256 + 64 * R0
        sbi = rp % 2
        s1, s2 = ps[sbi], ps[2 + sbi]
        _mm(P, s1[:, 0:512], nqT[:, tq:tq + 128], nkT[:, kb_:kb_ + 512], True, True, [nqb, nkb], [psb[sbi]])
        kb2 = kb_ + 512 if nr == 9 else kb_
        _mm(P, s2[:, 0:64], nqT[:, tq:tq + 128], nkT[:, kb2:kb2 + 64], True, True, [nqb, nkb], [psb[2 + sbi]])
        _mm(P, s2[:, 64:320], nqT[:, tq:tq + 128], nkT[:, 0:256], True, True, [nqb, nkb], [psb[2 + sbi]])
        s4 = rp % NB4
        s = s_t[s4]
        _stt(P, s[:, 0:512], s1[:, 0:512], SC, bias[:, ti, 0:512], ALU.mult, ALU.add, [psb[sbi], biasb], [s_b[s4]])
        _stt(P, s[:, 512:832], s2[:, 0:320], SC, bias[:, ti, 512:832], ALU.mult, ALU.add, [psb[2 + sbi], biasb], [s_b[s4]])
        t_base = 2 + R0 // 2
        pv = [(128 * j, 128, t_base + j) for j in range(4)]
        if nr == 9:
            pv.append((512, 64, t_base + 4))
        pv += [(576, 128, 0), (704, 128, 1)]
        jj = rp % 4
        softmax_pv(rp, 832, pv, ps[6], psb[6], jj * 128, sbi, s4)
        if jj == 3:
            _cp(P, "dve", nT[:, 256 + 512 * (rp // 4):256 + 512 * (rp // 4 + 1)], ps[6][0:64, 0:512], [psb[6]], [nTb])
    if need_ctx_out:
        for qt in range(2):
            sbi = qt
            _mm(P, ps[sbi][:, 0:256], nqT[:, qt * 128:(qt + 1) * 128], nkT[:, 0:256], True, True, [nqb, nkb], [psb[sbi]])
            _ts(P, "dve", s_t[sbi][:, 0:256], ps[sbi][:, 0:256], SC, None, ALU.mult, None, [psb[sbi]], [s_b[sbi]])
            softmax_pv(qt, 256, [(0, 128, 0), (128, 128, 1)], ps[7], psb[7], qt * 128, sbi)
        _cp(P, "dve", nT[:, 0:256], ps[7][0:64, 0:256], [psb[7]], [nTb])
    else:
        P.op("dve", lambda h: h.memset(nT[:, 0:256], 0.0), writes=[nTb])
    outs.append(_ld(P, "sp", out[3, :, :], nT[:, :], [P.buf()], reads=[nTb]))


def ret_part(P, dr, ps, psb, hT, hTb, wfm, wtm, wb, blocks, out, outs, need_ctx_out, cb, ones):
    KS = 0.125
    qT = A(P, [64, NTOK], BF16); kT = A(P, [64, NTOK], BF16); gT = A(P, [64, NTOK], BF16)
    qb_, kb_, gb_ = P.buf("q"), P.buf("k"), P.buf("g")
    rvT = A(P, [128, NCH, 64], BF16); rvb = P.buf("rv")
    Sbf = [A(P, [64, NCH, 64], BF16) for _ in range(2)]
    rc = P.buf("retc")
    dec = A(P, [128, 2], F32); lg = A(P, [128, 2], F32); jcol = A(P, [128, 2], F32); kdec = A(P, [128, 2], F32); g128 = A(P, [128, 2], F32)
    dist = A(P, [128, 128], F32); msk = A(P, [128, 2, 128], F32); DT = A(P, [128, 128], F32); DT2 = A(P, [128, 128], F32)
    irow = A(P, [64, 2, 128], F32); qdec = A(P, [64, 2, 128], F32)
    gn = A(P, [64, 1], F32); o64 = A(P, [64, 64], F32)
    _ld(P, "sp", dec[:], dr["r_dec"][:, :], [rc])
    _ld(P, "sp", jcol[:], dr["r_jcol"][:, :], [rc])
    _ld(P, "sp", dist[:], dr["r_dist"][:, :], [rc])
    _ld(P, "sp", msk[:], dr["r_mask"].rearrange("d j i -> j d i"), [rc])
    _ld(P, "sp", irow[:], dr["r_irow"].rearrange("d p i -> p d i"), [rc])
    _ld(P, "sp", gn[:], dr["r_gn"][:, :], [rc])
    P.op("dve", lambda h: h.memset(o64[:], 1.0 / 64), writes=[rc])
    _act(P, lg[:], dec[:], AF.Exp, [rc], [rc], scale=-1.0)
    _act(P, lg[:], lg[:], AF.Ln, [rc], [rc], bias=P.one_t[:, 0:1], scale=1.0)
    _ts(P, "dve", lg[:], lg[:], -1.0, None, ALU.mult, None, [rc], [rc])
    for dd in range(2):
        _act(P, kdec[:, dd:dd + 1], jcol[:, dd:dd + 1], AF.Exp, [rc], [rc], scale=lg[:, dd:dd + 1])
        _act(P, g128[:, dd:dd + 1], lg[:, dd:dd + 1], AF.Exp, [rc], [rc], scale=128.0)
        _act(P, qdec[:, dd, :], irow[:, dd, :], AF.Exp, [rc], [rc], scale=lg[0:64, dd:dd + 1])
    _ts(P, "dve", kdec[:], kdec[:], KS, None, ALU.mult, None, [rc], [rc])
    _act(P, DT[:], dist[:], AF.Exp, [rc], [rc], scale=lg[:, 0:1])
    _tt(P, "dve", DT[:], DT[:], msk[:, 0, :], ALU.mult, [rc], [rc])
    _act(P, DT2[:], dist[:], AF.Exp, [rc], [rc], scale=lg[:, 1:2])
    _tt(P, "dve", DT2[:], DT2[:], msk[:, 1, :], ALU.mult, [rc], [rc])
    _tt(P, "dve", DT[:], DT[:], DT2[:], ALU.add, [rc], [rc])
    if RET_STOP <= 0:
        return
    mark_k = P.a_cur
    kd = [A(P, [128, NCH, 64], BF16) for _ in range(2)]
    kdb = [P.buf(), P.buf()]
    mark = P.a_cur
    hbs = [A(P, [128, KC, 512], BF16) for _ in range(2)]
    hbsb = [P.buf(), P.buf()]
    cF = [A(P, [64, 512], F32) for _ in range(2)]; sF = [A(P, [64, 512], F32) for _ in range(2)]
    cTt = [A(P, [128, 4, 64], F32) for _ in range(2)]; sTt = [A(P, [128, 4, 64], F32) for _ in range(2)]
    tabb = [P.buf(), P.buf()]
    t1 = [A(P, [128, 512], F32) for _ in range(2)]; t1b = [P.buf(), P.buf()]
    t2 = [A(P, [128, 512], F32) for _ in range(2)]; t2b = [P.buf(), P.buf()]
    cnt = 0
    for bi, (t0, n, r) in enumerate(blocks):
        hb, hbb = load_h(P, hT, hTb, hbs, hbsb, bi, t0, n)
        lat = (r == 0)
        tb_ = tabb[bi % 2]
        if lat:
            m0 = t0 - 256
            _ld(P, "sp", cF[bi % 2][:, :], dr["r_cosF"][:, m0:m0 + 512], [tb_])
            _ld(P, "sp", sF[bi % 2][:, :], dr["r_sinF"][:, m0:m0 + 512], [tb_])
            _ld(P, "sp", cTt[bi % 2][:, :, :], dr["r_cosT"][:, m0 // 128:m0 // 128 + 4, :], [tb_])
            _ld(P, "sp", sTt[bi % 2][:, :, :], dr["r_sinT"][:, m0 // 128:m0 // 128 + 4, :], [tb_])

        def proj(g, pi_):
            for k in range(KC):
                _mm(P, ps[pi_][0:64, 0:n], wfm[:, k, g * 64:(g + 1) * 64], hb[:, k, 0:n], k == 0, k == KC - 1, [wb, hbb], [psb[pi_]])
        for (g, gsw, dst, dstb, scl) in ((2, 4, qT, qb_, 1.0), (3, 5, kT, kb_, KS)):
            if 'qk' in SKIP:
                continue
            proj(g, 0)
            if lat:
                proj(gsw, 1)
                i2 = cnt % 2
                cnt += 1
                _stt(P, t1[i2][0:64, 0:n], ps[0][0:64, 0:n], scl, cF[bi % 2][:, 0:n], ALU.mult, ALU.mult, [psb[0], tb_], [t1b[i2]])
                _stt(P, t2[i2][0:64, 0:n], ps[1][0:64, 0:n], scl, sF[bi % 2][:, 0:n], ALU.mult, ALU.mult, [psb[1], tb_], [t2b[i2]])
                _tt(P, "pool", dst[:, t0:t0 + n], t1[i2][0:64, 0:n], t2[i2][0:64, 0:n], ALU.add, [t1b[i2], t2b[i2]], [dstb])
            else:
                _act(P, dst[:, t0:t0 + n], ps[0][0:64, 0:n], AF.Copy, [psb[0]], [dstb], scale=scl)
        proj(6, 2)
        _cp(P, "act", gT[:, t0:t0 + n], ps[2][0:64, 0:n], [psb[2]], [gb_])
        for tt in range(n // 128):
            if 'tm' in SKIP:
                continue
            pi_ = 4 + (tt % 2)
            tile = t0 // 128 + tt
            for k in range(KC):
                _mm(P, ps[pi_][:, 0:192], hb[:, k, tt * 128:(tt + 1) * 128], wtm[:, k, 0:192], k == 0, k == KC - 1, [wb, hbb], [psb[pi_]])
            _cp(P, "act", rvT[:, tile, :], ps[pi_][:, 128:192], [psb[pi_]], [rvb])
            if 'kd' in SKIP:
                continue
            if ('kl' in SKIP and lat) or ('kc' in SKIP and not lat):
                continue
            if lat:
                i2 = cnt % 2
                cnt += 1
                _tt(P, "dve", t1[i2][:, 0:64], ps[pi_][:, 0:64], cTt[bi % 2][:, tt, :], ALU.mult, [psb[pi_], tb_], [t1b[i2]])
                _tt(P, "dve", t2[i2][:, 0:64], ps[pi_][:, 64:128], sTt[bi % 2][:, tt, :], ALU.mult, [psb[pi_], tb_], [t2b[i2]])
                if 'k1' in SKIP:
                    continue
                _tt(P, "dve", t1[i2][:, 0:64], t1[i2][:, 0:64], t2[i2][:, 0:64], ALU.add, [t1b[i2], t2b[i2]], [t1b[i2]])
                if 'k2' in SKIP:
                    continue
                for dd in range(2):
                    _act(P, kd[dd][:, tile, :], t1[i2][:, 0:64], AF.Identity, [t1b[i2], rc], [kdb[dd]], scale=kdec[:, dd:dd + 1])
            else:
                for dd in range(2):
                    _act(P, kd[dd][:, tile, :], ps[pi_][:, 0:64], AF.Identity, [psb[pi_], rc], [kdb[dd]], scale=kdec[:, dd:dd + 1])
    barrier(P)
    P.a_cur = mark
    if RET_STOP <= 1:
        return
    S32_ = A(P, [64, NCH, 64], F32)
    S32 = [S32_, S32_]
    Sb_ = P.buf()
    Sb = [Sb_, Sb_]
    for dd in range(2):
        pos = pos_of(dd)
        order = sorted(range(NCH), key=lambda c: pos[c])
        c0 = order[0]
        P.op("dve", lambda h, dd=dd, c0=c0: h.memset(S32[dd][:, c0, :], 0.0), writes=[Sb[dd]])
        for idx in range(NCH - 1):
            c, nxt = order[idx], order[idx + 1]
            pi_ = (idx // 8) % 2
            sl = idx % 8
            _mm(P, ps[pi_][0:64, sl * 64:(sl + 1) * 64], kd[dd][:, c, :], rvT[:, c, :], True, True, [kdb[dd], rvb], [psb[pi_]])
            _stt(P, S32[dd][:, nxt, :], S32[dd][:, c, :], g128[0:64, dd:dd + 1], ps[pi_][0:64, sl * 64:(sl + 1) * 64], ALU.mult, ALU.add, [Sb[dd], psb[pi_], rc], [Sb[dd]])
        _cp(P, "act", Sbf[dd][:], S32[dd][:], [Sb[dd]], [Sb[dd]])
    barrier(P)
    P.a_cur = mark_k
    if RET_STOP <= 2:
        return
    oT = A(P, [64, NTOK], F32); oTb = P.buf("oT")
    sc = [A(P, [128, 128], BF16) for _ in range(2)]; scb = [P.buf(), P.buf()]
    qd = [[A(P, [64, 128], BF16) for _ in range(2)] for _ in range(2)]
    qdb = [[P.buf(), P.buf()] for _ in range(2)]
    c_start = 0 if need_ctx_out else 2
    if not need_ctx_out:
        P.op("pool", lambda h: h.memset(oT[:, 0:256], 0.0), writes=[oTb])
    for c in range(c_start, NCH):
        tau = 128 * c
        i2 = c % 2
        _mm(P, ps[i2][:, 0:128], kT[:, tau:tau + 128], qT[:, tau:tau + 128], True, True, [kb_, qb_], [psb[i2]])
        _tt(P, "dve", sc[i2][:, :], ps[i2][:, 0:128], DT[:, :], ALU.mult, [psb[i2], rc], [scb[i2]])
        for dd in range(2):
            _tt(P, "pool", qd[dd][i2][:, :], qT[:, tau:tau + 128], qdec[:, dd, :], ALU.mult, [qb_, rc], [qdb[dd][i2]])
        jj = c % 4
        po = ps[4 + (c // 4) % 2]
        pob = psb[4 + (c // 4) % 2]
        _mm(P, po[0:64, jj * 128:(jj + 1) * 128], rvT[:, c, :], sc[i2][:, :], True, False, [rvb, scb[i2]], [pob])
        _mm(P, po[0:64, jj * 128:(jj + 1) * 128], Sbf[0][:, c, :], qd[0][i2][:, :], False, False, [Sb[0], qdb[0][i2]], [pob])
        _mm(P, po[0:64, jj * 128:(jj + 1) * 128], Sbf[1][:, c, :], qd[1][i2][:, :], False, True, [Sb[1], qdb[1][i2]], [pob])
        if jj == 3 or c == NCH - 1:
            b0 = (c // 4) * 512
            wid = (jj + 1) * 128
            lo = 0
            if (not need_ctx_out) and c // 4 == 0:
                lo = 256
            _cp(P, "act", oT[:, b0 + lo:b0 + wid], po[0:64, lo:wid], [pob], [oTb])
    if RET_STOP <= 3:
        return
    rT = A(P, [64, NTOK], BF16); rTb = P.buf("rT")
    o64b = A(P, [64, 64], BF16)
    P.op("dve", lambda h: h.memset(o64b[:], 1.0 / 64), writes=[rc])
    obf = [A(P, [64, 512], BF16) for _ in range(2)]; obfb = [P.buf(), P.buf()]
    cen = [A(P, [64, 512], F32) for _ in range(2)]; cenb = [P.buf(), P.buf()]
    sq_ = [A(P, [64, 512], BF16) for _ in range(2)]; sqb_ = [P.buf(), P.buf()]
    rs_ = [A(P, [64, 512], F32) for _ in range(2)]; rsb_ = [P.buf(), P.buf()]
    sg_ = [A(P, [64, 512], F32) for _ in range(2)]; sgb_ = [P.buf(), P.buf()]
    nblk = (NTOK + 511) // 512
    for bi in range(nblk):
        t0 = bi * 512
        n = min(512, NTOK - t0)
        i2 = bi % 2
        _cp(P, "act", obf[i2][:, 0:n], oT[:, t0:t0 + n], [oTb], [obfb[i2]])
        _mm(P, ps[2 + i2][0:64, 0:n], o64b[:, :], obf[i2][:, 0:n], True, True, [obfb[i2], rc], [psb[2 + i2]])
        _tt(P, "dve", cen[i2][:, 0:n], oT[:, t0:t0 + n], ps[2 + i2][0:64, 0:n], ALU.subtract, [oTb, psb[2 + i2]], [cenb[i2]])
        _act(P, sq_[i2][:, 0:n], cen[i2][:, 0:n], AF.Square, [cenb[i2]], [sqb_[i2]])
        _mm(P, ps[6 + i2][0:64, 0:n], o64b[:, :], sq_[i2][:, 0:n], True, True, [sqb_[i2], rc], [psb[6 + i2]])
        _act(P, rs_[i2][:, 0:n], ps[6 + i2][0:64, 0:n], AF.Ln, [psb[6 + i2]], [rsb_[i2]], bias=P.eps_t[0:64, 0:1], scale=1.0)
        _act(P, rs_[i2][:, 0:n], rs_[i2][:, 0:n], AF.Exp, [rsb_[i2]], [rsb_[i2]], scale=-0.5)
        _tt(P, "dve", cen[i2][:, 0:n], cen[i2][:, 0:n], rs_[i2][:, 0:n], ALU.mult, [cenb[i2], rsb_[i2]], [cenb[i2]])
        _act(P, sg_[i2][:, 0:n], gT[:, t0:t0 + n], AF.Silu, [gb_], [sgb_[i2]])
        _stt(P, rT[:, t0:t0 + n], cen[i2][:, 0:n], gn[:, 0:1], sg_[i2][:, 0:n], ALU.mult, ALU.mult, [cenb[i2], sgb_[i2], rc], [rTb])
    outs.append(_ld(P, "sp", out[2, :, :], rT[:, :], [P.buf()], reads=[rTb]))


def s5_part(P, dr, ps, psb, sT, sTb, out, outs, need_ctx_out, cb):
    pb = P.buf("s5param")

    def cplx_prep(are, aim, ldt, mk):
        T = {k: mk() for k in ("dt", "ar", "ai", "mag", "ph", "tmp", "sn", "cs", "abr", "abi", "nr", "den", "cr", "ci", "u")}
        ident_v = lambda t: t
        _act(P, T["dt"], ldt, AF.Exp, [pb], [pb])
        _tt(P, "dve", T["ar"], are, T["dt"], ALU.mult, [pb], [pb])
        _tt(P, "dve", T["ai"], aim, T["dt"], ALU.mult, [pb], [pb])
        _act(P, T["mag"], T["ar"], AF.Exp, [pb], [pb])
        _cp(P, "dve", T["ph"], T["ai"], [pb], [pb])
        range_reduce_sincos(P, T["ph"], T["sn"], T["cs"], T["tmp"], ident_v, pb)
        _tt(P, "dve", T["abr"], T["mag"], T["cs"], ALU.mult, [pb], [pb])
        _tt(P, "dve", T["abi"], T["mag"], T["sn"], ALU.mult, [pb], [pb])
        _ts(P, "dve", T["nr"], T["abr"], -1.0, None, ALU.add, None, [pb], [pb])
        _tt(P, "dve", T["den"], are, are, ALU.mult, [pb], [pb])
        _tt(P, "dve", T["u"], aim, aim, ALU.mult, [pb], [pb])
        _tt(P, "dve", T["den"], T["den"], T["u"], ALU.add, [pb], [pb])
        P.op("dve", lambda h: h.reciprocal(out=T["den"], in_=T["den"]), reads=[pb], writes=[pb])
        _tt(P, "dve", T["cr"], T["nr"], are, ALU.mult, [pb], [pb])
        _tt(P, "dve", T["u"], T["abi"], aim, ALU.mult, [pb], [pb])
        _tt(P, "dve", T["cr"], T["cr"], T["u"], ALU.add, [pb], [pb])
        _tt(P, "dve", T["cr"], T["cr"], T["den"], ALU.mult, [pb], [pb])
        _tt(P, "dve", T["ci"], T["abi"], are, ALU.mult, [pb], [pb])
        _tt(P, "dve", T["u"], T["nr"], aim, ALU.mult, [pb], [pb])
        _tt(P, "dve", T["ci"], T["ci"], T["u"], ALU.subtract, [pb], [pb])
        _tt(P, "dve", T["ci"], T["ci"], T["den"], ALU.mult, [pb], [pb])
        return T

    p_sm = A(P, [128, 2, 2, 3], F32); p_row = A(P, [128, 2, 3, 256], F32); p_hs = A(P, [64, 2, 3, 64], F32)
    Bhs = A(P, [64, 2, 2, 64], F32); Csm = A(P, [128, 2, 2, 2, 16], F32); dvec = A(P, [64, 1], F32)
    jrow = A(P, [128, 129], F32); jcol = A(P, [128, 1], F32); njcol = A(P, [128, 1], F32)
    LT = A(P, [128, 2, 128], BF16); mrow = A(P, [64, 4], F32); msm = A(P, [128, 2, 4], F32)
    for t_, src in ((p_sm[:], dr["s_sm"][:, :, :, :]), (p_row[:], dr["s_row"][:, :, :, :]), (p_hs[:], dr["s_hs"][:, :, :, :]), (Bhs[:], dr["s_B"][:, :, :, :]),
                    (Csm[:], dr["s_C"][:, :, :, :, :]), (dvec[:], dr["s_d"][:, :]), (jrow[:], dr["s_jrow"][:, :]), (jcol[:], dr["s_jcol"][:, :]),
                    (LT[:], dr["s_LT"].rearrange("d j i -> j d i")), (mrow[:], dr["s_mrow"][:, :]), (msm[:], dr["s_msm"][:, :, :])):
        _ld(P, "sp", t_, src, [pb])
    _ts(P, "dve", njcol[:], jcol[:], -1.0, None, ALU.mult, None, [pb], [pb])
    ones_col = A(P, [128, 1], BF16)
    P.op("dve", lambda h: h.memset(ones_col[:], 1.0), writes=[pb])

    BD = [A(P, [64, 512], BF16) for _ in range(2)]
    CT = [A(P, [128, 4, 64], BF16) for _ in range(2)]
    mark_prep = P.a_cur
    for dd in range(2):
        P.a_cur = mark_prep
        Ths = cplx_prep(p_hs[:, dd, 0, :], p_hs[:, dd, 1, :], p_hs[:, dd, 2, :], lambda: A(P, [64, 64], F32)[:, :])
        bbr = A(P, [64, 64], F32); bbi = A(P, [64, 64], F32); uu = A(P, [64, 64], F32)
        _tt(P, "dve", bbr[:], Ths["cr"], Bhs[:, dd, 0, :], ALU.mult, [pb], [pb])
        _tt(P, "dve", uu[:], Ths["ci"], Bhs[:, dd, 1, :], ALU.mult, [pb], [pb])
        _tt(P, "dve", bbr[:], bbr[:], uu[:], ALU.subtract, [pb], [pb])
        _tt(P, "dve", bbi[:], Ths["cr"], Bhs[:, dd, 1, :], ALU.mult, [pb], [pb])
        _tt(P, "dve", uu[:], Ths["ci"], Bhs[:, dd, 0, :], ALU.mult, [pb], [pb])
        _tt(P, "dve", bbi[:], bbi[:], uu[:], ALU.add, [pb], [pb])
        for g in range(4):
            _ts(P, "dve", BD[dd][:, g * 64:(g + 1) * 64], bbr[:], mrow[:, g:g + 1], None, ALU.mult, None, [pb], [pb])
            _ts(P, "dve", BD[dd][:, 256 + g * 64:256 + (g + 1) * 64], bbi[:], mrow[:, g:g + 1], None, ALU.mult, None, [pb], [pb])
        for ri in range(2):
            for st in range(2):
                for g in range(4):
                    _ts(P, "dve", CT[dd][:, ri * 2 + st, g * 16:(g + 1) * 16], Csm[:, dd, st, ri, :], msm[:, st, g:g + 1], (1.0 if ri == 0 else -1.0), ALU.mult, ALU.mult, [pb], [pb])
    P.a_cur = mark_prep
    TA = [[A(P, [128, 2, 129], F32) for _ in range(2)] for _ in range(2)]
    TW = [[A(P, [128, 2, 129], F32) for _ in range(2)] for _ in range(2)]
    PRE = [[A(P, [128, 256], F32) for _ in range(2)] for _ in range(2)]
    mark_t = P.a_cur
    for dd in range(2):
        P.a_cur = mark_t
        dt_ = A(P, [128, 2], F32); ar = A(P, [128, 2], F32); ai = A(P, [128, 2], F32); nar = A(P, [128, 2], F32)
        _act(P, dt_[:], p_sm[:, dd, :, 2], AF.Exp, [pb], [pb])
        _tt(P, "dve", ar[:], p_sm[:, dd, :, 0], dt_[:], ALU.mult, [pb], [pb])
        _tt(P, "dve", ai[:], p_sm[:, dd, :, 1], dt_[:], ALU.mult, [pb], [pb])
        _ts(P, "dve", nar[:], ar[:], -1.0, None, ALU.mult, None, [pb], [pb])
        mark_st = P.a_cur
        for st in range(2):
            P.a_cur = mark_st
            ph = A(P, [128, 129], F32); tmp = A(P, [128, 129], F32); sn = A(P, [128, 129], F32); cs = A(P, [128, 129], F32)
            mp = A(P, [128, 129], F32); mn = A(P, [128, 129], F32)
            _ts(P, "dve", ph[:], jrow[:], ai[:, st:st + 1], None, ALU.mult, None, [pb], [pb])
            range_reduce_sincos(P, ph[:], sn[:], cs[:], tmp[:], (lambda t: t), pb)
            _act(P, mp[:], jrow[:], AF.Exp, [pb], [pb], scale=ar[:, st:st + 1])
            _act(P, mn[:], jrow[:], AF.Exp, [pb], [pb], scale=nar[:, st:st + 1])
            _tt(P, "dve", TA[dd][0][:, st, :], mp[:], cs[:], ALU.mult, [pb], [pb])
            _tt(P, "dve", TA[dd][1][:, st, :], mp[:], sn[:], ALU.mult, [pb], [pb])
            _tt(P, "dve", TW[dd][0][:, st, :], mn[:], cs[:], ALU.mult, [pb], [pb])
            _stt(P, TW[dd][1][:, st, :], mn[:], -1.0, sn[:], ALU.mult, ALU.mult, [pb], [pb])
        P.a_cur = mark_t
        dtr = A(P, [128, 256], F32); arr = A(P, [128, 256], F32); air = A(P, [128, 256], F32)
        ph = A(P, [128, 256], F32); tmp = A(P, [128, 256], F32); sn = A(P, [128, 256], F32); cs = A(P, [128, 256], F32); mg = A(P, [128, 256], F32)
        _act(P, dtr[:], p_row[:, dd, 2, :], AF.Exp, [pb], [pb])
        _tt(P, "dve", arr[:], p_row[:, dd, 0, :], dtr[:], ALU.mult, [pb], [pb])
        _tt(P, "dve", air[:], p_row[:, dd, 1, :], dtr[:], ALU.mult, [pb], [pb])
        _ts(P, "dve", ph[:], air[:], jcol[:, 0:1], None, ALU.mult, None, [pb], [pb])
        range_reduce_sincos(P, ph[:], sn[:], cs[:], tmp[:], (lambda t: t), pb)
        _act(P, mg[:], arr[:], AF.Exp, [pb], [pb], scale=(njcol if dd == 0 else jcol)[:, 0:1])
        _tt(P, "dve", PRE[dd][0][:], mg[:], cs[:], ALU.mult, [pb], [pb])
        _stt(P, PRE[dd][1][:], mg[:], (-1.0 if dd == 0 else 1.0), sn[:], ALU.mult, ALU.mult, [pb], [pb])
        P.a_cur = mark_t
    barrier(P)
    P.a_cur = mark_t
    Xt = A(P, [128, NCH, 512], BF16); Xtb = [P.buf() for _ in range(NCH)]
    yacc = A(P, [64, NTOK], F32); yb = P.buf("yacc")
    E = A(P, [128, 4, NCH], F32); Eb = P.buf("E")
    H = [[A(P, [128, 2, NCH], F32) for _ in range(2)] for _ in range(2)]
    Hb = P.buf("H")
    cv = [A(P, [128, 2, NCH], F32) for _ in range(2)]
    pw = A(P, [128, 2, 8], F32)
    tq = [A(P, [128, 256], F32) for _ in range(4)]; tqb = [P.buf() for _ in range(4)]
    hs_ = [A(P, [128, 4, 128], BF16) for _ in range(2)]; hsb = [P.buf(), P.buf()]
    uq = [A(P, [128, 128], F32) for _ in range(4)]; uqb = [P.buf() for _ in range(4)]
    for dd in range(2):
        pos = pos_of(dd)
        for c in range(NCH):
            tau = 128 * c
            px = ps[c % 2]; pxb = psb[c % 2]
            _mm(P, px[:, 0:512], sT[:, tau:tau + 128], BD[dd][:, :], True, True, [sTb, pb], [pxb])
            i2 = (c % 2) * 2
            _tt(P, "dve", tq[i2][:, :], px[:, 0:256], PRE[dd][0][:, :], ALU.mult, [pxb, pb], [tqb[i2]])
            _tt(P, "dve", tq[i2 + 1][:, :], px[:, 256:512], PRE[dd][1][:, :], ALU.mult, [pxb, pb], [tqb[i2 + 1]])
            _tt(P, "pool", Xt[:, c, 0:256], tq[i2][:, :], tq[i2 + 1][:, :], ALU.subtract, [tqb[i2], tqb[i2 + 1]], [Xtb[c]])
            _tt(P, "dve", tq[i2][:, :], px[:, 0:256], PRE[dd][1][:, :], ALU.mult, [pxb, pb], [tqb[i2]])
            _tt(P, "dve", tq[i2 + 1][:, :], px[:, 256:512], PRE[dd][0][:, :], ALU.mult, [pxb, pb], [tqb[i2 + 1]])
            _tt(P, "pool", Xt[:, c, 256:512], tq[i2][:, :], tq[i2 + 1][:, :], ALU.add, [tqb[i2], tqb[i2 + 1]], [Xtb[c]])
            for tl in range(4):
                col = tl * NCH + pos[c]
                _mm(P, ps[6][:, col:col + 1], Xt[:, c, tl * 128:(tl + 1) * 128], ones_col[:, :], True, True, [Xtb[c], pb], [psb[6]])
        _cp(P, "dve", E[:], ps[6][:, 0:4 * NCH].rearrange("p (a b) -> p a b", b=NCH), [psb[6]], [Eb])
        H0r, H0i = H[0][0], H[0][1]
        if dd == 0:
            for st in range(2):
                a_r, a_i = TA[0][0][:, st, 127:128], TA[0][1][:, st, 127:128]
                _ts(P, "dve", uq[0][:, 0:NCH], E[:, 2 + st, :], a_i, None, ALU.mult, None, [Eb, pb], [uqb[0]])
                _stt(P, H0r[:, st, :], E[:, st, :], a_r, uq[0][:, 0:NCH], ALU.mult, ALU.subtract, [Eb, pb, uqb[0]], [Hb])
                _ts(P, "dve", uq[1][:, 0:NCH], E[:, st, :], a_i, None, ALU.mult, None, [Eb, pb], [uqb[1]])
                _stt(P, H0i[:, st, :], E[:, 2 + st, :], a_r, uq[1][:, 0:NCH], ALU.mult, ALU.add, [Eb, pb, uqb[1]], [Hb])
        else:
            _cp(P, "dve", H0r[:], E[:, 0:2, :], [Eb], [Hb])
            _cp(P, "dve", H0i[:], E[:, 2:4, :], [Eb], [Hb])
        _cp(P, "dve", pw[:, :, 0], TA[dd][0][:, :, 128], [pb], [Hb])
        _cp(P, "dve", pw[:, :, 1], TA[dd][1][:, :, 128], [pb], [Hb])
        cur = 0
        d = 1
        while d < NCH:
            _ts(P, "dve", pw[:, :, 2], pw[:, :, 1], -1.0, None, ALU.mult, None, [Hb], [Hb])
            o_, n_ = H[cur], H[1 - cur]
            for ri in range(2):
                _cp(P, "dve", n_[ri][:, :, 0:d], o_[ri][:, :, 0:d], [Hb], [Hb])
            for st in range(2):
                pr, pi, npi = pw[:, st, 0:1], pw[:, st, 1:2], pw[:, st, 2:3]
                m = NCH - d
                _stt(P, uq[0][:, 0:m], o_[0][:, st, 0:m], pr, o_[0][:, st, d:NCH], ALU.mult, ALU.add, [Hb], [uqb[0]])
                _stt(P, n_[0][:, st, d:NCH], o_[1][:, st, 0:m], npi, uq[0][:, 0:m], ALU.mult, ALU.add, [Hb, uqb[0]], [Hb])
                _stt(P, uq[1][:, 0:m], o_[1][:, st, 0:m], pr, o_[1][:, st, d:NCH], ALU.mult, ALU.add, [Hb], [uqb[1]])
                _stt(P, n_[1][:, st, d:NCH], o_[0][:, st, 0:m], pi, uq[1][:, 0:m], ALU.mult, ALU.add, [Hb, uqb[1]], [Hb])
            _tt(P, "dve", pw[:, :, 3], pw[:, :, 0], pw[:, :, 0], ALU.mult, [Hb], [Hb])
            _tt(P, "dve", pw[:, :, 4], pw[:, :, 1], pw[:, :, 1], ALU.mult, [Hb], [Hb])
            _tt(P, "dve", pw[:, :, 5], pw[:, :, 0], pw[:, :, 1], ALU.mult, [Hb], [Hb])
            _tt(P, "dve", pw[:, :, 0], pw[:, :, 3], pw[:, :, 4], ALU.subtract, [Hb], [Hb])
            _ts(P, "dve", pw[:, :, 1], pw[:, :, 5], 2.0, None, ALU.mult, None, [Hb], [Hb])
            cur = 1 - cur
            d *= 2
        Hf = H[cur]
        kidx = 1 if dd == 0 else 128
        P.op("dve", lambda h: h.memset(cv[0][:, :, 0:1], 0.0), writes=[Hb])
        P.op("dve", lambda h: h.memset(cv[1][:, :, 0:1], 0.0), writes=[Hb])
        for st in range(2):
            a_r, a_i = TA[dd][0][:, st, kidx:kidx + 1], TA[dd][1][:, st, kidx:kidx + 1]
            m = NCH - 1
            _ts(P, "dve", uq[0][:, 0:m], Hf[1][:, st, 0:m], a_i, None, ALU.mult, None, [Hb, pb], [uqb[0]])
            _stt(P, cv[0][:, st, 1:NCH], Hf[0][:, st, 0:m], a_r, uq[0][:, 0:m], ALU.mult, ALU.subtract, [Hb, pb, uqb[0]], [Hb])
            _ts(P, "dve", uq[1][:, 0:m], Hf[0][:, st, 0:m], a_i, None, ALU.mult, None, [Hb, pb], [uqb[1]])
            _stt(P, cv[1][:, st, 1:NCH], Hf[1][:, st, 0:m], a_r, uq[1][:, 0:m], ALU.mult, ALU.add, [Hb, pb, uqb[1]], [Hb])
        Tt = TA[dd] if dd == 0 else TW[dd]
        c_start = 0 if need_ctx_out else 2
        for c in range(c_start, NCH):
            pg = ps[2 + c % 2]; pgb = psb[2 + c % 2]
            for tl in range(4):
                _mm(P, pg[:, tl * 128:(tl + 1) * 128], Xt[:, c, tl * 128:(tl + 1) * 128], LT[:, dd, :], True, True, [Xtb[c], pb], [pgb])
            hh, hhb = hs_[c % 2], hsb[c % 2]
            pc = pos[c]
            for st in range(2):
                gr, gi = pg[:, st * 128:(st + 1) * 128], pg[:, (2 + st) * 128:(3 + st) * 128]
                c_r, c_i = cv[0][:, st, pc:pc + 1], cv[1][:, st, pc:pc + 1]
                Tr, Ti = Tt[0][:, st, 0:128], Tt[1][:, st, 0:128]
                _stt(P, uq[0][:, :], gr, c_r, Tr, ALU.add, ALU.mult, [pgb, Hb, pb], [uqb[0]])
                _stt(P, uq[1][:, :], gi, c_i, Ti, ALU.add, ALU.mult, [pgb, Hb, pb], [uqb[1]])
                _tt(P, "pool", hh[:, st, :], uq[0][:, :], uq[1][:, :], ALU.subtract, [uqb[0], uqb[1]], [hhb])
                _stt(P, uq[2][:, :], gi, c_i, Tr, ALU.add, ALU.mult, [pgb, Hb, pb], [uqb[2]])
                _stt(P, uq[3][:, :], gr, c_r, Ti, ALU.add, ALU.mult, [pgb, Hb, pb], [uqb[3]])
                _tt(P, "pool", hh[:, 2 + st, :], uq[2][:, :], uq[3][:, :], ALU.add, [uqb[2], uqb[3]], [hhb])
            jj = c % 4
            py = ps[4 + (c // 4) % 2]; pyb = psb[4 + (c // 4) % 2]
            for tl in range(4):
                _mm(P, py[0:64, jj * 128:(jj + 1) * 128], CT[dd][:, tl, :], hh[:, tl, :], tl == 0, tl == 3, [pb, hhb], [pyb])
            if jj == 3 or c == NCH - 1:
                b0 = (c // 4) * 512
                wid = (jj + 1) * 128
                lo = 256 if ((not need_ctx_out) and c // 4 == 0) else 0
                if dd == 0:
                    _cp(P, "act", yacc[:, b0 + lo:b0 + wid], py[0:64, lo:wid], [pyb], [yb])
                else:
                    _tt(P, "dve", yacc[:, b0 + lo:b0 + wid], yacc[:, b0 + lo:b0 + wid], py[0:64, lo:wid], ALU.add, [pyb, yb], [yb])
    zT = A(P, [64, NTOK], BF16); zb = P.buf("zT")
    g1 = [A(P, [64, 512], F32) for _ in range(2)]; g1b = [P.buf(), P.buf()]
    g2 = [A(P, [64, 512], F32) for _ in range(2)]; g2b = [P.buf(), P.buf()]
    lo_all = 0 if need_ctx_out else 256
    if not need_ctx_out:
        P.op("pool", lambda h: h.memset(zT[:, 0:256], 0.0), writes=[zb])
    nblk = (NTOK + 511) // 512
    for bi in range(nblk):
        t0 = max(bi * 512, lo_all)
        t1_ = min((bi + 1) * 512, NTOK)
        n = t1_ - t0
        i2 = bi % 2
        y = g1[i2]; w = g2[i2]
        _stt(P, y[:, 0:n], sT[:, t0:t1_], dvec[:, 0:1], yacc[:, t0:t1_], ALU.mult, ALU.add, [sTb, pb, yb], [g1b[i2]])
        _tt(P, "dve", w[:, 0:n], y[:, 0:n], y[:, 0:n], ALU.mult, [g1b[i2]], [g2b[i2]])
        _ts(P, "dve", w[:, 0:n], w[:, 0:n], 0.044715, 1.0, ALU.mult, ALU.add, [g2b[i2]], [g2b[i2]])
        _tt(P, "dve", w[:, 0:n], w[:, 0:n], y[:, 0:n], ALU.mult, [g2b[i2], g1b[i2]], [g2b[i2]])
        _act(P, w[:, 0:n], w[:, 0:n], AF.Sigmoid, [g2b[i2]], [g2b[i2]], scale=1.5957691216057308)
        _tt(P, "dve", zT[:, t0:t1_], w[:, 0:n], y[:, 0:n], ALU.mult, [g2b[i2], g1b[i2]], [zb])
    outs.append(_ld(P, "sp", out[1, :, :], zT[:, :], [P.buf()], reads=[zb]))


import ml_dtypes

BF = ml_dtypes.bfloat16
NTOK = 8448
f32 = np.float32


def fm(a):
    return np.ascontiguousarray(a.T.reshape(8, 128, a.shape[0]))


def prep_common(inp, layer, b):
    cond = np.stack([inp['c'][b], inp['c_ctx']], 0)
    condT = np.ascontiguousarray(cond.reshape(2, 8, 128).transpose(2, 1, 0))
    bm = inp['b_mod'][layer].reshape(48, 128).T
    b_modT = np.ascontiguousarray(np.stack([bm, bm], -1))
    ng = inp['norm_g'][layer].reshape(4, 8, 128).transpose(2, 0, 1)
    norm_gT = np.ascontiguousarray(np.stack([ng, ng], -1))
    return dict(condT=condT, b_modT=b_modT, norm_gT=norm_gT, w_mod=inp['w_mod'][layer])


_const_cache = {}


def consts():
    if _const_cache:
        return _const_cache
    C = _const_cache
    c = np.arange(64)
    ang = 2 * np.pi * (np.outer(c, c) % 64) / 64
    C['f_CS'] = np.concatenate([np.cos(ang), -np.sin(ang)], 1).astype(BF)
    ca, sa = np.cos(ang), np.sin(ang)
    C['f_RP'] = np.concatenate([ca, -sa], 1).astype(BF)
    C['f_RQ'] = np.concatenate([sa, ca], 1).astype(BF)
    m2 = np.arange(128)[:, None, None]
    n1 = np.arange(64)[None, :, None]
    n2 = np.arange(128)[None, None, :]
    be = 2 * np.pi * ((m2 * (n1 + 64 * n2)) % 8192) / 8192
    nrm = 1 / np.sqrt(64 * 8192)
    C['f_CB'] = (np.cos(be) * nrm).astype(BF)
    C['f_SB'] = (np.sin(be) * nrm).astype(BF)
    m = np.arange(256)
    a256 = 2 * np.pi * (np.outer(m, m) % 256) / 256
    nrm2 = 1 / np.sqrt(64 * 256)
    C['f_C256'] = np.ascontiguousarray((np.cos(a256) * nrm2).reshape(2, 128, 256).transpose(1, 0, 2)).astype(BF)
    C['f_S256'] = np.ascontiguousarray((np.sin(a256) * nrm2).reshape(2, 128, 256).transpose(1, 0, 2)).astype(BF)
    t = np.arange(8192)
    row = (t // 64).astype(f32)
    col = (t % 64).astype(f32)
    inv = (1.0 / (f32(10000.0) ** (np.arange(16, dtype=f32) / f32(16)))).astype(f32)
    angr = np.concatenate([row[:, None] * inv, col[:, None] * inv], -1).astype(f32)
    cs, sn = np.cos(angr).astype(f32), np.sin(angr).astype(f32)
    cos64 = np.concatenate([cs, cs], 1)
    sin64 = np.concatenate([-sn, sn], 1)
    C['r_cosF'] = np.ascontiguousarray(cos64.T)
    C['r_sinF'] = np.ascontiguousarray(sin64.T)
    C['r_cosT'] = np.ascontiguousarray(cos64.reshape(64, 128, 64).transpose(1, 0, 2))
    C['r_sinT'] = np.ascontiguousarray(sin64.reshape(64, 128, 64).transpose(1, 0, 2))
    j = np.arange(128, dtype=f32)
    C['r_jcol'] = np.stack([127 - j, j], 1).astype(f32)
    ii = np.arange(128)
    dist = np.abs(ii[None, :] - ii[:, None]).astype(f32)
    C['r_dist'] = dist
    C['r_mask'] = np.stack([(ii[None, :] >= ii[:, None]), (ii[:, None] >= ii[None, :])], 0).astype(f32)
    C['r_irow'] = np.stack([np.tile(j + 1, (64, 1)), np.tile(128 - j, (64, 1))], 0).astype(f32)
    C['s_jrow'] = np.tile(np.arange(129, dtype=f32), (128, 1))
    C['s_jcol'] = np.arange(128, dtype=f32)[:, None].copy()
    C['s_LT'] = np.stack([(ii[None, :] >= ii[:, None]), (ii[:, None] >= ii[None, :])], 0).astype(BF)
    g_of_row = np.arange(64) // 16
    C['s_mrow'] = (g_of_row[:, None] == np.arange(4)[None, :]).astype(f32)
    g_of_st = (np.arange(128)[:, None] // 64) + 2 * np.arange(2)[None, :]
    C['s_msm'] = (g_of_st[:, :, None] == np.arange(4)[None, None, :]).astype(f32)
    C['ident_bf'] = np.eye(128).astype(BF)
    C['ident_f'] = np.eye(128).astype(f32)
    def start(r):
        return int(np.clip(r - 4, 0, 120))
    qc = np.arange(64)
    cst = np.clip(qc - 8, 0, 48)
    kc = np.arange(64)
    colok = (kc[None, :] >= cst[:, None]) & (kc[None, :] < cst[:, None] + 16)
    types = [(0, 0, 8), (2, 0, 8), (10, 6, 9), (124, 120, 8), (126, 120, 8)]
    mask = np.full((5, 128, 832), -30000.0, f32)
    drs = np.zeros((5, 2, 9), np.int64)
    for ti, (r0, R0, nr) in enumerate(types):
        for qr in range(2):
            r = r0 + qr
            for i in range(9):
                kr = R0 + i
                dr = int(np.clip(kr - r + 7, 0, 14))
                drs[ti, qr, i] = dr
                if i < nr and start(r) <= kr < start(r) + 8:
                    blk = np.where(colok, 0.0, -30000.0)
                    mask[ti, qr * 64:(qr + 1) * 64, i * 64:(i + 1) * 64] = blk
        mask[ti, :, 576:] = 0.0
    C['n_mask'] = mask
    C['n_drs'] = drs
    C['n_types'] = types
    return C


def prep_M(inp, layer, c, xT_full):
    b, q = c // 4, c % 4
    C = consts()
    m = prep_common(inp, layer, b)
    m['xT'] = xT_full
    w_in = inp['w_in'][layer]
    o = q * 64
    sw = np.r_[32:64, 0:32]
    cols_fm = np.concatenate([np.arange(0 + o, 0 + o + 64), np.arange(256 + o, 256 + o + 64), np.arange(512 + o, 512 + o + 64),
                              np.arange(768 + o, 768 + o + 64), 512 + o + sw, 768 + o + sw, np.arange(1280 + o, 1280 + o + 64),
                              np.arange(1536 + o, 1536 + o + 64), np.arange(1792 + o, 1792 + o + 64)])
    cols_tm = np.concatenate([np.arange(768 + o, 768 + o + 64), 768 + o + sw, np.arange(1024 + o, 1024 + o + 64), np.arange(2048 + o, 2048 + o + 64)])
    m['w_fm'] = np.ascontiguousarray(w_in[:, cols_fm])
    m['w_tm'] = np.ascontiguousarray(w_in[:, cols_tm])
    for k in ('f_CS', 'f_RP', 'f_RQ', 'f_CB', 'f_SB', 'f_C256', 'f_S256', 'r_cosF', 'r_sinF', 'r_cosT', 'r_sinT', 'r_jcol', 'r_dist', 'r_mask', 'r_irow',
              's_jrow', 's_jcol', 's_LT', 's_mrow', 's_msm', 'ident_bf', 'ident_f', 'n_mask'):
        m[k] = C[k]
    gs = slice(4 * q, 4 * q + 4)
    L = layer
    are, aim = inp['s5_a_re'][L][:, gs], inp['s5_a_im'][L][:, gs]
    ldt = inp['s5_log_dt'][L][:, gs]
    def sm(a):
        return np.ascontiguousarray(a.reshape(2, 2, 128).transpose(2, 0, 1))
    ldt_b = np.broadcast_to(ldt[:, :, None], (2, 4, 64))
    m['s_sm'] = np.ascontiguousarray(np.stack([sm(are), sm(aim), sm(ldt_b)], -1))
    row = np.stack([are.reshape(2, 256), aim.reshape(2, 256), ldt_b.reshape(2, 256)], -1)
    m['s_row'] = np.ascontiguousarray(np.broadcast_to(row[None].transpose(0, 1, 3, 2), (128, 2, 3, 256)))
    hs = np.stack([are, aim, ldt_b], 2)
    hs = np.broadcast_to(hs[:, :, None], (2, 4, 16, 3, 64))
    m['s_hs'] = np.ascontiguousarray(hs.transpose(1, 2, 0, 3, 4).reshape(64, 2, 3, 64))
    bre, bim = inp['s5_b_re'][L][:, gs], inp['s5_b_im'][L][:, gs]
    B = np.stack([bre, bim], 2)
    m['s_B'] = np.ascontiguousarray(B.transpose(1, 4, 0, 2, 3).reshape(64, 2, 2, 64))
    cre, cim = inp['s5_c_re'][L][:, gs], inp['s5_c_im'][L][:, gs]
    Cc = np.stack([cre, cim], 2)
    Cc = Cc.transpose(1, 4, 0, 2, 3)
    Cc = Cc.reshape(2, 2, 64, 2, 2, 16).transpose(1, 2, 3, 0, 4, 5).reshape(128, 2, 2, 2, 16)
    m['s_C'] = np.ascontiguousarray(Cc)
    m['s_d'] = np.ascontiguousarray(inp['s5_d'][L][256 * 0 + 64 * q:64 * q + 64][:, None])
    rd = inp['ret_decay'][L][:, q]
    m['r_dec'] = np.ascontiguousarray(np.broadcast_to(rd[None, :], (128, 2))).astype(f32)
    m['r_gn'] = np.ascontiguousarray(inp['ret_gn'][L][64 * q:64 * q + 64][:, None])
    rpb = inp['na_rpb'][L][q]
    dc = np.clip(np.arange(64)[None, :] - np.arange(64)[:, None], -15, 15) + 15
    m['n_toep'] = np.ascontiguousarray(rpb[:, dc])
    return m


def _prep_F(inp, layer, c, xa, bra_bf, moe_a=False):
    b = c // 4
    m = prep_common(inp, layer, b)
    m.update(xT=fm(xa), brT=bra_bf, w_in=inp['w_in'][layer], w_br=inp['w_branch'][layer].reshape(1024, 1024), w_o=inp['w_out'][layer],
             w_glu=inp['s5_w_glu'][layer], b_gluT=np.ascontiguousarray(inp['s5_b_glu'][layer].reshape(2, 128).T))
    i = layer // 2
    if layer % 2 == 0:
        m.update(w_g=inp['ffn_w_gate'][i:i + 1], w_u=inp['ffn_w_up'][i:i + 1], w_d=inp['ffn_w_down'][i:i + 1])
    else:
        sel = np.zeros((8, 8, 128), np.float32)
        for e in range(8):
            sel[e, e, :] = 1
        m.update(w_r=inp['moe_w_router'][i], b_r=np.ascontiguousarray(np.broadcast_to(inp['moe_b_router'][i][None], (128, 8))),
                 ident=np.eye(128, dtype=np.float32), sel=sel)
    return m


def kernel(**inputs):
    inp = {k: np.asarray(v) for k, v in inputs.items()}
    NCORE = 8
    cores = list(range(NCORE))
    x = inp['x']
    ctx = inp['ctx']
    for layer in range(2):
        last = (layer == 1)
        ncM = build_M(not last)
        xfull = [fm(np.concatenate([ctx[b], x[b]], 0)) for b in range(2)]
        maps = [prep_M(inp, layer, c, xfull[c // 4]) for c in cores]
        resM = run_bass_kernel_spmd(ncM, maps, core_ids=cores).results
        del maps
        br_full = []
        for b in range(2):
            o = np.stack([np.asarray(resM[4 * b + q]['brT_out']) for q in range(4)], 1)
            br_full.append(o.reshape(1024, NTOK))
        del resM
        if not last:
            blocksA = [(i * 256, 256, 0) for i in range(8)] + [(2048, 64, 1)]
            blocksB = [(i * 512, 512, 0) for i in range(4)] + [(2048, 64, 1)]
            ncF = build_F(blocksA, blocksB, 1, 2816, False)
        else:
            blocksA = [(i * 256, 256, 0) for i in range(8)]
            blocksB = [(i * 512, 512, 0) for i in range(4)]
            ncF = build_F(blocksA, blocksB, 8, 3584, True, mode='moe_a')
        maps = []
        for c in cores:
            b, q = c // 4, c % 4
            lat = slice(256 + q * 2048, 256 + (q + 1) * 2048)
            if not last:
                xa = np.concatenate([x[b, q * 2048:(q + 1) * 2048], ctx[b, q * 64:(q + 1) * 64]], 0)
                bra = np.concatenate([br_full[b][:, lat], br_full[b][:, q * 64:(q + 1) * 64]], 1)
            else:
                xa = x[b, q * 2048:(q + 1) * 2048]
                bra = br_full[b][:, lat]
            bra = np.ascontiguousarray(bra.reshape(8, 128, bra.shape[1]))
            maps.append(_prep_F(inp, layer, c, xa, bra))
        resF = run_bass_kernel_spmd(ncF, maps, core_ids=cores).results
        del maps
        if not last:
            xn = np.empty_like(x)
            cn = np.empty_like(ctx)
            for c in cores:
                b, q = c // 4, c % 4
                o = np.asarray(resF[c]['xo']).reshape(1024, -1).T
                xn[b, q * 2048:(q + 1) * 2048] = o[:2048]
                cn[b, q * 64:(q + 1) * 64] = o[2048:]
            x, ctx = xn, cn
            continue
        i = layer // 2
        h2_all = np.concatenate([np.asarray(resF[c]['h2o']) for c in cores], 2)
        cb_all = np.concatenate([np.asarray(resF[c]['cbo']) for c in cores], 1)
        sel = np.concatenate([np.asarray(resF[c]['mko']) for c in cores], 1).astype(bool)
        idx = [np.flatnonzero(sel[e]) for e in cores]
        nb = max(1, -(-max(len(t) for t in idx) // 512))
        ng = -(-nb // 4)
        groups = tuple(nb // ng + (1 if g < nb % ng else 0) for g in range(ng))
        C = 512 * nb
        ncE = build_E(groups)
        maps = []
        for e in cores:
            n_e = len(idx[e])
            ii = np.zeros(C, np.int64)
            ii[:n_e] = idx[e]
            cbe = np.zeros(C, cb_all.dtype)
            cbe[:n_e] = cb_all[e, idx[e]]
            maps.append(dict(h2=np.ascontiguousarray(h2_all[:, :, ii]), cbe=np.ascontiguousarray(np.broadcast_to(cbe[None, :], (128, C))),
                             w_g=inp['moe_w_gate'][i][e], w_u=inp['moe_w_up'][i][e], w_d=inp['moe_w_down'][i][e]))
        resE = run_bass_kernel_spmd(ncE, maps, core_ids=cores).results
        del maps
        slot = np.cumsum(sel, axis=0) - sel
        nslot = max(1, int(sel.sum(0).max()))
        yp_all = np.zeros((nslot, 8, 128, sel.shape[1]), np.float32)
        for e in cores:
            ye = np.asarray(resE[e]['ye'])
            t = idx[e]
            sv = slot[e, t]
            for k in range(nslot):
                mk = sv == k
                yp_all[k][:, :, t[mk]] = ye[:, :, np.flatnonzero(mk)]
        ncC = build_Fc(nexp=nslot)
        maps = []
        for c in cores:
            m = prep_common(inp, layer, c // 4)
            m['xm'] = np.asarray(resF[c]['xo'])
            m['yp'] = np.ascontiguousarray(yp_all[:, :, :, c * 2048:(c + 1) * 2048])
            maps.append(m)
        resC = run_bass_kernel_spmd(ncC, maps, core_ids=cores).results
        xn = np.empty_like(x)
        for c in cores:
            b, q = c // 4, c % 4
            xn[b, q * 2048:(q + 1) * 2048] = np.asarray(resC[c]['xo']).reshape(1024, -1).T
        x = xn
    return x.astype(np.float32)
```

```python
import numpy as np
from contextlib import ExitStack
import concourse.bass as bass
import concourse.mybir as mybir
from concourse.bass_utils import run_bass_kernel_spmd

F32 = mybir.dt.float32
BF16 = mybir.dt.bfloat16
I32 = mybir.dt.int32
ALU = mybir.AluOpType
AF = mybir.ActivationFunctionType
AX = mybir.AxisListType

ENGS = ("pe", "act", "dve", "pool", "sp")
NDSEM = 12


class Buf:
    __slots__ = ("name", "lw", "rd", "psum")

    def __init__(self, name="", psum=False):
        self.name = name
        self.psum = psum
        self.lw = None
        self.rd = {}


class Prog:
    def __init__(self, nc):
        self.nc = nc
        self.stack = ExitStack()
        self.ops = {e: [] for e in ENGS}
        self.cnt = {e: 0 for e in ENGS}
        self.seen = {e: {} for e in ENGS}
        self.sems = {}
        for e in ENGS:
            self.sems[e] = self.stack.enter_context(nc.semaphore("s_" + e))
        self.dsem_use = {}
        self.dq_next = {}
        for q in ("sp", "act", "pool"):
            for i in range(NDSEM):
                k = "d_%s%d" % (q, i)
                self.sems[k] = self.stack.enter_context(nc.semaphore(k))
                self.dsem_use[k] = 0
            self.dq_next[q] = 0
        self.nbuf = 0

    def sb(self, name, shape, dt):
        return self.stack.enter_context(self.nc.sbuf_tensor(name, list(shape), dt))

    def ps(self, name, shape, dt=F32):
        return self.stack.enter_context(self.nc.psum_tensor(name, list(shape), dt))

    def buf(self, name=None, psum=None):
        self.nbuf += 1
        name = name or "b%d" % self.nbuf
        if psum is None:
            psum = name.startswith("ps")
        return Buf(name, psum)

    def _deps(self, eng, reads, writes, is_dma):
        w = {}

        def add(t):
            if t is None:
                return
            k, v = t
            if w.get(k, 0) < v:
                w[k] = v
        for b in reads:
            add(b.lw)
            if b.psum:
                for k, v in b.rd.items():
                    if k != eng:
                        add((k, v))
        for b in writes:
            if b.lw is not None:
                if not (eng == "pe" and b.lw[0] == "pe" and not is_dma):
                    add(b.lw)
            for k, v in b.rd.items():
                if k == eng and not is_dma and eng != "pool":
                    continue
                add((k, v))
        seen = self.seen[eng]
        out = []
        for k, v in w.items():
            if seen.get(k, 0) < v:
                seen[k] = v
                out.append((k, v))
        return out

    def _commit(self, ticket, reads, writes):
        for b in writes:
            b.lw = ticket
            b.rd = {}
        for b in reads:
            k, v = ticket
            if b.rd.get(k, 0) < v:
                b.rd[k] = v

    def op(self, eng, fn, reads=(), writes=()):
        waits = self._deps(eng, reads, writes, False)
        self.cnt[eng] += 1
        ticket = (eng, self.cnt[eng])
        self.ops[eng].append((waits, fn, (eng, 1)))
        self._commit(ticket, reads, writes)
        return ticket

    def dma(self, q, fn, reads=(), writes=()):
        i = self.dq_next[q]
        self.dq_next[q] = (i + 1) % NDSEM
        k = "d_%s%d" % (q, i)
        waits = self._deps(q, reads, writes, True)
        prev = self.dsem_use[k]
        if prev > 0 and self.seen[q].get(k, 0) < 16 * prev:
            self.seen[q][k] = 16 * prev
            waits.append((k, 16 * prev))
        self.dsem_use[k] = prev + 1
        ticket = (k, 16 * (prev + 1))
        self.ops[q].append((waits, fn, (k, 16)))
        self._commit(ticket, reads, writes)
        return ticket

    def finish_wait(self, eng, tickets):
        waits = []
        for k, v in tickets:
            if self.seen[eng].get(k, 0) < v:
                self.seen[eng][k] = v
                waits.append((k, v))
        self.ops[eng].append((waits, None, None))

    def emit(self):
        nc = self.nc
        sems = self.sems
        ops = self.ops

        def replay(e, h):
            for waits, fn, inc in ops[e]:
                for k, v in waits:
                    h.wait_ge(sems[k], v)
                if fn is not None:
                    ins = fn(h)
                    ins.then_inc(sems[inc[0]], inc[1])

        with nc.Block() as block:
            @block.sync
            def _(h):
                replay("sp", h)

            @block.scalar
            def _(h):
                replay("act", h)

            @block.vector
            def _(h):
                replay("dve", h)

            @block.gpsimd
            def _(h):
                replay("pool", h)

            @block.tensor
            def _(h):
                replay("pe", h)
        self.stack.close()


def _mm(P, out, lhsT, rhs, start, stop, reads, writes):
    return P.op("pe", lambda h: h.matmul(out, lhsT=lhsT, rhs=rhs, start=start, stop=stop), reads=reads, writes=writes)


def _tr(P, out, in_, ident, reads, writes):
    return P.op("pe", lambda h: h.transpose(out, in_, ident), reads=reads, writes=writes)


def _act(P, out, in_, func, reads, writes, scale=None, bias=None):
    kw = {}
    if scale is not None:
        kw["scale"] = scale
    if bias is not None:
        kw["bias"] = bias
    return P.op("act", lambda h: h.activation(out=out, in_=in_, func=func, **kw), reads=reads, writes=writes)


def _tt(P, eng, out, in0, in1, op, reads, writes):
    return P.op(eng, lambda h: h.tensor_tensor(out=out, in0=in0, in1=in1, op=op), reads=reads, writes=writes)


def _ts(P, eng, out, in0, s1, s2, op0, op1, reads, writes):
    if op1 is None:
        return P.op(eng, lambda h: h.tensor_scalar(out=out, in0=in0, scalar1=s1, scalar2=None, op0=op0), reads=reads, writes=writes)
    return P.op(eng, lambda h: h.tensor_scalar(out=out, in0=in0, scalar1=s1, scalar2=s2, op0=op0, op1=op1), reads=reads, writes=writes)


def _stt(P, out, in0, scalar, in1, op0, op1, reads, writes):
    return P.op("dve", lambda h: h.scalar_tensor_tensor(out=out, in0=in0, scalar=scalar, in1=in1, op0=op0, op1=op1), reads=reads, writes=writes)


def _cp(P, eng, out, in_, reads, writes):
    if eng == "act":
        return P.op("act", lambda h: h.activation(out=out, in_=in_, func=AF.Copy), reads=reads, writes=writes)
    return P.op(eng, lambda h: h.tensor_copy(out=out, in_=in_), reads=reads, writes=writes)


def _ld(P, q, out, in_, writes, reads=()):
    return P.dma(q, lambda h: h.dma_start(out=out, in_=in_), reads=reads, writes=writes)


D = 1024
KC = 8
EPS = 1e-6


def arena_init(P, nbytes=206 * 1024):
    lo, hi = P.nc.bump_sbuf(nbytes)
    P.a_lo, P.a_hi, P.a_cur = lo, hi, lo
    P.a_n = 0


def A(P, shape, dt):
    nb = int(np.prod(shape[1:])) * (4 if dt in (F32, I32) else 2)
    off = (P.a_cur + 31) // 32 * 32
    assert off + nb <= P.a_hi, ("SBUF arena overflow", off + nb - P.a_lo)
    P.a_cur = off + nb
    P.a_n += 1
    return P.nc.alloc_sbuf_tensor_at("t%d" % P.a_n, list(shape), dt, offset=off)


def barrier(P):
    tick = [(e, P.cnt[e]) for e in ENGS if P.cnt[e] > 0]
    tick += [(k, 16 * v) for k, v in P.dsem_use.items() if v > 0]
    for e in ENGS:
        P.finish_wait(e, tick)


def rms_rstd(P, src, srcb, n, sq, sqb, ss_ps, ssb, rstd, rstdb, ones):
    P.op("act", lambda h: h.activation(out=sq[:, :, 0:n], in_=src[:, :, 0:n], func=AF.Square), reads=[srcb], writes=[sqb])
    for k in range(KC):
        P.op("pe", lambda h, k=k: h.matmul(ss_ps[:, 0:n], lhsT=ones[:], rhs=sq[:, k, 0:n], start=(k == 0), stop=(k == KC - 1)),
             reads=[sqb], writes=[ssb])
    P.op("act", lambda h: h.activation(out=rstd[:, 0:n], in_=ss_ps[:, 0:n], func=AF.Ln, scale=1.0 / D, bias=P.eps_t[:, 0:1]), reads=[ssb], writes=[rstdb])
    P.op("act", lambda h: h.activation(out=rstd[:, 0:n], in_=rstd[:, 0:n], func=AF.Exp, scale=-0.5), reads=[rstdb], writes=[rstdb])


def norm_mod(P, src, srcb, n, rstd, rstdb, gm, sh, r, dst, dstb, tmp, tmpb, dst_off=0, dst32=None, dst32b=None):
    for k in range(KC):
        tb = tmpb[k % len(tmp)]
        tt = tmp[k % len(tmp)]
        P.op("dve", lambda h, k=k, tt=tt: h.tensor_tensor(out=tt[:, 0:n], in0=src[:, k, 0:n], in1=rstd[:, 0:n], op=ALU.mult),
             reads=[srcb, rstdb], writes=[tb])
        P.op("act", lambda h, k=k, tt=tt: h.activation(out=dst[:, k, dst_off:dst_off + n], in_=tt[:, 0:n], func=AF.Identity,
                                                     scale=gm[:, k, r:r + 1], bias=sh[:, k, r:r + 1]),
             reads=[tb, P.modb], writes=[dstb])
        if dst32 is not None:
            P.op("pool", lambda h, k=k, tt=tt: h.tensor_scalar(out=dst32[:, k, 0:n], in0=tt[:, 0:n], scalar1=gm[:, k, r:r + 1],
                                                             scalar2=sh[:, k, r:r + 1], op0=ALU.mult, op1=ALU.add),
                 reads=[tb, P.modb], writes=[dst32b])


def compute_mod(P, dr, which, mod_ps, modpb):
    nc = P.nc
    cs = A(P, [128, KC, 2], F32)
    csb = P.buf()
    P.dma("sp", lambda h: h.dma_start(out=cs[:], in_=dr["condT"][:, :, :]), writes=[csb])
    sig = A(P, [128, KC, 2], F32)
    P.op("act", lambda h: h.activation(out=sig[:], in_=cs[:], func=AF.Sigmoid), reads=[csb], writes=[csb])
    P.op("dve", lambda h: h.tensor_tensor(out=cs[:], in0=cs[:], in1=sig[:], op=ALU.mult), reads=[csb], writes=[csb])
    modT = A(P, [128, 48, 2], F32)
    P.modT = modT
    P.modb = P.buf("mod")
    bm = A(P, [128, 48, 2], F32)
    bmb = P.buf()
    P.dma("sp", lambda h: h.dma_start(out=bm[:], in_=dr["b_modT"][:, :, :]), writes=[bmb])
    ng = A(P, [128, 4, KC, 2], F32)
    P.ng = ng
    P.dma("sp", lambda h: h.dma_start(out=ng[:], in_=dr["norm_gT"][:, :, :, :]), writes=[P.modb])
    mark = P.a_cur
    wm = [A(P, [128, KC, 1024], F32) for _ in range(2)]
    wmb = [P.buf(), P.buf()]
    wsrc = dr["w_mod"].rearrange("(k p) f -> p k f", p=128)
    for i, j in enumerate(which):
        w = wm[i % 2]
        wb = wmb[i % 2]
        for k2 in range(2):
            P.dma("sp", lambda h, w=w, j=j, k2=k2: h.dma_start(out=w[:, 4 * k2:4 * k2 + 4, :], in_=wsrc[:, 4 * k2:4 * k2 + 4, j * 1024:(j + 1) * 1024]), writes=[wb])
        for fc in range(8):
            for k in range(KC):
                P.op("pe", lambda h, w=w, j=j, fc=fc, k=k: h.matmul(mod_ps[:, j * 8 + fc, :], lhsT=w[:, k, fc * 128:(fc + 1) * 128], rhs=cs[:, k, :],
                                                                   start=(k == 0), stop=(k == KC - 1)), reads=[wb, csb], writes=[modpb])
    for j in which:
        P.op("dve", lambda h, j=j: h.tensor_tensor(out=modT[:, j * 8:(j + 1) * 8, :], in0=mod_ps[:, j * 8:(j + 1) * 8, :], in1=bm[:, j * 8:(j + 1) * 8, :], op=ALU.add),
             reads=[modpb, bmb], writes=[P.modb])
    barrier(P)
    P.a_cur = mark


def mod_derived(P, jsc, jg, gi_norm, gi_gate):
    gm = A(P, [128, KC, 2], F32)
    gg = A(P, [128, KC, 2], F32)
    modT, ng = P.modT, P.ng
    P.op("dve", lambda h: h.scalar_tensor_tensor(out=gm[:], in0=modT[:, jsc * 8:(jsc + 1) * 8, :], scalar=1.0, in1=ng[:, gi_norm, :, :],
                                                 op0=ALU.add, op1=ALU.mult), reads=[P.modb], writes=[P.modb])
    if jg is not None:
        P.op("dve", lambda h: h.tensor_tensor(out=gg[:], in0=modT[:, jg * 8:(jg + 1) * 8, :], in1=ng[:, gi_gate, :, :], op=ALU.mult),
             reads=[P.modb], writes=[P.modb])
    return gm, gg


def build_F(blocks, blocksB, n_exp, dff, moe, DBG=False, mode='full'):
    TT = sum(b[1] for b in blocks)
    nc = bass.Bass("TRN2", target_bir_lowering=False)
    dr = {}

    def din(name, shape, dt=F32):
        dr[name] = nc.dram_tensor(name, list(shape), dt, kind="ExternalInput").ap()
    din("xT", [KC, 128, TT])
    din("brT", [KC, 128, TT], BF16)
    din("condT", [128, KC, 2])
    din("w_mod", [D, 6 * D])
    din("b_modT", [128, 48, 2])
    din("norm_gT", [128, 4, KC, 2])
    din("w_in", [D, 6400])
    din("w_br", [KC * 128, D])
    din("w_o", [D, D])
    din("w_glu", [256, 256])
    din("b_gluT", [128, 2])
    if mode == 'full':
        din("w_g", [n_exp, D, dff])
        din("w_u", [n_exp, D, dff])
        din("w_d", [n_exp, dff, D])
    if moe:
        din("w_r", [D, 8])
        din("b_r", [128, 8])
        din("ident", [128, 128])
        din("sel", [8, 8, 128])
    if mode == 'moe_a':
        h2o = nc.dram_tensor("h2o", [KC, 128, TT], BF16, kind="ExternalOutput").ap().rearrange("k p t -> p k t")
        cbo = nc.dram_tensor("cbo", [8, TT], BF16, kind="ExternalOutput").ap()
        mko = nc.dram_tensor("mko", [8, TT], BF16, kind="ExternalOutput").ap()
    xo = nc.dram_tensor("xo", [KC, 128, TT], F32, kind="ExternalOutput").ap()
    xoT = xo.rearrange("k p t -> p k t")
    if DBG: dbg_mod = nc.dram_tensor("dbg_mod", [128, 48, 2], F32, kind="ExternalOutput").ap()
    if DBG: dbg_xm = nc.dram_tensor("dbg_xm", [KC, 128, TT], F32, kind="ExternalOutput").ap().rearrange("k p t -> p k t")
    if DBG: dbg_h = nc.dram_tensor("dbg_h", [KC, 128, TT], BF16, kind="ExternalOutput").ap().rearrange("k p t -> p k t")
    if DBG: dbg_z = nc.dram_tensor("dbg_z", [KC, 128, TT], F32, kind="ExternalOutput").ap().rearrange("k p t -> p k t")
    if DBG: dbg_r = nc.dram_tensor("dbg_r", [128, TT], F32, kind="ExternalOutput").ap()
    if DBG: dbg_sq = nc.dram_tensor("dbg_sq", [KC, 128, TT], BF16, kind="ExternalOutput").ap().rearrange("k p t -> p k t")
    if DBG: dbg_ss = nc.dram_tensor("dbg_ss", [128, TT], F32, kind="ExternalOutput").ap()
    sscp = A(P, [128, 256], F32) if False else None
    if DBG: dbg_y = nc.dram_tensor("dbg_y", [KC, 128, TT], BF16, kind="ExternalOutput").ap().rearrange("k p t -> p k t")
    xT = dr["xT"].rearrange("k p t -> p k t")
    brT = dr["brT"].rearrange("k p t -> p k t")

    P = Prog(nc)
    arena_init(P)
    ps = [P.ps("ps%d" % i, [128, 512], F32) for i in range(8)]
    psb = [P.buf("ps%d" % i) for i in range(8)]
    ones = A(P, [128, 128], BF16)
    onesb = P.buf()
    P.op("dve", lambda h: h.memset(ones[:], 1.0), writes=[onesb])
    P.eps_t = A(P, [128, 1], F32)
    P.op("dve", lambda h: h.memset(P.eps_t[:], EPS), writes=[onesb])

    mod_ps = nc.alloc_psum_tensor
    mod_view = ps[7][:, 0:96].rearrange("p (j r) -> p j r", r=2)
    compute_mod(P, dr, [0, 1, 2, 3, 4, 5], mod_view, psb[7])
    gm_a, gg_a = mod_derived(P, 1, 2, 0, 1)
    gm_f, gg_f = mod_derived(P, 4, 5, 2, 3)
    sh_a = P.modT[:, 0:8, :]
    sh_f = P.modT[:, 24:32, :]
    P.dbgt = []
    if DBG: P.dbgt += [P.dma("sp", lambda h: h.dma_start(out=dbg_mod[:, :, :], in_=P.modT[:]), reads=[P.modb], writes=[P.buf()])]

    h2 = A(P, [128, KC, TT], BF16)
    h2b = P.buf("h2")
    if moe:
        cbT = A(P, [8, TT], BF16)
        cbTb = P.buf("cbT")
        mkT = A(P, [8, TT], BF16)
        mkTb = P.buf("mkT")
        ident = A(P, [128, 128], F32)
        P.dma("sp", lambda h: h.dma_start(out=ident[:], in_=dr["ident"][:, :]), writes=[onesb])
        wr = A(P, [128, KC, 8], F32)
        P.dma("sp", lambda h: h.dma_start(out=wr[:], in_=dr["w_r"].rearrange("(k p) e -> p k e", p=128)), writes=[onesb])
        br_t = A(P, [128, 8], F32)
        P.dma("sp", lambda h: h.dma_start(out=br_t[:], in_=dr["b_r"][:, :]), writes=[onesb])
        sel = A(P, [8, 8, 128], BF16)
        P.dma("pool", lambda h: h.dma_start(out=sel[:], in_=dr["sel"][:, :, :]), writes=[onesb])
    markA = P.a_cur
    wgt = A(P, [128, KC, 4096], BF16)
    wbr = A(P, [128, KC, D], BF16)
    wo = A(P, [128, KC, D], BF16)
    wAb = P.buf("wA")
    w_in_v = dr["w_in"].rearrange("(k p) c -> p k c", p=128)
    for k in range(KC):
        for c4 in range(2):
            P.dma("pool", lambda h, k=k, c4=c4: h.dma_start(out=wgt[:, k, c4 * 2048:(c4 + 1) * 2048], in_=w_in_v[:, k, 2304 + c4 * 2048:2304 + (c4 + 1) * 2048]), writes=[wAb])
    P.dma("pool", lambda h: h.dma_start(out=wbr[:, 0:4, :], in_=dr["w_br"].rearrange("(k p) c -> p k c", p=128)[:, 0:4, :]), writes=[wAb])
    P.dma("pool", lambda h: h.dma_start(out=wbr[:, 4:8, :], in_=dr["w_br"].rearrange("(k p) c -> p k c", p=128)[:, 4:8, :]), writes=[wAb])
    P.dma("pool", lambda h: h.dma_start(out=wo[:, 0:4, :], in_=dr["w_o"].rearrange("(k p) c -> p k c", p=128)[:, 0:4, :]), writes=[wAb])
    P.dma("pool", lambda h: h.dma_start(out=wo[:, 4:8, :], in_=dr["w_o"].rearrange("(k p) c -> p k c", p=128)[:, 4:8, :]), writes=[wAb])

    wglu = A(P, [128, 2, 256], BF16)
    bglu = A(P, [128, 2], F32)
    P.dma("pool", lambda h: h.dma_start(out=wglu[:], in_=dr["w_glu"].rearrange("(k p) c -> p k c", p=128)), writes=[wAb])
    P.dma("sp", lambda h: h.dma_start(out=bglu[:], in_=dr["b_gluT"][:, :]), writes=[wAb])
    glu_t = A(P, [128, 2, 256], BF16)
    glub = P.buf("glu")
    sgl = A(P, [128, 256], F32)
    sglb = P.buf("sgl")
    xb = [A(P, [128, KC, 256], F32) for _ in range(2)]
    xbb = [P.buf(), P.buf()]
    brb_t = [A(P, [128, KC, 256], BF16) for _ in range(2)]
    brbb = [P.buf(), P.buf()]
    sq = A(P, [128, KC, 256], BF16)
    sqb = P.buf()
    rstd = A(P, [128, 256], F32)
    rstdb = P.buf()
    tmp = [A(P, [128, 256], F32) for _ in range(2)]
    tmpb = [P.buf(), P.buf()]
    hb = A(P, [128, KC, 256], BF16)
    hbb = P.buf()
    yb = A(P, [128, KC, 256], BF16)
    ybb = P.buf()
    zb = A(P, [128, KC, 256], F32)
    zbb = P.buf()
    sg = [A(P, [128, 256], F32) for _ in range(2)]
    sgb = [P.buf(), P.buf()]
    tt2 = [A(P, [128, 256], F32) for _ in range(2)]
    tt2b = [P.buf(), P.buf()]
    accA = [A(P, [128, 256], F32) for _ in range(2)]
    accAb = [P.buf(), P.buf()]
    xob = P.buf("xo")
    h2fb = P.buf("h2f")
    if moe:
        h2f = A(P, [128, KC, 256], F32)
        lg = A(P, [128, 8], F32)
        mx8 = A(P, [128, 8], F32)
        msk = A(P, [128, 8], F32)
        ex = A(P, [128, 8], F32)
        den = A(P, [128, 1], F32)
        nmx = A(P, [128, 1], F32)
        rb = P.buf("router")
    cnt = 0
    P.sscp = A(P, [128, 256], F32)
    P.sscpb = P.buf()
    def _ldA(bi_):
        t0_, n_, _r = blocks[bi_]
        xx, xxb = xb[bi_ % 2], xbb[bi_ % 2]
        bb_, bbb = brb_t[bi_ % 2], brbb[bi_ % 2]
        P.dma("sp", lambda h: h.dma_start(out=xx[:, 0:4, 0:n_], in_=xT[:, 0:4, t0_:t0_ + n_]), writes=[xxb])
        P.dma("sp", lambda h: h.dma_start(out=xx[:, 4:8, 0:n_], in_=xT[:, 4:8, t0_:t0_ + n_]), writes=[xxb])
        P.dma("sp", lambda h: h.dma_start(out=bb_[:, :, 0:n_], in_=brT[:, :, t0_:t0_ + n_]), writes=[bbb])
    _ldA(0)
    for bi, (t0, n, r) in enumerate(blocks):
        x_t, x_b = xb[bi % 2], xbb[bi % 2]
        b_t, b_b = brb_t[bi % 2], brbb[bi % 2]
        if bi + 1 < len(blocks):
            _ldA(bi + 1)
        rms_rstd(P, x_t, x_b, n, sq, sqb, ps[6], psb[6], rstd, rstdb, ones)
        norm_mod(P, x_t, x_b, n, rstd, rstdb, gm_a, sh_a, r, hb, hbb, tmp, tmpb)
        for oc in range(2):
            for kc in range(2):
                P.op("pe", lambda h, oc=oc, kc=kc, n=n, b_t=b_t: h.matmul(ps[6][:, 0:n], lhsT=wglu[:, kc, oc * 128:(oc + 1) * 128], rhs=b_t[:, 2 + kc, 0:n],
                                                                      start=(kc == 0), stop=(kc == 1)), reads=[wAb, b_b], writes=[psb[6]])
            P.op("act", lambda h, oc=oc, n=n: h.activation(out=sgl[:, 0:n], in_=ps[6][:, 0:n], func=AF.Sigmoid, bias=bglu[:, oc:oc + 1], scale=1.0), reads=[psb[6], wAb], writes=[sglb])
            P.op("dve", lambda h, oc=oc, n=n, b_t=b_t: h.tensor_tensor(out=glu_t[:, oc, 0:n], in0=sgl[:, 0:n], in1=b_t[:, 2 + oc, 0:n], op=ALU.mult), reads=[sglb, b_b], writes=[glub])
        for fc in range(8):
            ac, acb = accA[fc % 2], accAb[fc % 2]
            for b in range(4):
                gi = cnt % 2
                cnt += 1
                gps, gpb = ps[gi], psb[gi]
                pps, ppb = ps[2 + gi], psb[2 + gi]
                for k in range(KC):
                    P.op("pe", lambda h, gps=gps, k=k, b=b, fc=fc, n=n: h.matmul(gps[:, 0:n], lhsT=wgt[:, k, b * 1024 + fc * 128:b * 1024 + (fc + 1) * 128], rhs=hb[:, k, 0:n],
                                                                                 start=(k == 0), stop=(k == KC - 1)), reads=[wAb, hbb], writes=[gpb])
                for hh in range(2):
                    rhs_ap = glu_t[:, hh, 0:n] if b == 1 else b_t[:, 2 * b + hh, 0:n]
                    P.op("pe", lambda h, pps=pps, hh=hh, b=b, fc=fc, n=n, rhs_ap=rhs_ap: h.matmul(pps[:, 0:n], lhsT=wbr[:, 2 * b + hh, fc * 128:(fc + 1) * 128], rhs=rhs_ap,
                                                                                          start=(hh == 0), stop=(hh == 1)), reads=[wAb, b_b, glub], writes=[ppb])
                s_t, s_b = sg[gi], sgb[gi]
                P.op("act", lambda h, s_t=s_t, gps=gps, n=n: h.activation(out=s_t[:, 0:n], in_=gps[:, 0:n], func=AF.Sigmoid), reads=[gpb], writes=[s_b])
                if b == 0:
                    P.op("dve", lambda h, ac=ac, s_t=s_t, pps=pps, n=n: h.tensor_tensor(out=ac[:, 0:n], in0=s_t[:, 0:n], in1=pps[:, 0:n], op=ALU.mult),
                         reads=[s_b, ppb], writes=[acb])
                else:
                    t_t, t_b = tt2[gi], tt2b[gi]
                    P.op("dve", lambda h, t_t=t_t, s_t=s_t, pps=pps, n=n: h.tensor_tensor(out=t_t[:, 0:n], in0=s_t[:, 0:n], in1=pps[:, 0:n], op=ALU.mult),
                         reads=[s_b, ppb], writes=[t_b])
                    if b < 3:
                        P.op("pool", lambda h, ac=ac, t_t=t_t, n=n: h.tensor_tensor(out=ac[:, 0:n], in0=ac[:, 0:n], in1=t_t[:, 0:n], op=ALU.add),
                             reads=[acb, t_b], writes=[acb])
                    else:
                        P.op("pool", lambda h, ac=ac, t_t=t_t, n=n, fc=fc: h.tensor_tensor(out=yb[:, fc, 0:n], in0=ac[:, 0:n], in1=t_t[:, 0:n], op=ALU.add),
                             reads=[acb, t_b], writes=[ybb])
        for fc in range(8):
            zi = 4 + fc % 2
            for k in range(KC):
                P.op("pe", lambda h, zi=zi, k=k, fc=fc, n=n: h.matmul(ps[zi][:, 0:n], lhsT=wo[:, k, fc * 128:(fc + 1) * 128], rhs=yb[:, k, 0:n], start=(k == 0), stop=(k == KC - 1)),
                     reads=[wAb, ybb], writes=[psb[zi]])
            P.op("act", lambda h, zi=zi, fc=fc, n=n: h.activation(out=zb[:, fc, 0:n], in_=ps[zi][:, 0:n], func=AF.Copy), reads=[psb[zi]], writes=[zbb])
        rms_rstd(P, zb, zbb, n, sq, sqb, ps[6], psb[6], rstd, rstdb, ones)
        if DBG: P.dbgt.append(P.dma("sp", lambda h, t0=t0, n=n: h.dma_start(out=dbg_z[:, :, t0:t0 + n], in_=zb[:, :, 0:n]), reads=[zbb], writes=[P.buf()]))
        if DBG: P.dbgt.append(P.dma("sp", lambda h, t0=t0, n=n: h.dma_start(out=dbg_r[:, t0:t0 + n], in_=rstd[:, 0:n]), reads=[rstdb], writes=[P.buf()]))
        if DBG: P.dbgt.append(P.dma("sp", lambda h, t0=t0, n=n: h.dma_start(out=dbg_sq[:, :, t0:t0 + n], in_=sq[:, :, 0:n]), reads=[sqb], writes=[P.buf()]))
        if DBG: P.op("dve", lambda h, n=n: h.tensor_copy(out=P.sscp[:, 0:n], in_=ps[6][:, 0:n]), reads=[psb[6]], writes=[P.sscpb])
        if DBG: P.dbgt.append(P.dma("sp", lambda h, t0=t0, n=n: h.dma_start(out=dbg_ss[:, t0:t0 + n], in_=P.sscp[:, 0:n]), reads=[P.sscpb], writes=[P.buf()]))
        for k in range(KC):
            tb_, tt_ = tmpb[k % 2], tmp[k % 2]
            P.op("dve", lambda h, k=k, tt_=tt_, n=n: h.tensor_tensor(out=tt_[:, 0:n], in0=zb[:, k, 0:n], in1=rstd[:, 0:n], op=ALU.mult), reads=[zbb, rstdb], writes=[tb_])
            P.op("dve", lambda h, k=k, tt_=tt_, n=n, x_t=x_t, r=r: h.scalar_tensor_tensor(out=x_t[:, k, 0:n], in0=tt_[:, 0:n], scalar=gg_a[:, k, r:r + 1], in1=x_t[:, k, 0:n],
                                                                                    op0=ALU.mult, op1=ALU.add), reads=[tb_, P.modb, x_b], writes=[x_b])
        P.dma("sp", lambda h, x_t=x_t, t0=t0, n=n: h.dma_start(out=xoT[:, :, t0:t0 + n], in_=x_t[:, :, 0:n]), reads=[x_b], writes=[xob])
        if DBG: P.dbgt.append(P.dma("sp", lambda h, x_t=x_t, t0=t0, n=n: h.dma_start(out=dbg_xm[:, :, t0:t0 + n], in_=x_t[:, :, 0:n]), reads=[x_b], writes=[P.buf()]))
        if DBG: P.dbgt.append(P.dma("sp", lambda h, t0=t0, n=n: h.dma_start(out=dbg_h[:, :, t0:t0 + n], in_=hb[:, :, 0:n]), reads=[hbb], writes=[P.buf()]))
        if DBG: P.dbgt.append(P.dma("sp", lambda h, t0=t0, n=n: h.dma_start(out=dbg_y[:, :, t0:t0 + n], in_=yb[:, :, 0:n]), reads=[ybb], writes=[P.buf()]))
        rms_rstd(P, x_t, x_b, n, sq, sqb, ps[6], psb[6], rstd, rstdb, ones)
        norm_mod(P, x_t, x_b, n, rstd, rstdb, gm_f, sh_f, r, h2, h2b, tmp, tmpb, dst_off=t0, dst32=(h2f if moe else None), dst32b=h2fb)
        if moe:
            for tt in range(n // 128):
                for k in range(KC):
                    P.op("pe", lambda h, k=k, tt=tt: h.matmul(ps[7][:, 0:8], lhsT=h2f[:, k, tt * 128:(tt + 1) * 128], rhs=wr[:, k, :], start=(k == 0), stop=(k == KC - 1)),
                         reads=[h2fb, onesb], writes=[psb[7]])
                P.op("dve", lambda h: h.tensor_tensor(out=lg[:], in0=ps[7][:, 0:8], in1=br_t[:], op=ALU.add), reads=[psb[7], onesb], writes=[rb])
                P.op("dve", lambda h: h.max(out=mx8[:], in_=lg[:]), reads=[rb], writes=[rb])
                P.op("dve", lambda h: h.tensor_scalar(out=msk[:], in0=lg[:], scalar1=mx8[:, 1:2], scalar2=None, op0=ALU.is_ge), reads=[rb], writes=[rb])
                P.op("dve", lambda h: h.tensor_scalar(out=nmx[:], in0=mx8[:, 0:1], scalar1=-1.0, scalar2=None, op0=ALU.mult), reads=[rb], writes=[rb])
                P.op("act", lambda h: h.activation(out=ex[:], in_=lg[:], func=AF.Exp, bias=nmx[:, 0:1], scale=1.0), reads=[rb], writes=[rb])
                P.op("dve", lambda h: h.tensor_tensor(out=ex[:], in0=ex[:], in1=msk[:], op=ALU.mult), reads=[rb], writes=[rb])
                P.op("dve", lambda h: h.reduce_sum(out=den[:], in_=ex[:], axis=AX.X), reads=[rb], writes=[rb])
                P.op("dve", lambda h: h.reciprocal(out=den[:], in_=den[:]), reads=[rb], writes=[rb])
                P.op("dve", lambda h: h.tensor_scalar(out=ex[:], in0=ex[:], scalar1=den[:, 0:1], scalar2=None, op0=ALU.mult), reads=[rb], writes=[rb])
                P.op("pe", lambda h: h.transpose(ps[7][0:8, 128:256], ex[:], ident[:]), reads=[rb, onesb], writes=[psb[7]])
                P.op("act", lambda h, t0=t0, tt=tt: h.activation(out=cbT[:, t0 + tt * 128:t0 + (tt + 1) * 128], in_=ps[7][0:8, 128:256], func=AF.Copy), reads=[psb[7]], writes=[cbTb])
                if mode == 'moe_a':
                    P.op("pe", lambda h: h.transpose(ps[7][0:8, 256:384], msk[:], ident[:]), reads=[rb, onesb], writes=[psb[7]])
                    P.op("act", lambda h, t0=t0, tt=tt: h.activation(out=mkT[:, t0 + tt * 128:t0 + (tt + 1) * 128], in_=ps[7][0:8, 256:384], func=AF.Copy), reads=[psb[7]], writes=[mkTb])
    barrier(P)
    P.a_cur = markA
    if mode == 'moe_a':
        fin = [P.dma("sp", lambda h: h.dma_start(out=h2o[:, :, :], in_=h2[:, :, :]), reads=[h2b], writes=[P.buf()]),
               P.dma("sp", lambda h: h.dma_start(out=cbo[:, :], in_=cbT[:, :]), reads=[cbTb], writes=[P.buf()]),
               P.dma("sp", lambda h: h.dma_start(out=mko[:, :], in_=mkT[:, :]), reads=[mkTb], writes=[P.buf()])]
        barrier(P)
        P.finish_wait("sp", fin + P.dbgt)
        P.emit()
        return nc
    blocks = blocksB
    acc = A(P, [128, KC, TT], F32)
    accb = [P.buf() for _ in blocks]
    markB = P.a_cur
    NSL = 4
    wg_s = [A(P, [128, KC, NSL * 128], BF16) for _ in range(2)]
    wu_s = [A(P, [128, KC, NSL * 128], BF16) for _ in range(2)]
    wd_s = [A(P, [128, NSL, D], BF16) for _ in range(2)]
    wsb = [P.buf(), P.buf()]
    hid = [A(P, [128, NSL, 512], BF16) for _ in range(2)]
    hidb = [P.buf(), P.buf()]
    ssb_t = [A(P, [128, 512], F32) for _ in range(2)]
    ssbb = [P.buf(), P.buf()]
    cbe = A(P, [128, 512], BF16)
    cbeb = P.buf()
    ntile = dff // 128
    slices = [(s0, min(NSL, ntile - s0)) for s0 in range(0, ntile, NSL)]
    si = 0
    hcnt = 0
    gcnt = 0
    work = [(e, s0, ns) for e in range(n_exp) for (s0, ns) in slices]

    def _ldW(widx):
        e_, s0_, ns_ = work[widx]
        wi_ = widx % 2
        wgv_ = dr["w_g"][e_].rearrange("(k p) f -> p k f", p=128)
        wuv_ = dr["w_u"][e_].rearrange("(k p) f -> p k f", p=128)
        wdv_ = dr["w_d"][e_].rearrange("(j p) c -> p j c", p=128)
        for k2 in range(2):
            P.dma("pool", lambda h, k2=k2: h.dma_start(out=wg_s[wi_][:, 4 * k2:4 * k2 + 4, 0:ns_ * 128], in_=wgv_[:, 4 * k2:4 * k2 + 4, s0_ * 128:(s0_ + ns_) * 128]), writes=[wsb[wi_]])
            P.dma("pool", lambda h, k2=k2: h.dma_start(out=wu_s[wi_][:, 4 * k2:4 * k2 + 4, 0:ns_ * 128], in_=wuv_[:, 4 * k2:4 * k2 + 4, s0_ * 128:(s0_ + ns_) * 128]), writes=[wsb[wi_]])
        for j in range(ns_):
            P.dma("pool", lambda h, j=j: h.dma_start(out=wd_s[wi_][:, j, :], in_=wdv_[:, s0_ + j, :]), writes=[wsb[wi_]])
    _ldW(0)
    for widx, (e, s0, ns) in enumerate(work):
        if True:
            wi = widx % 2
            if widx + 1 < len(work):
                _ldW(widx + 1)
            for bi, (t0, n, r) in enumerate(blocks):
                hi = hcnt % 2
                hcnt += 1
                if moe:
                    P.op("pe", lambda h, e=e, t0=t0, n=n: h.matmul(ps[7][:, 0:n], lhsT=sel[:, e, :], rhs=cbT[:, t0:t0 + n], start=True, stop=True), reads=[cbTb, onesb], writes=[psb[7]])
                    P.op("act", lambda h, n=n: h.activation(out=cbe[:, 0:n], in_=ps[7][:, 0:n], func=AF.Copy), reads=[psb[7]], writes=[cbeb])
                for j in range(ns):
                    gi = gcnt % 2
                    gcnt += 1
                    for k in range(KC):
                        P.op("pe", lambda h, gi=gi, wi=wi, j=j, k=k, t0=t0, n=n: h.matmul(ps[gi][:, 0:n], lhsT=wg_s[wi][:, k, j * 128:(j + 1) * 128], rhs=h2[:, k, t0:t0 + n], start=(k == 0), stop=(k == KC - 1)),
                             reads=[wsb[wi], h2b], writes=[psb[gi]])
                    for k in range(KC):
                        P.op("pe", lambda h, gi=gi, wi=wi, j=j, k=k, t0=t0, n=n: h.matmul(ps[2 + gi][:, 0:n], lhsT=wu_s[wi][:, k, j * 128:(j + 1) * 128], rhs=h2[:, k, t0:t0 + n], start=(k == 0), stop=(k == KC - 1)),
                             reads=[wsb[wi], h2b], writes=[psb[2 + gi]])
                    P.op("act", lambda h, gi=gi, n=n: h.activation(out=ssb_t[gi][:, 0:n], in_=ps[gi][:, 0:n], func=AF.Silu), reads=[psb[gi]], writes=[ssbb[gi]])
                    if moe:
                        P.op("dve", lambda h, gi=gi, n=n: h.tensor_tensor(out=ssb_t[gi][:, 0:n], in0=ssb_t[gi][:, 0:n], in1=ps[2 + gi][:, 0:n], op=ALU.mult),
                             reads=[ssbb[gi], psb[2 + gi]], writes=[ssbb[gi]])
                        P.op("pool", lambda h, gi=gi, hi=hi, j=j, n=n: h.tensor_tensor(out=hid[hi][:, j, 0:n], in0=ssb_t[gi][:, 0:n], in1=cbe[:, 0:n], op=ALU.mult),
                             reads=[ssbb[gi], cbeb], writes=[hidb[hi]])
                    else:
                        P.op("dve", lambda h, gi=gi, hi=hi, j=j, n=n: h.tensor_tensor(out=hid[hi][:, j, 0:n], in0=ssb_t[gi][:, 0:n], in1=ps[2 + gi][:, 0:n], op=ALU.mult),
                             reads=[ssbb[gi], psb[2 + gi]], writes=[hidb[hi]])
                first = (e == 0 and s0 == 0)
                for fc in range(8):
                    oi = 4 + fc % 2
                    for j in range(ns):
                        P.op("pe", lambda h, oi=oi, wi=wi, j=j, fc=fc, hi=hi, n=n, ns=ns: h.matmul(ps[oi][:, 0:n], lhsT=wd_s[wi][:, j, fc * 128:(fc + 1) * 128], rhs=hid[hi][:, j, 0:n], start=(j == 0), stop=(j == ns - 1)),
                             reads=[wsb[wi], hidb[hi]], writes=[psb[oi]])
                    if first:
                        P.op("act", lambda h, oi=oi, fc=fc, t0=t0, n=n: h.activation(out=acc[:, fc, t0:t0 + n], in_=ps[oi][:, 0:n], func=AF.Copy), reads=[psb[oi]], writes=[accb[bi]])
                    else:
                        P.op("dve", lambda h, oi=oi, fc=fc, t0=t0, n=n: h.tensor_tensor(out=acc[:, fc, t0:t0 + n], in0=acc[:, fc, t0:t0 + n], in1=ps[oi][:, 0:n], op=ALU.add),
                             reads=[psb[oi], accb[bi]], writes=[accb[bi]])
    barrier(P)
    P.a_cur = markB
    xm = [A(P, [128, KC, 512], F32) for _ in range(2)]
    xmb = [P.buf(), P.buf()]
    sqF = A(P, [128, KC, 512], BF16)
    rstdF = A(P, [128, 512], F32)
    tmpF = [A(P, [128, 512], F32) for _ in range(2)]
    outs = []
    for bi, (t0, n, r) in enumerate(blocks):
        x_t, x_b = xm[bi % 2], xmb[bi % 2]
        P.dma("sp", lambda h, x_t=x_t, t0=t0, n=n: h.dma_start(out=x_t[:, :, 0:n], in_=xoT[:, :, t0:t0 + n]), reads=[xob], writes=[x_b])
        accv = acc[:, :, t0:t0 + n]
        P.op("act", lambda h, accv=accv, n=n: h.activation(out=sqF[:, :, 0:n], in_=accv, func=AF.Square), reads=[accb[bi]], writes=[sqb])
        for k in range(KC):
            P.op("pe", lambda h, k=k, n=n: h.matmul(ps[6][:, 0:n], lhsT=ones[:], rhs=sqF[:, k, 0:n], start=(k == 0), stop=(k == KC - 1)), reads=[sqb, onesb], writes=[psb[6]])
        P.op("act", lambda h, n=n: h.activation(out=rstdF[:, 0:n], in_=ps[6][:, 0:n], func=AF.Ln, scale=1.0 / D, bias=P.eps_t[:, 0:1]), reads=[psb[6]], writes=[rstdb])
        P.op("act", lambda h, n=n: h.activation(out=rstdF[:, 0:n], in_=rstdF[:, 0:n], func=AF.Exp, scale=-0.5), reads=[rstdb], writes=[rstdb])
        for k in range(KC):
            tb_, tt_ = tmpb[k % 2], tmpF[k % 2]
            P.op("dve", lambda h, k=k, tt_=tt_, n=n, t0=t0: h.tensor_tensor(out=tt_[:, 0:n], in0=acc[:, k, t0:t0 + n], in1=rstdF[:, 0:n], op=ALU.mult), reads=[accb[bi], rstdb], writes=[tb_])
            P.op("dve", lambda h, k=k, tt_=tt_, n=n, x_t=x_t, r=r: h.scalar_tensor_tensor(out=x_t[:, k, 0:n], in0=tt_[:, 0:n], scalar=gg_f[:, k, r:r + 1], in1=x_t[:, k, 0:n],
                                                                                    op0=ALU.mult, op1=ALU.add), reads=[tb_, P.modb, x_b], writes=[x_b])
        outs.append(P.dma("sp", lambda h, x_t=x_t, t0=t0, n=n: h.dma_start(out=xoT[:, :, t0:t0 + n], in_=x_t[:, :, 0:n]), reads=[x_b], writes=[xob]))
    P.finish_wait("sp", outs + P.dbgt)
    P.emit()
    return nc


def build_E(groups=(4,) * 8, dff=3584):
    nc = bass.Bass("TRN2", target_bir_lowering=False)
    ngrp = len(groups)
    gtok = 512 * max(groups)
    NT = 512 * sum(groups)
    goff = [512 * sum(groups[:g]) for g in range(ngrp)]
    h2d = nc.dram_tensor("h2", [KC, 128, NT], BF16, kind="ExternalInput").ap().rearrange("k p t -> p k t")
    cbd = nc.dram_tensor("cbe", [128, NT], BF16, kind="ExternalInput").ap()
    wgd = nc.dram_tensor("w_g", [D, dff], F32, kind="ExternalInput").ap().rearrange("(k p) f -> p k f", p=128)
    wud = nc.dram_tensor("w_u", [D, dff], F32, kind="ExternalInput").ap().rearrange("(k p) f -> p k f", p=128)
    wdd = nc.dram_tensor("w_d", [dff, D], F32, kind="ExternalInput").ap().rearrange("(j p) c -> p j c", p=128)
    ye = nc.dram_tensor("ye", [KC, 128, NT], F32, kind="ExternalOutput").ap().rearrange("k p t -> p k t")
    P = Prog(nc)
    arena_init(P)
    ps = [P.ps("ps%d" % i, [128, 512], F32) for i in range(8)]
    psb = [P.buf("ps%d" % i) for i in range(8)]
    h2g = [A(P, [128, KC, gtok], BF16) for _ in range(2)]
    h2gb = [P.buf(), P.buf()]
    cbg = [A(P, [128, gtok], BF16) for _ in range(2)]
    acc = A(P, [128, KC, gtok], F32)
    NSL = 4
    wg_s = [A(P, [128, KC, NSL * 128], BF16) for _ in range(2)]
    wu_s = [A(P, [128, KC, NSL * 128], BF16) for _ in range(2)]
    wd_s = [A(P, [128, NSL, D], BF16) for _ in range(2)]
    wsb = [P.buf(), P.buf()]
    hid = [A(P, [128, NSL, 512], BF16) for _ in range(2)]
    hidb = [P.buf(), P.buf()]
    ssb_t = [A(P, [128, 512], F32) for _ in range(2)]
    ssbb = [P.buf(), P.buf()]
    ntile = dff // 128
    slices = [(s0, min(NSL, ntile - s0)) for s0 in range(0, ntile, NSL)]
    accb = [P.buf() for _ in range(max(groups))]
    si = hcnt = gcnt = 0
    outs = []
    work = [(g, sidx, s0, ns) for g in range(ngrp) for sidx, (s0, ns) in enumerate(slices)]

    def _ldG(g_):
        hg_, hgb_ = h2g[g_ % 2], h2gb[g_ % 2]
        gn_ = 512 * groups[g_]
        for k2 in range(2):
            _ldF(P, "sp", hg_[:, 4 * k2:4 * k2 + 4, 0:gn_], h2d[:, 4 * k2:4 * k2 + 4, goff[g_]:goff[g_] + gn_], [hgb_])
        _ldF(P, "sp", cbg[g_ % 2][:, 0:gn_], cbd[:, goff[g_]:goff[g_] + gn_], [hgb_])

    def _ldW(widx):
        _g, _sidx, s0_, ns_ = work[widx]
        wi_ = widx % 2
        for k2 in range(2):
            _ldF(P, "pool", wg_s[wi_][:, 4 * k2:4 * k2 + 4, 0:ns_ * 128], wgd[:, 4 * k2:4 * k2 + 4, s0_ * 128:(s0_ + ns_) * 128], [wsb[wi_]])
            _ldF(P, "pool", wu_s[wi_][:, 4 * k2:4 * k2 + 4, 0:ns_ * 128], wud[:, 4 * k2:4 * k2 + 4, s0_ * 128:(s0_ + ns_) * 128], [wsb[wi_]])
        for j in range(ns_):
            _ldF(P, "pool", wd_s[wi_][:, j, :], wdd[:, s0_ + j, :], [wsb[wi_]])
    _ldG(0)
    _ldW(0)
    for widx, (g, sidx, s0, ns) in enumerate(work):
        hg, hgb = h2g[g % 2], h2gb[g % 2]
        cg = cbg[g % 2]
        nblk = groups[g]
        if sidx == 0 and g + 1 < ngrp:
            _ldG(g + 1)
        if True:
            wi = widx % 2
            if widx + 1 < len(work):
                _ldW(widx + 1)
            for bi in range(nblk):
                t0, n = bi * 512, 512
                hi = hcnt % 2
                hcnt += 1
                for j in range(ns):
                    gi = gcnt % 2
                    gcnt += 1
                    for k in range(KC):
                        _mmF(P, ps[gi][:, 0:n], wg_s[wi][:, k, j * 128:(j + 1) * 128], hg[:, k, t0:t0 + n], k == 0, k == KC - 1, [wsb[wi], hgb], [psb[gi]])
                    for k in range(KC):
                        _mmF(P, ps[2 + gi][:, 0:n], wu_s[wi][:, k, j * 128:(j + 1) * 128], hg[:, k, t0:t0 + n], k == 0, k == KC - 1, [wsb[wi], hgb], [psb[2 + gi]])
                    st_, stb_ = ssb_t[gi], ssbb[gi]
                    P.op("act", lambda h, st_=st_, gi=gi, n=n: h.activation(out=st_[:, 0:n], in_=ps[gi][:, 0:n], func=AF.Silu), reads=[psb[gi]], writes=[stb_])
                    P.op("dve", lambda h, st_=st_, gi=gi, n=n: h.tensor_tensor(out=st_[:, 0:n], in0=st_[:, 0:n], in1=ps[2 + gi][:, 0:n], op=ALU.mult), reads=[stb_, psb[2 + gi]], writes=[stb_])
                    hd = hid[hi]
                    P.op("pool", lambda h, st_=st_, hd=hd, j=j, n=n, cg=cg, t0=t0: h.tensor_tensor(out=hd[:, j, 0:n], in0=st_[:, 0:n], in1=cg[:, t0:t0 + n], op=ALU.mult), reads=[stb_, hgb], writes=[hidb[hi]])
                for fc in range(8):
                    oi = 4 + fc % 2
                    for j in range(ns):
                        _mmF(P, ps[oi][:, 0:n], wd_s[wi][:, j, fc * 128:(fc + 1) * 128], hid[hi][:, j, 0:n], j == 0, j == ns - 1, [wsb[wi], hidb[hi]], [psb[oi]])
                    av = acc[:, fc, t0:t0 + n]
                    pv = ps[oi][:, 0:n]
                    if sidx == 0:
                        P.op("act", lambda h, av=av, pv=pv: h.activation(out=av, in_=pv, func=AF.Copy), reads=[psb[oi]], writes=[accb[bi]])
                    else:
                        P.op("dve", lambda h, av=av, pv=pv: h.tensor_tensor(out=av, in0=av, in1=pv, op=ALU.add), reads=[psb[oi], accb[bi]], writes=[accb[bi]])
        if sidx == len(slices) - 1:
            for bi in range(nblk):
                t0 = bi * 512
                outs.append(_ldF(P, "sp", ye[:, :, goff[g] + t0:goff[g] + t0 + 512], acc[:, :, t0:t0 + 512], [P.buf()], reads=[accb[bi]]))
    P.finish_wait("sp", outs)
    P.emit()
    return nc


def _ldF(P, q, out, in_, writes, reads=()):
    return P.dma(q, lambda h: h.dma_start(out=out, in_=in_), reads=reads, writes=writes)


def _mmF(P, out, lhsT, rhs, start, stop, reads, writes):
    return P.op("pe", lambda h: h.matmul(out, lhsT=lhsT, rhs=rhs, start=start, stop=stop), reads=reads, writes=writes)


def build_Fc(TT=2048, nexp=8):
    nc = bass.Bass("TRN2", target_bir_lowering=False)
    dr = {}

    def din(name, shape, dt=F32):
        dr[name] = nc.dram_tensor(name, list(shape), dt, kind="ExternalInput").ap()
    din("xm", [KC, 128, TT])
    din("yp", [nexp, KC, 128, TT])
    din("condT", [128, KC, 2])
    din("w_mod", [D, 6 * D])
    din("b_modT", [128, 48, 2])
    din("norm_gT", [128, 4, KC, 2])
    xo = nc.dram_tensor("xo", [KC, 128, TT], F32, kind="ExternalOutput").ap().rearrange("k p t -> p k t")
    xm = dr["xm"].rearrange("k p t -> p k t")
    P = Prog(nc)
    arena_init(P)
    ps = [P.ps("ps%d" % i, [128, 512], F32) for i in range(8)]
    psb = [P.buf("ps%d" % i) for i in range(8)]
    ones = A(P, [128, 128], BF16)
    onesb = P.buf()
    P.op("dve", lambda h: h.memset(ones[:], 1.0), writes=[onesb])
    P.eps_t = A(P, [128, 1], F32)
    P.op("dve", lambda h: h.memset(P.eps_t[:], EPS), writes=[onesb])
    mod_view = ps[7][:, 0:96].rearrange("p (j r) -> p j r", r=2)
    compute_mod(P, dr, [5], mod_view, psb[7])
    _, gg_f = mod_derived(P, 4, 5, 2, 3)
    acc = [A(P, [128, KC, 512], F32) for _ in range(2)]
    accb = [P.buf(), P.buf()]
    part = [A(P, [128, KC, 512], F32) for _ in range(3)]
    partb = [P.buf() for _ in range(3)]
    xt = [A(P, [128, KC, 512], F32) for _ in range(2)]
    xtb = [P.buf(), P.buf()]
    sq = A(P, [128, KC, 512], BF16)
    sqb = P.buf()
    rstd = A(P, [128, 512], F32)
    rstdb = P.buf()
    tmp = [A(P, [128, 512], F32) for _ in range(2)]
    tmpb = [P.buf(), P.buf()]
    outs = []
    pc = 0
    for bi in range(TT // 512):
        t0, n = bi * 512, 512
        a_t, a_b = acc[bi % 2], accb[bi % 2]
        x_t, x_b = xt[bi % 2], xtb[bi % 2]
        _ldF(P, "sp", x_t[:, :, :], xm[:, :, t0:t0 + n], [x_b])
        _ldF(P, "sp", a_t[:, :, :], dr["yp"][0].rearrange("k p t -> p k t")[:, :, t0:t0 + n], [a_b])
        for e in range(1, nexp):
            p_t, p_b = part[pc % 3], partb[pc % 3]
            pc += 1
            _ldF(P, "act" if e % 2 else "sp", p_t[:, :, :], dr["yp"][e].rearrange("k p t -> p k t")[:, :, t0:t0 + n], [p_b])
            eng = "dve" if e % 2 else "pool"
            P.op(eng, lambda h, a_t=a_t, p_t=p_t: h.tensor_tensor(out=a_t[:, :, :], in0=a_t[:, :, :], in1=p_t[:, :, :], op=ALU.add), reads=[a_b, p_b], writes=[a_b])
        rms_rstd(P, a_t, a_b, n, sq, sqb, ps[6], psb[6], rstd, rstdb, ones)
        for k in range(KC):
            tb_, tt_ = tmpb[k % 2], tmp[k % 2]
            P.op("dve", lambda h, k=k, tt_=tt_, a_t=a_t: h.tensor_tensor(out=tt_[:, :], in0=a_t[:, k, :], in1=rstd[:, :], op=ALU.mult), reads=[a_b, rstdb], writes=[tb_])
            P.op("dve", lambda h, k=k, tt_=tt_, x_t=x_t: h.scalar_tensor_tensor(out=x_t[:, k, :], in0=tt_[:, :], scalar=gg_f[:, k, 0:1], in1=x_t[:, k, :], op0=ALU.mult, op1=ALU.add),
                 reads=[tb_, P.modb, x_b], writes=[x_b])
        outs.append(_ldF(P, "sp", xo[:, :, t0:t0 + n], x_t[:, :, :], [P.buf()], reads=[x_b]))
    P.finish_wait("sp", outs)
    P.emit()
    return nc


import math, os
RET_STOP = int(os.environ.get('RET_STOP', '99'))
SKIP = os.environ.get('SKIP', '')

NTOK = 8448
NCH = 66
MAGIC = 12582912.0
TWO_PI = 2.0 * math.pi


def pos_of(dd):
    if dd == 0:
        return list(range(NCH))
    order = [1, 0] + list(range(65, 1, -1))
    pos = [0] * NCH
    for p_, c in enumerate(order):
        pos[c] = p_
    return pos


def range_reduce_sincos(P, ph, sn, cs, tmp, shape_ap, b):
    v = shape_ap
    _ts(P, "dve", v(tmp), v(ph), 1.0 / TWO_PI, MAGIC, ALU.mult, ALU.add, [b], [b])
    _ts(P, "dve", v(tmp), v(tmp), -MAGIC, None, ALU.add, None, [b], [b])
    _stt(P, v(ph), v(tmp), -TWO_PI, v(ph), ALU.mult, ALU.add, [b], [b])
    _ts(P, "dve", v(ph), v(ph), -math.pi, math.pi, ALU.max, ALU.min, [b], [b])
    _act(P, v(sn), v(ph), AF.Sin, [b], [b])
    _ts(P, "dve", v(tmp), v(ph), -1.0, None, ALU.mult, None, [b], [b])
    _tt(P, "dve", v(tmp), v(tmp), v(ph), ALU.max, [b], [b])
    _act(P, v(cs), v(tmp), AF.Sin, [b], [b], scale=-1.0, bias=P.halfpi[0:v(tmp).shape[0], 0:1])


def build_M(need_ctx_out, parts=("four", "s5", "ret", "na"), DBG=False):
    nc = bass.Bass("TRN2", target_bir_lowering=False)
    dr = {}

    def din(name, shape, dt=F32):
        dr[name] = nc.dram_tensor(name, list(shape), dt, kind="ExternalInput").ap()
    din("xT", [KC, 128, NTOK])
    din("condT", [128, KC, 2])
    din("w_mod", [D, 6 * D])
    din("b_modT", [128, 48, 2])
    din("norm_gT", [128, 4, KC, 2])
    din("w_fm", [D, 576])
    din("w_tm", [D, 256])
    din("f_CS", [64, 128], BF16); din("f_RP", [64, 128], BF16); din("f_RQ", [64, 128], BF16)
    din("f_CB", [128, 64, 128], BF16); din("f_SB", [128, 64, 128], BF16)
    din("f_C256", [128, 2, 256], BF16); din("f_S256", [128, 2, 256], BF16)
    din("r_cosF", [64, 8192]); din("r_sinF", [64, 8192]); din("r_cosT", [128, 64, 64]); din("r_sinT", [128, 64, 64])
    din("r_jcol", [128, 2]); din("r_dist", [128, 128]); din("r_mask", [2, 128, 128]); din("r_irow", [2, 64, 128])
    din("s_jrow", [128, 129]); din("s_jcol", [128, 1]); din("s_LT", [2, 128, 128], BF16); din("s_mrow", [64, 4]); din("s_msm", [128, 2, 4])
    din("ident_bf", [128, 128], BF16); din("ident_f", [128, 128])
    din("n_mask", [5, 128, 832]); din("n_toep", [15, 64, 64])
    din("s_sm", [128, 2, 2, 3]); din("s_row", [128, 2, 3, 256]); din("s_hs", [64, 2, 3, 64]); din("s_B", [64, 2, 2, 64])
    din("s_C", [128, 2, 2, 2, 16]); din("s_d", [64, 1])
    din("r_dec", [128, 2]); din("r_gn", [64, 1])
    out = nc.dram_tensor("brT_out", [4, 64, NTOK], BF16, kind="ExternalOutput").ap()
    hT = nc.dram_tensor("hT_scr", [KC, 128, NTOK], BF16, kind="Internal").ap().rearrange("k p t -> p k t")
    xT = dr["xT"].rearrange("k p t -> p k t")

    P = Prog(nc)
    arena_init(P)
    ps = [P.ps("ps%d" % i, [128, 512], F32) for i in range(8)]
    psb = [P.buf("ps%d" % i) for i in range(8)]
    cb = P.buf("consts")
    ones = A(P, [128, 128], BF16)
    P.op("dve", lambda h: h.memset(ones[:], 1.0), writes=[cb])
    P.eps_t = A(P, [128, 1], F32)
    P.op("dve", lambda h: h.memset(P.eps_t[:], EPS), writes=[cb])
    P.halfpi = A(P, [128, 1], F32)
    P.op("dve", lambda h: h.memset(P.halfpi[:], math.pi / 2), writes=[cb])
    P.one_t = A(P, [128, 1], F32)
    P.op("dve", lambda h: h.memset(P.one_t[:], 1.0), writes=[cb])
    ident = A(P, [128, 128], BF16)
    _ld(P, "sp", ident[:], dr["ident_bf"][:, :], [cb])
    mod_view = ps[7][:, 0:96].rearrange("p (j r) -> p j r", r=2)
    compute_mod(P, dr, [0, 1], mod_view, psb[7])
    gm_a, _ = mod_derived(P, 1, None, 0, 0)
    sh_a = P.modT[:, 0:8, :]
    wfm = A(P, [128, KC, 576], BF16)
    wtm = A(P, [128, KC, 256], BF16)
    wb = P.buf("w")
    _ld(P, "pool", wfm[:], dr["w_fm"].rearrange("(k p) c -> p k c", p=128), [wb])
    _ld(P, "pool", wtm[:], dr["w_tm"].rearrange("(k p) c -> p k c", p=128), [wb])
    blocks = [(0, 256, 1)] + [(256 + 512 * i, 512, 0) for i in range(16)]
    outs = []
    hTb = P.buf("hT")
    mark0 = P.a_cur

    def fm_proj(hb, hbb, n, g, pst, pstb):
        for k in range(KC):
            _mm(P, pst[0:64, 0:n], wfm[:, k, g * 64:(g + 1) * 64], hb[:, k, 0:n], k == 0, k == KC - 1, [wb, hbb], [pstb])

    sT = A(P, [64, NTOK], BF16)
    markS = P.a_cur
    fT = A(P, [64, NTOK], BF16)
    fTb, sTb = P.buf("fT"), P.buf("sT")
    markA = P.a_cur
    xb = [A(P, [128, KC, 512], F32) for _ in range(2)]
    xbb = [P.buf(), P.buf()]
    sq = A(P, [128, KC, 512], BF16)
    sqb = P.buf()
    rstd = A(P, [128, 512], F32)
    rstdb = P.buf()
    tmp = [A(P, [128, 512], F32) for _ in range(8)]
    tmpb = [P.buf() for _ in range(8)]
    hbs = [A(P, [128, KC, 512], BF16) for _ in range(2)]
    hbsb = [P.buf(), P.buf()]
    def _ldx(bi_):
        t0_, n_, _r = blocks[bi_]
        _ld(P, "sp", xb[bi_ % 2][:, 0:4, 0:n_], xT[:, 0:4, t0_:t0_ + n_], [xbb[bi_ % 2]])
        _ld(P, "sp", xb[bi_ % 2][:, 4:8, 0:n_], xT[:, 4:8, t0_:t0_ + n_], [xbb[bi_ % 2]])
    _ldx(0)
    for bi, (t0, n, r) in enumerate(blocks):
        x_t, x_b = xb[bi % 2], xbb[bi % 2]
        hb, hbb = hbs[bi % 2], hbsb[bi % 2]
        if bi + 1 < len(blocks):
            _ldx(bi + 1)
        rms_rstd(P, x_t, x_b, n, sq, sqb, ps[6], psb[6], rstd, rstdb, ones)
        norm_mod(P, x_t, x_b, n, rstd, rstdb, gm_a, sh_a, r, hb, hbb, tmp, tmpb)
        _ld(P, "sp", hT[:, :, t0:t0 + n], hb[:, :, 0:n], [hTb], reads=[hbb])
        for gi_, (g, dst, dstb) in enumerate(((0, fT, fTb), (1, sT, sTb))):
            pi_ = (2 * bi + gi_) % 4
            fm_proj(hb, hbb, n, g, ps[pi_], psb[pi_])
            _cp(P, "act" if gi_ == 0 else "dve", dst[:, t0:t0 + n], ps[pi_][0:64, 0:n], [psb[pi_]], [dstb])
    barrier(P)
    P.a_cur = markA

    if "four" in parts:
        markF = P.a_cur
        CS = A(P, [64, 128], BF16); RP = A(P, [64, 128], BF16); RQ = A(P, [64, 128], BF16)
        CB = A(P, [128, 64, 128], BF16); SB = A(P, [128, 64, 128], BF16)
        ftb = P.buf("ftab")
        for t_, nm in ((CS, "f_CS"), (RP, "f_RP"), (RQ, "f_RQ")):
            _ld(P, "sp", t_[:], dr[nm][:, :], [ftb])
        _ld(P, "sp", CB[:], dr["f_CB"][:, :, :], [ftb])
        _ld(P, "sp", SB[:], dr["f_SB"][:, :, :], [ftb])
        PQ = A(P, [64, 128, 128], BF16); PQb = P.buf("PQ")
        UVT = A(P, [128, 64, 128], BF16); UVTb = P.buf("UVT")
        aT = A(P, [64, NTOK], BF16); aTb = P.buf("aT")
        for g4 in range(32):
            pi_ = g4 % 2
            for jj in range(4):
                m2 = g4 * 4 + jj
                _mm(P, ps[pi_][0:64, jj * 128:(jj + 1) * 128], fT[:, 256 + m2:NTOK:128], CS[:, :], True, True, [fTb, ftb], [psb[pi_]])
            _cp(P, "act" if g4 % 2 else "dve", PQ[:, g4 * 4:(g4 + 1) * 4, :], ps[pi_][0:64, 0:512].rearrange("p (a b) -> p a b", b=128), [psb[pi_]], [PQb])
        for g4 in range(16):
            pi_ = 2 + g4 % 2
            for jj in range(4):
                d = g4 * 4 + jj
                _mm(P, ps[pi_][:, jj * 128:(jj + 1) * 128], PQ[:, :, d], RP[:, :], True, False, [PQb, ftb], [psb[pi_]])
                _mm(P, ps[pi_][:, jj * 128:(jj + 1) * 128], PQ[:, :, 64 + d], RQ[:, :], False, True, [PQb, ftb], [psb[pi_]])
            _cp(P, "act" if g4 % 2 else "dve", UVT[:, g4 * 4:(g4 + 1) * 4, :], ps[pi_][:, 0:512].rearrange("p (a b) -> p a b", b=128), [psb[pi_]], [UVTb])
        aT3 = aT[:, 256:NTOK].rearrange("p (a b) -> p a b", b=64)
        for g4 in range(16):
            pi_ = g4 % 2
            for jj in range(4):
                n1 = g4 * 4 + jj
                _mm(P, ps[pi_][0:64, jj * 128:(jj + 1) * 128], UVT[:, :, n1], CB[:, n1, :], True, False, [UVTb, ftb], [psb[pi_]])
                _mm(P, ps[pi_][0:64, jj * 128:(jj + 1) * 128], UVT[:, :, 64 + n1], SB[:, n1, :], False, True, [UVTb, ftb], [psb[pi_]])
            _cp(P, "act" if g4 % 2 else "dve", aT3[:, :, g4 * 4:(g4 + 1) * 4], ps[pi_][0:64, 0:512].rearrange("p (j n) -> p n j", n=128), [psb[pi_]], [aTb])
        if need_ctx_out:
            C256 = A(P, [128, 2, 256], BF16); S256 = A(P, [128, 2, 256], BF16)
            _ld(P, "sp", C256[:], dr["f_C256"][:, :, :], [ftb])
            _ld(P, "sp", S256[:], dr["f_S256"][:, :, :], [ftb])
            PQc = A(P, [128, 2, 128], BF16); PQcb = P.buf()
            for tt in range(2):
                _mm(P, ps[2 + tt][:, 0:128], fT[:, tt * 128:(tt + 1) * 128], CS[:, :], True, True, [fTb, ftb], [psb[2 + tt]])
                _cp(P, "dve", PQc[:, tt, :], ps[2 + tt][:, 0:128], [psb[2 + tt]], [PQcb])
            seq = [(tt, 0) for tt in range(2)] + [(tt, 1) for tt in range(2)]
            for i_, (tt, pq) in enumerate(seq):
                _mm(P, ps[4][0:64, 0:256], PQc[:, tt, pq * 64:(pq + 1) * 64], (C256 if pq == 0 else S256)[:, tt, :], i_ == 0, i_ == 3, [PQcb, ftb], [psb[4]])
            _cp(P, "dve", aT[:, 0:256], ps[4][0:64, 0:256], [psb[4]], [aTb])
        else:
            P.op("dve", lambda h: h.memset(aT[:, 0:256], 0.0), writes=[aTb])
        outs.append(_ld(P, "sp", out[0, :, :], aT[:, :], [P.buf()], reads=[aTb]))
        barrier(P)
    P.a_cur = markS

    if "s5" in parts:
        s5_part(P, dr, ps, psb, sT, sTb, out, outs, need_ctx_out, cb)
    barrier(P)
    P.a_cur = mark0

    if "ret" in parts:
        ret_part(P, dr, ps, psb, hT, hTb, wfm, wtm, wb, blocks, out, outs, need_ctx_out, cb, ones)
        barrier(P)
        P.a_cur = mark0
    if "na" in parts:
        na_part(P, dr, ps, psb, hT, hTb, wfm, wtm, wb, blocks, out, outs, need_ctx_out, cb, ident)
        barrier(P)
    P.finish_wait("sp", outs)
    P.emit()
    return nc


def load_h(P, hT, hTb, hbs, hbsb, bi, t0, n):
    hb, hbb = hbs[bi % 2], hbsb[bi % 2]
    _ld(P, "sp", hb[:, :, 0:n], hT[:, :, t0:t0 + n], [hbb], reads=[hTb])
    return hb, hbb


def na_part(P, dr, ps, psb, hT, hTb, wfm, wtm, wb, blocks, out, outs, need_ctx_out, cb, ident):
    Cn = consts()
    drs, types = Cn["n_drs"], Cn["n_types"]
    nqT = A(P, [64, NTOK], BF16); nkT = A(P, [64, NTOK], BF16); nvT = A(P, [128, NCH, 64], BF16)
    nqb, nkb, nvb = P.buf("nq"), P.buf("nk"), P.buf("nv")
    nT = A(P, [64, NTOK], BF16); nTb = P.buf("nT")
    bias = A(P, [128, 5, 832], F32); biasb = P.buf("bias")
    mask = A(P, [128, 5, 832], F32)
    P.op("pool", lambda h: h.memset(bias[:], 0.0), writes=[biasb])
    maskb = P.buf()
    _ld(P, "sp", mask[:], dr["n_mask"].rearrange("t p c -> p t c"), [maskb])
    for ti in range(5):
        for qr in range(2):
            for i in range(9):
                _ld(P, "sp" if (i % 2) else "act", bias[qr * 64:(qr + 1) * 64, ti, i * 64:(i + 1) * 64], dr["n_toep"][int(drs[ti, qr, i])], [biasb])
    _tt(P, "dve", bias[:], bias[:], mask[:], ALU.add, [biasb, maskb], [biasb])
    mark = P.a_cur
    hbs = [A(P, [128, KC, 512], BF16) for _ in range(2)]
    hbsb = [P.buf(), P.buf()]
    cnt = 0
    for bi, (t0, n, r) in enumerate(blocks):
        hb, hbb = load_h(P, hT, hTb, hbs, hbsb, bi, t0, n)
        for g, dst, dstb in ((7, nqT, nqb), (8, nkT, nkb)):
            pi_ = cnt % 4
            cnt += 1
            for k in range(KC):
                _mm(P, ps[pi_][0:64, 0:n], wfm[:, k, g * 64:(g + 1) * 64], hb[:, k, 0:n], k == 0, k == KC - 1, [wb, hbb], [psb[pi_]])
            _cp(P, "act" if g == 7 else "dve", dst[:, t0:t0 + n], ps[pi_][0:64, 0:n], [psb[pi_]], [dstb])
        for tt in range(n // 128):
            pi_ = 4 + (tt % 2)
            for k in range(KC):
                _mm(P, ps[pi_][:, 0:64], hb[:, k, tt * 128:(tt + 1) * 128], wtm[:, k, 192:256], k == 0, k == KC - 1, [wb, hbb], [psb[pi_]])
            _cp(P, "act", nvT[:, t0 // 128 + tt, :], ps[pi_][:, 0:64], [psb[pi_]], [nvb])
    barrier(P)
    P.a_cur = mark
    NB4 = 4
    s_t = [A(P, [128, 832], F32) for _ in range(NB4)]; s_b = [P.buf() for _ in range(NB4)]
    p_t = [A(P, [128, 832], BF16) for _ in range(NB4)]; p_b = [P.buf() for _ in range(NB4)]
    pT = [A(P, [128, 7, 128], BF16) for _ in range(NB4)]; pTb = [P.buf() for _ in range(NB4)]
    st_ = [A(P, [128, 4], F32) for _ in range(NB4)]; stb = [P.buf() for _ in range(NB4)]
    SC = 0.125

    def softmax_pv(qi, ncols, pv_list, o_ps, o_psb, o_cols, sbi, s4=None):
        if s4 is None:
            s4 = sbi
        s, sb_ = s_t[s4], s_b[s4]
        sm, smb = st_[s4], stb[s4]
        P.op("dve", lambda h: h.reduce_max(out=sm[:, 0:1], in_=s[:, 0:ncols], axis=AX.X), reads=[sb_], writes=[smb])
        _ts(P, "dve", sm[:, 1:2], sm[:, 0:1], -1.0, None, ALU.mult, None, [smb], [smb])
        _act(P, s[:, 0:ncols], s[:, 0:ncols], AF.Exp, [sb_, smb], [sb_], bias=sm[:, 1:2], scale=1.0)
        P.op("dve", lambda h: h.reduce_sum(out=sm[:, 2:3], in_=s[:, 0:ncols], axis=AX.X), reads=[sb_], writes=[smb])
        P.op("dve", lambda h: h.reciprocal(out=sm[:, 3:4], in_=sm[:, 2:3]), reads=[smb], writes=[smb])
        p, pb = p_t[s4], p_b[s4]
        _ts(P, "dve", p[:, 0:ncols], s[:, 0:ncols], sm[:, 3:4], None, ALU.mult, None, [sb_, smb], [pb])
        tp = ps[4 + sbi][:, :].bitcast(BF16)
        for ci, (c0, nk, tile) in enumerate(pv_list):
            _tr(P, tp[0:nk, ci * 128:(ci + 1) * 128], p[:, c0:c0 + nk], ident[:, :], [pb, cb], [psb[4 + sbi]])
        nchk = len(pv_list)
        pt_, ptb = pT[s4], pTb[s4]
        _cp(P, "act", pt_[:, 0:nchk, :], tp[:, 0:nchk * 128].rearrange("p (a b) -> p a b", b=128), [psb[4 + sbi]], [ptb])
        for ci, (c0, nk, tile) in enumerate(pv_list):
            _mm(P, o_ps[0:64, o_cols:o_cols + 128], nvT[0:nk, tile, :], pt_[0:nk, ci, :], ci == 0, ci == nchk - 1, [nvb, ptb], [o_psb])

    for rp in range(64):
        ti = {0: 0, 1: 1, 62: 3, 63: 4}.get(rp, 2)
        r0 = 2 * rp
        if ti == 2:
            R0, nr = r0 - 4, 9
        else:
            R0, nr = types[ti][1], 8
        tq = 256 + 128 * rp
        kb_ = 256 + 64 * R0
        sbi = rp % 2
        s1, s2 = ps[sbi], ps[2 + sbi]
        _mm(P, s1[:, 0:512], nqT[:, tq:tq + 128], nkT[:, kb_:kb_ + 512], True, True, [nqb, nkb], [psb[sbi]])
        kb2 = kb_ + 512 if nr == 9 else kb_
        _mm(P, s2[:, 0:64], nqT[:, tq:tq + 128], nkT[:, kb2:kb2 + 64], True, True, [nqb, nkb], [psb[2 + sbi]])
        _mm(P, s2[:, 64:320], nqT[:, tq:tq + 128], nkT[:, 0:256], True, True, [nqb, nkb], [psb[2 + sbi]])
        s4 = rp % NB4
        s = s_t[s4]
        _stt(P, s[:, 0:512], s1[:, 0:512], SC, bias[:, ti, 0:512], ALU.mult, ALU.add, [psb[sbi], biasb], [s_b[s4]])
        _stt(P, s[:, 512:832], s2[:, 0:320], SC, bias[:, ti, 512:832], ALU.mult, ALU.add, [psb[2 + sbi], biasb], [s_b[s4]])
        t_base = 2 + R0 // 2
        pv = [(128 * j, 128, t_base + j) for j in range(4)]
        if nr == 9:
            pv.append((512, 64, t_base + 4))
        pv += [(576, 128, 0), (704, 128, 1)]
        jj = rp % 4
        softmax_pv(rp, 832, pv, ps[6], psb[6], jj * 128, sbi, s4)
        if jj == 3:
            _cp(P, "dve", nT[:, 256 + 512 * (rp // 4):256 + 512 * (rp // 4 + 1)], ps[6][0:64, 0:512], [psb[6]], [nTb])
    if need_ctx_out:
        for qt in range(2):
            sbi = qt
            _mm(P, ps[sbi][:, 0:256], nqT[:, qt * 128:(qt + 1) * 128], nkT[:, 0:256], True, True, [nqb, nkb], [psb[sbi]])
            _ts(P, "dve", s_t[sbi][:, 0:256], ps[sbi][:, 0:256], SC, None, ALU.mult, None, [psb[sbi]], [s_b[sbi]])
            softmax_pv(qt, 256, [(0, 128, 0), (128, 128, 1)], ps[7], psb[7], qt * 128, sbi)
        _cp(P, "dve", nT[:, 0:256], ps[7][0:64, 0:256], [psb[7]], [nTb])
    else:
        P.op("dve", lambda h: h.memset(nT[:, 0:256], 0.0), writes=[nTb])
    outs.append(_ld(P, "sp", out[3, :, :], nT[:, :], [P.buf()], reads=[nTb]))


def ret_part(P, dr, ps, psb, hT, hTb, wfm, wtm, wb, blocks, out, outs, need_ctx_out, cb, ones):
    KS = 0.125
    qT = A(P, [64, NTOK], BF16); kT = A(P, [64, NTOK], BF16); gT = A(P, [64, NTOK], BF16)
    qb_, kb_, gb_ = P.buf("q"), P.buf("k"), P.buf("g")
    rvT = A(P, [128, NCH, 64], BF16); rvb = P.buf("rv")
    Sbf = [A(P, [64, NCH, 64], BF16) for _ in range(2)]
    rc = P.buf("retc")
    dec = A(P, [128, 2], F32); lg = A(P, [128, 2], F32); jcol = A(P, [128, 2], F32); kdec = A(P, [128, 2], F32); g128 = A(P, [128, 2], F32)
    dist = A(P, [128, 128], F32); msk = A(P, [128, 2, 128], F32); DT = A(P, [128, 128], F32); DT2 = A(P, [128, 128], F32)
    irow = A(P, [64, 2, 128], F32); qdec = A(P, [64, 2, 128], F32)
    gn = A(P, [64, 1], F32); o64 = A(P, [64, 64], F32)
    _ld(P, "sp", dec[:], dr["r_dec"][:, :], [rc])
    _ld(P, "sp", jcol[:], dr["r_jcol"][:, :], [rc])
    _ld(P, "sp", dist[:], dr["r_dist"][:, :], [rc])
    _ld(P, "sp", msk[:], dr["r_mask"].rearrange("d j i -> j d i"), [rc])
    _ld(P, "sp", irow[:], dr["r_irow"].rearrange("d p i -> p d i"), [rc])
    _ld(P, "sp", gn[:], dr["r_gn"][:, :], [rc])
    P.op("dve", lambda h: h.memset(o64[:], 1.0 / 64), writes=[rc])
    _act(P, lg[:], dec[:], AF.Exp, [rc], [rc], scale=-1.0)
    _act(P, lg[:], lg[:], AF.Ln, [rc], [rc], bias=P.one_t[:, 0:1], scale=1.0)
    _ts(P, "dve", lg[:], lg[:], -1.0, None, ALU.mult, None, [rc], [rc])
    for dd in range(2):
        _act(P, kdec[:, dd:dd + 1], jcol[:, dd:dd + 1], AF.Exp, [rc], [rc], scale=lg[:, dd:dd + 1])
        _act(P, g128[:, dd:dd + 1], lg[:, dd:dd + 1], AF.Exp, [rc], [rc], scale=128.0)
        _act(P, qdec[:, dd, :], irow[:, dd, :], AF.Exp, [rc], [rc], scale=lg[0:64, dd:dd + 1])
    _ts(P, "dve", kdec[:], kdec[:], KS, None, ALU.mult, None, [rc], [rc])
    _act(P, DT[:], dist[:], AF.Exp, [rc], [rc], scale=lg[:, 0:1])
    _tt(P, "dve", DT[:], DT[:], msk[:, 0, :], ALU.mult, [rc], [rc])
    _act(P, DT2[:], dist[:], AF.Exp, [rc], [rc], scale=lg[:, 1:2])
    _tt(P, "dve", DT2[:], DT2[:], msk[:, 1, :], ALU.mult, [rc], [rc])
    _tt(P, "dve", DT[:], DT[:], DT2[:], ALU.add, [rc], [rc])
    if RET_STOP <= 0:
        return
    mark_k = P.a_cur
    kd = [A(P, [128, NCH, 64], BF16) for _ in range(2)]
    kdb = [P.buf(), P.buf()]
    mark = P.a_cur
    hbs = [A(P, [128, KC, 512], BF16) for _ in range(2)]
    hbsb = [P.buf(), P.buf()]
    cF = [A(P, [64, 512], F32) for _ in range(2)]; sF = [A(P, [64, 512], F32) for _ in range(2)]
    cTt = [A(P, [128, 4, 64], F32) for _ in range(2)]; sTt = [A(P, [128, 4, 64], F32) for _ in range(2)]
    tabb = [P.buf(), P.buf()]
    t1 = [A(P, [128, 512], F32) for _ in range(2)]; t1b = [P.buf(), P.buf()]
    t2 = [A(P, [128, 512], F32) for _ in range(2)]; t2b = [P.buf(), P.buf()]
    cnt = 0
    for bi, (t0, n, r) in enumerate(blocks):
        hb, hbb = load_h(P, hT, hTb, hbs, hbsb, bi, t0, n)
        lat = (r == 0)
        tb_ = tabb[bi % 2]
        if lat:
            m0 = t0 - 256
            _ld(P, "sp", cF[bi % 2][:, :], dr["r_cosF"][:, m0:m0 + 512], [tb_])
            _ld(P, "sp", sF[bi % 2][:, :], dr["r_sinF"][:, m0:m0 + 512], [tb_])
            _ld(P, "sp", cTt[bi % 2][:, :, :], dr["r_cosT"][:, m0 // 128:m0 // 128 + 4, :], [tb_])
            _ld(P, "sp", sTt[bi % 2][:, :, :], dr["r_sinT"][:, m0 // 128:m0 // 128 + 4, :], [tb_])

        def proj(g, pi_):
            for k in range(KC):
                _mm(P, ps[pi_][0:64, 0:n], wfm[:, k, g * 64:(g + 1) * 64], hb[:, k, 0:n], k == 0, k == KC - 1, [wb, hbb], [psb[pi_]])
        for (g, gsw, dst, dstb, scl) in ((2, 4, qT, qb_, 1.0), (3, 5, kT, kb_, KS)):
            if 'qk' in SKIP:
                continue
            proj(g, 0)
            if lat:
                proj(gsw, 1)
                i2 = cnt % 2
                cnt += 1
                _stt(P, t1[i2][0:64, 0:n], ps[0][0:64, 0:n], scl, cF[bi % 2][:, 0:n], ALU.mult, ALU.mult, [psb[0], tb_], [t1b[i2]])
                _stt(P, t2[i2][0:64, 0:n], ps[1][0:64, 0:n], scl, sF[bi % 2][:, 0:n], ALU.mult, ALU.mult, [psb[1], tb_], [t2b[i2]])
                _tt(P, "pool", dst[:, t0:t0 + n], t1[i2][0:64, 0:n], t2[i2][0:64, 0:n], ALU.add, [t1b[i2], t2b[i2]], [dstb])
            else:
                _act(P, dst[:, t0:t0 + n], ps[0][0:64, 0:n], AF.Copy, [psb[0]], [dstb], scale=scl)
        proj(6, 2)
        _cp(P, "act", gT[:, t0:t0 + n], ps[2][0:64, 0:n], [psb[2]], [gb_])
        for tt in range(n // 128):
            if 'tm' in SKIP:
                continue
            pi_ = 4 + (tt % 2)
            tile = t0 // 128 + tt
            for k in range(KC):
                _mm(P, ps[pi_][:, 0:192], hb[:, k, tt * 128:(tt + 1) * 128], wtm[:, k, 0:192], k == 0, k == KC - 1, [wb, hbb], [psb[pi_]])
            _cp(P, "act", rvT[:, tile, :], ps[pi_][:, 128:192], [psb[pi_]], [rvb])
            if 'kd' in SKIP:
                continue
            if ('kl' in SKIP and lat) or ('kc' in SKIP and not lat):
                continue
            if lat:
                i2 = cnt % 2
                cnt += 1
                _tt(P, "dve", t1[i2][:, 0:64], ps[pi_][:, 0:64], cTt[bi % 2][:, tt, :], ALU.mult, [psb[pi_], tb_], [t1b[i2]])
                _tt(P, "dve", t2[i2][:, 0:64], ps[pi_][:, 64:128], sTt[bi % 2][:, tt, :], ALU.mult, [psb[pi_], tb_], [t2b[i2]])
                if 'k1' in SKIP:
                    continue
                _tt(P, "dve", t1[i2][:, 0:64], t1[i2][:, 0:64], t2[i2][:, 0:64], ALU.add, [t1b[i2], t2b[i2]], [t1b[i2]])
                if 'k2' in SKIP:
                    continue
                for dd in range(2):
                    _act(P, kd[dd][:, tile, :], t1[i2][:, 0:64], AF.Identity, [t1b[i2], rc], [kdb[dd]], scale=kdec[:, dd:dd + 1])
            else:
                for dd in range(2):
                    _act(P, kd[dd][:, tile, :], ps[pi_][:, 0:64], AF.Identity, [psb[pi_], rc], [kdb[dd]], scale=kdec[:, dd:dd + 1])
    barrier(P)
    P.a_cur = mark
    if RET_STOP <= 1:
        return
    S32_ = A(P, [64, NCH, 64], F32)
    S32 = [S32_, S32_]
    Sb_ = P.buf()
    Sb = [Sb_, Sb_]
    for dd in range(2):
        pos = pos_of(dd)
        order = sorted(range(NCH), key=lambda c: pos[c])
        c0 = order[0]
        P.op("dve", lambda h, dd=dd, c0=c0: h.memset(S32[dd][:, c0, :], 0.0), writes=[Sb[dd]])
        for idx in range(NCH - 1):
            c, nxt = order[idx], order[idx + 1]
            pi_ = (idx // 8) % 2
            sl = idx % 8
            _mm(P, ps[pi_][0:64, sl * 64:(sl + 1) * 64], kd[dd][:, c, :], rvT[:, c, :], True, True, [kdb[dd], rvb], [psb[pi_]])
            _stt(P, S32[dd][:, nxt, :], S32[dd][:, c, :], g128[0:64, dd:dd + 1], ps[pi_][0:64, sl * 64:(sl + 1) * 64], ALU.mult, ALU.add, [Sb[dd], psb[pi_], rc], [Sb[dd]])
        _cp(P, "act", Sbf[dd][:], S32[dd][:], [Sb[dd]], [Sb[dd]])
    barrier(P)
    P.a_cur = mark_k
    if RET_STOP <= 2:
        return
    oT = A(P, [64, NTOK], F32); oTb = P.buf("oT")
    sc = [A(P, [128, 128], BF16) for _ in range(2)]; scb = [P.buf(), P.buf()]
    qd = [[A(P, [64, 128], BF16) for _ in range(2)] for _ in range(2)]
    qdb = [[P.buf(), P.buf()] for _ in range(2)]
    c_start = 0 if need_ctx_out else 2
    if not need_ctx_out:
        P.op("pool", lambda h: h.memset(oT[:, 0:256], 0.0), writes=[oTb])
    for c in range(c_start, NCH):
        tau = 128 * c
        i2 = c % 2
        _mm(P, ps[i2][:, 0:128], kT[:, tau:tau + 128], qT[:, tau:tau + 128], True, True, [kb_, qb_], [psb[i2]])
        _tt(P, "dve", sc[i2][:, :], ps[i2][:, 0:128], DT[:, :], ALU.mult, [psb[i2], rc], [scb[i2]])
        for dd in range(2):
            _tt(P, "pool", qd[dd][i2][:, :], qT[:, tau:tau + 128], qdec[:, dd, :], ALU.mult, [qb_, rc], [qdb[dd][i2]])
        jj = c % 4
        po = ps[4 + (c // 4) % 2]
        pob = psb[4 + (c // 4) % 2]
        _mm(P, po[0:64, jj * 128:(jj + 1) * 128], rvT[:, c, :], sc[i2][:, :], True, False, [rvb, scb[i2]], [pob])
        _mm(P, po[0:64, jj * 128:(jj + 1) * 128], Sbf[0][:, c, :], qd[0][i2][:, :], False, False, [Sb[0], qdb[0][i2]], [pob])
        _mm(P, po[0:64, jj * 128:(jj + 1) * 128], Sbf[1][:, c, :], qd[1][i2][:, :], False, True, [Sb[1], qdb[1][i2]], [pob])
        if jj == 3 or c == NCH - 1:
            b0 = (c // 4) * 512
            wid = (jj + 1) * 128
            lo = 0
            if (not need_ctx_out) and c // 4 == 0:
                lo = 256
            _cp(P, "act", oT[:, b0 + lo:b0 + wid], po[0:64, lo:wid], [pob], [oTb])
    if RET_STOP <= 3:
        return
    rT = A(P, [64, NTOK], BF16); rTb = P.buf("rT")
    o64b = A(P, [64, 64], BF16)
    P.op("dve", lambda h: h.memset(o64b[:], 1.0 / 64), writes=[rc])
    obf = [A(P, [64, 512], BF16) for _ in range(2)]; obfb = [P.buf(), P.buf()]
    cen = [A(P, [64, 512], F32) for _ in range(2)]; cenb = [P.buf(), P.buf()]
    sq_ = [A(P, [64, 512], BF16) for _ in range(2)]; sqb_ = [P.buf(), P.buf()]
    rs_ = [A(P, [64, 512], F32) for _ in range(2)]; rsb_ = [P.buf(), P.buf()]
    sg_ = [A(P, [64, 512], F32) for _ in range(2)]; sgb_ = [P.buf(), P.buf()]
    nblk = (NTOK + 511) // 512
    for bi in range(nblk):
        t0 = bi * 512
        n = min(512, NTOK - t0)
        i2 = bi % 2
        _cp(P, "act", obf[i2][:, 0:n], oT[:, t0:t0 + n], [oTb], [obfb[i2]])
        _mm(P, ps[2 + i2][0:64, 0:n], o64b[:, :], obf[i2][:, 0:n], True, True, [obfb[i2], rc], [psb[2 + i2]])
        _tt(P, "dve", cen[i2][:, 0:n], oT[:, t0:t0 + n], ps[2 + i2][0:64, 0:n], ALU.subtract, [oTb, psb[2 + i2]], [cenb[i2]])
        _act(P, sq_[i2][:, 0:n], cen[i2][:, 0:n], AF.Square, [cenb[i2]], [sqb_[i2]])
        _mm(P, ps[6 + i2][0:64, 0:n], o64b[:, :], sq_[i2][:, 0:n], True, True, [sqb_[i2], rc], [psb[6 + i2]])
        _act(P, rs_[i2][:, 0:n], ps[6 + i2][0:64, 0:n], AF.Ln, [psb[6 + i2]], [rsb_[i2]], bias=P.eps_t[0:64, 0:1], scale=1.0)
        _act(P, rs_[i2][:, 0:n], rs_[i2][:, 0:n], AF.Exp, [rsb_[i2]], [rsb_[i2]], scale=-0.5)
        _tt(P, "dve", cen[i2][:, 0:n], cen[i2][:, 0:n], rs_[i2][:, 0:n], ALU.mult, [cenb[i2], rsb_[i2]], [cenb[i2]])
        _act(P, sg_[i2][:, 0:n], gT[:, t0:t0 + n], AF.Silu, [gb_], [sgb_[i2]])
        _stt(P, rT[:, t0:t0 + n], cen[i2][:, 0:n], gn[:, 0:1], sg_[i2][:, 0:n], ALU.mult, ALU.mult, [cenb[i2], sgb_[i2], rc], [rTb])
    outs.append(_ld(P, "sp", out[2, :, :], rT[:, :], [P.buf()], reads=[rTb]))


def s5_part(P, dr, ps, psb, sT, sTb, out, outs, need_ctx_out, cb):
    pb = P.buf("s5param")

    def cplx_prep(are, aim, ldt, mk):
        T = {k: mk() for k in ("dt", "ar", "ai", "mag", "ph", "tmp", "sn", "cs", "abr", "abi", "nr", "den", "cr", "ci", "u")}
        ident_v = lambda t: t
        _act(P, T["dt"], ldt, AF.Exp, [pb], [pb])
        _tt(P, "dve", T["ar"], are, T["dt"], ALU.mult, [pb], [pb])
        _tt(P, "dve", T["ai"], aim, T["dt"], ALU.mult, [pb], [pb])
        _act(P, T["mag"], T["ar"], AF.Exp, [pb], [pb])
        _cp(P, "dve", T["ph"], T["ai"], [pb], [pb])
        range_reduce_sincos(P, T["ph"], T["sn"], T["cs"], T["tmp"], ident_v, pb)
        _tt(P, "dve", T["abr"], T["mag"], T["cs"], ALU.mult, [pb], [pb])
        _tt(P, "dve", T["abi"], T["mag"], T["sn"], ALU.mult, [pb], [pb])
        _ts(P, "dve", T["nr"], T["abr"], -1.0, None, ALU.add, None, [pb], [pb])
        _tt(P, "dve", T["den"], are, are, ALU.mult, [pb], [pb])
        _tt(P, "dve", T["u"], aim, aim, ALU.mult, [pb], [pb])
        _tt(P, "dve", T["den"], T["den"], T["u"], ALU.add, [pb], [pb])
        P.op("dve", lambda h: h.reciprocal(out=T["den"], in_=T["den"]), reads=[pb], writes=[pb])
        _tt(P, "dve", T["cr"], T["nr"], are, ALU.mult, [pb], [pb])
        _tt(P, "dve", T["u"], T["abi"], aim, ALU.mult, [pb], [pb])
        _tt(P, "dve", T["cr"], T["cr"], T["u"], ALU.add, [pb], [pb])
        _tt(P, "dve", T["cr"], T["cr"], T["den"], ALU.mult, [pb], [pb])
        _tt(P, "dve", T["ci"], T["abi"], are, ALU.mult, [pb], [pb])
        _tt(P, "dve", T["u"], T["nr"], aim, ALU.mult, [pb], [pb])
        _tt(P, "dve", T["ci"], T["ci"], T["u"], ALU.subtract, [pb], [pb])
        _tt(P, "dve", T["ci"], T["ci"], T["den"], ALU.mult, [pb], [pb])
        return T

    p_sm = A(P, [128, 2, 2, 3], F32); p_row = A(P, [128, 2, 3, 256], F32); p_hs = A(P, [64, 2, 3, 64], F32)
    Bhs = A(P, [64, 2, 2, 64], F32); Csm = A(P, [128, 2, 2, 2, 16], F32); dvec = A(P, [64, 1], F32)
    jrow = A(P, [128, 129], F32); jcol = A(P, [128, 1], F32); njcol = A(P, [128, 1], F32)
    LT = A(P, [128, 2, 128], BF16); mrow = A(P, [64, 4], F32); msm = A(P, [128, 2, 4], F32)
    for t_, src in ((p_sm[:], dr["s_sm"][:, :, :, :]), (p_row[:], dr["s_row"][:, :, :, :]), (p_hs[:], dr["s_hs"][:, :, :, :]), (Bhs[:], dr["s_B"][:, :, :, :]),
                    (Csm[:], dr["s_C"][:, :, :, :, :]), (dvec[:], dr["s_d"][:, :]), (jrow[:], dr["s_jrow"][:, :]), (jcol[:], dr["s_jcol"][:, :]),
                    (LT[:], dr["s_LT"].rearrange("d j i -> j d i")), (mrow[:], dr["s_mrow"][:, :]), (msm[:], dr["s_msm"][:, :, :])):
        _ld(P, "sp", t_, src, [pb])
    _ts(P, "dve", njcol[:], jcol[:], -1.0, None, ALU.mult, None, [pb], [pb])
    ones_col = A(P, [128, 1], BF16)
    P.op("dve", lambda h: h.memset(ones_col[:], 1.0), writes=[pb])

    BD = [A(P, [64, 512], BF16) for _ in range(2)]
    CT = [A(P, [128, 4, 64], BF16) for _ in range(2)]
    mark_prep = P.a_cur
    for dd in range(2):
        P.a_cur = mark_prep
        Ths = cplx_prep(p_hs[:, dd, 0, :], p_hs[:, dd, 1, :], p_hs[:, dd, 2, :], lambda: A(P, [64, 64], F32)[:, :])
        bbr = A(P, [64, 64], F32); bbi = A(P, [64, 64], F32); uu = A(P, [64, 64], F32)
        _tt(P, "dve", bbr[:], Ths["cr"], Bhs[:, dd, 0, :], ALU.mult, [pb], [pb])
        _tt(P, "dve", uu[:], Ths["ci"], Bhs[:, dd, 1, :], ALU.mult, [pb], [pb])
        _tt(P, "dve", bbr[:], bbr[:], uu[:], ALU.subtract, [pb], [pb])
        _tt(P, "dve", bbi[:], Ths["cr"], Bhs[:, dd, 1, :], ALU.mult, [pb], [pb])
        _tt(P, "dve", uu[:], Ths["ci"], Bhs[:, dd, 0, :], ALU.mult, [pb], [pb])
        _tt(P, "dve", bbi[:], bbi[:], uu[:], ALU.add, [pb], [pb])
        for g in range(4):
            _ts(P, "dve", BD[dd][:, g * 64:(g + 1) * 64], bbr[:], mrow[:, g:g + 1], None, ALU.mult, None, [pb], [pb])
            _ts(P, "dve", BD[dd][:, 256 + g * 64:256 + (g + 1) * 64], bbi[:], mrow[:, g:g + 1], None, ALU.mult, None, [pb], [pb])
        for ri in range(2):
            for st in range(2):
                for g in range(4):
                    _ts(P, "dve", CT[dd][:, ri * 2 + st, g * 16:(g + 1) * 16], Csm[:, dd, st, ri, :], msm[:, st, g:g + 1], (1.0 if ri == 0 else -1.0), ALU.mult, ALU.mult, [pb], [pb])
    P.a_cur = mark_prep
    TA = [[A(P, [128, 2, 129], F32) for _ in range(2)] for _ in range(2)]
    TW = [[A(P, [128, 2, 129], F32) for _ in range(2)] for _ in range(2)]
    PRE = [[A(P, [128, 256], F32) for _ in range(2)] for _ in range(2)]
    mark_t = P.a_cur
    for dd in range(2):
        P.a_cur = mark_t
        dt_ = A(P, [128, 2], F32); ar = A(P, [128, 2], F32); ai = A(P, [128, 2], F32); nar = A(P, [128, 2], F32)
        _act(P, dt_[:], p_sm[:, dd, :, 2], AF.Exp, [pb], [pb])
        _tt(P, "dve", ar[:], p_sm[:, dd, :, 0], dt_[:], ALU.mult, [pb], [pb])
        _tt(P, "dve", ai[:], p_sm[:, dd, :, 1], dt_[:], ALU.mult, [pb], [pb])
        _ts(P, "dve", nar[:], ar[:], -1.0, None, ALU.mult, None, [pb], [pb])
        mark_st = P.a_cur
        for st in range(2):
            P.a_cur = mark_st
            ph = A(P, [128, 129], F32); tmp = A(P, [128, 129], F32); sn = A(P, [128, 129], F32); cs = A(P, [128, 129], F32)
            mp = A(P, [128, 129], F32); mn = A(P, [128, 129], F32)
            _ts(P, "dve", ph[:], jrow[:], ai[:, st:st + 1], None, ALU.mult, None, [pb], [pb])
            range_reduce_sincos(P, ph[:], sn[:], cs[:], tmp[:], (lambda t: t), pb)
            _act(P, mp[:], jrow[:], AF.Exp, [pb], [pb], scale=ar[:, st:st + 1])
            _act(P, mn[:], jrow[:], AF.Exp, [pb], [pb], scale=nar[:, st:st + 1])
            _tt(P, "dve", TA[dd][0][:, st, :], mp[:], cs[:], ALU.mult, [pb], [pb])
            _tt(P, "dve", TA[dd][1][:, st, :], mp[:], sn[:], ALU.mult, [pb], [pb])
            _tt(P, "dve", TW[dd][0][:, st, :], mn[:], cs[:], ALU.mult, [pb], [pb])
            _stt(P, TW[dd][1][:, st, :], mn[:], -1.0, sn[:], ALU.mult, ALU.mult, [pb], [pb])
        P.a_cur = mark_t
        dtr = A(P, [128, 256], F32); arr = A(P, [128, 256], F32); air = A(P, [128, 256], F32)
        ph = A(P, [128, 256], F32); tmp = A(P, [128, 256], F32); sn = A(P, [128, 256], F32); cs = A(P, [128, 256], F32); mg = A(P, [128, 256], F32)
        _act(P, dtr[:], p_row[:, dd, 2, :], AF.Exp, [pb], [pb])
        _tt(P, "dve", arr[:], p_row[:, dd, 0, :], dtr[:], ALU.mult, [pb], [pb])
        _tt(P, "dve", air[:], p_row[:, dd, 1, :], dtr[:], ALU.mult, [pb], [pb])
        _ts(P, "dve", ph[:], air[:], jcol[:, 0:1], None, ALU.mult, None, [pb], [pb])
        range_reduce_sincos(P, ph[:], sn[:], cs[:], tmp[:], (lambda t: t), pb)
        _act(P, mg[:], arr[:], AF.Exp, [pb], [pb], scale=(njcol if dd == 0 else jcol)[:, 0:1])
        _tt(P, "dve", PRE[dd][0][:], mg[:], cs[:], ALU.mult, [pb], [pb])
        _stt(P, PRE[dd][1][:], mg[:], (-1.0 if dd == 0 else 1.0), sn[:], ALU.mult, ALU.mult, [pb], [pb])
        P.a_cur = mark_t
    barrier(P)
    P.a_cur = mark_t
    Xt = A(P, [128, NCH, 512], BF16); Xtb = [P.buf() for _ in range(NCH)]
    yacc = A(P, [64, NTOK], F32); yb = P.buf("yacc")
    E = A(P, [128, 4, NCH], F32); Eb = P.buf("E")
    H = [[A(P, [128, 2, NCH], F32) for _ in range(2)] for _ in range(2)]
    Hb = P.buf("H")
    cv = [A(P, [128, 2, NCH], F32) for _ in range(2)]
    pw = A(P, [128, 2, 8], F32)
    tq = [A(P, [128, 256], F32) for _ in range(8)]; tqb = [P.buf() for _ in range(8)]
    hs_ = [A(P, [128, 4, 128], BF16) for _ in range(2)]; hsb = [P.buf(), P.buf()]
    uq = [A(P, [128, 128], F32) for _ in range(16)]; uqb = [P.buf() for _ in range(16)]
    for dd in range(2):
        pos = pos_of(dd)
        for c in range(NCH):
            tau = 128 * c
            px = ps[c % 2]; pxb = psb[c % 2]
            _mm(P, px[:, 0:512], sT[:, tau:tau + 128], BD[dd][:, :], True, True, [sTb, pb], [pxb])
            i2 = (c % 2) * 4
            _tt(P, "dve", tq[i2][:, :], px[:, 0:256], PRE[dd][0][:, :], ALU.mult, [pxb, pb], [tqb[i2]])
            _tt(P, "dve", tq[i2 + 1][:, :], px[:, 256:512], PRE[dd][1][:, :], ALU.mult, [pxb, pb], [tqb[i2 + 1]])
            _tt(P, "dve", tq[i2 + 2][:, :], px[:, 0:256], PRE[dd][1][:, :], ALU.mult, [pxb, pb], [tqb[i2 + 2]])
            _tt(P, "dve", tq[i2 + 3][:, :], px[:, 256:512], PRE[dd][0][:, :], ALU.mult, [pxb, pb], [tqb[i2 + 3]])
            _tt(P, "pool", Xt[:, c, 0:256], tq[i2][:, :], tq[i2 + 1][:, :], ALU.subtract, [tqb[i2], tqb[i2 + 1]], [Xtb[c]])
            _tt(P, "pool", Xt[:, c, 256:512], tq[i2 + 2][:, :], tq[i2 + 3][:, :], ALU.add, [tqb[i2 + 2], tqb[i2 + 3]], [Xtb[c]])
            for tl in range(4):
                col = tl * NCH + pos[c]
                _mm(P, ps[6][:, col:col + 1], Xt[:, c, tl * 128:(tl + 1) * 128], ones_col[:, :], True, True, [Xtb[c], pb], [psb[6]])
        _cp(P, "dve", E[:], ps[6][:, 0:4 * NCH].rearrange("p (a b) -> p a b", b=NCH), [psb[6]], [Eb])
        H0r, H0i = H[0][0], H[0][1]
        if dd == 0:
            for st in range(2):
                a_r, a_i = TA[0][0][:, st, 127:128], TA[0][1][:, st, 127:128]
                _ts(P, "dve", uq[0][:, 0:NCH], E[:, 2 + st, :], a_i, None, ALU.mult, None, [Eb, pb], [uqb[0]])
                _stt(P, H0r[:, st, :], E[:, st, :], a_r, uq[0][:, 0:NCH], ALU.mult, ALU.subtract, [Eb, pb, uqb[0]], [Hb])
                _ts(P, "dve", uq[1][:, 0:NCH], E[:, st, :], a_i, None, ALU.mult, None, [Eb, pb], [uqb[1]])
                _stt(P, H0i[:, st, :], E[:, 2 + st, :], a_r, uq[1][:, 0:NCH], ALU.mult, ALU.add, [Eb, pb, uqb[1]], [Hb])
        else:
            _cp(P, "dve", H0r[:], E[:, 0:2, :], [Eb], [Hb])
            _cp(P, "dve", H0i[:], E[:, 2:4, :], [Eb], [Hb])
        _cp(P, "dve", pw[:, :, 0], TA[dd][0][:, :, 128], [pb], [Hb])
        _cp(P, "dve", pw[:, :, 1], TA[dd][1][:, :, 128], [pb], [Hb])
        cur = 0
        d = 1
        while d < NCH:
            _ts(P, "dve", pw[:, :, 2], pw[:, :, 1], -1.0, None, ALU.mult, None, [Hb], [Hb])
            o_, n_ = H[cur], H[1 - cur]
            for ri in range(2):
                _cp(P, "dve", n_[ri][:, :, 0:d], o_[ri][:, :, 0:d], [Hb], [Hb])
            for st in range(2):
                pr, pi, npi = pw[:, st, 0:1], pw[:, st, 1:2], pw[:, st, 2:3]
                m = NCH - d
                _stt(P, uq[0][:, 0:m], o_[0][:, st, 0:m], pr, o_[0][:, st, d:NCH], ALU.mult, ALU.add, [Hb], [uqb[0]])
                _stt(P, n_[0][:, st, d:NCH], o_[1][:, st, 0:m], npi, uq[0][:, 0:m], ALU.mult, ALU.add, [Hb, uqb[0]], [Hb])
                _stt(P, uq[1][:, 0:m], o_[1][:, st, 0:m], pr, o_[1][:, st, d:NCH], ALU.mult, ALU.add, [Hb], [uqb[1]])
                _stt(P, n_[1][:, st, d:NCH], o_[0][:, st, 0:m], pi, uq[1][:, 0:m], ALU.mult, ALU.add, [Hb, uqb[1]], [Hb])
            _tt(P, "dve", pw[:, :, 3], pw[:, :, 0], pw[:, :, 0], ALU.mult, [Hb], [Hb])
            _tt(P, "dve", pw[:, :, 4], pw[:, :, 1], pw[:, :, 1], ALU.mult, [Hb], [Hb])
            _tt(P, "dve", pw[:, :, 5], pw[:, :, 0], pw[:, :, 1], ALU.mult, [Hb], [Hb])
            _tt(P, "dve", pw[:, :, 0], pw[:, :, 3], pw[:, :, 4], ALU.subtract, [Hb], [Hb])
            _ts(P, "dve", pw[:, :, 1], pw[:, :, 5], 2.0, None, ALU.mult, None, [Hb], [Hb])
            cur = 1 - cur
            d *= 2
        Hf = H[cur]
        kidx = 1 if dd == 0 else 128
        P.op("dve", lambda h: h.memset(cv[0][:, :, 0:1], 0.0), writes=[Hb])
        P.op("dve", lambda h: h.memset(cv[1][:, :, 0:1], 0.0), writes=[Hb])
        for st in range(2):
            a_r, a_i = TA[dd][0][:, st, kidx:kidx + 1], TA[dd][1][:, st, kidx:kidx + 1]
            m = NCH - 1
            _ts(P, "dve", uq[0][:, 0:m], Hf[1][:, st, 0:m], a_i, None, ALU.mult, None, [Hb, pb], [uqb[0]])
            _stt(P, cv[0][:, st, 1:NCH], Hf[0][:, st, 0:m], a_r, uq[0][:, 0:m], ALU.mult, ALU.subtract, [Hb, pb, uqb[0]], [Hb])
            _ts(P, "dve", uq[1][:, 0:m], Hf[0][:, st, 0:m], a_i, None, ALU.mult, None, [Hb, pb], [uqb[1]])
            _stt(P, cv[1][:, st, 1:NCH], Hf[1][:, st, 0:m], a_r, uq[1][:, 0:m], ALU.mult, ALU.add, [Hb, pb, uqb[1]], [Hb])
        Tt = TA[dd] if dd == 0 else TW[dd]
        c_start = 0 if need_ctx_out else 2
        for c in range(c_start, NCH):
            pg = ps[2 + c % 2]; pgb = psb[2 + c % 2]
            for tl in range(4):
                _mm(P, pg[:, tl * 128:(tl + 1) * 128], Xt[:, c, tl * 128:(tl + 1) * 128], LT[:, dd, :], True, True, [Xtb[c], pb], [pgb])
            hh, hhb = hs_[c % 2], hsb[c % 2]
            pc = pos[c]
            for st in range(2):
                gr, gi = pg[:, st * 128:(st + 1) * 128], pg[:, (2 + st) * 128:(3 + st) * 128]
                c_r, c_i = cv[0][:, st, pc:pc + 1], cv[1][:, st, pc:pc + 1]
                Tr, Ti = Tt[0][:, st, 0:128], Tt[1][:, st, 0:128]
                u0 = ((c % 2) * 2 + st) * 4
                _stt(P, uq[u0][:, :], gr, c_r, Tr, ALU.add, ALU.mult, [pgb, Hb, pb], [uqb[u0]])
                _stt(P, uq[u0 + 1][:, :], gi, c_i, Ti, ALU.add, ALU.mult, [pgb, Hb, pb], [uqb[u0 + 1]])
                _stt(P, uq[u0 + 2][:, :], gi, c_i, Tr, ALU.add, ALU.mult, [pgb, Hb, pb], [uqb[u0 + 2]])
                _stt(P, uq[u0 + 3][:, :], gr, c_r, Ti, ALU.add, ALU.mult, [pgb, Hb, pb], [uqb[u0 + 3]])
                _tt(P, "pool", hh[:, st, :], uq[u0][:, :], uq[u0 + 1][:, :], ALU.subtract, [uqb[u0], uqb[u0 + 1]], [hhb])
                _tt(P, "pool", hh[:, 2 + st, :], uq[u0 + 2][:, :], uq[u0 + 3][:, :], ALU.add, [uqb[u0 + 2], uqb[u0 + 3]], [hhb])
            jj = c % 4
            py = ps[4 + (c // 4) % 2]; pyb = psb[4 + (c // 4) % 2]
            for tl in range(4):
                _mm(P, py[0:64, jj * 128:(jj + 1) * 128], CT[dd][:, tl, :], hh[:, tl, :], tl == 0, tl == 3, [pb, hhb], [pyb])
            if jj == 3 or c == NCH - 1:
                b0 = (c // 4) * 512
                wid = (jj + 1) * 128
                lo = 256 if ((not need_ctx_out) and c // 4 == 0) else 0
                if dd == 0:
                    _cp(P, "act", yacc[:, b0 + lo:b0 + wid], py[0:64, lo:wid], [pyb], [yb])
                else:
                    _tt(P, "dve", yacc[:, b0 + lo:b0 + wid], yacc[:, b0 + lo:b0 + wid], py[0:64, lo:wid], ALU.add, [pyb, yb], [yb])
    zT = A(P, [64, NTOK], BF16); zb = P.buf("zT")
    g1 = [A(P, [64, 512], F32) for _ in range(2)]; g1b = [P.buf(), P.buf()]
    g2 = [A(P, [64, 512], F32) for _ in range(2)]; g2b = [P.buf(), P.buf()]
    lo_all = 0 if need_ctx_out else 256
    if not need_ctx_out:
        P.op("pool", lambda h: h.memset(zT[:, 0:256], 0.0), writes=[zb])
    nblk = (NTOK + 511) // 512
    for bi in range(nblk):
        t0 = max(bi * 512, lo_all)
        t1_ = min((bi + 1) * 512, NTOK)
        n = t1_ - t0
        i2 = bi % 2
        y = g1[i2]; w = g2[i2]
        _stt(P, y[:, 0:n], sT[:, t0:t1_], dvec[:, 0:1], yacc[:, t0:t1_], ALU.mult, ALU.add, [sTb, pb, yb], [g1b[i2]])
        _tt(P, "dve", w[:, 0:n], y[:, 0:n], y[:, 0:n], ALU.mult, [g1b[i2]], [g2b[i2]])
        _ts(P, "dve", w[:, 0:n], w[:, 0:n], 0.044715, 1.0, ALU.mult, ALU.add, [g2b[i2]], [g2b[i2]])
        _tt(P, "dve", w[:, 0:n], w[:, 0:n], y[:, 0:n], ALU.mult, [g2b[i2], g1b[i2]], [g2b[i2]])
        _act(P, w[:, 0:n], w[:, 0:n], AF.Sigmoid, [g2b[i2]], [g2b[i2]], scale=1.5957691216057308)
        _tt(P, "dve", zT[:, t0:t1_], w[:, 0:n], y[:, 0:n], ALU.mult, [g2b[i2], g1b[i2]], [zb])
    outs.append(_ld(P, "sp", out[1, :, :], zT[:, :], [P.buf()], reads=[zb]))


import ml_dtypes

BF = ml_dtypes.bfloat16
NTOK = 8448
f32 = np.float32


def fm(a):
    return np.ascontiguousarray(a.T.reshape(8, 128, a.shape[0]))


def prep_common(inp, layer, b):
    cond = np.stack([inp['c'][b], inp['c_ctx']], 0)
    condT = np.ascontiguousarray(cond.reshape(2, 8, 128).transpose(2, 1, 0))
    bm = inp['b_mod'][layer].reshape(48, 128).T
    b_modT = np.ascontiguousarray(np.stack([bm, bm], -1))
    ng = inp['norm_g'][layer].reshape(4, 8, 128).transpose(2, 0, 1)
    norm_gT = np.ascontiguousarray(np.stack([ng, ng], -1))
    return dict(condT=condT, b_modT=b_modT, norm_gT=norm_gT, w_mod=inp['w_mod'][layer])


_const_cache = {}


def consts():
    if _const_cache:
        return _const_cache
    C = _const_cache
    c = np.arange(64)
    ang = 2 * np.pi * (np.outer(c, c) % 64) / 64
    C['f_CS'] = np.concatenate([np.cos(ang), -np.sin(ang)], 1).astype(BF)
    ca, sa = np.cos(ang), np.sin(ang)
    C['f_RP'] = np.concatenate([ca, -sa], 1).astype(BF)
    C['f_RQ'] = np.concatenate([sa, ca], 1).astype(BF)
    m2 = np.arange(128)[:, None, None]
    n1 = np.arange(64)[None, :, None]
    n2 = np.arange(128)[None, None, :]
    be = 2 * np.pi * ((m2 * (n1 + 64 * n2)) % 8192) / 8192
    nrm = 1 / np.sqrt(64 * 8192)
    C['f_CB'] = (np.cos(be) * nrm).astype(BF)
    C['f_SB'] = (np.sin(be) * nrm).astype(BF)
    m = np.arange(256)
    a256 = 2 * np.pi * (np.outer(m, m) % 256) / 256
    nrm2 = 1 / np.sqrt(64 * 256)
    C['f_C256'] = np.ascontiguousarray((np.cos(a256) * nrm2).reshape(2, 128, 256).transpose(1, 0, 2)).astype(BF)
    C['f_S256'] = np.ascontiguousarray((np.sin(a256) * nrm2).reshape(2, 128, 256).transpose(1, 0, 2)).astype(BF)
    t = np.arange(8192)
    row = (t // 64).astype(f32)
    col = (t % 64).astype(f32)
    inv = (1.0 / (f32(10000.0) ** (np.arange(16, dtype=f32) / f32(16)))).astype(f32)
    angr = np.concatenate([row[:, None] * inv, col[:, None] * inv], -1).astype(f32)
    cs, sn = np.cos(angr).astype(f32), np.sin(angr).astype(f32)
    cos64 = np.concatenate([cs, cs], 1)
    sin64 = np.concatenate([-sn, sn], 1)
    C['r_cosF'] = np.ascontiguousarray(cos64.T)
    C['r_sinF'] = np.ascontiguousarray(sin64.T)
    C['r_cosT'] = np.ascontiguousarray(cos64.reshape(64, 128, 64).transpose(1, 0, 2))
    C['r_sinT'] = np.ascontiguousarray(sin64.reshape(64, 128, 64).transpose(1, 0, 2))
    j = np.arange(128, dtype=f32)
    C['r_jcol'] = np.stack([127 - j, j], 1).astype(f32)
    ii = np.arange(128)
    dist = np.abs(ii[None, :] - ii[:, None]).astype(f32)
    C['r_dist'] = dist
    C['r_mask'] = np.stack([(ii[None, :] >= ii[:, None]), (ii[:, None] >= ii[None, :])], 0).astype(f32)
    C['r_irow'] = np.stack([np.tile(j + 1, (64, 1)), np.tile(128 - j, (64, 1))], 0).astype(f32)
    C['s_jrow'] = np.tile(np.arange(129, dtype=f32), (128, 1))
    C['s_jcol'] = np.arange(128, dtype=f32)[:, None].copy()
    C['s_LT'] = np.stack([(ii[None, :] >= ii[:, None]), (ii[:, None] >= ii[None, :])], 0).astype(BF)
    g_of_row = np.arange(64) // 16
    C['s_mrow'] = (g_of_row[:, None] == np.arange(4)[None, :]).astype(f32)
    g_of_st = (np.arange(128)[:, None] // 64) + 2 * np.arange(2)[None, :]
    C['s_msm'] = (g_of_st[:, :, None] == np.arange(4)[None, None, :]).astype(f32)
    C['ident_bf'] = np.eye(128).astype(BF)
    C['ident_f'] = np.eye(128).astype(f32)
    def start(r):
        return int(np.clip(r - 4, 0, 120))
    qc = np.arange(64)
    cst = np.clip(qc - 8, 0, 48)
    kc = np.arange(64)
    colok = (kc[None, :] >= cst[:, None]) & (kc[None, :] < cst[:, None] + 16)
    types = [(0, 0, 8), (2, 0, 8), (10, 6, 9), (124, 120, 8), (126, 120, 8)]
    mask = np.full((5, 128, 832), -30000.0, f32)
    drs = np.zeros((5, 2, 9), np.int64)
    for ti, (r0, R0, nr) in enumerate(types):
        for qr in range(2):
            r = r0 + qr
            for i in range(9):
                kr = R0 + i
                dr = int(np.clip(kr - r + 7, 0, 14))
                drs[ti, qr, i] = dr
                if i < nr and start(r) <= kr < start(r) + 8:
                    blk = np.where(colok, 0.0, -30000.0)
                    mask[ti, qr * 64:(qr + 1) * 64, i * 64:(i + 1) * 64] = blk
        mask[ti, :, 576:] = 0.0
    C['n_mask'] = mask
    C['n_drs'] = drs
    C['n_types'] = types
    return C


def prep_M(inp, layer, c, xT_full):
    b, q = c // 4, c % 4
    C = consts()
    m = prep_common(inp, layer, b)
    m['xT'] = xT_full
    w_in = inp['w_in'][layer]
    o = q * 64
    sw = np.r_[32:64, 0:32]
    cols_fm = np.concatenate([np.arange(0 + o, 0 + o + 64), np.arange(256 + o, 256 + o + 64), np.arange(512 + o, 512 + o + 64),
                              np.arange(768 + o, 768 + o + 64), 512 + o + sw, 768 + o + sw, np.arange(1280 + o, 1280 + o + 64),
                              np.arange(1536 + o, 1536 + o + 64), np.arange(1792 + o, 1792 + o + 64)])
    cols_tm = np.concatenate([np.arange(768 + o, 768 + o + 64), 768 + o + sw, np.arange(1024 + o, 1024 + o + 64), np.arange(2048 + o, 2048 + o + 64)])
    m['w_fm'] = np.ascontiguousarray(w_in[:, cols_fm])
    m['w_tm'] = np.ascontiguousarray(w_in[:, cols_tm])
    for k in ('f_CS', 'f_RP', 'f_RQ', 'f_CB', 'f_SB', 'f_C256', 'f_S256', 'r_cosF', 'r_sinF', 'r_cosT', 'r_sinT', 'r_jcol', 'r_dist', 'r_mask', 'r_irow',
              's_jrow', 's_jcol', 's_LT', 's_mrow', 's_msm', 'ident_bf', 'ident_f', 'n_mask'):
        m[k] = C[k]
    gs = slice(4 * q, 4 * q + 4)
    L = layer
    are, aim = inp['s5_a_re'][L][:, gs], inp['s5_a_im'][L][:, gs]
    ldt = inp['s5_log_dt'][L][:, gs]
    def sm(a):
        return np.ascontiguousarray(a.reshape(2, 2, 128).transpose(2, 0, 1))
    ldt_b = np.broadcast_to(ldt[:, :, None], (2, 4, 64))
    m['s_sm'] = np.ascontiguousarray(np.stack([sm(are), sm(aim), sm(ldt_b)], -1))
    row = np.stack([are.reshape(2, 256), aim.reshape(2, 256), ldt_b.reshape(2, 256)], -1)
    m['s_row'] = np.ascontiguousarray(np.broadcast_to(row[None].transpose(0, 1, 3, 2), (128, 2, 3, 256)))
    hs = np.stack([are, aim, ldt_b], 2)
    hs = np.broadcast_to(hs[:, :, None], (2, 4, 16, 3, 64))
    m['s_hs'] = np.ascontiguousarray(hs.transpose(1, 2, 0, 3, 4).reshape(64, 2, 3, 64))
    bre, bim = inp['s5_b_re'][L][:, gs], inp['s5_b_im'][L][:, gs]
    B = np.stack([bre, bim], 2)
    m['s_B'] = np.ascontiguousarray(B.transpose(1, 4, 0, 2, 3).reshape(64, 2, 2, 64))
    cre, cim = inp['s5_c_re'][L][:, gs], inp['s5_c_im'][L][:, gs]
    Cc = np.stack([cre, cim], 2)
    Cc = Cc.transpose(1, 4, 0, 2, 3)
    Cc = Cc.reshape(2, 2, 64, 2, 2, 16).transpose(1, 2, 3, 0, 4, 5).reshape(128, 2, 2, 2, 16)
    m['s_C'] = np.ascontiguousarray(Cc)
    m['s_d'] = np.ascontiguousarray(inp['s5_d'][L][256 * 0 + 64 * q:64 * q + 64][:, None])
    rd = inp['ret_decay'][L][:, q]
    m['r_dec'] = np.ascontiguousarray(np.broadcast_to(rd[None, :], (128, 2))).astype(f32)
    m['r_gn'] = np.ascontiguousarray(inp['ret_gn'][L][64 * q:64 * q + 64][:, None])
    rpb = inp['na_rpb'][L][q]
    dc = np.clip(np.arange(64)[None, :] - np.arange(64)[:, None], -15, 15) + 15
    m['n_toep'] = np.ascontiguousarray(rpb[:, dc])
    return m


def _prep_F(inp, layer, c, xa, bra_bf, moe_a=False):
    b = c // 4
    m = prep_common(inp, layer, b)
    m.update(xT=fm(xa), brT=bra_bf, w_in=inp['w_in'][layer], w_br=inp['w_branch'][layer].reshape(1024, 1024), w_o=inp['w_out'][layer],
             w_glu=inp['s5_w_glu'][layer], b_gluT=np.ascontiguousarray(inp['s5_b_glu'][layer].reshape(2, 128).T))
    i = layer // 2
    if layer % 2 == 0:
        m.update(w_g=inp['ffn_w_gate'][i:i + 1], w_u=inp['ffn_w_up'][i:i + 1], w_d=inp['ffn_w_down'][i:i + 1])
    else:
        sel = np.zeros((8, 8, 128), np.float32)
        for e in range(8):
            sel[e, e, :] = 1
        m.update(w_r=inp['moe_w_router'][i], b_r=np.ascontiguousarray(np.broadcast_to(inp['moe_b_router'][i][None], (128, 8))),
                 ident=np.eye(128, dtype=np.float32), sel=sel)
    return m


def kernel(**inputs):
    inp = {k: np.asarray(v) for k, v in inputs.items()}
    NCORE = 8
    cores = list(range(NCORE))
    x = inp['x']
    ctx = inp['ctx']
    for layer in range(2):
        last = (layer == 1)
        ncM = build_M(not last)
        xfull = [fm(np.concatenate([ctx[b], x[b]], 0)) for b in range(2)]
        maps = [prep_M(inp, layer, c, xfull[c // 4]) for c in cores]
        resM = run_bass_kernel_spmd(ncM, maps, core_ids=cores).results
        del maps
        br_full = []
        for b in range(2):
            o = np.stack([np.asarray(resM[4 * b + q]['brT_out']) for q in range(4)], 1)
            br_full.append(o.reshape(1024, NTOK))
        del resM
        if not last:
            blocksA = [(i * 256, 256, 0) for i in range(8)] + [(2048, 64, 1)]
            blocksB = [(i * 512, 512, 0) for i in range(4)] + [(2048, 64, 1)]
            ncF = build_F(blocksA, blocksB, 1, 2816, False)
        else:
            blocksA = [(i * 256, 256, 0) for i in range(8)]
            blocksB = [(i * 512, 512, 0) for i in range(4)]
            ncF = build_F(blocksA, blocksB, 8, 3584, True, mode='moe_a')
        maps = []
        for c in cores:
            b, q = c // 4, c % 4
            lat = slice(256 + q * 2048, 256 + (q + 1) * 2048)
            if not last:
                xa = np.concatenate([x[b, q * 2048:(q + 1) * 2048], ctx[b, q * 64:(q + 1) * 64]], 0)
                bra = np.concatenate([br_full[b][:, lat], br_full[b][:, q * 64:(q + 1) * 64]], 1)
            else:
                xa = x[b, q * 2048:(q + 1) * 2048]
                bra = br_full[b][:, lat]
            bra = np.ascontiguousarray(bra.reshape(8, 128, bra.shape[1]))
            maps.append(_prep_F(inp, layer, c, xa, bra))
        resF = run_bass_kernel_spmd(ncF, maps, core_ids=cores).results
        del maps
        if not last:
            xn = np.empty_like(x)
            cn = np.empty_like(ctx)
            for c in cores:
                b, q = c // 4, c % 4
                o = np.asarray(resF[c]['xo']).reshape(1024, -1).T
                xn[b, q * 2048:(q + 1) * 2048] = o[:2048]
                cn[b, q * 64:(q + 1) * 64] = o[2048:]
            x, ctx = xn, cn
            continue
        i = layer // 2
        h2_all = np.concatenate([np.asarray(resF[c]['h2o']) for c in cores], 2)
        cb_all = np.concatenate([np.asarray(resF[c]['cbo']) for c in cores], 1)
        sel = np.concatenate([np.asarray(resF[c]['mko']) for c in cores], 1).astype(bool)
        idx = [np.flatnonzero(sel[e]) for e in cores]
        nb = max(1, -(-max(len(t) for t in idx) // 512))
        ng = -(-nb // 4)
        groups = tuple(nb // ng + (1 if g < nb % ng else 0) for g in range(ng))
        C = 512 * nb
        ncE = build_E(groups)
        maps = []
        for e in cores:
            n_e = len(idx[e])
            ii = np.zeros(C, np.int64)
            ii[:n_e] = idx[e]
            cbe = np.zeros(C, cb_all.dtype)
            cbe[:n_e] = cb_all[e, idx[e]]
            maps.append(dict(h2=np.ascontiguousarray(h2_all[:, :, ii]), cbe=np.ascontiguousarray(np.broadcast_to(cbe[None, :], (128, C))),
                             w_g=inp['moe_w_gate'][i][e], w_u=inp['moe_w_up'][i][e], w_d=inp['moe_w_down'][i][e]))
        resE = run_bass_kernel_spmd(ncE, maps, core_ids=cores).results
        del maps
        slot = np.cumsum(sel, axis=0) - sel
        nslot = max(1, int(sel.sum(0).max()))
        yp_all = np.zeros((nslot, 8, 128, sel.shape[1]), np.float32)
        for e in cores:
            ye = np.asarray(resE[e]['ye'])
            t = idx[e]
            sv = slot[e, t]
            for k in range(nslot):
                mk = sv == k
                yp_all[k][:, :, t[mk]] = ye[:, :, np.flatnonzero(mk)]
        ncC = build_Fc(nexp=nslot)
        maps = []
        for c in cores:
            m = prep_common(inp, layer, c // 4)
            m['xm'] = np.asarray(resF[c]['xo'])
            m['yp'] = np.ascontiguousarray(yp_all[:, :, :, c * 2048:(c + 1) * 2048])
            maps.append(m)
        resC = run_bass_kernel_spmd(ncC, maps, core_ids=cores).results
        xn = np.empty_like(x)
        for c in cores:
            b, q = c // 4, c % 4
            xn[b, q * 2048:(q + 1) * 2048] = np.asarray(resC[c]['xo']).reshape(1024, -1).T
        x = xn
    return x.astype(np.float32)
```

```python
import numpy as np
from contextlib import ExitStack
import concourse.bass as bass
import concourse.mybir as mybir
from concourse.bass_utils import run_bass_kernel_spmd

F32 = mybir.dt.float32
BF16 = mybir.dt.bfloat16
I32 = mybir.dt.int32
ALU = mybir.AluOpType
AF = mybir.ActivationFunctionType
AX = mybir.AxisListType

ENGS = ("pe", "act", "dve", "pool", "sp")
NDSEM = 12


class Buf:
    __slots__ = ("name", "lw", "rd", "psum")

    def __init__(self, name="", psum=False):
        self.name = name
        self.psum = psum
        self.lw = None
        self.rd = {}


class Prog:
    def __init__(self, nc):
        self.nc = nc
        self.stack = ExitStack()
        self.ops = {e: [] for e in ENGS}
        self.cnt = {e: 0 for e in ENGS}
        self.seen = {e: {} for e in ENGS}
        self.sems = {}
        for e in ENGS:
            self.sems[e] = self.stack.enter_context(nc.semaphore("s_" + e))
        self.dsem_use = {}
        self.dq_next = {}
        for q in ("sp", "act", "pool"):
            for i in range(NDSEM):
                k = "d_%s%d" % (q, i)
                self.sems[k] = self.stack.enter_context(nc.semaphore(k))
                self.dsem_use[k] = 0
            self.dq_next[q] = 0
        self.nbuf = 0

    def sb(self, name, shape, dt):
        return self.stack.enter_context(self.nc.sbuf_tensor(name, list(shape), dt))

    def ps(self, name, shape, dt=F32):
        return self.stack.enter_context(self.nc.psum_tensor(name, list(shape), dt))

    def buf(self, name=None, psum=None):
        self.nbuf += 1
        name = name or "b%d" % self.nbuf
        if psum is None:
            psum = name.startswith("ps")
        return Buf(name, psum)

    def _deps(self, eng, reads, writes, is_dma):
        w = {}

        def add(t):
            if t is None:
                return
            k, v = t
            if w.get(k, 0) < v:
                w[k] = v
        for b in reads:
            add(b.lw)
            if b.psum:
                for k, v in b.rd.items():
                    if k != eng:
                        add((k, v))
        for b in writes:
            if b.lw is not None:
                if not (eng == "pe" and b.lw[0] == "pe" and not is_dma):
                    add(b.lw)
            for k, v in b.rd.items():
                if k == eng and not is_dma and eng != "pool":
                    continue
                add((k, v))
        seen = self.seen[eng]
        out = []
        for k, v in w.items():
            if seen.get(k, 0) < v:
                seen[k] = v
                out.append((k, v))
        return out

    def _commit(self, ticket, reads, writes):
        for b in writes:
            b.lw = ticket
            b.rd = {}
        for b in reads:
            k, v = ticket
            if b.rd.get(k, 0) < v:
                b.rd[k] = v

    def op(self, eng, fn, reads=(), writes=()):
        waits = self._deps(eng, reads, writes, False)
        self.cnt[eng] += 1
        ticket = (eng, self.cnt[eng])
        self.ops[eng].append((waits, fn, (eng, 1)))
        self._commit(ticket, reads, writes)
        return ticket

    def dma(self, q, fn, reads=(), writes=()):
        i = self.dq_next[q]
        self.dq_next[q] = (i + 1) % NDSEM
        k = "d_%s%d" % (q, i)
        waits = self._deps(q, reads, writes, True)
        prev = self.dsem_use[k]
        if prev > 0 and self.seen[q].get(k, 0) < 16 * prev:
            self.seen[q][k] = 16 * prev
            waits.append((k, 16 * prev))
        self.dsem_use[k] = prev + 1
        ticket = (k, 16 * (prev + 1))
        self.ops[q].append((waits, fn, (k, 16)))
        self._commit(ticket, reads, writes)
        return ticket

    def finish_wait(self, eng, tickets):
        waits = []
        for k, v in tickets:
            if self.seen[eng].get(k, 0) < v:
                self.seen[eng][k] = v
                waits.append((k, v))
        self.ops[eng].append((waits, None, None))

    def emit(self):
        nc = self.nc
        sems = self.sems
        ops = self.ops

        def replay(e, h):
            for waits, fn, inc in ops[e]:
                for k, v in waits:
                    h.wait_ge(sems[k], v)
                if fn is not None:
                    ins = fn(h)
                    ins.then_inc(sems[inc[0]], inc[1])

        with nc.Block() as block:
            @block.sync
            def _(h):
                replay("sp", h)

            @block.scalar
            def _(h):
                replay("act", h)

            @block.vector
            def _(h):
                replay("dve", h)

            @block.gpsimd
            def _(h):
                replay("pool", h)

            @block.tensor
            def _(h):
                replay("pe", h)
        self.stack.close()


def _mm(P, out, lhsT, rhs, start, stop, reads, writes):
    return P.op("pe", lambda h: h.matmul(out, lhsT=lhsT, rhs=rhs, start=start, stop=stop), reads=reads, writes=writes)


def _tr(P, out, in_, ident, reads, writes):
    return P.op("pe", lambda h: h.transpose(out, in_, ident), reads=reads, writes=writes)


def _act(P, out, in_, func, reads, writes, scale=None, bias=None):
    kw = {}
    if scale is not None:
        kw["scale"] = scale
    if bias is not None:
        kw["bias"] = bias
    return P.op("act", lambda h: h.activation(out=out, in_=in_, func=func, **kw), reads=reads, writes=writes)


def _tt(P, eng, out, in0, in1, op, reads, writes):
    return P.op(eng, lambda h: h.tensor_tensor(out=out, in0=in0, in1=in1, op=op), reads=reads, writes=writes)


def _ts(P, eng, out, in0, s1, s2, op0, op1, reads, writes):
    if op1 is None:
        return P.op(eng, lambda h: h.tensor_scalar(out=out, in0=in0, scalar1=s1, scalar2=None, op0=op0), reads=reads, writes=writes)
    return P.op(eng, lambda h: h.tensor_scalar(out=out, in0=in0, scalar1=s1, scalar2=s2, op0=op0, op1=op1), reads=reads, writes=writes)


def _stt(P, out, in0, scalar, in1, op0, op1, reads, writes):
    return P.op("dve", lambda h: h.scalar_tensor_tensor(out=out, in0=in0, scalar=scalar, in1=in1, op0=op0, op1=op1), reads=reads, writes=writes)


def _cp(P, eng, out, in_, reads, writes):
    if eng == "act":
        return P.op("act", lambda h: h.activation(out=out, in_=in_, func=AF.Copy), reads=reads, writes=writes)
    return P.op(eng, lambda h: h.tensor_copy(out=out, in_=in_), reads=reads, writes=writes)


def _ld(P, q, out, in_, writes, reads=()):
    return P.dma(q, lambda h: h.dma_start(out=out, in_=in_), reads=reads, writes=writes)


D = 1024
KC = 8
EPS = 1e-6


def arena_init(P, nbytes=206 * 1024):
    lo, hi = P.nc.bump_sbuf(nbytes)
    P.a_lo, P.a_hi, P.a_cur = lo, hi, lo
    P.a_n = 0


def A(P, shape, dt):
    nb = int(np.prod(shape[1:])) * (4 if dt in (F32, I32) else 2)
    off = (P.a_cur + 31) // 32 * 32
    assert off + nb <= P.a_hi, ("SBUF arena overflow", off + nb - P.a_lo)
    P.a_cur = off + nb
    P.a_n += 1
    return P.nc.alloc_sbuf_tensor_at("t%d" % P.a_n, list(shape), dt, offset=off)


def barrier(P, queues=None):
    tick = [(e, P.cnt[e]) for e in ENGS if P.cnt[e] > 0]
    tick += [(k, 16 * v) for k, v in P.dsem_use.items() if v > 0 and (queues is None or any(k.startswith("d_" + q) for q in queues))]
    for e in ENGS:
        P.finish_wait(e, tick)


def rms_rstd(P, src, srcb, n, sq, sqb, ss_ps, ssb, rstd, rstdb, ones):
    P.op("act", lambda h: h.activation(out=sq[:, :, 0:n], in_=src[:, :, 0:n], func=AF.Square), reads=[srcb], writes=[sqb])
    for k in range(KC):
        P.op("pe", lambda h, k=k: h.matmul(ss_ps[:, 0:n], lhsT=ones[:], rhs=sq[:, k, 0:n], start=(k == 0), stop=(k == KC - 1)),
             reads=[sqb], writes=[ssb])
    P.op("act", lambda h: h.activation(out=rstd[:, 0:n], in_=ss_ps[:, 0:n], func=AF.Ln, scale=1.0 / D, bias=P.eps_t[:, 0:1]), reads=[ssb], writes=[rstdb])
    P.op("act", lambda h: h.activation(out=rstd[:, 0:n], in_=rstd[:, 0:n], func=AF.Exp, scale=-0.5), reads=[rstdb], writes=[rstdb])


def norm_mod(P, src, srcb, n, rstd, rstdb, gm, sh, r, dst, dstb, tmp, tmpb, dst_off=0, dst32=None, dst32b=None):
    for k in range(KC):
        tb = tmpb[k % len(tmp)]
        tt = tmp[k % len(tmp)]
        P.op("dve", lambda h, k=k, tt=tt: h.tensor_tensor(out=tt[:, 0:n], in0=src[:, k, 0:n], in1=rstd[:, 0:n], op=ALU.mult),
             reads=[srcb, rstdb], writes=[tb])
        P.op("act", lambda h, k=k, tt=tt: h.activation(out=dst[:, k, dst_off:dst_off + n], in_=tt[:, 0:n], func=AF.Identity,
                                                     scale=gm[:, k, r:r + 1], bias=sh[:, k, r:r + 1]),
             reads=[tb, P.modb], writes=[dstb])
        if dst32 is not None:
            P.op("pool", lambda h, k=k, tt=tt: h.tensor_scalar(out=dst32[:, k, 0:n], in0=tt[:, 0:n], scalar1=gm[:, k, r:r + 1],
                                                             scalar2=sh[:, k, r:r + 1], op0=ALU.mult, op1=ALU.add),
                 reads=[tb, P.modb], writes=[dst32b])


def compute_mod(P, dr, which, mod_ps, modpb, light=False):
    nc = P.nc
    cs = A(P, [128, KC, 2], F32)
    csb = P.buf()
    P.dma("sp", lambda h: h.dma_start(out=cs[:], in_=dr["condT"][:, :, :]), writes=[csb])
    sig = A(P, [128, KC, 2], F32)
    P.op("act", lambda h: h.activation(out=sig[:], in_=cs[:], func=AF.Sigmoid), reads=[csb], writes=[csb])
    P.op("dve", lambda h: h.tensor_tensor(out=cs[:], in0=cs[:], in1=sig[:], op=ALU.mult), reads=[csb], writes=[csb])
    modT = A(P, [128, 48, 2], F32)
    P.modT = modT
    P.modb = P.buf("mod")
    bm = A(P, [128, 48, 2], F32)
    bmb = P.buf()
    P.dma("sp", lambda h: h.dma_start(out=bm[:], in_=dr["b_modT"][:, :, :]), writes=[bmb])
    ng = A(P, [128, 4, KC, 2], F32)
    P.ng = ng
    P.dma("sp", lambda h: h.dma_start(out=ng[:], in_=dr["norm_gT"][:, :, :, :]), writes=[P.modb])
    mark = P.a_cur
    wm = [A(P, [128, KC, 1024], F32) for _ in range(2)]
    wmb = [P.buf(), P.buf()]
    wsrc = dr["w_mod"].rearrange("(k p) f -> p k f", p=128)
    for i, j in enumerate(which):
        w = wm[i % 2]
        wb = wmb[i % 2]
        for k2 in range(2):
            P.dma("sp", lambda h, w=w, j=j, k2=k2: h.dma_start(out=w[:, 4 * k2:4 * k2 + 4, :], in_=wsrc[:, 4 * k2:4 * k2 + 4, j * 1024:(j + 1) * 1024]), writes=[wb])
        for fc in range(8):
            for k in range(KC):
                P.op("pe", lambda h, w=w, j=j, fc=fc, k=k: h.matmul(mod_ps[:, j * 8 + fc, :], lhsT=w[:, k, fc * 128:(fc + 1) * 128], rhs=cs[:, k, :],
                                                                   start=(k == 0), stop=(k == KC - 1)), reads=[wb, csb], writes=[modpb])
    for j in which:
        P.op("dve", lambda h, j=j: h.tensor_tensor(out=modT[:, j * 8:(j + 1) * 8, :], in0=mod_ps[:, j * 8:(j + 1) * 8, :], in1=bm[:, j * 8:(j + 1) * 8, :], op=ALU.add),
             reads=[modpb, bmb], writes=[P.modb])
    barrier(P, ("sp",) if light else None)
    P.a_cur = mark


def mod_derived(P, jsc, jg, gi_norm, gi_gate):
    gm = A(P, [128, KC, 2], F32)
    gg = A(P, [128, KC, 2], F32)
    modT, ng = P.modT, P.ng
    P.op("dve", lambda h: h.scalar_tensor_tensor(out=gm[:], in0=modT[:, jsc * 8:(jsc + 1) * 8, :], scalar=1.0, in1=ng[:, gi_norm, :, :],
                                                 op0=ALU.add, op1=ALU.mult), reads=[P.modb], writes=[P.modb])
    if jg is not None:
        P.op("dve", lambda h: h.tensor_tensor(out=gg[:], in0=modT[:, jg * 8:(jg + 1) * 8, :], in1=ng[:, gi_gate, :, :], op=ALU.mult),
             reads=[P.modb], writes=[P.modb])
    return gm, gg


def build_F(blocks, blocksB, n_exp, dff, moe, DBG=False, mode='full'):
    TT = sum(b[1] for b in blocks)
    nc = bass.Bass("TRN2", target_bir_lowering=False)
    dr = {}

    def din(name, shape, dt=F32):
        dr[name] = nc.dram_tensor(name, list(shape), dt, kind="ExternalInput").ap()
    din("xT", [KC, 128, TT])
    din("brT", [KC, 128, TT], BF16)
    din("condT", [128, KC, 2])
    din("w_mod", [D, 6 * D])
    din("b_modT", [128, 48, 2])
    din("norm_gT", [128, 4, KC, 2])
    din("w_in", [D, 6400])
    din("w_br", [KC * 128, D])
    din("w_o", [D, D])
    din("w_glu", [256, 256])
    din("b_gluT", [128, 2])
    if mode == 'full':
        din("w_g", [n_exp, D, dff])
        din("w_u", [n_exp, D, dff])
        din("w_d", [n_exp, dff, D])
    if moe:
        din("w_r", [D, 8])
        din("b_r", [128, 8])
        din("ident", [128, 128])
        din("sel", [8, 8, 128])
    if mode == 'moe_a':
        h2o = nc.dram_tensor("h2o", [KC, 128, TT], BF16, kind="ExternalOutput").ap().rearrange("k p t -> p k t")
        cbo = nc.dram_tensor("cbo", [8, TT], BF16, kind="ExternalOutput").ap()
        mko = nc.dram_tensor("mko", [8, TT], BF16, kind="ExternalOutput").ap()
    xo = nc.dram_tensor("xo", [KC, 128, TT], F32, kind="ExternalOutput").ap()
    xoT = xo.rearrange("k p t -> p k t")
    if DBG: dbg_mod = nc.dram_tensor("dbg_mod", [128, 48, 2], F32, kind="ExternalOutput").ap()
    if DBG: dbg_xm = nc.dram_tensor("dbg_xm", [KC, 128, TT], F32, kind="ExternalOutput").ap().rearrange("k p t -> p k t")
    if DBG: dbg_h = nc.dram_tensor("dbg_h", [KC, 128, TT], BF16, kind="ExternalOutput").ap().rearrange("k p t -> p k t")
    if DBG: dbg_z = nc.dram_tensor("dbg_z", [KC, 128, TT], F32, kind="ExternalOutput").ap().rearrange("k p t -> p k t")
    if DBG: dbg_r = nc.dram_tensor("dbg_r", [128, TT], F32, kind="ExternalOutput").ap()
    if DBG: dbg_sq = nc.dram_tensor("dbg_sq", [KC, 128, TT], BF16, kind="ExternalOutput").ap().rearrange("k p t -> p k t")
    if DBG: dbg_ss = nc.dram_tensor("dbg_ss", [128, TT], F32, kind="ExternalOutput").ap()
    sscp = A(P, [128, 256], F32) if False else None
    if DBG: dbg_y = nc.dram_tensor("dbg_y", [KC, 128, TT], BF16, kind="ExternalOutput").ap().rearrange("k p t -> p k t")
    xT = dr["xT"].rearrange("k p t -> p k t")
    brT = dr["brT"].rearrange("k p t -> p k t")

    P = Prog(nc)
    arena_init(P)
    ps = [P.ps("ps%d" % i, [128, 512], F32) for i in range(8)]
    psb = [P.buf("ps%d" % i) for i in range(8)]
    ones = A(P, [128, 128], BF16)
    onesb = P.buf()
    P.op("dve", lambda h: h.memset(ones[:], 1.0), writes=[onesb])
    P.eps_t = A(P, [128, 1], F32)
    P.op("dve", lambda h: h.memset(P.eps_t[:], EPS), writes=[onesb])

    w_lo = P.a_cur
    wgt = A(P, [128, KC, 4096], BF16)
    wbr = A(P, [128, KC, D], BF16)
    wo = A(P, [128, KC, D], BF16)
    wAb = P.buf("wA")
    wbufs = []

    def _wb():
        wbufs.append(P.buf())
        return wbufs[-1]
    w_in_v = dr["w_in"].rearrange("(k p) c -> p k c", p=128)
    for k in range(KC):
        for c4 in range(2):
            P.dma("pool", lambda h, k=k, c4=c4: h.dma_start(out=wgt[:, k, c4 * 2048:(c4 + 1) * 2048], in_=w_in_v[:, k, 2304 + c4 * 2048:2304 + (c4 + 1) * 2048]), writes=[_wb()])
    P.dma("pool", lambda h: h.dma_start(out=wbr[:, 0:4, :], in_=dr["w_br"].rearrange("(k p) c -> p k c", p=128)[:, 0:4, :]), writes=[_wb()])
    P.dma("pool", lambda h: h.dma_start(out=wbr[:, 4:8, :], in_=dr["w_br"].rearrange("(k p) c -> p k c", p=128)[:, 4:8, :]), writes=[_wb()])
    P.dma("pool", lambda h: h.dma_start(out=wo[:, 0:4, :], in_=dr["w_o"].rearrange("(k p) c -> p k c", p=128)[:, 0:4, :]), writes=[_wb()])
    P.dma("pool", lambda h: h.dma_start(out=wo[:, 4:8, :], in_=dr["w_o"].rearrange("(k p) c -> p k c", p=128)[:, 4:8, :]), writes=[_wb()])

    wglu = A(P, [128, 2, 256], BF16)
    bglu = A(P, [128, 2], F32)
    P.dma("pool", lambda h: h.dma_start(out=wglu[:], in_=dr["w_glu"].rearrange("(k p) c -> p k c", p=128)), writes=[_wb()])
    P.dma("sp", lambda h: h.dma_start(out=bglu[:], in_=dr["b_gluT"][:, :]), writes=[_wb()])
    w_hi = P.a_cur
    mod_ps = nc.alloc_psum_tensor
    mod_view = ps[7][:, 0:96].rearrange("p (j r) -> p j r", r=2)
    compute_mod(P, dr, [0, 1, 2, 3, 4, 5], mod_view, psb[7], light=True)
    gm_a, gg_a = mod_derived(P, 1, 2, 0, 1)
    gm_f, gg_f = mod_derived(P, 4, 5, 2, 3)
    sh_a = P.modT[:, 0:8, :]
    sh_f = P.modT[:, 24:32, :]
    P.dbgt = []
    if DBG: P.dbgt += [P.dma("sp", lambda h: h.dma_start(out=dbg_mod[:, :, :], in_=P.modT[:]), reads=[P.modb], writes=[P.buf()])]

    h2 = A(P, [128, KC, TT], BF16)
    h2b = P.buf("h2")
    if moe:
        cbT = A(P, [8, TT], BF16)
        cbTb = P.buf("cbT")
        mkT = A(P, [8, TT], BF16)
        mkTb = P.buf("mkT")
        ident = A(P, [128, 128], F32)
        P.dma("sp", lambda h: h.dma_start(out=ident[:], in_=dr["ident"][:, :]), writes=[onesb])
        wr = A(P, [128, KC, 8], F32)
        P.dma("sp", lambda h: h.dma_start(out=wr[:], in_=dr["w_r"].rearrange("(k p) e -> p k e", p=128)), writes=[onesb])
        br_t = A(P, [128, 8], F32)
        P.dma("sp", lambda h: h.dma_start(out=br_t[:], in_=dr["b_r"][:, :]), writes=[onesb])
        sel = A(P, [8, 8, 128], BF16)
        P.dma("pool", lambda h: h.dma_start(out=sel[:], in_=dr["sel"][:, :, :]), writes=[onesb])
    markA = P.a_cur
    wjoin = A(P, [128, 1], F32)
    P.op("dve", lambda h: h.memset(wjoin[:], 0.0), reads=wbufs, writes=[wAb])
    glu_t = A(P, [128, 2, 256], BF16)
    glub = P.buf("glu")
    sgl = A(P, [128, 256], F32)
    sglb = P.buf("sgl")
    xb = [A(P, [128, KC, 256], F32) for _ in range(2)]
    xbb = [P.buf(), P.buf()]
    brb_t = [A(P, [128, KC, 256], BF16) for _ in range(2)]
    brbb = [P.buf(), P.buf()]
    sq = A(P, [128, KC, 256], BF16)
    sqb = P.buf()
    rstd = A(P, [128, 256], F32)
    rstdb = P.buf()
    tmp = [A(P, [128, 256], F32) for _ in range(2)]
    tmpb = [P.buf(), P.buf()]
    hb = A(P, [128, KC, 256], BF16)
    hbb = P.buf()
    yb = A(P, [128, KC, 256], BF16)
    ybb = P.buf()
    zb = A(P, [128, KC, 256], F32)
    zbb = P.buf()
    sg = [A(P, [128, 256], F32) for _ in range(2)]
    sgb = [P.buf(), P.buf()]
    tt2 = [A(P, [128, 256], F32) for _ in range(2)]
    tt2b = [P.buf(), P.buf()]
    accA = [A(P, [128, 256], F32) for _ in range(2)]
    accAb = [P.buf(), P.buf()]
    xob = P.buf("xo")
    h2fb = P.buf("h2f")
    if moe:
        h2f = A(P, [128, KC, 256], F32)
        lg = A(P, [128, 8], F32)
        mx8 = A(P, [128, 8], F32)
        msk = A(P, [128, 8], F32)
        ex = A(P, [128, 8], F32)
        den = A(P, [128, 1], F32)
        nmx = A(P, [128, 1], F32)
        rb = P.buf("router")
    cnt = 0
    P.sscp = A(P, [128, 256], F32)
    P.sscpb = P.buf()
    def _ldA(bi_):
        t0_, n_, _r = blocks[bi_]
        xx, xxb = xb[bi_ % 2], xbb[bi_ % 2]
        bb_, bbb = brb_t[bi_ % 2], brbb[bi_ % 2]
        P.dma("sp", lambda h: h.dma_start(out=xx[:, 0:4, 0:n_], in_=xT[:, 0:4, t0_:t0_ + n_]), writes=[xxb])
        P.dma("sp", lambda h: h.dma_start(out=xx[:, 4:8, 0:n_], in_=xT[:, 4:8, t0_:t0_ + n_]), writes=[xxb])
        P.dma("sp", lambda h: h.dma_start(out=bb_[:, :, 0:n_], in_=brT[:, :, t0_:t0_ + n_]), writes=[bbb])
    _ldA(0)
    for bi, (t0, n, r) in enumerate(blocks):
        x_t, x_b = xb[bi % 2], xbb[bi % 2]
        b_t, b_b = brb_t[bi % 2], brbb[bi % 2]
        if bi + 1 < len(blocks):
            _ldA(bi + 1)
        rms_rstd(P, x_t, x_b, n, sq, sqb, ps[6], psb[6], rstd, rstdb, ones)
        norm_mod(P, x_t, x_b, n, rstd, rstdb, gm_a, sh_a, r, hb, hbb, tmp, tmpb)
        for oc in range(2):
            for kc in range(2):
                P.op("pe", lambda h, oc=oc, kc=kc, n=n, b_t=b_t: h.matmul(ps[6][:, 0:n], lhsT=wglu[:, kc, oc * 128:(oc + 1) * 128], rhs=b_t[:, 2 + kc, 0:n],
                                                                      start=(kc == 0), stop=(kc == 1)), reads=[wAb, b_b], writes=[psb[6]])
            P.op("act", lambda h, oc=oc, n=n: h.activation(out=sgl[:, 0:n], in_=ps[6][:, 0:n], func=AF.Sigmoid, bias=bglu[:, oc:oc + 1], scale=1.0), reads=[psb[6], wAb], writes=[sglb])
            P.op("dve", lambda h, oc=oc, n=n, b_t=b_t: h.tensor_tensor(out=glu_t[:, oc, 0:n], in0=sgl[:, 0:n], in1=b_t[:, 2 + oc, 0:n], op=ALU.mult), reads=[sglb, b_b], writes=[glub])
        for fc in range(8):
            ac, acb = accA[fc % 2], accAb[fc % 2]
            for b in range(4):
                gi = cnt % 2
                cnt += 1
                gps, gpb = ps[gi], psb[gi]
                pps, ppb = ps[2 + gi], psb[2 + gi]
                for k in range(KC):
                    P.op("pe", lambda h, gps=gps, k=k, b=b, fc=fc, n=n: h.matmul(gps[:, 0:n], lhsT=wgt[:, k, b * 1024 + fc * 128:b * 1024 + (fc + 1) * 128], rhs=hb[:, k, 0:n],
                                                                                 start=(k == 0), stop=(k == KC - 1)), reads=[wAb, hbb], writes=[gpb])
                for hh in range(2):
                    rhs_ap = glu_t[:, hh, 0:n] if b == 1 else b_t[:, 2 * b + hh, 0:n]
                    P.op("pe", lambda h, pps=pps, hh=hh, b=b, fc=fc, n=n, rhs_ap=rhs_ap: h.matmul(pps[:, 0:n], lhsT=wbr[:, 2 * b + hh, fc * 128:(fc + 1) * 128], rhs=rhs_ap,
                                                                                          start=(hh == 0), stop=(hh == 1)), reads=[wAb, b_b, glub], writes=[ppb])
                s_t, s_b = sg[gi], sgb[gi]
                P.op("act", lambda h, s_t=s_t, gps=gps, n=n: h.activation(out=s_t[:, 0:n], in_=gps[:, 0:n], func=AF.Sigmoid), reads=[gpb], writes=[s_b])
                if b == 0:
                    P.op("dve", lambda h, ac=ac, s_t=s_t, pps=pps, n=n: h.tensor_tensor(out=ac[:, 0:n], in0=s_t[:, 0:n], in1=pps[:, 0:n], op=ALU.mult),
                         reads=[s_b, ppb], writes=[acb])
                else:
                    t_t, t_b = tt2[gi], tt2b[gi]
                    P.op("dve", lambda h, t_t=t_t, s_t=s_t, pps=pps, n=n: h.tensor_tensor(out=t_t[:, 0:n], in0=s_t[:, 0:n], in1=pps[:, 0:n], op=ALU.mult),
                         reads=[s_b, ppb], writes=[t_b])
                    if b < 3:
                        P.op("pool", lambda h, ac=ac, t_t=t_t, n=n: h.tensor_tensor(out=ac[:, 0:n], in0=ac[:, 0:n], in1=t_t[:, 0:n], op=ALU.add),
                             reads=[acb, t_b], writes=[acb])
                    else:
                        P.op("pool", lambda h, ac=ac, t_t=t_t, n=n, fc=fc: h.tensor_tensor(out=yb[:, fc, 0:n], in0=ac[:, 0:n], in1=t_t[:, 0:n], op=ALU.add),
                             reads=[acb, t_b], writes=[ybb])
        for fc in range(8):
            zi = 4 + fc % 2
            for k in range(KC):
                P.op("pe", lambda h, zi=zi, k=k, fc=fc, n=n: h.matmul(ps[zi][:, 0:n], lhsT=wo[:, k, fc * 128:(fc + 1) * 128], rhs=yb[:, k, 0:n], start=(k == 0), stop=(k == KC - 1)),
                     reads=[wAb, ybb], writes=[psb[zi]])
            P.op("act", lambda h, zi=zi, fc=fc, n=n: h.activation(out=zb[:, fc, 0:n], in_=ps[zi][:, 0:n], func=AF.Copy), reads=[psb[zi]], writes=[zbb])
        rms_rstd(P, zb, zbb, n, sq, sqb, ps[6], psb[6], rstd, rstdb, ones)
        if DBG: P.dbgt.append(P.dma("sp", lambda h, t0=t0, n=n: h.dma_start(out=dbg_z[:, :, t0:t0 + n], in_=zb[:, :, 0:n]), reads=[zbb], writes=[P.buf()]))
        if DBG: P.dbgt.append(P.dma("sp", lambda h, t0=t0, n=n: h.dma_start(out=dbg_r[:, t0:t0 + n], in_=rstd[:, 0:n]), reads=[rstdb], writes=[P.buf()]))
        if DBG: P.dbgt.append(P.dma("sp", lambda h, t0=t0, n=n: h.dma_start(out=dbg_sq[:, :, t0:t0 + n], in_=sq[:, :, 0:n]), reads=[sqb], writes=[P.buf()]))
        if DBG: P.op("dve", lambda h, n=n: h.tensor_copy(out=P.sscp[:, 0:n], in_=ps[6][:, 0:n]), reads=[psb[6]], writes=[P.sscpb])
        if DBG: P.dbgt.append(P.dma("sp", lambda h, t0=t0, n=n: h.dma_start(out=dbg_ss[:, t0:t0 + n], in_=P.sscp[:, 0:n]), reads=[P.sscpb], writes=[P.buf()]))
        for k in range(KC):
            tb_, tt_ = tmpb[k % 2], tmp[k % 2]
            P.op("dve", lambda h, k=k, tt_=tt_, n=n: h.tensor_tensor(out=tt_[:, 0:n], in0=zb[:, k, 0:n], in1=rstd[:, 0:n], op=ALU.mult), reads=[zbb, rstdb], writes=[tb_])
            P.op("dve", lambda h, k=k, tt_=tt_, n=n, x_t=x_t, r=r: h.scalar_tensor_tensor(out=x_t[:, k, 0:n], in0=tt_[:, 0:n], scalar=gg_a[:, k, r:r + 1], in1=x_t[:, k, 0:n],
                                                                                    op0=ALU.mult, op1=ALU.add), reads=[tb_, P.modb, x_b], writes=[x_b])
        P.dma("sp", lambda h, x_t=x_t, t0=t0, n=n: h.dma_start(out=xoT[:, :, t0:t0 + n], in_=x_t[:, :, 0:n]), reads=[x_b], writes=[xob])
        if DBG: P.dbgt.append(P.dma("sp", lambda h, x_t=x_t, t0=t0, n=n: h.dma_start(out=dbg_xm[:, :, t0:t0 + n], in_=x_t[:, :, 0:n]), reads=[x_b], writes=[P.buf()]))
        if DBG: P.dbgt.append(P.dma("sp", lambda h, t0=t0, n=n: h.dma_start(out=dbg_h[:, :, t0:t0 + n], in_=hb[:, :, 0:n]), reads=[hbb], writes=[P.buf()]))
        if DBG: P.dbgt.append(P.dma("sp", lambda h, t0=t0, n=n: h.dma_start(out=dbg_y[:, :, t0:t0 + n], in_=yb[:, :, 0:n]), reads=[ybb], writes=[P.buf()]))
        rms_rstd(P, x_t, x_b, n, sq, sqb, ps[6], psb[6], rstd, rstdb, ones)
        norm_mod(P, x_t, x_b, n, rstd, rstdb, gm_f, sh_f, r, h2, h2b, tmp, tmpb, dst_off=t0, dst32=(h2f if moe else None), dst32b=h2fb)
        if moe:
            for tt in range(n // 128):
                for k in range(KC):
                    P.op("pe", lambda h, k=k, tt=tt: h.matmul(ps[7][:, 0:8], lhsT=h2f[:, k, tt * 128:(tt + 1) * 128], rhs=wr[:, k, :], start=(k == 0), stop=(k == KC - 1)),
                         reads=[h2fb, onesb], writes=[psb[7]])
                P.op("dve", lambda h: h.tensor_tensor(out=lg[:], in0=ps[7][:, 0:8], in1=br_t[:], op=ALU.add), reads=[psb[7], onesb], writes=[rb])
                P.op("dve", lambda h: h.max(out=mx8[:], in_=lg[:]), reads=[rb], writes=[rb])
                P.op("dve", lambda h: h.tensor_scalar(out=msk[:], in0=lg[:], scalar1=mx8[:, 1:2], scalar2=None, op0=ALU.is_ge), reads=[rb], writes=[rb])
                P.op("dve", lambda h: h.tensor_scalar(out=nmx[:], in0=mx8[:, 0:1], scalar1=-1.0, scalar2=None, op0=ALU.mult), reads=[rb], writes=[rb])
                P.op("act", lambda h: h.activation(out=ex[:], in_=lg[:], func=AF.Exp, bias=nmx[:, 0:1], scale=1.0), reads=[rb], writes=[rb])
                P.op("dve", lambda h: h.tensor_tensor(out=ex[:], in0=ex[:], in1=msk[:], op=ALU.mult), reads=[rb], writes=[rb])
                P.op("dve", lambda h: h.reduce_sum(out=den[:], in_=ex[:], axis=AX.X), reads=[rb], writes=[rb])
                P.op("dve", lambda h: h.reciprocal(out=den[:], in_=den[:]), reads=[rb], writes=[rb])
                P.op("dve", lambda h: h.tensor_scalar(out=ex[:], in0=ex[:], scalar1=den[:, 0:1], scalar2=None, op0=ALU.mult), reads=[rb], writes=[rb])
                P.op("pe", lambda h: h.transpose(ps[7][0:8, 128:256], ex[:], ident[:]), reads=[rb, onesb], writes=[psb[7]])
                P.op("act", lambda h, t0=t0, tt=tt: h.activation(out=cbT[:, t0 + tt * 128:t0 + (tt + 1) * 128], in_=ps[7][0:8, 128:256], func=AF.Copy), reads=[psb[7]], writes=[cbTb])
                if mode == 'moe_a':
                    P.op("pe", lambda h: h.transpose(ps[7][0:8, 256:384], msk[:], ident[:]), reads=[rb, onesb], writes=[psb[7]])
                    P.op("act", lambda h, t0=t0, tt=tt: h.activation(out=mkT[:, t0 + tt * 128:t0 + (tt + 1) * 128], in_=ps[7][0:8, 256:384], func=AF.Copy), reads=[psb[7]], writes=[mkTb])
    barrier(P)
    P.a_cur = markA
    if mode == 'moe_a':
        fin = [P.dma("sp", lambda h: h.dma_start(out=h2o[:, :, :], in_=h2[:, :, :]), reads=[h2b], writes=[P.buf()]),
               P.dma("sp", lambda h: h.dma_start(out=cbo[:, :], in_=cbT[:, :]), reads=[cbTb], writes=[P.buf()]),
               P.dma("sp", lambda h: h.dma_start(out=mko[:, :], in_=mkT[:, :]), reads=[mkTb], writes=[P.buf()])]
        barrier(P)
        P.finish_wait("sp", fin + P.dbgt)
        P.emit()
        return nc
    blocks = blocksB
    NSL = 4
    P.a_cur = w_lo
    acc = A(P, [128, KC, TT], F32)
    accb = [P.buf() for _ in blocks]
    hid = [A(P, [128, NSL, 512], BF16) for _ in range(2)]
    hidb = [P.buf(), P.buf()]
    ssb_t = [A(P, [128, 512], F32) for _ in range(2)]
    ssbb = [P.buf(), P.buf()]
    cbe = A(P, [128, 512], BF16)
    cbeb = P.buf()
    assert P.a_cur <= w_hi, "stage-B tiles overflow the weight region"
    P.a_cur = markA
    markB = markA
    wg_s = [A(P, [128, KC, NSL * 128], BF16) for _ in range(2)]
    wu_s = [A(P, [128, KC, NSL * 128], BF16) for _ in range(2)]
    wd_s = [A(P, [128, NSL, D], BF16) for _ in range(2)]
    wsb = [P.buf(), P.buf()]
    ntile = dff // 128
    slices = [(s0, min(NSL, ntile - s0)) for s0 in range(0, ntile, NSL)]
    si = 0
    hcnt = 0
    gcnt = 0
    work = [(e, s0, ns) for e in range(n_exp) for (s0, ns) in slices]

    def _ldW(widx):
        e_, s0_, ns_ = work[widx]
        wi_ = widx % 2
        wgv_ = dr["w_g"][e_].rearrange("(k p) f -> p k f", p=128)
        wuv_ = dr["w_u"][e_].rearrange("(k p) f -> p k f", p=128)
        wdv_ = dr["w_d"][e_].rearrange("(j p) c -> p j c", p=128)
        for k2 in range(2):
            P.dma("pool", lambda h, k2=k2: h.dma_start(out=wg_s[wi_][:, 4 * k2:4 * k2 + 4, 0:ns_ * 128], in_=wgv_[:, 4 * k2:4 * k2 + 4, s0_ * 128:(s0_ + ns_) * 128]), writes=[wsb[wi_]])
            P.dma("pool", lambda h, k2=k2: h.dma_start(out=wu_s[wi_][:, 4 * k2:4 * k2 + 4, 0:ns_ * 128], in_=wuv_[:, 4 * k2:4 * k2 + 4, s0_ * 128:(s0_ + ns_) * 128]), writes=[wsb[wi_]])
        for j in range(ns_):
            P.dma("pool", lambda h, j=j: h.dma_start(out=wd_s[wi_][:, j, :], in_=wdv_[:, s0_ + j, :]), writes=[wsb[wi_]])
    _ldW(0)
    for widx, (e, s0, ns) in enumerate(work):
        if True:
            wi = widx % 2
            if widx + 1 < len(work):
                _ldW(widx + 1)
            for bi, (t0, n, r) in enumerate(blocks):
                hi = hcnt % 2
                hcnt += 1
                if moe:
                    P.op("pe", lambda h, e=e, t0=t0, n=n: h.matmul(ps[7][:, 0:n], lhsT=sel[:, e, :], rhs=cbT[:, t0:t0 + n], start=True, stop=True), reads=[cbTb, onesb], writes=[psb[7]])
                    P.op("act", lambda h, n=n: h.activation(out=cbe[:, 0:n], in_=ps[7][:, 0:n], func=AF.Copy), reads=[psb[7]], writes=[cbeb])
                for j in range(ns):
                    gi = gcnt % 2
                    gcnt += 1
                    for k in range(KC):
                        P.op("pe", lambda h, gi=gi, wi=wi, j=j, k=k, t0=t0, n=n: h.matmul(ps[gi][:, 0:n], lhsT=wg_s[wi][:, k, j * 128:(j + 1) * 128], rhs=h2[:, k, t0:t0 + n], start=(k == 0), stop=(k == KC - 1)),
                             reads=[wsb[wi], h2b], writes=[psb[gi]])
                    for k in range(KC):
                        P.op("pe", lambda h, gi=gi, wi=wi, j=j, k=k, t0=t0, n=n: h.matmul(ps[2 + gi][:, 0:n], lhsT=wu_s[wi][:, k, j * 128:(j + 1) * 128], rhs=h2[:, k, t0:t0 + n], start=(k == 0), stop=(k == KC - 1)),
                             reads=[wsb[wi], h2b], writes=[psb[2 + gi]])
                    P.op("act", lambda h, gi=gi, n=n: h.activation(out=ssb_t[gi][:, 0:n], in_=ps[gi][:, 0:n], func=AF.Silu), reads=[psb[gi]], writes=[ssbb[gi]])
                    if moe:
                        P.op("dve", lambda h, gi=gi, n=n: h.tensor_tensor(out=ssb_t[gi][:, 0:n], in0=ssb_t[gi][:, 0:n], in1=ps[2 + gi][:, 0:n], op=ALU.mult),
                             reads=[ssbb[gi], psb[2 + gi]], writes=[ssbb[gi]])
                        P.op("pool", lambda h, gi=gi, hi=hi, j=j, n=n: h.tensor_tensor(out=hid[hi][:, j, 0:n], in0=ssb_t[gi][:, 0:n], in1=cbe[:, 0:n], op=ALU.mult),
                             reads=[ssbb[gi], cbeb], writes=[hidb[hi]])
                    else:
                        P.op("dve", lambda h, gi=gi, hi=hi, j=j, n=n: h.tensor_tensor(out=hid[hi][:, j, 0:n], in0=ssb_t[gi][:, 0:n], in1=ps[2 + gi][:, 0:n], op=ALU.mult),
                             reads=[ssbb[gi], psb[2 + gi]], writes=[hidb[hi]])
                first = (e == 0 and s0 == 0)
                for fc in range(8):
                    oi = 4 + fc % 2
                    for j in range(ns):
                        P.op("pe", lambda h, oi=oi, wi=wi, j=j, fc=fc, hi=hi, n=n, ns=ns: h.matmul(ps[oi][:, 0:n], lhsT=wd_s[wi][:, j, fc * 128:(fc + 1) * 128], rhs=hid[hi][:, j, 0:n], start=(j == 0), stop=(j == ns - 1)),
                             reads=[wsb[wi], hidb[hi]], writes=[psb[oi]])
                    if first:
                        P.op("act", lambda h, oi=oi, fc=fc, t0=t0, n=n: h.activation(out=acc[:, fc, t0:t0 + n], in_=ps[oi][:, 0:n], func=AF.Copy), reads=[psb[oi]], writes=[accb[bi]])
                    else:
                        P.op("dve", lambda h, oi=oi, fc=fc, t0=t0, n=n: h.tensor_tensor(out=acc[:, fc, t0:t0 + n], in0=acc[:, fc, t0:t0 + n], in1=ps[oi][:, 0:n], op=ALU.add),
                             reads=[psb[oi], accb[bi]], writes=[accb[bi]])
    barrier(P)
    P.a_cur = markB
    xm = [A(P, [128, KC, 512], F32) for _ in range(2)]
    xmb = [P.buf(), P.buf()]
    sqF = A(P, [128, KC, 512], BF16)
    rstdF = A(P, [128, 512], F32)
    tmpF = [A(P, [128, 512], F32) for _ in range(2)]
    outs = []
    for bi, (t0, n, r) in enumerate(blocks):
        x_t, x_b = xm[bi % 2], xmb[bi % 2]
        P.dma("sp", lambda h, x_t=x_t, t0=t0, n=n: h.dma_start(out=x_t[:, :, 0:n], in_=xoT[:, :, t0:t0 + n]), reads=[xob], writes=[x_b])
        accv = acc[:, :, t0:t0 + n]
        P.op("act", lambda h, accv=accv, n=n: h.activation(out=sqF[:, :, 0:n], in_=accv, func=AF.Square), reads=[accb[bi]], writes=[sqb])
        for k in range(KC):
            P.op("pe", lambda h, k=k, n=n: h.matmul(ps[6][:, 0:n], lhsT=ones[:], rhs=sqF[:, k, 0:n], start=(k == 0), stop=(k == KC - 1)), reads=[sqb, onesb], writes=[psb[6]])
        P.op("act", lambda h, n=n: h.activation(out=rstdF[:, 0:n], in_=ps[6][:, 0:n], func=AF.Ln, scale=1.0 / D, bias=P.eps_t[:, 0:1]), reads=[psb[6]], writes=[rstdb])
        P.op("act", lambda h, n=n: h.activation(out=rstdF[:, 0:n], in_=rstdF[:, 0:n], func=AF.Exp, scale=-0.5), reads=[rstdb], writes=[rstdb])
        for k in range(KC):
            tb_, tt_ = tmpb[k % 2], tmpF[k % 2]
            P.op("dve", lambda h, k=k, tt_=tt_, n=n, t0=t0: h.tensor_tensor(out=tt_[:, 0:n], in0=acc[:, k, t0:t0 + n], in1=rstdF[:, 0:n], op=ALU.mult), reads=[accb[bi], rstdb], writes=[tb_])
            P.op("dve", lambda h, k=k, tt_=tt_, n=n, x_t=x_t, r=r: h.scalar_tensor_tensor(out=x_t[:, k, 0:n], in0=tt_[:, 0:n], scalar=gg_f[:, k, r:r + 1], in1=x_t[:, k, 0:n],
                                                                                    op0=ALU.mult, op1=ALU.add), reads=[tb_, P.modb, x_b], writes=[x_b])
        outs.append(P.dma("sp", lambda h, x_t=x_t, t0=t0, n=n: h.dma_start(out=xoT[:, :, t0:t0 + n], in_=x_t[:, :, 0:n]), reads=[x_b], writes=[xob]))
    P.finish_wait("sp", outs + P.dbgt)
    P.emit()
    return nc


def build_E(groups=(4,) * 8, dff=3584):
    nc = bass.Bass("TRN2", target_bir_lowering=False)
    ngrp = len(groups)
    gtok = 512 * max(groups)
    NT = 512 * sum(groups)
    goff = [512 * sum(groups[:g]) for g in range(ngrp)]
    h2d = nc.dram_tensor("h2", [KC, 128, NT], BF16, kind="ExternalInput").ap().rearrange("k p t -> p k t")
    cbd = nc.dram_tensor("cbe", [128, NT], BF16, kind="ExternalInput").ap()
    wgd = nc.dram_tensor("w_g", [D, dff], F32, kind="ExternalInput").ap().rearrange("(k p) f -> p k f", p=128)
    wud = nc.dram_tensor("w_u", [D, dff], F32, kind="ExternalInput").ap().rearrange("(k p) f -> p k f", p=128)
    wdd = nc.dram_tensor("w_d", [dff, D], F32, kind="ExternalInput").ap().rearrange("(j p) c -> p j c", p=128)
    ye = nc.dram_tensor("ye", [KC, 128, NT], F32, kind="ExternalOutput").ap().rearrange("k p t -> p k t")
    P = Prog(nc)
    arena_init(P)
    ps = [P.ps("ps%d" % i, [128, 512], F32) for i in range(8)]
    psb = [P.buf("ps%d" % i) for i in range(8)]
    h2g = [A(P, [128, KC, gtok], BF16) for _ in range(2)]
    h2gb = [P.buf(), P.buf()]
    cbg = [A(P, [128, gtok], BF16) for _ in range(2)]
    acc = A(P, [128, KC, gtok], F32)
    NSL = 4
    wg_s = [A(P, [128, KC, NSL * 128], BF16) for _ in range(2)]
    wu_s = [A(P, [128, KC, NSL * 128], BF16) for _ in range(2)]
    wd_s = [A(P, [128, NSL, D], BF16) for _ in range(2)]
    wsb = [P.buf(), P.buf()]
    hid = [A(P, [128, NSL, 512], BF16) for _ in range(2)]
    hidb = [P.buf(), P.buf()]
    ssb_t = [A(P, [128, 512], F32) for _ in range(2)]
    ssbb = [P.buf(), P.buf()]
    ntile = dff // 128
    slices = [(s0, min(NSL, ntile - s0)) for s0 in range(0, ntile, NSL)]
    accb = [P.buf() for _ in range(max(groups))]
    si = hcnt = gcnt = 0
    outs = []
    work = [(g, sidx, s0, ns) for g in range(ngrp) for sidx, (s0, ns) in enumerate(slices)]

    def _ldG(g_):
        hg_, hgb_ = h2g[g_ % 2], h2gb[g_ % 2]
        gn_ = 512 * groups[g_]
        for k2 in range(2):
            _ldF(P, "sp", hg_[:, 4 * k2:4 * k2 + 4, 0:gn_], h2d[:, 4 * k2:4 * k2 + 4, goff[g_]:goff[g_] + gn_], [hgb_])
        _ldF(P, "sp", cbg[g_ % 2][:, 0:gn_], cbd[:, goff[g_]:goff[g_] + gn_], [hgb_])

    def _ldW(widx):
        _g, _sidx, s0_, ns_ = work[widx]
        wi_ = widx % 2
        for k2 in range(2):
            _ldF(P, "pool", wg_s[wi_][:, 4 * k2:4 * k2 + 4, 0:ns_ * 128], wgd[:, 4 * k2:4 * k2 + 4, s0_ * 128:(s0_ + ns_) * 128], [wsb[wi_]])
            _ldF(P, "pool", wu_s[wi_][:, 4 * k2:4 * k2 + 4, 0:ns_ * 128], wud[:, 4 * k2:4 * k2 + 4, s0_ * 128:(s0_ + ns_) * 128], [wsb[wi_]])
        for j in range(ns_):
            _ldF(P, "pool", wd_s[wi_][:, j, :], wdd[:, s0_ + j, :], [wsb[wi_]])
    _ldG(0)
    _ldW(0)
    for widx, (g, sidx, s0, ns) in enumerate(work):
        hg, hgb = h2g[g % 2], h2gb[g % 2]
        cg = cbg[g % 2]
        nblk = groups[g]
        if sidx == 0 and g + 1 < ngrp:
            _ldG(g + 1)
        if True:
            wi = widx % 2
            if widx + 1 < len(work):
                _ldW(widx + 1)
            for bi in range(nblk):
                t0, n = bi * 512, 512
                hi = hcnt % 2
                hcnt += 1
                for j in range(ns):
                    gi = gcnt % 2
                    gcnt += 1
                    for k in range(KC):
                        _mmF(P, ps[gi][:, 0:n], wg_s[wi][:, k, j * 128:(j + 1) * 128], hg[:, k, t0:t0 + n], k == 0, k == KC - 1, [wsb[wi], hgb], [psb[gi]])
                    for k in range(KC):
                        _mmF(P, ps[2 + gi][:, 0:n], wu_s[wi][:, k, j * 128:(j + 1) * 128], hg[:, k, t0:t0 + n], k == 0, k == KC - 1, [wsb[wi], hgb], [psb[2 + gi]])
                    st_, stb_ = ssb_t[gi], ssbb[gi]
                    P.op("act", lambda h, st_=st_, gi=gi, n=n: h.activation(out=st_[:, 0:n], in_=ps[gi][:, 0:n], func=AF.Silu), reads=[psb[gi]], writes=[stb_])
                    P.op("dve", lambda h, st_=st_, gi=gi, n=n: h.tensor_tensor(out=st_[:, 0:n], in0=st_[:, 0:n], in1=ps[2 + gi][:, 0:n], op=ALU.mult), reads=[stb_, psb[2 + gi]], writes=[stb_])
                    hd = hid[hi]
                    P.op("pool", lambda h, st_=st_, hd=hd, j=j, n=n, cg=cg, t0=t0: h.tensor_tensor(out=hd[:, j, 0:n], in0=st_[:, 0:n], in1=cg[:, t0:t0 + n], op=ALU.mult), reads=[stb_, hgb], writes=[hidb[hi]])
                for fc in range(8):
                    oi = 4 + fc % 2
                    for j in range(ns):
                        _mmF(P, ps[oi][:, 0:n], wd_s[wi][:, j, fc * 128:(fc + 1) * 128], hid[hi][:, j, 0:n], j == 0, j == ns - 1, [wsb[wi], hidb[hi]], [psb[oi]])
                    av = acc[:, fc, t0:t0 + n]
                    pv = ps[oi][:, 0:n]
                    if sidx == 0:
                        P.op("act", lambda h, av=av, pv=pv: h.activation(out=av, in_=pv, func=AF.Copy), reads=[psb[oi]], writes=[accb[bi]])
                    else:
                        P.op("dve", lambda h, av=av, pv=pv: h.tensor_tensor(out=av, in0=av, in1=pv, op=ALU.add), reads=[psb[oi], accb[bi]], writes=[accb[bi]])
        if sidx == len(slices) - 1:
            for bi in range(nblk):
                t0 = bi * 512
                outs.append(_ldF(P, "sp", ye[:, :, goff[g] + t0:goff[g] + t0 + 512], acc[:, :, t0:t0 + 512], [P.buf()], reads=[accb[bi]]))
    P.finish_wait("sp", outs)
    P.emit()
    return nc


def _ldF(P, q, out, in_, writes, reads=()):
    return P.dma(q, lambda h: h.dma_start(out=out, in_=in_), reads=reads, writes=writes)


def _mmF(P, out, lhsT, rhs, start, stop, reads, writes):
    return P.op("pe", lambda h: h.matmul(out, lhsT=lhsT, rhs=rhs, start=start, stop=stop), reads=reads, writes=writes)


def build_Fc(TT=2048, nexp=8):
    nc = bass.Bass("TRN2", target_bir_lowering=False)
    dr = {}

    def din(name, shape, dt=F32):
        dr[name] = nc.dram_tensor(name, list(shape), dt, kind="ExternalInput").ap()
    din("xm", [KC, 128, TT])
    din("yp", [nexp, KC, 128, TT])
    din("condT", [128, KC, 2])
    din("w_mod", [D, 6 * D])
    din("b_modT", [128, 48, 2])
    din("norm_gT", [128, 4, KC, 2])
    xo = nc.dram_tensor("xo", [KC, 128, TT], F32, kind="ExternalOutput").ap().rearrange("k p t -> p k t")
    xm = dr["xm"].rearrange("k p t -> p k t")
    P = Prog(nc)
    arena_init(P)
    ps = [P.ps("ps%d" % i, [128, 512], F32) for i in range(8)]
    psb = [P.buf("ps%d" % i) for i in range(8)]
    ones = A(P, [128, 128], BF16)
    onesb = P.buf()
    P.op("dve", lambda h: h.memset(ones[:], 1.0), writes=[onesb])
    P.eps_t = A(P, [128, 1], F32)
    P.op("dve", lambda h: h.memset(P.eps_t[:], EPS), writes=[onesb])
    mod_view = ps[7][:, 0:96].rearrange("p (j r) -> p j r", r=2)
    compute_mod(P, dr, [5], mod_view, psb[7])
    _, gg_f = mod_derived(P, 4, 5, 2, 3)
    acc = [A(P, [128, KC, 512], F32) for _ in range(2)]
    accb = [P.buf(), P.buf()]
    part = [A(P, [128, KC, 512], F32) for _ in range(3)]
    partb = [P.buf() for _ in range(3)]
    xt = [A(P, [128, KC, 512], F32) for _ in range(2)]
    xtb = [P.buf(), P.buf()]
    sq = A(P, [128, KC, 512], BF16)
    sqb = P.buf()
    rstd = A(P, [128, 512], F32)
    rstdb = P.buf()
    tmp = [A(P, [128, 512], F32) for _ in range(2)]
    tmpb = [P.buf(), P.buf()]
    outs = []
    pc = 0
    for bi in range(TT // 512):
        t0, n = bi * 512, 512
        a_t, a_b = acc[bi % 2], accb[bi % 2]
        x_t, x_b = xt[bi % 2], xtb[bi % 2]
        _ldF(P, "sp", x_t[:, :, :], xm[:, :, t0:t0 + n], [x_b])
        _ldF(P, "sp", a_t[:, :, :], dr["yp"][0].rearrange("k p t -> p k t")[:, :, t0:t0 + n], [a_b])
        for e in range(1, nexp):
            p_t, p_b = part[pc % 3], partb[pc % 3]
            pc += 1
            _ldF(P, "act" if e % 2 else "sp", p_t[:, :, :], dr["yp"][e].rearrange("k p t -> p k t")[:, :, t0:t0 + n], [p_b])
            eng = "dve" if e % 2 else "pool"
            P.op(eng, lambda h, a_t=a_t, p_t=p_t: h.tensor_tensor(out=a_t[:, :, :], in0=a_t[:, :, :], in1=p_t[:, :, :], op=ALU.add), reads=[a_b, p_b], writes=[a_b])
        rms_rstd(P, a_t, a_b, n, sq, sqb, ps[6], psb[6], rstd, rstdb, ones)
        for k in range(KC):
            tb_, tt_ = tmpb[k % 2], tmp[k % 2]
            P.op("dve", lambda h, k=k, tt_=tt_, a_t=a_t: h.tensor_tensor(out=tt_[:, :], in0=a_t[:, k, :], in1=rstd[:, :], op=ALU.mult), reads=[a_b, rstdb], writes=[tb_])
            P.op("dve", lambda h, k=k, tt_=tt_, x_t=x_t: h.scalar_tensor_tensor(out=x_t[:, k, :], in0=tt_[:, :], scalar=gg_f[:, k, 0:1], in1=x_t[:, k, :], op0=ALU.mult, op1=ALU.add),
                 reads=[tb_, P.modb, x_b], writes=[x_b])
        outs.append(_ldF(P, "sp", xo[:, :, t0:t0 + n], x_t[:, :, :], [P.buf()], reads=[x_b]))
    P.finish_wait("sp", outs)
    P.emit()
    return nc


import math, os
RET_STOP = int(os.environ.get('RET_STOP', '99'))
SKIP = os.environ.get('SKIP', '')

NTOK = 8448
NCH = 66
MAGIC = 12582912.0
TWO_PI = 2.0 * math.pi


def pos_of(dd):
    if dd == 0:
        return list(range(NCH))
    order = [1, 0] + list(range(65, 1, -1))
    pos = [0] * NCH
    for p_, c in enumerate(order):
        pos[c] = p_
    return pos


def range_reduce_sincos(P, ph, sn, cs, tmp, shape_ap, b):
    v = shape_ap
    _ts(P, "dve", v(tmp), v(ph), 1.0 / TWO_PI, MAGIC, ALU.mult, ALU.add, [b], [b])
    _ts(P, "dve", v(tmp), v(tmp), -MAGIC, None, ALU.add, None, [b], [b])
    _stt(P, v(ph), v(tmp), -TWO_PI, v(ph), ALU.mult, ALU.add, [b], [b])
    _ts(P, "dve", v(ph), v(ph), -math.pi, math.pi, ALU.max, ALU.min, [b], [b])
    _act(P, v(sn), v(ph), AF.Sin, [b], [b])
    _ts(P, "dve", v(tmp), v(ph), -1.0, None, ALU.mult, None, [b], [b])
    _tt(P, "dve", v(tmp), v(tmp), v(ph), ALU.max, [b], [b])
    _act(P, v(cs), v(tmp), AF.Sin, [b], [b], scale=-1.0, bias=P.halfpi[0:v(tmp).shape[0], 0:1])


def build_M(need_ctx_out, parts=("four", "s5", "ret", "na"), DBG=False):
    nc = bass.Bass("TRN2", target_bir_lowering=False)
    dr = {}

    def din(name, shape, dt=F32):
        dr[name] = nc.dram_tensor(name, list(shape), dt, kind="ExternalInput").ap()
    din("xT", [KC, 128, NTOK])
    din("condT", [128, KC, 2])
    din("w_mod", [D, 6 * D])
    din("b_modT", [128, 48, 2])
    din("norm_gT", [128, 4, KC, 2])
    din("w_fm", [D, 576])
    din("w_tm", [D, 256])
    din("f_CS", [64, 128], BF16); din("f_RP", [64, 128], BF16); din("f_RQ", [64, 128], BF16)
    din("f_CB", [128, 64, 128], BF16); din("f_SB", [128, 64, 128], BF16)
    din("f_C256", [128, 2, 256], BF16); din("f_S256", [128, 2, 256], BF16)
    din("r_cosF", [64, 8192]); din("r_sinF", [64, 8192]); din("r_cosT", [128, 64, 64]); din("r_sinT", [128, 64, 64])
    din("r_jcol", [128, 2]); din("r_dist", [128, 128]); din("r_mask", [2, 128, 128]); din("r_irow", [2, 64, 128])
    din("s_jrow", [128, 129]); din("s_jcol", [128, 1]); din("s_LT", [2, 128, 128], BF16); din("s_mrow", [64, 4]); din("s_msm", [128, 2, 4])
    din("ident_bf", [128, 128], BF16); din("ident_f", [128, 128])
    din("n_mask", [5, 128, 832]); din("n_toep", [15, 64, 64])
    din("s_sm", [128, 2, 2, 3]); din("s_row", [128, 2, 3, 256]); din("s_hs", [64, 2, 3, 64]); din("s_B", [64, 2, 2, 64])
    din("s_C", [128, 2, 2, 2, 16]); din("s_d", [64, 1])
    din("r_dec", [128, 2]); din("r_gn", [64, 1])
    out = nc.dram_tensor("brT_out", [4, 64, NTOK], BF16, kind="ExternalOutput").ap()
    hT = nc.dram_tensor("hT_scr", [KC, 128, NTOK], BF16, kind="Internal").ap().rearrange("k p t -> p k t")
    xT = dr["xT"].rearrange("k p t -> p k t")

    P = Prog(nc)
    arena_init(P)
    ps = [P.ps("ps%d" % i, [128, 512], F32) for i in range(8)]
    psb = [P.buf("ps%d" % i) for i in range(8)]
    cb = P.buf("consts")
    ones = A(P, [128, 128], BF16)
    P.op("dve", lambda h: h.memset(ones[:], 1.0), writes=[cb])
    P.eps_t = A(P, [128, 1], F32)
    P.op("dve", lambda h: h.memset(P.eps_t[:], EPS), writes=[cb])
    P.halfpi = A(P, [128, 1], F32)
    P.op("dve", lambda h: h.memset(P.halfpi[:], math.pi / 2), writes=[cb])
    P.one_t = A(P, [128, 1], F32)
    P.op("dve", lambda h: h.memset(P.one_t[:], 1.0), writes=[cb])
    ident = A(P, [128, 128], BF16)
    _ld(P, "sp", ident[:], dr["ident_bf"][:, :], [cb])
    mod_view = ps[7][:, 0:96].rearrange("p (j r) -> p j r", r=2)
    compute_mod(P, dr, [0, 1], mod_view, psb[7])
    gm_a, _ = mod_derived(P, 1, None, 0, 0)
    sh_a = P.modT[:, 0:8, :]
    wfm = A(P, [128, KC, 576], BF16)
    wtm = A(P, [128, KC, 256], BF16)
    wb = P.buf("w")
    _ld(P, "pool", wfm[:], dr["w_fm"].rearrange("(k p) c -> p k c", p=128), [wb])
    _ld(P, "pool", wtm[:], dr["w_tm"].rearrange("(k p) c -> p k c", p=128), [wb])
    blocks = [(0, 256, 1)] + [(256 + 512 * i, 512, 0) for i in range(16)]
    outs = []
    hTb = P.buf("hT")
    mark0 = P.a_cur

    def fm_proj(hb, hbb, n, g, pst, pstb):
        for k in range(KC):
            _mm(P, pst[0:64, 0:n], wfm[:, k, g * 64:(g + 1) * 64], hb[:, k, 0:n], k == 0, k == KC - 1, [wb, hbb], [pstb])

    sT = A(P, [64, NTOK], BF16)
    markS = P.a_cur
    fT = A(P, [64, NTOK], BF16)
    fTb, sTb = P.buf("fT"), P.buf("sT")
    markA = P.a_cur
    xb = [A(P, [128, KC, 512], F32) for _ in range(2)]
    xbb = [P.buf(), P.buf()]
    sq = A(P, [128, KC, 512], BF16)
    sqb = P.buf()
    rstd = A(P, [128, 512], F32)
    rstdb = P.buf()
    tmp = [A(P, [128, 512], F32) for _ in range(8)]
    tmpb = [P.buf() for _ in range(8)]
    hbs = [A(P, [128, KC, 512], BF16) for _ in range(2)]
    hbsb = [P.buf(), P.buf()]
    def _ldx(bi_):
        t0_, n_, _r = blocks[bi_]
        _ld(P, "sp", xb[bi_ % 2][:, 0:4, 0:n_], xT[:, 0:4, t0_:t0_ + n_], [xbb[bi_ % 2]])
        _ld(P, "sp", xb[bi_ % 2][:, 4:8, 0:n_], xT[:, 4:8, t0_:t0_ + n_], [xbb[bi_ % 2]])
    _ldx(0)
    for bi, (t0, n, r) in enumerate(blocks):
        x_t, x_b = xb[bi % 2], xbb[bi % 2]
        hb, hbb = hbs[bi % 2], hbsb[bi % 2]
        if bi + 1 < len(blocks):
            _ldx(bi + 1)
        rms_rstd(P, x_t, x_b, n, sq, sqb, ps[6], psb[6], rstd, rstdb, ones)
        norm_mod(P, x_t, x_b, n, rstd, rstdb, gm_a, sh_a, r, hb, hbb, tmp, tmpb)
        _ld(P, "sp", hT[:, :, t0:t0 + n], hb[:, :, 0:n], [hTb], reads=[hbb])
        for gi_, (g, dst, dstb) in enumerate(((0, fT, fTb), (1, sT, sTb))):
            pi_ = (2 * bi + gi_) % 4
            fm_proj(hb, hbb, n, g, ps[pi_], psb[pi_])
            _cp(P, "act" if gi_ == 0 else "dve", dst[:, t0:t0 + n], ps[pi_][0:64, 0:n], [psb[pi_]], [dstb])
    barrier(P)
    P.a_cur = markA

    if "four" in parts:
        markF = P.a_cur
        CS = A(P, [64, 128], BF16); RP = A(P, [64, 128], BF16); RQ = A(P, [64, 128], BF16)
        CB = A(P, [128, 64, 128], BF16); SB = A(P, [128, 64, 128], BF16)
        ftb = P.buf("ftab")
        for t_, nm in ((CS, "f_CS"), (RP, "f_RP"), (RQ, "f_RQ")):
            _ld(P, "sp", t_[:], dr[nm][:, :], [ftb])
        _ld(P, "sp", CB[:], dr["f_CB"][:, :, :], [ftb])
        _ld(P, "sp", SB[:], dr["f_SB"][:, :, :], [ftb])
        PQ = A(P, [64, 128, 128], BF16); PQb = P.buf("PQ")
        UVT = A(P, [128, 64, 128], BF16); UVTb = P.buf("UVT")
        aT = A(P, [64, NTOK], BF16); aTb = P.buf("aT")
        for g4 in range(32):
            pi_ = g4 % 2
            for jj in range(4):
                m2 = g4 * 4 + jj
                _mm(P, ps[pi_][0:64, jj * 128:(jj + 1) * 128], fT[:, 256 + m2:NTOK:128], CS[:, :], True, True, [fTb, ftb], [psb[pi_]])
            _cp(P, "act" if g4 % 2 else "dve", PQ[:, g4 * 4:(g4 + 1) * 4, :], ps[pi_][0:64, 0:512].rearrange("p (a b) -> p a b", b=128), [psb[pi_]], [PQb])
        for g4 in range(16):
            pi_ = 2 + g4 % 2
            for jj in range(4):
                d = g4 * 4 + jj
                _mm(P, ps[pi_][:, jj * 128:(jj + 1) * 128], PQ[:, :, d], RP[:, :], True, False, [PQb, ftb], [psb[pi_]])
                _mm(P, ps[pi_][:, jj * 128:(jj + 1) * 128], PQ[:, :, 64 + d], RQ[:, :], False, True, [PQb, ftb], [psb[pi_]])
            _cp(P, "act" if g4 % 2 else "dve", UVT[:, g4 * 4:(g4 + 1) * 4, :], ps[pi_][:, 0:512].rearrange("p (a b) -> p a b", b=128), [psb[pi_]], [UVTb])
        aT3 = aT[:, 256:NTOK].rearrange("p (a b) -> p a b", b=64)
        for g4 in range(16):
            pi_ = g4 % 2
            for jj in range(4):
                n1 = g4 * 4 + jj
                _mm(P, ps[pi_][0:64, jj * 128:(jj + 1) * 128], UVT[:, :, n1], CB[:, n1, :], True, False, [UVTb, ftb], [psb[pi_]])
                _mm(P, ps[pi_][0:64, jj * 128:(jj + 1) * 128], UVT[:, :, 64 + n1], SB[:, n1, :], False, True, [UVTb, ftb], [psb[pi_]])
            _cp(P, "act" if g4 % 2 else "dve", aT3[:, :, g4 * 4:(g4 + 1) * 4], ps[pi_][0:64, 0:512].rearrange("p (j n) -> p n j", n=128), [psb[pi_]], [aTb])
        if need_ctx_out:
            C256 = A(P, [128, 2, 256], BF16); S256 = A(P, [128, 2, 256], BF16)
            _ld(P, "sp", C256[:], dr["f_C256"][:, :, :], [ftb])
            _ld(P, "sp", S256[:], dr["f_S256"][:, :, :], [ftb])
            PQc = A(P, [128, 2, 128], BF16); PQcb = P.buf()
            for tt in range(2):
                _mm(P, ps[2 + tt][:, 0:128], fT[:, tt * 128:(tt + 1) * 128], CS[:, :], True, True, [fTb, ftb], [psb[2 + tt]])
                _cp(P, "dve", PQc[:, tt, :], ps[2 + tt][:, 0:128], [psb[2 + tt]], [PQcb])
            seq = [(tt, 0) for tt in range(2)] + [(tt, 1) for tt in range(2)]
            for i_, (tt, pq) in enumerate(seq):
                _mm(P, ps[4][0:64, 0:256], PQc[:, tt, pq * 64:(pq + 1) * 64], (C256 if pq == 0 else S256)[:, tt, :], i_ == 0, i_ == 3, [PQcb, ftb], [psb[4]])
            _cp(P, "dve", aT[:, 0:256], ps[4][0:64, 0:256], [psb[4]], [aTb])
        else:
            P.op("dve", lambda h: h.memset(aT[:, 0:256], 0.0), writes=[aTb])
        outs.append(_ld(P, "sp", out[0, :, :], aT[:, :], [P.buf()], reads=[aTb]))
        barrier(P)
    P.a_cur = markS

    if "s5" in parts:
        s5_part(P, dr, ps, psb, sT, sTb, out, outs, need_ctx_out, cb)
    barrier(P)
    P.a_cur = mark0

    if "ret" in parts:
        ret_part(P, dr, ps, psb, hT, hTb, wfm, wtm, wb, blocks, out, outs, need_ctx_out, cb, ones)
        barrier(P)
        P.a_cur = mark0
    if "na" in parts:
        na_part(P, dr, ps, psb, hT, hTb, wfm, wtm, wb, blocks, out, outs, need_ctx_out, cb, ident)
        barrier(P)
    P.finish_wait("sp", outs)
    P.emit()
    return nc


def load_h(P, hT, hTb, hbs, hbsb, bi, t0, n):
    hb, hbb = hbs[bi % 2], hbsb[bi % 2]
    _ld(P, "sp", hb[:, :, 0:n], hT[:, :, t0:t0 + n], [hbb], reads=[hTb])
    return hb, hbb


def na_part(P, dr, ps, psb, hT, hTb, wfm, wtm, wb, blocks, out, outs, need_ctx_out, cb, ident):
    Cn = consts()
    drs, types = Cn["n_drs"], Cn["n_types"]
    nqT = A(P, [64, NTOK], BF16); nkT = A(P, [64, NTOK], BF16); nvT = A(P, [128, NCH, 64], BF16)
    nqb, nkb, nvb = P.buf("nq"), P.buf("nk"), P.buf("nv")
    nT = A(P, [64, NTOK], BF16); nTb = P.buf("nT")
    bias = A(P, [128, 5, 832], F32); biasb = P.buf("bias")
    mask = A(P, [128, 5, 832], F32)
    P.op("pool", lambda h: h.memset(bias[:], 0.0), writes=[biasb])
    maskb = P.buf()
    _ld(P, "sp", mask[:], dr["n_mask"].rearrange("t p c -> p t c"), [maskb])
    for ti in range(5):
        for qr in range(2):
            for i in range(9):
                _ld(P, "sp" if (i % 2) else "act", bias[qr * 64:(qr + 1) * 64, ti, i * 64:(i + 1) * 64], dr["n_toep"][int(drs[ti, qr, i])], [biasb])
    _tt(P, "dve", bias[:], bias[:], mask[:], ALU.add, [biasb, maskb], [biasb])
    mark = P.a_cur
    hbs = [A(P, [128, KC, 512], BF16) for _ in range(2)]
    hbsb = [P.buf(), P.buf()]
    cnt = 0
    for bi, (t0, n, r) in enumerate(blocks):
        hb, hbb = load_h(P, hT, hTb, hbs, hbsb, bi, t0, n)
        for g, dst, dstb in ((7, nqT, nqb), (8, nkT, nkb)):
            pi_ = cnt % 4
            cnt += 1
            for k in range(KC):
                _mm(P, ps[pi_][0:64, 0:n], wfm[:, k, g * 64:(g + 1) * 64], hb[:, k, 0:n], k == 0, k == KC - 1, [wb, hbb], [psb[pi_]])
            _cp(P, "act" if g == 7 else "dve", dst[:, t0:t0 + n], ps[pi_][0:64, 0:n], [psb[pi_]], [dstb])
        for tt in range(n // 128):
            pi_ = 4 + (tt % 2)
            for k in range(KC):
                _mm(P, ps[pi_][:, 0:64], hb[:, k, tt * 128:(tt + 1) * 128], wtm[:, k, 192:256], k == 0, k == KC - 1, [wb, hbb], [psb[pi_]])
            _cp(P, "act", nvT[:, t0 // 128 + tt, :], ps[pi_][:, 0:64], [psb[pi_]], [nvb])
    barrier(P)
    P.a_cur = mark
    NB4 = 4
    s_t = [A(P, [128, 832], F32) for _ in range(NB4)]; s_b = [P.buf() for _ in range(NB4)]
    p_t = [A(P, [128, 832], BF16) for _ in range(NB4)]; p_b = [P.buf() for _ in range(NB4)]
    pT = [A(P, [128, 7, 128], BF16) for _ in range(NB4)]; pTb = [P.buf() for _ in range(NB4)]
    st_ = [A(P, [128, 4], F32) for _ in range(NB4)]; stb = [P.buf() for _ in range(NB4)]
    SC = 0.125

    def softmax_pv(qi, ncols, pv_list, o_ps, o_psb, o_cols, sbi, s4=None):
        if s4 is None:
            s4 = sbi
        s, sb_ = s_t[s4], s_b[s4]
        sm, smb = st_[s4], stb[s4]
        P.op("dve", lambda h: h.reduce_max(out=sm[:, 0:1], in_=s[:, 0:ncols], axis=AX.X), reads=[sb_], writes=[smb])
        _ts(P, "dve", sm[:, 1:2], sm[:, 0:1], -1.0, None, ALU.mult, None, [smb], [smb])
        _act(P, s[:, 0:ncols], s[:, 0:ncols], AF.Exp, [sb_, smb], [sb_], bias=sm[:, 1:2], scale=1.0)
        P.op("dve", lambda h: h.reduce_sum(out=sm[:, 2:3], in_=s[:, 0:ncols], axis=AX.X), reads=[sb_], writes=[smb])
        P.op("dve", lambda h: h.reciprocal(out=sm[:, 3:4], in_=sm[:, 2:3]), reads=[smb], writes=[smb])
        p, pb = p_t[s4], p_b[s4]
        _ts(P, "dve", p[:, 0:ncols], s[:, 0:ncols], sm[:, 3:4], None, ALU.mult, None, [sb_, smb], [pb])
        tp = ps[4 + sbi][:, :].bitcast(BF16)
        for ci, (c0, nk, tile) in enumerate(pv_list):
            _tr(P, tp[0:nk, ci * 128:(ci + 1) * 128], p[:, c0:c0 + nk], ident[:, :], [pb, cb], [psb[4 + sbi]])
        nchk = len(pv_list)
        pt_, ptb = pT[s4], pTb[s4]
        _cp(P, "act", pt_[:, 0:nchk, :], tp[:, 0:nchk * 128].rearrange("p (a b) -> p a b", b=128), [psb[4 + sbi]], [ptb])
        for ci, (c0, nk, tile) in enumerate(pv_list):
            _mm(P, o_ps[0:64, o_cols:o_cols + 128], nvT[0:nk, tile, :], pt_[0:nk, ci, :], ci == 0, ci == nchk - 1, [nvb, ptb], [o_psb])

    for rp in range(64):
        ti = {0: 0, 1: 1, 62: 3, 63: 4}.get(rp, 2)
        r0 = 2 * rp
        if ti == 2:
            R0, nr = r0 - 4, 9
        else:
            R0, nr = types[ti][1], 8
        tq = 256 + 128 * rp
        kb_ = 256 + 64 * R0
        sbi = rp % 2
        s1, s2 = ps[sbi], ps[2 + sbi]
        _mm(P, s1[:, 0:512], nqT[:, tq:tq + 128], nkT[:, kb_:kb_ + 512], True, True, [nqb, nkb], [psb[sbi]])
        kb2 = kb_ + 512 if nr == 9 else kb_
        _mm(P, s2[:, 0:64], nqT[:, tq:tq + 128], nkT[:, kb2:kb2 + 64], True, True, [nqb, nkb], [psb[2 + sbi]])
        _mm(P, s2[:, 64:320], nqT[:, tq:tq + 128], nkT[:, 0:256], True, True, [nqb, nkb], [psb[2 + sbi]])
        s4 = rp % NB4
        s = s_t[s4]
        _stt(P, s[:, 0:512], s1[:, 0:512], SC, bias[:, ti, 0:512], ALU.mult, ALU.add, [psb[sbi], biasb], [s_b[s4]])
        _stt(P, s[:, 512:832], s2[:, 0:320], SC, bias[:, ti, 512:832], ALU.mult, ALU.add, [psb[2 + sbi], biasb], [s_b[s4]])
        t_base = 2 + R0 // 2
        pv = [(128 * j, 128, t_base + j) for j in range(4)]
        if nr == 9:
            pv.append((512, 64, t_base + 4))
        pv += [(576, 128, 0), (704, 128, 1)]
        jj = rp % 4
        softmax_pv(rp, 832, pv, ps[6], psb[6], jj * 128, sbi, s4)
        if jj == 3:
            _cp(P, "dve", nT[:, 256 + 512 * (rp // 4):256 + 512 * (rp // 4 + 1)], ps[6][0:64, 0:512], [psb[6]], [nTb])
    if need_ctx_out:
        for qt in range(2):
            sbi = qt
            _mm(P, ps[sbi][:, 0:256], nqT[:, qt * 128:(qt + 1) * 128], nkT[:, 0:256], True, True, [nqb, nkb], [psb[sbi]])
            _ts(P, "dve", s_t[sbi][:, 0:256], ps[sbi][:, 0:256], SC, None, ALU.mult, None, [psb[sbi]], [s_b[sbi]])
            softmax_pv(qt, 256, [(0, 128, 0), (128, 128, 1)], ps[7], psb[7], qt * 128, sbi)
        _cp(P, "dve", nT[:, 0:256], ps[7][0:64, 0:256], [psb[7]], [nTb])
    else:
        P.op("dve", lambda h: h.memset(nT[:, 0:256], 0.0), writes=[nTb])
    outs.append(_ld(P, "sp", out[3, :, :], nT[:, :], [P.buf()], reads=[nTb]))


def ret_part(P, dr, ps, psb, hT, hTb, wfm, wtm, wb, blocks, out, outs, need_ctx_out, cb, ones):
    KS = 0.125
    qT = A(P, [64, NTOK], BF16); kT = A(P, [64, NTOK], BF16); gT = A(P, [64, NTOK], BF16)
    qb_, kb_, gb_ = P.buf("q"), P.buf("k"), P.buf("g")
    rvT = A(P, [128, NCH, 64], BF16); rvb = P.buf("rv")
    Sbf = [A(P, [64, NCH, 64], BF16) for _ in range(2)]
    rc = P.buf("retc")
    dec = A(P, [128, 2], F32); lg = A(P, [128, 2], F32); jcol = A(P, [128, 2], F32); kdec = A(P, [128, 2], F32); g128 = A(P, [128, 2], F32)
    dist = A(P, [128, 128], F32); msk = A(P, [128, 2, 128], F32); DT = A(P, [128, 128], F32); DT2 = A(P, [128, 128], F32)
    irow = A(P, [64, 2, 128], F32); qdec = A(P, [64, 2, 128], F32)
    gn = A(P, [64, 1], F32); o64 = A(P, [64, 64], F32)
    _ld(P, "sp", dec[:], dr["r_dec"][:, :], [rc])
    _ld(P, "sp", jcol[:], dr["r_jcol"][:, :], [rc])
    _ld(P, "sp", dist[:], dr["r_dist"][:, :], [rc])
    _ld(P, "sp", msk[:], dr["r_mask"].rearrange("d j i -> j d i"), [rc])
    _ld(P, "sp", irow[:], dr["r_irow"].rearrange("d p i -> p d i"), [rc])
    _ld(P, "sp", gn[:], dr["r_gn"][:, :], [rc])
    P.op("dve", lambda h: h.memset(o64[:], 1.0 / 64), writes=[rc])
    _act(P, lg[:], dec[:], AF.Exp, [rc], [rc], scale=-1.0)
    _act(P, lg[:], lg[:], AF.Ln, [rc], [rc], bias=P.one_t[:, 0:1], scale=1.0)
    _ts(P, "dve", lg[:], lg[:], -1.0, None, ALU.mult, None, [rc], [rc])
    for dd in range(2):
        _act(P, kdec[:, dd:dd + 1], jcol[:, dd:dd + 1], AF.Exp, [rc], [rc], scale=lg[:, dd:dd + 1])
        _act(P, g128[:, dd:dd + 1], lg[:, dd:dd + 1], AF.Exp, [rc], [rc], scale=128.0)
        _act(P, qdec[:, dd, :], irow[:, dd, :], AF.Exp, [rc], [rc], scale=lg[0:64, dd:dd + 1])
    _ts(P, "dve", kdec[:], kdec[:], KS, None, ALU.mult, None, [rc], [rc])
    _act(P, DT[:], dist[:], AF.Exp, [rc], [rc], scale=lg[:, 0:1])
    _tt(P, "dve", DT[:], DT[:], msk[:, 0, :], ALU.mult, [rc], [rc])
    _act(P, DT2[:], dist[:], AF.Exp, [rc], [rc], scale=lg[:, 1:2])
    _tt(P, "dve", DT2[:], DT2[:], msk[:, 1, :], ALU.mult, [rc], [rc])
    _tt(P, "dve", DT[:], DT[:], DT2[:], ALU.add, [rc], [rc])
    if RET_STOP <= 0:
        return
    mark_k = P.a_cur
    kd = [A(P, [128, NCH, 64], BF16) for _ in range(2)]
    kdb = [P.buf(), P.buf()]
    mark = P.a_cur
    hbs = [A(P, [128, KC, 512], BF16) for _ in range(2)]
    hbsb = [P.buf(), P.buf()]
    cF = [A(P, [64, 512], F32) for _ in range(2)]; sF = [A(P, [64, 512], F32) for _ in range(2)]
    cTt = [A(P, [128, 4, 64], F32) for _ in range(2)]; sTt = [A(P, [128, 4, 64], F32) for _ in range(2)]
    tabb = [P.buf(), P.buf()]
    t1 = [A(P, [128, 512], F32) for _ in range(2)]; t1b = [P.buf(), P.buf()]
    t2 = [A(P, [128, 512], F32) for _ in range(2)]; t2b = [P.buf(), P.buf()]
    cnt = 0
    for bi, (t0, n, r) in enumerate(blocks):
        hb, hbb = load_h(P, hT, hTb, hbs, hbsb, bi, t0, n)
        lat = (r == 0)
        tb_ = tabb[bi % 2]
        if lat:
            m0 = t0 - 256
            _ld(P, "sp", cF[bi % 2][:, :], dr["r_cosF"][:, m0:m0 + 512], [tb_])
            _ld(P, "sp", sF[bi % 2][:, :], dr["r_sinF"][:, m0:m0 + 512], [tb_])
            _ld(P, "sp", cTt[bi % 2][:, :, :], dr["r_cosT"][:, m0 // 128:m0 // 128 + 4, :], [tb_])
            _ld(P, "sp", sTt[bi % 2][:, :, :], dr["r_sinT"][:, m0 // 128:m0 // 128 + 4, :], [tb_])

        def proj(g, pi_):
            for k in range(KC):
                _mm(P, ps[pi_][0:64, 0:n], wfm[:, k, g * 64:(g + 1) * 64], hb[:, k, 0:n], k == 0, k == KC - 1, [wb, hbb], [psb[pi_]])
        for (g, gsw, dst, dstb, scl) in ((2, 4, qT, qb_, 1.0), (3, 5, kT, kb_, KS)):
            if 'qk' in SKIP:
                continue
            proj(g, 0)
            if lat:
                proj(gsw, 1)
                i2 = cnt % 2
                cnt += 1
                _stt(P, t1[i2][0:64, 0:n], ps[0][0:64, 0:n], scl, cF[bi % 2][:, 0:n], ALU.mult, ALU.mult, [psb[0], tb_], [t1b[i2]])
                _stt(P, t2[i2][0:64, 0:n], ps[1][0:64, 0:n], scl, sF[bi % 2][:, 0:n], ALU.mult, ALU.mult, [psb[1], tb_], [t2b[i2]])
                _tt(P, "pool", dst[:, t0:t0 + n], t1[i2][0:64, 0:n], t2[i2][0:64, 0:n], ALU.add, [t1b[i2], t2b[i2]], [dstb])
            else:
                _act(P, dst[:, t0:t0 + n], ps[0][0:64, 0:n], AF.Copy, [psb[0]], [dstb], scale=scl)
        proj(6, 2)
        _cp(P, "act", gT[:, t0:t0 + n], ps[2][0:64, 0:n], [psb[2]], [gb_])
        for tt in range(n // 128):
            if 'tm' in SKIP:
                continue
            pi_ = 4 + (tt % 2)
            tile = t0 // 128 + tt
            for k in range(KC):
                _mm(P, ps[pi_][:, 0:192], hb[:, k, tt * 128:(tt + 1) * 128], wtm[:, k, 0:192], k == 0, k == KC - 1, [wb, hbb], [psb[pi_]])
            _cp(P, "act", rvT[:, tile, :], ps[pi_][:, 128:192], [psb[pi_]], [rvb])
            if 'kd' in SKIP:
                continue
            if ('kl' in SKIP and lat) or ('kc' in SKIP and not lat):
                continue
            if lat:
                i2 = cnt % 2
                cnt += 1
                _tt(P, "dve", t1[i2][:, 0:64], ps[pi_][:, 0:64], cTt[bi % 2][:, tt, :], ALU.mult, [psb[pi_], tb_], [t1b[i2]])
                _tt(P, "dve", t2[i2][:, 0:64], ps[pi_][:, 64:128], sTt[bi % 2][:, tt, :], ALU.mult, [psb[pi_], tb_], [t2b[i2]])
                if 'k1' in SKIP:
                    continue
                _tt(P, "dve", t1[i2][:, 0:64], t1[i2][:, 0:64], t2[i2][:, 0:64], ALU.add, [t1b[i2], t2b[i2]], [t1b[i2]])
                if 'k2' in SKIP:
                    continue
                for dd in range(2):
                    _act(P, kd[dd][:, tile, :], t1[i2][:, 0:64], AF.Identity, [t1b[i2], rc], [kdb[dd]], scale=kdec[:, dd:dd + 1])
            else:
                for dd in range(2):
                    _act(P, kd[dd][:, tile, :], ps[pi_][:, 0:64], AF.Identity, [psb[pi_], rc], [kdb[dd]], scale=kdec[:, dd:dd + 1])
    barrier(P)
    P.a_cur = mark
    if RET_STOP <= 1:
        return
    S32_ = A(P, [64, NCH, 64], F32)
    S32 = [S32_, S32_]
    Sb_ = P.buf()
    Sb = [Sb_, Sb_]
    for dd in range(2):
        pos = pos_of(dd)
        order = sorted(range(NCH), key=lambda c: pos[c])
        c0 = order[0]
        P.op("dve", lambda h, dd=dd, c0=c0: h.memset(S32[dd][:, c0, :], 0.0), writes=[Sb[dd]])
        for idx in range(NCH - 1):
            c, nxt = order[idx], order[idx + 1]
            pi_ = (idx // 8) % 2
            sl = idx % 8
            _mm(P, ps[pi_][0:64, sl * 64:(sl + 1) * 64], kd[dd][:, c, :], rvT[:, c, :], True, True, [kdb[dd], rvb], [psb[pi_]])
            _stt(P, S32[dd][:, nxt, :], S32[dd][:, c, :], g128[0:64, dd:dd + 1], ps[pi_][0:64, sl * 64:(sl + 1) * 64], ALU.mult, ALU.add, [Sb[dd], psb[pi_], rc], [Sb[dd]])
        _cp(P, "act", Sbf[dd][:], S32[dd][:], [Sb[dd]], [Sb[dd]])
    barrier(P)
    P.a_cur = mark_k
    if RET_STOP <= 2:
        return
    oT = A(P, [64, NTOK], F32); oTb = P.buf("oT")
    sc = [A(P, [128, 128], BF16) for _ in range(2)]; scb = [P.buf(), P.buf()]
    qd = [[A(P, [64, 128], BF16) for _ in range(2)] for _ in range(2)]
    qdb = [[P.buf(), P.buf()] for _ in range(2)]
    c_start = 0 if need_ctx_out else 2
    if not need_ctx_out:
        P.op("pool", lambda h: h.memset(oT[:, 0:256], 0.0), writes=[oTb])
    for c in range(c_start, NCH):
        tau = 128 * c
        i2 = c % 2
        _mm(P, ps[i2][:, 0:128], kT[:, tau:tau + 128], qT[:, tau:tau + 128], True, True, [kb_, qb_], [psb[i2]])
        _tt(P, "dve", sc[i2][:, :], ps[i2][:, 0:128], DT[:, :], ALU.mult, [psb[i2], rc], [scb[i2]])
        for dd in range(2):
            _tt(P, "pool", qd[dd][i2][:, :], qT[:, tau:tau + 128], qdec[:, dd, :], ALU.mult, [qb_, rc], [qdb[dd][i2]])
        jj = c % 4
        po = ps[4 + (c // 4) % 2]
        pob = psb[4 + (c // 4) % 2]
        _mm(P, po[0:64, jj * 128:(jj + 1) * 128], rvT[:, c, :], sc[i2][:, :], True, False, [rvb, scb[i2]], [pob])
        _mm(P, po[0:64, jj * 128:(jj + 1) * 128], Sbf[0][:, c, :], qd[0][i2][:, :], False, False, [Sb[0], qdb[0][i2]], [pob])
        _mm(P, po[0:64, jj * 128:(jj + 1) * 128], Sbf[1][:, c, :], qd[1][i2][:, :], False, True, [Sb[1], qdb[1][i2]], [pob])
        if jj == 3 or c == NCH - 1:
            b0 = (c // 4) * 512
            wid = (jj + 1) * 128
            lo = 0
            if (not need_ctx_out) and c // 4 == 0:
                lo = 256
            _cp(P, "act", oT[:, b0 + lo:b0 + wid], po[0:64, lo:wid], [pob], [oTb])
    if RET_STOP <= 3:
        return
    rT = A(P, [64, NTOK], BF16); rTb = P.buf("rT")
    o64b = A(P, [64, 64], BF16)
    P.op("dve", lambda h: h.memset(o64b[:], 1.0 / 64), writes=[rc])
    obf = [A(P, [64, 512], BF16) for _ in range(2)]; obfb = [P.buf(), P.buf()]
    cen = [A(P, [64, 512], F32) for _ in range(2)]; cenb = [P.buf(), P.buf()]
    sq_ = [A(P, [64, 512], BF16) for _ in range(2)]; sqb_ = [P.buf(), P.buf()]
    rs_ = [A(P, [64, 512], F32) for _ in range(2)]; rsb_ = [P.buf(), P.buf()]
    sg_ = [A(P, [64, 512], F32) for _ in range(2)]; sgb_ = [P.buf(), P.buf()]
    nblk = (NTOK + 511) // 512
    for bi in range(nblk):
        t0 = bi * 512
        n = min(512, NTOK - t0)
        i2 = bi % 2
        _cp(P, "act", obf[i2][:, 0:n], oT[:, t0:t0 + n], [oTb], [obfb[i2]])
        _mm(P, ps[2 + i2][0:64, 0:n], o64b[:, :], obf[i2][:, 0:n], True, True, [obfb[i2], rc], [psb[2 + i2]])
        _tt(P, "dve", cen[i2][:, 0:n], oT[:, t0:t0 + n], ps[2 + i2][0:64, 0:n], ALU.subtract, [oTb, psb[2 + i2]], [cenb[i2]])
        _act(P, sq_[i2][:, 0:n], cen[i2][:, 0:n], AF.Square, [cenb[i2]], [sqb_[i2]])
        _mm(P, ps[6 + i2][0:64, 0:n], o64b[:, :], sq_[i2][:, 0:n], True, True, [sqb_[i2], rc], [psb[6 + i2]])
        _act(P, rs_[i2][:, 0:n], ps[6 + i2][0:64, 0:n], AF.Ln, [psb[6 + i2]], [rsb_[i2]], bias=P.eps_t[0:64, 0:1], scale=1.0)
        _act(P, rs_[i2][:, 0:n], rs_[i2][:, 0:n], AF.Exp, [rsb_[i2]], [rsb_[i2]], scale=-0.5)
        _tt(P, "dve", cen[i2][:, 0:n], cen[i2][:, 0:n], rs_[i2][:, 0:n], ALU.mult, [cenb[i2], rsb_[i2]], [cenb[i2]])
        _act(P, sg_[i2][:, 0:n], gT[:, t0:t0 + n], AF.Silu, [gb_], [sgb_[i2]])
        _stt(P, rT[:, t0:t0 + n], cen[i2][:, 0:n], gn[:, 0:1], sg_[i2][:, 0:n], ALU.mult, ALU.mult, [cenb[i2], sgb_[i2], rc], [rTb])
    outs.append(_ld(P, "sp", out[2, :, :], rT[:, :], [P.buf()], reads=[rTb]))


def s5_part(P, dr, ps, psb, sT, sTb, out, outs, need_ctx_out, cb):
    pb = P.buf("s5param")

    def cplx_prep(are, aim, ldt, mk):
        T = {k: mk() for k in ("dt", "ar", "ai", "mag", "ph", "tmp", "sn", "cs", "abr", "abi", "nr", "den", "cr", "ci", "u")}
        ident_v = lambda t: t
        _act(P, T["dt"], ldt, AF.Exp, [pb], [pb])
        _tt(P, "dve", T["ar"], are, T["dt"], ALU.mult, [pb], [pb])
        _tt(P, "dve", T["ai"], aim, T["dt"], ALU.mult, [pb], [pb])
        _act(P, T["mag"], T["ar"], AF.Exp, [pb], [pb])
        _cp(P, "dve", T["ph"], T["ai"], [pb], [pb])
        range_reduce_sincos(P, T["ph"], T["sn"], T["cs"], T["tmp"], ident_v, pb)
        _tt(P, "dve", T["abr"], T["mag"], T["cs"], ALU.mult, [pb], [pb])
        _tt(P, "dve", T["abi"], T["mag"], T["sn"], ALU.mult, [pb], [pb])
        _ts(P, "dve", T["nr"], T["abr"], -1.0, None, ALU.add, None, [pb], [pb])
        _tt(P, "dve", T["den"], are, are, ALU.mult, [pb], [pb])
        _tt(P, "dve", T["u"], aim, aim, ALU.mult, [pb], [pb])
        _tt(P, "dve", T["den"], T["den"], T["u"], ALU.add, [pb], [pb])
        P.op("dve", lambda h: h.reciprocal(out=T["den"], in_=T["den"]), reads=[pb], writes=[pb])
        _tt(P, "dve", T["cr"], T["nr"], are, ALU.mult, [pb], [pb])
        _tt(P, "dve", T["u"], T["abi"], aim, ALU.mult, [pb], [pb])
        _tt(P, "dve", T["cr"], T["cr"], T["u"], ALU.add, [pb], [pb])
        _tt(P, "dve", T["cr"], T["cr"], T["den"], ALU.mult, [pb], [pb])
        _tt(P, "dve", T["ci"], T["abi"], are, ALU.mult, [pb], [pb])
        _tt(P, "dve", T["u"], T["nr"], aim, ALU.mult, [pb], [pb])
        _tt(P, "dve", T["ci"], T["ci"], T["u"], ALU.subtract, [pb], [pb])
        _tt(P, "dve", T["ci"], T["ci"], T["den"], ALU.mult, [pb], [pb])
        return T

    p_sm = A(P, [128, 2, 2, 3], F32); p_row = A(P, [128, 2, 3, 256], F32); p_hs = A(P, [64, 2, 3, 64], F32)
    Bhs = A(P, [64, 2, 2, 64], F32); Csm = A(P, [128, 2, 2, 2, 16], F32); dvec = A(P, [64, 1], F32)
    jrow = A(P, [128, 129], F32); jcol = A(P, [128, 1], F32); njcol = A(P, [128, 1], F32)
    LT = A(P, [128, 2, 128], BF16); mrow = A(P, [64, 4], F32); msm = A(P, [128, 2, 4], F32)
    for t_, src in ((p_sm[:], dr["s_sm"][:, :, :, :]), (p_row[:], dr["s_row"][:, :, :, :]), (p_hs[:], dr["s_hs"][:, :, :, :]), (Bhs[:], dr["s_B"][:, :, :, :]),
                    (Csm[:], dr["s_C"][:, :, :, :, :]), (dvec[:], dr["s_d"][:, :]), (jrow[:], dr["s_jrow"][:, :]), (jcol[:], dr["s_jcol"][:, :]),
                    (LT[:], dr["s_LT"].rearrange("d j i -> j d i")), (mrow[:], dr["s_mrow"][:, :]), (msm[:], dr["s_msm"][:, :, :])):
        _ld(P, "sp", t_, src, [pb])
    _ts(P, "dve", njcol[:], jcol[:], -1.0, None, ALU.mult, None, [pb], [pb])
    ones_col = A(P, [128, 1], BF16)
    P.op("dve", lambda h: h.memset(ones_col[:], 1.0), writes=[pb])

    BD = [A(P, [64, 512], BF16) for _ in range(2)]
    CT = [A(P, [128, 4, 64], BF16) for _ in range(2)]
    mark_prep = P.a_cur
    for dd in range(2):
        P.a_cur = mark_prep
        Ths = cplx_prep(p_hs[:, dd, 0, :], p_hs[:, dd, 1, :], p_hs[:, dd, 2, :], lambda: A(P, [64, 64], F32)[:, :])
        bbr = A(P, [64, 64], F32); bbi = A(P, [64, 64], F32); uu = A(P, [64, 64], F32)
        _tt(P, "dve", bbr[:], Ths["cr"], Bhs[:, dd, 0, :], ALU.mult, [pb], [pb])
        _tt(P, "dve", uu[:], Ths["ci"], Bhs[:, dd, 1, :], ALU.mult, [pb], [pb])
        _tt(P, "dve", bbr[:], bbr[:], uu[:], ALU.subtract, [pb], [pb])
        _tt(P, "dve", bbi[:], Ths["cr"], Bhs[:, dd, 1, :], ALU.mult, [pb], [pb])
        _tt(P, "dve", uu[:], Ths["ci"], Bhs[:, dd, 0, :], ALU.mult, [pb], [pb])
        _tt(P, "dve", bbi[:], bbi[:], uu[:], ALU.add, [pb], [pb])
        for g in range(4):
            _ts(P, "dve", BD[dd][:, g * 64:(g + 1) * 64], bbr[:], mrow[:, g:g + 1], None, ALU.mult, None, [pb], [pb])
            _ts(P, "dve", BD[dd][:, 256 + g * 64:256 + (g + 1) * 64], bbi[:], mrow[:, g:g + 1], None, ALU.mult, None, [pb], [pb])
        for ri in range(2):
            for st in range(2):
                for g in range(4):
                    _ts(P, "dve", CT[dd][:, ri * 2 + st, g * 16:(g + 1) * 16], Csm[:, dd, st, ri, :], msm[:, st, g:g + 1], (1.0 if ri == 0 else -1.0), ALU.mult, ALU.mult, [pb], [pb])
    P.a_cur = mark_prep
    TA = [[A(P, [128, 2, 129], F32) for _ in range(2)] for _ in range(2)]
    TW = [[A(P, [128, 2, 129], F32) for _ in range(2)] for _ in range(2)]
    PRE = [[A(P, [128, 256], F32) for _ in range(2)] for _ in range(2)]
    mark_t = P.a_cur
    for dd in range(2):
        P.a_cur = mark_t
        dt_ = A(P, [128, 2], F32); ar = A(P, [128, 2], F32); ai = A(P, [128, 2], F32); nar = A(P, [128, 2], F32)
        _act(P, dt_[:], p_sm[:, dd, :, 2], AF.Exp, [pb], [pb])
        _tt(P, "dve", ar[:], p_sm[:, dd, :, 0], dt_[:], ALU.mult, [pb], [pb])
        _tt(P, "dve", ai[:], p_sm[:, dd, :, 1], dt_[:], ALU.mult, [pb], [pb])
        _ts(P, "dve", nar[:], ar[:], -1.0, None, ALU.mult, None, [pb], [pb])
        mark_st = P.a_cur
        for st in range(2):
            P.a_cur = mark_st
            ph = A(P, [128, 129], F32); tmp = A(P, [128, 129], F32); sn = A(P, [128, 129], F32); cs = A(P, [128, 129], F32)
            mp = A(P, [128, 129], F32); mn = A(P, [128, 129], F32)
            _ts(P, "dve", ph[:], jrow[:], ai[:, st:st + 1], None, ALU.mult, None, [pb], [pb])
            range_reduce_sincos(P, ph[:], sn[:], cs[:], tmp[:], (lambda t: t), pb)
            _act(P, mp[:], jrow[:], AF.Exp, [pb], [pb], scale=ar[:, st:st + 1])
            _act(P, mn[:], jrow[:], AF.Exp, [pb], [pb], scale=nar[:, st:st + 1])
            _tt(P, "dve", TA[dd][0][:, st, :], mp[:], cs[:], ALU.mult, [pb], [pb])
            _tt(P, "dve", TA[dd][1][:, st, :], mp[:], sn[:], ALU.mult, [pb], [pb])
            _tt(P, "dve", TW[dd][0][:, st, :], mn[:], cs[:], ALU.mult, [pb], [pb])
            _stt(P, TW[dd][1][:, st, :], mn[:], -1.0, sn[:], ALU.mult, ALU.mult, [pb], [pb])
        P.a_cur = mark_t
        dtr = A(P, [128, 256], F32); arr = A(P, [128, 256], F32); air = A(P, [128, 256], F32)
        ph = A(P, [128, 256], F32); tmp = A(P, [128, 256], F32); sn = A(P, [128, 256], F32); cs = A(P, [128, 256], F32); mg = A(P, [128, 256], F32)
        _act(P, dtr[:], p_row[:, dd, 2, :], AF.Exp, [pb], [pb])
        _tt(P, "dve", arr[:], p_row[:, dd, 0, :], dtr[:], ALU.mult, [pb], [pb])
        _tt(P, "dve", air[:], p_row[:, dd, 1, :], dtr[:], ALU.mult, [pb], [pb])
        _ts(P, "dve", ph[:], air[:], jcol[:, 0:1], None, ALU.mult, None, [pb], [pb])
        range_reduce_sincos(P, ph[:], sn[:], cs[:], tmp[:], (lambda t: t), pb)
        _act(P, mg[:], arr[:], AF.Exp, [pb], [pb], scale=(njcol if dd == 0 else jcol)[:, 0:1])
        _tt(P, "dve", PRE[dd][0][:], mg[:], cs[:], ALU.mult, [pb], [pb])
        _stt(P, PRE[dd][1][:], mg[:], (-1.0 if dd == 0 else 1.0), sn[:], ALU.mult, ALU.mult, [pb], [pb])
        P.a_cur = mark_t
    barrier(P)
    P.a_cur = mark_t
    Xt = A(P, [128, NCH, 512], BF16); Xtb = [P.buf() for _ in range(NCH)]
    yacc = A(P, [64, NTOK], F32); yb = P.buf("yacc")
    E = A(P, [128, 4, NCH], F32); Eb = P.buf("E")
    H = [[A(P, [128, 2, NCH], F32) for _ in range(2)] for _ in range(2)]
    Hb = P.buf("H")
    cv = [A(P, [128, 2, NCH], F32) for _ in range(2)]
    pw = A(P, [128, 2, 8], F32)
    tq = [A(P, [128, 256], F32) for _ in range(8)]; tqb = [P.buf() for _ in range(8)]
    hs_ = [A(P, [128, 4, 128], BF16) for _ in range(2)]; hsb = [P.buf(), P.buf()]
    uq = [A(P, [128, 128], F32) for _ in range(16)]; uqb = [P.buf() for _ in range(16)]
    for dd in range(2):
        pos = pos_of(dd)
        for c in range(NCH):
            tau = 128 * c
            px = ps[c % 2]; pxb = psb[c % 2]
            _mm(P, px[:, 0:512], sT[:, tau:tau + 128], BD[dd][:, :], True, True, [sTb, pb], [pxb])
            i2 = (c % 2) * 4
            _tt(P, "dve", tq[i2][:, :], px[:, 0:256], PRE[dd][0][:, :], ALU.mult, [pxb, pb], [tqb[i2]])
            _tt(P, "dve", tq[i2 + 1][:, :], px[:, 256:512], PRE[dd][1][:, :], ALU.mult, [pxb, pb], [tqb[i2 + 1]])
            _tt(P, "dve", tq[i2 + 2][:, :], px[:, 0:256], PRE[dd][1][:, :], ALU.mult, [pxb, pb], [tqb[i2 + 2]])
            _tt(P, "dve", tq[i2 + 3][:, :], px[:, 256:512], PRE[dd][0][:, :], ALU.mult, [pxb, pb], [tqb[i2 + 3]])
            _tt(P, "pool", Xt[:, c, 0:256], tq[i2][:, :], tq[i2 + 1][:, :], ALU.subtract, [tqb[i2], tqb[i2 + 1]], [Xtb[c]])
            _tt(P, "pool", Xt[:, c, 256:512], tq[i2 + 2][:, :], tq[i2 + 3][:, :], ALU.add, [tqb[i2 + 2], tqb[i2 + 3]], [Xtb[c]])
            for tl in range(4):
                col = tl * NCH + pos[c]
                _mm(P, ps[6][:, col:col + 1], Xt[:, c, tl * 128:(tl + 1) * 128], ones_col[:, :], True, True, [Xtb[c], pb], [psb[6]])
        _cp(P, "dve", E[:], ps[6][:, 0:4 * NCH].rearrange("p (a b) -> p a b", b=NCH), [psb[6]], [Eb])
        H0r, H0i = H[0][0], H[0][1]
        if dd == 0:
            for st in range(2):
                a_r, a_i = TA[0][0][:, st, 127:128], TA[0][1][:, st, 127:128]
                _ts(P, "dve", uq[0][:, 0:NCH], E[:, 2 + st, :], a_i, None, ALU.mult, None, [Eb, pb], [uqb[0]])
                _stt(P, H0r[:, st, :], E[:, st, :], a_r, uq[0][:, 0:NCH], ALU.mult, ALU.subtract, [Eb, pb, uqb[0]], [Hb])
                _ts(P, "dve", uq[1][:, 0:NCH], E[:, st, :], a_i, None, ALU.mult, None, [Eb, pb], [uqb[1]])
                _stt(P, H0i[:, st, :], E[:, 2 + st, :], a_r, uq[1][:, 0:NCH], ALU.mult, ALU.add, [Eb, pb, uqb[1]], [Hb])
        else:
            _cp(P, "dve", H0r[:], E[:, 0:2, :], [Eb], [Hb])
            _cp(P, "dve", H0i[:], E[:, 2:4, :], [Eb], [Hb])
        _cp(P, "dve", pw[:, :, 0], TA[dd][0][:, :, 128], [pb], [Hb])
        _cp(P, "dve", pw[:, :, 1], TA[dd][1][:, :, 128], [pb], [Hb])
        cur = 0
        d = 1
        while d < NCH:
            _ts(P, "dve", pw[:, :, 2], pw[:, :, 1], -1.0, None, ALU.mult, None, [Hb], [Hb])
            o_, n_ = H[cur], H[1 - cur]
            for ri in range(2):
                _cp(P, "dve", n_[ri][:, :, 0:d], o_[ri][:, :, 0:d], [Hb], [Hb])
            for st in range(2):
                pr, pi, npi = pw[:, st, 0:1], pw[:, st, 1:2], pw[:, st, 2:3]
                m = NCH - d
                _stt(P, uq[0][:, 0:m], o_[0][:, st, 0:m], pr, o_[0][:, st, d:NCH], ALU.mult, ALU.add, [Hb], [uqb[0]])
                _stt(P, n_[0][:, st, d:NCH], o_[1][:, st, 0:m], npi, uq[0][:, 0:m], ALU.mult, ALU.add, [Hb, uqb[0]], [Hb])
                _stt(P, uq[1][:, 0:m], o_[1][:, st, 0:m], pr, o_[1][:, st, d:NCH], ALU.mult, ALU.add, [Hb], [uqb[1]])
                _stt(P, n_[1][:, st, d:NCH], o_[0][:, st, 0:m], pi, uq[1][:, 0:m], ALU.mult, ALU.add, [Hb, uqb[1]], [Hb])
            _tt(P, "dve", pw[:, :, 3], pw[:, :, 0], pw[:, :, 0], ALU.mult, [Hb], [Hb])
            _tt(P, "dve", pw[:, :, 4], pw[:, :, 1], pw[:, :, 1], ALU.mult, [Hb], [Hb])
            _tt(P, "dve", pw[:, :, 5], pw[:, :, 0], pw[:, :, 1], ALU.mult, [Hb], [Hb])
            _tt(P, "dve", pw[:, :, 0], pw[:, :, 3], pw[:, :, 4], ALU.subtract, [Hb], [Hb])
            _ts(P, "dve", pw[:, :, 1], pw[:, :, 5], 2.0, None, ALU.mult, None, [Hb], [Hb])
            cur = 1 - cur
            d *= 2
        Hf = H[cur]
        kidx = 1 if dd == 0 else 128
        P.op("dve", lambda h: h.memset(cv[0][:, :, 0:1], 0.0), writes=[Hb])
        P.op("dve", lambda h: h.memset(cv[1][:, :, 0:1], 0.0), writes=[Hb])
        for st in range(2):
            a_r, a_i = TA[dd][0][:, st, kidx:kidx + 1], TA[dd][1][:, st, kidx:kidx + 1]
            m = NCH - 1
            _ts(P, "dve", uq[0][:, 0:m], Hf[1][:, st, 0:m], a_i, None, ALU.mult, None, [Hb, pb], [uqb[0]])
            _stt(P, cv[0][:, st, 1:NCH], Hf[0][:, st, 0:m], a_r, uq[0][:, 0:m], ALU.mult, ALU.subtract, [Hb, pb, uqb[0]], [Hb])
            _ts(P, "dve", uq[1][:, 0:m], Hf[0][:, st, 0:m], a_i, None, ALU.mult, None, [Hb, pb], [uqb[1]])
            _stt(P, cv[1][:, st, 1:NCH], Hf[1][:, st, 0:m], a_r, uq[1][:, 0:m], ALU.mult, ALU.add, [Hb, pb, uqb[1]], [Hb])
        Tt = TA[dd] if dd == 0 else TW[dd]
        c_start = 0 if need_ctx_out else 2
        for c in range(c_start, NCH):
            pg = ps[2 + c % 2]; pgb = psb[2 + c % 2]
            for tl in range(4):
                _mm(P, pg[:, tl * 128:(tl + 1) * 128], Xt[:, c, tl * 128:(tl + 1) * 128], LT[:, dd, :], True, True, [Xtb[c], pb], [pgb])
            hh, hhb = hs_[c % 2], hsb[c % 2]
            pc = pos[c]
            for st in range(2):
                gr, gi = pg[:, st * 128:(st + 1) * 128], pg[:, (2 + st) * 128:(3 + st) * 128]
                c_r, c_i = cv[0][:, st, pc:pc + 1], cv[1][:, st, pc:pc + 1]
                Tr, Ti = Tt[0][:, st, 0:128], Tt[1][:, st, 0:128]
                u0 = ((c % 2) * 2 + st) * 4
                _stt(P, uq[u0][:, :], gr, c_r, Tr, ALU.add, ALU.mult, [pgb, Hb, pb], [uqb[u0]])
                _stt(P, uq[u0 + 1][:, :], gi, c_i, Ti, ALU.add, ALU.mult, [pgb, Hb, pb], [uqb[u0 + 1]])
                _stt(P, uq[u0 + 2][:, :], gi, c_i, Tr, ALU.add, ALU.mult, [pgb, Hb, pb], [uqb[u0 + 2]])
                _stt(P, uq[u0 + 3][:, :], gr, c_r, Ti, ALU.add, ALU.mult, [pgb, Hb, pb], [uqb[u0 + 3]])
                _tt(P, "pool", hh[:, st, :], uq[u0][:, :], uq[u0 + 1][:, :], ALU.subtract, [uqb[u0], uqb[u0 + 1]], [hhb])
                _tt(P, "pool", hh[:, 2 + st, :], uq[u0 + 2][:, :], uq[u0 + 3][:, :], ALU.add, [uqb[u0 + 2], uqb[u0 + 3]], [hhb])
            jj = c % 4
            py = ps[4 + (c // 4) % 2]; pyb = psb[4 + (c // 4) % 2]
            for tl in range(4):
                _mm(P, py[0:64, jj * 128:(jj + 1) * 128], CT[dd][:, tl, :], hh[:, tl, :], tl == 0, tl == 3, [pb, hhb], [pyb])
            if jj == 3 or c == NCH - 1:
                b0 = (c // 4) * 512
                wid = (jj + 1) * 128
                lo = 256 if ((not need_ctx_out) and c // 4 == 0) else 0
                if dd == 0:
                    _cp(P, "act", yacc[:, b0 + lo:b0 + wid], py[0:64, lo:wid], [pyb], [yb])
                else:
                    _tt(P, "dve", yacc[:, b0 + lo:b0 + wid], yacc[:, b0 + lo:b0 + wid], py[0:64, lo:wid], ALU.add, [pyb, yb], [yb])
    zT = A(P, [64, NTOK], BF16); zb = P.buf("zT")
    g1 = [A(P, [64, 512], F32) for _ in range(2)]; g1b = [P.buf(), P.buf()]
    g2 = [A(P, [64, 512], F32) for _ in range(2)]; g2b = [P.buf(), P.buf()]
    lo_all = 0 if need_ctx_out else 256
    if not need_ctx_out:
        P.op("pool", lambda h: h.memset(zT[:, 0:256], 0.0), writes=[zb])
    nblk = (NTOK + 511) // 512
    for bi in range(nblk):
        t0 = max(bi * 512, lo_all)
        t1_ = min((bi + 1) * 512, NTOK)
        n = t1_ - t0
        i2 = bi % 2
        y = g1[i2]; w = g2[i2]
        _stt(P, y[:, 0:n], sT[:, t0:t1_], dvec[:, 0:1], yacc[:, t0:t1_], ALU.mult, ALU.add, [sTb, pb, yb], [g1b[i2]])
        _tt(P, "dve", w[:, 0:n], y[:, 0:n], y[:, 0:n], ALU.mult, [g1b[i2]], [g2b[i2]])
        _ts(P, "dve", w[:, 0:n], w[:, 0:n], 0.044715, 1.0, ALU.mult, ALU.add, [g2b[i2]], [g2b[i2]])
        _tt(P, "dve", w[:, 0:n], w[:, 0:n], y[:, 0:n], ALU.mult, [g2b[i2], g1b[i2]], [g2b[i2]])
        _act(P, w[:, 0:n], w[:, 0:n], AF.Sigmoid, [g2b[i2]], [g2b[i2]], scale=1.5957691216057308)
        _tt(P, "dve", zT[:, t0:t1_], w[:, 0:n], y[:, 0:n], ALU.mult, [g2b[i2], g1b[i2]], [zb])
    outs.append(_ld(P, "sp", out[1, :, :], zT[:, :], [P.buf()], reads=[zb]))


import ml_dtypes

BF = ml_dtypes.bfloat16
NTOK = 8448
f32 = np.float32


def fm(a):
    return np.ascontiguousarray(a.T.reshape(8, 128, a.shape[0]))


def prep_common(inp, layer, b):
    cond = np.stack([inp['c'][b], inp['c_ctx']], 0)
    condT = np.ascontiguousarray(cond.reshape(2, 8, 128).transpose(2, 1, 0))
    bm = inp['b_mod'][layer].reshape(48, 128).T
    b_modT = np.ascontiguousarray(np.stack([bm, bm], -1))
    ng = inp['norm_g'][layer].reshape(4, 8, 128).transpose(2, 0, 1)
    norm_gT = np.ascontiguousarray(np.stack([ng, ng], -1))
    return dict(condT=condT, b_modT=b_modT, norm_gT=norm_gT, w_mod=inp['w_mod'][layer])


_const_cache = {}


def consts():
    if _const_cache:
        return _const_cache
    C = _const_cache
    c = np.arange(64)
    ang = 2 * np.pi * (np.outer(c, c) % 64) / 64
    C['f_CS'] = np.concatenate([np.cos(ang), -np.sin(ang)], 1).astype(BF)
    ca, sa = np.cos(ang), np.sin(ang)
    C['f_RP'] = np.concatenate([ca, -sa], 1).astype(BF)
    C['f_RQ'] = np.concatenate([sa, ca], 1).astype(BF)
    m2 = np.arange(128)[:, None, None]
    n1 = np.arange(64)[None, :, None]
    n2 = np.arange(128)[None, None, :]
    be = 2 * np.pi * ((m2 * (n1 + 64 * n2)) % 8192) / 8192
    nrm = 1 / np.sqrt(64 * 8192)
    C['f_CB'] = (np.cos(be) * nrm).astype(BF)
    C['f_SB'] = (np.sin(be) * nrm).astype(BF)
    m = np.arange(256)
    a256 = 2 * np.pi * (np.outer(m, m) % 256) / 256
    nrm2 = 1 / np.sqrt(64 * 256)
    C['f_C256'] = np.ascontiguousarray((np.cos(a256) * nrm2).reshape(2, 128, 256).transpose(1, 0, 2)).astype(BF)
    C['f_S256'] = np.ascontiguousarray((np.sin(a256) * nrm2).reshape(2, 128, 256).transpose(1, 0, 2)).astype(BF)
    t = np.arange(8192)
    row = (t // 64).astype(f32)
    col = (t % 64).astype(f32)
    inv = (1.0 / (f32(10000.0) ** (np.arange(16, dtype=f32) / f32(16)))).astype(f32)
    angr = np.concatenate([row[:, None] * inv, col[:, None] * inv], -1).astype(f32)
    cs, sn = np.cos(angr).astype(f32), np.sin(angr).astype(f32)
    cos64 = np.concatenate([cs, cs], 1)
    sin64 = np.concatenate([-sn, sn], 1)
    C['r_cosF'] = np.ascontiguousarray(cos64.T)
    C['r_sinF'] = np.ascontiguousarray(sin64.T)
    C['r_cosT'] = np.ascontiguousarray(cos64.reshape(64, 128, 64).transpose(1, 0, 2))
    C['r_sinT'] = np.ascontiguousarray(sin64.reshape(64, 128, 64).transpose(1, 0, 2))
    j = np.arange(128, dtype=f32)
    C['r_jcol'] = np.stack([127 - j, j], 1).astype(f32)
    ii = np.arange(128)
    dist = np.abs(ii[None, :] - ii[:, None]).astype(f32)
    C['r_dist'] = dist
    C['r_mask'] = np.stack([(ii[None, :] >= ii[:, None]), (ii[:, None] >= ii[None, :])], 0).astype(f32)
    C['r_irow'] = np.stack([np.tile(j + 1, (64, 1)), np.tile(128 - j, (64, 1))], 0).astype(f32)
    C['s_jrow'] = np.tile(np.arange(129, dtype=f32), (128, 1))
    C['s_jcol'] = np.arange(128, dtype=f32)[:, None].copy()
    C['s_LT'] = np.stack([(ii[None, :] >= ii[:, None]), (ii[:, None] >= ii[None, :])], 0).astype(BF)
    g_of_row = np.arange(64) // 16
    C['s_mrow'] = (g_of_row[:, None] == np.arange(4)[None, :]).astype(f32)
    g_of_st = (np.arange(128)[:, None] // 64) + 2 * np.arange(2)[None, :]
    C['s_msm'] = (g_of_st[:, :, None] == np.arange(4)[None, None, :]).astype(f32)
    C['ident_bf'] = np.eye(128).astype(BF)
    C['ident_f'] = np.eye(128).astype(f32)
    def start(r):
        return int(np.clip(r - 4, 0, 120))
    qc = np.arange(64)
    cst = np.clip(qc - 8, 0, 48)
    kc = np.arange(64)
    colok = (kc[None, :] >= cst[:, None]) & (kc[None, :] < cst[:, None] + 16)
    types = [(0, 0, 8), (2, 0, 8), (10, 6, 9), (124, 120, 8), (126, 120, 8)]
    mask = np.full((5, 128, 832), -30000.0, f32)
    drs = np.zeros((5, 2, 9), np.int64)
    for ti, (r0, R0, nr) in enumerate(types):
        for qr in range(2):
            r = r0 + qr
            for i in range(9):
                kr = R0 + i
                dr = int(np.clip(kr - r + 7, 0, 14))
                drs[ti, qr, i] = dr
                if i < nr and start(r) <= kr < start(r) + 8:
                    blk = np.where(colok, 0.0, -30000.0)
                    mask[ti, qr * 64:(qr + 1) * 64, i * 64:(i + 1) * 64] = blk
        mask[ti, :, 576:] = 0.0
    C['n_mask'] = mask
    C['n_drs'] = drs
    C['n_types'] = types
    return C


def prep_M(inp, layer, c, xT_full):
    b, q = c // 4, c % 4
    C = consts()
    m = prep_common(inp, layer, b)
    m['xT'] = xT_full
    w_in = inp['w_in'][layer]
    o = q * 64
    sw = np.r_[32:64, 0:32]
    cols_fm = np.concatenate([np.arange(0 + o, 0 + o + 64), np.arange(256 + o, 256 + o + 64), np.arange(512 + o, 512 + o + 64),
                              np.arange(768 + o, 768 + o + 64), 512 + o + sw, 768 + o + sw, np.arange(1280 + o, 1280 + o + 64),
                              np.arange(1536 + o, 1536 + o + 64), np.arange(1792 + o, 1792 + o + 64)])
    cols_tm = np.concatenate([np.arange(768 + o, 768 + o + 64), 768 + o + sw, np.arange(1024 + o, 1024 + o + 64), np.arange(2048 + o, 2048 + o + 64)])
    m['w_fm'] = np.ascontiguousarray(w_in[:, cols_fm])
    m['w_tm'] = np.ascontiguousarray(w_in[:, cols_tm])
    for k in ('f_CS', 'f_RP', 'f_RQ', 'f_CB', 'f_SB', 'f_C256', 'f_S256', 'r_cosF', 'r_sinF', 'r_cosT', 'r_sinT', 'r_jcol', 'r_dist', 'r_mask', 'r_irow',
              's_jrow', 's_jcol', 's_LT', 's_mrow', 's_msm', 'ident_bf', 'ident_f', 'n_mask'):
        m[k] = C[k]
    gs = slice(4 * q, 4 * q + 4)
    L = layer
    are, aim = inp['s5_a_re'][L][:, gs], inp['s5_a_im'][L][:, gs]
    ldt = inp['s5_log_dt'][L][:, gs]
    def sm(a):
        return np.ascontiguousarray(a.reshape(2, 2, 128).transpose(2, 0, 1))
    ldt_b = np.broadcast_to(ldt[:, :, None], (2, 4, 64))
    m['s_sm'] = np.ascontiguousarray(np.stack([sm(are), sm(aim), sm(ldt_b)], -1))
    row = np.stack([are.reshape(2, 256), aim.reshape(2, 256), ldt_b.reshape(2, 256)], -1)
    m['s_row'] = np.ascontiguousarray(np.broadcast_to(row[None].transpose(0, 1, 3, 2), (128, 2, 3, 256)))
    hs = np.stack([are, aim, ldt_b], 2)
    hs = np.broadcast_to(hs[:, :, None], (2, 4, 16, 3, 64))
    m['s_hs'] = np.ascontiguousarray(hs.transpose(1, 2, 0, 3, 4).reshape(64, 2, 3, 64))
    bre, bim = inp['s5_b_re'][L][:, gs], inp['s5_b_im'][L][:, gs]
    B = np.stack([bre, bim], 2)
    m['s_B'] = np.ascontiguousarray(B.transpose(1, 4, 0, 2, 3).reshape(64, 2, 2, 64))
    cre, cim = inp['s5_c_re'][L][:, gs], inp['s5_c_im'][L][:, gs]
    Cc = np.stack([cre, cim], 2)
    Cc = Cc.transpose(1, 4, 0, 2, 3)
    Cc = Cc.reshape(2, 2, 64, 2, 2, 16).transpose(1, 2, 3, 0, 4, 5).reshape(128, 2, 2, 2, 16)
    m['s_C'] = np.ascontiguousarray(Cc)
    m['s_d'] = np.ascontiguousarray(inp['s5_d'][L][256 * 0 + 64 * q:64 * q + 64][:, None])
    rd = inp['ret_decay'][L][:, q]
    m['r_dec'] = np.ascontiguousarray(np.broadcast_to(rd[None, :], (128, 2))).astype(f32)
    m['r_gn'] = np.ascontiguousarray(inp['ret_gn'][L][64 * q:64 * q + 64][:, None])
    rpb = inp['na_rpb'][L][q]
    dc = np.clip(np.arange(64)[None, :] - np.arange(64)[:, None], -15, 15) + 15
    m['n_toep'] = np.ascontiguousarray(rpb[:, dc])
    return m


def _prep_F(inp, layer, c, xa, bra_bf, moe_a=False):
    b = c // 4
    m = prep_common(inp, layer, b)
    m.update(xT=fm(xa), brT=bra_bf, w_in=inp['w_in'][layer], w_br=inp['w_branch'][layer].reshape(1024, 1024), w_o=inp['w_out'][layer],
             w_glu=inp['s5_w_glu'][layer], b_gluT=np.ascontiguousarray(inp['s5_b_glu'][layer].reshape(2, 128).T))
    i = layer // 2
    if layer % 2 == 0:
        m.update(w_g=inp['ffn_w_gate'][i:i + 1], w_u=inp['ffn_w_up'][i:i + 1], w_d=inp['ffn_w_down'][i:i + 1])
    else:
        sel = np.zeros((8, 8, 128), np.float32)
        for e in range(8):
            sel[e, e, :] = 1
        m.update(w_r=inp['moe_w_router'][i], b_r=np.ascontiguousarray(np.broadcast_to(inp['moe_b_router'][i][None], (128, 8))),
                 ident=np.eye(128, dtype=np.float32), sel=sel)
    return m


def kernel(**inputs):
    inp = {k: np.asarray(v) for k, v in inputs.items()}
    NCORE = 8
    cores = list(range(NCORE))
    x = inp['x']
    ctx = inp['ctx']
    for layer in range(2):
        last = (layer == 1)
        ncM = build_M(not last)
        xfull = [fm(np.concatenate([ctx[b], x[b]], 0)) for b in range(2)]
        maps = [prep_M(inp, layer, c, xfull[c // 4]) for c in cores]
        resM = run_bass_kernel_spmd(ncM, maps, core_ids=cores).results
        del maps
        br_full = []
        for b in range(2):
            o = np.stack([np.asarray(resM[4 * b + q]['brT_out']) for q in range(4)], 1)
            br_full.append(o.reshape(1024, NTOK))
        del resM
        if not last:
            blocksA = [(i * 256, 256, 0) for i in range(8)] + [(2048, 64, 1)]
            blocksB = [(i * 512, 512, 0) for i in range(4)] + [(2048, 64, 1)]
            ncF = build_F(blocksA, blocksB, 1, 2816, False)
        else:
            blocksA = [(i * 256, 256, 0) for i in range(8)]
            blocksB = [(i * 512, 512, 0) for i in range(4)]
            ncF = build_F(blocksA, blocksB, 8, 3584, True, mode='moe_a')
        maps = []
        for c in cores:
            b, q = c // 4, c % 4
            lat = slice(256 + q * 2048, 256 + (q + 1) * 2048)
            if not last:
                xa = np.concatenate([x[b, q * 2048:(q + 1) * 2048], ctx[b, q * 64:(q + 1) * 64]], 0)
                bra = np.concatenate([br_full[b][:, lat], br_full[b][:, q * 64:(q + 1) * 64]], 1)
            else:
                xa = x[b, q * 2048:(q + 1) * 2048]
                bra = br_full[b][:, lat]
            bra = np.ascontiguousarray(bra.reshape(8, 128, bra.shape[1]))
            maps.append(_prep_F(inp, layer, c, xa, bra))
        resF = run_bass_kernel_spmd(ncF, maps, core_ids=cores).results
        del maps
        if not last:
            xn = np.empty_like(x)
            cn = np.empty_like(ctx)
            for c in cores:
                b, q = c // 4, c % 4
                o = np.asarray(resF[c]['xo']).reshape(1024, -1).T
                xn[b, q * 2048:(q + 1) * 2048] = o[:2048]
                cn[b, q * 64:(q + 1) * 64] = o[2048:]
            x, ctx = xn, cn
            continue
        i = layer // 2
        h2_all = np.concatenate([np.asarray(resF[c]['h2o']) for c in cores], 2)
        cb_all = np.concatenate([np.asarray(resF[c]['cbo']) for c in cores], 1)
        sel = np.concatenate([np.asarray(resF[c]['mko']) for c in cores], 1).astype(bool)
        idx = [np.flatnonzero(sel[e]) for e in cores]
        nb = max(1, -(-max(len(t) for t in idx) // 512))
        ng = -(-nb // 4)
        groups = tuple(nb // ng + (1 if g < nb % ng else 0) for g in range(ng))
        C = 512 * nb
        ncE = build_E(groups)
        maps = []
        for e in cores:
            n_e = len(idx[e])
            ii = np.zeros(C, np.int64)
            ii[:n_e] = idx[e]
            cbe = np.zeros(C, cb_all.dtype)
            cbe[:n_e] = cb_all[e, idx[e]]
            maps.append(dict(h2=np.ascontiguousarray(h2_all[:, :, ii]), cbe=np.ascontiguousarray(np.broadcast_to(cbe[None, :], (128, C))),
                             w_g=inp['moe_w_gate'][i][e], w_u=inp['moe_w_up'][i][e], w_d=inp['moe_w_down'][i][e]))
        resE = run_bass_kernel_spmd(ncE, maps, core_ids=cores).results
        del maps
        slot = np.cumsum(sel, axis=0) - sel
        nslot = max(1, int(sel.sum(0).max()))
        yp_all = np.zeros((nslot, 8, 128, sel.shape[1]), np.float32)
        for e in cores:
            ye = np.asarray(resE[e]['ye'])
            t = idx[e]
            sv = slot[e, t]
            for k in range(nslot):
                mk = sv == k
                yp_all[k][:, :, t[mk]] = ye[:, :, np.flatnonzero(mk)]
        ncC = build_Fc(nexp=nslot)
        maps = []
        for c in cores:
            m = prep_common(inp, layer, c // 4)
            m['xm'] = np.asarray(resF[c]['xo'])
            m['yp'] = np.ascontiguousarray(yp_all[:, :, :, c * 2048:(c + 1) * 2048])
            maps.append(m)
        resC = run_bass_kernel_spmd(ncC, maps, core_ids=cores).results
        xn = np.empty_like(x)
        for c in cores:
            b, q = c // 4, c % 4
            xn[b, q * 2048:(q + 1) * 2048] = np.asarray(resC[c]['xo']).reshape(1024, -1).T
        x = xn
    return x.astype(np.float32)
```

```python
import numpy as np
from contextlib import ExitStack
import concourse.bass as bass
import concourse.mybir as mybir
from concourse.bass_utils import run_bass_kernel_spmd

F32 = mybir.dt.float32
BF16 = mybir.dt.bfloat16
I32 = mybir.dt.int32
ALU = mybir.AluOpType
AF = mybir.ActivationFunctionType
AX = mybir.AxisListType

ENGS = ("pe", "act", "dve", "pool", "sp")
NDSEM = 12


class Buf:
    __slots__ = ("name", "lw", "rd", "psum")

    def __init__(self, name="", psum=False):
        self.name = name
        self.psum = psum
        self.lw = None
        self.rd = {}


class Prog:
    def __init__(self, nc):
        self.nc = nc
        self.stack = ExitStack()
        self.ops = {e: [] for e in ENGS}
        self.cnt = {e: 0 for e in ENGS}
        self.seen = {e: {} for e in ENGS}
        self.sems = {}
        for e in ENGS:
            self.sems[e] = self.stack.enter_context(nc.semaphore("s_" + e))
        self.dsem_use = {}
        self.dq_next = {}
        for q in ("sp", "act", "pool"):
            for i in range(NDSEM):
                k = "d_%s%d" % (q, i)
                self.sems[k] = self.stack.enter_context(nc.semaphore(k))
                self.dsem_use[k] = 0
            self.dq_next[q] = 0
        self.nbuf = 0

    def sb(self, name, shape, dt):
        return self.stack.enter_context(self.nc.sbuf_tensor(name, list(shape), dt))

    def ps(self, name, shape, dt=F32):
        return self.stack.enter_context(self.nc.psum_tensor(name, list(shape), dt))

    def buf(self, name=None, psum=None):
        self.nbuf += 1
        name = name or "b%d" % self.nbuf
        if psum is None:
            psum = name.startswith("ps")
        return Buf(name, psum)

    def _deps(self, eng, reads, writes, is_dma):
        w = {}

        def add(t):
            if t is None:
                return
            k, v = t
            if w.get(k, 0) < v:
                w[k] = v
        for b in reads:
            add(b.lw)
            if b.psum:
                for k, v in b.rd.items():
                    if k != eng:
                        add((k, v))
        for b in writes:
            if b.lw is not None:
                if not (eng == "pe" and b.lw[0] == "pe" and not is_dma):
                    add(b.lw)
            for k, v in b.rd.items():
                if k == eng and not is_dma and eng != "pool":
                    continue
                add((k, v))
        seen = self.seen[eng]
        out = []
        for k, v in w.items():
            if seen.get(k, 0) < v:
                seen[k] = v
                out.append((k, v))
        return out

    def _commit(self, ticket, reads, writes):
        for b in writes:
            b.lw = ticket
            b.rd = {}
        for b in reads:
            k, v = ticket
            if b.rd.get(k, 0) < v:
                b.rd[k] = v

    def op(self, eng, fn, reads=(), writes=()):
        waits = self._deps(eng, reads, writes, False)
        self.cnt[eng] += 1
        ticket = (eng, self.cnt[eng])
        self.ops[eng].append((waits, fn, (eng, 1)))
        self._commit(ticket, reads, writes)
        return ticket

    def dma(self, q, fn, reads=(), writes=()):
        i = self.dq_next[q]
        self.dq_next[q] = (i + 1) % NDSEM
        k = "d_%s%d" % (q, i)
        waits = self._deps(q, reads, writes, True)
        prev = self.dsem_use[k]
        if prev > 0 and self.seen[q].get(k, 0) < 16 * prev:
            self.seen[q][k] = 16 * prev
            waits.append((k, 16 * prev))
        self.dsem_use[k] = prev + 1
        ticket = (k, 16 * (prev + 1))
        self.ops[q].append((waits, fn, (k, 16)))
        self._commit(ticket, reads, writes)
        return ticket

    def finish_wait(self, eng, tickets):
        waits = []
        for k, v in tickets:
            if self.seen[eng].get(k, 0) < v:
                self.seen[eng][k] = v
                waits.append((k, v))
        self.ops[eng].append((waits, None, None))

    def emit(self):
        nc = self.nc
        sems = self.sems
        ops = self.ops

        def replay(e, h):
            for waits, fn, inc in ops[e]:
                for k, v in waits:
                    h.wait_ge(sems[k], v)
                if fn is not None:
                    ins = fn(h)
                    ins.then_inc(sems[inc[0]], inc[1])

        with nc.Block() as block:
            @block.sync
            def _(h):
                replay("sp", h)

            @block.scalar
            def _(h):
                replay("act", h)

            @block.vector
            def _(h):
                replay("dve", h)

            @block.gpsimd
            def _(h):
                replay("pool", h)

            @block.tensor
            def _(h):
                replay("pe", h)
        self.stack.close()


def _mm(P, out, lhsT, rhs, start, stop, reads, writes):
    return P.op("pe", lambda h: h.matmul(out, lhsT=lhsT, rhs=rhs, start=start, stop=stop), reads=reads, writes=writes)


def _tr(P, out, in_, ident, reads, writes):
    return P.op("pe", lambda h: h.transpose(out, in_, ident), reads=reads, writes=writes)


def _act(P, out, in_, func, reads, writes, scale=None, bias=None):
    kw = {}
    if scale is not None:
        kw["scale"] = scale
    if bias is not None:
        kw["bias"] = bias
    return P.op("act", lambda h: h.activation(out=out, in_=in_, func=func, **kw), reads=reads, writes=writes)


def _tt(P, eng, out, in0, in1, op, reads, writes):
    return P.op(eng, lambda h: h.tensor_tensor(out=out, in0=in0, in1=in1, op=op), reads=reads, writes=writes)


def _ts(P, eng, out, in0, s1, s2, op0, op1, reads, writes):
    if op1 is None:
        return P.op(eng, lambda h: h.tensor_scalar(out=out, in0=in0, scalar1=s1, scalar2=None, op0=op0), reads=reads, writes=writes)
    return P.op(eng, lambda h: h.tensor_scalar(out=out, in0=in0, scalar1=s1, scalar2=s2, op0=op0, op1=op1), reads=reads, writes=writes)


def _stt(P, out, in0, scalar, in1, op0, op1, reads, writes):
    return P.op("dve", lambda h: h.scalar_tensor_tensor(out=out, in0=in0, scalar=scalar, in1=in1, op0=op0, op1=op1), reads=reads, writes=writes)


def _cp(P, eng, out, in_, reads, writes):
    if eng == "act":
        return P.op("act", lambda h: h.activation(out=out, in_=in_, func=AF.Copy), reads=reads, writes=writes)
    return P.op(eng, lambda h: h.tensor_copy(out=out, in_=in_), reads=reads, writes=writes)


def _ld(P, q, out, in_, writes, reads=()):
    return P.dma(q, lambda h: h.dma_start(out=out, in_=in_), reads=reads, writes=writes)


D = 1024
KC = 8
EPS = 1e-6


def arena_init(P, nbytes=206 * 1024):
    lo, hi = P.nc.bump_sbuf(nbytes)
    P.a_lo, P.a_hi, P.a_cur = lo, hi, lo
    P.a_n = 0


def A(P, shape, dt):
    nb = int(np.prod(shape[1:])) * (4 if dt in (F32, I32) else 2)
    off = (P.a_cur + 31) // 32 * 32
    assert off + nb <= P.a_hi, ("SBUF arena overflow", off + nb - P.a_lo)
    P.a_cur = off + nb
    P.a_n += 1
    return P.nc.alloc_sbuf_tensor_at("t%d" % P.a_n, list(shape), dt, offset=off)


def barrier(P, queues=None):
    tick = [(e, P.cnt[e]) for e in ENGS if P.cnt[e] > 0]
    tick += [(k, 16 * v) for k, v in P.dsem_use.items() if v > 0 and (queues is None or any(k.startswith("d_" + q) for q in queues))]
    for e in ENGS:
        P.finish_wait(e, tick)


def rms_rstd(P, src, srcb, n, sq, sqb, ss_ps, ssb, rstd, rstdb, ones):
    P.op("act", lambda h: h.activation(out=sq[:, :, 0:n], in_=src[:, :, 0:n], func=AF.Square), reads=[srcb], writes=[sqb])
    for k in range(KC):
        P.op("pe", lambda h, k=k: h.matmul(ss_ps[:, 0:n], lhsT=ones[:], rhs=sq[:, k, 0:n], start=(k == 0), stop=(k == KC - 1)),
             reads=[sqb], writes=[ssb])
    P.op("act", lambda h: h.activation(out=rstd[:, 0:n], in_=ss_ps[:, 0:n], func=AF.Ln, scale=1.0 / D, bias=P.eps_t[:, 0:1]), reads=[ssb], writes=[rstdb])
    P.op("act", lambda h: h.activation(out=rstd[:, 0:n], in_=rstd[:, 0:n], func=AF.Exp, scale=-0.5), reads=[rstdb], writes=[rstdb])


def norm_mod(P, src, srcb, n, rstd, rstdb, gm, sh, r, dst, dstb, tmp, tmpb, dst_off=0, dst32=None, dst32b=None):
    for k in range(KC):
        tb = tmpb[k % len(tmp)]
        tt = tmp[k % len(tmp)]
        P.op("dve", lambda h, k=k, tt=tt: h.tensor_tensor(out=tt[:, 0:n], in0=src[:, k, 0:n], in1=rstd[:, 0:n], op=ALU.mult),
             reads=[srcb, rstdb], writes=[tb])
        P.op("act", lambda h, k=k, tt=tt: h.activation(out=dst[:, k, dst_off:dst_off + n], in_=tt[:, 0:n], func=AF.Identity,
                                                     scale=gm[:, k, r:r + 1], bias=sh[:, k, r:r + 1]),
             reads=[tb, P.modb], writes=[dstb])
        if dst32 is not None:
            P.op("pool", lambda h, k=k, tt=tt: h.tensor_scalar(out=dst32[:, k, 0:n], in0=tt[:, 0:n], scalar1=gm[:, k, r:r + 1],
                                                             scalar2=sh[:, k, r:r + 1], op0=ALU.mult, op1=ALU.add),
                 reads=[tb, P.modb], writes=[dst32b])


def compute_mod(P, dr, which, mod_ps, modpb, light=False):
    nc = P.nc
    cs = A(P, [128, KC, 2], F32)
    csb = P.buf()
    P.dma("sp", lambda h: h.dma_start(out=cs[:], in_=dr["condT"][:, :, :]), writes=[csb])
    sig = A(P, [128, KC, 2], F32)
    P.op("act", lambda h: h.activation(out=sig[:], in_=cs[:], func=AF.Sigmoid), reads=[csb], writes=[csb])
    P.op("dve", lambda h: h.tensor_tensor(out=cs[:], in0=cs[:], in1=sig[:], op=ALU.mult), reads=[csb], writes=[csb])
    modT = A(P, [128, 48, 2], F32)
    P.modT = modT
    P.modb = P.buf("mod")
    bm = A(P, [128, 48, 2], F32)
    bmb = P.buf()
    P.dma("sp", lambda h: h.dma_start(out=bm[:], in_=dr["b_modT"][:, :, :]), writes=[bmb])
    ng = A(P, [128, 4, KC, 2], F32)
    P.ng = ng
    P.dma("sp", lambda h: h.dma_start(out=ng[:], in_=dr["norm_gT"][:, :, :, :]), writes=[P.modb])
    mark = P.a_cur
    wm = [A(P, [128, KC, 1024], F32) for _ in range(2)]
    wmb = [P.buf(), P.buf()]
    wsrc = dr["w_mod"].rearrange("(k p) f -> p k f", p=128)
    for i, j in enumerate(which):
        w = wm[i % 2]
        wb = wmb[i % 2]
        for k2 in range(2):
            P.dma("sp", lambda h, w=w, j=j, k2=k2: h.dma_start(out=w[:, 4 * k2:4 * k2 + 4, :], in_=wsrc[:, 4 * k2:4 * k2 + 4, j * 1024:(j + 1) * 1024]), writes=[wb])
        for fc in range(8):
            for k in range(KC):
                P.op("pe", lambda h, w=w, j=j, fc=fc, k=k: h.matmul(mod_ps[:, j * 8 + fc, :], lhsT=w[:, k, fc * 128:(fc + 1) * 128], rhs=cs[:, k, :],
                                                                   start=(k == 0), stop=(k == KC - 1)), reads=[wb, csb], writes=[modpb])
    for j in which:
        P.op("dve", lambda h, j=j: h.tensor_tensor(out=modT[:, j * 8:(j + 1) * 8, :], in0=mod_ps[:, j * 8:(j + 1) * 8, :], in1=bm[:, j * 8:(j + 1) * 8, :], op=ALU.add),
             reads=[modpb, bmb], writes=[P.modb])
    barrier(P, ("sp",) if light else None)
    P.a_cur = mark


def mod_derived(P, jsc, jg, gi_norm, gi_gate):
    gm = A(P, [128, KC, 2], F32)
    gg = A(P, [128, KC, 2], F32)
    modT, ng = P.modT, P.ng
    P.op("dve", lambda h: h.scalar_tensor_tensor(out=gm[:], in0=modT[:, jsc * 8:(jsc + 1) * 8, :], scalar=1.0, in1=ng[:, gi_norm, :, :],
                                                 op0=ALU.add, op1=ALU.mult), reads=[P.modb], writes=[P.modb])
    if jg is not None:
        P.op("dve", lambda h: h.tensor_tensor(out=gg[:], in0=modT[:, jg * 8:(jg + 1) * 8, :], in1=ng[:, gi_gate, :, :], op=ALU.mult),
             reads=[P.modb], writes=[P.modb])
    return gm, gg


def build_F(blocks, blocksB, n_exp, dff, moe, DBG=False, mode='full'):
    TT = sum(b[1] for b in blocks)
    nc = bass.Bass("TRN2", target_bir_lowering=False)
    dr = {}

    def din(name, shape, dt=F32):
        dr[name] = nc.dram_tensor(name, list(shape), dt, kind="ExternalInput").ap()
    din("xT", [KC, 128, TT])
    din("brT", [KC, 128, TT], BF16)
    din("condT", [128, KC, 2])
    din("w_mod", [D, 6 * D])
    din("b_modT", [128, 48, 2])
    din("norm_gT", [128, 4, KC, 2])
    din("w_in", [D, 6400])
    din("w_br", [KC * 128, D])
    din("w_o", [D, D])
    din("w_glu", [256, 256])
    din("b_gluT", [128, 2])
    if mode == 'full':
        din("w_g", [n_exp, D, dff])
        din("w_u", [n_exp, D, dff])
        din("w_d", [n_exp, dff, D])
    if moe:
        din("w_r", [D, 8])
        din("b_r", [128, 8])
        din("ident", [128, 128])
        din("sel", [8, 8, 128])
    if mode == 'moe_a':
        h2o = nc.dram_tensor("h2o", [KC, 128, TT], BF16, kind="ExternalOutput").ap().rearrange("k p t -> p k t")
        cbo = nc.dram_tensor("cbo", [8, TT], BF16, kind="ExternalOutput").ap()
        mko = nc.dram_tensor("mko", [8, TT], BF16, kind="ExternalOutput").ap()
    xo = nc.dram_tensor("xo", [KC, 128, TT], F32, kind="ExternalOutput").ap()
    xoT = xo.rearrange("k p t -> p k t")
    if DBG: dbg_mod = nc.dram_tensor("dbg_mod", [128, 48, 2], F32, kind="ExternalOutput").ap()
    if DBG: dbg_xm = nc.dram_tensor("dbg_xm", [KC, 128, TT], F32, kind="ExternalOutput").ap().rearrange("k p t -> p k t")
    if DBG: dbg_h = nc.dram_tensor("dbg_h", [KC, 128, TT], BF16, kind="ExternalOutput").ap().rearrange("k p t -> p k t")
    if DBG: dbg_z = nc.dram_tensor("dbg_z", [KC, 128, TT], F32, kind="ExternalOutput").ap().rearrange("k p t -> p k t")
    if DBG: dbg_r = nc.dram_tensor("dbg_r", [128, TT], F32, kind="ExternalOutput").ap()
    if DBG: dbg_sq = nc.dram_tensor("dbg_sq", [KC, 128, TT], BF16, kind="ExternalOutput").ap().rearrange("k p t -> p k t")
    if DBG: dbg_ss = nc.dram_tensor("dbg_ss", [128, TT], F32, kind="ExternalOutput").ap()
    sscp = A(P, [128, 256], F32) if False else None
    if DBG: dbg_y = nc.dram_tensor("dbg_y", [KC, 128, TT], BF16, kind="ExternalOutput").ap().rearrange("k p t -> p k t")
    xT = dr["xT"].rearrange("k p t -> p k t")
    brT = dr["brT"].rearrange("k p t -> p k t")

    P = Prog(nc)
    arena_init(P)
    ps = [P.ps("ps%d" % i, [128, 512], F32) for i in range(8)]
    psb = [P.buf("ps%d" % i) for i in range(8)]
    ones = A(P, [128, 128], BF16)
    onesb = P.buf()
    P.op("dve", lambda h: h.memset(ones[:], 1.0), writes=[onesb])
    P.eps_t = A(P, [128, 1], F32)
    P.op("dve", lambda h: h.memset(P.eps_t[:], EPS), writes=[onesb])

    w_lo = P.a_cur
    wgt = A(P, [128, KC, 4096], BF16)
    wbr = A(P, [128, KC, D], BF16)
    wo = A(P, [128, KC, D], BF16)
    wAb = P.buf("wA")
    wbufs = []

    def _wb():
        wbufs.append(P.buf())
        return wbufs[-1]
    w_in_v = dr["w_in"].rearrange("(k p) c -> p k c", p=128)
    for k in range(KC):
        for c4 in range(2):
            P.dma("pool", lambda h, k=k, c4=c4: h.dma_start(out=wgt[:, k, c4 * 2048:(c4 + 1) * 2048], in_=w_in_v[:, k, 2304 + c4 * 2048:2304 + (c4 + 1) * 2048]), writes=[_wb()])
    P.dma("pool", lambda h: h.dma_start(out=wbr[:, 0:4, :], in_=dr["w_br"].rearrange("(k p) c -> p k c", p=128)[:, 0:4, :]), writes=[_wb()])
    P.dma("pool", lambda h: h.dma_start(out=wbr[:, 4:8, :], in_=dr["w_br"].rearrange("(k p) c -> p k c", p=128)[:, 4:8, :]), writes=[_wb()])
    P.dma("pool", lambda h: h.dma_start(out=wo[:, 0:4, :], in_=dr["w_o"].rearrange("(k p) c -> p k c", p=128)[:, 0:4, :]), writes=[_wb()])
    P.dma("pool", lambda h: h.dma_start(out=wo[:, 4:8, :], in_=dr["w_o"].rearrange("(k p) c -> p k c", p=128)[:, 4:8, :]), writes=[_wb()])

    wglu = A(P, [128, 2, 256], BF16)
    bglu = A(P, [128, 2], F32)
    P.dma("pool", lambda h: h.dma_start(out=wglu[:], in_=dr["w_glu"].rearrange("(k p) c -> p k c", p=128)), writes=[_wb()])
    P.dma("sp", lambda h: h.dma_start(out=bglu[:], in_=dr["b_gluT"][:, :]), writes=[_wb()])
    w_hi = P.a_cur
    mod_ps = nc.alloc_psum_tensor
    mod_view = ps[7][:, 0:96].rearrange("p (j r) -> p j r", r=2)
    compute_mod(P, dr, [0, 1, 2, 3, 4, 5], mod_view, psb[7], light=True)
    gm_a, gg_a = mod_derived(P, 1, 2, 0, 1)
    gm_f, gg_f = mod_derived(P, 4, 5, 2, 3)
    sh_a = P.modT[:, 0:8, :]
    sh_f = P.modT[:, 24:32, :]
    P.dbgt = []
    if DBG: P.dbgt += [P.dma("sp", lambda h: h.dma_start(out=dbg_mod[:, :, :], in_=P.modT[:]), reads=[P.modb], writes=[P.buf()])]

    h2 = A(P, [128, KC, TT], BF16)
    h2b = P.buf("h2")
    if moe:
        cbT = A(P, [8, TT], BF16)
        cbTb = P.buf("cbT")
        mkT = A(P, [8, TT], BF16)
        mkTb = P.buf("mkT")
        ident = A(P, [128, 128], F32)
        P.dma("sp", lambda h: h.dma_start(out=ident[:], in_=dr["ident"][:, :]), writes=[onesb])
        wr = A(P, [128, KC, 8], F32)
        P.dma("sp", lambda h: h.dma_start(out=wr[:], in_=dr["w_r"].rearrange("(k p) e -> p k e", p=128)), writes=[onesb])
        br_t = A(P, [128, 8], F32)
        P.dma("sp", lambda h: h.dma_start(out=br_t[:], in_=dr["b_r"][:, :]), writes=[onesb])
        sel = A(P, [8, 8, 128], BF16)
        P.dma("pool", lambda h: h.dma_start(out=sel[:], in_=dr["sel"][:, :, :]), writes=[onesb])
    markA = P.a_cur
    wjoin = A(P, [128, 1], F32)
    P.op("dve", lambda h: h.memset(wjoin[:], 0.0), reads=wbufs, writes=[wAb])
    glu_t = A(P, [128, 2, 256], BF16)
    glub = P.buf("glu")
    sgl = A(P, [128, 256], F32)
    sglb = P.buf("sgl")
    xb = [A(P, [128, KC, 256], F32) for _ in range(2)]
    xbb = [P.buf(), P.buf()]
    brb_t = [A(P, [128, KC, 256], BF16) for _ in range(2)]
    brbb = [P.buf(), P.buf()]
    sq = A(P, [128, KC, 256], BF16)
    sqb = P.buf()
    rstd = A(P, [128, 256], F32)
    rstdb = P.buf()
    tmp = [A(P, [128, 256], F32) for _ in range(2)]
    tmpb = [P.buf(), P.buf()]
    hb = A(P, [128, KC, 256], BF16)
    hbb = P.buf()
    yb = A(P, [128, KC, 256], BF16)
    ybb = P.buf()
    zb = A(P, [128, KC, 256], F32)
    zbb = P.buf()
    sg = [A(P, [128, 256], F32) for _ in range(2)]
    sgb = [P.buf(), P.buf()]
    tt2 = [A(P, [128, 256], F32) for _ in range(2)]
    tt2b = [P.buf(), P.buf()]
    accA = [A(P, [128, 256], F32) for _ in range(2)]
    accAb = [P.buf(), P.buf()]
    xob = P.buf("xo")
    h2fb = P.buf("h2f")
    if moe:
        h2f = A(P, [128, KC, 256], F32)
        lg = A(P, [128, 8], F32)
        mx8 = A(P, [128, 8], F32)
        msk = A(P, [128, 8], F32)
        ex = A(P, [128, 8], F32)
        den = A(P, [128, 1], F32)
        nmx = A(P, [128, 1], F32)
        rb = P.buf("router")
    cnt = 0
    P.sscp = A(P, [128, 256], F32)
    P.sscpb = P.buf()
    def _ldA(bi_):
        t0_, n_, _r = blocks[bi_]
        xx, xxb = xb[bi_ % 2], xbb[bi_ % 2]
        bb_, bbb = brb_t[bi_ % 2], brbb[bi_ % 2]
        P.dma("sp", lambda h: h.dma_start(out=xx[:, 0:4, 0:n_], in_=xT[:, 0:4, t0_:t0_ + n_]), writes=[xxb])
        P.dma("sp", lambda h: h.dma_start(out=xx[:, 4:8, 0:n_], in_=xT[:, 4:8, t0_:t0_ + n_]), writes=[xxb])
        P.dma("sp", lambda h: h.dma_start(out=bb_[:, :, 0:n_], in_=brT[:, :, t0_:t0_ + n_]), writes=[bbb])
    _ldA(0)
    for bi, (t0, n, r) in enumerate(blocks):
        x_t, x_b = xb[bi % 2], xbb[bi % 2]
        b_t, b_b = brb_t[bi % 2], brbb[bi % 2]
        if bi + 1 < len(blocks):
            _ldA(bi + 1)
        rms_rstd(P, x_t, x_b, n, sq, sqb, ps[6], psb[6], rstd, rstdb, ones)
        norm_mod(P, x_t, x_b, n, rstd, rstdb, gm_a, sh_a, r, hb, hbb, tmp, tmpb)
        for oc in range(2):
            for kc in range(2):
                P.op("pe", lambda h, oc=oc, kc=kc, n=n, b_t=b_t: h.matmul(ps[6][:, 0:n], lhsT=wglu[:, kc, oc * 128:(oc + 1) * 128], rhs=b_t[:, 2 + kc, 0:n],
                                                                      start=(kc == 0), stop=(kc == 1)), reads=[wAb, b_b], writes=[psb[6]])
            P.op("act", lambda h, oc=oc, n=n: h.activation(out=sgl[:, 0:n], in_=ps[6][:, 0:n], func=AF.Sigmoid, bias=bglu[:, oc:oc + 1], scale=1.0), reads=[psb[6], wAb], writes=[sglb])
            P.op("dve", lambda h, oc=oc, n=n, b_t=b_t: h.tensor_tensor(out=glu_t[:, oc, 0:n], in0=sgl[:, 0:n], in1=b_t[:, 2 + oc, 0:n], op=ALU.mult), reads=[sglb, b_b], writes=[glub])
        for fc in range(8):
            ac, acb = accA[fc % 2], accAb[fc % 2]
            for b in range(4):
                gi = cnt % 2
                cnt += 1
                gps, gpb = ps[gi], psb[gi]
                pps, ppb = ps[2 + gi], psb[2 + gi]
                for k in range(KC):
                    P.op("pe", lambda h, gps=gps, k=k, b=b, fc=fc, n=n: h.matmul(gps[:, 0:n], lhsT=wgt[:, k, b * 1024 + fc * 128:b * 1024 + (fc + 1) * 128], rhs=hb[:, k, 0:n],
                                                                                 start=(k == 0), stop=(k == KC - 1)), reads=[wAb, hbb], writes=[gpb])
                for hh in range(2):
                    rhs_ap = glu_t[:, hh, 0:n] if b == 1 else b_t[:, 2 * b + hh, 0:n]
                    P.op("pe", lambda h, pps=pps, hh=hh, b=b, fc=fc, n=n, rhs_ap=rhs_ap: h.matmul(pps[:, 0:n], lhsT=wbr[:, 2 * b + hh, fc * 128:(fc + 1) * 128], rhs=rhs_ap,
                                                                                          start=(hh == 0), stop=(hh == 1)), reads=[wAb, b_b, glub], writes=[ppb])
                s_t, s_b = sg[gi], sgb[gi]
                P.op("act", lambda h, s_t=s_t, gps=gps, n=n: h.activation(out=s_t[:, 0:n], in_=gps[:, 0:n], func=AF.Sigmoid), reads=[gpb], writes=[s_b])
                if b == 0:
                    P.op("dve", lambda h, ac=ac, s_t=s_t, pps=pps, n=n: h.tensor_tensor(out=ac[:, 0:n], in0=s_t[:, 0:n], in1=pps[:, 0:n], op=ALU.mult),
                         reads=[s_b, ppb], writes=[acb])
                else:
                    t_t, t_b = tt2[gi], tt2b[gi]
                    P.op("dve", lambda h, t_t=t_t, s_t=s_t, pps=pps, n=n: h.tensor_tensor(out=t_t[:, 0:n], in0=s_t[:, 0:n], in1=pps[:, 0:n], op=ALU.mult),
                         reads=[s_b, ppb], writes=[t_b])
                    if b < 3:
                        P.op("pool", lambda h, ac=ac, t_t=t_t, n=n: h.tensor_tensor(out=ac[:, 0:n], in0=ac[:, 0:n], in1=t_t[:, 0:n], op=ALU.add),
                             reads=[acb, t_b], writes=[acb])
                    else:
                        P.op("pool", lambda h, ac=ac, t_t=t_t, n=n, fc=fc: h.tensor_tensor(out=yb[:, fc, 0:n], in0=ac[:, 0:n], in1=t_t[:, 0:n], op=ALU.add),
                             reads=[acb, t_b], writes=[ybb])
        for fc in range(8):
            zi = 4 + fc % 2
            for k in range(KC):
                P.op("pe", lambda h, zi=zi, k=k, fc=fc, n=n: h.matmul(ps[zi][:, 0:n], lhsT=wo[:, k, fc * 128:(fc + 1) * 128], rhs=yb[:, k, 0:n], start=(k == 0), stop=(k == KC - 1)),
                     reads=[wAb, ybb], writes=[psb[zi]])
            P.op("act", lambda h, zi=zi, fc=fc, n=n: h.activation(out=zb[:, fc, 0:n], in_=ps[zi][:, 0:n], func=AF.Copy), reads=[psb[zi]], writes=[zbb])
        rms_rstd(P, zb, zbb, n, sq, sqb, ps[6], psb[6], rstd, rstdb, ones)
        if DBG: P.dbgt.append(P.dma("sp", lambda h, t0=t0, n=n: h.dma_start(out=dbg_z[:, :, t0:t0 + n], in_=zb[:, :, 0:n]), reads=[zbb], writes=[P.buf()]))
        if DBG: P.dbgt.append(P.dma("sp", lambda h, t0=t0, n=n: h.dma_start(out=dbg_r[:, t0:t0 + n], in_=rstd[:, 0:n]), reads=[rstdb], writes=[P.buf()]))
        if DBG: P.dbgt.append(P.dma("sp", lambda h, t0=t0, n=n: h.dma_start(out=dbg_sq[:, :, t0:t0 + n], in_=sq[:, :, 0:n]), reads=[sqb], writes=[P.buf()]))
        if DBG: P.op("dve", lambda h, n=n: h.tensor_copy(out=P.sscp[:, 0:n], in_=ps[6][:, 0:n]), reads=[psb[6]], writes=[P.sscpb])
        if DBG: P.dbgt.append(P.dma("sp", lambda h, t0=t0, n=n: h.dma_start(out=dbg_ss[:, t0:t0 + n], in_=P.sscp[:, 0:n]), reads=[P.sscpb], writes=[P.buf()]))
        for k in range(KC):
            tb_, tt_ = tmpb[k % 2], tmp[k % 2]
            P.op("dve", lambda h, k=k, tt_=tt_, n=n: h.tensor_tensor(out=tt_[:, 0:n], in0=zb[:, k, 0:n], in1=rstd[:, 0:n], op=ALU.mult), reads=[zbb, rstdb], writes=[tb_])
            P.op("dve", lambda h, k=k, tt_=tt_, n=n, x_t=x_t, r=r: h.scalar_tensor_tensor(out=x_t[:, k, 0:n], in0=tt_[:, 0:n], scalar=gg_a[:, k, r:r + 1], in1=x_t[:, k, 0:n],
                                                                                    op0=ALU.mult, op1=ALU.add), reads=[tb_, P.modb, x_b], writes=[x_b])
        P.dma("sp", lambda h, x_t=x_t, t0=t0, n=n: h.dma_start(out=xoT[:, :, t0:t0 + n], in_=x_t[:, :, 0:n]), reads=[x_b], writes=[xob])
        if DBG: P.dbgt.append(P.dma("sp", lambda h, x_t=x_t, t0=t0, n=n: h.dma_start(out=dbg_xm[:, :, t0:t0 + n], in_=x_t[:, :, 0:n]), reads=[x_b], writes=[P.buf()]))
        if DBG: P.dbgt.append(P.dma("sp", lambda h, t0=t0, n=n: h.dma_start(out=dbg_h[:, :, t0:t0 + n], in_=hb[:, :, 0:n]), reads=[hbb], writes=[P.buf()]))
        if DBG: P.dbgt.append(P.dma("sp", lambda h, t0=t0, n=n: h.dma_start(out=dbg_y[:, :, t0:t0 + n], in_=yb[:, :, 0:n]), reads=[ybb], writes=[P.buf()]))
        rms_rstd(P, x_t, x_b, n, sq, sqb, ps[6], psb[6], rstd, rstdb, ones)
        norm_mod(P, x_t, x_b, n, rstd, rstdb, gm_f, sh_f, r, h2, h2b, tmp, tmpb, dst_off=t0, dst32=(h2f if moe else None), dst32b=h2fb)
        if moe:
            for tt in range(n // 128):
                for k in range(KC):
                    P.op("pe", lambda h, k=k, tt=tt: h.matmul(ps[7][:, 0:8], lhsT=h2f[:, k, tt * 128:(tt + 1) * 128], rhs=wr[:, k, :], start=(k == 0), stop=(k == KC - 1)),
                         reads=[h2fb, onesb], writes=[psb[7]])
                P.op("dve", lambda h: h.tensor_tensor(out=lg[:], in0=ps[7][:, 0:8], in1=br_t[:], op=ALU.add), reads=[psb[7], onesb], writes=[rb])
                P.op("dve", lambda h: h.max(out=mx8[:], in_=lg[:]), reads=[rb], writes=[rb])
                P.op("dve", lambda h: h.tensor_scalar(out=msk[:], in0=lg[:], scalar1=mx8[:, 1:2], scalar2=None, op0=ALU.is_ge), reads=[rb], writes=[rb])
                P.op("dve", lambda h: h.tensor_scalar(out=nmx[:], in0=mx8[:, 0:1], scalar1=-1.0, scalar2=None, op0=ALU.mult), reads=[rb], writes=[rb])
                P.op("act", lambda h: h.activation(out=ex[:], in_=lg[:], func=AF.Exp, bias=nmx[:, 0:1], scale=1.0), reads=[rb], writes=[rb])
                P.op("dve", lambda h: h.tensor_tensor(out=ex[:], in0=ex[:], in1=msk[:], op=ALU.mult), reads=[rb], writes=[rb])
                P.op("dve", lambda h: h.reduce_sum(out=den[:], in_=ex[:], axis=AX.X), reads=[rb], writes=[rb])
                P.op("dve", lambda h: h.reciprocal(out=den[:], in_=den[:]), reads=[rb], writes=[rb])
                P.op("dve", lambda h: h.tensor_scalar(out=ex[:], in0=ex[:], scalar1=den[:, 0:1], scalar2=None, op0=ALU.mult), reads=[rb], writes=[rb])
                P.op("pe", lambda h: h.transpose(ps[7][0:8, 128:256], ex[:], ident[:]), reads=[rb, onesb], writes=[psb[7]])
                P.op("act", lambda h, t0=t0, tt=tt: h.activation(out=cbT[:, t0 + tt * 128:t0 + (tt + 1) * 128], in_=ps[7][0:8, 128:256], func=AF.Copy), reads=[psb[7]], writes=[cbTb])
                if mode == 'moe_a':
                    P.op("pe", lambda h: h.transpose(ps[7][0:8, 256:384], msk[:], ident[:]), reads=[rb, onesb], writes=[psb[7]])
                    P.op("act", lambda h, t0=t0, tt=tt: h.activation(out=mkT[:, t0 + tt * 128:t0 + (tt + 1) * 128], in_=ps[7][0:8, 256:384], func=AF.Copy), reads=[psb[7]], writes=[mkTb])
    barrier(P)
    P.a_cur = markA
    if mode == 'moe_a':
        fin = [P.dma("sp", lambda h: h.dma_start(out=h2o[:, :, :], in_=h2[:, :, :]), reads=[h2b], writes=[P.buf()]),
               P.dma("sp", lambda h: h.dma_start(out=cbo[:, :], in_=cbT[:, :]), reads=[cbTb], writes=[P.buf()]),
               P.dma("sp", lambda h: h.dma_start(out=mko[:, :], in_=mkT[:, :]), reads=[mkTb], writes=[P.buf()])]
        barrier(P)
        P.finish_wait("sp", fin + P.dbgt)
        P.emit()
        return nc
    blocks = blocksB
    NSL = 4
    P.a_cur = w_lo
    acc = A(P, [128, KC, TT], F32)
    accb = [P.buf() for _ in blocks]
    hid = [A(P, [128, NSL, 512], BF16) for _ in range(2)]
    hidb = [P.buf(), P.buf()]
    ssb_t = [A(P, [128, 512], F32) for _ in range(2)]
    ssbb = [P.buf(), P.buf()]
    cbe = A(P, [128, 512], BF16)
    cbeb = P.buf()
    assert P.a_cur <= w_hi, "stage-B tiles overflow the weight region"
    P.a_cur = markA
    markB = markA
    wg_s = [A(P, [128, KC, NSL * 128], BF16) for _ in range(2)]
    wu_s = [A(P, [128, KC, NSL * 128], BF16) for _ in range(2)]
    wd_s = [A(P, [128, NSL, D], BF16) for _ in range(2)]
    wsb = [P.buf(), P.buf()]
    ntile = dff // 128
    slices = [(s0, min(NSL, ntile - s0)) for s0 in range(0, ntile, NSL)]
    si = 0
    hcnt = 0
    gcnt = 0
    work = [(e, s0, ns) for e in range(n_exp) for (s0, ns) in slices]

    def _ldW(widx):
        e_, s0_, ns_ = work[widx]
        wi_ = widx % 2
        wgv_ = dr["w_g"][e_].rearrange("(k p) f -> p k f", p=128)
        wuv_ = dr["w_u"][e_].rearrange("(k p) f -> p k f", p=128)
        wdv_ = dr["w_d"][e_].rearrange("(j p) c -> p j c", p=128)
        for k2 in range(2):
            P.dma("pool", lambda h, k2=k2: h.dma_start(out=wg_s[wi_][:, 4 * k2:4 * k2 + 4, 0:ns_ * 128], in_=wgv_[:, 4 * k2:4 * k2 + 4, s0_ * 128:(s0_ + ns_) * 128]), writes=[wsb[wi_]])
            P.dma("pool", lambda h, k2=k2: h.dma_start(out=wu_s[wi_][:, 4 * k2:4 * k2 + 4, 0:ns_ * 128], in_=wuv_[:, 4 * k2:4 * k2 + 4, s0_ * 128:(s0_ + ns_) * 128]), writes=[wsb[wi_]])
        for j in range(ns_):
            P.dma("pool", lambda h, j=j: h.dma_start(out=wd_s[wi_][:, j, :], in_=wdv_[:, s0_ + j, :]), writes=[wsb[wi_]])
    _ldW(0)
    for widx, (e, s0, ns) in enumerate(work):
        if True:
            wi = widx % 2
            if widx + 1 < len(work):
                _ldW(widx + 1)
            for bi, (t0, n, r) in enumerate(blocks):
                hi = hcnt % 2
                hcnt += 1
                if moe:
                    P.op("pe", lambda h, e=e, t0=t0, n=n: h.matmul(ps[7][:, 0:n], lhsT=sel[:, e, :], rhs=cbT[:, t0:t0 + n], start=True, stop=True), reads=[cbTb, onesb], writes=[psb[7]])
                    P.op("act", lambda h, n=n: h.activation(out=cbe[:, 0:n], in_=ps[7][:, 0:n], func=AF.Copy), reads=[psb[7]], writes=[cbeb])
                for j in range(ns):
                    gi = gcnt % 2
                    gcnt += 1
                    for k in range(KC):
                        P.op("pe", lambda h, gi=gi, wi=wi, j=j, k=k, t0=t0, n=n: h.matmul(ps[gi][:, 0:n], lhsT=wg_s[wi][:, k, j * 128:(j + 1) * 128], rhs=h2[:, k, t0:t0 + n], start=(k == 0), stop=(k == KC - 1)),
                             reads=[wsb[wi], h2b], writes=[psb[gi]])
                    for k in range(KC):
                        P.op("pe", lambda h, gi=gi, wi=wi, j=j, k=k, t0=t0, n=n: h.matmul(ps[2 + gi][:, 0:n], lhsT=wu_s[wi][:, k, j * 128:(j + 1) * 128], rhs=h2[:, k, t0:t0 + n], start=(k == 0), stop=(k == KC - 1)),
                             reads=[wsb[wi], h2b], writes=[psb[2 + gi]])
                    P.op("act", lambda h, gi=gi, n=n: h.activation(out=ssb_t[gi][:, 0:n], in_=ps[gi][:, 0:n], func=AF.Silu), reads=[psb[gi]], writes=[ssbb[gi]])
                    if moe:
                        P.op("dve", lambda h, gi=gi, n=n: h.tensor_tensor(out=ssb_t[gi][:, 0:n], in0=ssb_t[gi][:, 0:n], in1=ps[2 + gi][:, 0:n], op=ALU.mult),
                             reads=[ssbb[gi], psb[2 + gi]], writes=[ssbb[gi]])
                        P.op("pool", lambda h, gi=gi, hi=hi, j=j, n=n: h.tensor_tensor(out=hid[hi][:, j, 0:n], in0=ssb_t[gi][:, 0:n], in1=cbe[:, 0:n], op=ALU.mult),
                             reads=[ssbb[gi], cbeb], writes=[hidb[hi]])
                    else:
                        P.op("dve", lambda h, gi=gi, hi=hi, j=j, n=n: h.tensor_tensor(out=hid[hi][:, j, 0:n], in0=ssb_t[gi][:, 0:n], in1=ps[2 + gi][:, 0:n], op=ALU.mult),
                             reads=[ssbb[gi], psb[2 + gi]], writes=[hidb[hi]])
                first = (e == 0 and s0 == 0)
                for fc in range(8):
                    oi = 4 + fc % 2
                    for j in range(ns):
                        P.op("pe", lambda h, oi=oi, wi=wi, j=j, fc=fc, hi=hi, n=n, ns=ns: h.matmul(ps[oi][:, 0:n], lhsT=wd_s[wi][:, j, fc * 128:(fc + 1) * 128], rhs=hid[hi][:, j, 0:n], start=(j == 0), stop=(j == ns - 1)),
                             reads=[wsb[wi], hidb[hi]], writes=[psb[oi]])
                    if first:
                        P.op("act", lambda h, oi=oi, fc=fc, t0=t0, n=n: h.activation(out=acc[:, fc, t0:t0 + n], in_=ps[oi][:, 0:n], func=AF.Copy), reads=[psb[oi]], writes=[accb[bi]])
                    else:
                        P.op("dve", lambda h, oi=oi, fc=fc, t0=t0, n=n: h.tensor_tensor(out=acc[:, fc, t0:t0 + n], in0=acc[:, fc, t0:t0 + n], in1=ps[oi][:, 0:n], op=ALU.add),
                             reads=[psb[oi], accb[bi]], writes=[accb[bi]])
    barrier(P)
    P.a_cur = markB
    xm = [A(P, [128, KC, 512], F32) for _ in range(2)]
    xmb = [P.buf(), P.buf()]
    sqF = A(P, [128, KC, 512], BF16)
    rstdF = A(P, [128, 512], F32)
    tmpF = [A(P, [128, 512], F32) for _ in range(2)]
    outs = []
    for bi, (t0, n, r) in enumerate(blocks):
        x_t, x_b = xm[bi % 2], xmb[bi % 2]
        P.dma("sp", lambda h, x_t=x_t, t0=t0, n=n: h.dma_start(out=x_t[:, :, 0:n], in_=xoT[:, :, t0:t0 + n]), reads=[xob], writes=[x_b])
        accv = acc[:, :, t0:t0 + n]
        P.op("act", lambda h, accv=accv, n=n: h.activation(out=sqF[:, :, 0:n], in_=accv, func=AF.Square), reads=[accb[bi]], writes=[sqb])
        for k in range(KC):
            P.op("pe", lambda h, k=k, n=n: h.matmul(ps[6][:, 0:n], lhsT=ones[:], rhs=sqF[:, k, 0:n], start=(k == 0), stop=(k == KC - 1)), reads=[sqb, onesb], writes=[psb[6]])
        P.op("act", lambda h, n=n: h.activation(out=rstdF[:, 0:n], in_=ps[6][:, 0:n], func=AF.Ln, scale=1.0 / D, bias=P.eps_t[:, 0:1]), reads=[psb[6]], writes=[rstdb])
        P.op("act", lambda h, n=n: h.activation(out=rstdF[:, 0:n], in_=rstdF[:, 0:n], func=AF.Exp, scale=-0.5), reads=[rstdb], writes=[rstdb])
        for k in range(KC):
            tb_, tt_ = tmpb[k % 2], tmpF[k % 2]
            P.op("dve", lambda h, k=k, tt_=tt_, n=n, t0=t0: h.tensor_tensor(out=tt_[:, 0:n], in0=acc[:, k, t0:t0 + n], in1=rstdF[:, 0:n], op=ALU.mult), reads=[accb[bi], rstdb], writes=[tb_])
            P.op("dve", lambda h, k=k, tt_=tt_, n=n, x_t=x_t, r=r: h.scalar_tensor_tensor(out=x_t[:, k, 0:n], in0=tt_[:, 0:n], scalar=gg_f[:, k, r:r + 1], in1=x_t[:, k, 0:n],
                                                                                    op0=ALU.mult, op1=ALU.add), reads=[tb_, P.modb, x_b], writes=[x_b])
        outs.append(P.dma("sp", lambda h, x_t=x_t, t0=t0, n=n: h.dma_start(out=xoT[:, :, t0:t0 + n], in_=x_t[:, :, 0:n]), reads=[x_b], writes=[xob]))
    P.finish_wait("sp", outs + P.dbgt)
    P.emit()
    return nc


def build_E(groups=(4,) * 8, dff=3584):
    nc = bass.Bass("TRN2", target_bir_lowering=False)
    ngrp = len(groups)
    gtok = 512 * max(groups)
    NT = 512 * sum(groups)
    goff = [512 * sum(groups[:g]) for g in range(ngrp)]
    h2d = nc.dram_tensor("h2", [KC, 128, NT], BF16, kind="ExternalInput").ap().rearrange("k p t -> p k t")
    cbd = nc.dram_tensor("cbe", [128, NT], BF16, kind="ExternalInput").ap()
    wgd = nc.dram_tensor("w_g", [D, dff], F32, kind="ExternalInput").ap().rearrange("(k p) f -> p k f", p=128)
    wud = nc.dram_tensor("w_u", [D, dff], F32, kind="ExternalInput").ap().rearrange("(k p) f -> p k f", p=128)
    wdd = nc.dram_tensor("w_d", [dff, D], F32, kind="ExternalInput").ap().rearrange("(j p) c -> p j c", p=128)
    ye = nc.dram_tensor("ye", [KC, 128, NT], F32, kind="ExternalOutput").ap().rearrange("k p t -> p k t")
    P = Prog(nc)
    arena_init(P)
    ps = [P.ps("ps%d" % i, [128, 512], F32) for i in range(8)]
    psb = [P.buf("ps%d" % i) for i in range(8)]
    h2g = [A(P, [128, KC, gtok], BF16) for _ in range(2)]
    h2gb = [P.buf(), P.buf()]
    cbg = [A(P, [128, gtok], BF16) for _ in range(2)]
    acc = A(P, [128, KC, gtok], F32)
    NSL = 4
    wg_s = [A(P, [128, KC, NSL * 128], BF16) for _ in range(2)]
    wu_s = [A(P, [128, KC, NSL * 128], BF16) for _ in range(2)]
    wd_s = [A(P, [128, NSL, D], BF16) for _ in range(2)]
    wsb = [P.buf(), P.buf()]
    hid = [A(P, [128, NSL, 512], BF16) for _ in range(2)]
    hidb = [P.buf(), P.buf()]
    ssb_t = [A(P, [128, 512], F32) for _ in range(2)]
    ssbb = [P.buf(), P.buf()]
    ntile = dff // 128
    slices = [(s0, min(NSL, ntile - s0)) for s0 in range(0, ntile, NSL)]
    accb = [P.buf() for _ in range(max(groups))]
    si = hcnt = gcnt = 0
    outs = []
    work = [(g, sidx, s0, ns) for g in range(ngrp) for sidx, (s0, ns) in enumerate(slices)]

    def _ldG(g_):
        hg_, hgb_ = h2g[g_ % 2], h2gb[g_ % 2]
        gn_ = 512 * groups[g_]
        for k2 in range(2):
            _ldF(P, "sp", hg_[:, 4 * k2:4 * k2 + 4, 0:gn_], h2d[:, 4 * k2:4 * k2 + 4, goff[g_]:goff[g_] + gn_], [hgb_])
        _ldF(P, "sp", cbg[g_ % 2][:, 0:gn_], cbd[:, goff[g_]:goff[g_] + gn_], [hgb_])

    def _ldW(widx):
        _g, _sidx, s0_, ns_ = work[widx]
        wi_ = widx % 2
        for k2 in range(2):
            _ldF(P, "pool", wg_s[wi_][:, 4 * k2:4 * k2 + 4, 0:ns_ * 128], wgd[:, 4 * k2:4 * k2 + 4, s0_ * 128:(s0_ + ns_) * 128], [wsb[wi_]])
            _ldF(P, "pool", wu_s[wi_][:, 4 * k2:4 * k2 + 4, 0:ns_ * 128], wud[:, 4 * k2:4 * k2 + 4, s0_ * 128:(s0_ + ns_) * 128], [wsb[wi_]])
        for j in range(ns_):
            _ldF(P, "pool", wd_s[wi_][:, j, :], wdd[:, s0_ + j, :], [wsb[wi_]])
    _ldG(0)
    _ldW(0)
    for widx, (g, sidx, s0, ns) in enumerate(work):
        hg, hgb = h2g[g % 2], h2gb[g % 2]
        cg = cbg[g % 2]
        nblk = groups[g]
        if sidx == 0 and g + 1 < ngrp:
            _ldG(g + 1)
        if True:
            wi = widx % 2
            if widx + 1 < len(work):
                _ldW(widx + 1)
            for bi in range(nblk):
                t0, n = bi * 512, 512
                hi = hcnt % 2
                hcnt += 1
                for j in range(ns):
                    gi = gcnt % 2
                    gcnt += 1
                    for k in range(KC):
                        _mmF(P, ps[gi][:, 0:n], wg_s[wi][:, k, j * 128:(j + 1) * 128], hg[:, k, t0:t0 + n], k == 0, k == KC - 1, [wsb[wi], hgb], [psb[gi]])
                    for k in range(KC):
                        _mmF(P, ps[2 + gi][:, 0:n], wu_s[wi][:, k, j * 128:(j + 1) * 128], hg[:, k, t0:t0 + n], k == 0, k == KC - 1, [wsb[wi], hgb], [psb[2 + gi]])
                    st_, stb_ = ssb_t[gi], ssbb[gi]
                    P.op("act", lambda h, st_=st_, gi=gi, n=n: h.activation(out=st_[:, 0:n], in_=ps[gi][:, 0:n], func=AF.Silu), reads=[psb[gi]], writes=[stb_])
                    P.op("dve", lambda h, st_=st_, gi=gi, n=n: h.tensor_tensor(out=st_[:, 0:n], in0=st_[:, 0:n], in1=ps[2 + gi][:, 0:n], op=ALU.mult), reads=[stb_, psb[2 + gi]], writes=[stb_])
                    hd = hid[hi]
                    P.op("pool", lambda h, st_=st_, hd=hd, j=j, n=n, cg=cg, t0=t0: h.tensor_tensor(out=hd[:, j, 0:n], in0=st_[:, 0:n], in1=cg[:, t0:t0 + n], op=ALU.mult), reads=[stb_, hgb], writes=[hidb[hi]])
                for fc in range(8):
                    oi = 4 + fc % 2
                    for j in range(ns):
                        _mmF(P, ps[oi][:, 0:n], wd_s[wi][:, j, fc * 128:(fc + 1) * 128], hid[hi][:, j, 0:n], j == 0, j == ns - 1, [wsb[wi], hidb[hi]], [psb[oi]])
                    av = acc[:, fc, t0:t0 + n]
                    pv = ps[oi][:, 0:n]
                    if sidx == 0:
                        P.op("act", lambda h, av=av, pv=pv: h.activation(out=av, in_=pv, func=AF.Copy), reads=[psb[oi]], writes=[accb[bi]])
                    else:
                        P.op("dve", lambda h, av=av, pv=pv: h.tensor_tensor(out=av, in0=av, in1=pv, op=ALU.add), reads=[psb[oi], accb[bi]], writes=[accb[bi]])
        if sidx == len(slices) - 1:
            for bi in range(nblk):
                t0 = bi * 512
                outs.append(_ldF(P, "sp", ye[:, :, goff[g] + t0:goff[g] + t0 + 512], acc[:, :, t0:t0 + 512], [P.buf()], reads=[accb[bi]]))
    P.finish_wait("sp", outs)
    P.emit()
    return nc


def _ldF(P, q, out, in_, writes, reads=()):
    return P.dma(q, lambda h: h.dma_start(out=out, in_=in_), reads=reads, writes=writes)


def _mmF(P, out, lhsT, rhs, start, stop, reads, writes):
    return P.op("pe", lambda h: h.matmul(out, lhsT=lhsT, rhs=rhs, start=start, stop=stop), reads=reads, writes=writes)


def build_Fc(TT=2048, nexp=8):
    nc = bass.Bass("TRN2", target_bir_lowering=False)
    dr = {}

    def din(name, shape, dt=F32):
        dr[name] = nc.dram_tensor(name, list(shape), dt, kind="ExternalInput").ap()
    din("xm", [KC, 128, TT])
    din("yp", [nexp, KC, 128, TT])
    din("condT", [128, KC, 2])
    din("w_mod", [D, 6 * D])
    din("b_modT", [128, 48, 2])
    din("norm_gT", [128, 4, KC, 2])
    xo = nc.dram_tensor("xo", [KC, 128, TT], F32, kind="ExternalOutput").ap().rearrange("k p t -> p k t")
    xm = dr["xm"].rearrange("k p t -> p k t")
    P = Prog(nc)
    arena_init(P)
    ps = [P.ps("ps%d" % i, [128, 512], F32) for i in range(8)]
    psb = [P.buf("ps%d" % i) for i in range(8)]
    ones = A(P, [128, 128], BF16)
    onesb = P.buf()
    P.op("dve", lambda h: h.memset(ones[:], 1.0), writes=[onesb])
    P.eps_t = A(P, [128, 1], F32)
    P.op("dve", lambda h: h.memset(P.eps_t[:], EPS), writes=[onesb])
    mod_view = ps[7][:, 0:96].rearrange("p (j r) -> p j r", r=2)
    compute_mod(P, dr, [5], mod_view, psb[7])
    _, gg_f = mod_derived(P, 4, 5, 2, 3)
    acc = [A(P, [128, KC, 512], F32) for _ in range(2)]
    accb = [P.buf(), P.buf()]
    part = [A(P, [128, KC, 512], F32) for _ in range(3)]
    partb = [P.buf() for _ in range(3)]
    xt = [A(P, [128, KC, 512], F32) for _ in range(2)]
    xtb = [P.buf(), P.buf()]
    sq = A(P, [128, KC, 512], BF16)
    sqb = P.buf()
    rstd = A(P, [128, 512], F32)
    rstdb = P.buf()
    tmp = [A(P, [128, 512], F32) for _ in range(2)]
    tmpb = [P.buf(), P.buf()]
    outs = []
    pc = 0
    for bi in range(TT // 512):
        t0, n = bi * 512, 512
        a_t, a_b = acc[bi % 2], accb[bi % 2]
        x_t, x_b = xt[bi % 2], xtb[bi % 2]
        _ldF(P, "sp", x_t[:, :, :], xm[:, :, t0:t0 + n], [x_b])
        _ldF(P, "sp", a_t[:, :, :], dr["yp"][0].rearrange("k p t -> p k t")[:, :, t0:t0 + n], [a_b])
        for e in range(1, nexp):
            p_t, p_b = part[pc % 3], partb[pc % 3]
            pc += 1
            _ldF(P, "act" if e % 2 else "sp", p_t[:, :, :], dr["yp"][e].rearrange("k p t -> p k t")[:, :, t0:t0 + n], [p_b])
            eng = "dve" if e % 2 else "pool"
            P.op(eng, lambda h, a_t=a_t, p_t=p_t: h.tensor_tensor(out=a_t[:, :, :], in0=a_t[:, :, :], in1=p_t[:, :, :], op=ALU.add), reads=[a_b, p_b], writes=[a_b])
        rms_rstd(P, a_t, a_b, n, sq, sqb, ps[6], psb[6], rstd, rstdb, ones)
        for k in range(KC):
            tb_, tt_ = tmpb[k % 2], tmp[k % 2]
            P.op("dve", lambda h, k=k, tt_=tt_, a_t=a_t: h.tensor_tensor(out=tt_[:, :], in0=a_t[:, k, :], in1=rstd[:, :], op=ALU.mult), reads=[a_b, rstdb], writes=[tb_])
            P.op("dve", lambda h, k=k, tt_=tt_, x_t=x_t: h.scalar_tensor_tensor(out=x_t[:, k, :], in0=tt_[:, :], scalar=gg_f[:, k, 0:1], in1=x_t[:, k, :], op0=ALU.mult, op1=ALU.add),
                 reads=[tb_, P.modb, x_b], writes=[x_b])
        outs.append(_ldF(P, "sp", xo[:, :, t0:t0 + n], x_t[:, :, :], [P.buf()], reads=[x_b]))
    P.finish_wait("sp", outs)
    P.emit()
    return nc


import math, os
RET_STOP = int(os.environ.get('RET_STOP', '99'))
SKIP = os.environ.get('SKIP', '')

NTOK = 8448
NCH = 66
MAGIC = 12582912.0
TWO_PI = 2.0 * math.pi


def pos_of(dd):
    if dd == 0:
        return list(range(NCH))
    order = [1, 0] + list(range(65, 1, -1))
    pos = [0] * NCH
    for p_, c in enumerate(order):
        pos[c] = p_
    return pos


def range_reduce_sincos(P, ph, sn, cs, tmp, shape_ap, b):
    v = shape_ap
    _ts(P, "dve", v(tmp), v(ph), 1.0 / TWO_PI, MAGIC, ALU.mult, ALU.add, [b], [b])
    _ts(P, "dve", v(tmp), v(tmp), -MAGIC, None, ALU.add, None, [b], [b])
    _stt(P, v(ph), v(tmp), -TWO_PI, v(ph), ALU.mult, ALU.add, [b], [b])
    _ts(P, "dve", v(ph), v(ph), -math.pi, math.pi, ALU.max, ALU.min, [b], [b])
    _act(P, v(sn), v(ph), AF.Sin, [b], [b])
    _ts(P, "dve", v(tmp), v(ph), -1.0, None, ALU.mult, None, [b], [b])
    _tt(P, "dve", v(tmp), v(tmp), v(ph), ALU.max, [b], [b])
    _act(P, v(cs), v(tmp), AF.Sin, [b], [b], scale=-1.0, bias=P.halfpi[0:v(tmp).shape[0], 0:1])


def build_M(need_ctx_out, parts=("four", "s5", "ret", "na"), DBG=False):
    nc = bass.Bass("TRN2", target_bir_lowering=False)
    dr = {}

    def din(name, shape, dt=F32):
        dr[name] = nc.dram_tensor(name, list(shape), dt, kind="ExternalInput").ap()
    din("xT", [KC, 128, NTOK])
    din("condT", [128, KC, 2])
    din("w_mod", [D, 6 * D])
    din("b_modT", [128, 48, 2])
    din("norm_gT", [128, 4, KC, 2])
    din("w_fm", [D, 576])
    din("w_tm", [D, 256])
    din("f_CS", [64, 128], BF16); din("f_RP", [64, 128], BF16); din("f_RQ", [64, 128], BF16)
    din("f_CB", [128, 64, 128], BF16); din("f_SB", [128, 64, 128], BF16)
    din("f_C256", [128, 2, 256], BF16); din("f_S256", [128, 2, 256], BF16)
    din("r_cosF", [64, 8192]); din("r_sinF", [64, 8192]); din("r_cosT", [128, 64, 64]); din("r_sinT", [128, 64, 64])
    din("r_jcol", [128, 2]); din("r_dist", [128, 128]); din("r_mask", [2, 128, 128]); din("r_irow", [2, 64, 128])
    din("s_jrow", [128, 129]); din("s_jcol", [128, 1]); din("s_LT", [2, 128, 128], BF16); din("s_mrow", [64, 4]); din("s_msm", [128, 2, 4])
    din("ident_bf", [128, 128], BF16); din("ident_f", [128, 128])
    din("n_mask", [5, 128, 832]); din("n_toep", [15, 64, 64])
    din("s_sm", [128, 2, 2, 3]); din("s_row", [128, 2, 3, 256]); din("s_hs", [64, 2, 3, 64]); din("s_B", [64, 2, 2, 64])
    din("s_C", [128, 2, 2, 2, 16]); din("s_d", [64, 1])
    din("r_dec", [128, 2]); din("r_gn", [64, 1])
    out = nc.dram_tensor("brT_out", [4, 64, NTOK], BF16, kind="ExternalOutput").ap()
    hT = nc.dram_tensor("hT_scr", [KC, 128, NTOK], BF16, kind="Internal").ap().rearrange("k p t -> p k t")
    xT = dr["xT"].rearrange("k p t -> p k t")

    P = Prog(nc)
    arena_init(P)
    ps = [P.ps("ps%d" % i, [128, 512], F32) for i in range(8)]
    psb = [P.buf("ps%d" % i) for i in range(8)]
    cb = P.buf("consts")
    ones = A(P, [128, 128], BF16)
    P.op("dve", lambda h: h.memset(ones[:], 1.0), writes=[cb])
    P.eps_t = A(P, [128, 1], F32)
    P.op("dve", lambda h: h.memset(P.eps_t[:], EPS), writes=[cb])
    P.halfpi = A(P, [128, 1], F32)
    P.op("dve", lambda h: h.memset(P.halfpi[:], math.pi / 2), writes=[cb])
    P.one_t = A(P, [128, 1], F32)
    P.op("dve", lambda h: h.memset(P.one_t[:], 1.0), writes=[cb])
    ident = A(P, [128, 128], BF16)
    _ld(P, "sp", ident[:], dr["ident_bf"][:, :], [cb])
    mod_view = ps[7][:, 0:96].rearrange("p (j r) -> p j r", r=2)
    compute_mod(P, dr, [0, 1], mod_view, psb[7])
    gm_a, _ = mod_derived(P, 1, None, 0, 0)
    sh_a = P.modT[:, 0:8, :]
    wfm = A(P, [128, KC, 576], BF16)
    wtm = A(P, [128, KC, 256], BF16)
    wb = P.buf("w")
    _ld(P, "pool", wfm[:], dr["w_fm"].rearrange("(k p) c -> p k c", p=128), [wb])
    _ld(P, "pool", wtm[:], dr["w_tm"].rearrange("(k p) c -> p k c", p=128), [wb])
    blocks = [(0, 256, 1)] + [(256 + 512 * i, 512, 0) for i in range(16)]
    outs = []
    hTb = P.buf("hT")
    mark0 = P.a_cur

    def fm_proj(hb, hbb, n, g, pst, pstb):
        for k in range(KC):
            _mm(P, pst[0:64, 0:n], wfm[:, k, g * 64:(g + 1) * 64], hb[:, k, 0:n], k == 0, k == KC - 1, [wb, hbb], [pstb])

    sT = A(P, [64, NTOK], BF16)
    markS = P.a_cur
    fT = A(P, [64, NTOK], BF16)
    fTb, sTb = P.buf("fT"), P.buf("sT")
    markA = P.a_cur
    xb = [A(P, [128, KC, 512], F32) for _ in range(2)]
    xbb = [P.buf(), P.buf()]
    sq = A(P, [128, KC, 512], BF16)
    sqb = P.buf()
    rstd = A(P, [128, 512], F32)
    rstdb = P.buf()
    tmp = [A(P, [128, 512], F32) for _ in range(8)]
    tmpb = [P.buf() for _ in range(8)]
    hbs = [A(P, [128, KC, 512], BF16) for _ in range(2)]
    hbsb = [P.buf(), P.buf()]
    def _ldx(bi_):
        t0_, n_, _r = blocks[bi_]
        _ld(P, "sp", xb[bi_ % 2][:, 0:4, 0:n_], xT[:, 0:4, t0_:t0_ + n_], [xbb[bi_ % 2]])
        _ld(P, "sp", xb[bi_ % 2][:, 4:8, 0:n_], xT[:, 4:8, t0_:t0_ + n_], [xbb[bi_ % 2]])
    _ldx(0)
    for bi, (t0, n, r) in enumerate(blocks):
        x_t, x_b = xb[bi % 2], xbb[bi % 2]
        hb, hbb = hbs[bi % 2], hbsb[bi % 2]
        if bi + 1 < len(blocks):
            _ldx(bi + 1)
        rms_rstd(P, x_t, x_b, n, sq, sqb, ps[6], psb[6], rstd, rstdb, ones)
        norm_mod(P, x_t, x_b, n, rstd, rstdb, gm_a, sh_a, r, hb, hbb, tmp, tmpb)
        _ld(P, "sp", hT[:, :, t0:t0 + n], hb[:, :, 0:n], [hTb], reads=[hbb])
        for gi_, (g, dst, dstb) in enumerate(((0, fT, fTb), (1, sT, sTb))):
            pi_ = (2 * bi + gi_) % 4
            fm_proj(hb, hbb, n, g, ps[pi_], psb[pi_])
            _cp(P, "act" if gi_ == 0 else "dve", dst[:, t0:t0 + n], ps[pi_][0:64, 0:n], [psb[pi_]], [dstb])
    barrier(P)
    P.a_cur = markA

    if "four" in parts:
        markF = P.a_cur
        CS = A(P, [64, 128], BF16); RP = A(P, [64, 128], BF16); RQ = A(P, [64, 128], BF16)
        CB = A(P, [128, 64, 128], BF16); SB = A(P, [128, 64, 128], BF16)
        ftb = P.buf("ftab")
        for t_, nm in ((CS, "f_CS"), (RP, "f_RP"), (RQ, "f_RQ")):
            _ld(P, "sp", t_[:], dr[nm][:, :], [ftb])
        _ld(P, "sp", CB[:], dr["f_CB"][:, :, :], [ftb])
        _ld(P, "sp", SB[:], dr["f_SB"][:, :, :], [ftb])
        PQ = A(P, [64, 128, 128], BF16); PQb = P.buf("PQ")
        UVT = A(P, [128, 64, 128], BF16); UVTb = P.buf("UVT")
        aT = A(P, [64, NTOK], BF16); aTb = P.buf("aT")
        for g4 in range(32):
            pi_ = g4 % 2
            for jj in range(4):
                m2 = g4 * 4 + jj
                _mm(P, ps[pi_][0:64, jj * 128:(jj + 1) * 128], fT[:, 256 + m2:NTOK:128], CS[:, :], True, True, [fTb, ftb], [psb[pi_]])
            _cp(P, "act" if g4 % 2 else "dve", PQ[:, g4 * 4:(g4 + 1) * 4, :], ps[pi_][0:64, 0:512].rearrange("p (a b) -> p a b", b=128), [psb[pi_]], [PQb])
        for g4 in range(16):
            pi_ = 2 + g4 % 2
            for jj in range(4):
                d = g4 * 4 + jj
                _mm(P, ps[pi_][:, jj * 128:(jj + 1) * 128], PQ[:, :, d], RP[:, :], True, False, [PQb, ftb], [psb[pi_]])
                _mm(P, ps[pi_][:, jj * 128:(jj + 1) * 128], PQ[:, :, 64 + d], RQ[:, :], False, True, [PQb, ftb], [psb[pi_]])
            _cp(P, "act" if g4 % 2 else "dve", UVT[:, g4 * 4:(g4 + 1) * 4, :], ps[pi_][:, 0:512].rearrange("p (a b) -> p a b", b=128), [psb[pi_]], [UVTb])
        aT3 = aT[:, 256:NTOK].rearrange("p (a b) -> p a b", b=64)
        for g4 in range(16):
            pi_ = g4 % 2
            for jj in range(4):
                n1 = g4 * 4 + jj
                _mm(P, ps[pi_][0:64, jj * 128:(jj + 1) * 128], UVT[:, :, n1], CB[:, n1, :], True, False, [UVTb, ftb], [psb[pi_]])
                _mm(P, ps[pi_][0:64, jj * 128:(jj + 1) * 128], UVT[:, :, 64 + n1], SB[:, n1, :], False, True, [UVTb, ftb], [psb[pi_]])
            _cp(P, "act" if g4 % 2 else "dve", aT3[:, :, g4 * 4:(g4 + 1) * 4], ps[pi_][0:64, 0:512].rearrange("p (j n) -> p n j", n=128), [psb[pi_]], [aTb])
        if need_ctx_out:
            C256 = A(P, [128, 2, 256], BF16); S256 = A(P, [128, 2, 256], BF16)
            _ld(P, "sp", C256[:], dr["f_C256"][:, :, :], [ftb])
            _ld(P, "sp", S256[:], dr["f_S256"][:, :, :], [ftb])
            PQc = A(P, [128, 2, 128], BF16); PQcb = P.buf()
            for tt in range(2):
                _mm(P, ps[2 + tt][:, 0:128], fT[:, tt * 128:(tt + 1) * 128], CS[:, :], True, True, [fTb, ftb], [psb[2 + tt]])
                _cp(P, "dve", PQc[:, tt, :], ps[2 + tt][:, 0:128], [psb[2 + tt]], [PQcb])
            seq = [(tt, 0) for tt in range(2)] + [(tt, 1) for tt in range(2)]
            for i_, (tt, pq) in enumerate(seq):
                _mm(P, ps[4][0:64, 0:256], PQc[:, tt, pq * 64:(pq + 1) * 64], (C256 if pq == 0 else S256)[:, tt, :], i_ == 0, i_ == 3, [PQcb, ftb], [psb[4]])
            _cp(P, "dve", aT[:, 0:256], ps[4][0:64, 0:256], [psb[4]], [aTb])
        else:
            P.op("dve", lambda h: h.memset(aT[:, 0:256], 0.0), writes=[aTb])
        outs.append(_ld(P, "sp", out[0, :, :], aT[:, :], [P.buf()], reads=[aTb]))
        barrier(P)
    P.a_cur = markS

    if "s5" in parts:
        s5_part(P, dr, ps, psb, sT, sTb, out, outs, need_ctx_out, cb)
    barrier(P)
    P.a_cur = mark0

    if "ret" in parts:
        ret_part(P, dr, ps, psb, hT, hTb, wfm, wtm, wb, blocks, out, outs, need_ctx_out, cb, ones)
        barrier(P)
        P.a_cur = mark0
    if "na" in parts:
        na_part(P, dr, ps, psb, hT, hTb, wfm, wtm, wb, blocks, out, outs, need_ctx_out, cb, ident)
        barrier(P)
    P.finish_wait("sp", outs)
    P.emit()
    return nc


def load_h(P, hT, hTb, hbs, hbsb, bi, t0, n):
    hb, hbb = hbs[bi % 2], hbsb[bi % 2]
    _ld(P, "sp", hb[:, :, 0:n], hT[:, :, t0:t0 + n], [hbb], reads=[hTb])
    return hb, hbb


def na_part(P, dr, ps, psb, hT, hTb, wfm, wtm, wb, blocks, out, outs, need_ctx_out, cb, ident):
    Cn = consts()
    drs, types = Cn["n_drs"], Cn["n_types"]
    nqT = A(P, [64, NTOK], BF16); nkT = A(P, [64, NTOK], BF16); nvT = A(P, [128, NCH, 64], BF16)
    nqb, nkb, nvb = P.buf("nq"), P.buf("nk"), P.buf("nv")
    nT = A(P, [64, NTOK], BF16); nTb = P.buf("nT")
    bias = A(P, [128, 5, 832], F32); biasb = P.buf("bias")
    mask = A(P, [128, 5, 832], F32)
    P.op("pool", lambda h: h.memset(bias[:], 0.0), writes=[biasb])
    maskb = P.buf()
    _ld(P, "sp", mask[:], dr["n_mask"].rearrange("t p c -> p t c"), [maskb])
    for ti in range(5):
        for qr in range(2):
            for i in range(9):
                _ld(P, "sp" if (i % 2) else "act", bias[qr * 64:(qr + 1) * 64, ti, i * 64:(i + 1) * 64], dr["n_toep"][int(drs[ti, qr, i])], [biasb])
    _tt(P, "dve", bias[:], bias[:], mask[:], ALU.add, [biasb, maskb], [biasb])
    mark = P.a_cur
    hbs = [A(P, [128, KC, 512], BF16) for _ in range(2)]
    hbsb = [P.buf(), P.buf()]
    cnt = 0
    for bi, (t0, n, r) in enumerate(blocks):
        hb, hbb = load_h(P, hT, hTb, hbs, hbsb, bi, t0, n)
        for g, dst, dstb in ((7, nqT, nqb), (8, nkT, nkb)):
            pi_ = cnt % 4
            cnt += 1
            for k in range(KC):
                _mm(P, ps[pi_][0:64, 0:n], wfm[:, k, g * 64:(g + 1) * 64], hb[:, k, 0:n], k == 0, k == KC - 1, [wb, hbb], [psb[pi_]])
            _cp(P, "act" if g == 7 else "dve", dst[:, t0:t0 + n], ps[pi_][0:64, 0:n], [psb[pi_]], [dstb])
        for tt in range(n // 128):
            pi_ = 4 + (tt % 2)
            for k in range(KC):
                _mm(P, ps[pi_][:, 0:64], hb[:, k, tt * 128:(tt + 1) * 128], wtm[:, k, 192:256], k == 0, k == KC - 1, [wb, hbb], [psb[pi_]])
            _cp(P, "act", nvT[:, t0 // 128 + tt, :], ps[pi_][:, 0:64], [psb[pi_]], [nvb])
    barrier(P)
    P.a_cur = mark
    NB4 = 4
    s_t = [A(P, [128, 832], F32) for _ in range(NB4)]; s_b = [P.buf() for _ in range(NB4)]
    p_t = [A(P, [128, 832], BF16) for _ in range(NB4)]; p_b = [P.buf() for _ in range(NB4)]
    pT = [A(P, [128, 7, 128], BF16) for _ in range(NB4)]; pTb = [P.buf() for _ in range(NB4)]
    st_ = [A(P, [128, 4], F32) for _ in range(NB4)]; stb = [P.buf() for _ in range(NB4)]
    SC = 0.125

    def softmax_pv(qi, ncols, pv_list, o_ps, o_psb, o_cols, sbi, s4=None):
        if s4 is None:
            s4 = sbi
        s, sb_ = s_t[s4], s_b[s4]
        sm, smb = st_[s4], stb[s4]
        P.op("dve", lambda h: h.reduce_max(out=sm[:, 1:2], in_=s[:, 0:ncols], axis=AX.X, negate=True), reads=[sb_], writes=[smb])
        _act(P, s[:, 0:ncols], s[:, 0:ncols], AF.Exp, [sb_, smb], [sb_], bias=sm[:, 1:2], scale=1.0)
        P.op("dve", lambda h: h.reduce_sum(out=sm[:, 2:3], in_=s[:, 0:ncols], axis=AX.X), reads=[sb_], writes=[smb])
        P.op("dve", lambda h: h.reciprocal(out=sm[:, 3:4], in_=sm[:, 2:3]), reads=[smb], writes=[smb])
        p, pb = p_t[s4], p_b[s4]
        _ts(P, "dve", p[:, 0:ncols], s[:, 0:ncols], sm[:, 3:4], None, ALU.mult, None, [sb_, smb], [pb])
        tp = ps[4 + sbi][:, :].bitcast(BF16)
        for ci, (c0, nk, tile) in enumerate(pv_list):
            _tr(P, tp[0:nk, ci * 128:(ci + 1) * 128], p[:, c0:c0 + nk], ident[:, :], [pb, cb], [psb[4 + sbi]])
        nchk = len(pv_list)
        pt_, ptb = pT[s4], pTb[s4]
        _cp(P, "act", pt_[:, 0:nchk, :], tp[:, 0:nchk * 128].rearrange("p (a b) -> p a b", b=128), [psb[4 + sbi]], [ptb])
        for ci, (c0, nk, tile) in enumerate(pv_list):
            _mm(P, o_ps[0:64, o_cols:o_cols + 128], nvT[0:nk, tile, :], pt_[0:nk, ci, :], ci == 0, ci == nchk - 1, [nvb, ptb], [o_psb])

    for rp in range(64):
        ti = {0: 0, 1: 1, 62: 3, 63: 4}.get(rp, 2)
        r0 = 2 * rp
        if ti == 2:
            R0, nr = r0 - 4, 9
        else:
            R0, nr = types[ti][1], 8
        tq = 256 + 128 * rp
        kb_ = 256 + 64 * R0
        sbi = rp % 2
        s1, s2 = ps[sbi], ps[2 + sbi]
        _mm(P, s1[:, 0:512], nqT[:, tq:tq + 128], nkT[:, kb_:kb_ + 512], True, True, [nqb, nkb], [psb[sbi]])
        kb2 = kb_ + 512 if nr == 9 else kb_
        _mm(P, s2[:, 0:64], nqT[:, tq:tq + 128], nkT[:, kb2:kb2 + 64], True, True, [nqb, nkb], [psb[2 + sbi]])
        _mm(P, s2[:, 64:320], nqT[:, tq:tq + 128], nkT[:, 0:256], True, True, [nqb, nkb], [psb[2 + sbi]])
        s4 = rp % NB4
        s = s_t[s4]
        _stt(P, s[:, 0:512], s1[:, 0:512], SC, bias[:, ti, 0:512], ALU.mult, ALU.add, [psb[sbi], biasb], [s_b[s4]])
        _stt(P, s[:, 512:832], s2[:, 0:320], SC, bias[:, ti, 512:832], ALU.mult, ALU.add, [psb[2 + sbi], biasb], [s_b[s4]])
        t_base = 2 + R0 // 2
        pv = [(128 * j, 128, t_base + j) for j in range(4)]
        if nr == 9:
            pv.append((512, 64, t_base + 4))
        pv += [(576, 128, 0), (704, 128, 1)]
        jj = rp % 4
        softmax_pv(rp, 832, pv, ps[6], psb[6], jj * 128, sbi, s4)
        if jj == 3:
            _cp(P, "dve", nT[:, 256 + 512 * (rp // 4):256 + 512 * (rp // 4 + 1)], ps[6][0:64, 0:512], [psb[6]], [nTb])
    if need_ctx_out:
        for qt in range(2):
            sbi = qt
            _mm(P, ps[sbi][:, 0:256], nqT[:, qt * 128:(qt + 1) * 128], nkT[:, 0:256], True, True, [nqb, nkb], [psb[sbi]])
            _ts(P, "dve", s_t[sbi][:, 0:256], ps[sbi][:, 0:256], SC, None, ALU.mult, None, [psb[sbi]], [s_b[sbi]])
            softmax_pv(qt, 256, [(0, 128, 0), (128, 128, 1)], ps[7], psb[7], qt * 128, sbi)
        _cp(P, "dve", nT[:, 0:256], ps[7][0:64, 0:256], [psb[7]], [nTb])
    else:
        P.op("dve", lambda h: h.memset(nT[:, 0:256], 0.0), writes=[nTb])
    outs.append(_ld(P, "sp", out[3, :, :], nT[:, :], [P.buf()], reads=[nTb]))


def ret_part(P, dr, ps, psb, hT, hTb, wfm, wtm, wb, blocks, out, outs, need_ctx_out, cb, ones):
    KS = 0.125
    qT = A(P, [64, NTOK], BF16); kT = A(P, [64, NTOK], BF16); gT = A(P, [64, NTOK], BF16)
    qb_, kb_, gb_ = P.buf("q"), P.buf("k"), P.buf("g")
    rvT = A(P, [128, NCH, 64], BF16); rvb = P.buf("rv")
    Sbf = [A(P, [64, NCH, 64], BF16) for _ in range(2)]
    rc = P.buf("retc")
    dec = A(P, [128, 2], F32); lg = A(P, [128, 2], F32); jcol = A(P, [128, 2], F32); kdec = A(P, [128, 2], F32); g128 = A(P, [128, 2], F32)
    dist = A(P, [128, 128], F32); msk = A(P, [128, 2, 128], F32); DT = A(P, [128, 128], F32); DT2 = A(P, [128, 128], F32)
    irow = A(P, [64, 2, 128], F32); qdec = A(P, [64, 2, 128], F32)
    gn = A(P, [64, 1], F32); o64 = A(P, [64, 64], F32)
    _ld(P, "sp", dec[:], dr["r_dec"][:, :], [rc])
    _ld(P, "sp", jcol[:], dr["r_jcol"][:, :], [rc])
    _ld(P, "sp", dist[:], dr["r_dist"][:, :], [rc])
    _ld(P, "sp", msk[:], dr["r_mask"].rearrange("d j i -> j d i"), [rc])
    _ld(P, "sp", irow[:], dr["r_irow"].rearrange("d p i -> p d i"), [rc])
    _ld(P, "sp", gn[:], dr["r_gn"][:, :], [rc])
    P.op("dve", lambda h: h.memset(o64[:], 1.0 / 64), writes=[rc])
    _act(P, lg[:], dec[:], AF.Exp, [rc], [rc], scale=-1.0)
    _act(P, lg[:], lg[:], AF.Ln, [rc], [rc], bias=P.one_t[:, 0:1], scale=1.0)
    _ts(P, "dve", lg[:], lg[:], -1.0, None, ALU.mult, None, [rc], [rc])
    for dd in range(2):
        _act(P, kdec[:, dd:dd + 1], jcol[:, dd:dd + 1], AF.Exp, [rc], [rc], scale=lg[:, dd:dd + 1])
        _act(P, g128[:, dd:dd + 1], lg[:, dd:dd + 1], AF.Exp, [rc], [rc], scale=128.0)
        _act(P, qdec[:, dd, :], irow[:, dd, :], AF.Exp, [rc], [rc], scale=lg[0:64, dd:dd + 1])
    _ts(P, "dve", kdec[:], kdec[:], KS, None, ALU.mult, None, [rc], [rc])
    _act(P, DT[:], dist[:], AF.Exp, [rc], [rc], scale=lg[:, 0:1])
    _tt(P, "dve", DT[:], DT[:], msk[:, 0, :], ALU.mult, [rc], [rc])
    _act(P, DT2[:], dist[:], AF.Exp, [rc], [rc], scale=lg[:, 1:2])
    _tt(P, "dve", DT2[:], DT2[:], msk[:, 1, :], ALU.mult, [rc], [rc])
    _tt(P, "dve", DT[:], DT[:], DT2[:], ALU.add, [rc], [rc])
    if RET_STOP <= 0:
        return
    mark_k = P.a_cur
    kd = [A(P, [128, NCH, 64], BF16) for _ in range(2)]
    kdb = [P.buf(), P.buf()]
    mark = P.a_cur
    hbs = [A(P, [128, KC, 512], BF16) for _ in range(2)]
    hbsb = [P.buf(), P.buf()]
    cF = [A(P, [64, 512], F32) for _ in range(2)]; sF = [A(P, [64, 512], F32) for _ in range(2)]
    cTt = [A(P, [128, 4, 64], F32) for _ in range(2)]; sTt = [A(P, [128, 4, 64], F32) for _ in range(2)]
    tabb = [P.buf(), P.buf()]
    t1 = [A(P, [128, 512], F32) for _ in range(2)]; t1b = [P.buf(), P.buf()]
    t2 = [A(P, [128, 512], F32) for _ in range(2)]; t2b = [P.buf(), P.buf()]
    cnt = 0
    for bi, (t0, n, r) in enumerate(blocks):
        hb, hbb = load_h(P, hT, hTb, hbs, hbsb, bi, t0, n)
        lat = (r == 0)
        tb_ = tabb[bi % 2]
        if lat:
            m0 = t0 - 256
            _ld(P, "sp", cF[bi % 2][:, :], dr["r_cosF"][:, m0:m0 + 512], [tb_])
            _ld(P, "sp", sF[bi % 2][:, :], dr["r_sinF"][:, m0:m0 + 512], [tb_])
            _ld(P, "sp", cTt[bi % 2][:, :, :], dr["r_cosT"][:, m0 // 128:m0 // 128 + 4, :], [tb_])
            _ld(P, "sp", sTt[bi % 2][:, :, :], dr["r_sinT"][:, m0 // 128:m0 // 128 + 4, :], [tb_])

        def proj(g, pi_):
            for k in range(KC):
                _mm(P, ps[pi_][0:64, 0:n], wfm[:, k, g * 64:(g + 1) * 64], hb[:, k, 0:n], k == 0, k == KC - 1, [wb, hbb], [psb[pi_]])
        for (g, gsw, dst, dstb, scl) in ((2, 4, qT, qb_, 1.0), (3, 5, kT, kb_, KS)):
            if 'qk' in SKIP:
                continue
            proj(g, 0)
            if lat:
                proj(gsw, 1)
                i2 = cnt % 2
                cnt += 1
                _stt(P, t1[i2][0:64, 0:n], ps[0][0:64, 0:n], scl, cF[bi % 2][:, 0:n], ALU.mult, ALU.mult, [psb[0], tb_], [t1b[i2]])
                _stt(P, t2[i2][0:64, 0:n], ps[1][0:64, 0:n], scl, sF[bi % 2][:, 0:n], ALU.mult, ALU.mult, [psb[1], tb_], [t2b[i2]])
                _tt(P, "pool", dst[:, t0:t0 + n], t1[i2][0:64, 0:n], t2[i2][0:64, 0:n], ALU.add, [t1b[i2], t2b[i2]], [dstb])
            else:
                _act(P, dst[:, t0:t0 + n], ps[0][0:64, 0:n], AF.Copy, [psb[0]], [dstb], scale=scl)
        proj(6, 2)
        _cp(P, "act", gT[:, t0:t0 + n], ps[2][0:64, 0:n], [psb[2]], [gb_])
        for tt in range(n // 128):
            if 'tm' in SKIP:
                continue
            pi_ = 4 + (tt % 2)
            tile = t0 // 128 + tt
            for k in range(KC):
                _mm(P, ps[pi_][:, 0:192], hb[:, k, tt * 128:(tt + 1) * 128], wtm[:, k, 0:192], k == 0, k == KC - 1, [wb, hbb], [psb[pi_]])
            _cp(P, "act", rvT[:, tile, :], ps[pi_][:, 128:192], [psb[pi_]], [rvb])
            if 'kd' in SKIP:
                continue
            if ('kl' in SKIP and lat) or ('kc' in SKIP and not lat):
                continue
            if lat:
                i2 = cnt % 2
                cnt += 1
                _tt(P, "dve", t1[i2][:, 0:64], ps[pi_][:, 0:64], cTt[bi % 2][:, tt, :], ALU.mult, [psb[pi_], tb_], [t1b[i2]])
                _tt(P, "dve", t2[i2][:, 0:64], ps[pi_][:, 64:128], sTt[bi % 2][:, tt, :], ALU.mult, [psb[pi_], tb_], [t2b[i2]])
                if 'k1' in SKIP:
                    continue
                _tt(P, "dve", t1[i2][:, 0:64], t1[i2][:, 0:64], t2[i2][:, 0:64], ALU.add, [t1b[i2], t2b[i2]], [t1b[i2]])
                if 'k2' in SKIP:
                    continue
                for dd in range(2):
                    _act(P, kd[dd][:, tile, :], t1[i2][:, 0:64], AF.Identity, [t1b[i2], rc], [kdb[dd]], scale=kdec[:, dd:dd + 1])
            else:
                for dd in range(2):
                    _act(P, kd[dd][:, tile, :], ps[pi_][:, 0:64], AF.Identity, [psb[pi_], rc], [kdb[dd]], scale=kdec[:, dd:dd + 1])
    barrier(P)
    P.a_cur = mark
    if RET_STOP <= 1:
        return
    S32 = [A(P, [64, NCH, 64], F32) for _ in range(2)]
    Sb = [P.buf(), P.buf()]
    orders = []
    for dd in range(2):
        pos = pos_of(dd)
        orders.append(sorted(range(NCH), key=lambda c, pos=pos: pos[c]))
        c0 = orders[dd][0]
        P.op("dve", lambda h, dd=dd, c0=c0: h.memset(S32[dd][:, c0, :], 0.0), writes=[Sb[dd]])
    for idx in range(NCH - 1):
        for dd in range(2):
            c, nxt = orders[dd][idx], orders[dd][idx + 1]
            pi_ = 2 * dd + (idx // 8) % 2
            sl = idx % 8
            _mm(P, ps[pi_][0:64, sl * 64:(sl + 1) * 64], kd[dd][:, c, :], rvT[:, c, :], True, True, [kdb[dd], rvb], [psb[pi_]])
            _stt(P, S32[dd][:, nxt, :], S32[dd][:, c, :], g128[0:64, dd:dd + 1], ps[pi_][0:64, sl * 64:(sl + 1) * 64], ALU.mult, ALU.add, [Sb[dd], psb[pi_], rc], [Sb[dd]])
    for dd in range(2):
        _cp(P, "act", Sbf[dd][:], S32[dd][:], [Sb[dd]], [Sb[dd]])
    barrier(P)
    P.a_cur = mark_k
    if RET_STOP <= 2:
        return
    oT = A(P, [64, NTOK], F32); oTb = P.buf("oT")
    sc = [A(P, [128, 128], BF16) for _ in range(2)]; scb = [P.buf(), P.buf()]
    qd = [[A(P, [64, 128], BF16) for _ in range(2)] for _ in range(2)]
    qdb = [[P.buf(), P.buf()] for _ in range(2)]
    c_start = 0 if need_ctx_out else 2
    if not need_ctx_out:
        P.op("pool", lambda h: h.memset(oT[:, 0:256], 0.0), writes=[oTb])
    for c in range(c_start, NCH):
        tau = 128 * c
        i2 = c % 2
        _mm(P, ps[i2][:, 0:128], kT[:, tau:tau + 128], qT[:, tau:tau + 128], True, True, [kb_, qb_], [psb[i2]])
        _tt(P, "dve", sc[i2][:, :], ps[i2][:, 0:128], DT[:, :], ALU.mult, [psb[i2], rc], [scb[i2]])
        for dd in range(2):
            _tt(P, "pool", qd[dd][i2][:, :], qT[:, tau:tau + 128], qdec[:, dd, :], ALU.mult, [qb_, rc], [qdb[dd][i2]])
        jj = c % 4
        po = ps[4 + (c // 4) % 2]
        pob = psb[4 + (c // 4) % 2]
        _mm(P, po[0:64, jj * 128:(jj + 1) * 128], rvT[:, c, :], sc[i2][:, :], True, False, [rvb, scb[i2]], [pob])
        _mm(P, po[0:64, jj * 128:(jj + 1) * 128], Sbf[0][:, c, :], qd[0][i2][:, :], False, False, [Sb[0], qdb[0][i2]], [pob])
        _mm(P, po[0:64, jj * 128:(jj + 1) * 128], Sbf[1][:, c, :], qd[1][i2][:, :], False, True, [Sb[1], qdb[1][i2]], [pob])
        if jj == 3 or c == NCH - 1:
            b0 = (c // 4) * 512
            wid = (jj + 1) * 128
            lo = 0
            if (not need_ctx_out) and c // 4 == 0:
                lo = 256
            _cp(P, "act", oT[:, b0 + lo:b0 + wid], po[0:64, lo:wid], [pob], [oTb])
    if RET_STOP <= 3:
        return
    rT = A(P, [64, NTOK], BF16); rTb = P.buf("rT")
    o64b = A(P, [64, 64], BF16)
    P.op("dve", lambda h: h.memset(o64b[:], 1.0 / 64), writes=[rc])
    obf = [A(P, [64, 512], BF16) for _ in range(2)]; obfb = [P.buf(), P.buf()]
    cen = [A(P, [64, 512], F32) for _ in range(2)]; cenb = [P.buf(), P.buf()]
    sq_ = [A(P, [64, 512], BF16) for _ in range(2)]; sqb_ = [P.buf(), P.buf()]
    rs_ = [A(P, [64, 512], F32) for _ in range(2)]; rsb_ = [P.buf(), P.buf()]
    sg_ = [A(P, [64, 512], F32) for _ in range(2)]; sgb_ = [P.buf(), P.buf()]
    nblk = (NTOK + 511) // 512
    for bi in range(nblk):
        t0 = bi * 512
        n = min(512, NTOK - t0)
        i2 = bi % 2
        _cp(P, "act", obf[i2][:, 0:n], oT[:, t0:t0 + n], [oTb], [obfb[i2]])
        _mm(P, ps[2 + i2][0:64, 0:n], o64b[:, :], obf[i2][:, 0:n], True, True, [obfb[i2], rc], [psb[2 + i2]])
        _tt(P, "dve", cen[i2][:, 0:n], oT[:, t0:t0 + n], ps[2 + i2][0:64, 0:n], ALU.subtract, [oTb, psb[2 + i2]], [cenb[i2]])
        _act(P, sq_[i2][:, 0:n], cen[i2][:, 0:n], AF.Square, [cenb[i2]], [sqb_[i2]])
        _mm(P, ps[6 + i2][0:64, 0:n], o64b[:, :], sq_[i2][:, 0:n], True, True, [sqb_[i2], rc], [psb[6 + i2]])
        _act(P, rs_[i2][:, 0:n], ps[6 + i2][0:64, 0:n], AF.Ln, [psb[6 + i2]], [rsb_[i2]], bias=P.eps_t[0:64, 0:1], scale=1.0)
        _act(P, rs_[i2][:, 0:n], rs_[i2][:, 0:n], AF.Exp, [rsb_[i2]], [rsb_[i2]], scale=-0.5)
        _tt(P, "dve", cen[i2][:, 0:n], cen[i2][:, 0:n], rs_[i2][:, 0:n], ALU.mult, [cenb[i2], rsb_[i2]], [cenb[i2]])
        _act(P, sg_[i2][:, 0:n], gT[:, t0:t0 + n], AF.Silu, [gb_], [sgb_[i2]])
        _stt(P, rT[:, t0:t0 + n], cen[i2][:, 0:n], gn[:, 0:1], sg_[i2][:, 0:n], ALU.mult, ALU.mult, [cenb[i2], sgb_[i2], rc], [rTb])
    outs.append(_ld(P, "sp", out[2, :, :], rT[:, :], [P.buf()], reads=[rTb]))


def s5_part(P, dr, ps, psb, sT, sTb, out, outs, need_ctx_out, cb):
    pb = P.buf("s5param")

    def cplx_prep(are, aim, ldt, mk):
        T = {k: mk() for k in ("dt", "ar", "ai", "mag", "ph", "tmp", "sn", "cs", "abr", "abi", "nr", "den", "cr", "ci", "u")}
        ident_v = lambda t: t
        _act(P, T["dt"], ldt, AF.Exp, [pb], [pb])
        _tt(P, "dve", T["ar"], are, T["dt"], ALU.mult, [pb], [pb])
        _tt(P, "dve", T["ai"], aim, T["dt"], ALU.mult, [pb], [pb])
        _act(P, T["mag"], T["ar"], AF.Exp, [pb], [pb])
        _cp(P, "dve", T["ph"], T["ai"], [pb], [pb])
        range_reduce_sincos(P, T["ph"], T["sn"], T["cs"], T["tmp"], ident_v, pb)
        _tt(P, "dve", T["abr"], T["mag"], T["cs"], ALU.mult, [pb], [pb])
        _tt(P, "dve", T["abi"], T["mag"], T["sn"], ALU.mult, [pb], [pb])
        _ts(P, "dve", T["nr"], T["abr"], -1.0, None, ALU.add, None, [pb], [pb])
        _tt(P, "dve", T["den"], are, are, ALU.mult, [pb], [pb])
        _tt(P, "dve", T["u"], aim, aim, ALU.mult, [pb], [pb])
        _tt(P, "dve", T["den"], T["den"], T["u"], ALU.add, [pb], [pb])
        P.op("dve", lambda h: h.reciprocal(out=T["den"], in_=T["den"]), reads=[pb], writes=[pb])
        _tt(P, "dve", T["cr"], T["nr"], are, ALU.mult, [pb], [pb])
        _tt(P, "dve", T["u"], T["abi"], aim, ALU.mult, [pb], [pb])
        _tt(P, "dve", T["cr"], T["cr"], T["u"], ALU.add, [pb], [pb])
        _tt(P, "dve", T["cr"], T["cr"], T["den"], ALU.mult, [pb], [pb])
        _tt(P, "dve", T["ci"], T["abi"], are, ALU.mult, [pb], [pb])
        _tt(P, "dve", T["u"], T["nr"], aim, ALU.mult, [pb], [pb])
        _tt(P, "dve", T["ci"], T["ci"], T["u"], ALU.subtract, [pb], [pb])
        _tt(P, "dve", T["ci"], T["ci"], T["den"], ALU.mult, [pb], [pb])
        return T

    p_sm = A(P, [128, 2, 2, 3], F32); p_row = A(P, [128, 2, 3, 256], F32); p_hs = A(P, [64, 2, 3, 64], F32)
    Bhs = A(P, [64, 2, 2, 64], F32); Csm = A(P, [128, 2, 2, 2, 16], F32); dvec = A(P, [64, 1], F32)
    jrow = A(P, [128, 129], F32); jcol = A(P, [128, 1], F32); njcol = A(P, [128, 1], F32)
    LT = A(P, [128, 2, 128], BF16); mrow = A(P, [64, 4], F32); msm = A(P, [128, 2, 4], F32)
    for t_, src in ((p_sm[:], dr["s_sm"][:, :, :, :]), (p_row[:], dr["s_row"][:, :, :, :]), (p_hs[:], dr["s_hs"][:, :, :, :]), (Bhs[:], dr["s_B"][:, :, :, :]),
                    (Csm[:], dr["s_C"][:, :, :, :, :]), (dvec[:], dr["s_d"][:, :]), (jrow[:], dr["s_jrow"][:, :]), (jcol[:], dr["s_jcol"][:, :]),
                    (LT[:], dr["s_LT"].rearrange("d j i -> j d i")), (mrow[:], dr["s_mrow"][:, :]), (msm[:], dr["s_msm"][:, :, :])):
        _ld(P, "sp", t_, src, [pb])
    _ts(P, "dve", njcol[:], jcol[:], -1.0, None, ALU.mult, None, [pb], [pb])
    ones_col = A(P, [128, 1], BF16)
    P.op("dve", lambda h: h.memset(ones_col[:], 1.0), writes=[pb])

    BD = [A(P, [64, 512], BF16) for _ in range(2)]
    CT = [A(P, [128, 4, 64], BF16) for _ in range(2)]
    mark_prep = P.a_cur
    for dd in range(2):
        P.a_cur = mark_prep
        Ths = cplx_prep(p_hs[:, dd, 0, :], p_hs[:, dd, 1, :], p_hs[:, dd, 2, :], lambda: A(P, [64, 64], F32)[:, :])
        bbr = A(P, [64, 64], F32); bbi = A(P, [64, 64], F32); uu = A(P, [64, 64], F32)
        _tt(P, "dve", bbr[:], Ths["cr"], Bhs[:, dd, 0, :], ALU.mult, [pb], [pb])
        _tt(P, "dve", uu[:], Ths["ci"], Bhs[:, dd, 1, :], ALU.mult, [pb], [pb])
        _tt(P, "dve", bbr[:], bbr[:], uu[:], ALU.subtract, [pb], [pb])
        _tt(P, "dve", bbi[:], Ths["cr"], Bhs[:, dd, 1, :], ALU.mult, [pb], [pb])
        _tt(P, "dve", uu[:], Ths["ci"], Bhs[:, dd, 0, :], ALU.mult, [pb], [pb])
        _tt(P, "dve", bbi[:], bbi[:], uu[:], ALU.add, [pb], [pb])
        for g in range(4):
            _ts(P, "dve", BD[dd][:, g * 64:(g + 1) * 64], bbr[:], mrow[:, g:g + 1], None, ALU.mult, None, [pb], [pb])
            _ts(P, "dve", BD[dd][:, 256 + g * 64:256 + (g + 1) * 64], bbi[:], mrow[:, g:g + 1], None, ALU.mult, None, [pb], [pb])
        for ri in range(2):
            for st in range(2):
                for g in range(4):
                    _ts(P, "dve", CT[dd][:, ri * 2 + st, g * 16:(g + 1) * 16], Csm[:, dd, st, ri, :], msm[:, st, g:g + 1], (1.0 if ri == 0 else -1.0), ALU.mult, ALU.mult, [pb], [pb])
    P.a_cur = mark_prep
    TA = [[A(P, [128, 2, 129], F32) for _ in range(2)] for _ in range(2)]
    TW = [[A(P, [128, 2, 129], F32) for _ in range(2)] for _ in range(2)]
    PRE = [[A(P, [128, 256], F32) for _ in range(2)] for _ in range(2)]
    mark_t = P.a_cur
    for dd in range(2):
        P.a_cur = mark_t
        dt_ = A(P, [128, 2], F32); ar = A(P, [128, 2], F32); ai = A(P, [128, 2], F32); nar = A(P, [128, 2], F32)
        _act(P, dt_[:], p_sm[:, dd, :, 2], AF.Exp, [pb], [pb])
        _tt(P, "dve", ar[:], p_sm[:, dd, :, 0], dt_[:], ALU.mult, [pb], [pb])
        _tt(P, "dve", ai[:], p_sm[:, dd, :, 1], dt_[:], ALU.mult, [pb], [pb])
        _ts(P, "dve", nar[:], ar[:], -1.0, None, ALU.mult, None, [pb], [pb])
        mark_st = P.a_cur
        for st in range(2):
            P.a_cur = mark_st
            ph = A(P, [128, 129], F32); tmp = A(P, [128, 129], F32); sn = A(P, [128, 129], F32); cs = A(P, [128, 129], F32)
            mp = A(P, [128, 129], F32); mn = A(P, [128, 129], F32)
            _ts(P, "dve", ph[:], jrow[:], ai[:, st:st + 1], None, ALU.mult, None, [pb], [pb])
            range_reduce_sincos(P, ph[:], sn[:], cs[:], tmp[:], (lambda t: t), pb)
            _act(P, mp[:], jrow[:], AF.Exp, [pb], [pb], scale=ar[:, st:st + 1])
            _act(P, mn[:], jrow[:], AF.Exp, [pb], [pb], scale=nar[:, st:st + 1])
            _tt(P, "dve", TA[dd][0][:, st, :], mp[:], cs[:], ALU.mult, [pb], [pb])
            _tt(P, "dve", TA[dd][1][:, st, :], mp[:], sn[:], ALU.mult, [pb], [pb])
            _tt(P, "dve", TW[dd][0][:, st, :], mn[:], cs[:], ALU.mult, [pb], [pb])
            _stt(P, TW[dd][1][:, st, :], mn[:], -1.0, sn[:], ALU.mult, ALU.mult, [pb], [pb])
        P.a_cur = mark_t
        dtr = A(P, [128, 256], F32); arr = A(P, [128, 256], F32); air = A(P, [128, 256], F32)
        ph = A(P, [128, 256], F32); tmp = A(P, [128, 256], F32); sn = A(P, [128, 256], F32); cs = A(P, [128, 256], F32); mg = A(P, [128, 256], F32)
        _act(P, dtr[:], p_row[:, dd, 2, :], AF.Exp, [pb], [pb])
        _tt(P, "dve", arr[:], p_row[:, dd, 0, :], dtr[:], ALU.mult, [pb], [pb])
        _tt(P, "dve", air[:], p_row[:, dd, 1, :], dtr[:], ALU.mult, [pb], [pb])
        _ts(P, "dve", ph[:], air[:], jcol[:, 0:1], None, ALU.mult, None, [pb], [pb])
        range_reduce_sincos(P, ph[:], sn[:], cs[:], tmp[:], (lambda t: t), pb)
        _act(P, mg[:], arr[:], AF.Exp, [pb], [pb], scale=(njcol if dd == 0 else jcol)[:, 0:1])
        _tt(P, "dve", PRE[dd][0][:], mg[:], cs[:], ALU.mult, [pb], [pb])
        _stt(P, PRE[dd][1][:], mg[:], (-1.0 if dd == 0 else 1.0), sn[:], ALU.mult, ALU.mult, [pb], [pb])
        P.a_cur = mark_t
    barrier(P)
    P.a_cur = mark_t
    Xt = A(P, [128, NCH, 512], BF16); Xtb = [P.buf() for _ in range(NCH)]
    yacc = A(P, [64, NTOK], F32); yb = P.buf("yacc")
    E = A(P, [128, 4, NCH], F32); Eb = P.buf("E")
    H = [[A(P, [128, 2, NCH], F32) for _ in range(2)] for _ in range(2)]
    Hb = P.buf("H")
    cv = [A(P, [128, 2, NCH], F32) for _ in range(2)]
    pw = A(P, [128, 2, 8], F32)
    tq = [A(P, [128, 256], F32) for _ in range(8)]; tqb = [P.buf() for _ in range(8)]
    hs_ = [A(P, [128, 4, 128], BF16) for _ in range(2)]; hsb = [P.buf(), P.buf()]
    uq = [A(P, [128, 128], F32) for _ in range(16)]; uqb = [P.buf() for _ in range(16)]
    for dd in range(2):
        pos = pos_of(dd)
        for c in range(NCH):
            tau = 128 * c
            px = ps[c % 2]; pxb = psb[c % 2]
            _mm(P, px[:, 0:512], sT[:, tau:tau + 128], BD[dd][:, :], True, True, [sTb, pb], [pxb])
            i2 = (c % 2) * 4
            _tt(P, "dve", tq[i2][:, :], px[:, 0:256], PRE[dd][0][:, :], ALU.mult, [pxb, pb], [tqb[i2]])
            _tt(P, "dve", tq[i2 + 1][:, :], px[:, 256:512], PRE[dd][1][:, :], ALU.mult, [pxb, pb], [tqb[i2 + 1]])
            _tt(P, "dve", tq[i2 + 2][:, :], px[:, 0:256], PRE[dd][1][:, :], ALU.mult, [pxb, pb], [tqb[i2 + 2]])
            _tt(P, "dve", tq[i2 + 3][:, :], px[:, 256:512], PRE[dd][0][:, :], ALU.mult, [pxb, pb], [tqb[i2 + 3]])
            _tt(P, "pool", Xt[:, c, 0:256], tq[i2][:, :], tq[i2 + 1][:, :], ALU.subtract, [tqb[i2], tqb[i2 + 1]], [Xtb[c]])
            _tt(P, "pool", Xt[:, c, 256:512], tq[i2 + 2][:, :], tq[i2 + 3][:, :], ALU.add, [tqb[i2 + 2], tqb[i2 + 3]], [Xtb[c]])
            for tl in range(4):
                col = tl * NCH + pos[c]
                _mm(P, ps[6][:, col:col + 1], Xt[:, c, tl * 128:(tl + 1) * 128], ones_col[:, :], True, True, [Xtb[c], pb], [psb[6]])
        _cp(P, "dve", E[:], ps[6][:, 0:4 * NCH].rearrange("p (a b) -> p a b", b=NCH), [psb[6]], [Eb])
        H0r, H0i = H[0][0], H[0][1]
        if dd == 0:
            for st in range(2):
                a_r, a_i = TA[0][0][:, st, 127:128], TA[0][1][:, st, 127:128]
                _ts(P, "dve", uq[0][:, 0:NCH], E[:, 2 + st, :], a_i, None, ALU.mult, None, [Eb, pb], [uqb[0]])
                _stt(P, H0r[:, st, :], E[:, st, :], a_r, uq[0][:, 0:NCH], ALU.mult, ALU.subtract, [Eb, pb, uqb[0]], [Hb])
                _ts(P, "dve", uq[1][:, 0:NCH], E[:, st, :], a_i, None, ALU.mult, None, [Eb, pb], [uqb[1]])
                _stt(P, H0i[:, st, :], E[:, 2 + st, :], a_r, uq[1][:, 0:NCH], ALU.mult, ALU.add, [Eb, pb, uqb[1]], [Hb])
        else:
            _cp(P, "dve", H0r[:], E[:, 0:2, :], [Eb], [Hb])
            _cp(P, "dve", H0i[:], E[:, 2:4, :], [Eb], [Hb])
        _cp(P, "dve", pw[:, :, 0], TA[dd][0][:, :, 128], [pb], [Hb])
        _cp(P, "dve", pw[:, :, 1], TA[dd][1][:, :, 128], [pb], [Hb])
        cur = 0
        d = 1
        while d < NCH:
            _ts(P, "dve", pw[:, :, 2], pw[:, :, 1], -1.0, None, ALU.mult, None, [Hb], [Hb])
            o_, n_ = H[cur], H[1 - cur]
            for ri in range(2):
                _cp(P, "dve", n_[ri][:, :, 0:d], o_[ri][:, :, 0:d], [Hb], [Hb])
            for st in range(2):
                pr, pi, npi = pw[:, st, 0:1], pw[:, st, 1:2], pw[:, st, 2:3]
                m = NCH - d
                _stt(P, uq[0][:, 0:m], o_[0][:, st, 0:m], pr, o_[0][:, st, d:NCH], ALU.mult, ALU.add, [Hb], [uqb[0]])
                _stt(P, n_[0][:, st, d:NCH], o_[1][:, st, 0:m], npi, uq[0][:, 0:m], ALU.mult, ALU.add, [Hb, uqb[0]], [Hb])
                _stt(P, uq[1][:, 0:m], o_[1][:, st, 0:m], pr, o_[1][:, st, d:NCH], ALU.mult, ALU.add, [Hb], [uqb[1]])
                _stt(P, n_[1][:, st, d:NCH], o_[0][:, st, 0:m], pi, uq[1][:, 0:m], ALU.mult, ALU.add, [Hb, uqb[1]], [Hb])
            _tt(P, "dve", pw[:, :, 3], pw[:, :, 0], pw[:, :, 0], ALU.mult, [Hb], [Hb])
            _tt(P, "dve", pw[:, :, 4], pw[:, :, 1], pw[:, :, 1], ALU.mult, [Hb], [Hb])
            _tt(P, "dve", pw[:, :, 5], pw[:, :, 0], pw[:, :, 1], ALU.mult, [Hb], [Hb])
            _tt(P, "dve", pw[:, :, 0], pw[:, :, 3], pw[:, :, 4], ALU.subtract, [Hb], [Hb])
            _ts(P, "dve", pw[:, :, 1], pw[:, :, 5], 2.0, None, ALU.mult, None, [Hb], [Hb])
            cur = 1 - cur
            d *= 2
        Hf = H[cur]
        kidx = 1 if dd == 0 else 128
        P.op("dve", lambda h: h.memset(cv[0][:, :, 0:1], 0.0), writes=[Hb])
        P.op("dve", lambda h: h.memset(cv[1][:, :, 0:1], 0.0), writes=[Hb])
        for st in range(2):
            a_r, a_i = TA[dd][0][:, st, kidx:kidx + 1], TA[dd][1][:, st, kidx:kidx + 1]
            m = NCH - 1
            _ts(P, "dve", uq[0][:, 0:m], Hf[1][:, st, 0:m], a_i, None, ALU.mult, None, [Hb, pb], [uqb[0]])
            _stt(P, cv[0][:, st, 1:NCH], Hf[0][:, st, 0:m], a_r, uq[0][:, 0:m], ALU.mult, ALU.subtract, [Hb, pb, uqb[0]], [Hb])
            _ts(P, "dve", uq[1][:, 0:m], Hf[0][:, st, 0:m], a_i, None, ALU.mult, None, [Hb, pb], [uqb[1]])
            _stt(P, cv[1][:, st, 1:NCH], Hf[1][:, st, 0:m], a_r, uq[1][:, 0:m], ALU.mult, ALU.add, [Hb, pb, uqb[1]], [Hb])
        Tt = TA[dd] if dd == 0 else TW[dd]
        c_start = 0 if need_ctx_out else 2
        for c in range(c_start, NCH):
            pg = ps[2 + c % 2]; pgb = psb[2 + c % 2]
            for tl in range(4):
                _mm(P, pg[:, tl * 128:(tl + 1) * 128], Xt[:, c, tl * 128:(tl + 1) * 128], LT[:, dd, :], True, True, [Xtb[c], pb], [pgb])
            hh, hhb = hs_[c % 2], hsb[c % 2]
            pc = pos[c]
            for st in range(2):
                gr, gi = pg[:, st * 128:(st + 1) * 128], pg[:, (2 + st) * 128:(3 + st) * 128]
                c_r, c_i = cv[0][:, st, pc:pc + 1], cv[1][:, st, pc:pc + 1]
                Tr, Ti = Tt[0][:, st, 0:128], Tt[1][:, st, 0:128]
                u0 = ((c % 2) * 2 + st) * 4
                _stt(P, uq[u0][:, :], gr, c_r, Tr, ALU.add, ALU.mult, [pgb, Hb, pb], [uqb[u0]])
                _stt(P, uq[u0 + 1][:, :], gi, c_i, Ti, ALU.add, ALU.mult, [pgb, Hb, pb], [uqb[u0 + 1]])
                _stt(P, uq[u0 + 2][:, :], gi, c_i, Tr, ALU.add, ALU.mult, [pgb, Hb, pb], [uqb[u0 + 2]])
                _stt(P, uq[u0 + 3][:, :], gr, c_r, Ti, ALU.add, ALU.mult, [pgb, Hb, pb], [uqb[u0 + 3]])
                _tt(P, "pool", hh[:, st, :], uq[u0][:, :], uq[u0 + 1][:, :], ALU.subtract, [uqb[u0], uqb[u0 + 1]], [hhb])
                _tt(P, "pool", hh[:, 2 + st, :], uq[u0 + 2][:, :], uq[u0 + 3][:, :], ALU.add, [uqb[u0 + 2], uqb[u0 + 3]], [hhb])
            jj = c % 4
            py = ps[4 + (c // 4) % 2]; pyb = psb[4 + (c // 4) % 2]
            for tl in range(4):
                _mm(P, py[0:64, jj * 128:(jj + 1) * 128], CT[dd][:, tl, :], hh[:, tl, :], tl == 0, tl == 3, [pb, hhb], [pyb])
            if jj == 3 or c == NCH - 1:
                b0 = (c // 4) * 512
                wid = (jj + 1) * 128
                lo = 256 if ((not need_ctx_out) and c // 4 == 0) else 0
                if dd == 0:
                    _cp(P, "act", yacc[:, b0 + lo:b0 + wid], py[0:64, lo:wid], [pyb], [yb])
                else:
                    _tt(P, "dve", yacc[:, b0 + lo:b0 + wid], yacc[:, b0 + lo:b0 + wid], py[0:64, lo:wid], ALU.add, [pyb, yb], [yb])
    zT = A(P, [64, NTOK], BF16); zb = P.buf("zT")
    g1 = [A(P, [64, 512], F32) for _ in range(2)]; g1b = [P.buf(), P.buf()]
    g2 = [A(P, [64, 512], F32) for _ in range(2)]; g2b = [P.buf(), P.buf()]
    lo_all = 0 if need_ctx_out else 256
    if not need_ctx_out:
        P.op("pool", lambda h: h.memset(zT[:, 0:256], 0.0), writes=[zb])
    nblk = (NTOK + 511) // 512
    for bi in range(nblk):
        t0 = max(bi * 512, lo_all)
        t1_ = min((bi + 1) * 512, NTOK)
        n = t1_ - t0
        i2 = bi % 2
        y = g1[i2]; w = g2[i2]
        _stt(P, y[:, 0:n], sT[:, t0:t1_], dvec[:, 0:1], yacc[:, t0:t1_], ALU.mult, ALU.add, [sTb, pb, yb], [g1b[i2]])
        _tt(P, "dve", w[:, 0:n], y[:, 0:n], y[:, 0:n], ALU.mult, [g1b[i2]], [g2b[i2]])
        _ts(P, "dve", w[:, 0:n], w[:, 0:n], 0.044715, 1.0, ALU.mult, ALU.add, [g2b[i2]], [g2b[i2]])
        _tt(P, "dve", w[:, 0:n], w[:, 0:n], y[:, 0:n], ALU.mult, [g2b[i2], g1b[i2]], [g2b[i2]])
        _act(P, w[:, 0:n], w[:, 0:n], AF.Sigmoid, [g2b[i2]], [g2b[i2]], scale=1.5957691216057308)
        _tt(P, "dve", zT[:, t0:t1_], w[:, 0:n], y[:, 0:n], ALU.mult, [g2b[i2], g1b[i2]], [zb])
    outs.append(_ld(P, "sp", out[1, :, :], zT[:, :], [P.buf()], reads=[zb]))


import ml_dtypes

BF = ml_dtypes.bfloat16
NTOK = 8448
f32 = np.float32


def fm(a):
    return np.ascontiguousarray(a.T.reshape(8, 128, a.shape[0]))


def prep_common(inp, layer, b):
    cond = np.stack([inp['c'][b], inp['c_ctx']], 0)
    condT = np.ascontiguousarray(cond.reshape(2, 8, 128).transpose(2, 1, 0))
    bm = inp['b_mod'][layer].reshape(48, 128).T
    b_modT = np.ascontiguousarray(np.stack([bm, bm], -1))
    ng = inp['norm_g'][layer].reshape(4, 8, 128).transpose(2, 0, 1)
    norm_gT = np.ascontiguousarray(np.stack([ng, ng], -1))
    return dict(condT=condT, b_modT=b_modT, norm_gT=norm_gT, w_mod=inp['w_mod'][layer])


_const_cache = {}


def consts():
    if _const_cache:
        return _const_cache
    C = _const_cache
    c = np.arange(64)
    ang = 2 * np.pi * (np.outer(c, c) % 64) / 64
    C['f_CS'] = np.concatenate([np.cos(ang), -np.sin(ang)], 1).astype(BF)
    ca, sa = np.cos(ang), np.sin(ang)
    C['f_RP'] = np.concatenate([ca, -sa], 1).astype(BF)
    C['f_RQ'] = np.concatenate([sa, ca], 1).astype(BF)
    m2 = np.arange(128)[:, None, None]
    n1 = np.arange(64)[None, :, None]
    n2 = np.arange(128)[None, None, :]
    be = 2 * np.pi * ((m2 * (n1 + 64 * n2)) % 8192) / 8192
    nrm = 1 / np.sqrt(64 * 8192)
    C['f_CB'] = (np.cos(be) * nrm).astype(BF)
    C['f_SB'] = (np.sin(be) * nrm).astype(BF)
    m = np.arange(256)
    a256 = 2 * np.pi * (np.outer(m, m) % 256) / 256
    nrm2 = 1 / np.sqrt(64 * 256)
    C['f_C256'] = np.ascontiguousarray((np.cos(a256) * nrm2).reshape(2, 128, 256).transpose(1, 0, 2)).astype(BF)
    C['f_S256'] = np.ascontiguousarray((np.sin(a256) * nrm2).reshape(2, 128, 256).transpose(1, 0, 2)).astype(BF)
    t = np.arange(8192)
    row = (t // 64).astype(f32)
    col = (t % 64).astype(f32)
    inv = (1.0 / (f32(10000.0) ** (np.arange(16, dtype=f32) / f32(16)))).astype(f32)
    angr = np.concatenate([row[:, None] * inv, col[:, None] * inv], -1).astype(f32)
    cs, sn = np.cos(angr).astype(f32), np.sin(angr).astype(f32)
    cos64 = np.concatenate([cs, cs], 1)
    sin64 = np.concatenate([-sn, sn], 1)
    C['r_cosF'] = np.ascontiguousarray(cos64.T)
    C['r_sinF'] = np.ascontiguousarray(sin64.T)
    C['r_cosT'] = np.ascontiguousarray(cos64.reshape(64, 128, 64).transpose(1, 0, 2))
    C['r_sinT'] = np.ascontiguousarray(sin64.reshape(64, 128, 64).transpose(1, 0, 2))
    j = np.arange(128, dtype=f32)
    C['r_jcol'] = np.stack([127 - j, j], 1).astype(f32)
    ii = np.arange(128)
    dist = np.abs(ii[None, :] - ii[:, None]).astype(f32)
    C['r_dist'] = dist
    C['r_mask'] = np.stack([(ii[None, :] >= ii[:, None]), (ii[:, None] >= ii[None, :])], 0).astype(f32)
    C['r_irow'] = np.stack([np.tile(j + 1, (64, 1)), np.tile(128 - j, (64, 1))], 0).astype(f32)
    C['s_jrow'] = np.tile(np.arange(129, dtype=f32), (128, 1))
    C['s_jcol'] = np.arange(128, dtype=f32)[:, None].copy()
    C['s_LT'] = np.stack([(ii[None, :] >= ii[:, None]), (ii[:, None] >= ii[None, :])], 0).astype(BF)
    g_of_row = np.arange(64) // 16
    C['s_mrow'] = (g_of_row[:, None] == np.arange(4)[None, :]).astype(f32)
    g_of_st = (np.arange(128)[:, None] // 64) + 2 * np.arange(2)[None, :]
    C['s_msm'] = (g_of_st[:, :, None] == np.arange(4)[None, None, :]).astype(f32)
    C['ident_bf'] = np.eye(128).astype(BF)
    C['ident_f'] = np.eye(128).astype(f32)
    def start(r):
        return int(np.clip(r - 4, 0, 120))
    qc = np.arange(64)
    cst = np.clip(qc - 8, 0, 48)
    kc = np.arange(64)
    colok = (kc[None, :] >= cst[:, None]) & (kc[None, :] < cst[:, None] + 16)
    types = [(0, 0, 8), (2, 0, 8), (10, 6, 9), (124, 120, 8), (126, 120, 8)]
    mask = np.full((5, 128, 832), -30000.0, f32)
    drs = np.zeros((5, 2, 9), np.int64)
    for ti, (r0, R0, nr) in enumerate(types):
        for qr in range(2):
            r = r0 + qr
            for i in range(9):
                kr = R0 + i
                dr = int(np.clip(kr - r + 7, 0, 14))
                drs[ti, qr, i] = dr
                if i < nr and start(r) <= kr < start(r) + 8:
                    blk = np.where(colok, 0.0, -30000.0)
                    mask[ti, qr * 64:(qr + 1) * 64, i * 64:(i + 1) * 64] = blk
        mask[ti, :, 576:] = 0.0
    C['n_mask'] = mask
    C['n_drs'] = drs
    C['n_types'] = types
    return C


def prep_M(inp, layer, c, xT_full):
    b, q = c // 4, c % 4
    C = consts()
    m = prep_common(inp, layer, b)
    m['xT'] = xT_full
    w_in = inp['w_in'][layer]
    o = q * 64
    sw = np.r_[32:64, 0:32]
    cols_fm = np.concatenate([np.arange(0 + o, 0 + o + 64), np.arange(256 + o, 256 + o + 64), np.arange(512 + o, 512 + o + 64),
                              np.arange(768 + o, 768 + o + 64), 512 + o + sw, 768 + o + sw, np.arange(1280 + o, 1280 + o + 64),
                              np.arange(1536 + o, 1536 + o + 64), np.arange(1792 + o, 1792 + o + 64)])
    cols_tm = np.concatenate([np.arange(768 + o, 768 + o + 64), 768 + o + sw, np.arange(1024 + o, 1024 + o + 64), np.arange(2048 + o, 2048 + o + 64)])
    m['w_fm'] = np.ascontiguousarray(w_in[:, cols_fm])
    m['w_tm'] = np.ascontiguousarray(w_in[:, cols_tm])
    for k in ('f_CS', 'f_RP', 'f_RQ', 'f_CB', 'f_SB', 'f_C256', 'f_S256', 'r_cosF', 'r_sinF', 'r_cosT', 'r_sinT', 'r_jcol', 'r_dist', 'r_mask', 'r_irow',
              's_jrow', 's_jcol', 's_LT', 's_mrow', 's_msm', 'ident_bf', 'ident_f', 'n_mask'):
        m[k] = C[k]
    gs = slice(4 * q, 4 * q + 4)
    L = layer
    are, aim = inp['s5_a_re'][L][:, gs], inp['s5_a_im'][L][:, gs]
    ldt = inp['s5_log_dt'][L][:, gs]
    def sm(a):
        return np.ascontiguousarray(a.reshape(2, 2, 128).transpose(2, 0, 1))
    ldt_b = np.broadcast_to(ldt[:, :, None], (2, 4, 64))
    m['s_sm'] = np.ascontiguousarray(np.stack([sm(are), sm(aim), sm(ldt_b)], -1))
    row = np.stack([are.reshape(2, 256), aim.reshape(2, 256), ldt_b.reshape(2, 256)], -1)
    m['s_row'] = np.ascontiguousarray(np.broadcast_to(row[None].transpose(0, 1, 3, 2), (128, 2, 3, 256)))
    hs = np.stack([are, aim, ldt_b], 2)
    hs = np.broadcast_to(hs[:, :, None], (2, 4, 16, 3, 64))
    m['s_hs'] = np.ascontiguousarray(hs.transpose(1, 2, 0, 3, 4).reshape(64, 2, 3, 64))
    bre, bim = inp['s5_b_re'][L][:, gs], inp['s5_b_im'][L][:, gs]
    B = np.stack([bre, bim], 2)
    m['s_B'] = np.ascontiguousarray(B.transpose(1, 4, 0, 2, 3).reshape(64, 2, 2, 64))
    cre, cim = inp['s5_c_re'][L][:, gs], inp['s5_c_im'][L][:, gs]
    Cc = np.stack([cre, cim], 2)
    Cc = Cc.transpose(1, 4, 0, 2, 3)
    Cc = Cc.reshape(2, 2, 64, 2, 2, 16).transpose(1, 2, 3, 0, 4, 5).reshape(128, 2, 2, 2, 16)
    m['s_C'] = np.ascontiguousarray(Cc)
    m['s_d'] = np.ascontiguousarray(inp['s5_d'][L][256 * 0 + 64 * q:64 * q + 64][:, None])
    rd = inp['ret_decay'][L][:, q]
    m['r_dec'] = np.ascontiguousarray(np.broadcast_to(rd[None, :], (128, 2))).astype(f32)
    m['r_gn'] = np.ascontiguousarray(inp['ret_gn'][L][64 * q:64 * q + 64][:, None])
    rpb = inp['na_rpb'][L][q]
    dc = np.clip(np.arange(64)[None, :] - np.arange(64)[:, None], -15, 15) + 15
    m['n_toep'] = np.ascontiguousarray(rpb[:, dc])
    return m


def _prep_F(inp, layer, c, xa, bra_bf, moe_a=False):
    b = c // 4
    m = prep_common(inp, layer, b)
    m.update(xT=fm(xa), brT=bra_bf, w_in=inp['w_in'][layer], w_br=inp['w_branch'][layer].reshape(1024, 1024), w_o=inp['w_out'][layer],
             w_glu=inp['s5_w_glu'][layer], b_gluT=np.ascontiguousarray(inp['s5_b_glu'][layer].reshape(2, 128).T))
    i = layer // 2
    if layer % 2 == 0:
        m.update(w_g=inp['ffn_w_gate'][i:i + 1], w_u=inp['ffn_w_up'][i:i + 1], w_d=inp['ffn_w_down'][i:i + 1])
    else:
        sel = np.zeros((8, 8, 128), np.float32)
        for e in range(8):
            sel[e, e, :] = 1
        m.update(w_r=inp['moe_w_router'][i], b_r=np.ascontiguousarray(np.broadcast_to(inp['moe_b_router'][i][None], (128, 8))),
                 ident=np.eye(128, dtype=np.float32), sel=sel)
    return m


def kernel(**inputs):
    inp = {k: np.asarray(v) for k, v in inputs.items()}
    NCORE = 8
    cores = list(range(NCORE))
    x = inp['x']
    ctx = inp['ctx']
    for layer in range(2):
        last = (layer == 1)
        ncM = build_M(not last)
        xfull = [fm(np.concatenate([ctx[b], x[b]], 0)) for b in range(2)]
        maps = [prep_M(inp, layer, c, xfull[c // 4]) for c in cores]
        resM = run_bass_kernel_spmd(ncM, maps, core_ids=cores).results
        del maps
        br_full = []
        for b in range(2):
            o = np.stack([np.asarray(resM[4 * b + q]['brT_out']) for q in range(4)], 1)
            br_full.append(o.reshape(1024, NTOK))
        del resM
        if not last:
            blocksA = [(i * 256, 256, 0) for i in range(8)] + [(2048, 64, 1)]
            blocksB = [(i * 512, 512, 0) for i in range(4)] + [(2048, 64, 1)]
            ncF = build_F(blocksA, blocksB, 1, 2816, False)
        else:
            blocksA = [(i * 256, 256, 0) for i in range(8)]
            blocksB = [(i * 512, 512, 0) for i in range(4)]
            ncF = build_F(blocksA, blocksB, 8, 3584, True, mode='moe_a')
        maps = []
        for c in cores:
            b, q = c // 4, c % 4
            lat = slice(256 + q * 2048, 256 + (q + 1) * 2048)
            if not last:
                xa = np.concatenate([x[b, q * 2048:(q + 1) * 2048], ctx[b, q * 64:(q + 1) * 64]], 0)
                bra = np.concatenate([br_full[b][:, lat], br_full[b][:, q * 64:(q + 1) * 64]], 1)
            else:
                xa = x[b, q * 2048:(q + 1) * 2048]
                bra = br_full[b][:, lat]
            bra = np.ascontiguousarray(bra.reshape(8, 128, bra.shape[1]))
            maps.append(_prep_F(inp, layer, c, xa, bra))
        resF = run_bass_kernel_spmd(ncF, maps, core_ids=cores).results
        del maps
        if not last:
            xn = np.empty_like(x)
            cn = np.empty_like(ctx)
            for c in cores:
                b, q = c // 4, c % 4
                o = np.asarray(resF[c]['xo']).reshape(1024, -1).T
                xn[b, q * 2048:(q + 1) * 2048] = o[:2048]
                cn[b, q * 64:(q + 1) * 64] = o[2048:]
            x, ctx = xn, cn
            continue
        i = layer // 2
        h2_all = np.concatenate([np.asarray(resF[c]['h2o']) for c in cores], 2)
        cb_all = np.concatenate([np.asarray(resF[c]['cbo']) for c in cores], 1)
        sel = np.concatenate([np.asarray(resF[c]['mko']) for c in cores], 1).astype(bool)
        idx = [np.flatnonzero(sel[e]) for e in cores]
        nb = max(1, -(-max(len(t) for t in idx) // 512))
        ng = -(-nb // 4)
        groups = tuple(nb // ng + (1 if g < nb % ng else 0) for g in range(ng))
        C = 512 * nb
        ncE = build_E(groups)
        maps = []
        for e in cores:
            n_e = len(idx[e])
            ii = np.zeros(C, np.int64)
            ii[:n_e] = idx[e]
            cbe = np.zeros(C, cb_all.dtype)
            cbe[:n_e] = cb_all[e, idx[e]]
            maps.append(dict(h2=np.ascontiguousarray(h2_all[:, :, ii]), cbe=np.ascontiguousarray(np.broadcast_to(cbe[None, :], (128, C))),
                             w_g=inp['moe_w_gate'][i][e], w_u=inp['moe_w_up'][i][e], w_d=inp['moe_w_down'][i][e]))
        resE = run_bass_kernel_spmd(ncE, maps, core_ids=cores).results
        del maps
        slot = np.cumsum(sel, axis=0) - sel
        nslot = max(1, int(sel.sum(0).max()))
        yp_all = np.zeros((nslot, 8, 128, sel.shape[1]), np.float32)
        for e in cores:
            ye = np.asarray(resE[e]['ye'])
            t = idx[e]
            sv = slot[e, t]
            for k in range(nslot):
                mk = sv == k
                yp_all[k][:, :, t[mk]] = ye[:, :, np.flatnonzero(mk)]
        ncC = build_Fc(nexp=nslot)
        maps = []
        for c in cores:
            m = prep_common(inp, layer, c // 4)
            m['xm'] = np.asarray(resF[c]['xo'])
            m['yp'] = np.ascontiguousarray(yp_all[:, :, :, c * 2048:(c + 1) * 2048])
            maps.append(m)
        resC = run_bass_kernel_spmd(ncC, maps, core_ids=cores).results
        xn = np.empty_like(x)
        for c in cores:
            b, q = c // 4, c % 4
            xn[b, q * 2048:(q + 1) * 2048] = np.asarray(resC[c]['xo']).reshape(1024, -1).T
        x = xn
    return x.astype(np.float32)
```

```python
import numpy as np
from contextlib import ExitStack
import concourse.bass as bass
import concourse.mybir as mybir
from concourse.bass_utils import run_bass_kernel_spmd

F32 = mybir.dt.float32
BF16 = mybir.dt.bfloat16
I32 = mybir.dt.int32
ALU = mybir.AluOpType
AF = mybir.ActivationFunctionType
AX = mybir.AxisListType

ENGS = ("pe", "act", "dve", "pool", "sp")
NDSEM = 12


class Buf:
    __slots__ = ("name", "lw", "rd", "psum")

    def __init__(self, name="", psum=False):
        self.name = name
        self.psum = psum
        self.lw = None
        self.rd = {}


class Prog:
    def __init__(self, nc):
        self.nc = nc
        self.stack = ExitStack()
        self.ops = {e: [] for e in ENGS}
        self.cnt = {e: 0 for e in ENGS}
        self.seen = {e: {} for e in ENGS}
        self.sems = {}
        for e in ENGS:
            self.sems[e] = self.stack.enter_context(nc.semaphore("s_" + e))
        self.dsem_use = {}
        self.dq_next = {}
        for q in ("sp", "act", "pool"):
            for i in range(NDSEM):
                k = "d_%s%d" % (q, i)
                self.sems[k] = self.stack.enter_context(nc.semaphore(k))
                self.dsem_use[k] = 0
            self.dq_next[q] = 0
        self.nbuf = 0

    def sb(self, name, shape, dt):
        return self.stack.enter_context(self.nc.sbuf_tensor(name, list(shape), dt))

    def ps(self, name, shape, dt=F32):
        return self.stack.enter_context(self.nc.psum_tensor(name, list(shape), dt))

    def buf(self, name=None, psum=None):
        self.nbuf += 1
        name = name or "b%d" % self.nbuf
        if psum is None:
            psum = name.startswith("ps")
        return Buf(name, psum)

    def _deps(self, eng, reads, writes, is_dma):
        w = {}

        def add(t):
            if t is None:
                return
            k, v = t
            if w.get(k, 0) < v:
                w[k] = v
        for b in reads:
            add(b.lw)
            if b.psum:
                for k, v in b.rd.items():
                    if k != eng:
                        add((k, v))
        for b in writes:
            if b.lw is not None:
                if not (eng == "pe" and b.lw[0] == "pe" and not is_dma):
                    add(b.lw)
            for k, v in b.rd.items():
                if k == eng and not is_dma and eng != "pool":
                    continue
                add((k, v))
        seen = self.seen[eng]
        out = []
        for k, v in w.items():
            if seen.get(k, 0) < v:
                seen[k] = v
                out.append((k, v))
        return out

    def _commit(self, ticket, reads, writes):
        for b in writes:
            b.lw = ticket
            b.rd = {}
        for b in reads:
            k, v = ticket
            if b.rd.get(k, 0) < v:
                b.rd[k] = v

    def op(self, eng, fn, reads=(), writes=()):
        waits = self._deps(eng, reads, writes, False)
        self.cnt[eng] += 1
        ticket = (eng, self.cnt[eng])
        self.ops[eng].append((waits, fn, (eng, 1)))
        self._commit(ticket, reads, writes)
        return ticket

    def dma(self, q, fn, reads=(), writes=()):
        i = self.dq_next[q]
        self.dq_next[q] = (i + 1) % NDSEM
        k = "d_%s%d" % (q, i)
        waits = self._deps(q, reads, writes, True)
        prev = self.dsem_use[k]
        if prev > 0 and self.seen[q].get(k, 0) < 16 * prev:
            self.seen[q][k] = 16 * prev
            waits.append((k, 16 * prev))
        self.dsem_use[k] = prev + 1
        ticket = (k, 16 * (prev + 1))
        self.ops[q].append((waits, fn, (k, 16)))
        self._commit(ticket, reads, writes)
        return ticket

    def finish_wait(self, eng, tickets):
        waits = []
        for k, v in tickets:
            if self.seen[eng].get(k, 0) < v:
                self.seen[eng][k] = v
                waits.append((k, v))
        self.ops[eng].append((waits, None, None))

    def emit(self):
        nc = self.nc
        sems = self.sems
        ops = self.ops

        def replay(e, h):
            for waits, fn, inc in ops[e]:
                for k, v in waits:
                    h.wait_ge(sems[k], v)
                if fn is not None:
                    ins = fn(h)
                    ins.then_inc(sems[inc[0]], inc[1])

        with nc.Block() as block:
            @block.sync
            def _(h):
                replay("sp", h)

            @block.scalar
            def _(h):
                replay("act", h)

            @block.vector
            def _(h):
                replay("dve", h)

            @block.gpsimd
            def _(h):
                replay("pool", h)

            @block.tensor
            def _(h):
                replay("pe", h)
        self.stack.close()


def _mm(P, out, lhsT, rhs, start, stop, reads, writes):
    return P.op("pe", lambda h: h.matmul(out, lhsT=lhsT, rhs=rhs, start=start, stop=stop), reads=reads, writes=writes)


def _tr(P, out, in_, ident, reads, writes):
    return P.op("pe", lambda h: h.transpose(out, in_, ident), reads=reads, writes=writes)


def _act(P, out, in_, func, reads, writes, scale=None, bias=None):
    kw = {}
    if scale is not None:
        kw["scale"] = scale
    if bias is not None:
        kw["bias"] = bias
    return P.op("act", lambda h: h.activation(out=out, in_=in_, func=func, **kw), reads=reads, writes=writes)


def _tt(P, eng, out, in0, in1, op, reads, writes):
    return P.op(eng, lambda h: h.tensor_tensor(out=out, in0=in0, in1=in1, op=op), reads=reads, writes=writes)


def _ts(P, eng, out, in0, s1, s2, op0, op1, reads, writes):
    if op1 is None:
        return P.op(eng, lambda h: h.tensor_scalar(out=out, in0=in0, scalar1=s1, scalar2=None, op0=op0), reads=reads, writes=writes)
    return P.op(eng, lambda h: h.tensor_scalar(out=out, in0=in0, scalar1=s1, scalar2=s2, op0=op0, op1=op1), reads=reads, writes=writes)


def _stt(P, out, in0, scalar, in1, op0, op1, reads, writes):
    return P.op("dve", lambda h: h.scalar_tensor_tensor(out=out, in0=in0, scalar=scalar, in1=in1, op0=op0, op1=op1), reads=reads, writes=writes)


def _cp(P, eng, out, in_, reads, writes):
    if eng == "act":
        return P.op("act", lambda h: h.activation(out=out, in_=in_, func=AF.Copy), reads=reads, writes=writes)
    return P.op(eng, lambda h: h.tensor_copy(out=out, in_=in_), reads=reads, writes=writes)


def _ld(P, q, out, in_, writes, reads=()):
    return P.dma(q, lambda h: h.dma_start(out=out, in_=in_), reads=reads, writes=writes)


D = 1024
KC = 8
EPS = 1e-6


def arena_init(P, nbytes=206 * 1024):
    lo, hi = P.nc.bump_sbuf(nbytes)
    P.a_lo, P.a_hi, P.a_cur = lo, hi, lo
    P.a_n = 0


def A(P, shape, dt):
    nb = int(np.prod(shape[1:])) * (4 if dt in (F32, I32) else 2)
    off = (P.a_cur + 31) // 32 * 32
    assert off + nb <= P.a_hi, ("SBUF arena overflow", off + nb - P.a_lo)
    P.a_cur = off + nb
    P.a_n += 1
    return P.nc.alloc_sbuf_tensor_at("t%d" % P.a_n, list(shape), dt, offset=off)


def barrier(P, queues=None):
    tick = [(e, P.cnt[e]) for e in ENGS if P.cnt[e] > 0]
    tick += [(k, 16 * v) for k, v in P.dsem_use.items() if v > 0 and (queues is None or any(k.startswith("d_" + q) for q in queues))]
    for e in ENGS:
        P.finish_wait(e, tick)


def rms_rstd(P, src, srcb, n, sq, sqb, ss_ps, ssb, rstd, rstdb, ones):
    P.op("act", lambda h: h.activation(out=sq[:, :, 0:n], in_=src[:, :, 0:n], func=AF.Square), reads=[srcb], writes=[sqb])
    for k in range(KC):
        P.op("pe", lambda h, k=k: h.matmul(ss_ps[:, 0:n], lhsT=ones[:], rhs=sq[:, k, 0:n], start=(k == 0), stop=(k == KC - 1)),
             reads=[sqb], writes=[ssb])
    P.op("act", lambda h: h.activation(out=rstd[:, 0:n], in_=ss_ps[:, 0:n], func=AF.Ln, scale=1.0 / D, bias=P.eps_t[:, 0:1]), reads=[ssb], writes=[rstdb])
    P.op("act", lambda h: h.activation(out=rstd[:, 0:n], in_=rstd[:, 0:n], func=AF.Exp, scale=-0.5), reads=[rstdb], writes=[rstdb])


def norm_mod(P, src, srcb, n, rstd, rstdb, gm, sh, r, dst, dstb, tmp, tmpb, dst_off=0, dst32=None, dst32b=None):
    for k in range(KC):
        tb = tmpb[k % len(tmp)]
        tt = tmp[k % len(tmp)]
        P.op("dve", lambda h, k=k, tt=tt: h.tensor_tensor(out=tt[:, 0:n], in0=src[:, k, 0:n], in1=rstd[:, 0:n], op=ALU.mult),
             reads=[srcb, rstdb], writes=[tb])
        P.op("act", lambda h, k=k, tt=tt: h.activation(out=dst[:, k, dst_off:dst_off + n], in_=tt[:, 0:n], func=AF.Identity,
                                                     scale=gm[:, k, r:r + 1], bias=sh[:, k, r:r + 1]),
             reads=[tb, P.modb], writes=[dstb])
        if dst32 is not None:
            P.op("pool", lambda h, k=k, tt=tt: h.tensor_scalar(out=dst32[:, k, 0:n], in0=tt[:, 0:n], scalar1=gm[:, k, r:r + 1],
                                                             scalar2=sh[:, k, r:r + 1], op0=ALU.mult, op1=ALU.add),
                 reads=[tb, P.modb], writes=[dst32b])


def compute_mod(P, dr, which, mod_ps, modpb, light=False):
    nc = P.nc
    cs = A(P, [128, KC, 2], F32)
    csb = P.buf()
    P.dma("sp", lambda h: h.dma_start(out=cs[:], in_=dr["condT"][:, :, :]), writes=[csb])
    sig = A(P, [128, KC, 2], F32)
    P.op("act", lambda h: h.activation(out=sig[:], in_=cs[:], func=AF.Sigmoid), reads=[csb], writes=[csb])
    P.op("dve", lambda h: h.tensor_tensor(out=cs[:], in0=cs[:], in1=sig[:], op=ALU.mult), reads=[csb], writes=[csb])
    modT = A(P, [128, 48, 2], F32)
    P.modT = modT
    P.modb = P.buf("mod")
    bm = A(P, [128, 48, 2], F32)
    bmb = P.buf()
    P.dma("sp", lambda h: h.dma_start(out=bm[:], in_=dr["b_modT"][:, :, :]), writes=[bmb])
    ng = A(P, [128, 4, KC, 2], F32)
    P.ng = ng
    P.dma("sp", lambda h: h.dma_start(out=ng[:], in_=dr["norm_gT"][:, :, :, :]), writes=[P.modb])
    mark = P.a_cur
    wm = [A(P, [128, KC, 1024], F32) for _ in range(2)]
    wmb = [P.buf(), P.buf()]
    wsrc = dr["w_mod"].rearrange("(k p) f -> p k f", p=128)
    for i, j in enumerate(which):
        w = wm[i % 2]
        wb = wmb[i % 2]
        for k2 in range(2):
            P.dma("sp", lambda h, w=w, j=j, k2=k2: h.dma_start(out=w[:, 4 * k2:4 * k2 + 4, :], in_=wsrc[:, 4 * k2:4 * k2 + 4, j * 1024:(j + 1) * 1024]), writes=[wb])
        for fc in range(8):
            for k in range(KC):
                P.op("pe", lambda h, w=w, j=j, fc=fc, k=k: h.matmul(mod_ps[:, j * 8 + fc, :], lhsT=w[:, k, fc * 128:(fc + 1) * 128], rhs=cs[:, k, :],
                                                                   start=(k == 0), stop=(k == KC - 1)), reads=[wb, csb], writes=[modpb])
    for j in which:
        P.op("dve", lambda h, j=j: h.tensor_tensor(out=modT[:, j * 8:(j + 1) * 8, :], in0=mod_ps[:, j * 8:(j + 1) * 8, :], in1=bm[:, j * 8:(j + 1) * 8, :], op=ALU.add),
             reads=[modpb, bmb], writes=[P.modb])
    barrier(P, ("sp",) if light else None)
    P.a_cur = mark


def mod_derived(P, jsc, jg, gi_norm, gi_gate):
    gm = A(P, [128, KC, 2], F32)
    gg = A(P, [128, KC, 2], F32)
    modT, ng = P.modT, P.ng
    P.op("dve", lambda h: h.scalar_tensor_tensor(out=gm[:], in0=modT[:, jsc * 8:(jsc + 1) * 8, :], scalar=1.0, in1=ng[:, gi_norm, :, :],
                                                 op0=ALU.add, op1=ALU.mult), reads=[P.modb], writes=[P.modb])
    if jg is not None:
        P.op("dve", lambda h: h.tensor_tensor(out=gg[:], in0=modT[:, jg * 8:(jg + 1) * 8, :], in1=ng[:, gi_gate, :, :], op=ALU.mult),
             reads=[P.modb], writes=[P.modb])
    return gm, gg


def build_F(blocks, blocksB, n_exp, dff, moe, DBG=False, mode='full'):
    TT = sum(b[1] for b in blocks)
    nc = bass.Bass("TRN2", target_bir_lowering=False)
    dr = {}

    def din(name, shape, dt=F32):
        dr[name] = nc.dram_tensor(name, list(shape), dt, kind="ExternalInput").ap()
    din("xT", [KC, 128, TT])
    din("brT", [KC, 128, TT], BF16)
    din("condT", [128, KC, 2])
    din("w_mod", [D, 6 * D])
    din("b_modT", [128, 48, 2])
    din("norm_gT", [128, 4, KC, 2])
    din("w_in", [D, 6400])
    din("w_br", [KC * 128, D])
    din("w_o", [D, D])
    din("w_glu", [256, 256])
    din("b_gluT", [128, 2])
    if mode == 'full':
        din("w_g", [n_exp, D, dff])
        din("w_u", [n_exp, D, dff])
        din("w_d", [n_exp, dff, D])
    if moe:
        din("w_r", [D, 8])
        din("b_r", [128, 8])
        din("ident", [128, 128])
        din("sel", [8, 8, 128])
    if mode == 'moe_a':
        h2o = nc.dram_tensor("h2o", [KC, 128, TT], BF16, kind="ExternalOutput").ap().rearrange("k p t -> p k t")
        cbo = nc.dram_tensor("cbo", [8, TT], BF16, kind="ExternalOutput").ap()
        mko = nc.dram_tensor("mko", [8, TT], BF16, kind="ExternalOutput").ap()
    xo = nc.dram_tensor("xo", [KC, 128, TT], F32, kind="ExternalOutput").ap()
    xoT = xo.rearrange("k p t -> p k t")
    if DBG: dbg_mod = nc.dram_tensor("dbg_mod", [128, 48, 2], F32, kind="ExternalOutput").ap()
    if DBG: dbg_xm = nc.dram_tensor("dbg_xm", [KC, 128, TT], F32, kind="ExternalOutput").ap().rearrange("k p t -> p k t")
    if DBG: dbg_h = nc.dram_tensor("dbg_h", [KC, 128, TT], BF16, kind="ExternalOutput").ap().rearrange("k p t -> p k t")
    if DBG: dbg_z = nc.dram_tensor("dbg_z", [KC, 128, TT], F32, kind="ExternalOutput").ap().rearrange("k p t -> p k t")
    if DBG: dbg_r = nc.dram_tensor("dbg_r", [128, TT], F32, kind="ExternalOutput").ap()
    if DBG: dbg_sq = nc.dram_tensor("dbg_sq", [KC, 128, TT], BF16, kind="ExternalOutput").ap().rearrange("k p t -> p k t")
    if DBG: dbg_ss = nc.dram_tensor("dbg_ss", [128, TT], F32, kind="ExternalOutput").ap()
    sscp = A(P, [128, 256], F32) if False else None
    if DBG: dbg_y = nc.dram_tensor("dbg_y", [KC, 128, TT], BF16, kind="ExternalOutput").ap().rearrange("k p t -> p k t")
    xT = dr["xT"].rearrange("k p t -> p k t")
    brT = dr["brT"].rearrange("k p t -> p k t")

    P = Prog(nc)
    arena_init(P)
    ps = [P.ps("ps%d" % i, [128, 512], F32) for i in range(8)]
    psb = [P.buf("ps%d" % i) for i in range(8)]
    ones = A(P, [128, 128], BF16)
    onesb = P.buf()
    P.op("dve", lambda h: h.memset(ones[:], 1.0), writes=[onesb])
    P.eps_t = A(P, [128, 1], F32)
    P.op("dve", lambda h: h.memset(P.eps_t[:], EPS), writes=[onesb])

    w_lo = P.a_cur
    wgt = A(P, [128, KC, 4096], BF16)
    wbr = A(P, [128, KC, D], BF16)
    wo = A(P, [128, KC, D], BF16)
    wAb = P.buf("wA")
    wbufs = []

    def _wb():
        wbufs.append(P.buf())
        return wbufs[-1]
    w_in_v = dr["w_in"].rearrange("(k p) c -> p k c", p=128)
    for k in range(KC):
        for c4 in range(2):
            P.dma("pool", lambda h, k=k, c4=c4: h.dma_start(out=wgt[:, k, c4 * 2048:(c4 + 1) * 2048], in_=w_in_v[:, k, 2304 + c4 * 2048:2304 + (c4 + 1) * 2048]), writes=[_wb()])
    P.dma("pool", lambda h: h.dma_start(out=wbr[:, 0:4, :], in_=dr["w_br"].rearrange("(k p) c -> p k c", p=128)[:, 0:4, :]), writes=[_wb()])
    P.dma("pool", lambda h: h.dma_start(out=wbr[:, 4:8, :], in_=dr["w_br"].rearrange("(k p) c -> p k c", p=128)[:, 4:8, :]), writes=[_wb()])
    P.dma("pool", lambda h: h.dma_start(out=wo[:, 0:4, :], in_=dr["w_o"].rearrange("(k p) c -> p k c", p=128)[:, 0:4, :]), writes=[_wb()])
    P.dma("pool", lambda h: h.dma_start(out=wo[:, 4:8, :], in_=dr["w_o"].rearrange("(k p) c -> p k c", p=128)[:, 4:8, :]), writes=[_wb()])

    wglu = A(P, [128, 2, 256], BF16)
    bglu = A(P, [128, 2], F32)
    P.dma("pool", lambda h: h.dma_start(out=wglu[:], in_=dr["w_glu"].rearrange("(k p) c -> p k c", p=128)), writes=[_wb()])
    P.dma("sp", lambda h: h.dma_start(out=bglu[:], in_=dr["b_gluT"][:, :]), writes=[_wb()])
    w_hi = P.a_cur
    mod_ps = nc.alloc_psum_tensor
    mod_view = ps[7][:, 0:96].rearrange("p (j r) -> p j r", r=2)
    compute_mod(P, dr, [0, 1, 2, 3, 4, 5], mod_view, psb[7], light=True)
    gm_a, gg_a = mod_derived(P, 1, 2, 0, 1)
    gm_f, gg_f = mod_derived(P, 4, 5, 2, 3)
    sh_a = P.modT[:, 0:8, :]
    sh_f = P.modT[:, 24:32, :]
    P.dbgt = []
    if DBG: P.dbgt += [P.dma("sp", lambda h: h.dma_start(out=dbg_mod[:, :, :], in_=P.modT[:]), reads=[P.modb], writes=[P.buf()])]

    h2 = A(P, [128, KC, TT], BF16)
    h2b = P.buf("h2")
    if moe:
        cbT = A(P, [8, TT], BF16)
        cbTb = P.buf("cbT")
        mkT = A(P, [8, TT], BF16)
        mkTb = P.buf("mkT")
        ident = A(P, [128, 128], F32)
        P.dma("sp", lambda h: h.dma_start(out=ident[:], in_=dr["ident"][:, :]), writes=[onesb])
        wr = A(P, [128, KC, 8], F32)
        P.dma("sp", lambda h: h.dma_start(out=wr[:], in_=dr["w_r"].rearrange("(k p) e -> p k e", p=128)), writes=[onesb])
        br_t = A(P, [128, 8], F32)
        P.dma("sp", lambda h: h.dma_start(out=br_t[:], in_=dr["b_r"][:, :]), writes=[onesb])
        sel = A(P, [8, 8, 128], BF16)
        P.dma("pool", lambda h: h.dma_start(out=sel[:], in_=dr["sel"][:, :, :]), writes=[onesb])
    markA = P.a_cur
    wjoin = A(P, [128, 1], F32)
    P.op("dve", lambda h: h.memset(wjoin[:], 0.0), reads=wbufs, writes=[wAb])
    glu_t = A(P, [128, 2, 256], BF16)
    glub = P.buf("glu")
    sgl = A(P, [128, 256], F32)
    sglb = P.buf("sgl")
    xb = [A(P, [128, KC, 256], F32) for _ in range(2)]
    xbb = [P.buf(), P.buf()]
    brb_t = [A(P, [128, KC, 256], BF16) for _ in range(2)]
    brbb = [P.buf(), P.buf()]
    sq = A(P, [128, KC, 256], BF16)
    sqb = P.buf()
    rstd = A(P, [128, 256], F32)
    rstdb = P.buf()
    tmp = [A(P, [128, 256], F32) for _ in range(2)]
    tmpb = [P.buf(), P.buf()]
    hb = A(P, [128, KC, 256], BF16)
    hbb = P.buf()
    yb = A(P, [128, KC, 256], BF16)
    ybb = P.buf()
    zb = A(P, [128, KC, 256], F32)
    zbb = P.buf()
    sg = [A(P, [128, 256], F32) for _ in range(2)]
    sgb = [P.buf(), P.buf()]
    tt2 = [A(P, [128, 256], F32) for _ in range(2)]
    tt2b = [P.buf(), P.buf()]
    accA = [A(P, [128, 256], F32) for _ in range(2)]
    accAb = [P.buf(), P.buf()]
    xob = P.buf("xo")
    h2fb = P.buf("h2f")
    if moe:
        h2f = A(P, [128, KC, 256], F32)
        lg = A(P, [128, 8], F32)
        mx8 = A(P, [128, 8], F32)
        msk = A(P, [128, 8], F32)
        ex = A(P, [128, 8], F32)
        den = A(P, [128, 1], F32)
        nmx = A(P, [128, 1], F32)
        rb = P.buf("router")
    cnt = 0
    P.sscp = A(P, [128, 256], F32)
    P.sscpb = P.buf()
    def _ldA(bi_):
        t0_, n_, _r = blocks[bi_]
        xx, xxb = xb[bi_ % 2], xbb[bi_ % 2]
        bb_, bbb = brb_t[bi_ % 2], brbb[bi_ % 2]
        P.dma("sp", lambda h: h.dma_start(out=xx[:, 0:4, 0:n_], in_=xT[:, 0:4, t0_:t0_ + n_]), writes=[xxb])
        P.dma("sp", lambda h: h.dma_start(out=xx[:, 4:8, 0:n_], in_=xT[:, 4:8, t0_:t0_ + n_]), writes=[xxb])
        P.dma("sp", lambda h: h.dma_start(out=bb_[:, :, 0:n_], in_=brT[:, :, t0_:t0_ + n_]), writes=[bbb])
    _ldA(0)
    for bi, (t0, n, r) in enumerate(blocks):
        x_t, x_b = xb[bi % 2], xbb[bi % 2]
        b_t, b_b = brb_t[bi % 2], brbb[bi % 2]
        if bi + 1 < len(blocks):
            _ldA(bi + 1)
        rms_rstd(P, x_t, x_b, n, sq, sqb, ps[6], psb[6], rstd, rstdb, ones)
        norm_mod(P, x_t, x_b, n, rstd, rstdb, gm_a, sh_a, r, hb, hbb, tmp, tmpb)
        for oc in range(2):
            for kc in range(2):
                P.op("pe", lambda h, oc=oc, kc=kc, n=n, b_t=b_t: h.matmul(ps[6][:, 0:n], lhsT=wglu[:, kc, oc * 128:(oc + 1) * 128], rhs=b_t[:, 2 + kc, 0:n],
                                                                      start=(kc == 0), stop=(kc == 1)), reads=[wAb, b_b], writes=[psb[6]])
            P.op("act", lambda h, oc=oc, n=n: h.activation(out=sgl[:, 0:n], in_=ps[6][:, 0:n], func=AF.Sigmoid, bias=bglu[:, oc:oc + 1], scale=1.0), reads=[psb[6], wAb], writes=[sglb])
            P.op("dve", lambda h, oc=oc, n=n, b_t=b_t: h.tensor_tensor(out=glu_t[:, oc, 0:n], in0=sgl[:, 0:n], in1=b_t[:, 2 + oc, 0:n], op=ALU.mult), reads=[sglb, b_b], writes=[glub])
        for fc in range(8):
            ac, acb = accA[fc % 2], accAb[fc % 2]
            for b in range(4):
                gi = cnt % 2
                cnt += 1
                gps, gpb = ps[gi], psb[gi]
                pps, ppb = ps[2 + gi], psb[2 + gi]
                for k in range(KC):
                    P.op("pe", lambda h, gps=gps, k=k, b=b, fc=fc, n=n: h.matmul(gps[:, 0:n], lhsT=wgt[:, k, b * 1024 + fc * 128:b * 1024 + (fc + 1) * 128], rhs=hb[:, k, 0:n],
                                                                                 start=(k == 0), stop=(k == KC - 1)), reads=[wAb, hbb], writes=[gpb])
                for hh in range(2):
                    rhs_ap = glu_t[:, hh, 0:n] if b == 1 else b_t[:, 2 * b + hh, 0:n]
                    P.op("pe", lambda h, pps=pps, hh=hh, b=b, fc=fc, n=n, rhs_ap=rhs_ap: h.matmul(pps[:, 0:n], lhsT=wbr[:, 2 * b + hh, fc * 128:(fc + 1) * 128], rhs=rhs_ap,
                                                                                          start=(hh == 0), stop=(hh == 1)), reads=[wAb, b_b, glub], writes=[ppb])
                s_t, s_b = sg[gi], sgb[gi]
                P.op("act", lambda h, s_t=s_t, gps=gps, n=n: h.activation(out=s_t[:, 0:n], in_=gps[:, 0:n], func=AF.Sigmoid), reads=[gpb], writes=[s_b])
                if b == 0:
                    P.op("dve", lambda h, ac=ac, s_t=s_t, pps=pps, n=n: h.tensor_tensor(out=ac[:, 0:n], in0=s_t[:, 0:n], in1=pps[:, 0:n], op=ALU.mult),
                         reads=[s_b, ppb], writes=[acb])
                else:
                    t_t, t_b = tt2[gi], tt2b[gi]
                    P.op("dve", lambda h, t_t=t_t, s_t=s_t, pps=pps, n=n: h.tensor_tensor(out=t_t[:, 0:n], in0=s_t[:, 0:n], in1=pps[:, 0:n], op=ALU.mult),
                         reads=[s_b, ppb], writes=[t_b])
                    if b < 3:
                        P.op("pool", lambda h, ac=ac, t_t=t_t, n=n: h.tensor_tensor(out=ac[:, 0:n], in0=ac[:, 0:n], in1=t_t[:, 0:n], op=ALU.add),
                             reads=[acb, t_b], writes=[acb])
                    else:
                        P.op("pool", lambda h, ac=ac, t_t=t_t, n=n, fc=fc: h.tensor_tensor(out=yb[:, fc, 0:n], in0=ac[:, 0:n], in1=t_t[:, 0:n], op=ALU.add),
                             reads=[acb, t_b], writes=[ybb])
        for fc in range(8):
            zi = 4 + fc % 2
            for k in range(KC):
                P.op("pe", lambda h, zi=zi, k=k, fc=fc, n=n: h.matmul(ps[zi][:, 0:n], lhsT=wo[:, k, fc * 128:(fc + 1) * 128], rhs=yb[:, k, 0:n], start=(k == 0), stop=(k == KC - 1)),
                     reads=[wAb, ybb], writes=[psb[zi]])
            P.op("act", lambda h, zi=zi, fc=fc, n=n: h.activation(out=zb[:, fc, 0:n], in_=ps[zi][:, 0:n], func=AF.Copy), reads=[psb[zi]], writes=[zbb])
        rms_rstd(P, zb, zbb, n, sq, sqb, ps[6], psb[6], rstd, rstdb, ones)
        if DBG: P.dbgt.append(P.dma("sp", lambda h, t0=t0, n=n: h.dma_start(out=dbg_z[:, :, t0:t0 + n], in_=zb[:, :, 0:n]), reads=[zbb], writes=[P.buf()]))
        if DBG: P.dbgt.append(P.dma("sp", lambda h, t0=t0, n=n: h.dma_start(out=dbg_r[:, t0:t0 + n], in_=rstd[:, 0:n]), reads=[rstdb], writes=[P.buf()]))
        if DBG: P.dbgt.append(P.dma("sp", lambda h, t0=t0, n=n: h.dma_start(out=dbg_sq[:, :, t0:t0 + n], in_=sq[:, :, 0:n]), reads=[sqb], writes=[P.buf()]))
        if DBG: P.op("dve", lambda h, n=n: h.tensor_copy(out=P.sscp[:, 0:n], in_=ps[6][:, 0:n]), reads=[psb[6]], writes=[P.sscpb])
        if DBG: P.dbgt.append(P.dma("sp", lambda h, t0=t0, n=n: h.dma_start(out=dbg_ss[:, t0:t0 + n], in_=P.sscp[:, 0:n]), reads=[P.sscpb], writes=[P.buf()]))
        for k in range(KC):
            tb_, tt_ = tmpb[k % 2], tmp[k % 2]
            P.op("dve", lambda h, k=k, tt_=tt_, n=n: h.tensor_tensor(out=tt_[:, 0:n], in0=zb[:, k, 0:n], in1=rstd[:, 0:n], op=ALU.mult), reads=[zbb, rstdb], writes=[tb_])
            P.op("dve", lambda h, k=k, tt_=tt_, n=n, x_t=x_t, r=r: h.scalar_tensor_tensor(out=x_t[:, k, 0:n], in0=tt_[:, 0:n], scalar=gg_a[:, k, r:r + 1], in1=x_t[:, k, 0:n],
                                                                                    op0=ALU.mult, op1=ALU.add), reads=[tb_, P.modb, x_b], writes=[x_b])
        P.dma("sp", lambda h, x_t=x_t, t0=t0, n=n: h.dma_start(out=xoT[:, :, t0:t0 + n], in_=x_t[:, :, 0:n]), reads=[x_b], writes=[xob])
        if DBG: P.dbgt.append(P.dma("sp", lambda h, x_t=x_t, t0=t0, n=n: h.dma_start(out=dbg_xm[:, :, t0:t0 + n], in_=x_t[:, :, 0:n]), reads=[x_b], writes=[P.buf()]))
        if DBG: P.dbgt.append(P.dma("sp", lambda h, t0=t0, n=n: h.dma_start(out=dbg_h[:, :, t0:t0 + n], in_=hb[:, :, 0:n]), reads=[hbb], writes=[P.buf()]))
        if DBG: P.dbgt.append(P.dma("sp", lambda h, t0=t0, n=n: h.dma_start(out=dbg_y[:, :, t0:t0 + n], in_=yb[:, :, 0:n]), reads=[ybb], writes=[P.buf()]))
        rms_rstd(P, x_t, x_b, n, sq, sqb, ps[6], psb[6], rstd, rstdb, ones)
        norm_mod(P, x_t, x_b, n, rstd, rstdb, gm_f, sh_f, r, h2, h2b, tmp, tmpb, dst_off=t0, dst32=(h2f if moe else None), dst32b=h2fb)
        if moe:
            for tt in range(n // 128):
                for k in range(KC):
                    P.op("pe", lambda h, k=k, tt=tt: h.matmul(ps[7][:, 0:8], lhsT=h2f[:, k, tt * 128:(tt + 1) * 128], rhs=wr[:, k, :], start=(k == 0), stop=(k == KC - 1)),
                         reads=[h2fb, onesb], writes=[psb[7]])
                P.op("dve", lambda h: h.tensor_tensor(out=lg[:], in0=ps[7][:, 0:8], in1=br_t[:], op=ALU.add), reads=[psb[7], onesb], writes=[rb])
                P.op("dve", lambda h: h.max(out=mx8[:], in_=lg[:]), reads=[rb], writes=[rb])
                P.op("dve", lambda h: h.tensor_scalar(out=msk[:], in0=lg[:], scalar1=mx8[:, 1:2], scalar2=None, op0=ALU.is_ge), reads=[rb], writes=[rb])
                P.op("dve", lambda h: h.tensor_scalar(out=nmx[:], in0=mx8[:, 0:1], scalar1=-1.0, scalar2=None, op0=ALU.mult), reads=[rb], writes=[rb])
                P.op("act", lambda h: h.activation(out=ex[:], in_=lg[:], func=AF.Exp, bias=nmx[:, 0:1], scale=1.0), reads=[rb], writes=[rb])
                P.op("dve", lambda h: h.tensor_tensor(out=ex[:], in0=ex[:], in1=msk[:], op=ALU.mult), reads=[rb], writes=[rb])
                P.op("dve", lambda h: h.reduce_sum(out=den[:], in_=ex[:], axis=AX.X), reads=[rb], writes=[rb])
                P.op("dve", lambda h: h.reciprocal(out=den[:], in_=den[:]), reads=[rb], writes=[rb])
                P.op("dve", lambda h: h.tensor_scalar(out=ex[:], in0=ex[:], scalar1=den[:, 0:1], scalar2=None, op0=ALU.mult), reads=[rb], writes=[rb])
                P.op("pe", lambda h: h.transpose(ps[7][0:8, 128:256], ex[:], ident[:]), reads=[rb, onesb], writes=[psb[7]])
                P.op("act", lambda h, t0=t0, tt=tt: h.activation(out=cbT[:, t0 + tt * 128:t0 + (tt + 1) * 128], in_=ps[7][0:8, 128:256], func=AF.Copy), reads=[psb[7]], writes=[cbTb])
                if mode == 'moe_a':
                    P.op("pe", lambda h: h.transpose(ps[7][0:8, 256:384], msk[:], ident[:]), reads=[rb, onesb], writes=[psb[7]])
                    P.op("act", lambda h, t0=t0, tt=tt: h.activation(out=mkT[:, t0 + tt * 128:t0 + (tt + 1) * 128], in_=ps[7][0:8, 256:384], func=AF.Copy), reads=[psb[7]], writes=[mkTb])
    barrier(P)
    P.a_cur = markA
    if mode == 'moe_a':
        fin = [P.dma("sp", lambda h: h.dma_start(out=h2o[:, :, :], in_=h2[:, :, :]), reads=[h2b], writes=[P.buf()]),
               P.dma("sp", lambda h: h.dma_start(out=cbo[:, :], in_=cbT[:, :]), reads=[cbTb], writes=[P.buf()]),
               P.dma("sp", lambda h: h.dma_start(out=mko[:, :], in_=mkT[:, :]), reads=[mkTb], writes=[P.buf()])]
        barrier(P)
        P.finish_wait("sp", fin + P.dbgt)
        P.emit()
        return nc
    blocks = blocksB
    NSL = 4
    P.a_cur = w_lo
    acc = A(P, [128, KC, TT], F32)
    accb = [P.buf() for _ in blocks]
    hid = [A(P, [128, NSL, 512], BF16) for _ in range(2)]
    hidb = [P.buf(), P.buf()]
    ssb_t = [A(P, [128, 512], F32) for _ in range(2)]
    ssbb = [P.buf(), P.buf()]
    cbe = A(P, [128, 512], BF16)
    cbeb = P.buf()
    assert P.a_cur <= w_hi, "stage-B tiles overflow the weight region"
    P.a_cur = markA
    markB = markA
    wg_s = [A(P, [128, KC, NSL * 128], BF16) for _ in range(2)]
    wu_s = [A(P, [128, KC, NSL * 128], BF16) for _ in range(2)]
    wd_s = [A(P, [128, NSL, D], BF16) for _ in range(2)]
    wsb = [P.buf(), P.buf()]
    ntile = dff // 128
    slices = [(s0, min(NSL, ntile - s0)) for s0 in range(0, ntile, NSL)]
    si = 0
    hcnt = 0
    gcnt = 0
    work = [(e, s0, ns) for e in range(n_exp) for (s0, ns) in slices]

    def _ldW(widx):
        e_, s0_, ns_ = work[widx]
        wi_ = widx % 2
        wgv_ = dr["w_g"][e_].rearrange("(k p) f -> p k f", p=128)
        wuv_ = dr["w_u"][e_].rearrange("(k p) f -> p k f", p=128)
        wdv_ = dr["w_d"][e_].rearrange("(j p) c -> p j c", p=128)
        for k2 in range(2):
            P.dma("pool", lambda h, k2=k2: h.dma_start(out=wg_s[wi_][:, 4 * k2:4 * k2 + 4, 0:ns_ * 128], in_=wgv_[:, 4 * k2:4 * k2 + 4, s0_ * 128:(s0_ + ns_) * 128]), writes=[wsb[wi_]])
            P.dma("pool", lambda h, k2=k2: h.dma_start(out=wu_s[wi_][:, 4 * k2:4 * k2 + 4, 0:ns_ * 128], in_=wuv_[:, 4 * k2:4 * k2 + 4, s0_ * 128:(s0_ + ns_) * 128]), writes=[wsb[wi_]])
        for j in range(ns_):
            P.dma("pool", lambda h, j=j: h.dma_start(out=wd_s[wi_][:, j, :], in_=wdv_[:, s0_ + j, :]), writes=[wsb[wi_]])
    _ldW(0)
    for widx, (e, s0, ns) in enumerate(work):
        if True:
            wi = widx % 2
            if widx + 1 < len(work):
                _ldW(widx + 1)
            for bi, (t0, n, r) in enumerate(blocks):
                hi = hcnt % 2
                hcnt += 1
                if moe:
                    P.op("pe", lambda h, e=e, t0=t0, n=n: h.matmul(ps[7][:, 0:n], lhsT=sel[:, e, :], rhs=cbT[:, t0:t0 + n], start=True, stop=True), reads=[cbTb, onesb], writes=[psb[7]])
                    P.op("act", lambda h, n=n: h.activation(out=cbe[:, 0:n], in_=ps[7][:, 0:n], func=AF.Copy), reads=[psb[7]], writes=[cbeb])
                for j in range(ns):
                    gi = gcnt % 2
                    gcnt += 1
                    for k in range(KC):
                        P.op("pe", lambda h, gi=gi, wi=wi, j=j, k=k, t0=t0, n=n: h.matmul(ps[gi][:, 0:n], lhsT=wg_s[wi][:, k, j * 128:(j + 1) * 128], rhs=h2[:, k, t0:t0 + n], start=(k == 0), stop=(k == KC - 1)),
                             reads=[wsb[wi], h2b], writes=[psb[gi]])
                    for k in range(KC):
                        P.op("pe", lambda h, gi=gi, wi=wi, j=j, k=k, t0=t0, n=n: h.matmul(ps[2 + gi][:, 0:n], lhsT=wu_s[wi][:, k, j * 128:(j + 1) * 128], rhs=h2[:, k, t0:t0 + n], start=(k == 0), stop=(k == KC - 1)),
                             reads=[wsb[wi], h2b], writes=[psb[2 + gi]])
                    P.op("act", lambda h, gi=gi, n=n: h.activation(out=ssb_t[gi][:, 0:n], in_=ps[gi][:, 0:n], func=AF.Silu), reads=[psb[gi]], writes=[ssbb[gi]])
                    if moe:
                        P.op("dve", lambda h, gi=gi, n=n: h.tensor_tensor(out=ssb_t[gi][:, 0:n], in0=ssb_t[gi][:, 0:n], in1=ps[2 + gi][:, 0:n], op=ALU.mult),
                             reads=[ssbb[gi], psb[2 + gi]], writes=[ssbb[gi]])
                        P.op("pool", lambda h, gi=gi, hi=hi, j=j, n=n: h.tensor_tensor(out=hid[hi][:, j, 0:n], in0=ssb_t[gi][:, 0:n], in1=cbe[:, 0:n], op=ALU.mult),
                             reads=[ssbb[gi], cbeb], writes=[hidb[hi]])
                    else:
                        P.op("dve", lambda h, gi=gi, hi=hi, j=j, n=n: h.tensor_tensor(out=hid[hi][:, j, 0:n], in0=ssb_t[gi][:, 0:n], in1=ps[2 + gi][:, 0:n], op=ALU.mult),
                             reads=[ssbb[gi], psb[2 + gi]], writes=[hidb[hi]])
                first = (e == 0 and s0 == 0)
                for fc in range(8):
                    oi = 4 + fc % 2
                    for j in range(ns):
                        P.op("pe", lambda h, oi=oi, wi=wi, j=j, fc=fc, hi=hi, n=n, ns=ns: h.matmul(ps[oi][:, 0:n], lhsT=wd_s[wi][:, j, fc * 128:(fc + 1) * 128], rhs=hid[hi][:, j, 0:n], start=(j == 0), stop=(j == ns - 1)),
                             reads=[wsb[wi], hidb[hi]], writes=[psb[oi]])
                    if first:
                        P.op("act", lambda h, oi=oi, fc=fc, t0=t0, n=n: h.activation(out=acc[:, fc, t0:t0 + n], in_=ps[oi][:, 0:n], func=AF.Copy), reads=[psb[oi]], writes=[accb[bi]])
                    else:
                        P.op("dve", lambda h, oi=oi, fc=fc, t0=t0, n=n: h.tensor_tensor(out=acc[:, fc, t0:t0 + n], in0=acc[:, fc, t0:t0 + n], in1=ps[oi][:, 0:n], op=ALU.add),
                             reads=[psb[oi], accb[bi]], writes=[accb[bi]])
    barrier(P)
    P.a_cur = markB
    xm = [A(P, [128, KC, 512], F32) for _ in range(2)]
    xmb = [P.buf(), P.buf()]
    sqF = A(P, [128, KC, 512], BF16)
    rstdF = A(P, [128, 512], F32)
    tmpF = [A(P, [128, 512], F32) for _ in range(2)]
    outs = []
    for bi, (t0, n, r) in enumerate(blocks):
        x_t, x_b = xm[bi % 2], xmb[bi % 2]
        P.dma("sp", lambda h, x_t=x_t, t0=t0, n=n: h.dma_start(out=x_t[:, :, 0:n], in_=xoT[:, :, t0:t0 + n]), reads=[xob], writes=[x_b])
        accv = acc[:, :, t0:t0 + n]
        P.op("act", lambda h, accv=accv, n=n: h.activation(out=sqF[:, :, 0:n], in_=accv, func=AF.Square), reads=[accb[bi]], writes=[sqb])
        for k in range(KC):
            P.op("pe", lambda h, k=k, n=n: h.matmul(ps[6][:, 0:n], lhsT=ones[:], rhs=sqF[:, k, 0:n], start=(k == 0), stop=(k == KC - 1)), reads=[sqb, onesb], writes=[psb[6]])
        P.op("act", lambda h, n=n: h.activation(out=rstdF[:, 0:n], in_=ps[6][:, 0:n], func=AF.Ln, scale=1.0 / D, bias=P.eps_t[:, 0:1]), reads=[psb[6]], writes=[rstdb])
        P.op("act", lambda h, n=n: h.activation(out=rstdF[:, 0:n], in_=rstdF[:, 0:n], func=AF.Exp, scale=-0.5), reads=[rstdb], writes=[rstdb])
        for k in range(KC):
            tb_, tt_ = tmpb[k % 2], tmpF[k % 2]
            P.op("dve", lambda h, k=k, tt_=tt_, n=n, t0=t0: h.tensor_tensor(out=tt_[:, 0:n], in0=acc[:, k, t0:t0 + n], in1=rstdF[:, 0:n], op=ALU.mult), reads=[accb[bi], rstdb], writes=[tb_])
            P.op("dve", lambda h, k=k, tt_=tt_, n=n, x_t=x_t, r=r: h.scalar_tensor_tensor(out=x_t[:, k, 0:n], in0=tt_[:, 0:n], scalar=gg_f[:, k, r:r + 1], in1=x_t[:, k, 0:n],
                                                                                    op0=ALU.mult, op1=ALU.add), reads=[tb_, P.modb, x_b], writes=[x_b])
        outs.append(P.dma("sp", lambda h, x_t=x_t, t0=t0, n=n: h.dma_start(out=xoT[:, :, t0:t0 + n], in_=x_t[:, :, 0:n]), reads=[x_b], writes=[xob]))
    P.finish_wait("sp", outs + P.dbgt)
    P.emit()
    return nc


def build_E(groups=(4,) * 8, dff=3584):
    nc = bass.Bass("TRN2", target_bir_lowering=False)
    ngrp = len(groups)
    gtok = 512 * max(groups)
    NT = 512 * sum(groups)
    goff = [512 * sum(groups[:g]) for g in range(ngrp)]
    h2d = nc.dram_tensor("h2", [KC, 128, NT], BF16, kind="ExternalInput").ap().rearrange("k p t -> p k t")
    cbd = nc.dram_tensor("cbe", [128, NT], BF16, kind="ExternalInput").ap()
    wgd = nc.dram_tensor("w_g", [D, dff], F32, kind="ExternalInput").ap().rearrange("(k p) f -> p k f", p=128)
    wud = nc.dram_tensor("w_u", [D, dff], F32, kind="ExternalInput").ap().rearrange("(k p) f -> p k f", p=128)
    wdd = nc.dram_tensor("w_d", [dff, D], F32, kind="ExternalInput").ap().rearrange("(j p) c -> p j c", p=128)
    ye = nc.dram_tensor("ye", [KC, 128, NT], F32, kind="ExternalOutput").ap().rearrange("k p t -> p k t")
    P = Prog(nc)
    arena_init(P)
    ps = [P.ps("ps%d" % i, [128, 512], F32) for i in range(8)]
    psb = [P.buf("ps%d" % i) for i in range(8)]
    h2g = [A(P, [128, KC, gtok], BF16) for _ in range(2)]
    h2gb = [P.buf(), P.buf()]
    cbg = [A(P, [128, gtok], BF16) for _ in range(2)]
    acc = A(P, [128, KC, gtok], F32)
    NSL = 4
    wg_s = [A(P, [128, KC, NSL * 128], BF16) for _ in range(2)]
    wu_s = [A(P, [128, KC, NSL * 128], BF16) for _ in range(2)]
    wd_s = [A(P, [128, NSL, D], BF16) for _ in range(2)]
    wsb = [P.buf(), P.buf()]
    hid = [A(P, [128, NSL, 512], BF16) for _ in range(2)]
    hidb = [P.buf(), P.buf()]
    ssb_t = [A(P, [128, 512], F32) for _ in range(2)]
    ssbb = [P.buf(), P.buf()]
    ntile = dff // 128
    slices = [(s0, min(NSL, ntile - s0)) for s0 in range(0, ntile, NSL)]
    accb = [P.buf() for _ in range(max(groups))]
    si = hcnt = gcnt = 0
    outs = []
    work = [(g, sidx, s0, ns) for g in range(ngrp) for sidx, (s0, ns) in enumerate(slices)]

    def _ldG(g_):
        hg_, hgb_ = h2g[g_ % 2], h2gb[g_ % 2]
        gn_ = 512 * groups[g_]
        for k2 in range(2):
            _ldF(P, "sp", hg_[:, 4 * k2:4 * k2 + 4, 0:gn_], h2d[:, 4 * k2:4 * k2 + 4, goff[g_]:goff[g_] + gn_], [hgb_])
        _ldF(P, "sp", cbg[g_ % 2][:, 0:gn_], cbd[:, goff[g_]:goff[g_] + gn_], [hgb_])

    def _ldW(widx):
        _g, _sidx, s0_, ns_ = work[widx]
        wi_ = widx % 2
        for k2 in range(2):
            _ldF(P, "pool", wg_s[wi_][:, 4 * k2:4 * k2 + 4, 0:ns_ * 128], wgd[:, 4 * k2:4 * k2 + 4, s0_ * 128:(s0_ + ns_) * 128], [wsb[wi_]])
            _ldF(P, "pool", wu_s[wi_][:, 4 * k2:4 * k2 + 4, 0:ns_ * 128], wud[:, 4 * k2:4 * k2 + 4, s0_ * 128:(s0_ + ns_) * 128], [wsb[wi_]])
        for j in range(ns_):
            _ldF(P, "pool", wd_s[wi_][:, j, :], wdd[:, s0_ + j, :], [wsb[wi_]])
    _ldG(0)
    _ldW(0)
    pending = []

    def _down(u):
        (g_, sidx_, ns_, wi_, bi_, hi_, last_) = u
        t0_, n_ = bi_ * 512, 512
        for fc in range(8):
            oi = 4 + fc % 2
            for j in range(ns_):
                _mmF(P, ps[oi][:, 0:n_], wd_s[wi_][:, j, fc * 128:(fc + 1) * 128], hid[hi_][:, j, 0:n_], j == 0, j == ns_ - 1, [wsb[wi_], hidb[hi_]], [psb[oi]])
            av = acc[:, fc, t0_:t0_ + n_]
            pv = ps[oi][:, 0:n_]
            if sidx_ == 0:
                P.op("act", lambda h, av=av, pv=pv: h.activation(out=av, in_=pv, func=AF.Copy), reads=[psb[oi]], writes=[accb[bi_]])
            else:
                P.op("dve", lambda h, av=av, pv=pv: h.tensor_tensor(out=av, in0=av, in1=pv, op=ALU.add), reads=[psb[oi], accb[bi_]], writes=[accb[bi_]])
        if last_:
            outs.append(_ldF(P, "sp", ye[:, :, goff[g_] + t0_:goff[g_] + t0_ + 512], acc[:, :, t0_:t0_ + 512], [P.buf()], reads=[accb[bi_]]))

    for widx, (g, sidx, s0, ns) in enumerate(work):
        hg, hgb = h2g[g % 2], h2gb[g % 2]
        cg = cbg[g % 2]
        nblk = groups[g]
        if sidx == 0 and g + 1 < ngrp:
            _ldG(g + 1)
        wi = widx % 2
        for bi in range(nblk):
            t0, n = bi * 512, 512
            hi = hcnt % 2
            hcnt += 1
            for j in range(ns):
                gi = gcnt % 2
                gcnt += 1
                for k in range(KC):
                    _mmF(P, ps[gi][:, 0:n], wg_s[wi][:, k, j * 128:(j + 1) * 128], hg[:, k, t0:t0 + n], k == 0, k == KC - 1, [wsb[wi], hgb], [psb[gi]])
                for k in range(KC):
                    _mmF(P, ps[2 + gi][:, 0:n], wu_s[wi][:, k, j * 128:(j + 1) * 128], hg[:, k, t0:t0 + n], k == 0, k == KC - 1, [wsb[wi], hgb], [psb[2 + gi]])
                st_, stb_ = ssb_t[gi], ssbb[gi]
                P.op("act", lambda h, st_=st_, gi=gi, n=n: h.activation(out=st_[:, 0:n], in_=ps[gi][:, 0:n], func=AF.Silu), reads=[psb[gi]], writes=[stb_])
                P.op("dve", lambda h, st_=st_, gi=gi, n=n: h.tensor_tensor(out=st_[:, 0:n], in0=st_[:, 0:n], in1=ps[2 + gi][:, 0:n], op=ALU.mult), reads=[stb_, psb[2 + gi]], writes=[stb_])
                hd = hid[hi]
                P.op("pool", lambda h, st_=st_, hd=hd, j=j, n=n, cg=cg, t0=t0: h.tensor_tensor(out=hd[:, j, 0:n], in0=st_[:, 0:n], in1=cg[:, t0:t0 + n], op=ALU.mult), reads=[stb_, hgb], writes=[hidb[hi]])
            if pending:
                _down(pending.pop(0))
            if bi == 0 and widx + 1 < len(work):
                _ldW(widx + 1)
            pending.append((g, sidx, ns, wi, bi, hi, sidx == len(slices) - 1))
    while pending:
        _down(pending.pop(0))
    P.finish_wait("sp", outs)
    P.emit()
    return nc


def _ldF(P, q, out, in_, writes, reads=()):
    return P.dma(q, lambda h: h.dma_start(out=out, in_=in_), reads=reads, writes=writes)


def _mmF(P, out, lhsT, rhs, start, stop, reads, writes):
    return P.op("pe", lambda h: h.matmul(out, lhsT=lhsT, rhs=rhs, start=start, stop=stop), reads=reads, writes=writes)


def build_Fc(TT=2048, nexp=8):
    nc = bass.Bass("TRN2", target_bir_lowering=False)
    dr = {}

    def din(name, shape, dt=F32):
        dr[name] = nc.dram_tensor(name, list(shape), dt, kind="ExternalInput").ap()
    din("xm", [KC, 128, TT])
    din("yp", [nexp, KC, 128, TT])
    din("condT", [128, KC, 2])
    din("w_mod", [D, 6 * D])
    din("b_modT", [128, 48, 2])
    din("norm_gT", [128, 4, KC, 2])
    xo = nc.dram_tensor("xo", [KC, 128, TT], F32, kind="ExternalOutput").ap().rearrange("k p t -> p k t")
    xm = dr["xm"].rearrange("k p t -> p k t")
    P = Prog(nc)
    arena_init(P)
    ps = [P.ps("ps%d" % i, [128, 512], F32) for i in range(8)]
    psb = [P.buf("ps%d" % i) for i in range(8)]
    ones = A(P, [128, 128], BF16)
    onesb = P.buf()
    P.op("dve", lambda h: h.memset(ones[:], 1.0), writes=[onesb])
    P.eps_t = A(P, [128, 1], F32)
    P.op("dve", lambda h: h.memset(P.eps_t[:], EPS), writes=[onesb])
    mod_view = ps[7][:, 0:96].rearrange("p (j r) -> p j r", r=2)
    compute_mod(P, dr, [5], mod_view, psb[7])
    _, gg_f = mod_derived(P, 4, 5, 2, 3)
    acc = [A(P, [128, KC, 512], F32) for _ in range(2)]
    accb = [P.buf(), P.buf()]
    part = [A(P, [128, KC, 512], F32) for _ in range(3)]
    partb = [P.buf() for _ in range(3)]
    xt = [A(P, [128, KC, 512], F32) for _ in range(2)]
    xtb = [P.buf(), P.buf()]
    sq = A(P, [128, KC, 512], BF16)
    sqb = P.buf()
    rstd = A(P, [128, 512], F32)
    rstdb = P.buf()
    tmp = [A(P, [128, 512], F32) for _ in range(2)]
    tmpb = [P.buf(), P.buf()]
    outs = []
    pc = 0
    for bi in range(TT // 512):
        t0, n = bi * 512, 512
        a_t, a_b = acc[bi % 2], accb[bi % 2]
        x_t, x_b = xt[bi % 2], xtb[bi % 2]
        _ldF(P, "sp", x_t[:, :, :], xm[:, :, t0:t0 + n], [x_b])
        _ldF(P, "sp", a_t[:, :, :], dr["yp"][0].rearrange("k p t -> p k t")[:, :, t0:t0 + n], [a_b])
        for e in range(1, nexp):
            p_t, p_b = part[pc % 3], partb[pc % 3]
            pc += 1
            _ldF(P, "act" if e % 2 else "sp", p_t[:, :, :], dr["yp"][e].rearrange("k p t -> p k t")[:, :, t0:t0 + n], [p_b])
            eng = "dve" if e % 2 else "pool"
            P.op(eng, lambda h, a_t=a_t, p_t=p_t: h.tensor_tensor(out=a_t[:, :, :], in0=a_t[:, :, :], in1=p_t[:, :, :], op=ALU.add), reads=[a_b, p_b], writes=[a_b])
        rms_rstd(P, a_t, a_b, n, sq, sqb, ps[6], psb[6], rstd, rstdb, ones)
        for k in range(KC):
            tb_, tt_ = tmpb[k % 2], tmp[k % 2]
            P.op("dve", lambda h, k=k, tt_=tt_, a_t=a_t: h.tensor_tensor(out=tt_[:, :], in0=a_t[:, k, :], in1=rstd[:, :], op=ALU.mult), reads=[a_b, rstdb], writes=[tb_])
            P.op("dve", lambda h, k=k, tt_=tt_, x_t=x_t: h.scalar_tensor_tensor(out=x_t[:, k, :], in0=tt_[:, :], scalar=gg_f[:, k, 0:1], in1=x_t[:, k, :], op0=ALU.mult, op1=ALU.add),
                 reads=[tb_, P.modb, x_b], writes=[x_b])
        outs.append(_ldF(P, "sp", xo[:, :, t0:t0 + n], x_t[:, :, :], [P.buf()], reads=[x_b]))
    P.finish_wait("sp", outs)
    P.emit()
    return nc


import math, os
RET_STOP = int(os.environ.get('RET_STOP', '99'))
SKIP = os.environ.get('SKIP', '')

NTOK = 8448
NCH = 66
MAGIC = 12582912.0
TWO_PI = 2.0 * math.pi


def pos_of(dd):
    if dd == 0:
        return list(range(NCH))
    order = [1, 0] + list(range(65, 1, -1))
    pos = [0] * NCH
    for p_, c in enumerate(order):
        pos[c] = p_
    return pos


def range_reduce_sincos(P, ph, sn, cs, tmp, shape_ap, b):
    v = shape_ap
    _ts(P, "dve", v(tmp), v(ph), 1.0 / TWO_PI, MAGIC, ALU.mult, ALU.add, [b], [b])
    _ts(P, "dve", v(tmp), v(tmp), -MAGIC, None, ALU.add, None, [b], [b])
    _stt(P, v(ph), v(tmp), -TWO_PI, v(ph), ALU.mult, ALU.add, [b], [b])
    _ts(P, "dve", v(ph), v(ph), -math.pi, math.pi, ALU.max, ALU.min, [b], [b])
    _act(P, v(sn), v(ph), AF.Sin, [b], [b])
    _ts(P, "dve", v(tmp), v(ph), -1.0, None, ALU.mult, None, [b], [b])
    _tt(P, "dve", v(tmp), v(tmp), v(ph), ALU.max, [b], [b])
    _act(P, v(cs), v(tmp), AF.Sin, [b], [b], scale=-1.0, bias=P.halfpi[0:v(tmp).shape[0], 0:1])


def build_M(need_ctx_out, parts=("four", "s5", "ret", "na"), DBG=False):
    nc = bass.Bass("TRN2", target_bir_lowering=False)
    dr = {}

    def din(name, shape, dt=F32):
        dr[name] = nc.dram_tensor(name, list(shape), dt, kind="ExternalInput").ap()
    din("xT", [KC, 128, NTOK])
    din("condT", [128, KC, 2])
    din("w_mod", [D, 6 * D])
    din("b_modT", [128, 48, 2])
    din("norm_gT", [128, 4, KC, 2])
    din("w_fm", [D, 576])
    din("w_tm", [D, 256])
    din("f_CS", [64, 128], BF16); din("f_RP", [64, 128], BF16); din("f_RQ", [64, 128], BF16)
    din("f_CB", [128, 64, 128], BF16); din("f_SB", [128, 64, 128], BF16)
    din("f_C256", [128, 2, 256], BF16); din("f_S256", [128, 2, 256], BF16)
    din("r_cosF", [64, 8192]); din("r_sinF", [64, 8192]); din("r_cosT", [128, 64, 64]); din("r_sinT", [128, 64, 64])
    din("r_jcol", [128, 2]); din("r_dist", [128, 128]); din("r_mask", [2, 128, 128]); din("r_irow", [2, 64, 128])
    din("s_jrow", [128, 129]); din("s_jcol", [128, 1]); din("s_LT", [2, 128, 128], BF16); din("s_mrow", [64, 4]); din("s_msm", [128, 2, 4])
    din("ident_bf", [128, 128], BF16); din("ident_f", [128, 128])
    din("n_mask", [5, 128, 832]); din("n_toep", [15, 64, 64])
    din("s_sm", [128, 2, 2, 3]); din("s_row", [128, 2, 3, 256]); din("s_hs", [64, 2, 3, 64]); din("s_B", [64, 2, 2, 64])
    din("s_C", [128, 2, 2, 2, 16]); din("s_d", [64, 1])
    din("r_dec", [128, 2]); din("r_gn", [64, 1])
    out = nc.dram_tensor("brT_out", [4, 64, NTOK], BF16, kind="ExternalOutput").ap()
    hT = nc.dram_tensor("hT_scr", [KC, 128, NTOK], BF16, kind="Internal").ap().rearrange("k p t -> p k t")
    xT = dr["xT"].rearrange("k p t -> p k t")

    P = Prog(nc)
    arena_init(P)
    ps = [P.ps("ps%d" % i, [128, 512], F32) for i in range(8)]
    psb = [P.buf("ps%d" % i) for i in range(8)]
    cb = P.buf("consts")
    ones = A(P, [128, 128], BF16)
    P.op("dve", lambda h: h.memset(ones[:], 1.0), writes=[cb])
    P.eps_t = A(P, [128, 1], F32)
    P.op("dve", lambda h: h.memset(P.eps_t[:], EPS), writes=[cb])
    P.halfpi = A(P, [128, 1], F32)
    P.op("dve", lambda h: h.memset(P.halfpi[:], math.pi / 2), writes=[cb])
    P.one_t = A(P, [128, 1], F32)
    P.op("dve", lambda h: h.memset(P.one_t[:], 1.0), writes=[cb])
    ident = A(P, [128, 128], BF16)
    _ld(P, "sp", ident[:], dr["ident_bf"][:, :], [cb])
    mod_view = ps[7][:, 0:96].rearrange("p (j r) -> p j r", r=2)
    compute_mod(P, dr, [0, 1], mod_view, psb[7])
    gm_a, _ = mod_derived(P, 1, None, 0, 0)
    sh_a = P.modT[:, 0:8, :]
    wfm = A(P, [128, KC, 576], BF16)
    wtm = A(P, [128, KC, 256], BF16)
    wb = P.buf("w")
    _ld(P, "pool", wfm[:], dr["w_fm"].rearrange("(k p) c -> p k c", p=128), [wb])
    _ld(P, "pool", wtm[:], dr["w_tm"].rearrange("(k p) c -> p k c", p=128), [wb])
    blocks = [(0, 256, 1)] + [(256 + 512 * i, 512, 0) for i in range(16)]
    outs = []
    hTb = P.buf("hT")
    mark0 = P.a_cur

    def fm_proj(hb, hbb, n, g, pst, pstb):
        for k in range(KC):
            _mm(P, pst[0:64, 0:n], wfm[:, k, g * 64:(g + 1) * 64], hb[:, k, 0:n], k == 0, k == KC - 1, [wb, hbb], [pstb])

    sT = A(P, [64, NTOK], BF16)
    markS = P.a_cur
    fT = A(P, [64, NTOK], BF16)
    fTb, sTb = P.buf("fT"), P.buf("sT")
    markA = P.a_cur
    xb = [A(P, [128, KC, 512], F32) for _ in range(2)]
    xbb = [P.buf(), P.buf()]
    sq = A(P, [128, KC, 512], BF16)
    sqb = P.buf()
    rstd = A(P, [128, 512], F32)
    rstdb = P.buf()
    tmp = [A(P, [128, 512], F32) for _ in range(8)]
    tmpb = [P.buf() for _ in range(8)]
    hbs = [A(P, [128, KC, 512], BF16) for _ in range(2)]
    hbsb = [P.buf(), P.buf()]
    def _ldx(bi_):
        t0_, n_, _r = blocks[bi_]
        _ld(P, "sp", xb[bi_ % 2][:, 0:4, 0:n_], xT[:, 0:4, t0_:t0_ + n_], [xbb[bi_ % 2]])
        _ld(P, "sp", xb[bi_ % 2][:, 4:8, 0:n_], xT[:, 4:8, t0_:t0_ + n_], [xbb[bi_ % 2]])
    _ldx(0)
    for bi, (t0, n, r) in enumerate(blocks):
        x_t, x_b = xb[bi % 2], xbb[bi % 2]
        hb, hbb = hbs[bi % 2], hbsb[bi % 2]
        if bi + 1 < len(blocks):
            _ldx(bi + 1)
        rms_rstd(P, x_t, x_b, n, sq, sqb, ps[6], psb[6], rstd, rstdb, ones)
        norm_mod(P, x_t, x_b, n, rstd, rstdb, gm_a, sh_a, r, hb, hbb, tmp, tmpb)
        _ld(P, "sp", hT[:, :, t0:t0 + n], hb[:, :, 0:n], [hTb], reads=[hbb])
        for gi_, (g, dst, dstb) in enumerate(((0, fT, fTb), (1, sT, sTb))):
            pi_ = (2 * bi + gi_) % 4
            fm_proj(hb, hbb, n, g, ps[pi_], psb[pi_])
            _cp(P, "act" if gi_ == 0 else "dve", dst[:, t0:t0 + n], ps[pi_][0:64, 0:n], [psb[pi_]], [dstb])
    barrier(P)
    P.a_cur = markA

    if "four" in parts:
        markF = P.a_cur
        CS = A(P, [64, 128], BF16); RP = A(P, [64, 128], BF16); RQ = A(P, [64, 128], BF16)
        CB = A(P, [128, 64, 128], BF16); SB = A(P, [128, 64, 128], BF16)
        ftb = P.buf("ftab")
        for t_, nm in ((CS, "f_CS"), (RP, "f_RP"), (RQ, "f_RQ")):
            _ld(P, "sp", t_[:], dr[nm][:, :], [ftb])
        _ld(P, "sp", CB[:], dr["f_CB"][:, :, :], [ftb])
        _ld(P, "sp", SB[:], dr["f_SB"][:, :, :], [ftb])
        PQ = A(P, [64, 128, 128], BF16); PQb = P.buf("PQ")
        UVT = A(P, [128, 64, 128], BF16); UVTb = P.buf("UVT")
        aT = A(P, [64, NTOK], BF16); aTb = P.buf("aT")
        for g4 in range(32):
            pi_ = g4 % 2
            for jj in range(4):
                m2 = g4 * 4 + jj
                _mm(P, ps[pi_][0:64, jj * 128:(jj + 1) * 128], fT[:, 256 + m2:NTOK:128], CS[:, :], True, True, [fTb, ftb], [psb[pi_]])
            _cp(P, "act" if g4 % 2 else "dve", PQ[:, g4 * 4:(g4 + 1) * 4, :], ps[pi_][0:64, 0:512].rearrange("p (a b) -> p a b", b=128), [psb[pi_]], [PQb])
        for g4 in range(16):
            pi_ = 2 + g4 % 2
            for jj in range(4):
                d = g4 * 4 + jj
                _mm(P, ps[pi_][:, jj * 128:(jj + 1) * 128], PQ[:, :, d], RP[:, :], True, False, [PQb, ftb], [psb[pi_]])
                _mm(P, ps[pi_][:, jj * 128:(jj + 1) * 128], PQ[:, :, 64 + d], RQ[:, :], False, True, [PQb, ftb], [psb[pi_]])
            _cp(P, "act" if g4 % 2 else "dve", UVT[:, g4 * 4:(g4 + 1) * 4, :], ps[pi_][:, 0:512].rearrange("p (a b) -> p a b", b=128), [psb[pi_]], [UVTb])
        aT3 = aT[:, 256:NTOK].rearrange("p (a b) -> p a b", b=64)
        for g4 in range(16):
            pi_ = g4 % 2
            for jj in range(4):
                n1 = g4 * 4 + jj
                _mm(P, ps[pi_][0:64, jj * 128:(jj + 1) * 128], UVT[:, :, n1], CB[:, n1, :], True, False, [UVTb, ftb], [psb[pi_]])
                _mm(P, ps[pi_][0:64, jj * 128:(jj + 1) * 128], UVT[:, :, 64 + n1], SB[:, n1, :], False, True, [UVTb, ftb], [psb[pi_]])
            _cp(P, "act" if g4 % 2 else "dve", aT3[:, :, g4 * 4:(g4 + 1) * 4], ps[pi_][0:64, 0:512].rearrange("p (j n) -> p n j", n=128), [psb[pi_]], [aTb])
        if need_ctx_out:
            C256 = A(P, [128, 2, 256], BF16); S256 = A(P, [128, 2, 256], BF16)
            _ld(P, "sp", C256[:], dr["f_C256"][:, :, :], [ftb])
            _ld(P, "sp", S256[:], dr["f_S256"][:, :, :], [ftb])
            PQc = A(P, [128, 2, 128], BF16); PQcb = P.buf()
            for tt in range(2):
                _mm(P, ps[2 + tt][:, 0:128], fT[:, tt * 128:(tt + 1) * 128], CS[:, :], True, True, [fTb, ftb], [psb[2 + tt]])
                _cp(P, "dve", PQc[:, tt, :], ps[2 + tt][:, 0:128], [psb[2 + tt]], [PQcb])
            seq = [(tt, 0) for tt in range(2)] + [(tt, 1) for tt in range(2)]
            for i_, (tt, pq) in enumerate(seq):
                _mm(P, ps[4][0:64, 0:256], PQc[:, tt, pq * 64:(pq + 1) * 64], (C256 if pq == 0 else S256)[:, tt, :], i_ == 0, i_ == 3, [PQcb, ftb], [psb[4]])
            _cp(P, "dve", aT[:, 0:256], ps[4][0:64, 0:256], [psb[4]], [aTb])
        else:
            P.op("dve", lambda h: h.memset(aT[:, 0:256], 0.0), writes=[aTb])
        outs.append(_ld(P, "sp", out[0, :, :], aT[:, :], [P.buf()], reads=[aTb]))
        barrier(P)
    P.a_cur = markS

    if "s5" in parts:
        s5_part(P, dr, ps, psb, sT, sTb, out, outs, need_ctx_out, cb)
    barrier(P)
    P.a_cur = mark0

    if "ret" in parts:
        ret_part(P, dr, ps, psb, hT, hTb, wfm, wtm, wb, blocks, out, outs, need_ctx_out, cb, ones)
        barrier(P)
        P.a_cur = mark0
    if "na" in parts:
        na_part(P, dr, ps, psb, hT, hTb, wfm, wtm, wb, blocks, out, outs, need_ctx_out, cb, ident)
        barrier(P)
    P.finish_wait("sp", outs)
    P.emit()
    return nc


def load_h(P, hT, hTb, hbs, hbsb, bi, t0, n):
    hb, hbb = hbs[bi % 2], hbsb[bi % 2]
    _ld(P, "sp", hb[:, :, 0:n], hT[:, :, t0:t0 + n], [hbb], reads=[hTb])
    return hb, hbb


def na_part(P, dr, ps, psb, hT, hTb, wfm, wtm, wb, blocks, out, outs, need_ctx_out, cb, ident):
    Cn = consts()
    drs, types = Cn["n_drs"], Cn["n_types"]
    nqT = A(P, [64, NTOK], BF16); nkT = A(P, [64, NTOK], BF16); nvT = A(P, [128, NCH, 64], BF16)
    nqb, nkb, nvb = P.buf("nq"), P.buf("nk"), P.buf("nv")
    nT = A(P, [64, NTOK], BF16); nTb = P.buf("nT")
    bias = A(P, [128, 5, 832], F32); biasb = P.buf("bias")
    mask = A(P, [128, 5, 832], F32)
    P.op("pool", lambda h: h.memset(bias[:], 0.0), writes=[biasb])
    maskb = P.buf()
    _ld(P, "sp", mask[:], dr["n_mask"].rearrange("t p c -> p t c"), [maskb])
    for ti in range(5):
        for qr in range(2):
            for i in range(9):
                _ld(P, "sp" if (i % 2) else "act", bias[qr * 64:(qr + 1) * 64, ti, i * 64:(i + 1) * 64], dr["n_toep"][int(drs[ti, qr, i])], [biasb])
    _tt(P, "dve", bias[:], bias[:], mask[:], ALU.add, [biasb, maskb], [biasb])
    mark = P.a_cur
    hbs = [A(P, [128, KC, 512], BF16) for _ in range(2)]
    hbsb = [P.buf(), P.buf()]
    cnt = 0
    for bi, (t0, n, r) in enumerate(blocks):
        hb, hbb = load_h(P, hT, hTb, hbs, hbsb, bi, t0, n)
        for g, dst, dstb in ((7, nqT, nqb), (8, nkT, nkb)):
            pi_ = cnt % 4
            cnt += 1
            for k in range(KC):
                _mm(P, ps[pi_][0:64, 0:n], wfm[:, k, g * 64:(g + 1) * 64], hb[:, k, 0:n], k == 0, k == KC - 1, [wb, hbb], [psb[pi_]])
            _cp(P, "act" if g == 7 else "dve", dst[:, t0:t0 + n], ps[pi_][0:64, 0:n], [psb[pi_]], [dstb])
        for tt in range(n // 128):
            pi_ = 4 + (tt % 2)
            for k in range(KC):
                _mm(P, ps[pi_][:, 0:64], hb[:, k, tt * 128:(tt + 1) * 128], wtm[:, k, 192:256], k == 0, k == KC - 1, [wb, hbb], [psb[pi_]])
            _cp(P, "act", nvT[:, t0 // 128 + tt, :], ps[pi_][:, 0:64], [psb[pi_]], [nvb])
    barrier(P)
    P.a_cur = mark
    NB4 = 4
    s_t = [A(P, [128, 832], F32) for _ in range(NB4)]; s_b = [P.buf() for _ in range(NB4)]
    p_t = [A(P, [128, 832], BF16) for _ in range(NB4)]; p_b = [P.buf() for _ in range(NB4)]
    pT = [A(P, [128, 7, 128], BF16) for _ in range(NB4)]; pTb = [P.buf() for _ in range(NB4)]
    st_ = [A(P, [128, 4], F32) for _ in range(NB4)]; stb = [P.buf() for _ in range(NB4)]
    SC = 0.125

    def softmax_pv(qi, ncols, pv_list, o_ps, o_psb, o_cols, sbi, s4=None):
        if s4 is None:
            s4 = sbi
        s, sb_ = s_t[s4], s_b[s4]
        sm, smb = st_[s4], stb[s4]
        P.op("dve", lambda h: h.reduce_max(out=sm[:, 1:2], in_=s[:, 0:ncols], axis=AX.X, negate=True), reads=[sb_], writes=[smb])
        _act(P, s[:, 0:ncols], s[:, 0:ncols], AF.Exp, [sb_, smb], [sb_], bias=sm[:, 1:2], scale=1.0)
        P.op("dve", lambda h: h.reduce_sum(out=sm[:, 2:3], in_=s[:, 0:ncols], axis=AX.X), reads=[sb_], writes=[smb])
        P.op("dve", lambda h: h.reciprocal(out=sm[:, 3:4], in_=sm[:, 2:3]), reads=[smb], writes=[smb])
        p, pb = p_t[s4], p_b[s4]
        _ts(P, "dve", p[:, 0:ncols], s[:, 0:ncols], sm[:, 3:4], None, ALU.mult, None, [sb_, smb], [pb])
        tp = ps[4 + sbi][:, :].bitcast(BF16)
        for ci, (c0, nk, tile) in enumerate(pv_list):
            _tr(P, tp[0:nk, ci * 128:(ci + 1) * 128], p[:, c0:c0 + nk], ident[:, :], [pb, cb], [psb[4 + sbi]])
        nchk = len(pv_list)
        pt_, ptb = pT[s4], pTb[s4]
        _cp(P, "act", pt_[:, 0:nchk, :], tp[:, 0:nchk * 128].rearrange("p (a b) -> p a b", b=128), [psb[4 + sbi]], [ptb])
        for ci, (c0, nk, tile) in enumerate(pv_list):
            _mm(P, o_ps[0:64, o_cols:o_cols + 128], nvT[0:nk, tile, :], pt_[0:nk, ci, :], ci == 0, ci == nchk - 1, [nvb, ptb], [o_psb])

    for rp in range(64):
        ti = {0: 0, 1: 1, 62: 3, 63: 4}.get(rp, 2)
        r0 = 2 * rp
        if ti == 2:
            R0, nr = r0 - 4, 9
        else:
            R0, nr = types[ti][1], 8
        tq = 256 + 128 * rp
        kb_ = 256 + 64 * R0
        sbi = rp % 2
        s1, s2 = ps[sbi], ps[2 + sbi]
        _mm(P, s1[:, 0:512], nqT[:, tq:tq + 128], nkT[:, kb_:kb_ + 512], True, True, [nqb, nkb], [psb[sbi]])
        kb2 = kb_ + 512 if nr == 9 else kb_
        _mm(P, s2[:, 0:64], nqT[:, tq:tq + 128], nkT[:, kb2:kb2 + 64], True, True, [nqb, nkb], [psb[2 + sbi]])
        _mm(P, s2[:, 64:320], nqT[:, tq:tq + 128], nkT[:, 0:256], True, True, [nqb, nkb], [psb[2 + sbi]])
        s4 = rp % NB4
        s = s_t[s4]
        _stt(P, s[:, 0:512], s1[:, 0:512], SC, bias[:, ti, 0:512], ALU.mult, ALU.add, [psb[sbi], biasb], [s_b[s4]])
        _stt(P, s[:, 512:832], s2[:, 0:320], SC, bias[:, ti, 512:832], ALU.mult, ALU.add, [psb[2 + sbi], biasb], [s_b[s4]])
        t_base = 2 + R0 // 2
        pv = [(128 * j, 128, t_base + j) for j in range(4)]
        if nr == 9:
            pv.append((512, 64, t_base + 4))
        pv += [(576, 128, 0), (704, 128, 1)]
        jj = rp % 4
        softmax_pv(rp, 832, pv, ps[6], psb[6], jj * 128, sbi, s4)
        if jj == 3:
            _cp(P, "dve", nT[:, 256 + 512 * (rp // 4):256 + 512 * (rp // 4 + 1)], ps[6][0:64, 0:512], [psb[6]], [nTb])
    if need_ctx_out:
        for qt in range(2):
            sbi = qt
            _mm(P, ps[sbi][:, 0:256], nqT[:, qt * 128:(qt + 1) * 128], nkT[:, 0:256], True, True, [nqb, nkb], [psb[sbi]])
            _ts(P, "dve", s_t[sbi][:, 0:256], ps[sbi][:, 0:256], SC, None, ALU.mult, None, [psb[sbi]], [s_b[sbi]])
            softmax_pv(qt, 256, [(0, 128, 0), (128, 128, 1)], ps[7], psb[7], qt * 128, sbi)
        _cp(P, "dve", nT[:, 0:256], ps[7][0:64, 0:256], [psb[7]], [nTb])
    else:
        P.op("dve", lambda h: h.memset(nT[:, 0:256], 0.0), writes=[nTb])
    outs.append(_ld(P, "sp", out[3, :, :], nT[:, :], [P.buf()], reads=[nTb]))


def ret_part(P, dr, ps, psb, hT, hTb, wfm, wtm, wb, blocks, out, outs, need_ctx_out, cb, ones):
    KS = 0.125
    qT = A(P, [64, NTOK], BF16); kT = A(P, [64, NTOK], BF16); gT = A(P, [64, NTOK], BF16)
    qb_, kb_, gb_ = P.buf("q"), P.buf("k"), P.buf("g")
    rvT = A(P, [128, NCH, 64], BF16); rvb = P.buf("rv")
    Sbf = [A(P, [64, NCH, 64], BF16) for _ in range(2)]
    rc = P.buf("retc")
    dec = A(P, [128, 2], F32); lg = A(P, [128, 2], F32); jcol = A(P, [128, 2], F32); kdec = A(P, [128, 2], F32); g128 = A(P, [128, 2], F32)
    dist = A(P, [128, 128], F32); msk = A(P, [128, 2, 128], F32); DT = A(P, [128, 128], F32); DT2 = A(P, [128, 128], F32)
    irow = A(P, [64, 2, 128], F32); qdec = A(P, [64, 2, 128], F32)
    gn = A(P, [64, 1], F32); o64 = A(P, [64, 64], F32)
    _ld(P, "sp", dec[:], dr["r_dec"][:, :], [rc])
    _ld(P, "sp", jcol[:], dr["r_jcol"][:, :], [rc])
    _ld(P, "sp", dist[:], dr["r_dist"][:, :], [rc])
    _ld(P, "sp", msk[:], dr["r_mask"].rearrange("d j i -> j d i"), [rc])
    _ld(P, "sp", irow[:], dr["r_irow"].rearrange("d p i -> p d i"), [rc])
    _ld(P, "sp", gn[:], dr["r_gn"][:, :], [rc])
    P.op("dve", lambda h: h.memset(o64[:], 1.0 / 64), writes=[rc])
    _act(P, lg[:], dec[:], AF.Exp, [rc], [rc], scale=-1.0)
    _act(P, lg[:], lg[:], AF.Ln, [rc], [rc], bias=P.one_t[:, 0:1], scale=1.0)
    _ts(P, "dve", lg[:], lg[:], -1.0, None, ALU.mult, None, [rc], [rc])
    for dd in range(2):
        _act(P, kdec[:, dd:dd + 1], jcol[:, dd:dd + 1], AF.Exp, [rc], [rc], scale=lg[:, dd:dd + 1])
        _act(P, g128[:, dd:dd + 1], lg[:, dd:dd + 1], AF.Exp, [rc], [rc], scale=128.0)
        _act(P, qdec[:, dd, :], irow[:, dd, :], AF.Exp, [rc], [rc], scale=lg[0:64, dd:dd + 1])
    _ts(P, "dve", kdec[:], kdec[:], KS, None, ALU.mult, None, [rc], [rc])
    _act(P, DT[:], dist[:], AF.Exp, [rc], [rc], scale=lg[:, 0:1])
    _tt(P, "dve", DT[:], DT[:], msk[:, 0, :], ALU.mult, [rc], [rc])
    _act(P, DT2[:], dist[:], AF.Exp, [rc], [rc], scale=lg[:, 1:2])
    _tt(P, "dve", DT2[:], DT2[:], msk[:, 1, :], ALU.mult, [rc], [rc])
    _tt(P, "dve", DT[:], DT[:], DT2[:], ALU.add, [rc], [rc])
    if RET_STOP <= 0:
        return
    mark_k = P.a_cur
    kd = [A(P, [128, NCH, 64], BF16) for _ in range(2)]
    kdb = [P.buf(), P.buf()]
    mark = P.a_cur
    hbs = [A(P, [128, KC, 512], BF16) for _ in range(2)]
    hbsb = [P.buf(), P.buf()]
    cF = [A(P, [64, 512], F32) for _ in range(2)]; sF = [A(P, [64, 512], F32) for _ in range(2)]
    cTt = [A(P, [128, 4, 64], F32) for _ in range(2)]; sTt = [A(P, [128, 4, 64], F32) for _ in range(2)]
    tabb = [P.buf(), P.buf()]
    t1 = [A(P, [128, 512], F32) for _ in range(2)]; t1b = [P.buf(), P.buf()]
    t2 = [A(P, [128, 512], F32) for _ in range(2)]; t2b = [P.buf(), P.buf()]
    cnt = 0
    for bi, (t0, n, r) in enumerate(blocks):
        hb, hbb = load_h(P, hT, hTb, hbs, hbsb, bi, t0, n)
        lat = (r == 0)
        tb_ = tabb[bi % 2]
        if lat:
            m0 = t0 - 256
            _ld(P, "sp", cF[bi % 2][:, :], dr["r_cosF"][:, m0:m0 + 512], [tb_])
            _ld(P, "sp", sF[bi % 2][:, :], dr["r_sinF"][:, m0:m0 + 512], [tb_])
            _ld(P, "sp", cTt[bi % 2][:, :, :], dr["r_cosT"][:, m0 // 128:m0 // 128 + 4, :], [tb_])
            _ld(P, "sp", sTt[bi % 2][:, :, :], dr["r_sinT"][:, m0 // 128:m0 // 128 + 4, :], [tb_])

        def proj(g, pi_):
            for k in range(KC):
                _mm(P, ps[pi_][0:64, 0:n], wfm[:, k, g * 64:(g + 1) * 64], hb[:, k, 0:n], k == 0, k == KC - 1, [wb, hbb], [psb[pi_]])
        for (g, gsw, dst, dstb, scl) in ((2, 4, qT, qb_, 1.0), (3, 5, kT, kb_, KS)):
            if 'qk' in SKIP:
                continue
            proj(g, 0)
            if lat:
                proj(gsw, 1)
                i2 = cnt % 2
                cnt += 1
                _stt(P, t1[i2][0:64, 0:n], ps[0][0:64, 0:n], scl, cF[bi % 2][:, 0:n], ALU.mult, ALU.mult, [psb[0], tb_], [t1b[i2]])
                _stt(P, t2[i2][0:64, 0:n], ps[1][0:64, 0:n], scl, sF[bi % 2][:, 0:n], ALU.mult, ALU.mult, [psb[1], tb_], [t2b[i2]])
                _tt(P, "pool", dst[:, t0:t0 + n], t1[i2][0:64, 0:n], t2[i2][0:64, 0:n], ALU.add, [t1b[i2], t2b[i2]], [dstb])
            else:
                _act(P, dst[:, t0:t0 + n], ps[0][0:64, 0:n], AF.Copy, [psb[0]], [dstb], scale=scl)
        proj(6, 2)
        _cp(P, "act", gT[:, t0:t0 + n], ps[2][0:64, 0:n], [psb[2]], [gb_])
        for tt in range(n // 128):
            if 'tm' in SKIP:
                continue
            pi_ = 4 + (tt % 2)
            tile = t0 // 128 + tt
            for k in range(KC):
                _mm(P, ps[pi_][:, 0:192], hb[:, k, tt * 128:(tt + 1) * 128], wtm[:, k, 0:192], k == 0, k == KC - 1, [wb, hbb], [psb[pi_]])
            _cp(P, "act", rvT[:, tile, :], ps[pi_][:, 128:192], [psb[pi_]], [rvb])
            if 'kd' in SKIP:
                continue
            if ('kl' in SKIP and lat) or ('kc' in SKIP and not lat):
                continue
            if lat:
                i2 = cnt % 2
                cnt += 1
                _tt(P, "dve", t1[i2][:, 0:64], ps[pi_][:, 0:64], cTt[bi % 2][:, tt, :], ALU.mult, [psb[pi_], tb_], [t1b[i2]])
                _tt(P, "dve", t2[i2][:, 0:64], ps[pi_][:, 64:128], sTt[bi % 2][:, tt, :], ALU.mult, [psb[pi_], tb_], [t2b[i2]])
                if 'k1' in SKIP:
                    continue
                _tt(P, "dve", t1[i2][:, 0:64], t1[i2][:, 0:64], t2[i2][:, 0:64], ALU.add, [t1b[i2], t2b[i2]], [t1b[i2]])
                if 'k2' in SKIP:
                    continue
                for dd in range(2):
                    _act(P, kd[dd][:, tile, :], t1[i2][:, 0:64], AF.Identity, [t1b[i2], rc], [kdb[dd]], scale=kdec[:, dd:dd + 1])
            else:
                for dd in range(2):
                    _act(P, kd[dd][:, tile, :], ps[pi_][:, 0:64], AF.Identity, [psb[pi_], rc], [kdb[dd]], scale=kdec[:, dd:dd + 1])
    barrier(P)
    P.a_cur = mark
    if RET_STOP <= 1:
        return
    S32 = [A(P, [64, NCH, 64], F32) for _ in range(2)]
    Sb = [P.buf(), P.buf()]
    orders = []
    for dd in range(2):
        pos = pos_of(dd)
        orders.append(sorted(range(NCH), key=lambda c, pos=pos: pos[c]))
        c0 = orders[dd][0]
        P.op("dve", lambda h, dd=dd, c0=c0: h.memset(S32[dd][:, c0, :], 0.0), writes=[Sb[dd]])
    for idx in range(NCH - 1):
        for dd in range(2):
            c, nxt = orders[dd][idx], orders[dd][idx + 1]
            pi_ = 2 * dd + (idx // 8) % 2
            sl = idx % 8
            _mm(P, ps[pi_][0:64, sl * 64:(sl + 1) * 64], kd[dd][:, c, :], rvT[:, c, :], True, True, [kdb[dd], rvb], [psb[pi_]])
            _stt(P, S32[dd][:, nxt, :], S32[dd][:, c, :], g128[0:64, dd:dd + 1], ps[pi_][0:64, sl * 64:(sl + 1) * 64], ALU.mult, ALU.add, [Sb[dd], psb[pi_], rc], [Sb[dd]])
    for dd in range(2):
        _cp(P, "act", Sbf[dd][:], S32[dd][:], [Sb[dd]], [Sb[dd]])
    barrier(P)
    P.a_cur = mark_k
    if RET_STOP <= 2:
        return
    oT = A(P, [64, NTOK], F32); oTb = P.buf("oT")
    sc = [A(P, [128, 128], BF16) for _ in range(2)]; scb = [P.buf(), P.buf()]
    qd = [[A(P, [64, 128], BF16) for _ in range(2)] for _ in range(2)]
    qdb = [[P.buf(), P.buf()] for _ in range(2)]
    c_start = 0 if need_ctx_out else 2
    if not need_ctx_out:
        P.op("pool", lambda h: h.memset(oT[:, 0:256], 0.0), writes=[oTb])
    for c in range(c_start, NCH):
        tau = 128 * c
        i2 = c % 2
        _mm(P, ps[i2][:, 0:128], kT[:, tau:tau + 128], qT[:, tau:tau + 128], True, True, [kb_, qb_], [psb[i2]])
        _tt(P, "dve", sc[i2][:, :], ps[i2][:, 0:128], DT[:, :], ALU.mult, [psb[i2], rc], [scb[i2]])
        for dd in range(2):
            _tt(P, "pool", qd[dd][i2][:, :], qT[:, tau:tau + 128], qdec[:, dd, :], ALU.mult, [qb_, rc], [qdb[dd][i2]])
        jj = c % 4
        po = ps[4 + (c // 4) % 2]
        pob = psb[4 + (c // 4) % 2]
        _mm(P, po[0:64, jj * 128:(jj + 1) * 128], rvT[:, c, :], sc[i2][:, :], True, False, [rvb, scb[i2]], [pob])
        _mm(P, po[0:64, jj * 128:(jj + 1) * 128], Sbf[0][:, c, :], qd[0][i2][:, :], False, False, [Sb[0], qdb[0][i2]], [pob])
        _mm(P, po[0:64, jj * 128:(jj + 1) * 128], Sbf[1][:, c, :], qd[1][i2][:, :], False, True, [Sb[1], qdb[1][i2]], [pob])
        if jj == 3 or c == NCH - 1:
            b0 = (c // 4) * 512
            wid = (jj + 1) * 128
            lo = 0
            if (not need_ctx_out) and c // 4 == 0:
                lo = 256
            _cp(P, "act", oT[:, b0 + lo:b0 + wid], po[0:64, lo:wid], [pob], [oTb])
    if RET_STOP <= 3:
        return
    rT = A(P, [64, NTOK], BF16); rTb = P.buf("rT")
    o64b = A(P, [64, 64], BF16)
    P.op("dve", lambda h: h.memset(o64b[:], 1.0 / 64), writes=[rc])
    obf = [A(P, [64, 512], BF16) for _ in range(2)]; obfb = [P.buf(), P.buf()]
    cen = [A(P, [64, 512], F32) for _ in range(2)]; cenb = [P.buf(), P.buf()]
    sq_ = [A(P, [64, 512], BF16) for _ in range(2)]; sqb_ = [P.buf(), P.buf()]
    rs_ = [A(P, [64, 512], F32) for _ in range(2)]; rsb_ = [P.buf(), P.buf()]
    sg_ = [A(P, [64, 512], F32) for _ in range(2)]; sgb_ = [P.buf(), P.buf()]
    nblk = (NTOK + 511) // 512
    for bi in range(nblk):
        t0 = bi * 512
        n = min(512, NTOK - t0)
        i2 = bi % 2
        _cp(P, "act", obf[i2][:, 0:n], oT[:, t0:t0 + n], [oTb], [obfb[i2]])
        _mm(P, ps[2 + i2][0:64, 0:n], o64b[:, :], obf[i2][:, 0:n], True, True, [obfb[i2], rc], [psb[2 + i2]])
        _tt(P, "dve", cen[i2][:, 0:n], oT[:, t0:t0 + n], ps[2 + i2][0:64, 0:n], ALU.subtract, [oTb, psb[2 + i2]], [cenb[i2]])
        _act(P, sq_[i2][:, 0:n], cen[i2][:, 0:n], AF.Square, [cenb[i2]], [sqb_[i2]])
        _mm(P, ps[6 + i2][0:64, 0:n], o64b[:, :], sq_[i2][:, 0:n], True, True, [sqb_[i2], rc], [psb[6 + i2]])
        _act(P, rs_[i2][:, 0:n], ps[6 + i2][0:64, 0:n], AF.Ln, [psb[6 + i2]], [rsb_[i2]], bias=P.eps_t[0:64, 0:1], scale=1.0)
        _act(P, rs_[i2][:, 0:n], rs_[i2][:, 0:n], AF.Exp, [rsb_[i2]], [rsb_[i2]], scale=-0.5)
        _tt(P, "dve", cen[i2][:, 0:n], cen[i2][:, 0:n], rs_[i2][:, 0:n], ALU.mult, [cenb[i2], rsb_[i2]], [cenb[i2]])
        _act(P, sg_[i2][:, 0:n], gT[:, t0:t0 + n], AF.Silu, [gb_], [sgb_[i2]])
        _stt(P, rT[:, t0:t0 + n], cen[i2][:, 0:n], gn[:, 0:1], sg_[i2][:, 0:n], ALU.mult, ALU.mult, [cenb[i2], sgb_[i2], rc], [rTb])
    outs.append(_ld(P, "sp", out[2, :, :], rT[:, :], [P.buf()], reads=[rTb]))


def s5_part(P, dr, ps, psb, sT, sTb, out, outs, need_ctx_out, cb):
    pb = P.buf("s5param")

    def cplx_prep(are, aim, ldt, mk):
        T = {k: mk() for k in ("dt", "ar", "ai", "mag", "ph", "tmp", "sn", "cs", "abr", "abi", "nr", "den", "cr", "ci", "u")}
        ident_v = lambda t: t
        _act(P, T["dt"], ldt, AF.Exp, [pb], [pb])
        _tt(P, "dve", T["ar"], are, T["dt"], ALU.mult, [pb], [pb])
        _tt(P, "dve", T["ai"], aim, T["dt"], ALU.mult, [pb], [pb])
        _act(P, T["mag"], T["ar"], AF.Exp, [pb], [pb])
        _cp(P, "dve", T["ph"], T["ai"], [pb], [pb])
        range_reduce_sincos(P, T["ph"], T["sn"], T["cs"], T["tmp"], ident_v, pb)
        _tt(P, "dve", T["abr"], T["mag"], T["cs"], ALU.mult, [pb], [pb])
        _tt(P, "dve", T["abi"], T["mag"], T["sn"], ALU.mult, [pb], [pb])
        _ts(P, "dve", T["nr"], T["abr"], -1.0, None, ALU.add, None, [pb], [pb])
        _tt(P, "dve", T["den"], are, are, ALU.mult, [pb], [pb])
        _tt(P, "dve", T["u"], aim, aim, ALU.mult, [pb], [pb])
        _tt(P, "dve", T["den"], T["den"], T["u"], ALU.add, [pb], [pb])
        P.op("dve", lambda h: h.reciprocal(out=T["den"], in_=T["den"]), reads=[pb], writes=[pb])
        _tt(P, "dve", T["cr"], T["nr"], are, ALU.mult, [pb], [pb])
        _tt(P, "dve", T["u"], T["abi"], aim, ALU.mult, [pb], [pb])
        _tt(P, "dve", T["cr"], T["cr"], T["u"], ALU.add, [pb], [pb])
        _tt(P, "dve", T["cr"], T["cr"], T["den"], ALU.mult, [pb], [pb])
        _tt(P, "dve", T["ci"], T["abi"], are, ALU.mult, [pb], [pb])
        _tt(P, "dve", T["u"], T["nr"], aim, ALU.mult, [pb], [pb])
        _tt(P, "dve", T["ci"], T["ci"], T["u"], ALU.subtract, [pb], [pb])
        _tt(P, "dve", T["ci"], T["ci"], T["den"], ALU.mult, [pb], [pb])
        return T

    p_sm = A(P, [128, 2, 2, 3], F32); p_row = A(P, [128, 2, 3, 256], F32); p_hs = A(P, [64, 2, 3, 64], F32)
    Bhs = A(P, [64, 2, 2, 64], F32); Csm = A(P, [128, 2, 2, 2, 16], F32); dvec = A(P, [64, 1], F32)
    jrow = A(P, [128, 129], F32); jcol = A(P, [128, 1], F32); njcol = A(P, [128, 1], F32)
    LT = A(P, [128, 2, 128], BF16); mrow = A(P, [64, 4], F32); msm = A(P, [128, 2, 4], F32)
    for t_, src in ((p_sm[:], dr["s_sm"][:, :, :, :]), (p_row[:], dr["s_row"][:, :, :, :]), (p_hs[:], dr["s_hs"][:, :, :, :]), (Bhs[:], dr["s_B"][:, :, :, :]),
                    (Csm[:], dr["s_C"][:, :, :, :, :]), (dvec[:], dr["s_d"][:, :]), (jrow[:], dr["s_jrow"][:, :]), (jcol[:], dr["s_jcol"][:, :]),
                    (LT[:], dr["s_LT"].rearrange("d j i -> j d i")), (mrow[:], dr["s_mrow"][:, :]), (msm[:], dr["s_msm"][:, :, :])):
        _ld(P, "sp", t_, src, [pb])
    _ts(P, "dve", njcol[:], jcol[:], -1.0, None, ALU.mult, None, [pb], [pb])
    ones_col = A(P, [128, 1], BF16)
    P.op("dve", lambda h: h.memset(ones_col[:], 1.0), writes=[pb])

    BD = [A(P, [64, 512], BF16) for _ in range(2)]
    CT = [A(P, [128, 4, 64], BF16) for _ in range(2)]
    mark_prep = P.a_cur
    for dd in range(2):
        P.a_cur = mark_prep
        Ths = cplx_prep(p_hs[:, dd, 0, :], p_hs[:, dd, 1, :], p_hs[:, dd, 2, :], lambda: A(P, [64, 64], F32)[:, :])
        bbr = A(P, [64, 64], F32); bbi = A(P, [64, 64], F32); uu = A(P, [64, 64], F32)
        _tt(P, "dve", bbr[:], Ths["cr"], Bhs[:, dd, 0, :], ALU.mult, [pb], [pb])
        _tt(P, "dve", uu[:], Ths["ci"], Bhs[:, dd, 1, :], ALU.mult, [pb], [pb])
        _tt(P, "dve", bbr[:], bbr[:], uu[:], ALU.subtract, [pb], [pb])
        _tt(P, "dve", bbi[:], Ths["cr"], Bhs[:, dd, 1, :], ALU.mult, [pb], [pb])
        _tt(P, "dve", uu[:], Ths["ci"], Bhs[:, dd, 0, :], ALU.mult, [pb], [pb])
        _tt(P, "dve", bbi[:], bbi[:], uu[:], ALU.add, [pb], [pb])
        for g in range(4):
            _ts(P, "dve", BD[dd][:, g * 64:(g + 1) * 64], bbr[:], mrow[:, g:g + 1], None, ALU.mult, None, [pb], [pb])
            _ts(P, "dve", BD[dd][:, 256 + g * 64:256 + (g + 1) * 64], bbi[:], mrow[:, g:g + 1], None, ALU.mult, None, [pb], [pb])
        for ri in range(2):
            for st in range(2):
                for g in range(4):
                    _ts(P, "dve", CT[dd][:, ri * 2 + st, g * 16:(g + 1) * 16], Csm[:, dd, st, ri, :], msm[:, st, g:g + 1], (1.0 if ri == 0 else -1.0), ALU.mult, ALU.mult, [pb], [pb])
    P.a_cur = mark_prep
    TA = [[A(P, [128, 2, 129], F32) for _ in range(2)] for _ in range(2)]
    TW = [[A(P, [128, 2, 129], F32) for _ in range(2)] for _ in range(2)]
    PRE = [[A(P, [128, 256], F32) for _ in range(2)] for _ in range(2)]
    mark_t = P.a_cur
    for dd in range(2):
        P.a_cur = mark_t
        dt_ = A(P, [128, 2], F32); ar = A(P, [128, 2], F32); ai = A(P, [128, 2], F32); nar = A(P, [128, 2], F32)
        _act(P, dt_[:], p_sm[:, dd, :, 2], AF.Exp, [pb], [pb])
        _tt(P, "dve", ar[:], p_sm[:, dd, :, 0], dt_[:], ALU.mult, [pb], [pb])
        _tt(P, "dve", ai[:], p_sm[:, dd, :, 1], dt_[:], ALU.mult, [pb], [pb])
        _ts(P, "dve", nar[:], ar[:], -1.0, None, ALU.mult, None, [pb], [pb])
        mark_st = P.a_cur
        for st in range(2):
            P.a_cur = mark_st
            ph = A(P, [128, 129], F32); tmp = A(P, [128, 129], F32); sn = A(P, [128, 129], F32); cs = A(P, [128, 129], F32)
            mp = A(P, [128, 129], F32); mn = A(P, [128, 129], F32)
            _ts(P, "dve", ph[:], jrow[:], ai[:, st:st + 1], None, ALU.mult, None, [pb], [pb])
            range_reduce_sincos(P, ph[:], sn[:], cs[:], tmp[:], (lambda t: t), pb)
            _act(P, mp[:], jrow[:], AF.Exp, [pb], [pb], scale=ar[:, st:st + 1])
            _act(P, mn[:], jrow[:], AF.Exp, [pb], [pb], scale=nar[:, st:st + 1])
            _tt(P, "dve", TA[dd][0][:, st, :], mp[:], cs[:], ALU.mult, [pb], [pb])
            _tt(P, "dve", TA[dd][1][:, st, :], mp[:], sn[:], ALU.mult, [pb], [pb])
            _tt(P, "dve", TW[dd][0][:, st, :], mn[:], cs[:], ALU.mult, [pb], [pb])
            _stt(P, TW[dd][1][:, st, :], mn[:], -1.0, sn[:], ALU.mult, ALU.mult, [pb], [pb])
        P.a_cur = mark_t
        dtr = A(P, [128, 256], F32); arr = A(P, [128, 256], F32); air = A(P, [128, 256], F32)
        ph = A(P, [128, 256], F32); tmp = A(P, [128, 256], F32); sn = A(P, [128, 256], F32); cs = A(P, [128, 256], F32); mg = A(P, [128, 256], F32)
        _act(P, dtr[:], p_row[:, dd, 2, :], AF.Exp, [pb], [pb])
        _tt(P, "dve", arr[:], p_row[:, dd, 0, :], dtr[:], ALU.mult, [pb], [pb])
        _tt(P, "dve", air[:], p_row[:, dd, 1, :], dtr[:], ALU.mult, [pb], [pb])
        _ts(P, "dve", ph[:], air[:], jcol[:, 0:1], None, ALU.mult, None, [pb], [pb])
        range_reduce_sincos(P, ph[:], sn[:], cs[:], tmp[:], (lambda t: t), pb)
        _act(P, mg[:], arr[:], AF.Exp, [pb], [pb], scale=(njcol if dd == 0 else jcol)[:, 0:1])
        _tt(P, "dve", PRE[dd][0][:], mg[:], cs[:], ALU.mult, [pb], [pb])
        _stt(P, PRE[dd][1][:], mg[:], (-1.0 if dd == 0 else 1.0), sn[:], ALU.mult, ALU.mult, [pb], [pb])
        P.a_cur = mark_t
    barrier(P)
    P.a_cur = mark_t
    Xt = A(P, [128, NCH, 512], BF16); Xtb = [P.buf() for _ in range(NCH)]
    yacc = A(P, [64, NTOK], F32); yb = P.buf("yacc")
    E = A(P, [128, 4, NCH], F32); Eb = P.buf("E")
    H = [[A(P, [128, 2, NCH], F32) for _ in range(2)] for _ in range(2)]
    Hb = P.buf("H")
    cv = [A(P, [128, 2, NCH], F32) for _ in range(2)]
    pw = A(P, [128, 2, 8], F32)
    tq = [A(P, [128, 256], F32) for _ in range(8)]; tqb = [P.buf() for _ in range(8)]
    hs_ = [A(P, [128, 4, 128], BF16) for _ in range(2)]; hsb = [P.buf(), P.buf()]
    uq = [A(P, [128, 128], F32) for _ in range(16)]; uqb = [P.buf() for _ in range(16)]
    for dd in range(2):
        pos = pos_of(dd)
        for c in range(NCH):
            tau = 128 * c
            px = ps[c % 2]; pxb = psb[c % 2]
            _mm(P, px[:, 0:512], sT[:, tau:tau + 128], BD[dd][:, :], True, True, [sTb, pb], [pxb])
            i2 = (c % 2) * 4
            _tt(P, "dve", tq[i2][:, :], px[:, 0:256], PRE[dd][0][:, :], ALU.mult, [pxb, pb], [tqb[i2]])
            _tt(P, "dve", tq[i2 + 1][:, :], px[:, 256:512], PRE[dd][1][:, :], ALU.mult, [pxb, pb], [tqb[i2 + 1]])
            _tt(P, "dve", tq[i2 + 2][:, :], px[:, 0:256], PRE[dd][1][:, :], ALU.mult, [pxb, pb], [tqb[i2 + 2]])
            _tt(P, "dve", tq[i2 + 3][:, :], px[:, 256:512], PRE[dd][0][:, :], ALU.mult, [pxb, pb], [tqb[i2 + 3]])
            _tt(P, "pool", Xt[:, c, 0:256], tq[i2][:, :], tq[i2 + 1][:, :], ALU.subtract, [tqb[i2], tqb[i2 + 1]], [Xtb[c]])
            _tt(P, "pool", Xt[:, c, 256:512], tq[i2 + 2][:, :], tq[i2 + 3][:, :], ALU.add, [tqb[i2 + 2], tqb[i2 + 3]], [Xtb[c]])
            for tl in range(4):
                col = tl * NCH + pos[c]
                _mm(P, ps[6][:, col:col + 1], Xt[:, c, tl * 128:(tl + 1) * 128], ones_col[:, :], True, True, [Xtb[c], pb], [psb[6]])
        _cp(P, "dve", E[:], ps[6][:, 0:4 * NCH].rearrange("p (a b) -> p a b", b=NCH), [psb[6]], [Eb])
        H0r, H0i = H[0][0], H[0][1]
        if dd == 0:
            for st in range(2):
                a_r, a_i = TA[0][0][:, st, 127:128], TA[0][1][:, st, 127:128]
                _ts(P, "dve", uq[0][:, 0:NCH], E[:, 2 + st, :], a_i, None, ALU.mult, None, [Eb, pb], [uqb[0]])
                _stt(P, H0r[:, st, :], E[:, st, :], a_r, uq[0][:, 0:NCH], ALU.mult, ALU.subtract, [Eb, pb, uqb[0]], [Hb])
                _ts(P, "dve", uq[1][:, 0:NCH], E[:, st, :], a_i, None, ALU.mult, None, [Eb, pb], [uqb[1]])
                _stt(P, H0i[:, st, :], E[:, 2 + st, :], a_r, uq[1][:, 0:NCH], ALU.mult, ALU.add, [Eb, pb, uqb[1]], [Hb])
        else:
            _cp(P, "dve", H0r[:], E[:, 0:2, :], [Eb], [Hb])
            _cp(P, "dve", H0i[:], E[:, 2:4, :], [Eb], [Hb])
        _cp(P, "dve", pw[:, :, 0], TA[dd][0][:, :, 128], [pb], [Hb])
        _cp(P, "dve", pw[:, :, 1], TA[dd][1][:, :, 128], [pb], [Hb])
        cur = 0
        d = 1
        while d < NCH:
            _ts(P, "dve", pw[:, :, 2], pw[:, :, 1], -1.0, None, ALU.mult, None, [Hb], [Hb])
            o_, n_ = H[cur], H[1 - cur]
            for ri in range(2):
                _cp(P, "dve", n_[ri][:, :, 0:d], o_[ri][:, :, 0:d], [Hb], [Hb])
            for st in range(2):
                pr, pi, npi = pw[:, st, 0:1], pw[:, st, 1:2], pw[:, st, 2:3]
                m = NCH - d
                _stt(P, uq[0][:, 0:m], o_[0][:, st, 0:m], pr, o_[0][:, st, d:NCH], ALU.mult, ALU.add, [Hb], [uqb[0]])
                _stt(P, n_[0][:, st, d:NCH], o_[1][:, st, 0:m], npi, uq[0][:, 0:m], ALU.mult, ALU.add, [Hb, uqb[0]], [Hb])
                _stt(P, uq[1][:, 0:m], o_[1][:, st, 0:m], pr, o_[1][:, st, d:NCH], ALU.mult, ALU.add, [Hb], [uqb[1]])
                _stt(P, n_[1][:, st, d:NCH], o_[0][:, st, 0:m], pi, uq[1][:, 0:m], ALU.mult, ALU.add, [Hb, uqb[1]], [Hb])
            _tt(P, "dve", pw[:, :, 3], pw[:, :, 0], pw[:, :, 0], ALU.mult, [Hb], [Hb])
            _tt(P, "dve", pw[:, :, 4], pw[:, :, 1], pw[:, :, 1], ALU.mult, [Hb], [Hb])
            _tt(P, "dve", pw[:, :, 5], pw[:, :, 0], pw[:, :, 1], ALU.mult, [Hb], [Hb])
            _tt(P, "dve", pw[:, :, 0], pw[:, :, 3], pw[:, :, 4], ALU.subtract, [Hb], [Hb])
            _ts(P, "dve", pw[:, :, 1], pw[:, :, 5], 2.0, None, ALU.mult, None, [Hb], [Hb])
            cur = 1 - cur
            d *= 2
        Hf = H[cur]
        kidx = 1 if dd == 0 else 128
        P.op("dve", lambda h: h.memset(cv[0][:, :, 0:1], 0.0), writes=[Hb])
        P.op("dve", lambda h: h.memset(cv[1][:, :, 0:1], 0.0), writes=[Hb])
        for st in range(2):
            a_r, a_i = TA[dd][0][:, st, kidx:kidx + 1], TA[dd][1][:, st, kidx:kidx + 1]
            m = NCH - 1
            _ts(P, "dve", uq[0][:, 0:m], Hf[1][:, st, 0:m], a_i, None, ALU.mult, None, [Hb, pb], [uqb[0]])
            _stt(P, cv[0][:, st, 1:NCH], Hf[0][:, st, 0:m], a_r, uq[0][:, 0:m], ALU.mult, ALU.subtract, [Hb, pb, uqb[0]], [Hb])
            _ts(P, "dve", uq[1][:, 0:m], Hf[0][:, st, 0:m], a_i, None, ALU.mult, None, [Hb, pb], [uqb[1]])
            _stt(P, cv[1][:, st, 1:NCH], Hf[1][:, st, 0:m], a_r, uq[1][:, 0:m], ALU.mult, ALU.add, [Hb, pb, uqb[1]], [Hb])
        Tt = TA[dd] if dd == 0 else TW[dd]
        c_start = 0 if need_ctx_out else 2
        for c in range(c_start, NCH):
            pg = ps[2 + c % 2]; pgb = psb[2 + c % 2]
            for tl in range(4):
                _mm(P, pg[:, tl * 128:(tl + 1) * 128], Xt[:, c, tl * 128:(tl + 1) * 128], LT[:, dd, :], True, True, [Xtb[c], pb], [pgb])
            hh, hhb = hs_[c % 2], hsb[c % 2]
            pc = pos[c]
            for st in range(2):
                gr, gi = pg[:, st * 128:(st + 1) * 128], pg[:, (2 + st) * 128:(3 + st) * 128]
                c_r, c_i = cv[0][:, st, pc:pc + 1], cv[1][:, st, pc:pc + 1]
                Tr, Ti = Tt[0][:, st, 0:128], Tt[1][:, st, 0:128]
                u0 = ((c % 2) * 2 + st) * 4
                _stt(P, uq[u0][:, :], gr, c_r, Tr, ALU.add, ALU.mult, [pgb, Hb, pb], [uqb[u0]])
                _stt(P, uq[u0 + 1][:, :], gi, c_i, Ti, ALU.add, ALU.mult, [pgb, Hb, pb], [uqb[u0 + 1]])
                _stt(P, uq[u0 + 2][:, :], gi, c_i, Tr, ALU.add, ALU.mult, [pgb, Hb, pb], [uqb[u0 + 2]])
                _stt(P, uq[u0 + 3][:, :], gr, c_r, Ti, ALU.add, ALU.mult, [pgb, Hb, pb], [uqb[u0 + 3]])
                _tt(P, "pool", hh[:, st, :], uq[u0][:, :], uq[u0 + 1][:, :], ALU.subtract, [uqb[u0], uqb[u0 + 1]], [hhb])
                _tt(P, "pool", hh[:, 2 + st, :], uq[u0 + 2][:, :], uq[u0 + 3][:, :], ALU.add, [uqb[u0 + 2], uqb[u0 + 3]], [hhb])
            jj = c % 4
            py = ps[4 + (c // 4) % 2]; pyb = psb[4 + (c // 4) % 2]
            for tl in range(4):
                _mm(P, py[0:64, jj * 128:(jj + 1) * 128], CT[dd][:, tl, :], hh[:, tl, :], tl == 0, tl == 3, [pb, hhb], [pyb])
            if jj == 3 or c == NCH - 1:
                b0 = (c // 4) * 512
                wid = (jj + 1) * 128
                lo = 256 if ((not need_ctx_out) and c // 4 == 0) else 0
                if dd == 0:
                    _cp(P, "act", yacc[:, b0 + lo:b0 + wid], py[0:64, lo:wid], [pyb], [yb])
                else:
                    _tt(P, "dve", yacc[:, b0 + lo:b0 + wid], yacc[:, b0 + lo:b0 + wid], py[0:64, lo:wid], ALU.add, [pyb, yb], [yb])
    zT = A(P, [64, NTOK], BF16); zb = P.buf("zT")
    g1 = [A(P, [64, 512], F32) for _ in range(2)]; g1b = [P.buf(), P.buf()]
    g2 = [A(P, [64, 512], F32) for _ in range(2)]; g2b = [P.buf(), P.buf()]
    lo_all = 0 if need_ctx_out else 256
    if not need_ctx_out:
        P.op("pool", lambda h: h.memset(zT[:, 0:256], 0.0), writes=[zb])
    nblk = (NTOK + 511) // 512
    for bi in range(nblk):
        t0 = max(bi * 512, lo_all)
        t1_ = min((bi + 1) * 512, NTOK)
        n = t1_ - t0
        i2 = bi % 2
        y = g1[i2]; w = g2[i2]
        _stt(P, y[:, 0:n], sT[:, t0:t1_], dvec[:, 0:1], yacc[:, t0:t1_], ALU.mult, ALU.add, [sTb, pb, yb], [g1b[i2]])
        _tt(P, "dve", w[:, 0:n], y[:, 0:n], y[:, 0:n], ALU.mult, [g1b[i2]], [g2b[i2]])
        _ts(P, "dve", w[:, 0:n], w[:, 0:n], 0.044715, 1.0, ALU.mult, ALU.add, [g2b[i2]], [g2b[i2]])
        _tt(P, "dve", w[:, 0:n], w[:, 0:n], y[:, 0:n], ALU.mult, [g2b[i2], g1b[i2]], [g2b[i2]])
        _act(P, w[:, 0:n], w[:, 0:n], AF.Sigmoid, [g2b[i2]], [g2b[i2]], scale=1.5957691216057308)
        _tt(P, "dve", zT[:, t0:t1_], w[:, 0:n], y[:, 0:n], ALU.mult, [g2b[i2], g1b[i2]], [zb])
    outs.append(_ld(P, "sp", out[1, :, :], zT[:, :], [P.buf()], reads=[zb]))


import ml_dtypes

BF = ml_dtypes.bfloat16
NTOK = 8448
f32 = np.float32


def fm(a):
    return np.ascontiguousarray(a.T.reshape(8, 128, a.shape[0]))


def prep_common(inp, layer, b):
    cond = np.stack([inp['c'][b], inp['c_ctx']], 0)
    condT = np.ascontiguousarray(cond.reshape(2, 8, 128).transpose(2, 1, 0))
    bm = inp['b_mod'][layer].reshape(48, 128).T
    b_modT = np.ascontiguousarray(np.stack([bm, bm], -1))
    ng = inp['norm_g'][layer].reshape(4, 8, 128).transpose(2, 0, 1)
    norm_gT = np.ascontiguousarray(np.stack([ng, ng], -1))
    return dict(condT=condT, b_modT=b_modT, norm_gT=norm_gT, w_mod=inp['w_mod'][layer])


_const_cache = {}


def consts():
    if _const_cache:
        return _const_cache
    C = _const_cache
    c = np.arange(64)
    ang = 2 * np.pi * (np.outer(c, c) % 64) / 64
    C['f_CS'] = np.concatenate([np.cos(ang), -np.sin(ang)], 1).astype(BF)
    ca, sa = np.cos(ang), np.sin(ang)
    C['f_RP'] = np.concatenate([ca, -sa], 1).astype(BF)
    C['f_RQ'] = np.concatenate([sa, ca], 1).astype(BF)
    m2 = np.arange(128)[:, None, None]
    n1 = np.arange(64)[None, :, None]
    n2 = np.arange(128)[None, None, :]
    be = 2 * np.pi * ((m2 * (n1 + 64 * n2)) % 8192) / 8192
    nrm = 1 / np.sqrt(64 * 8192)
    C['f_CB'] = (np.cos(be) * nrm).astype(BF)
    C['f_SB'] = (np.sin(be) * nrm).astype(BF)
    m = np.arange(256)
    a256 = 2 * np.pi * (np.outer(m, m) % 256) / 256
    nrm2 = 1 / np.sqrt(64 * 256)
    C['f_C256'] = np.ascontiguousarray((np.cos(a256) * nrm2).reshape(2, 128, 256).transpose(1, 0, 2)).astype(BF)
    C['f_S256'] = np.ascontiguousarray((np.sin(a256) * nrm2).reshape(2, 128, 256).transpose(1, 0, 2)).astype(BF)
    t = np.arange(8192)
    row = (t // 64).astype(f32)
    col = (t % 64).astype(f32)
    inv = (1.0 / (f32(10000.0) ** (np.arange(16, dtype=f32) / f32(16)))).astype(f32)
    angr = np.concatenate([row[:, None] * inv, col[:, None] * inv], -1).astype(f32)
    cs, sn = np.cos(angr).astype(f32), np.sin(angr).astype(f32)
    cos64 = np.concatenate([cs, cs], 1)
    sin64 = np.concatenate([-sn, sn], 1)
    C['r_cosF'] = np.ascontiguousarray(cos64.T)
    C['r_sinF'] = np.ascontiguousarray(sin64.T)
    C['r_cosT'] = np.ascontiguousarray(cos64.reshape(64, 128, 64).transpose(1, 0, 2))
    C['r_sinT'] = np.ascontiguousarray(sin64.reshape(64, 128, 64).transpose(1, 0, 2))
    j = np.arange(128, dtype=f32)
    C['r_jcol'] = np.stack([127 - j, j], 1).astype(f32)
    ii = np.arange(128)
    dist = np.abs(ii[None, :] - ii[:, None]).astype(f32)
    C['r_dist'] = dist
    C['r_mask'] = np.stack([(ii[None, :] >= ii[:, None]), (ii[:, None] >= ii[None, :])], 0).astype(f32)
    C['r_irow'] = np.stack([np.tile(j + 1, (64, 1)), np.tile(128 - j, (64, 1))], 0).astype(f32)
    C['s_jrow'] = np.tile(np.arange(129, dtype=f32), (128, 1))
    C['s_jcol'] = np.arange(128, dtype=f32)[:, None].copy()
    C['s_LT'] = np.stack([(ii[None, :] >= ii[:, None]), (ii[:, None] >= ii[None, :])], 0).astype(BF)
    g_of_row = np.arange(64) // 16
    C['s_mrow'] = (g_of_row[:, None] == np.arange(4)[None, :]).astype(f32)
    g_of_st = (np.arange(128)[:, None] // 64) + 2 * np.arange(2)[None, :]
    C['s_msm'] = (g_of_st[:, :, None] == np.arange(4)[None, None, :]).astype(f32)
    C['ident_bf'] = np.eye(128).astype(BF)
    C['ident_f'] = np.eye(128).astype(f32)
    def start(r):
        return int(np.clip(r - 4, 0, 120))
    qc = np.arange(64)
    cst = np.clip(qc - 8, 0, 48)
    kc = np.arange(64)
    colok = (kc[None, :] >= cst[:, None]) & (kc[None, :] < cst[:, None] + 16)
    types = [(0, 0, 8), (2, 0, 8), (10, 6, 9), (124, 120, 8), (126, 120, 8)]
    mask = np.full((5, 128, 832), -30000.0, f32)
    drs = np.zeros((5, 2, 9), np.int64)
    for ti, (r0, R0, nr) in enumerate(types):
        for qr in range(2):
            r = r0 + qr
            for i in range(9):
                kr = R0 + i
                dr = int(np.clip(kr - r + 7, 0, 14))
                drs[ti, qr, i] = dr
                if i < nr and start(r) <= kr < start(r) + 8:
                    blk = np.where(colok, 0.0, -30000.0)
                    mask[ti, qr * 64:(qr + 1) * 64, i * 64:(i + 1) * 64] = blk
        mask[ti, :, 576:] = 0.0
    C['n_mask'] = mask
    C['n_drs'] = drs
    C['n_types'] = types
    return C


def prep_M(inp, layer, c, xT_full):
    b, q = c // 4, c % 4
    C = consts()
    m = prep_common(inp, layer, b)
    m['xT'] = xT_full
    w_in = inp['w_in'][layer]
    o = q * 64
    sw = np.r_[32:64, 0:32]
    cols_fm = np.concatenate([np.arange(0 + o, 0 + o + 64), np.arange(256 + o, 256 + o + 64), np.arange(512 + o, 512 + o + 64),
                              np.arange(768 + o, 768 + o + 64), 512 + o + sw, 768 + o + sw, np.arange(1280 + o, 1280 + o + 64),
                              np.arange(1536 + o, 1536 + o + 64), np.arange(1792 + o, 1792 + o + 64)])
    cols_tm = np.concatenate([np.arange(768 + o, 768 + o + 64), 768 + o + sw, np.arange(1024 + o, 1024 + o + 64), np.arange(2048 + o, 2048 + o + 64)])
    m['w_fm'] = np.ascontiguousarray(w_in[:, cols_fm])
    m['w_tm'] = np.ascontiguousarray(w_in[:, cols_tm])
    for k in ('f_CS', 'f_RP', 'f_RQ', 'f_CB', 'f_SB', 'f_C256', 'f_S256', 'r_cosF', 'r_sinF', 'r_cosT', 'r_sinT', 'r_jcol', 'r_dist', 'r_mask', 'r_irow',
              's_jrow', 's_jcol', 's_LT', 's_mrow', 's_msm', 'ident_bf', 'ident_f', 'n_mask'):
        m[k] = C[k]
    gs = slice(4 * q, 4 * q + 4)
    L = layer
    are, aim = inp['s5_a_re'][L][:, gs], inp['s5_a_im'][L][:, gs]
    ldt = inp['s5_log_dt'][L][:, gs]
    def sm(a):
        return np.ascontiguousarray(a.reshape(2, 2, 128).transpose(2, 0, 1))
    ldt_b = np.broadcast_to(ldt[:, :, None], (2, 4, 64))
    m['s_sm'] = np.ascontiguousarray(np.stack([sm(are), sm(aim), sm(ldt_b)], -1))
    row = np.stack([are.reshape(2, 256), aim.reshape(2, 256), ldt_b.reshape(2, 256)], -1)
    m['s_row'] = np.ascontiguousarray(np.broadcast_to(row[None].transpose(0, 1, 3, 2), (128, 2, 3, 256)))
    hs = np.stack([are, aim, ldt_b], 2)
    hs = np.broadcast_to(hs[:, :, None], (2, 4, 16, 3, 64))
    m['s_hs'] = np.ascontiguousarray(hs.transpose(1, 2, 0, 3, 4).reshape(64, 2, 3, 64))
    bre, bim = inp['s5_b_re'][L][:, gs], inp['s5_b_im'][L][:, gs]
    B = np.stack([bre, bim], 2)
    m['s_B'] = np.ascontiguousarray(B.transpose(1, 4, 0, 2, 3).reshape(64, 2, 2, 64))
    cre, cim = inp['s5_c_re'][L][:, gs], inp['s5_c_im'][L][:, gs]
    Cc = np.stack([cre, cim], 2)
    Cc = Cc.transpose(1, 4, 0, 2, 3)
    Cc = Cc.reshape(2, 2, 64, 2, 2, 16).transpose(1, 2, 3, 0, 4, 5).reshape(128, 2, 2, 2, 16)
    m['s_C'] = np.ascontiguousarray(Cc)
    m['s_d'] = np.ascontiguousarray(inp['s5_d'][L][256 * 0 + 64 * q:64 * q + 64][:, None])
    rd = inp['ret_decay'][L][:, q]
    m['r_dec'] = np.ascontiguousarray(np.broadcast_to(rd[None, :], (128, 2))).astype(f32)
    m['r_gn'] = np.ascontiguousarray(inp['ret_gn'][L][64 * q:64 * q + 64][:, None])
    rpb = inp['na_rpb'][L][q]
    dc = np.clip(np.arange(64)[None, :] - np.arange(64)[:, None], -15, 15) + 15
    m['n_toep'] = np.ascontiguousarray(rpb[:, dc])
    return m


def _prep_F(inp, layer, c, xa, bra_bf, moe_a=False):
    b = c // 4
    m = prep_common(inp, layer, b)
    m.update(xT=fm(xa), brT=bra_bf, w_in=inp['w_in'][layer], w_br=inp['w_branch'][layer].reshape(1024, 1024), w_o=inp['w_out'][layer],
             w_glu=inp['s5_w_glu'][layer], b_gluT=np.ascontiguousarray(inp['s5_b_glu'][layer].reshape(2, 128).T))
    i = layer // 2
    if layer % 2 == 0:
        m.update(w_g=inp['ffn_w_gate'][i:i + 1], w_u=inp['ffn_w_up'][i:i + 1], w_d=inp['ffn_w_down'][i:i + 1])
    else:
        sel = np.zeros((8, 8, 128), np.float32)
        for e in range(8):
            sel[e, e, :] = 1
        m.update(w_r=inp['moe_w_router'][i], b_r=np.ascontiguousarray(np.broadcast_to(inp['moe_b_router'][i][None], (128, 8))),
                 ident=np.eye(128, dtype=np.float32), sel=sel)
    return m


def kernel(**inputs):
    inp = {k: np.asarray(v) for k, v in inputs.items()}
    NCORE = 8
    cores = list(range(NCORE))
    x = inp['x']
    ctx = inp['ctx']
    for layer in range(2):
        last = (layer == 1)
        ncM = build_M(not last)
        xfull = [fm(np.concatenate([ctx[b], x[b]], 0)) for b in range(2)]
        maps = [prep_M(inp, layer, c, xfull[c // 4]) for c in cores]
        resM = run_bass_kernel_spmd(ncM, maps, core_ids=cores).results
        del maps
        br_full = []
        for b in range(2):
            o = np.stack([np.asarray(resM[4 * b + q]['brT_out']) for q in range(4)], 1)
            br_full.append(o.reshape(1024, NTOK))
        del resM
        if not last:
            blocksA = [(i * 256, 256, 0) for i in range(8)] + [(2048, 64, 1)]
            blocksB = [(i * 512, 512, 0) for i in range(4)] + [(2048, 64, 1)]
            ncF = build_F(blocksA, blocksB, 1, 2816, False)
        else:
            blocksA = [(i * 256, 256, 0) for i in range(8)]
            blocksB = [(i * 512, 512, 0) for i in range(4)]
            ncF = build_F(blocksA, blocksB, 8, 3584, True, mode='moe_a')
        maps = []
        for c in cores:
            b, q = c // 4, c % 4
            lat = slice(256 + q * 2048, 256 + (q + 1) * 2048)
            if not last:
                xa = np.concatenate([x[b, q * 2048:(q + 1) * 2048], ctx[b, q * 64:(q + 1) * 64]], 0)
                bra = np.concatenate([br_full[b][:, lat], br_full[b][:, q * 64:(q + 1) * 64]], 1)
            else:
                xa = x[b, q * 2048:(q + 1) * 2048]
                bra = br_full[b][:, lat]
            bra = np.ascontiguousarray(bra.reshape(8, 128, bra.shape[1]))
            maps.append(_prep_F(inp, layer, c, xa, bra))
        resF = run_bass_kernel_spmd(ncF, maps, core_ids=cores).results
        del maps
        if not last:
            xn = np.empty_like(x)
            cn = np.empty_like(ctx)
            for c in cores:
                b, q = c // 4, c % 4
                o = np.asarray(resF[c]['xo']).reshape(1024, -1).T
                xn[b, q * 2048:(q + 1) * 2048] = o[:2048]
                cn[b, q * 64:(q + 1) * 64] = o[2048:]
            x, ctx = xn, cn
            continue
        i = layer // 2
        h2_all = np.concatenate([np.asarray(resF[c]['h2o']) for c in cores], 2)
        cb_all = np.concatenate([np.asarray(resF[c]['cbo']) for c in cores], 1)
        sel = np.concatenate([np.asarray(resF[c]['mko']) for c in cores], 1).astype(bool)
        idx = [np.flatnonzero(sel[e]) for e in cores]
        nb = max(1, -(-max(len(t) for t in idx) // 512))
        ng = -(-nb // 4)
        groups = tuple(nb // ng + (1 if g < nb % ng else 0) for g in range(ng))
        C = 512 * nb
        ncE = build_E(groups)
        maps = []
        for e in cores:
            n_e = len(idx[e])
            ii = np.zeros(C, np.int64)
            ii[:n_e] = idx[e]
            cbe = np.zeros(C, cb_all.dtype)
            cbe[:n_e] = cb_all[e, idx[e]]
            maps.append(dict(h2=np.ascontiguousarray(h2_all[:, :, ii]), cbe=np.ascontiguousarray(np.broadcast_to(cbe[None, :], (128, C))),
                             w_g=inp['moe_w_gate'][i][e], w_u=inp['moe_w_up'][i][e], w_d=inp['moe_w_down'][i][e]))
        resE = run_bass_kernel_spmd(ncE, maps, core_ids=cores).results
        del maps
        slot = np.cumsum(sel, axis=0) - sel
        nslot = max(1, int(sel.sum(0).max()))
        yp_all = np.zeros((nslot, 8, 128, sel.shape[1]), np.float32)
        for e in cores:
            ye = np.asarray(resE[e]['ye'])
            t = idx[e]
            sv = slot[e, t]
            for k in range(nslot):
                mk = sv == k
                yp_all[k][:, :, t[mk]] = ye[:, :, np.flatnonzero(mk)]
        ncC = build_Fc(nexp=nslot)
        maps = []
        for c in cores:
            m = prep_common(inp, layer, c // 4)
            m['xm'] = np.asarray(resF[c]['xo'])
            m['yp'] = np.ascontiguousarray(yp_all[:, :, :, c * 2048:(c + 1) * 2048])
            maps.append(m)
        resC = run_bass_kernel_spmd(ncC, maps, core_ids=cores).results
        xn = np.empty_like(x)
        for c in cores:
            b, q = c // 4, c % 4
            xn[b, q * 2048:(q + 1) * 2048] = np.asarray(resC[c]['xo']).reshape(1024, -1).T
        x = xn
    return x.astype(np.float32)
```

```python
import numpy as np
from contextlib import ExitStack
import concourse.bass as bass
import concourse.mybir as mybir
from concourse.bass_utils import run_bass_kernel_spmd

F32 = mybir.dt.float32
BF16 = mybir.dt.bfloat16
I32 = mybir.dt.int32
ALU = mybir.AluOpType
AF = mybir.ActivationFunctionType
AX = mybir.AxisListType

ENGS = ("pe", "act", "dve", "pool", "sp")
NDSEM = 12


class Buf:
    __slots__ = ("name", "lw", "rd", "psum")

    def __init__(self, name="", psum=False):
        self.name = name
        self.psum = psum
        self.lw = None
        self.rd = {}


class Prog:
    def __init__(self, nc):
        self.nc = nc
        self.stack = ExitStack()
        self.ops = {e: [] for e in ENGS}
        self.cnt = {e: 0 for e in ENGS}
        self.seen = {e: {} for e in ENGS}
        self.sems = {}
        for e in ENGS:
            self.sems[e] = self.stack.enter_context(nc.semaphore("s_" + e))
        self.dsem_use = {}
        self.dq_next = {}
        for q in ("sp", "act", "pool"):
            for i in range(NDSEM):
                k = "d_%s%d" % (q, i)
                self.sems[k] = self.stack.enter_context(nc.semaphore(k))
                self.dsem_use[k] = 0
            self.dq_next[q] = 0
        self.nbuf = 0

    def sb(self, name, shape, dt):
        return self.stack.enter_context(self.nc.sbuf_tensor(name, list(shape), dt))

    def ps(self, name, shape, dt=F32):
        return self.stack.enter_context(self.nc.psum_tensor(name, list(shape), dt))

    def buf(self, name=None, psum=None):
        self.nbuf += 1
        name = name or "b%d" % self.nbuf
        if psum is None:
            psum = name.startswith("ps")
        return Buf(name, psum)

    def _deps(self, eng, reads, writes, is_dma):
        w = {}

        def add(t):
            if t is None:
                return
            k, v = t
            if w.get(k, 0) < v:
                w[k] = v
        for b in reads:
            add(b.lw)
            if b.psum:
                for k, v in b.rd.items():
                    if k != eng:
                        add((k, v))
        for b in writes:
            if b.lw is not None:
                if not (eng == "pe" and b.lw[0] == "pe" and not is_dma):
                    add(b.lw)
            for k, v in b.rd.items():
                if k == eng and not is_dma and eng != "pool":
                    continue
                add((k, v))
        seen = self.seen[eng]
        out = []
        for k, v in w.items():
            if seen.get(k, 0) < v:
                seen[k] = v
                out.append((k, v))
        return out

    def _commit(self, ticket, reads, writes):
        for b in writes:
            b.lw = ticket
            b.rd = {}
        for b in reads:
            k, v = ticket
            if b.rd.get(k, 0) < v:
                b.rd[k] = v

    def op(self, eng, fn, reads=(), writes=()):
        waits = self._deps(eng, reads, writes, False)
        self.cnt[eng] += 1
        ticket = (eng, self.cnt[eng])
        self.ops[eng].append((waits, fn, (eng, 1)))
        self._commit(ticket, reads, writes)
        return ticket

    def dma(self, q, fn, reads=(), writes=()):
        i = self.dq_next[q]
        self.dq_next[q] = (i + 1) % NDSEM
        k = "d_%s%d" % (q, i)
        waits = self._deps(q, reads, writes, True)
        prev = self.dsem_use[k]
        if prev > 0 and self.seen[q].get(k, 0) < 16 * prev:
            self.seen[q][k] = 16 * prev
            waits.append((k, 16 * prev))
        self.dsem_use[k] = prev + 1
        ticket = (k, 16 * (prev + 1))
        self.ops[q].append((waits, fn, (k, 16)))
        self._commit(ticket, reads, writes)
        return ticket

    def finish_wait(self, eng, tickets):
        waits = []
        for k, v in tickets:
            if self.seen[eng].get(k, 0) < v:
                self.seen[eng][k] = v
                waits.append((k, v))
        self.ops[eng].append((waits, None, None))

    def emit(self):
        nc = self.nc
        sems = self.sems
        ops = self.ops

        def replay(e, h):
            for waits, fn, inc in ops[e]:
                for k, v in waits:
                    h.wait_ge(sems[k], v)
                if fn is not None:
                    ins = fn(h)
                    ins.then_inc(sems[inc[0]], inc[1])

        with nc.Block() as block:
            @block.sync
            def _(h):
                replay("sp", h)

            @block.scalar
            def _(h):
                replay("act", h)

            @block.vector
            def _(h):
                replay("dve", h)

            @block.gpsimd
            def _(h):
                replay("pool", h)

            @block.tensor
            def _(h):
                replay("pe", h)
        self.stack.close()


def _mm(P, out, lhsT, rhs, start, stop, reads, writes):
    return P.op("pe", lambda h: h.matmul(out, lhsT=lhsT, rhs=rhs, start=start, stop=stop), reads=reads, writes=writes)


def _tr(P, out, in_, ident, reads, writes):
    return P.op("pe", lambda h: h.transpose(out, in_, ident), reads=reads, writes=writes)


def _act(P, out, in_, func, reads, writes, scale=None, bias=None):
    kw = {}
    if scale is not None:
        kw["scale"] = scale
    if bias is not None:
        kw["bias"] = bias
    return P.op("act", lambda h: h.activation(out=out, in_=in_, func=func, **kw), reads=reads, writes=writes)


def _tt(P, eng, out, in0, in1, op, reads, writes):
    return P.op(eng, lambda h: h.tensor_tensor(out=out, in0=in0, in1=in1, op=op), reads=reads, writes=writes)


def _ts(P, eng, out, in0, s1, s2, op0, op1, reads, writes):
    if op1 is None:
        return P.op(eng, lambda h: h.tensor_scalar(out=out, in0=in0, scalar1=s1, scalar2=None, op0=op0), reads=reads, writes=writes)
    return P.op(eng, lambda h: h.tensor_scalar(out=out, in0=in0, scalar1=s1, scalar2=s2, op0=op0, op1=op1), reads=reads, writes=writes)


def _stt(P, out, in0, scalar, in1, op0, op1, reads, writes):
    return P.op("dve", lambda h: h.scalar_tensor_tensor(out=out, in0=in0, scalar=scalar, in1=in1, op0=op0, op1=op1), reads=reads, writes=writes)


def _cp(P, eng, out, in_, reads, writes):
    if eng == "act":
        return P.op("act", lambda h: h.activation(out=out, in_=in_, func=AF.Copy), reads=reads, writes=writes)
    return P.op(eng, lambda h: h.tensor_copy(out=out, in_=in_), reads=reads, writes=writes)


def _ld(P, q, out, in_, writes, reads=()):
    return P.dma(q, lambda h: h.dma_start(out=out, in_=in_), reads=reads, writes=writes)


D = 1024
KC = 8
EPS = 1e-6


def arena_init(P, nbytes=206 * 1024):
    lo, hi = P.nc.bump_sbuf(nbytes)
    P.a_lo, P.a_hi, P.a_cur = lo, hi, lo
    P.a_n = 0


def A(P, shape, dt):
    nb = int(np.prod(shape[1:])) * (4 if dt in (F32, I32) else 2)
    off = (P.a_cur + 31) // 32 * 32
    assert off + nb <= P.a_hi, ("SBUF arena overflow", off + nb - P.a_lo)
    P.a_cur = off + nb
    P.a_n += 1
    return P.nc.alloc_sbuf_tensor_at("t%d" % P.a_n, list(shape), dt, offset=off)


def barrier(P, queues=None):
    tick = [(e, P.cnt[e]) for e in ENGS if P.cnt[e] > 0]
    tick += [(k, 16 * v) for k, v in P.dsem_use.items() if v > 0 and (queues is None or any(k.startswith("d_" + q) for q in queues))]
    for e in ENGS:
        P.finish_wait(e, tick)


def rms_rstd(P, src, srcb, n, sq, sqb, ss_ps, ssb, rstd, rstdb, ones):
    P.op("act", lambda h: h.activation(out=sq[:, :, 0:n], in_=src[:, :, 0:n], func=AF.Square), reads=[srcb], writes=[sqb])
    for k in range(KC):
        P.op("pe", lambda h, k=k: h.matmul(ss_ps[:, 0:n], lhsT=ones[:], rhs=sq[:, k, 0:n], start=(k == 0), stop=(k == KC - 1)),
             reads=[sqb], writes=[ssb])
    P.op("act", lambda h: h.activation(out=rstd[:, 0:n], in_=ss_ps[:, 0:n], func=AF.Ln, scale=1.0 / D, bias=P.eps_t[:, 0:1]), reads=[ssb], writes=[rstdb])
    P.op("act", lambda h: h.activation(out=rstd[:, 0:n], in_=rstd[:, 0:n], func=AF.Exp, scale=-0.5), reads=[rstdb], writes=[rstdb])


def norm_mod(P, src, srcb, n, rstd, rstdb, gm, sh, r, dst, dstb, tmp, tmpb, dst_off=0, dst32=None, dst32b=None):
    for k in range(KC):
        tb = tmpb[k % len(tmp)]
        tt = tmp[k % len(tmp)]
        P.op("dve", lambda h, k=k, tt=tt: h.tensor_tensor(out=tt[:, 0:n], in0=src[:, k, 0:n], in1=rstd[:, 0:n], op=ALU.mult),
             reads=[srcb, rstdb], writes=[tb])
        P.op("act", lambda h, k=k, tt=tt: h.activation(out=dst[:, k, dst_off:dst_off + n], in_=tt[:, 0:n], func=AF.Identity,
                                                     scale=gm[:, k, r:r + 1], bias=sh[:, k, r:r + 1]),
             reads=[tb, P.modb], writes=[dstb])
        if dst32 is not None:
            P.op("pool", lambda h, k=k, tt=tt: h.tensor_scalar(out=dst32[:, k, 0:n], in0=tt[:, 0:n], scalar1=gm[:, k, r:r + 1],
                                                             scalar2=sh[:, k, r:r + 1], op0=ALU.mult, op1=ALU.add),
                 reads=[tb, P.modb], writes=[dst32b])


def compute_mod(P, dr, which, mod_ps, modpb, light=False):
    nc = P.nc
    cs = A(P, [128, KC, 2], F32)
    csb = P.buf()
    P.dma("sp", lambda h: h.dma_start(out=cs[:], in_=dr["condT"][:, :, :]), writes=[csb])
    sig = A(P, [128, KC, 2], F32)
    P.op("act", lambda h: h.activation(out=sig[:], in_=cs[:], func=AF.Sigmoid), reads=[csb], writes=[csb])
    P.op("dve", lambda h: h.tensor_tensor(out=cs[:], in0=cs[:], in1=sig[:], op=ALU.mult), reads=[csb], writes=[csb])
    modT = A(P, [128, 48, 2], F32)
    P.modT = modT
    P.modb = P.buf("mod")
    bm = A(P, [128, 48, 2], F32)
    bmb = P.buf()
    P.dma("sp", lambda h: h.dma_start(out=bm[:], in_=dr["b_modT"][:, :, :]), writes=[bmb])
    ng = A(P, [128, 4, KC, 2], F32)
    P.ng = ng
    P.dma("sp", lambda h: h.dma_start(out=ng[:], in_=dr["norm_gT"][:, :, :, :]), writes=[P.modb])
    mark = P.a_cur
    wm = [A(P, [128, KC, 1024], F32) for _ in range(2)]
    wmb = [P.buf(), P.buf()]
    wsrc = dr["w_mod"].rearrange("(k p) f -> p k f", p=128)
    for i, j in enumerate(which):
        w = wm[i % 2]
        wb = wmb[i % 2]
        for k2 in range(2):
            P.dma("sp", lambda h, w=w, j=j, k2=k2: h.dma_start(out=w[:, 4 * k2:4 * k2 + 4, :], in_=wsrc[:, 4 * k2:4 * k2 + 4, j * 1024:(j + 1) * 1024]), writes=[wb])
        for fc in range(8):
            for k in range(KC):
                P.op("pe", lambda h, w=w, j=j, fc=fc, k=k: h.matmul(mod_ps[:, j * 8 + fc, :], lhsT=w[:, k, fc * 128:(fc + 1) * 128], rhs=cs[:, k, :],
                                                                   start=(k == 0), stop=(k == KC - 1)), reads=[wb, csb], writes=[modpb])
    for j in which:
        P.op("dve", lambda h, j=j: h.tensor_tensor(out=modT[:, j * 8:(j + 1) * 8, :], in0=mod_ps[:, j * 8:(j + 1) * 8, :], in1=bm[:, j * 8:(j + 1) * 8, :], op=ALU.add),
             reads=[modpb, bmb], writes=[P.modb])
    barrier(P, ("sp",) if light else None)
    P.a_cur = mark


def mod_derived(P, jsc, jg, gi_norm, gi_gate):
    gm = A(P, [128, KC, 2], F32)
    gg = A(P, [128, KC, 2], F32)
    modT, ng = P.modT, P.ng
    P.op("dve", lambda h: h.scalar_tensor_tensor(out=gm[:], in0=modT[:, jsc * 8:(jsc + 1) * 8, :], scalar=1.0, in1=ng[:, gi_norm, :, :],
                                                 op0=ALU.add, op1=ALU.mult), reads=[P.modb], writes=[P.modb])
    if jg is not None:
        P.op("dve", lambda h: h.tensor_tensor(out=gg[:], in0=modT[:, jg * 8:(jg + 1) * 8, :], in1=ng[:, gi_gate, :, :], op=ALU.mult),
             reads=[P.modb], writes=[P.modb])
    return gm, gg


def build_F(blocks, blocksB, n_exp, dff, moe, DBG=False, mode='full'):
    TT = sum(b[1] for b in blocks)
    nc = bass.Bass("TRN2", target_bir_lowering=False)
    dr = {}

    def din(name, shape, dt=F32):
        dr[name] = nc.dram_tensor(name, list(shape), dt, kind="ExternalInput").ap()
    din("xT", [KC, 128, TT])
    din("brT", [KC, 128, TT], BF16)
    din("condT", [128, KC, 2])
    din("w_mod", [D, 6 * D])
    din("b_modT", [128, 48, 2])
    din("norm_gT", [128, 4, KC, 2])
    din("w_in", [D, 6400])
    din("w_br", [KC * 128, D])
    din("w_o", [D, D])
    din("w_glu", [256, 256])
    din("b_gluT", [128, 2])
    if mode == 'full':
        din("w_g", [n_exp, D, dff])
        din("w_u", [n_exp, D, dff])
        din("w_d", [n_exp, dff, D])
    if moe:
        din("w_r", [D, 8])
        din("b_r", [128, 8])
        din("ident", [128, 128])
        din("sel", [8, 8, 128])
    if mode == 'moe_a':
        h2o = nc.dram_tensor("h2o", [KC, 128, TT], BF16, kind="ExternalOutput").ap().rearrange("k p t -> p k t")
        cbo = nc.dram_tensor("cbo", [8, TT], BF16, kind="ExternalOutput").ap()
        mko = nc.dram_tensor("mko", [8, TT], BF16, kind="ExternalOutput").ap()
    xo = nc.dram_tensor("xo", [KC, 128, TT], F32, kind="ExternalOutput").ap()
    xoT = xo.rearrange("k p t -> p k t")
    if DBG: dbg_mod = nc.dram_tensor("dbg_mod", [128, 48, 2], F32, kind="ExternalOutput").ap()
    if DBG: dbg_xm = nc.dram_tensor("dbg_xm", [KC, 128, TT], F32, kind="ExternalOutput").ap().rearrange("k p t -> p k t")
    if DBG: dbg_h = nc.dram_tensor("dbg_h", [KC, 128, TT], BF16, kind="ExternalOutput").ap().rearrange("k p t -> p k t")
    if DBG: dbg_z = nc.dram_tensor("dbg_z", [KC, 128, TT], F32, kind="ExternalOutput").ap().rearrange("k p t -> p k t")
    if DBG: dbg_r = nc.dram_tensor("dbg_r", [128, TT], F32, kind="ExternalOutput").ap()
    if DBG: dbg_sq = nc.dram_tensor("dbg_sq", [KC, 128, TT], BF16, kind="ExternalOutput").ap().rearrange("k p t -> p k t")
    if DBG: dbg_ss = nc.dram_tensor("dbg_ss", [128, TT], F32, kind="ExternalOutput").ap()
    sscp = A(P, [128, 256], F32) if False else None
    if DBG: dbg_y = nc.dram_tensor("dbg_y", [KC, 128, TT], BF16, kind="ExternalOutput").ap().rearrange("k p t -> p k t")
    xT = dr["xT"].rearrange("k p t -> p k t")
    brT = dr["brT"].rearrange("k p t -> p k t")

    P = Prog(nc)
    arena_init(P)
    ps = [P.ps("ps%d" % i, [128, 512], F32) for i in range(8)]
    psb = [P.buf("ps%d" % i) for i in range(8)]
    ones = A(P, [128, 128], BF16)
    onesb = P.buf()
    P.op("dve", lambda h: h.memset(ones[:], 1.0), writes=[onesb])
    P.eps_t = A(P, [128, 1], F32)
    P.op("dve", lambda h: h.memset(P.eps_t[:], EPS), writes=[onesb])

    w_lo = P.a_cur
    wgt = A(P, [128, KC, 4096], BF16)
    wbr = A(P, [128, KC, D], BF16)
    wo = A(P, [128, KC, D], BF16)
    wAb = P.buf("wA")
    wbufs = []

    def _wb():
        wbufs.append(P.buf())
        return wbufs[-1]
    w_in_v = dr["w_in"].rearrange("(k p) c -> p k c", p=128)
    for k in range(KC):
        for c4 in range(2):
            P.dma("pool", lambda h, k=k, c4=c4: h.dma_start(out=wgt[:, k, c4 * 2048:(c4 + 1) * 2048], in_=w_in_v[:, k, 2304 + c4 * 2048:2304 + (c4 + 1) * 2048]), writes=[_wb()])
    P.dma("pool", lambda h: h.dma_start(out=wbr[:, 0:4, :], in_=dr["w_br"].rearrange("(k p) c -> p k c", p=128)[:, 0:4, :]), writes=[_wb()])
    P.dma("pool", lambda h: h.dma_start(out=wbr[:, 4:8, :], in_=dr["w_br"].rearrange("(k p) c -> p k c", p=128)[:, 4:8, :]), writes=[_wb()])
    P.dma("pool", lambda h: h.dma_start(out=wo[:, 0:4, :], in_=dr["w_o"].rearrange("(k p) c -> p k c", p=128)[:, 0:4, :]), writes=[_wb()])
    P.dma("pool", lambda h: h.dma_start(out=wo[:, 4:8, :], in_=dr["w_o"].rearrange("(k p) c -> p k c", p=128)[:, 4:8, :]), writes=[_wb()])

    wglu = A(P, [128, 2, 256], BF16)
    bglu = A(P, [128, 2], F32)
    P.dma("pool", lambda h: h.dma_start(out=wglu[:], in_=dr["w_glu"].rearrange("(k p) c -> p k c", p=128)), writes=[_wb()])
    P.dma("sp", lambda h: h.dma_start(out=bglu[:], in_=dr["b_gluT"][:, :]), writes=[_wb()])
    w_hi = P.a_cur
    mod_ps = nc.alloc_psum_tensor
    mod_view = ps[7][:, 0:96].rearrange("p (j r) -> p j r", r=2)
    compute_mod(P, dr, [0, 1, 2, 3, 4, 5], mod_view, psb[7], light=True)
    gm_a, gg_a = mod_derived(P, 1, 2, 0, 1)
    gm_f, gg_f = mod_derived(P, 4, 5, 2, 3)
    sh_a = P.modT[:, 0:8, :]
    sh_f = P.modT[:, 24:32, :]
    P.dbgt = []
    if DBG: P.dbgt += [P.dma("sp", lambda h: h.dma_start(out=dbg_mod[:, :, :], in_=P.modT[:]), reads=[P.modb], writes=[P.buf()])]

    h2 = A(P, [128, KC, TT], BF16)
    h2b = P.buf("h2")
    if moe:
        cbT = A(P, [8, TT], BF16)
        cbTb = P.buf("cbT")
        mkT = A(P, [8, TT], BF16)
        mkTb = P.buf("mkT")
        ident = A(P, [128, 128], F32)
        P.dma("sp", lambda h: h.dma_start(out=ident[:], in_=dr["ident"][:, :]), writes=[onesb])
        wr = A(P, [128, KC, 8], F32)
        P.dma("sp", lambda h: h.dma_start(out=wr[:], in_=dr["w_r"].rearrange("(k p) e -> p k e", p=128)), writes=[onesb])
        br_t = A(P, [128, 8], F32)
        P.dma("sp", lambda h: h.dma_start(out=br_t[:], in_=dr["b_r"][:, :]), writes=[onesb])
        sel = A(P, [8, 8, 128], BF16)
        P.dma("pool", lambda h: h.dma_start(out=sel[:], in_=dr["sel"][:, :, :]), writes=[onesb])
    markA = P.a_cur
    wjoin = A(P, [128, 1], F32)
    P.op("dve", lambda h: h.memset(wjoin[:], 0.0), reads=wbufs, writes=[wAb])
    glu_t = A(P, [128, 2, 256], BF16)
    glub = P.buf("glu")
    sgl = A(P, [128, 256], F32)
    sglb = P.buf("sgl")
    xb = [A(P, [128, KC, 256], F32) for _ in range(2)]
    xbb = [P.buf(), P.buf()]
    brb_t = [A(P, [128, KC, 256], BF16) for _ in range(2)]
    brbb = [P.buf(), P.buf()]
    sq = A(P, [128, KC, 256], BF16)
    sqb = P.buf()
    rstd = A(P, [128, 256], F32)
    rstdb = P.buf()
    tmp = [A(P, [128, 256], F32) for _ in range(2)]
    tmpb = [P.buf(), P.buf()]
    hb = A(P, [128, KC, 256], BF16)
    hbb = P.buf()
    yb = A(P, [128, KC, 256], BF16)
    ybb = P.buf()
    zb = A(P, [128, KC, 256], F32)
    zbb = P.buf()
    sg = [A(P, [128, 256], F32) for _ in range(2)]
    sgb = [P.buf(), P.buf()]
    tt2 = [A(P, [128, 256], F32) for _ in range(2)]
    tt2b = [P.buf(), P.buf()]
    accA = [A(P, [128, 256], F32) for _ in range(2)]
    accAb = [P.buf(), P.buf()]
    xob = P.buf("xo")
    h2fb = P.buf("h2f")
    if moe:
        h2f = A(P, [128, KC, 256], F32)
        lg = A(P, [128, 8], F32)
        mx8 = A(P, [128, 8], F32)
        msk = A(P, [128, 8], F32)
        ex = A(P, [128, 8], F32)
        den = A(P, [128, 1], F32)
        nmx = A(P, [128, 1], F32)
        rb = P.buf("router")
    cnt = 0
    P.sscp = A(P, [128, 256], F32)
    P.sscpb = P.buf()
    def _ldA(bi_):
        t0_, n_, _r = blocks[bi_]
        xx, xxb = xb[bi_ % 2], xbb[bi_ % 2]
        bb_, bbb = brb_t[bi_ % 2], brbb[bi_ % 2]
        P.dma("sp", lambda h: h.dma_start(out=xx[:, 0:4, 0:n_], in_=xT[:, 0:4, t0_:t0_ + n_]), writes=[xxb])
        P.dma("sp", lambda h: h.dma_start(out=xx[:, 4:8, 0:n_], in_=xT[:, 4:8, t0_:t0_ + n_]), writes=[xxb])
        P.dma("sp", lambda h: h.dma_start(out=bb_[:, :, 0:n_], in_=brT[:, :, t0_:t0_ + n_]), writes=[bbb])
    _ldA(0)
    for bi, (t0, n, r) in enumerate(blocks):
        x_t, x_b = xb[bi % 2], xbb[bi % 2]
        b_t, b_b = brb_t[bi % 2], brbb[bi % 2]
        if bi + 1 < len(blocks):
            _ldA(bi + 1)
        rms_rstd(P, x_t, x_b, n, sq, sqb, ps[6], psb[6], rstd, rstdb, ones)
        norm_mod(P, x_t, x_b, n, rstd, rstdb, gm_a, sh_a, r, hb, hbb, tmp, tmpb)
        for oc in range(2):
            for kc in range(2):
                P.op("pe", lambda h, oc=oc, kc=kc, n=n, b_t=b_t: h.matmul(ps[6][:, 0:n], lhsT=wglu[:, kc, oc * 128:(oc + 1) * 128], rhs=b_t[:, 2 + kc, 0:n],
                                                                      start=(kc == 0), stop=(kc == 1)), reads=[wAb, b_b], writes=[psb[6]])
            P.op("act", lambda h, oc=oc, n=n: h.activation(out=sgl[:, 0:n], in_=ps[6][:, 0:n], func=AF.Sigmoid, bias=bglu[:, oc:oc + 1], scale=1.0), reads=[psb[6], wAb], writes=[sglb])
            P.op("dve", lambda h, oc=oc, n=n, b_t=b_t: h.tensor_tensor(out=glu_t[:, oc, 0:n], in0=sgl[:, 0:n], in1=b_t[:, 2 + oc, 0:n], op=ALU.mult), reads=[sglb, b_b], writes=[glub])
        for fc in range(8):
            ac, acb = accA[fc % 2], accAb[fc % 2]
            for b in range(4):
                gi = cnt % 2
                cnt += 1
                gps, gpb = ps[gi], psb[gi]
                pps, ppb = ps[2 + gi], psb[2 + gi]
                for k in range(KC):
                    P.op("pe", lambda h, gps=gps, k=k, b=b, fc=fc, n=n: h.matmul(gps[:, 0:n], lhsT=wgt[:, k, b * 1024 + fc * 128:b * 1024 + (fc + 1) * 128], rhs=hb[:, k, 0:n],
                                                                                 start=(k == 0), stop=(k == KC - 1)), reads=[wAb, hbb], writes=[gpb])
                for hh in range(2):
                    rhs_ap = glu_t[:, hh, 0:n] if b == 1 else b_t[:, 2 * b + hh, 0:n]
                    P.op("pe", lambda h, pps=pps, hh=hh, b=b, fc=fc, n=n, rhs_ap=rhs_ap: h.matmul(pps[:, 0:n], lhsT=wbr[:, 2 * b + hh, fc * 128:(fc + 1) * 128], rhs=rhs_ap,
                                                                                          start=(hh == 0), stop=(hh == 1)), reads=[wAb, b_b, glub], writes=[ppb])
                s_t, s_b = sg[gi], sgb[gi]
                P.op("act", lambda h, s_t=s_t, gps=gps, n=n: h.activation(out=s_t[:, 0:n], in_=gps[:, 0:n], func=AF.Sigmoid), reads=[gpb], writes=[s_b])
                if b == 0:
                    P.op("dve", lambda h, ac=ac, s_t=s_t, pps=pps, n=n: h.tensor_tensor(out=ac[:, 0:n], in0=s_t[:, 0:n], in1=pps[:, 0:n], op=ALU.mult),
                         reads=[s_b, ppb], writes=[acb])
                else:
                    t_t, t_b = tt2[gi], tt2b[gi]
                    P.op("dve", lambda h, t_t=t_t, s_t=s_t, pps=pps, n=n: h.tensor_tensor(out=t_t[:, 0:n], in0=s_t[:, 0:n], in1=pps[:, 0:n], op=ALU.mult),
                         reads=[s_b, ppb], writes=[t_b])
                    if b < 3:
                        P.op("pool", lambda h, ac=ac, t_t=t_t, n=n: h.tensor_tensor(out=ac[:, 0:n], in0=ac[:, 0:n], in1=t_t[:, 0:n], op=ALU.add),
                             reads=[acb, t_b], writes=[acb])
                    else:
                        P.op("pool", lambda h, ac=ac, t_t=t_t, n=n, fc=fc: h.tensor_tensor(out=yb[:, fc, 0:n], in0=ac[:, 0:n], in1=t_t[:, 0:n], op=ALU.add),
                             reads=[acb, t_b], writes=[ybb])
        for fc in range(8):
            zi = 4 + fc % 2
            for k in range(KC):
                P.op("pe", lambda h, zi=zi, k=k, fc=fc, n=n: h.matmul(ps[zi][:, 0:n], lhsT=wo[:, k, fc * 128:(fc + 1) * 128], rhs=yb[:, k, 0:n], start=(k == 0), stop=(k == KC - 1)),
                     reads=[wAb, ybb], writes=[psb[zi]])
            P.op("act", lambda h, zi=zi, fc=fc, n=n: h.activation(out=zb[:, fc, 0:n], in_=ps[zi][:, 0:n], func=AF.Copy), reads=[psb[zi]], writes=[zbb])
        rms_rstd(P, zb, zbb, n, sq, sqb, ps[6], psb[6], rstd, rstdb, ones)
        if DBG: P.dbgt.append(P.dma("sp", lambda h, t0=t0, n=n: h.dma_start(out=dbg_z[:, :, t0:t0 + n], in_=zb[:, :, 0:n]), reads=[zbb], writes=[P.buf()]))
        if DBG: P.dbgt.append(P.dma("sp", lambda h, t0=t0, n=n: h.dma_start(out=dbg_r[:, t0:t0 + n], in_=rstd[:, 0:n]), reads=[rstdb], writes=[P.buf()]))
        if DBG: P.dbgt.append(P.dma("sp", lambda h, t0=t0, n=n: h.dma_start(out=dbg_sq[:, :, t0:t0 + n], in_=sq[:, :, 0:n]), reads=[sqb], writes=[P.buf()]))
        if DBG: P.op("dve", lambda h, n=n: h.tensor_copy(out=P.sscp[:, 0:n], in_=ps[6][:, 0:n]), reads=[psb[6]], writes=[P.sscpb])
        if DBG: P.dbgt.append(P.dma("sp", lambda h, t0=t0, n=n: h.dma_start(out=dbg_ss[:, t0:t0 + n], in_=P.sscp[:, 0:n]), reads=[P.sscpb], writes=[P.buf()]))
        for k in range(KC):
            tb_, tt_ = tmpb[k % 2], tmp[k % 2]
            P.op("dve", lambda h, k=k, tt_=tt_, n=n: h.tensor_tensor(out=tt_[:, 0:n], in0=zb[:, k, 0:n], in1=rstd[:, 0:n], op=ALU.mult), reads=[zbb, rstdb], writes=[tb_])
            P.op("dve", lambda h, k=k, tt_=tt_, n=n, x_t=x_t, r=r: h.scalar_tensor_tensor(out=x_t[:, k, 0:n], in0=tt_[:, 0:n], scalar=gg_a[:, k, r:r + 1], in1=x_t[:, k, 0:n],
                                                                                    op0=ALU.mult, op1=ALU.add), reads=[tb_, P.modb, x_b], writes=[x_b])
        P.dma("sp", lambda h, x_t=x_t, t0=t0, n=n: h.dma_start(out=xoT[:, :, t0:t0 + n], in_=x_t[:, :, 0:n]), reads=[x_b], writes=[xob])
        if DBG: P.dbgt.append(P.dma("sp", lambda h, x_t=x_t, t0=t0, n=n: h.dma_start(out=dbg_xm[:, :, t0:t0 + n], in_=x_t[:, :, 0:n]), reads=[x_b], writes=[P.buf()]))
        if DBG: P.dbgt.append(P.dma("sp", lambda h, t0=t0, n=n: h.dma_start(out=dbg_h[:, :, t0:t0 + n], in_=hb[:, :, 0:n]), reads=[hbb], writes=[P.buf()]))
        if DBG: P.dbgt.append(P.dma("sp", lambda h, t0=t0, n=n: h.dma_start(out=dbg_y[:, :, t0:t0 + n], in_=yb[:, :, 0:n]), reads=[ybb], writes=[P.buf()]))
        rms_rstd(P, x_t, x_b, n, sq, sqb, ps[6], psb[6], rstd, rstdb, ones)
        norm_mod(P, x_t, x_b, n, rstd, rstdb, gm_f, sh_f, r, h2, h2b, tmp, tmpb, dst_off=t0, dst32=(h2f if moe else None), dst32b=h2fb)
        if moe:
            for tt in range(n // 128):
                for k in range(KC):
                    P.op("pe", lambda h, k=k, tt=tt: h.matmul(ps[7][:, 0:8], lhsT=h2f[:, k, tt * 128:(tt + 1) * 128], rhs=wr[:, k, :], start=(k == 0), stop=(k == KC - 1)),
                         reads=[h2fb, onesb], writes=[psb[7]])
                P.op("dve", lambda h: h.tensor_tensor(out=lg[:], in0=ps[7][:, 0:8], in1=br_t[:], op=ALU.add), reads=[psb[7], onesb], writes=[rb])
                P.op("dve", lambda h: h.max(out=mx8[:], in_=lg[:]), reads=[rb], writes=[rb])
                P.op("dve", lambda h: h.tensor_scalar(out=msk[:], in0=lg[:], scalar1=mx8[:, 1:2], scalar2=None, op0=ALU.is_ge), reads=[rb], writes=[rb])
                P.op("dve", lambda h: h.tensor_scalar(out=nmx[:], in0=mx8[:, 0:1], scalar1=-1.0, scalar2=None, op0=ALU.mult), reads=[rb], writes=[rb])
                P.op("act", lambda h: h.activation(out=ex[:], in_=lg[:], func=AF.Exp, bias=nmx[:, 0:1], scale=1.0), reads=[rb], writes=[rb])
                P.op("dve", lambda h: h.tensor_tensor(out=ex[:], in0=ex[:], in1=msk[:], op=ALU.mult), reads=[rb], writes=[rb])
                P.op("dve", lambda h: h.reduce_sum(out=den[:], in_=ex[:], axis=AX.X), reads=[rb], writes=[rb])
                P.op("dve", lambda h: h.reciprocal(out=den[:], in_=den[:]), reads=[rb], writes=[rb])
                P.op("dve", lambda h: h.tensor_scalar(out=ex[:], in0=ex[:], scalar1=den[:, 0:1], scalar2=None, op0=ALU.mult), reads=[rb], writes=[rb])
                P.op("pe", lambda h: h.transpose(ps[7][0:8, 128:256], ex[:], ident[:]), reads=[rb, onesb], writes=[psb[7]])
                P.op("act", lambda h, t0=t0, tt=tt: h.activation(out=cbT[:, t0 + tt * 128:t0 + (tt + 1) * 128], in_=ps[7][0:8, 128:256], func=AF.Copy), reads=[psb[7]], writes=[cbTb])
                if mode == 'moe_a':
                    P.op("pe", lambda h: h.transpose(ps[7][0:8, 256:384], msk[:], ident[:]), reads=[rb, onesb], writes=[psb[7]])
                    P.op("act", lambda h, t0=t0, tt=tt: h.activation(out=mkT[:, t0 + tt * 128:t0 + (tt + 1) * 128], in_=ps[7][0:8, 256:384], func=AF.Copy), reads=[psb[7]], writes=[mkTb])
    barrier(P)
    P.a_cur = markA
    if mode == 'moe_a':
        fin = [P.dma("sp", lambda h: h.dma_start(out=h2o[:, :, :], in_=h2[:, :, :]), reads=[h2b], writes=[P.buf()]),
               P.dma("sp", lambda h: h.dma_start(out=cbo[:, :], in_=cbT[:, :]), reads=[cbTb], writes=[P.buf()]),
               P.dma("sp", lambda h: h.dma_start(out=mko[:, :], in_=mkT[:, :]), reads=[mkTb], writes=[P.buf()])]
        barrier(P)
        P.finish_wait("sp", fin + P.dbgt)
        P.emit()
        return nc
    blocks = blocksB
    NSL = 4
    P.a_cur = w_lo
    acc = A(P, [128, KC, TT], F32)
    accb = [P.buf() for _ in blocks]
    hid = [A(P, [128, NSL, 512], BF16) for _ in range(2)]
    hidb = [P.buf(), P.buf()]
    ssb_t = [A(P, [128, 512], F32) for _ in range(2)]
    ssbb = [P.buf(), P.buf()]
    cbe = A(P, [128, 512], BF16)
    cbeb = P.buf()
    assert P.a_cur <= w_hi, "stage-B tiles overflow the weight region"
    P.a_cur = markA
    markB = markA
    wg_s = [A(P, [128, KC, NSL * 128], BF16) for _ in range(2)]
    wu_s = [A(P, [128, KC, NSL * 128], BF16) for _ in range(2)]
    wd_s = [A(P, [128, NSL, D], BF16) for _ in range(2)]
    wsb = [P.buf(), P.buf()]
    ntile = dff // 128
    slices = [(s0, min(NSL, ntile - s0)) for s0 in range(0, ntile, NSL)]
    si = 0
    hcnt = 0
    gcnt = 0
    work = [(e, s0, ns) for e in range(n_exp) for (s0, ns) in slices]

    def _ldW(widx):
        e_, s0_, ns_ = work[widx]
        wi_ = widx % 2
        wgv_ = dr["w_g"][e_].rearrange("(k p) f -> p k f", p=128)
        wuv_ = dr["w_u"][e_].rearrange("(k p) f -> p k f", p=128)
        wdv_ = dr["w_d"][e_].rearrange("(j p) c -> p j c", p=128)
        for k2 in range(2):
            P.dma("pool", lambda h, k2=k2: h.dma_start(out=wg_s[wi_][:, 4 * k2:4 * k2 + 4, 0:ns_ * 128], in_=wgv_[:, 4 * k2:4 * k2 + 4, s0_ * 128:(s0_ + ns_) * 128]), writes=[wsb[wi_]])
            P.dma("pool", lambda h, k2=k2: h.dma_start(out=wu_s[wi_][:, 4 * k2:4 * k2 + 4, 0:ns_ * 128], in_=wuv_[:, 4 * k2:4 * k2 + 4, s0_ * 128:(s0_ + ns_) * 128]), writes=[wsb[wi_]])
        for j in range(ns_):
            P.dma("pool", lambda h, j=j: h.dma_start(out=wd_s[wi_][:, j, :], in_=wdv_[:, s0_ + j, :]), writes=[wsb[wi_]])
    _ldW(0)
    for widx, (e, s0, ns) in enumerate(work):
        if True:
            wi = widx % 2
            if widx + 1 < len(work):
                _ldW(widx + 1)
            for bi, (t0, n, r) in enumerate(blocks):
                hi = hcnt % 2
                hcnt += 1
                if moe:
                    P.op("pe", lambda h, e=e, t0=t0, n=n: h.matmul(ps[7][:, 0:n], lhsT=sel[:, e, :], rhs=cbT[:, t0:t0 + n], start=True, stop=True), reads=[cbTb, onesb], writes=[psb[7]])
                    P.op("act", lambda h, n=n: h.activation(out=cbe[:, 0:n], in_=ps[7][:, 0:n], func=AF.Copy), reads=[psb[7]], writes=[cbeb])
                for j in range(ns):
                    gi = gcnt % 2
                    gcnt += 1
                    for k in range(KC):
                        P.op("pe", lambda h, gi=gi, wi=wi, j=j, k=k, t0=t0, n=n: h.matmul(ps[gi][:, 0:n], lhsT=wg_s[wi][:, k, j * 128:(j + 1) * 128], rhs=h2[:, k, t0:t0 + n], start=(k == 0), stop=(k == KC - 1)),
                             reads=[wsb[wi], h2b], writes=[psb[gi]])
                    for k in range(KC):
                        P.op("pe", lambda h, gi=gi, wi=wi, j=j, k=k, t0=t0, n=n: h.matmul(ps[2 + gi][:, 0:n], lhsT=wu_s[wi][:, k, j * 128:(j + 1) * 128], rhs=h2[:, k, t0:t0 + n], start=(k == 0), stop=(k == KC - 1)),
                             reads=[wsb[wi], h2b], writes=[psb[2 + gi]])
                    P.op("act", lambda h, gi=gi, n=n: h.activation(out=ssb_t[gi][:, 0:n], in_=ps[gi][:, 0:n], func=AF.Silu), reads=[psb[gi]], writes=[ssbb[gi]])
                    if moe:
                        P.op("dve", lambda h, gi=gi, n=n: h.tensor_tensor(out=ssb_t[gi][:, 0:n], in0=ssb_t[gi][:, 0:n], in1=ps[2 + gi][:, 0:n], op=ALU.mult),
                             reads=[ssbb[gi], psb[2 + gi]], writes=[ssbb[gi]])
                        P.op("pool", lambda h, gi=gi, hi=hi, j=j, n=n: h.tensor_tensor(out=hid[hi][:, j, 0:n], in0=ssb_t[gi][:, 0:n], in1=cbe[:, 0:n], op=ALU.mult),
                             reads=[ssbb[gi], cbeb], writes=[hidb[hi]])
                    else:
                        P.op("dve", lambda h, gi=gi, hi=hi, j=j, n=n: h.tensor_tensor(out=hid[hi][:, j, 0:n], in0=ssb_t[gi][:, 0:n], in1=ps[2 + gi][:, 0:n], op=ALU.mult),
                             reads=[ssbb[gi], psb[2 + gi]], writes=[hidb[hi]])
                first = (e == 0 and s0 == 0)
                for fc in range(8):
                    oi = 4 + fc % 2
                    for j in range(ns):
                        P.op("pe", lambda h, oi=oi, wi=wi, j=j, fc=fc, hi=hi, n=n, ns=ns: h.matmul(ps[oi][:, 0:n], lhsT=wd_s[wi][:, j, fc * 128:(fc + 1) * 128], rhs=hid[hi][:, j, 0:n], start=(j == 0), stop=(j == ns - 1)),
                             reads=[wsb[wi], hidb[hi]], writes=[psb[oi]])
                    if first:
                        P.op("act", lambda h, oi=oi, fc=fc, t0=t0, n=n: h.activation(out=acc[:, fc, t0:t0 + n], in_=ps[oi][:, 0:n], func=AF.Copy), reads=[psb[oi]], writes=[accb[bi]])
                    else:
                        P.op("dve", lambda h, oi=oi, fc=fc, t0=t0, n=n: h.tensor_tensor(out=acc[:, fc, t0:t0 + n], in0=acc[:, fc, t0:t0 + n], in1=ps[oi][:, 0:n], op=ALU.add),
                             reads=[psb[oi], accb[bi]], writes=[accb[bi]])
    barrier(P)
    P.a_cur = markB
    xm = [A(P, [128, KC, 512], F32) for _ in range(2)]
    xmb = [P.buf(), P.buf()]
    sqF = A(P, [128, KC, 512], BF16)
    rstdF = A(P, [128, 512], F32)
    tmpF = [A(P, [128, 512], F32) for _ in range(2)]
    outs = []
    for bi, (t0, n, r) in enumerate(blocks):
        x_t, x_b = xm[bi % 2], xmb[bi % 2]
        P.dma("sp", lambda h, x_t=x_t, t0=t0, n=n: h.dma_start(out=x_t[:, :, 0:n], in_=xoT[:, :, t0:t0 + n]), reads=[xob], writes=[x_b])
        accv = acc[:, :, t0:t0 + n]
        P.op("act", lambda h, accv=accv, n=n: h.activation(out=sqF[:, :, 0:n], in_=accv, func=AF.Square), reads=[accb[bi]], writes=[sqb])
        for k in range(KC):
            P.op("pe", lambda h, k=k, n=n: h.matmul(ps[6][:, 0:n], lhsT=ones[:], rhs=sqF[:, k, 0:n], start=(k == 0), stop=(k == KC - 1)), reads=[sqb, onesb], writes=[psb[6]])
        P.op("act", lambda h, n=n: h.activation(out=rstdF[:, 0:n], in_=ps[6][:, 0:n], func=AF.Ln, scale=1.0 / D, bias=P.eps_t[:, 0:1]), reads=[psb[6]], writes=[rstdb])
        P.op("act", lambda h, n=n: h.activation(out=rstdF[:, 0:n], in_=rstdF[:, 0:n], func=AF.Exp, scale=-0.5), reads=[rstdb], writes=[rstdb])
        for k in range(KC):
            tb_, tt_ = tmpb[k % 2], tmpF[k % 2]
            P.op("dve", lambda h, k=k, tt_=tt_, n=n, t0=t0: h.tensor_tensor(out=tt_[:, 0:n], in0=acc[:, k, t0:t0 + n], in1=rstdF[:, 0:n], op=ALU.mult), reads=[accb[bi], rstdb], writes=[tb_])
            P.op("dve", lambda h, k=k, tt_=tt_, n=n, x_t=x_t, r=r: h.scalar_tensor_tensor(out=x_t[:, k, 0:n], in0=tt_[:, 0:n], scalar=gg_f[:, k, r:r + 1], in1=x_t[:, k, 0:n],
                                                                                    op0=ALU.mult, op1=ALU.add), reads=[tb_, P.modb, x_b], writes=[x_b])
        outs.append(P.dma("sp", lambda h, x_t=x_t, t0=t0, n=n: h.dma_start(out=xoT[:, :, t0:t0 + n], in_=x_t[:, :, 0:n]), reads=[x_b], writes=[xob]))
    P.finish_wait("sp", outs + P.dbgt)
    P.emit()
    return nc


def build_E(groups=(4,) * 8, dff=3584):
    nc = bass.Bass("TRN2", target_bir_lowering=False)
    ngrp = len(groups)
    gtok = 512 * max(groups)
    NT = 512 * sum(groups)
    goff = [512 * sum(groups[:g]) for g in range(ngrp)]
    h2d = nc.dram_tensor("h2", [KC, 128, NT], BF16, kind="ExternalInput").ap().rearrange("k p t -> p k t")
    cbd = nc.dram_tensor("cbe", [128, NT], BF16, kind="ExternalInput").ap()
    wgd = nc.dram_tensor("w_g", [D, dff], F32, kind="ExternalInput").ap().rearrange("(k p) f -> p k f", p=128)
    wud = nc.dram_tensor("w_u", [D, dff], F32, kind="ExternalInput").ap().rearrange("(k p) f -> p k f", p=128)
    wdd = nc.dram_tensor("w_d", [dff, D], F32, kind="ExternalInput").ap().rearrange("(j p) c -> p j c", p=128)
    ye = nc.dram_tensor("ye", [KC, 128, NT], F32, kind="ExternalOutput").ap().rearrange("k p t -> p k t")
    P = Prog(nc)
    arena_init(P)
    ps = [P.ps("ps%d" % i, [128, 512], F32) for i in range(8)]
    psb = [P.buf("ps%d" % i) for i in range(8)]
    h2g = [A(P, [128, KC, gtok], BF16) for _ in range(2)]
    h2gb = [P.buf(), P.buf()]
    cbg = [A(P, [128, gtok], BF16) for _ in range(2)]
    acc = A(P, [128, KC, gtok], F32)
    NSL = 4
    wg_s = [A(P, [128, KC, NSL * 128], BF16) for _ in range(2)]
    wu_s = [A(P, [128, KC, NSL * 128], BF16) for _ in range(2)]
    wd_s = [A(P, [128, NSL, D], BF16) for _ in range(2)]
    wsb = [P.buf(), P.buf()]
    hid = [A(P, [128, NSL, 512], BF16) for _ in range(2)]
    hidb = [P.buf(), P.buf()]
    ssb_t = [A(P, [128, 512], F32) for _ in range(2)]
    ssbb = [P.buf(), P.buf()]
    ntile = dff // 128
    slices = [(s0, min(NSL, ntile - s0)) for s0 in range(0, ntile, NSL)]
    accb = [P.buf() for _ in range(max(groups))]
    si = hcnt = gcnt = 0
    outs = []
    work = [(g, sidx, s0, ns) for g in range(ngrp) for sidx, (s0, ns) in enumerate(slices)]

    def _ldG(g_):
        hg_, hgb_ = h2g[g_ % 2], h2gb[g_ % 2]
        gn_ = 512 * groups[g_]
        for k2 in range(2):
            _ldF(P, "sp", hg_[:, 4 * k2:4 * k2 + 4, 0:gn_], h2d[:, 4 * k2:4 * k2 + 4, goff[g_]:goff[g_] + gn_], [hgb_])
        _ldF(P, "sp", cbg[g_ % 2][:, 0:gn_], cbd[:, goff[g_]:goff[g_] + gn_], [hgb_])

    def _ldW(widx):
        _g, _sidx, s0_, ns_ = work[widx]
        wi_ = widx % 2
        for k2 in range(2):
            _ldF(P, "pool", wg_s[wi_][:, 4 * k2:4 * k2 + 4, 0:ns_ * 128], wgd[:, 4 * k2:4 * k2 + 4, s0_ * 128:(s0_ + ns_) * 128], [wsb[wi_]])
            _ldF(P, "pool", wu_s[wi_][:, 4 * k2:4 * k2 + 4, 0:ns_ * 128], wud[:, 4 * k2:4 * k2 + 4, s0_ * 128:(s0_ + ns_) * 128], [wsb[wi_]])
        for j in range(ns_):
            _ldF(P, "pool", wd_s[wi_][:, j, :], wdd[:, s0_ + j, :], [wsb[wi_]])
    _ldG(0)
    _ldW(0)
    pending = []

    def _down(u):
        (g_, sidx_, ns_, wi_, bi_, hi_, last_) = u
        t0_, n_ = bi_ * 512, 512
        for fc in range(8):
            oi = 4 + fc % 2
            for j in range(ns_):
                _mmF(P, ps[oi][:, 0:n_], wd_s[wi_][:, j, fc * 128:(fc + 1) * 128], hid[hi_][:, j, 0:n_], j == 0, j == ns_ - 1, [wsb[wi_], hidb[hi_]], [psb[oi]])
            av = acc[:, fc, t0_:t0_ + n_]
            pv = ps[oi][:, 0:n_]
            if sidx_ == 0:
                P.op("act", lambda h, av=av, pv=pv: h.activation(out=av, in_=pv, func=AF.Copy), reads=[psb[oi]], writes=[accb[bi_]])
            else:
                P.op("dve", lambda h, av=av, pv=pv: h.tensor_tensor(out=av, in0=av, in1=pv, op=ALU.add), reads=[psb[oi], accb[bi_]], writes=[accb[bi_]])
        if last_:
            outs.append(_ldF(P, "sp", ye[:, :, goff[g_] + t0_:goff[g_] + t0_ + 512], acc[:, :, t0_:t0_ + 512], [P.buf()], reads=[accb[bi_]]))

    for widx, (g, sidx, s0, ns) in enumerate(work):
        hg, hgb = h2g[g % 2], h2gb[g % 2]
        cg = cbg[g % 2]
        nblk = groups[g]
        if sidx == 0 and g + 1 < ngrp:
            _ldG(g + 1)
        wi = widx % 2
        for bi in range(nblk):
            t0, n = bi * 512, 512
            hi = hcnt % 2
            hcnt += 1
            for j in range(ns):
                gi = gcnt % 2
                gcnt += 1
                for k in range(KC):
                    _mmF(P, ps[gi][:, 0:n], wg_s[wi][:, k, j * 128:(j + 1) * 128], hg[:, k, t0:t0 + n], k == 0, k == KC - 1, [wsb[wi], hgb], [psb[gi]])
                for k in range(KC):
                    _mmF(P, ps[2 + gi][:, 0:n], wu_s[wi][:, k, j * 128:(j + 1) * 128], hg[:, k, t0:t0 + n], k == 0, k == KC - 1, [wsb[wi], hgb], [psb[2 + gi]])
                st_, stb_ = ssb_t[gi], ssbb[gi]
                P.op("act", lambda h, st_=st_, gi=gi, n=n: h.activation(out=st_[:, 0:n], in_=ps[gi][:, 0:n], func=AF.Silu), reads=[psb[gi]], writes=[stb_])
                P.op("dve", lambda h, st_=st_, gi=gi, n=n: h.tensor_tensor(out=st_[:, 0:n], in0=st_[:, 0:n], in1=ps[2 + gi][:, 0:n], op=ALU.mult), reads=[stb_, psb[2 + gi]], writes=[stb_])
                hd = hid[hi]
                P.op("pool", lambda h, st_=st_, hd=hd, j=j, n=n, cg=cg, t0=t0: h.tensor_tensor(out=hd[:, j, 0:n], in0=st_[:, 0:n], in1=cg[:, t0:t0 + n], op=ALU.mult), reads=[stb_, hgb], writes=[hidb[hi]])
            if pending:
                _down(pending.pop(0))
            if bi == 0 and widx + 1 < len(work):
                _ldW(widx + 1)
            pending.append((g, sidx, ns, wi, bi, hi, sidx == len(slices) - 1))
    while pending:
        _down(pending.pop(0))
    P.finish_wait("sp", outs)
    P.emit()
    return nc


def _ldF(P, q, out, in_, writes, reads=()):
    return P.dma(q, lambda h: h.dma_start(out=out, in_=in_), reads=reads, writes=writes)


def _mmF(P, out, lhsT, rhs, start, stop, reads, writes):
    return P.op("pe", lambda h: h.matmul(out, lhsT=lhsT, rhs=rhs, start=start, stop=stop), reads=reads, writes=writes)


def build_Fc(TT=2048, nexp=8):
    nc = bass.Bass("TRN2", target_bir_lowering=False)
    dr = {}

    def din(name, shape, dt=F32):
        dr[name] = nc.dram_tensor(name, list(shape), dt, kind="ExternalInput").ap()
    din("xm", [KC, 128, TT])
    din("yp", [nexp, KC, 128, TT])
    din("condT", [128, KC, 2])
    din("w_mod", [D, 6 * D])
    din("b_modT", [128, 48, 2])
    din("norm_gT", [128, 4, KC, 2])
    xo = nc.dram_tensor("xo", [KC, 128, TT], F32, kind="ExternalOutput").ap().rearrange("k p t -> p k t")
    xm = dr["xm"].rearrange("k p t -> p k t")
    P = Prog(nc)
    arena_init(P)
    ps = [P.ps("ps%d" % i, [128, 512], F32) for i in range(8)]
    psb = [P.buf("ps%d" % i) for i in range(8)]
    ones = A(P, [128, 128], BF16)
    onesb = P.buf()
    P.op("dve", lambda h: h.memset(ones[:], 1.0), writes=[onesb])
    P.eps_t = A(P, [128, 1], F32)
    P.op("dve", lambda h: h.memset(P.eps_t[:], EPS), writes=[onesb])
    mod_view = ps[7][:, 0:96].rearrange("p (j r) -> p j r", r=2)
    compute_mod(P, dr, [5], mod_view, psb[7])
    _, gg_f = mod_derived(P, 4, 5, 2, 3)
    acc = [A(P, [128, KC, 512], F32) for _ in range(2)]
    accb = [P.buf(), P.buf()]
    part = [A(P, [128, KC, 512], F32) for _ in range(3)]
    partb = [P.buf() for _ in range(3)]
    xt = [A(P, [128, KC, 512], F32) for _ in range(2)]
    xtb = [P.buf(), P.buf()]
    sq = A(P, [128, KC, 512], BF16)
    sqb = P.buf()
    rstd = A(P, [128, 512], F32)
    rstdb = P.buf()
    tmp = [A(P, [128, 512], F32) for _ in range(2)]
    tmpb = [P.buf(), P.buf()]
    outs = []
    pc = 0
    for bi in range(TT // 512):
        t0, n = bi * 512, 512
        a_t, a_b = acc[bi % 2], accb[bi % 2]
        x_t, x_b = xt[bi % 2], xtb[bi % 2]
        _ldF(P, "sp", x_t[:, :, :], xm[:, :, t0:t0 + n], [x_b])
        _ldF(P, "sp", a_t[:, :, :], dr["yp"][0].rearrange("k p t -> p k t")[:, :, t0:t0 + n], [a_b])
        for e in range(1, nexp):
            p_t, p_b = part[pc % 3], partb[pc % 3]
            pc += 1
            _ldF(P, "act" if e % 2 else "sp", p_t[:, :, :], dr["yp"][e].rearrange("k p t -> p k t")[:, :, t0:t0 + n], [p_b])
            eng = "dve" if e % 2 else "pool"
            P.op(eng, lambda h, a_t=a_t, p_t=p_t: h.tensor_tensor(out=a_t[:, :, :], in0=a_t[:, :, :], in1=p_t[:, :, :], op=ALU.add), reads=[a_b, p_b], writes=[a_b])
        rms_rstd(P, a_t, a_b, n, sq, sqb, ps[6], psb[6], rstd, rstdb, ones)
        for k in range(KC):
            tb_, tt_ = tmpb[k % 2], tmp[k % 2]
            P.op("dve", lambda h, k=k, tt_=tt_, a_t=a_t: h.tensor_tensor(out=tt_[:, :], in0=a_t[:, k, :], in1=rstd[:, :], op=ALU.mult), reads=[a_b, rstdb], writes=[tb_])
            P.op("dve", lambda h, k=k, tt_=tt_, x_t=x_t: h.scalar_tensor_tensor(out=x_t[:, k, :], in0=tt_[:, :], scalar=gg_f[:, k, 0:1], in1=x_t[:, k, :], op0=ALU.mult, op1=ALU.add),
                 reads=[tb_, P.modb, x_b], writes=[x_b])
        outs.append(_ldF(P, "sp", xo[:, :, t0:t0 + n], x_t[:, :, :], [P.buf()], reads=[x_b]))
    P.finish_wait("sp", outs)
    P.emit()
    return nc


import math, os
RET_STOP = int(os.environ.get('RET_STOP', '99'))
SKIP = os.environ.get('SKIP', '')

NTOK = 8448
NCH = 66
MAGIC = 12582912.0
TWO_PI = 2.0 * math.pi


def pos_of(dd):
    if dd == 0:
        return list(range(NCH))
    order = [1, 0] + list(range(65, 1, -1))
    pos = [0] * NCH
    for p_, c in enumerate(order):
        pos[c] = p_
    return pos


def range_reduce_sincos(P, ph, sn, cs, tmp, shape_ap, b):
    v = shape_ap
    _ts(P, "dve", v(tmp), v(ph), 1.0 / TWO_PI, MAGIC, ALU.mult, ALU.add, [b], [b])
    _ts(P, "dve", v(tmp), v(tmp), -MAGIC, None, ALU.add, None, [b], [b])
    _stt(P, v(ph), v(tmp), -TWO_PI, v(ph), ALU.mult, ALU.add, [b], [b])
    _ts(P, "dve", v(ph), v(ph), -math.pi, math.pi, ALU.max, ALU.min, [b], [b])
    _act(P, v(sn), v(ph), AF.Sin, [b], [b])
    _ts(P, "dve", v(tmp), v(ph), -1.0, None, ALU.mult, None, [b], [b])
    _tt(P, "dve", v(tmp), v(tmp), v(ph), ALU.max, [b], [b])
    _act(P, v(cs), v(tmp), AF.Sin, [b], [b], scale=-1.0, bias=P.halfpi[0:v(tmp).shape[0], 0:1])


def build_M(need_ctx_out, parts=("four", "s5", "ret", "na"), DBG=False):
    nc = bass.Bass("TRN2", target_bir_lowering=False)
    dr = {}

    def din(name, shape, dt=F32):
        dr[name] = nc.dram_tensor(name, list(shape), dt, kind="ExternalInput").ap()
    din("xT", [KC, 128, NTOK])
    din("condT", [128, KC, 2])
    din("w_mod", [D, 6 * D])
    din("b_modT", [128, 48, 2])
    din("norm_gT", [128, 4, KC, 2])
    din("w_fm", [D, 576])
    din("w_tm", [D, 256])
    din("f_CS", [64, 128], BF16); din("f_RP", [64, 128], BF16); din("f_RQ", [64, 128], BF16)
    din("f_CB", [128, 64, 128], BF16); din("f_SB", [128, 64, 128], BF16)
    din("f_C256", [128, 2, 256], BF16); din("f_S256", [128, 2, 256], BF16)
    din("r_cosF", [64, 8192]); din("r_sinF", [64, 8192]); din("r_cosT", [128, 64, 64]); din("r_sinT", [128, 64, 64])
    din("r_jcol", [128, 2]); din("r_dist", [128, 128]); din("r_mask", [2, 128, 128]); din("r_irow", [2, 64, 128])
    din("s_jrow", [128, 129]); din("s_jcol", [128, 1]); din("s_LT", [2, 128, 128], BF16); din("s_mrow", [64, 4]); din("s_msm", [128, 2, 4])
    din("ident_bf", [128, 128], BF16); din("ident_f", [128, 128])
    din("n_mask", [5, 128, 832]); din("n_toep", [15, 64, 64])
    din("s_sm", [128, 2, 2, 3]); din("s_row", [128, 2, 3, 256]); din("s_hs", [64, 2, 3, 64]); din("s_B", [64, 2, 2, 64])
    din("s_C", [128, 2, 2, 2, 16]); din("s_d", [64, 1])
    din("r_dec", [128, 2]); din("r_gn", [64, 1])
    out = nc.dram_tensor("brT_out", [4, 64, NTOK], BF16, kind="ExternalOutput").ap()
    hT = nc.dram_tensor("hT_scr", [KC, 128, NTOK], BF16, kind="Internal").ap().rearrange("k p t -> p k t")
    xT = dr["xT"].rearrange("k p t -> p k t")

    P = Prog(nc)
    arena_init(P)
    ps = [P.ps("ps%d" % i, [128, 512], F32) for i in range(8)]
    psb = [P.buf("ps%d" % i) for i in range(8)]
    cb = P.buf("consts")
    ones = A(P, [128, 128], BF16)
    P.op("dve", lambda h: h.memset(ones[:], 1.0), writes=[cb])
    P.eps_t = A(P, [128, 1], F32)
    P.op("dve", lambda h: h.memset(P.eps_t[:], EPS), writes=[cb])
    P.halfpi = A(P, [128, 1], F32)
    P.op("dve", lambda h: h.memset(P.halfpi[:], math.pi / 2), writes=[cb])
    P.one_t = A(P, [128, 1], F32)
    P.op("dve", lambda h: h.memset(P.one_t[:], 1.0), writes=[cb])
    ident = A(P, [128, 128], BF16)
    _ld(P, "sp", ident[:], dr["ident_bf"][:, :], [cb])
    mod_view = ps[7][:, 0:96].rearrange("p (j r) -> p j r", r=2)
    compute_mod(P, dr, [0, 1], mod_view, psb[7])
    gm_a, _ = mod_derived(P, 1, None, 0, 0)
    sh_a = P.modT[:, 0:8, :]
    wfm = A(P, [128, KC, 576], BF16)
    wtm = A(P, [128, KC, 256], BF16)
    wb = P.buf("w")
    _ld(P, "pool", wfm[:], dr["w_fm"].rearrange("(k p) c -> p k c", p=128), [wb])
    _ld(P, "pool", wtm[:], dr["w_tm"].rearrange("(k p) c -> p k c", p=128), [wb])
    blocks = [(0, 256, 1)] + [(256 + 512 * i, 512, 0) for i in range(16)]
    outs = []
    hTb = P.buf("hT")
    mark0 = P.a_cur

    def fm_proj(hb, hbb, n, g, pst, pstb):
        for k in range(KC):
            _mm(P, pst[0:64, 0:n], wfm[:, k, g * 64:(g + 1) * 64], hb[:, k, 0:n], k == 0, k == KC - 1, [wb, hbb], [pstb])

    sT = A(P, [64, NTOK], BF16)
    markS = P.a_cur
    fT = A(P, [64, NTOK], BF16)
    fTb, sTb = P.buf("fT"), P.buf("sT")
    markA = P.a_cur
    xb = [A(P, [128, KC, 512], F32) for _ in range(2)]
    xbb = [P.buf(), P.buf()]
    sq = A(P, [128, KC, 512], BF16)
    sqb = P.buf()
    rstd = A(P, [128, 512], F32)
    rstdb = P.buf()
    tmp = [A(P, [128, 512], F32) for _ in range(8)]
    tmpb = [P.buf() for _ in range(8)]
    hbs = [A(P, [128, KC, 512], BF16) for _ in range(2)]
    hbsb = [P.buf(), P.buf()]
    def _ldx(bi_):
        t0_, n_, _r = blocks[bi_]
        _ld(P, "sp", xb[bi_ % 2][:, 0:4, 0:n_], xT[:, 0:4, t0_:t0_ + n_], [xbb[bi_ % 2]])
        _ld(P, "sp", xb[bi_ % 2][:, 4:8, 0:n_], xT[:, 4:8, t0_:t0_ + n_], [xbb[bi_ % 2]])
    _ldx(0)
    for bi, (t0, n, r) in enumerate(blocks):
        x_t, x_b = xb[bi % 2], xbb[bi % 2]
        hb, hbb = hbs[bi % 2], hbsb[bi % 2]
        if bi + 1 < len(blocks):
            _ldx(bi + 1)
        rms_rstd(P, x_t, x_b, n, sq, sqb, ps[6], psb[6], rstd, rstdb, ones)
        norm_mod(P, x_t, x_b, n, rstd, rstdb, gm_a, sh_a, r, hb, hbb, tmp, tmpb)
        _ld(P, "sp", hT[:, :, t0:t0 + n], hb[:, :, 0:n], [hTb], reads=[hbb])
        for gi_, (g, dst, dstb) in enumerate(((0, fT, fTb), (1, sT, sTb))):
            pi_ = (2 * bi + gi_) % 4
            fm_proj(hb, hbb, n, g, ps[pi_], psb[pi_])
            _cp(P, "act" if gi_ == 0 else "dve", dst[:, t0:t0 + n], ps[pi_][0:64, 0:n], [psb[pi_]], [dstb])
    barrier(P)
    P.a_cur = markA

    if "four" in parts:
        markF = P.a_cur
        CS = A(P, [64, 128], BF16); RP = A(P, [64, 128], BF16); RQ = A(P, [64, 128], BF16)
        CB = A(P, [128, 64, 128], BF16); SB = A(P, [128, 64, 128], BF16)
        ftb = P.buf("ftab")
        for t_, nm in ((CS, "f_CS"), (RP, "f_RP"), (RQ, "f_RQ")):
            _ld(P, "sp", t_[:], dr[nm][:, :], [ftb])
        _ld(P, "sp", CB[:], dr["f_CB"][:, :, :], [ftb])
        _ld(P, "sp", SB[:], dr["f_SB"][:, :, :], [ftb])
        PQ = A(P, [64, 128, 128], BF16); PQb = P.buf("PQ")
        UVT = A(P, [128, 64, 128], BF16); UVTb = P.buf("UVT")
        aT = A(P, [64, NTOK], BF16); aTb = P.buf("aT")
        for g4 in range(32):
            pi_ = g4 % 2
            for jj in range(4):
                m2 = g4 * 4 + jj
                _mm(P, ps[pi_][0:64, jj * 128:(jj + 1) * 128], fT[:, 256 + m2:NTOK:128], CS[:, :], True, True, [fTb, ftb], [psb[pi_]])
            _cp(P, "act" if g4 % 2 else "dve", PQ[:, g4 * 4:(g4 + 1) * 4, :], ps[pi_][0:64, 0:512].rearrange("p (a b) -> p a b", b=128), [psb[pi_]], [PQb])
        for g4 in range(16):
            pi_ = 2 + g4 % 2
            for jj in range(4):
                d = g4 * 4 + jj
                _mm(P, ps[pi_][:, jj * 128:(jj + 1) * 128], PQ[:, :, d], RP[:, :], True, False, [PQb, ftb], [psb[pi_]])
                _mm(P, ps[pi_][:, jj * 128:(jj + 1) * 128], PQ[:, :, 64 + d], RQ[:, :], False, True, [PQb, ftb], [psb[pi_]])
            _cp(P, "act" if g4 % 2 else "dve", UVT[:, g4 * 4:(g4 + 1) * 4, :], ps[pi_][:, 0:512].rearrange("p (a b) -> p a b", b=128), [psb[pi_]], [UVTb])
        aT3 = aT[:, 256:NTOK].rearrange("p (a b) -> p a b", b=64)
        for g4 in range(16):
            pi_ = g4 % 2
            for jj in range(4):
                n1 = g4 * 4 + jj
                _mm(P, ps[pi_][0:64, jj * 128:(jj + 1) * 128], UVT[:, :, n1], CB[:, n1, :], True, False, [UVTb, ftb], [psb[pi_]])
                _mm(P, ps[pi_][0:64, jj * 128:(jj + 1) * 128], UVT[:, :, 64 + n1], SB[:, n1, :], False, True, [UVTb, ftb], [psb[pi_]])
            _cp(P, "act" if g4 % 2 else "dve", aT3[:, :, g4 * 4:(g4 + 1) * 4], ps[pi_][0:64, 0:512].rearrange("p (j n) -> p n j", n=128), [psb[pi_]], [aTb])
        if need_ctx_out:
            C256 = A(P, [128, 2, 256], BF16); S256 = A(P, [128, 2, 256], BF16)
            _ld(P, "sp", C256[:], dr["f_C256"][:, :, :], [ftb])
            _ld(P, "sp", S256[:], dr["f_S256"][:, :, :], [ftb])
            PQc = A(P, [128, 2, 128], BF16); PQcb = P.buf()
            for tt in range(2):
                _mm(P, ps[2 + tt][:, 0:128], fT[:, tt * 128:(tt + 1) * 128], CS[:, :], True, True, [fTb, ftb], [psb[2 + tt]])
                _cp(P, "dve", PQc[:, tt, :], ps[2 + tt][:, 0:128], [psb[2 + tt]], [PQcb])
            seq = [(tt, 0) for tt in range(2)] + [(tt, 1) for tt in range(2)]
            for i_, (tt, pq) in enumerate(seq):
                _mm(P, ps[4][0:64, 0:256], PQc[:, tt, pq * 64:(pq + 1) * 64], (C256 if pq == 0 else S256)[:, tt, :], i_ == 0, i_ == 3, [PQcb, ftb], [psb[4]])
            _cp(P, "dve", aT[:, 0:256], ps[4][0:64, 0:256], [psb[4]], [aTb])
        else:
            P.op("dve", lambda h: h.memset(aT[:, 0:256], 0.0), writes=[aTb])
        outs.append(_ld(P, "sp", out[0, :, :], aT[:, :], [P.buf()], reads=[aTb]))
        barrier(P)
    P.a_cur = markS

    if "s5" in parts:
        s5_part(P, dr, ps, psb, sT, sTb, out, outs, need_ctx_out, cb)
    barrier(P)
    P.a_cur = mark0

    if "ret" in parts:
        ret_part(P, dr, ps, psb, hT, hTb, wfm, wtm, wb, blocks, out, outs, need_ctx_out, cb, ones)
        barrier(P)
        P.a_cur = mark0
    if "na" in parts:
        na_part(P, dr, ps, psb, hT, hTb, wfm, wtm, wb, blocks, out, outs, need_ctx_out, cb, ident)
        barrier(P)
    P.finish_wait("sp", outs)
    P.emit()
    return nc


def load_h(P, hT, hTb, hbs, hbsb, bi, t0, n):
    hb, hbb = hbs[bi % 2], hbsb[bi % 2]
    _ld(P, "sp", hb[:, :, 0:n], hT[:, :, t0:t0 + n], [hbb], reads=[hTb])
    return hb, hbb


def na_part(P, dr, ps, psb, hT, hTb, wfm, wtm, wb, blocks, out, outs, need_ctx_out, cb, ident):
    Cn = consts()
    drs, types = Cn["n_drs"], Cn["n_types"]
    nqT = A(P, [64, NTOK], BF16); nkT = A(P, [64, NTOK], BF16); nvT = A(P, [128, NCH, 64], BF16)
    nqb, nkb, nvb = P.buf("nq"), P.buf("nk"), P.buf("nv")
    nT = A(P, [64, NTOK], BF16); nTb = P.buf("nT")
    bias = A(P, [128, 5, 832], F32); biasb = P.buf("bias")
    mask = A(P, [128, 5, 832], F32)
    P.op("pool", lambda h: h.memset(bias[:], 0.0), writes=[biasb])
    maskb = P.buf()
    _ld(P, "sp", mask[:], dr["n_mask"].rearrange("t p c -> p t c"), [maskb])
    for ti in range(5):
        for qr in range(2):
            for i in range(9):
                _ld(P, "sp" if (i % 2) else "act", bias[qr * 64:(qr + 1) * 64, ti, i * 64:(i + 1) * 64], dr["n_toep"][int(drs[ti, qr, i])], [biasb])
    _tt(P, "dve", bias[:], bias[:], mask[:], ALU.add, [biasb, maskb], [biasb])
    mark = P.a_cur
    hbs = [A(P, [128, KC, 512], BF16) for _ in range(2)]
    hbsb = [P.buf(), P.buf()]
    cnt = 0
    for bi, (t0, n, r) in enumerate(blocks):
        hb, hbb = load_h(P, hT, hTb, hbs, hbsb, bi, t0, n)
        for g, dst, dstb in ((7, nqT, nqb), (8, nkT, nkb)):
            pi_ = cnt % 4
            cnt += 1
            for k in range(KC):
                _mm(P, ps[pi_][0:64, 0:n], wfm[:, k, g * 64:(g + 1) * 64], hb[:, k, 0:n], k == 0, k == KC - 1, [wb, hbb], [psb[pi_]])
            _cp(P, "act" if g == 7 else "dve", dst[:, t0:t0 + n], ps[pi_][0:64, 0:n], [psb[pi_]], [dstb])
        for tt in range(n // 128):
            pi_ = 4 + (tt % 2)
            for k in range(KC):
                _mm(P, ps[pi_][:, 0:64], hb[:, k, tt * 128:(tt + 1) * 128], wtm[:, k, 192:256], k == 0, k == KC - 1, [wb, hbb], [psb[pi_]])
            _cp(P, "act", nvT[:, t0 // 128 + tt, :], ps[pi_][:, 0:64], [psb[pi_]], [nvb])
    barrier(P)
    P.a_cur = mark
    NB4 = 4
    s_t = [A(P, [128, 832], F32) for _ in range(NB4)]; s_b = [P.buf() for _ in range(NB4)]
    p_t = [A(P, [128, 832], BF16) for _ in range(NB4)]; p_b = [P.buf() for _ in range(NB4)]
    pT = [A(P, [128, 7, 128], BF16) for _ in range(NB4)]; pTb = [P.buf() for _ in range(NB4)]
    st_ = [A(P, [128, 4], F32) for _ in range(NB4)]; stb = [P.buf() for _ in range(NB4)]
    SC = 0.125

    def softmax_pv(qi, ncols, pv_list, o_ps, o_psb, o_cols, sbi, s4=None):
        if s4 is None:
            s4 = sbi
        s, sb_ = s_t[s4], s_b[s4]
        sm, smb = st_[s4], stb[s4]
        P.op("dve", lambda h: h.reduce_max(out=sm[:, 1:2], in_=s[:, 0:ncols], axis=AX.X, negate=True), reads=[sb_], writes=[smb])
        _act(P, s[:, 0:ncols], s[:, 0:ncols], AF.Exp, [sb_, smb], [sb_], bias=sm[:, 1:2], scale=1.0)
        P.op("dve", lambda h: h.reduce_sum(out=sm[:, 2:3], in_=s[:, 0:ncols], axis=AX.X), reads=[sb_], writes=[smb])
        P.op("dve", lambda h: h.reciprocal(out=sm[:, 3:4], in_=sm[:, 2:3]), reads=[smb], writes=[smb])
        p, pb = p_t[s4], p_b[s4]
        _ts(P, "dve", p[:, 0:ncols], s[:, 0:ncols], sm[:, 3:4], None, ALU.mult, None, [sb_, smb], [pb])
        tp = ps[4 + sbi][:, :].bitcast(BF16)
        for ci, (c0, nk, tile) in enumerate(pv_list):
            _tr(P, tp[0:nk, ci * 128:(ci + 1) * 128], p[:, c0:c0 + nk], ident[:, :], [pb, cb], [psb[4 + sbi]])
        nchk = len(pv_list)
        pt_, ptb = pT[s4], pTb[s4]
        _cp(P, "act", pt_[:, 0:nchk, :], tp[:, 0:nchk * 128].rearrange("p (a b) -> p a b", b=128), [psb[4 + sbi]], [ptb])
        for ci, (c0, nk, tile) in enumerate(pv_list):
            _mm(P, o_ps[0:64, o_cols:o_cols + 128], nvT[0:nk, tile, :], pt_[0:nk, ci, :], ci == 0, ci == nchk - 1, [nvb, ptb], [o_psb])

    def tile_geo(rp):
        ti = {0: 0, 1: 1, 62: 3, 63: 4}.get(rp, 2)
        r0 = 2 * rp
        if ti == 2:
            R0, nr = r0 - 4, 9
        else:
            R0, nr = types[ti][1], 8
        t_base = 2 + R0 // 2
        pv = [(128 * j, 128, t_base + j) for j in range(4)]
        if nr == 9:
            pv.append((512, 64, t_base + 4))
        pv += [(576, 128, 0), (704, 128, 1)]
        return ti, R0, nr, pv

    def stage_S(rp):
        ti, R0, nr, pv = tile_geo(rp)
        tq = 256 + 128 * rp
        kb_ = 256 + 64 * R0
        sbi = rp % 2
        s1, s2 = ps[sbi], ps[2 + sbi]
        _mm(P, s1[:, 0:512], nqT[:, tq:tq + 128], nkT[:, kb_:kb_ + 512], True, True, [nqb, nkb], [psb[sbi]])
        kb2 = kb_ + 512 if nr == 9 else kb_
        _mm(P, s2[:, 0:64], nqT[:, tq:tq + 128], nkT[:, kb2:kb2 + 64], True, True, [nqb, nkb], [psb[2 + sbi]])
        _mm(P, s2[:, 64:320], nqT[:, tq:tq + 128], nkT[:, 0:256], True, True, [nqb, nkb], [psb[2 + sbi]])
        s4 = rp % NB4
        s = s_t[s4]
        _stt(P, s[:, 0:512], s1[:, 0:512], SC, bias[:, ti, 0:512], ALU.mult, ALU.add, [psb[sbi], biasb], [s_b[s4]])
        _stt(P, s[:, 512:832], s2[:, 0:320], SC, bias[:, ti, 512:832], ALU.mult, ALU.add, [psb[2 + sbi], biasb], [s_b[s4]])

    def stage_M(rp, ncols=832):
        s4 = rp % NB4
        s, sb_ = s_t[s4], s_b[s4]
        sm, smb = st_[s4], stb[s4]
        P.op("dve", lambda h: h.reduce_max(out=sm[:, 1:2], in_=s[:, 0:ncols], axis=AX.X, negate=True), reads=[sb_], writes=[smb])
        _act(P, s[:, 0:ncols], s[:, 0:ncols], AF.Exp, [sb_, smb], [sb_], bias=sm[:, 1:2], scale=1.0)
        P.op("dve", lambda h: h.reduce_sum(out=sm[:, 2:3], in_=s[:, 0:ncols], axis=AX.X), reads=[sb_], writes=[smb])
        P.op("dve", lambda h: h.reciprocal(out=sm[:, 3:4], in_=sm[:, 2:3]), reads=[smb], writes=[smb])
        _ts(P, "dve", p_t[s4][:, 0:ncols], s[:, 0:ncols], sm[:, 3:4], None, ALU.mult, None, [sb_, smb], [p_b[s4]])

    def stage_TV(rp):
        ti, R0, nr, pv_list = tile_geo(rp)
        sbi = rp % 2
        s4 = rp % NB4
        p, pb = p_t[s4], p_b[s4]
        tp = ps[4 + sbi][:, :].bitcast(BF16)
        for ci, (c0, nk, tile) in enumerate(pv_list):
            _tr(P, tp[0:nk, ci * 128:(ci + 1) * 128], p[:, c0:c0 + nk], ident[:, :], [pb, cb], [psb[4 + sbi]])
        nchk = len(pv_list)
        pt_, ptb = pT[s4], pTb[s4]
        _cp(P, "act", pt_[:, 0:nchk, :], tp[:, 0:nchk * 128].rearrange("p (a b) -> p a b", b=128), [psb[4 + sbi]], [ptb])
        jj = rp % 4
        for ci, (c0, nk, tile) in enumerate(pv_list):
            _mm(P, ps[6][0:64, jj * 128:(jj + 1) * 128], nvT[0:nk, tile, :], pt_[0:nk, ci, :], ci == 0, ci == nchk - 1, [nvb, ptb], [psb[6]])
        if jj == 3:
            _cp(P, "dve", nT[:, 256 + 512 * (rp // 4):256 + 512 * (rp // 4 + 1)], ps[6][0:64, 0:512], [psb[6]], [nTb])

    stage_S(0)
    stage_S(1)
    stage_M(0)
    for rp in range(64):
        if rp + 2 < 64:
            stage_S(rp + 2)
        if rp + 1 < 64:
            stage_M(rp + 1)
        stage_TV(rp)
    if need_ctx_out:
        for qt in range(2):
            sbi = qt
            _mm(P, ps[sbi][:, 0:256], nqT[:, qt * 128:(qt + 1) * 128], nkT[:, 0:256], True, True, [nqb, nkb], [psb[sbi]])
            _ts(P, "dve", s_t[sbi][:, 0:256], ps[sbi][:, 0:256], SC, None, ALU.mult, None, [psb[sbi]], [s_b[sbi]])
            softmax_pv(qt, 256, [(0, 128, 0), (128, 128, 1)], ps[7], psb[7], qt * 128, sbi)
        _cp(P, "dve", nT[:, 0:256], ps[7][0:64, 0:256], [psb[7]], [nTb])
    else:
        P.op("dve", lambda h: h.memset(nT[:, 0:256], 0.0), writes=[nTb])
    outs.append(_ld(P, "sp", out[3, :, :], nT[:, :], [P.buf()], reads=[nTb]))


def ret_part(P, dr, ps, psb, hT, hTb, wfm, wtm, wb, blocks, out, outs, need_ctx_out, cb, ones):
    KS = 0.125
    qT = A(P, [64, NTOK], BF16); kT = A(P, [64, NTOK], BF16); gT = A(P, [64, NTOK], BF16)
    qb_, kb_, gb_ = P.buf("q"), P.buf("k"), P.buf("g")
    rvT = A(P, [128, NCH, 64], BF16); rvb = P.buf("rv")
    Sbf = [A(P, [64, NCH, 64], BF16) for _ in range(2)]
    rc = P.buf("retc")
    dec = A(P, [128, 2], F32); lg = A(P, [128, 2], F32); jcol = A(P, [128, 2], F32); kdec = A(P, [128, 2], F32); g128 = A(P, [128, 2], F32)
    dist = A(P, [128, 128], F32); msk = A(P, [128, 2, 128], F32); DT = A(P, [128, 128], F32); DT2 = A(P, [128, 128], F32)
    irow = A(P, [64, 2, 128], F32); qdec = A(P, [64, 2, 128], F32)
    gn = A(P, [64, 1], F32); o64 = A(P, [64, 64], F32)
    _ld(P, "sp", dec[:], dr["r_dec"][:, :], [rc])
    _ld(P, "sp", jcol[:], dr["r_jcol"][:, :], [rc])
    _ld(P, "sp", dist[:], dr["r_dist"][:, :], [rc])
    _ld(P, "sp", msk[:], dr["r_mask"].rearrange("d j i -> j d i"), [rc])
    _ld(P, "sp", irow[:], dr["r_irow"].rearrange("d p i -> p d i"), [rc])
    _ld(P, "sp", gn[:], dr["r_gn"][:, :], [rc])
    P.op("dve", lambda h: h.memset(o64[:], 1.0 / 64), writes=[rc])
    _act(P, lg[:], dec[:], AF.Exp, [rc], [rc], scale=-1.0)
    _act(P, lg[:], lg[:], AF.Ln, [rc], [rc], bias=P.one_t[:, 0:1], scale=1.0)
    _ts(P, "dve", lg[:], lg[:], -1.0, None, ALU.mult, None, [rc], [rc])
    for dd in range(2):
        _act(P, kdec[:, dd:dd + 1], jcol[:, dd:dd + 1], AF.Exp, [rc], [rc], scale=lg[:, dd:dd + 1])
        _act(P, g128[:, dd:dd + 1], lg[:, dd:dd + 1], AF.Exp, [rc], [rc], scale=128.0)
        _act(P, qdec[:, dd, :], irow[:, dd, :], AF.Exp, [rc], [rc], scale=lg[0:64, dd:dd + 1])
    _ts(P, "dve", kdec[:], kdec[:], KS, None, ALU.mult, None, [rc], [rc])
    _act(P, DT[:], dist[:], AF.Exp, [rc], [rc], scale=lg[:, 0:1])
    _tt(P, "dve", DT[:], DT[:], msk[:, 0, :], ALU.mult, [rc], [rc])
    _act(P, DT2[:], dist[:], AF.Exp, [rc], [rc], scale=lg[:, 1:2])
    _tt(P, "dve", DT2[:], DT2[:], msk[:, 1, :], ALU.mult, [rc], [rc])
    _tt(P, "dve", DT[:], DT[:], DT2[:], ALU.add, [rc], [rc])
    if RET_STOP <= 0:
        return
    mark_k = P.a_cur
    kd = [A(P, [128, NCH, 64], BF16) for _ in range(2)]
    kdb = [P.buf(), P.buf()]
    mark = P.a_cur
    hbs = [A(P, [128, KC, 512], BF16) for _ in range(2)]
    hbsb = [P.buf(), P.buf()]
    cF = [A(P, [64, 512], F32) for _ in range(2)]; sF = [A(P, [64, 512], F32) for _ in range(2)]
    cTt = [A(P, [128, 4, 64], F32) for _ in range(2)]; sTt = [A(P, [128, 4, 64], F32) for _ in range(2)]
    tabb = [P.buf(), P.buf()]
    t1 = [A(P, [128, 512], F32) for _ in range(2)]; t1b = [P.buf(), P.buf()]
    t2 = [A(P, [128, 512], F32) for _ in range(2)]; t2b = [P.buf(), P.buf()]
    cnt = 0
    for bi, (t0, n, r) in enumerate(blocks):
        hb, hbb = load_h(P, hT, hTb, hbs, hbsb, bi, t0, n)
        lat = (r == 0)
        tb_ = tabb[bi % 2]
        if lat:
            m0 = t0 - 256
            _ld(P, "sp", cF[bi % 2][:, :], dr["r_cosF"][:, m0:m0 + 512], [tb_])
            _ld(P, "sp", sF[bi % 2][:, :], dr["r_sinF"][:, m0:m0 + 512], [tb_])
            _ld(P, "sp", cTt[bi % 2][:, :, :], dr["r_cosT"][:, m0 // 128:m0 // 128 + 4, :], [tb_])
            _ld(P, "sp", sTt[bi % 2][:, :, :], dr["r_sinT"][:, m0 // 128:m0 // 128 + 4, :], [tb_])

        def proj(g, pi_):
            for k in range(KC):
                _mm(P, ps[pi_][0:64, 0:n], wfm[:, k, g * 64:(g + 1) * 64], hb[:, k, 0:n], k == 0, k == KC - 1, [wb, hbb], [psb[pi_]])
        for (g, gsw, dst, dstb, scl) in ((2, 4, qT, qb_, 1.0), (3, 5, kT, kb_, KS)):
            if 'qk' in SKIP:
                continue
            proj(g, 0)
            if lat:
                proj(gsw, 1)
                i2 = cnt % 2
                cnt += 1
                _stt(P, t1[i2][0:64, 0:n], ps[0][0:64, 0:n], scl, cF[bi % 2][:, 0:n], ALU.mult, ALU.mult, [psb[0], tb_], [t1b[i2]])
                _stt(P, t2[i2][0:64, 0:n], ps[1][0:64, 0:n], scl, sF[bi % 2][:, 0:n], ALU.mult, ALU.mult, [psb[1], tb_], [t2b[i2]])
                _tt(P, "pool", dst[:, t0:t0 + n], t1[i2][0:64, 0:n], t2[i2][0:64, 0:n], ALU.add, [t1b[i2], t2b[i2]], [dstb])
            else:
                _act(P, dst[:, t0:t0 + n], ps[0][0:64, 0:n], AF.Copy, [psb[0]], [dstb], scale=scl)
        proj(6, 2)
        _cp(P, "act", gT[:, t0:t0 + n], ps[2][0:64, 0:n], [psb[2]], [gb_])
        for tt in range(n // 128):
            if 'tm' in SKIP:
                continue
            pi_ = 4 + (tt % 2)
            tile = t0 // 128 + tt
            for k in range(KC):
                _mm(P, ps[pi_][:, 0:192], hb[:, k, tt * 128:(tt + 1) * 128], wtm[:, k, 0:192], k == 0, k == KC - 1, [wb, hbb], [psb[pi_]])
            _cp(P, "act", rvT[:, tile, :], ps[pi_][:, 128:192], [psb[pi_]], [rvb])
            if 'kd' in SKIP:
                continue
            if ('kl' in SKIP and lat) or ('kc' in SKIP and not lat):
                continue
            if lat:
                i2 = cnt % 2
                cnt += 1
                _tt(P, "dve", t1[i2][:, 0:64], ps[pi_][:, 0:64], cTt[bi % 2][:, tt, :], ALU.mult, [psb[pi_], tb_], [t1b[i2]])
                _tt(P, "dve", t2[i2][:, 0:64], ps[pi_][:, 64:128], sTt[bi % 2][:, tt, :], ALU.mult, [psb[pi_], tb_], [t2b[i2]])
                if 'k1' in SKIP:
                    continue
                _tt(P, "dve", t1[i2][:, 0:64], t1[i2][:, 0:64], t2[i2][:, 0:64], ALU.add, [t1b[i2], t2b[i2]], [t1b[i2]])
                if 'k2' in SKIP:
                    continue
                for dd in range(2):
                    _act(P, kd[dd][:, tile, :], t1[i2][:, 0:64], AF.Identity, [t1b[i2], rc], [kdb[dd]], scale=kdec[:, dd:dd + 1])
            else:
                for dd in range(2):
                    _act(P, kd[dd][:, tile, :], ps[pi_][:, 0:64], AF.Identity, [psb[pi_], rc], [kdb[dd]], scale=kdec[:, dd:dd + 1])
    barrier(P)
    P.a_cur = mark
    if RET_STOP <= 1:
        return
    S32 = [A(P, [64, NCH, 64], F32) for _ in range(2)]
    Sb = [P.buf(), P.buf()]
    orders = []
    for dd in range(2):
        pos = pos_of(dd)
        orders.append(sorted(range(NCH), key=lambda c, pos=pos: pos[c]))
        c0 = orders[dd][0]
        P.op("dve", lambda h, dd=dd, c0=c0: h.memset(S32[dd][:, c0, :], 0.0), writes=[Sb[dd]])
    for idx in range(NCH - 1):
        for dd in range(2):
            c, nxt = orders[dd][idx], orders[dd][idx + 1]
            pi_ = 2 * dd + (idx // 8) % 2
            sl = idx % 8
            _mm(P, ps[pi_][0:64, sl * 64:(sl + 1) * 64], kd[dd][:, c, :], rvT[:, c, :], True, True, [kdb[dd], rvb], [psb[pi_]])
            _stt(P, S32[dd][:, nxt, :], S32[dd][:, c, :], g128[0:64, dd:dd + 1], ps[pi_][0:64, sl * 64:(sl + 1) * 64], ALU.mult, ALU.add, [Sb[dd], psb[pi_], rc], [Sb[dd]])
    for dd in range(2):
        _cp(P, "act", Sbf[dd][:], S32[dd][:], [Sb[dd]], [Sb[dd]])
    barrier(P)
    P.a_cur = mark_k
    if RET_STOP <= 2:
        return
    oT = A(P, [64, NTOK], F32); oTb = P.buf("oT")
    sc = [A(P, [128, 128], BF16) for _ in range(2)]; scb = [P.buf(), P.buf()]
    qd = [[A(P, [64, 128], BF16) for _ in range(2)] for _ in range(2)]
    qdb = [[P.buf(), P.buf()] for _ in range(2)]
    c_start = 0 if need_ctx_out else 2
    if not need_ctx_out:
        P.op("pool", lambda h: h.memset(oT[:, 0:256], 0.0), writes=[oTb])
    for c in range(c_start, NCH):
        tau = 128 * c
        i2 = c % 2
        _mm(P, ps[i2][:, 0:128], kT[:, tau:tau + 128], qT[:, tau:tau + 128], True, True, [kb_, qb_], [psb[i2]])
        _tt(P, "dve", sc[i2][:, :], ps[i2][:, 0:128], DT[:, :], ALU.mult, [psb[i2], rc], [scb[i2]])
        for dd in range(2):
            _tt(P, "pool", qd[dd][i2][:, :], qT[:, tau:tau + 128], qdec[:, dd, :], ALU.mult, [qb_, rc], [qdb[dd][i2]])
        jj = c % 4
        po = ps[4 + (c // 4) % 2]
        pob = psb[4 + (c // 4) % 2]
        _mm(P, po[0:64, jj * 128:(jj + 1) * 128], rvT[:, c, :], sc[i2][:, :], True, False, [rvb, scb[i2]], [pob])
        _mm(P, po[0:64, jj * 128:(jj + 1) * 128], Sbf[0][:, c, :], qd[0][i2][:, :], False, False, [Sb[0], qdb[0][i2]], [pob])
        _mm(P, po[0:64, jj * 128:(jj + 1) * 128], Sbf[1][:, c, :], qd[1][i2][:, :], False, True, [Sb[1], qdb[1][i2]], [pob])
        if jj == 3 or c == NCH - 1:
            b0 = (c // 4) * 512
            wid = (jj + 1) * 128
            lo = 0
            if (not need_ctx_out) and c // 4 == 0:
                lo = 256
            _cp(P, "act", oT[:, b0 + lo:b0 + wid], po[0:64, lo:wid], [pob], [oTb])
    if RET_STOP <= 3:
        return
    rT = A(P, [64, NTOK], BF16); rTb = P.buf("rT")
    o64b = A(P, [64, 64], BF16)
    P.op("dve", lambda h: h.memset(o64b[:], 1.0 / 64), writes=[rc])
    obf = [A(P, [64, 512], BF16) for _ in range(2)]; obfb = [P.buf(), P.buf()]
    cen = [A(P, [64, 512], F32) for _ in range(2)]; cenb = [P.buf(), P.buf()]
    sq_ = [A(P, [64, 512], BF16) for _ in range(2)]; sqb_ = [P.buf(), P.buf()]
    rs_ = [A(P, [64, 512], F32) for _ in range(2)]; rsb_ = [P.buf(), P.buf()]
    sg_ = [A(P, [64, 512], F32) for _ in range(2)]; sgb_ = [P.buf(), P.buf()]
    nblk = (NTOK + 511) // 512
    for bi in range(nblk):
        t0 = bi * 512
        n = min(512, NTOK - t0)
        i2 = bi % 2
        _cp(P, "act", obf[i2][:, 0:n], oT[:, t0:t0 + n], [oTb], [obfb[i2]])
        _mm(P, ps[2 + i2][0:64, 0:n], o64b[:, :], obf[i2][:, 0:n], True, True, [obfb[i2], rc], [psb[2 + i2]])
        _tt(P, "dve", cen[i2][:, 0:n], oT[:, t0:t0 + n], ps[2 + i2][0:64, 0:n], ALU.subtract, [oTb, psb[2 + i2]], [cenb[i2]])
        _act(P, sq_[i2][:, 0:n], cen[i2][:, 0:n], AF.Square, [cenb[i2]], [sqb_[i2]])
        _mm(P, ps[6 + i2][0:64, 0:n], o64b[:, :], sq_[i2][:, 0:n], True, True, [sqb_[i2], rc], [psb[6 + i2]])
        _act(P, rs_[i2][:, 0:n], ps[6 + i2][0:64, 0:n], AF.Ln, [psb[6 + i2]], [rsb_[i2]], bias=P.eps_t[0:64, 0:1], scale=1.0)
        _act(P, rs_[i2][:, 0:n], rs_[i2][:, 0:n], AF.Exp, [rsb_[i2]], [rsb_[i2]], scale=-0.5)
        _tt(P, "dve", cen[i2][:, 0:n], cen[i2][:, 0:n], rs_[i2][:, 0:n], ALU.mult, [cenb[i2], rsb_[i2]], [cenb[i2]])
        _act(P, sg_[i2][:, 0:n], gT[:, t0:t0 + n], AF.Silu, [gb_], [sgb_[i2]])
        _stt(P, rT[:, t0:t0 + n], cen[i2][:, 0:n], gn[:, 0:1], sg_[i2][:, 0:n], ALU.mult, ALU.mult, [cenb[i2], sgb_[i2], rc], [rTb])
    outs.append(_ld(P, "sp", out[2, :, :], rT[:, :], [P.buf()], reads=[rTb]))


def s5_part(P, dr, ps, psb, sT, sTb, out, outs, need_ctx_out, cb):
    pb = P.buf("s5param")

    def cplx_prep(are, aim, ldt, mk):
        T = {k: mk() for k in ("dt", "ar", "ai", "mag", "ph", "tmp", "sn", "cs", "abr", "abi", "nr", "den", "cr", "ci", "u")}
        ident_v = lambda t: t
        _act(P, T["dt"], ldt, AF.Exp, [pb], [pb])
        _tt(P, "dve", T["ar"], are, T["dt"], ALU.mult, [pb], [pb])
        _tt(P, "dve", T["ai"], aim, T["dt"], ALU.mult, [pb], [pb])
        _act(P, T["mag"], T["ar"], AF.Exp, [pb], [pb])
        _cp(P, "dve", T["ph"], T["ai"], [pb], [pb])
        range_reduce_sincos(P, T["ph"], T["sn"], T["cs"], T["tmp"], ident_v, pb)
        _tt(P, "dve", T["abr"], T["mag"], T["cs"], ALU.mult, [pb], [pb])
        _tt(P, "dve", T["abi"], T["mag"], T["sn"], ALU.mult, [pb], [pb])
        _ts(P, "dve", T["nr"], T["abr"], -1.0, None, ALU.add, None, [pb], [pb])
        _tt(P, "dve", T["den"], are, are, ALU.mult, [pb], [pb])
        _tt(P, "dve", T["u"], aim, aim, ALU.mult, [pb], [pb])
        _tt(P, "dve", T["den"], T["den"], T["u"], ALU.add, [pb], [pb])
        P.op("dve", lambda h: h.reciprocal(out=T["den"], in_=T["den"]), reads=[pb], writes=[pb])
        _tt(P, "dve", T["cr"], T["nr"], are, ALU.mult, [pb], [pb])
        _tt(P, "dve", T["u"], T["abi"], aim, ALU.mult, [pb], [pb])
        _tt(P, "dve", T["cr"], T["cr"], T["u"], ALU.add, [pb], [pb])
        _tt(P, "dve", T["cr"], T["cr"], T["den"], ALU.mult, [pb], [pb])
        _tt(P, "dve", T["ci"], T["abi"], are, ALU.mult, [pb], [pb])
        _tt(P, "dve", T["u"], T["nr"], aim, ALU.mult, [pb], [pb])
        _tt(P, "dve", T["ci"], T["ci"], T["u"], ALU.subtract, [pb], [pb])
        _tt(P, "dve", T["ci"], T["ci"], T["den"], ALU.mult, [pb], [pb])
        return T

    p_sm = A(P, [128, 2, 2, 3], F32); p_row = A(P, [128, 2, 3, 256], F32); p_hs = A(P, [64, 2, 3, 64], F32)
    Bhs = A(P, [64, 2, 2, 64], F32); Csm = A(P, [128, 2, 2, 2, 16], F32); dvec = A(P, [64, 1], F32)
    jrow = A(P, [128, 129], F32); jcol = A(P, [128, 1], F32); njcol = A(P, [128, 1], F32)
    LT = A(P, [128, 2, 128], BF16); mrow = A(P, [64, 4], F32); msm = A(P, [128, 2, 4], F32)
    for t_, src in ((p_sm[:], dr["s_sm"][:, :, :, :]), (p_row[:], dr["s_row"][:, :, :, :]), (p_hs[:], dr["s_hs"][:, :, :, :]), (Bhs[:], dr["s_B"][:, :, :, :]),
                    (Csm[:], dr["s_C"][:, :, :, :, :]), (dvec[:], dr["s_d"][:, :]), (jrow[:], dr["s_jrow"][:, :]), (jcol[:], dr["s_jcol"][:, :]),
                    (LT[:], dr["s_LT"].rearrange("d j i -> j d i")), (mrow[:], dr["s_mrow"][:, :]), (msm[:], dr["s_msm"][:, :, :])):
        _ld(P, "sp", t_, src, [pb])
    _ts(P, "dve", njcol[:], jcol[:], -1.0, None, ALU.mult, None, [pb], [pb])
    ones_col = A(P, [128, 1], BF16)
    P.op("dve", lambda h: h.memset(ones_col[:], 1.0), writes=[pb])

    BD = [A(P, [64, 512], BF16) for _ in range(2)]
    CT = [A(P, [128, 4, 64], BF16) for _ in range(2)]
    mark_prep = P.a_cur
    for dd in range(2):
        P.a_cur = mark_prep
        Ths = cplx_prep(p_hs[:, dd, 0, :], p_hs[:, dd, 1, :], p_hs[:, dd, 2, :], lambda: A(P, [64, 64], F32)[:, :])
        bbr = A(P, [64, 64], F32); bbi = A(P, [64, 64], F32); uu = A(P, [64, 64], F32)
        _tt(P, "dve", bbr[:], Ths["cr"], Bhs[:, dd, 0, :], ALU.mult, [pb], [pb])
        _tt(P, "dve", uu[:], Ths["ci"], Bhs[:, dd, 1, :], ALU.mult, [pb], [pb])
        _tt(P, "dve", bbr[:], bbr[:], uu[:], ALU.subtract, [pb], [pb])
        _tt(P, "dve", bbi[:], Ths["cr"], Bhs[:, dd, 1, :], ALU.mult, [pb], [pb])
        _tt(P, "dve", uu[:], Ths["ci"], Bhs[:, dd, 0, :], ALU.mult, [pb], [pb])
        _tt(P, "dve", bbi[:], bbi[:], uu[:], ALU.add, [pb], [pb])
        for g in range(4):
            _ts(P, "dve", BD[dd][:, g * 64:(g + 1) * 64], bbr[:], mrow[:, g:g + 1], None, ALU.mult, None, [pb], [pb])
            _ts(P, "dve", BD[dd][:, 256 + g * 64:256 + (g + 1) * 64], bbi[:], mrow[:, g:g + 1], None, ALU.mult, None, [pb], [pb])
        for ri in range(2):
            for st in range(2):
                for g in range(4):
                    _ts(P, "dve", CT[dd][:, ri * 2 + st, g * 16:(g + 1) * 16], Csm[:, dd, st, ri, :], msm[:, st, g:g + 1], (1.0 if ri == 0 else -1.0), ALU.mult, ALU.mult, [pb], [pb])
    P.a_cur = mark_prep
    TA = [[A(P, [128, 2, 129], F32) for _ in range(2)] for _ in range(2)]
    TW = [[A(P, [128, 2, 129], F32) for _ in range(2)] for _ in range(2)]
    PRE = [[A(P, [128, 256], F32) for _ in range(2)] for _ in range(2)]
    mark_t = P.a_cur
    for dd in range(2):
        P.a_cur = mark_t
        dt_ = A(P, [128, 2], F32); ar = A(P, [128, 2], F32); ai = A(P, [128, 2], F32); nar = A(P, [128, 2], F32)
        _act(P, dt_[:], p_sm[:, dd, :, 2], AF.Exp, [pb], [pb])
        _tt(P, "dve", ar[:], p_sm[:, dd, :, 0], dt_[:], ALU.mult, [pb], [pb])
        _tt(P, "dve", ai[:], p_sm[:, dd, :, 1], dt_[:], ALU.mult, [pb], [pb])
        _ts(P, "dve", nar[:], ar[:], -1.0, None, ALU.mult, None, [pb], [pb])
        mark_st = P.a_cur
        for st in range(2):
            P.a_cur = mark_st
            ph = A(P, [128, 129], F32); tmp = A(P, [128, 129], F32); sn = A(P, [128, 129], F32); cs = A(P, [128, 129], F32)
            mp = A(P, [128, 129], F32); mn = A(P, [128, 129], F32)
            _ts(P, "dve", ph[:], jrow[:], ai[:, st:st + 1], None, ALU.mult, None, [pb], [pb])
            range_reduce_sincos(P, ph[:], sn[:], cs[:], tmp[:], (lambda t: t), pb)
            _act(P, mp[:], jrow[:], AF.Exp, [pb], [pb], scale=ar[:, st:st + 1])
            _act(P, mn[:], jrow[:], AF.Exp, [pb], [pb], scale=nar[:, st:st + 1])
            _tt(P, "dve", TA[dd][0][:, st, :], mp[:], cs[:], ALU.mult, [pb], [pb])
            _tt(P, "dve", TA[dd][1][:, st, :], mp[:], sn[:], ALU.mult, [pb], [pb])
            _tt(P, "dve", TW[dd][0][:, st, :], mn[:], cs[:], ALU.mult, [pb], [pb])
            _stt(P, TW[dd][1][:, st, :], mn[:], -1.0, sn[:], ALU.mult, ALU.mult, [pb], [pb])
        P.a_cur = mark_t
        dtr = A(P, [128, 256], F32); arr = A(P, [128, 256], F32); air = A(P, [128, 256], F32)
        ph = A(P, [128, 256], F32); tmp = A(P, [128, 256], F32); sn = A(P, [128, 256], F32); cs = A(P, [128, 256], F32); mg = A(P, [128, 256], F32)
        _act(P, dtr[:], p_row[:, dd, 2, :], AF.Exp, [pb], [pb])
        _tt(P, "dve", arr[:], p_row[:, dd, 0, :], dtr[:], ALU.mult, [pb], [pb])
        _tt(P, "dve", air[:], p_row[:, dd, 1, :], dtr[:], ALU.mult, [pb], [pb])
        _ts(P, "dve", ph[:], air[:], jcol[:, 0:1], None, ALU.mult, None, [pb], [pb])
        range_reduce_sincos(P, ph[:], sn[:], cs[:], tmp[:], (lambda t: t), pb)
        _act(P, mg[:], arr[:], AF.Exp, [pb], [pb], scale=(njcol if dd == 0 else jcol)[:, 0:1])
        _tt(P, "dve", PRE[dd][0][:], mg[:], cs[:], ALU.mult, [pb], [pb])
        _stt(P, PRE[dd][1][:], mg[:], (-1.0 if dd == 0 else 1.0), sn[:], ALU.mult, ALU.mult, [pb], [pb])
        P.a_cur = mark_t
    barrier(P)
    P.a_cur = mark_t
    Xt = A(P, [128, NCH, 512], BF16); Xtb = [P.buf() for _ in range(NCH)]
    yacc = A(P, [64, NTOK], F32); yb = P.buf("yacc")
    E = A(P, [128, 4, NCH], F32); Eb = P.buf("E")
    H = [[A(P, [128, 2, NCH], F32) for _ in range(2)] for _ in range(2)]
    Hb = P.buf("H")
    cv = [A(P, [128, 2, NCH], F32) for _ in range(2)]
    pw = A(P, [128, 2, 8], F32)
    tq = [A(P, [128, 256], F32) for _ in range(8)]; tqb = [P.buf() for _ in range(8)]
    hs_ = [A(P, [128, 4, 128], BF16) for _ in range(2)]; hsb = [P.buf(), P.buf()]
    uq = [A(P, [128, 128], F32) for _ in range(16)]; uqb = [P.buf() for _ in range(16)]
    for dd in range(2):
        pos = pos_of(dd)
        for c in range(NCH):
            tau = 128 * c
            px = ps[c % 2]; pxb = psb[c % 2]
            _mm(P, px[:, 0:512], sT[:, tau:tau + 128], BD[dd][:, :], True, True, [sTb, pb], [pxb])
            i2 = (c % 2) * 4
            _tt(P, "dve", tq[i2][:, :], px[:, 0:256], PRE[dd][0][:, :], ALU.mult, [pxb, pb], [tqb[i2]])
            _tt(P, "dve", tq[i2 + 1][:, :], px[:, 256:512], PRE[dd][1][:, :], ALU.mult, [pxb, pb], [tqb[i2 + 1]])
            _tt(P, "dve", tq[i2 + 2][:, :], px[:, 0:256], PRE[dd][1][:, :], ALU.mult, [pxb, pb], [tqb[i2 + 2]])
            _tt(P, "dve", tq[i2 + 3][:, :], px[:, 256:512], PRE[dd][0][:, :], ALU.mult, [pxb, pb], [tqb[i2 + 3]])
            _tt(P, "pool", Xt[:, c, 0:256], tq[i2][:, :], tq[i2 + 1][:, :], ALU.subtract, [tqb[i2], tqb[i2 + 1]], [Xtb[c]])
            _tt(P, "pool", Xt[:, c, 256:512], tq[i2 + 2][:, :], tq[i2 + 3][:, :], ALU.add, [tqb[i2 + 2], tqb[i2 + 3]], [Xtb[c]])
            for tl in range(4):
                col = tl * NCH + pos[c]
                _mm(P, ps[6][:, col:col + 1], Xt[:, c, tl * 128:(tl + 1) * 128], ones_col[:, :], True, True, [Xtb[c], pb], [psb[6]])
        _cp(P, "dve", E[:], ps[6][:, 0:4 * NCH].rearrange("p (a b) -> p a b", b=NCH), [psb[6]], [Eb])
        H0r, H0i = H[0][0], H[0][1]
        if dd == 0:
            for st in range(2):
                a_r, a_i = TA[0][0][:, st, 127:128], TA[0][1][:, st, 127:128]
                _ts(P, "dve", uq[0][:, 0:NCH], E[:, 2 + st, :], a_i, None, ALU.mult, None, [Eb, pb], [uqb[0]])
                _stt(P, H0r[:, st, :], E[:, st, :], a_r, uq[0][:, 0:NCH], ALU.mult, ALU.subtract, [Eb, pb, uqb[0]], [Hb])
                _ts(P, "dve", uq[1][:, 0:NCH], E[:, st, :], a_i, None, ALU.mult, None, [Eb, pb], [uqb[1]])
                _stt(P, H0i[:, st, :], E[:, 2 + st, :], a_r, uq[1][:, 0:NCH], ALU.mult, ALU.add, [Eb, pb, uqb[1]], [Hb])
        else:
            _cp(P, "dve", H0r[:], E[:, 0:2, :], [Eb], [Hb])
            _cp(P, "dve", H0i[:], E[:, 2:4, :], [Eb], [Hb])
        _cp(P, "dve", pw[:, :, 0], TA[dd][0][:, :, 128], [pb], [Hb])
        _cp(P, "dve", pw[:, :, 1], TA[dd][1][:, :, 128], [pb], [Hb])
        cur = 0
        d = 1
        while d < NCH:
            _ts(P, "dve", pw[:, :, 2], pw[:, :, 1], -1.0, None, ALU.mult, None, [Hb], [Hb])
            o_, n_ = H[cur], H[1 - cur]
            for ri in range(2):
                _cp(P, "dve", n_[ri][:, :, 0:d], o_[ri][:, :, 0:d], [Hb], [Hb])
            for st in range(2):
                pr, pi, npi = pw[:, st, 0:1], pw[:, st, 1:2], pw[:, st, 2:3]
                m = NCH - d
                _stt(P, uq[0][:, 0:m], o_[0][:, st, 0:m], pr, o_[0][:, st, d:NCH], ALU.mult, ALU.add, [Hb], [uqb[0]])
                _stt(P, n_[0][:, st, d:NCH], o_[1][:, st, 0:m], npi, uq[0][:, 0:m], ALU.mult, ALU.add, [Hb, uqb[0]], [Hb])
                _stt(P, uq[1][:, 0:m], o_[1][:, st, 0:m], pr, o_[1][:, st, d:NCH], ALU.mult, ALU.add, [Hb], [uqb[1]])
                _stt(P, n_[1][:, st, d:NCH], o_[0][:, st, 0:m], pi, uq[1][:, 0:m], ALU.mult, ALU.add, [Hb, uqb[1]], [Hb])
            _tt(P, "dve", pw[:, :, 3], pw[:, :, 0], pw[:, :, 0], ALU.mult, [Hb], [Hb])
            _tt(P, "dve", pw[:, :, 4], pw[:, :, 1], pw[:, :, 1], ALU.mult, [Hb], [Hb])
            _tt(P, "dve", pw[:, :, 5], pw[:, :, 0], pw[:, :, 1], ALU.mult, [Hb], [Hb])
            _tt(P, "dve", pw[:, :, 0], pw[:, :, 3], pw[:, :, 4], ALU.subtract, [Hb], [Hb])
            _ts(P, "dve", pw[:, :, 1], pw[:, :, 5], 2.0, None, ALU.mult, None, [Hb], [Hb])
            cur = 1 - cur
            d *= 2
        Hf = H[cur]
        kidx = 1 if dd == 0 else 128
        P.op("dve", lambda h: h.memset(cv[0][:, :, 0:1], 0.0), writes=[Hb])
        P.op("dve", lambda h: h.memset(cv[1][:, :, 0:1], 0.0), writes=[Hb])
        for st in range(2):
            a_r, a_i = TA[dd][0][:, st, kidx:kidx + 1], TA[dd][1][:, st, kidx:kidx + 1]
            m = NCH - 1
            _ts(P, "dve", uq[0][:, 0:m], Hf[1][:, st, 0:m], a_i, None, ALU.mult, None, [Hb, pb], [uqb[0]])
            _stt(P, cv[0][:, st, 1:NCH], Hf[0][:, st, 0:m], a_r, uq[0][:, 0:m], ALU.mult, ALU.subtract, [Hb, pb, uqb[0]], [Hb])
            _ts(P, "dve", uq[1][:, 0:m], Hf[0][:, st, 0:m], a_i, None, ALU.mult, None, [Hb, pb], [uqb[1]])
            _stt(P, cv[1][:, st, 1:NCH], Hf[1][:, st, 0:m], a_r, uq[1][:, 0:m], ALU.mult, ALU.add, [Hb, pb, uqb[1]], [Hb])
        Tt = TA[dd] if dd == 0 else TW[dd]
        c_start = 0 if need_ctx_out else 2
        for c in range(c_start, NCH):
            pg = ps[2 + c % 2]; pgb = psb[2 + c % 2]
            for tl in range(4):
                _mm(P, pg[:, tl * 128:(tl + 1) * 128], Xt[:, c, tl * 128:(tl + 1) * 128], LT[:, dd, :], True, True, [Xtb[c], pb], [pgb])
            hh, hhb = hs_[c % 2], hsb[c % 2]
            pc = pos[c]
            for st in range(2):
                gr, gi = pg[:, st * 128:(st + 1) * 128], pg[:, (2 + st) * 128:(3 + st) * 128]
                c_r, c_i = cv[0][:, st, pc:pc + 1], cv[1][:, st, pc:pc + 1]
                Tr, Ti = Tt[0][:, st, 0:128], Tt[1][:, st, 0:128]
                u0 = ((c % 2) * 2 + st) * 4
                _stt(P, uq[u0][:, :], gr, c_r, Tr, ALU.add, ALU.mult, [pgb, Hb, pb], [uqb[u0]])
                _stt(P, uq[u0 + 1][:, :], gi, c_i, Ti, ALU.add, ALU.mult, [pgb, Hb, pb], [uqb[u0 + 1]])
                _stt(P, uq[u0 + 2][:, :], gi, c_i, Tr, ALU.add, ALU.mult, [pgb, Hb, pb], [uqb[u0 + 2]])
                _stt(P, uq[u0 + 3][:, :], gr, c_r, Ti, ALU.add, ALU.mult, [pgb, Hb, pb], [uqb[u0 + 3]])
                _tt(P, "pool", hh[:, st, :], uq[u0][:, :], uq[u0 + 1][:, :], ALU.subtract, [uqb[u0], uqb[u0 + 1]], [hhb])
                _tt(P, "pool", hh[:, 2 + st, :], uq[u0 + 2][:, :], uq[u0 + 3][:, :], ALU.add, [uqb[u0 + 2], uqb[u0 + 3]], [hhb])
            jj = c % 4
            py = ps[4 + (c // 4) % 2]; pyb = psb[4 + (c // 4) % 2]
            for tl in range(4):
                _mm(P, py[0:64, jj * 128:(jj + 1) * 128], CT[dd][:, tl, :], hh[:, tl, :], tl == 0, tl == 3, [pb, hhb], [pyb])
            if jj == 3 or c == NCH - 1:
                b0 = (c // 4) * 512
                wid = (jj + 1) * 128
                lo = 256 if ((not need_ctx_out) and c // 4 == 0) else 0
                if dd == 0:
                    _cp(P, "act", yacc[:, b0 + lo:b0 + wid], py[0:64, lo:wid], [pyb], [yb])
                else:
                    _tt(P, "dve", yacc[:, b0 + lo:b0 + wid], yacc[:, b0 + lo:b0 + wid], py[0:64, lo:wid], ALU.add, [pyb, yb], [yb])
    zT = A(P, [64, NTOK], BF16); zb = P.buf("zT")
    g1 = [A(P, [64, 512], F32) for _ in range(2)]; g1b = [P.buf(), P.buf()]
    g2 = [A(P, [64, 512], F32) for _ in range(2)]; g2b = [P.buf(), P.buf()]
    lo_all = 0 if need_ctx_out else 256
    if not need_ctx_out:
        P.op("pool", lambda h: h.memset(zT[:, 0:256], 0.0), writes=[zb])
    nblk = (NTOK + 511) // 512
    for bi in range(nblk):
        t0 = max(bi * 512, lo_all)
        t1_ = min((bi + 1) * 512, NTOK)
        n = t1_ - t0
        i2 = bi % 2
        y = g1[i2]; w = g2[i2]
        _stt(P, y[:, 0:n], sT[:, t0:t1_], dvec[:, 0:1], yacc[:, t0:t1_], ALU.mult, ALU.add, [sTb, pb, yb], [g1b[i2]])
        _tt(P, "dve", w[:, 0:n], y[:, 0:n], y[:, 0:n], ALU.mult, [g1b[i2]], [g2b[i2]])
        _ts(P, "dve", w[:, 0:n], w[:, 0:n], 0.044715, 1.0, ALU.mult, ALU.add, [g2b[i2]], [g2b[i2]])
        _tt(P, "dve", w[:, 0:n], w[:, 0:n], y[:, 0:n], ALU.mult, [g2b[i2], g1b[i2]], [g2b[i2]])
        _act(P, w[:, 0:n], w[:, 0:n], AF.Sigmoid, [g2b[i2]], [g2b[i2]], scale=1.5957691216057308)
        _tt(P, "dve", zT[:, t0:t1_], w[:, 0:n], y[:, 0:n], ALU.mult, [g2b[i2], g1b[i2]], [zb])
    outs.append(_ld(P, "sp", out[1, :, :], zT[:, :], [P.buf()], reads=[zb]))


import ml_dtypes

BF = ml_dtypes.bfloat16
NTOK = 8448
f32 = np.float32


def fm(a):
    return np.ascontiguousarray(a.T.reshape(8, 128, a.shape[0]))


def prep_common(inp, layer, b):
    cond = np.stack([inp['c'][b], inp['c_ctx']], 0)
    condT = np.ascontiguousarray(cond.reshape(2, 8, 128).transpose(2, 1, 0))
    bm = inp['b_mod'][layer].reshape(48, 128).T
    b_modT = np.ascontiguousarray(np.stack([bm, bm], -1))
    ng = inp['norm_g'][layer].reshape(4, 8, 128).transpose(2, 0, 1)
    norm_gT = np.ascontiguousarray(np.stack([ng, ng], -1))
    return dict(condT=condT, b_modT=b_modT, norm_gT=norm_gT, w_mod=inp['w_mod'][layer])


_const_cache = {}


def consts():
    if _const_cache:
        return _const_cache
    C = _const_cache
    c = np.arange(64)
    ang = 2 * np.pi * (np.outer(c, c) % 64) / 64
    C['f_CS'] = np.concatenate([np.cos(ang), -np.sin(ang)], 1).astype(BF)
    ca, sa = np.cos(ang), np.sin(ang)
    C['f_RP'] = np.concatenate([ca, -sa], 1).astype(BF)
    C['f_RQ'] = np.concatenate([sa, ca], 1).astype(BF)
    m2 = np.arange(128)[:, None, None]
    n1 = np.arange(64)[None, :, None]
    n2 = np.arange(128)[None, None, :]
    be = 2 * np.pi * ((m2 * (n1 + 64 * n2)) % 8192) / 8192
    nrm = 1 / np.sqrt(64 * 8192)
    C['f_CB'] = (np.cos(be) * nrm).astype(BF)
    C['f_SB'] = (np.sin(be) * nrm).astype(BF)
    m = np.arange(256)
    a256 = 2 * np.pi * (np.outer(m, m) % 256) / 256
    nrm2 = 1 / np.sqrt(64 * 256)
    C['f_C256'] = np.ascontiguousarray((np.cos(a256) * nrm2).reshape(2, 128, 256).transpose(1, 0, 2)).astype(BF)
    C['f_S256'] = np.ascontiguousarray((np.sin(a256) * nrm2).reshape(2, 128, 256).transpose(1, 0, 2)).astype(BF)
    t = np.arange(8192)
    row = (t // 64).astype(f32)
    col = (t % 64).astype(f32)
    inv = (1.0 / (f32(10000.0) ** (np.arange(16, dtype=f32) / f32(16)))).astype(f32)
    angr = np.concatenate([row[:, None] * inv, col[:, None] * inv], -1).astype(f32)
    cs, sn = np.cos(angr).astype(f32), np.sin(angr).astype(f32)
    cos64 = np.concatenate([cs, cs], 1)
    sin64 = np.concatenate([-sn, sn], 1)
    C['r_cosF'] = np.ascontiguousarray(cos64.T)
    C['r_sinF'] = np.ascontiguousarray(sin64.T)
    C['r_cosT'] = np.ascontiguousarray(cos64.reshape(64, 128, 64).transpose(1, 0, 2))
    C['r_sinT'] = np.ascontiguousarray(sin64.reshape(64, 128, 64).transpose(1, 0, 2))
    j = np.arange(128, dtype=f32)
    C['r_jcol'] = np.stack([127 - j, j], 1).astype(f32)
    ii = np.arange(128)
    dist = np.abs(ii[None, :] - ii[:, None]).astype(f32)
    C['r_dist'] = dist
    C['r_mask'] = np.stack([(ii[None, :] >= ii[:, None]), (ii[:, None] >= ii[None, :])], 0).astype(f32)
    C['r_irow'] = np.stack([np.tile(j + 1, (64, 1)), np.tile(128 - j, (64, 1))], 0).astype(f32)
    C['s_jrow'] = np.tile(np.arange(129, dtype=f32), (128, 1))
    C['s_jcol'] = np.arange(128, dtype=f32)[:, None].copy()
    C['s_LT'] = np.stack([(ii[None, :] >= ii[:, None]), (ii[:, None] >= ii[None, :])], 0).astype(BF)
    g_of_row = np.arange(64) // 16
    C['s_mrow'] = (g_of_row[:, None] == np.arange(4)[None, :]).astype(f32)
    g_of_st = (np.arange(128)[:, None] // 64) + 2 * np.arange(2)[None, :]
    C['s_msm'] = (g_of_st[:, :, None] == np.arange(4)[None, None, :]).astype(f32)
    C['ident_bf'] = np.eye(128).astype(BF)
    C['ident_f'] = np.eye(128).astype(f32)
    def start(r):
        return int(np.clip(r - 4, 0, 120))
    qc = np.arange(64)
    cst = np.clip(qc - 8, 0, 48)
    kc = np.arange(64)
    colok = (kc[None, :] >= cst[:, None]) & (kc[None, :] < cst[:, None] + 16)
    types = [(0, 0, 8), (2, 0, 8), (10, 6, 9), (124, 120, 8), (126, 120, 8)]
    mask = np.full((5, 128, 832), -30000.0, f32)
    drs = np.zeros((5, 2, 9), np.int64)
    for ti, (r0, R0, nr) in enumerate(types):
        for qr in range(2):
            r = r0 + qr
            for i in range(9):
                kr = R0 + i
                dr = int(np.clip(kr - r + 7, 0, 14))
                drs[ti, qr, i] = dr
                if i < nr and start(r) <= kr < start(r) + 8:
                    blk = np.where(colok, 0.0, -30000.0)
                    mask[ti, qr * 64:(qr + 1) * 64, i * 64:(i + 1) * 64] = blk
        mask[ti, :, 576:] = 0.0
    C['n_mask'] = mask
    C['n_drs'] = drs
    C['n_types'] = types
    return C


def prep_M(inp, layer, c, xT_full):
    b, q = c // 4, c % 4
    C = consts()
    m = prep_common(inp, layer, b)
    m['xT'] = xT_full
    w_in = inp['w_in'][layer]
    o = q * 64
    sw = np.r_[32:64, 0:32]
    cols_fm = np.concatenate([np.arange(0 + o, 0 + o + 64), np.arange(256 + o, 256 + o + 64), np.arange(512 + o, 512 + o + 64),
                              np.arange(768 + o, 768 + o + 64), 512 + o + sw, 768 + o + sw, np.arange(1280 + o, 1280 + o + 64),
                              np.arange(1536 + o, 1536 + o + 64), np.arange(1792 + o, 1792 + o + 64)])
    cols_tm = np.concatenate([np.arange(768 + o, 768 + o + 64), 768 + o + sw, np.arange(1024 + o, 1024 + o + 64), np.arange(2048 + o, 2048 + o + 64)])
    m['w_fm'] = np.ascontiguousarray(w_in[:, cols_fm])
    m['w_tm'] = np.ascontiguousarray(w_in[:, cols_tm])
    for k in ('f_CS', 'f_RP', 'f_RQ', 'f_CB', 'f_SB', 'f_C256', 'f_S256', 'r_cosF', 'r_sinF', 'r_cosT', 'r_sinT', 'r_jcol', 'r_dist', 'r_mask', 'r_irow',
              's_jrow', 's_jcol', 's_LT', 's_mrow', 's_msm', 'ident_bf', 'ident_f', 'n_mask'):
        m[k] = C[k]
    gs = slice(4 * q, 4 * q + 4)
    L = layer
    are, aim = inp['s5_a_re'][L][:, gs], inp['s5_a_im'][L][:, gs]
    ldt = inp['s5_log_dt'][L][:, gs]
    def sm(a):
        return np.ascontiguousarray(a.reshape(2, 2, 128).transpose(2, 0, 1))
    ldt_b = np.broadcast_to(ldt[:, :, None], (2, 4, 64))
    m['s_sm'] = np.ascontiguousarray(np.stack([sm(are), sm(aim), sm(ldt_b)], -1))
    row = np.stack([are.reshape(2, 256), aim.reshape(2, 256), ldt_b.reshape(2, 256)], -1)
    m['s_row'] = np.ascontiguousarray(np.broadcast_to(row[None].transpose(0, 1, 3, 2), (128, 2, 3, 256)))
    hs = np.stack([are, aim, ldt_b], 2)
    hs = np.broadcast_to(hs[:, :, None], (2, 4, 16, 3, 64))
    m['s_hs'] = np.ascontiguousarray(hs.transpose(1, 2, 0, 3, 4).reshape(64, 2, 3, 64))
    bre, bim = inp['s5_b_re'][L][:, gs], inp['s5_b_im'][L][:, gs]
    B = np.stack([bre, bim], 2)
    m['s_B'] = np.ascontiguousarray(B.transpose(1, 4, 0, 2, 3).reshape(64, 2, 2, 64))
    cre, cim = inp['s5_c_re'][L][:, gs], inp['s5_c_im'][L][:, gs]
    Cc = np.stack([cre, cim], 2)
    Cc = Cc.transpose(1, 4, 0, 2, 3)
    Cc = Cc.reshape(2, 2, 64, 2, 2, 16).transpose(1, 2, 3, 0, 4, 5).reshape(128, 2, 2, 2, 16)
    m['s_C'] = np.ascontiguousarray(Cc)
    m['s_d'] = np.ascontiguousarray(inp['s5_d'][L][256 * 0 + 64 * q:64 * q + 64][:, None])
    rd = inp['ret_decay'][L][:, q]
    m['r_dec'] = np.ascontiguousarray(np.broadcast_to(rd[None, :], (128, 2))).astype(f32)
    m['r_gn'] = np.ascontiguousarray(inp['ret_gn'][L][64 * q:64 * q + 64][:, None])
    rpb = inp['na_rpb'][L][q]
    dc = np.clip(np.arange(64)[None, :] - np.arange(64)[:, None], -15, 15) + 15
    m['n_toep'] = np.ascontiguousarray(rpb[:, dc])
    return m


def _prep_F(inp, layer, c, xa, bra_bf, moe_a=False):
    b = c // 4
    m = prep_common(inp, layer, b)
    m.update(xT=fm(xa), brT=bra_bf, w_in=inp['w_in'][layer], w_br=inp['w_branch'][layer].reshape(1024, 1024), w_o=inp['w_out'][layer],
             w_glu=inp['s5_w_glu'][layer], b_gluT=np.ascontiguousarray(inp['s5_b_glu'][layer].reshape(2, 128).T))
    i = layer // 2
    if layer % 2 == 0:
        m.update(w_g=inp['ffn_w_gate'][i:i + 1], w_u=inp['ffn_w_up'][i:i + 1], w_d=inp['ffn_w_down'][i:i + 1])
    else:
        sel = np.zeros((8, 8, 128), np.float32)
        for e in range(8):
            sel[e, e, :] = 1
        m.update(w_r=inp['moe_w_router'][i], b_r=np.ascontiguousarray(np.broadcast_to(inp['moe_b_router'][i][None], (128, 8))),
                 ident=np.eye(128, dtype=np.float32), sel=sel)
    return m


def kernel(**inputs):
    inp = {k: np.asarray(v) for k, v in inputs.items()}
    NCORE = 8
    cores = list(range(NCORE))
    x = inp['x']
    ctx = inp['ctx']
    for layer in range(2):
        last = (layer == 1)
        ncM = build_M(not last)
        xfull = [fm(np.concatenate([ctx[b], x[b]], 0)) for b in range(2)]
        maps = [prep_M(inp, layer, c, xfull[c // 4]) for c in cores]
        resM = run_bass_kernel_spmd(ncM, maps, core_ids=cores).results
        del maps
        br_full = []
        for b in range(2):
            o = np.stack([np.asarray(resM[4 * b + q]['brT_out']) for q in range(4)], 1)
            br_full.append(o.reshape(1024, NTOK))
        del resM
        if not last:
            blocksA = [(i * 256, 256, 0) for i in range(8)] + [(2048, 64, 1)]
            blocksB = [(i * 512, 512, 0) for i in range(4)] + [(2048, 64, 1)]
            ncF = build_F(blocksA, blocksB, 1, 2816, False)
        else:
            blocksA = [(i * 256, 256, 0) for i in range(8)]
            blocksB = [(i * 512, 512, 0) for i in range(4)]
            ncF = build_F(blocksA, blocksB, 8, 3584, True, mode='moe_a')
        maps = []
        for c in cores:
            b, q = c // 4, c % 4
            lat = slice(256 + q * 2048, 256 + (q + 1) * 2048)
            if not last:
                xa = np.concatenate([x[b, q * 2048:(q + 1) * 2048], ctx[b, q * 64:(q + 1) * 64]], 0)
                bra = np.concatenate([br_full[b][:, lat], br_full[b][:, q * 64:(q + 1) * 64]], 1)
            else:
                xa = x[b, q * 2048:(q + 1) * 2048]
                bra = br_full[b][:, lat]
            bra = np.ascontiguousarray(bra.reshape(8, 128, bra.shape[1]))
            maps.append(_prep_F(inp, layer, c, xa, bra))
        resF = run_bass_kernel_spmd(ncF, maps, core_ids=cores).results
        del maps
        if not last:
            xn = np.empty_like(x)
            cn = np.empty_like(ctx)
            for c in cores:
                b, q = c // 4, c % 4
                o = np.asarray(resF[c]['xo']).reshape(1024, -1).T
                xn[b, q * 2048:(q + 1) * 2048] = o[:2048]
                cn[b, q * 64:(q + 1) * 64] = o[2048:]
            x, ctx = xn, cn
            continue
        i = layer // 2
        h2_all = np.concatenate([np.asarray(resF[c]['h2o']) for c in cores], 2)
        cb_all = np.concatenate([np.asarray(resF[c]['cbo']) for c in cores], 1)
        sel = np.concatenate([np.asarray(resF[c]['mko']) for c in cores], 1).astype(bool)
        idx = [np.flatnonzero(sel[e]) for e in cores]
        nb = max(1, -(-max(len(t) for t in idx) // 512))
        ng = -(-nb // 4)
        groups = tuple(nb // ng + (1 if g < nb % ng else 0) for g in range(ng))
        C = 512 * nb
        ncE = build_E(groups)
        maps = []
        for e in cores:
            n_e = len(idx[e])
            ii = np.zeros(C, np.int64)
            ii[:n_e] = idx[e]
            cbe = np.zeros(C, cb_all.dtype)
            cbe[:n_e] = cb_all[e, idx[e]]
            maps.append(dict(h2=np.ascontiguousarray(h2_all[:, :, ii]), cbe=np.ascontiguousarray(np.broadcast_to(cbe[None, :], (128, C))),
                             w_g=inp['moe_w_gate'][i][e], w_u=inp['moe_w_up'][i][e], w_d=inp['moe_w_down'][i][e]))
        resE = run_bass_kernel_spmd(ncE, maps, core_ids=cores).results
        del maps
        slot = np.cumsum(sel, axis=0) - sel
        nslot = max(1, int(sel.sum(0).max()))
        yp_all = np.zeros((nslot, 8, 128, sel.shape[1]), np.float32)
        for e in cores:
            ye = np.asarray(resE[e]['ye'])
            t = idx[e]
            sv = slot[e, t]
            for k in range(nslot):
                mk = sv == k
                yp_all[k][:, :, t[mk]] = ye[:, :, np.flatnonzero(mk)]
        ncC = build_Fc(nexp=nslot)
        maps = []
        for c in cores:
            m = prep_common(inp, layer, c // 4)
            m['xm'] = np.asarray(resF[c]['xo'])
            m['yp'] = np.ascontiguousarray(yp_all[:, :, :, c * 2048:(c + 1) * 2048])
            maps.append(m)
        resC = run_bass_kernel_spmd(ncC, maps, core_ids=cores).results
        xn = np.empty_like(x)
        for c in cores:
            b, q = c // 4, c % 4
            xn[b, q * 2048:(q + 1) * 2048] = np.asarray(resC[c]['xo']).reshape(1024, -1).T
        x = xn
    return x.astype(np.float32)
```

```python
import numpy as np
from contextlib import ExitStack
import concourse.bass as bass
import concourse.mybir as mybir
from concourse.bass_utils import run_bass_kernel_spmd

F32 = mybir.dt.float32
BF16 = mybir.dt.bfloat16
I32 = mybir.dt.int32
ALU = mybir.AluOpType
AF = mybir.ActivationFunctionType
AX = mybir.AxisListType

ENGS = ("pe", "act", "dve", "pool", "sp")
NDSEM = 12


class Buf:
    __slots__ = ("name", "lw", "rd", "psum")

    def __init__(self, name="", psum=False):
        self.name = name
        self.psum = psum
        self.lw = None
        self.rd = {}


class Prog:
    def __init__(self, nc):
        self.nc = nc
        self.stack = ExitStack()
        self.ops = {e: [] for e in ENGS}
        self.cnt = {e: 0 for e in ENGS}
        self.seen = {e: {} for e in ENGS}
        self.sems = {}
        for e in ENGS:
            self.sems[e] = self.stack.enter_context(nc.semaphore("s_" + e))
        self.dsem_use = {}
        self.dq_next = {}
        for q in ("sp", "act", "pool"):
            for i in range(NDSEM):
                k = "d_%s%d" % (q, i)
                self.sems[k] = self.stack.enter_context(nc.semaphore(k))
                self.dsem_use[k] = 0
            self.dq_next[q] = 0
        self.nbuf = 0

    def sb(self, name, shape, dt):
        return self.stack.enter_context(self.nc.sbuf_tensor(name, list(shape), dt))

    def ps(self, name, shape, dt=F32):
        return self.stack.enter_context(self.nc.psum_tensor(name, list(shape), dt))

    def buf(self, name=None, psum=None):
        self.nbuf += 1
        name = name or "b%d" % self.nbuf
        if psum is None:
            psum = name.startswith("ps")
        return Buf(name, psum)

    def _deps(self, eng, reads, writes, is_dma):
        w = {}

        def add(t):
            if t is None:
                return
            k, v = t
            if w.get(k, 0) < v:
                w[k] = v
        for b in reads:
            add(b.lw)
            if b.psum:
                for k, v in b.rd.items():
                    if k != eng:
                        add((k, v))
        for b in writes:
            if b.lw is not None:
                if not (eng == "pe" and b.lw[0] == "pe" and not is_dma):
                    add(b.lw)
            for k, v in b.rd.items():
                if k == eng and not is_dma and eng != "pool":
                    continue
                add((k, v))
        seen = self.seen[eng]
        out = []
        for k, v in w.items():
            if seen.get(k, 0) < v:
                seen[k] = v
                out.append((k, v))
        return out

    def _commit(self, ticket, reads, writes):
        for b in writes:
            b.lw = ticket
            b.rd = {}
        for b in reads:
            k, v = ticket
            if b.rd.get(k, 0) < v:
                b.rd[k] = v

    def op(self, eng, fn, reads=(), writes=()):
        waits = self._deps(eng, reads, writes, False)
        self.cnt[eng] += 1
        ticket = (eng, self.cnt[eng])
        self.ops[eng].append((waits, fn, (eng, 1)))
        self._commit(ticket, reads, writes)
        return ticket

    def dma(self, q, fn, reads=(), writes=()):
        i = self.dq_next[q]
        self.dq_next[q] = (i + 1) % NDSEM
        k = "d_%s%d" % (q, i)
        waits = self._deps(q, reads, writes, True)
        prev = self.dsem_use[k]
        if prev > 0 and self.seen[q].get(k, 0) < 16 * prev:
            self.seen[q][k] = 16 * prev
            waits.append((k, 16 * prev))
        self.dsem_use[k] = prev + 1
        ticket = (k, 16 * (prev + 1))
        self.ops[q].append((waits, fn, (k, 16)))
        self._commit(ticket, reads, writes)
        return ticket

    def finish_wait(self, eng, tickets):
        waits = []
        for k, v in tickets:
            if self.seen[eng].get(k, 0) < v:
                self.seen[eng][k] = v
                waits.append((k, v))
        self.ops[eng].append((waits, None, None))

    def emit(self):
        nc = self.nc
        sems = self.sems
        ops = self.ops

        def replay(e, h):
            for waits, fn, inc in ops[e]:
                for k, v in waits:
                    h.wait_ge(sems[k], v)
                if fn is not None:
                    ins = fn(h)
                    ins.then_inc(sems[inc[0]], inc[1])

        with nc.Block() as block:
            @block.sync
            def _(h):
                replay("sp", h)

            @block.scalar
            def _(h):
                replay("act", h)

            @block.vector
            def _(h):
                replay("dve", h)

            @block.gpsimd
            def _(h):
                replay("pool", h)

            @block.tensor
            def _(h):
                replay("pe", h)
        self.stack.close()


def _mm(P, out, lhsT, rhs, start, stop, reads, writes):
    return P.op("pe", lambda h: h.matmul(out, lhsT=lhsT, rhs=rhs, start=start, stop=stop), reads=reads, writes=writes)


def _tr(P, out, in_, ident, reads, writes):
    return P.op("pe", lambda h: h.transpose(out, in_, ident), reads=reads, writes=writes)


def _act(P, out, in_, func, reads, writes, scale=None, bias=None):
    kw = {}
    if scale is not None:
        kw["scale"] = scale
    if bias is not None:
        kw["bias"] = bias
    return P.op("act", lambda h: h.activation(out=out, in_=in_, func=func, **kw), reads=reads, writes=writes)


def _tt(P, eng, out, in0, in1, op, reads, writes):
    return P.op(eng, lambda h: h.tensor_tensor(out=out, in0=in0, in1=in1, op=op), reads=reads, writes=writes)


def _ts(P, eng, out, in0, s1, s2, op0, op1, reads, writes):
    if op1 is None:
        return P.op(eng, lambda h: h.tensor_scalar(out=out, in0=in0, scalar1=s1, scalar2=None, op0=op0), reads=reads, writes=writes)
    return P.op(eng, lambda h: h.tensor_scalar(out=out, in0=in0, scalar1=s1, scalar2=s2, op0=op0, op1=op1), reads=reads, writes=writes)


def _stt(P, out, in0, scalar, in1, op0, op1, reads, writes):
    return P.op("dve", lambda h: h.scalar_tensor_tensor(out=out, in0=in0, scalar=scalar, in1=in1, op0=op0, op1=op1), reads=reads, writes=writes)


def _cp(P, eng, out, in_, reads, writes):
    if eng == "act":
        return P.op("act", lambda h: h.activation(out=out, in_=in_, func=AF.Copy), reads=reads, writes=writes)
    return P.op(eng, lambda h: h.tensor_copy(out=out, in_=in_), reads=reads, writes=writes)


def _ld(P, q, out, in_, writes, reads=()):
    return P.dma(q, lambda h: h.dma_start(out=out, in_=in_), reads=reads, writes=writes)


D = 1024
KC = 8
EPS = 1e-6


def arena_init(P, nbytes=206 * 1024):
    lo, hi = P.nc.bump_sbuf(nbytes)
    P.a_lo, P.a_hi, P.a_cur = lo, hi, lo
    P.a_n = 0


def A(P, shape, dt):
    nb = int(np.prod(shape[1:])) * (4 if dt in (F32, I32) else 2)
    off = (P.a_cur + 31) // 32 * 32
    assert off + nb <= P.a_hi, ("SBUF arena overflow", off + nb - P.a_lo)
    P.a_cur = off + nb
    P.a_n += 1
    return P.nc.alloc_sbuf_tensor_at("t%d" % P.a_n, list(shape), dt, offset=off)


def barrier(P, queues=None):
    tick = [(e, P.cnt[e]) for e in ENGS if P.cnt[e] > 0]
    tick += [(k, 16 * v) for k, v in P.dsem_use.items() if v > 0 and (queues is None or any(k.startswith("d_" + q) for q in queues))]
    for e in ENGS:
        P.finish_wait(e, tick)


def rms_rstd(P, src, srcb, n, sq, sqb, ss_ps, ssb, rstd, rstdb, ones):
    P.op("act", lambda h: h.activation(out=sq[:, :, 0:n], in_=src[:, :, 0:n], func=AF.Square), reads=[srcb], writes=[sqb])
    for k in range(KC):
        P.op("pe", lambda h, k=k: h.matmul(ss_ps[:, 0:n], lhsT=ones[:], rhs=sq[:, k, 0:n], start=(k == 0), stop=(k == KC - 1)),
             reads=[sqb], writes=[ssb])
    P.op("act", lambda h: h.activation(out=rstd[:, 0:n], in_=ss_ps[:, 0:n], func=AF.Ln, scale=1.0 / D, bias=P.eps_t[:, 0:1]), reads=[ssb], writes=[rstdb])
    P.op("act", lambda h: h.activation(out=rstd[:, 0:n], in_=rstd[:, 0:n], func=AF.Exp, scale=-0.5), reads=[rstdb], writes=[rstdb])


def norm_mod(P, src, srcb, n, rstd, rstdb, gm, sh, r, dst, dstb, tmp, tmpb, dst_off=0, dst32=None, dst32b=None):
    for k in range(KC):
        tb = tmpb[k % len(tmp)]
        tt = tmp[k % len(tmp)]
        P.op("dve", lambda h, k=k, tt=tt: h.tensor_tensor(out=tt[:, 0:n], in0=src[:, k, 0:n], in1=rstd[:, 0:n], op=ALU.mult),
             reads=[srcb, rstdb], writes=[tb])
        P.op("act", lambda h, k=k, tt=tt: h.activation(out=dst[:, k, dst_off:dst_off + n], in_=tt[:, 0:n], func=AF.Identity,
                                                     scale=gm[:, k, r:r + 1], bias=sh[:, k, r:r + 1]),
             reads=[tb, P.modb], writes=[dstb])
        if dst32 is not None:
            P.op("pool", lambda h, k=k, tt=tt: h.tensor_scalar(out=dst32[:, k, 0:n], in0=tt[:, 0:n], scalar1=gm[:, k, r:r + 1],
                                                             scalar2=sh[:, k, r:r + 1], op0=ALU.mult, op1=ALU.add),
                 reads=[tb, P.modb], writes=[dst32b])


def compute_mod(P, dr, which, mod_ps, modpb, light=False):
    nc = P.nc
    cs = A(P, [128, KC, 2], F32)
    csb = P.buf()
    P.dma("sp", lambda h: h.dma_start(out=cs[:], in_=dr["condT"][:, :, :]), writes=[csb])
    sig = A(P, [128, KC, 2], F32)
    P.op("act", lambda h: h.activation(out=sig[:], in_=cs[:], func=AF.Sigmoid), reads=[csb], writes=[csb])
    P.op("dve", lambda h: h.tensor_tensor(out=cs[:], in0=cs[:], in1=sig[:], op=ALU.mult), reads=[csb], writes=[csb])
    modT = A(P, [128, 48, 2], F32)
    P.modT = modT
    P.modb = P.buf("mod")
    bm = A(P, [128, 48, 2], F32)
    bmb = P.buf()
    P.dma("sp", lambda h: h.dma_start(out=bm[:], in_=dr["b_modT"][:, :, :]), writes=[bmb])
    ng = A(P, [128, 4, KC, 2], F32)
    P.ng = ng
    P.dma("sp", lambda h: h.dma_start(out=ng[:], in_=dr["norm_gT"][:, :, :, :]), writes=[P.modb])
    mark = P.a_cur
    wm = [A(P, [128, KC, 1024], F32) for _ in range(2)]
    wmb = [P.buf(), P.buf()]
    wsrc = dr["w_mod"].rearrange("(k p) f -> p k f", p=128)
    for i, j in enumerate(which):
        w = wm[i % 2]
        wb = wmb[i % 2]
        for k2 in range(2):
            P.dma("sp", lambda h, w=w, j=j, k2=k2: h.dma_start(out=w[:, 4 * k2:4 * k2 + 4, :], in_=wsrc[:, 4 * k2:4 * k2 + 4, j * 1024:(j + 1) * 1024]), writes=[wb])
        for fc in range(8):
            for k in range(KC):
                P.op("pe", lambda h, w=w, j=j, fc=fc, k=k: h.matmul(mod_ps[:, j * 8 + fc, :], lhsT=w[:, k, fc * 128:(fc + 1) * 128], rhs=cs[:, k, :],
                                                                   start=(k == 0), stop=(k == KC - 1)), reads=[wb, csb], writes=[modpb])
    for j in which:
        P.op("dve", lambda h, j=j: h.tensor_tensor(out=modT[:, j * 8:(j + 1) * 8, :], in0=mod_ps[:, j * 8:(j + 1) * 8, :], in1=bm[:, j * 8:(j + 1) * 8, :], op=ALU.add),
             reads=[modpb, bmb], writes=[P.modb])
    barrier(P, ("sp",) if light else None)
    P.a_cur = mark


def mod_derived(P, jsc, jg, gi_norm, gi_gate):
    gm = A(P, [128, KC, 2], F32)
    gg = A(P, [128, KC, 2], F32)
    modT, ng = P.modT, P.ng
    P.op("dve", lambda h: h.scalar_tensor_tensor(out=gm[:], in0=modT[:, jsc * 8:(jsc + 1) * 8, :], scalar=1.0, in1=ng[:, gi_norm, :, :],
                                                 op0=ALU.add, op1=ALU.mult), reads=[P.modb], writes=[P.modb])
    if jg is not None:
        P.op("dve", lambda h: h.tensor_tensor(out=gg[:], in0=modT[:, jg * 8:(jg + 1) * 8, :], in1=ng[:, gi_gate, :, :], op=ALU.mult),
             reads=[P.modb], writes=[P.modb])
    return gm, gg


def build_F(blocks, blocksB, n_exp, dff, moe, DBG=False, mode='full'):
    TT = sum(b[1] for b in blocks)
    nc = bass.Bass("TRN2", target_bir_lowering=False)
    dr = {}

    def din(name, shape, dt=F32):
        dr[name] = nc.dram_tensor(name, list(shape), dt, kind="ExternalInput").ap()
    din("xT", [KC, 128, TT])
    din("brT", [KC, 128, TT], BF16)
    din("condT", [128, KC, 2])
    din("w_mod", [D, 6 * D])
    din("b_modT", [128, 48, 2])
    din("norm_gT", [128, 4, KC, 2])
    din("w_in", [D, 6400])
    din("w_br", [KC * 128, D])
    din("w_o", [D, D])
    din("w_glu", [256, 256])
    din("b_gluT", [128, 2])
    if mode == 'full':
        din("w_g", [n_exp, D, dff])
        din("w_u", [n_exp, D, dff])
        din("w_d", [n_exp, dff, D])
    if moe:
        din("w_r", [D, 8])
        din("b_r", [128, 8])
        din("ident", [128, 128])
        din("sel", [8, 8, 128])
    if mode == 'moe_a':
        h2o = nc.dram_tensor("h2o", [KC, 128, TT], BF16, kind="ExternalOutput").ap().rearrange("k p t -> p k t")
        cbo = nc.dram_tensor("cbo", [8, TT], BF16, kind="ExternalOutput").ap()
        mko = nc.dram_tensor("mko", [8, TT], BF16, kind="ExternalOutput").ap()
    xo = nc.dram_tensor("xo", [KC, 128, TT], F32, kind="ExternalOutput").ap()
    xoT = xo.rearrange("k p t -> p k t")
    if DBG: dbg_mod = nc.dram_tensor("dbg_mod", [128, 48, 2], F32, kind="ExternalOutput").ap()
    if DBG: dbg_xm = nc.dram_tensor("dbg_xm", [KC, 128, TT], F32, kind="ExternalOutput").ap().rearrange("k p t -> p k t")
    if DBG: dbg_h = nc.dram_tensor("dbg_h", [KC, 128, TT], BF16, kind="ExternalOutput").ap().rearrange("k p t -> p k t")
    if DBG: dbg_z = nc.dram_tensor("dbg_z", [KC, 128, TT], F32, kind="ExternalOutput").ap().rearrange("k p t -> p k t")
    if DBG: dbg_r = nc.dram_tensor("dbg_r", [128, TT], F32, kind="ExternalOutput").ap()
    if DBG: dbg_sq = nc.dram_tensor("dbg_sq", [KC, 128, TT], BF16, kind="ExternalOutput").ap().rearrange("k p t -> p k t")
    if DBG: dbg_ss = nc.dram_tensor("dbg_ss", [128, TT], F32, kind="ExternalOutput").ap()
    sscp = A(P, [128, 256], F32) if False else None
    if DBG: dbg_y = nc.dram_tensor("dbg_y", [KC, 128, TT], BF16, kind="ExternalOutput").ap().rearrange("k p t -> p k t")
    xT = dr["xT"].rearrange("k p t -> p k t")
    brT = dr["brT"].rearrange("k p t -> p k t")

    P = Prog(nc)
    arena_init(P)
    ps = [P.ps("ps%d" % i, [128, 512], F32) for i in range(8)]
    psb = [P.buf("ps%d" % i) for i in range(8)]
    ones = A(P, [128, 128], BF16)
    onesb = P.buf()
    P.op("dve", lambda h: h.memset(ones[:], 1.0), writes=[onesb])
    P.eps_t = A(P, [128, 1], F32)
    P.op("dve", lambda h: h.memset(P.eps_t[:], EPS), writes=[onesb])

    w_lo = P.a_cur
    wgt = A(P, [128, KC, 4096], BF16)
    wbr = A(P, [128, KC, D], BF16)
    wo = A(P, [128, KC, D], BF16)
    wAb = P.buf("wA")
    wbufs = []

    def _wb():
        wbufs.append(P.buf())
        return wbufs[-1]
    w_in_v = dr["w_in"].rearrange("(k p) c -> p k c", p=128)
    for k in range(KC):
        for c4 in range(2):
            P.dma("pool", lambda h, k=k, c4=c4: h.dma_start(out=wgt[:, k, c4 * 2048:(c4 + 1) * 2048], in_=w_in_v[:, k, 2304 + c4 * 2048:2304 + (c4 + 1) * 2048]), writes=[_wb()])
    P.dma("pool", lambda h: h.dma_start(out=wbr[:, 0:4, :], in_=dr["w_br"].rearrange("(k p) c -> p k c", p=128)[:, 0:4, :]), writes=[_wb()])
    P.dma("pool", lambda h: h.dma_start(out=wbr[:, 4:8, :], in_=dr["w_br"].rearrange("(k p) c -> p k c", p=128)[:, 4:8, :]), writes=[_wb()])
    P.dma("pool", lambda h: h.dma_start(out=wo[:, 0:4, :], in_=dr["w_o"].rearrange("(k p) c -> p k c", p=128)[:, 0:4, :]), writes=[_wb()])
    P.dma("pool", lambda h: h.dma_start(out=wo[:, 4:8, :], in_=dr["w_o"].rearrange("(k p) c -> p k c", p=128)[:, 4:8, :]), writes=[_wb()])

    wglu = A(P, [128, 2, 256], BF16)
    bglu = A(P, [128, 2], F32)
    P.dma("pool", lambda h: h.dma_start(out=wglu[:], in_=dr["w_glu"].rearrange("(k p) c -> p k c", p=128)), writes=[_wb()])
    P.dma("sp", lambda h: h.dma_start(out=bglu[:], in_=dr["b_gluT"][:, :]), writes=[_wb()])
    w_hi = P.a_cur
    mod_ps = nc.alloc_psum_tensor
    mod_view = ps[7][:, 0:96].rearrange("p (j r) -> p j r", r=2)
    compute_mod(P, dr, [0, 1, 2, 3, 4, 5], mod_view, psb[7], light=True)
    gm_a, gg_a = mod_derived(P, 1, 2, 0, 1)
    gm_f, gg_f = mod_derived(P, 4, 5, 2, 3)
    sh_a = P.modT[:, 0:8, :]
    sh_f = P.modT[:, 24:32, :]
    P.dbgt = []
    if DBG: P.dbgt += [P.dma("sp", lambda h: h.dma_start(out=dbg_mod[:, :, :], in_=P.modT[:]), reads=[P.modb], writes=[P.buf()])]

    h2 = A(P, [128, KC, TT], BF16)
    h2b = P.buf("h2")
    if moe:
        cbT = A(P, [8, TT], BF16)
        cbTb = P.buf("cbT")
        mkT = A(P, [8, TT], BF16)
        mkTb = P.buf("mkT")
        ident = A(P, [128, 128], F32)
        P.dma("sp", lambda h: h.dma_start(out=ident[:], in_=dr["ident"][:, :]), writes=[onesb])
        wr = A(P, [128, KC, 8], F32)
        P.dma("sp", lambda h: h.dma_start(out=wr[:], in_=dr["w_r"].rearrange("(k p) e -> p k e", p=128)), writes=[onesb])
        br_t = A(P, [128, 8], F32)
        P.dma("sp", lambda h: h.dma_start(out=br_t[:], in_=dr["b_r"][:, :]), writes=[onesb])
        sel = A(P, [8, 8, 128], BF16)
        P.dma("pool", lambda h: h.dma_start(out=sel[:], in_=dr["sel"][:, :, :]), writes=[onesb])
    markA = P.a_cur
    wjoin = A(P, [128, 1], F32)
    P.op("dve", lambda h: h.memset(wjoin[:], 0.0), reads=wbufs, writes=[wAb])
    glu_t = A(P, [128, 2, 256], BF16)
    glub = P.buf("glu")
    sgl = A(P, [128, 256], F32)
    sglb = P.buf("sgl")
    xb = [A(P, [128, KC, 256], F32) for _ in range(2)]
    xbb = [P.buf(), P.buf()]
    brb_t = [A(P, [128, KC, 256], BF16) for _ in range(2)]
    brbb = [P.buf(), P.buf()]
    sq = A(P, [128, KC, 256], BF16)
    sqb = P.buf()
    rstd = A(P, [128, 256], F32)
    rstdb = P.buf()
    tmp = [A(P, [128, 256], F32) for _ in range(2)]
    tmpb = [P.buf(), P.buf()]
    hb = A(P, [128, KC, 256], BF16)
    hbb = P.buf()
    yb = A(P, [128, KC, 256], BF16)
    ybb = P.buf()
    zb = A(P, [128, KC, 256], F32)
    zbb = P.buf()
    sg = [A(P, [128, 256], F32) for _ in range(2)]
    sgb = [P.buf(), P.buf()]
    tt2 = [A(P, [128, 256], F32) for _ in range(2)]
    tt2b = [P.buf(), P.buf()]
    accA = [A(P, [128, 256], F32) for _ in range(2)]
    accAb = [P.buf(), P.buf()]
    xob = P.buf("xo")
    h2fb = P.buf("h2f")
    if moe:
        h2f = A(P, [128, KC, 256], F32)
        lg = A(P, [128, 8], F32)
        mx8 = A(P, [128, 8], F32)
        msk = A(P, [128, 8], F32)
        ex = A(P, [128, 8], F32)
        den = A(P, [128, 1], F32)
        nmx = A(P, [128, 1], F32)
        rb = P.buf("router")
    cnt = 0
    P.sscp = A(P, [128, 256], F32)
    P.sscpb = P.buf()
    def _ldA(bi_):
        t0_, n_, _r = blocks[bi_]
        xx, xxb = xb[bi_ % 2], xbb[bi_ % 2]
        bb_, bbb = brb_t[bi_ % 2], brbb[bi_ % 2]
        P.dma("sp", lambda h: h.dma_start(out=xx[:, 0:4, 0:n_], in_=xT[:, 0:4, t0_:t0_ + n_]), writes=[xxb])
        P.dma("sp", lambda h: h.dma_start(out=xx[:, 4:8, 0:n_], in_=xT[:, 4:8, t0_:t0_ + n_]), writes=[xxb])
        P.dma("sp", lambda h: h.dma_start(out=bb_[:, :, 0:n_], in_=brT[:, :, t0_:t0_ + n_]), writes=[bbb])
    _ldA(0)
    for bi, (t0, n, r) in enumerate(blocks):
        x_t, x_b = xb[bi % 2], xbb[bi % 2]
        b_t, b_b = brb_t[bi % 2], brbb[bi % 2]
        if bi + 1 < len(blocks):
            _ldA(bi + 1)
        rms_rstd(P, x_t, x_b, n, sq, sqb, ps[6], psb[6], rstd, rstdb, ones)
        norm_mod(P, x_t, x_b, n, rstd, rstdb, gm_a, sh_a, r, hb, hbb, tmp, tmpb)
        for oc in range(2):
            for kc in range(2):
                P.op("pe", lambda h, oc=oc, kc=kc, n=n, b_t=b_t: h.matmul(ps[6][:, 0:n], lhsT=wglu[:, kc, oc * 128:(oc + 1) * 128], rhs=b_t[:, 2 + kc, 0:n],
                                                                      start=(kc == 0), stop=(kc == 1)), reads=[wAb, b_b], writes=[psb[6]])
            P.op("act", lambda h, oc=oc, n=n: h.activation(out=sgl[:, 0:n], in_=ps[6][:, 0:n], func=AF.Sigmoid, bias=bglu[:, oc:oc + 1], scale=1.0), reads=[psb[6], wAb], writes=[sglb])
            P.op("dve", lambda h, oc=oc, n=n, b_t=b_t: h.tensor_tensor(out=glu_t[:, oc, 0:n], in0=sgl[:, 0:n], in1=b_t[:, 2 + oc, 0:n], op=ALU.mult), reads=[sglb, b_b], writes=[glub])
        for fc in range(8):
            ac, acb = accA[fc % 2], accAb[fc % 2]
            for b in range(4):
                gi = cnt % 2
                cnt += 1
                gps, gpb = ps[gi], psb[gi]
                pps, ppb = ps[2 + gi], psb[2 + gi]
                for k in range(KC):
                    P.op("pe", lambda h, gps=gps, k=k, b=b, fc=fc, n=n: h.matmul(gps[:, 0:n], lhsT=wgt[:, k, b * 1024 + fc * 128:b * 1024 + (fc + 1) * 128], rhs=hb[:, k, 0:n],
                                                                                 start=(k == 0), stop=(k == KC - 1)), reads=[wAb, hbb], writes=[gpb])
                for hh in range(2):
                    rhs_ap = glu_t[:, hh, 0:n] if b == 1 else b_t[:, 2 * b + hh, 0:n]
                    P.op("pe", lambda h, pps=pps, hh=hh, b=b, fc=fc, n=n, rhs_ap=rhs_ap: h.matmul(pps[:, 0:n], lhsT=wbr[:, 2 * b + hh, fc * 128:(fc + 1) * 128], rhs=rhs_ap,
                                                                                          start=(hh == 0), stop=(hh == 1)), reads=[wAb, b_b, glub], writes=[ppb])
                s_t, s_b = sg[gi], sgb[gi]
                P.op("act", lambda h, s_t=s_t, gps=gps, n=n: h.activation(out=s_t[:, 0:n], in_=gps[:, 0:n], func=AF.Sigmoid), reads=[gpb], writes=[s_b])
                if b == 0:
                    P.op("dve", lambda h, ac=ac, s_t=s_t, pps=pps, n=n: h.tensor_tensor(out=ac[:, 0:n], in0=s_t[:, 0:n], in1=pps[:, 0:n], op=ALU.mult),
                         reads=[s_b, ppb], writes=[acb])
                else:
                    t_t, t_b = tt2[gi], tt2b[gi]
                    P.op("dve", lambda h, t_t=t_t, s_t=s_t, pps=pps, n=n: h.tensor_tensor(out=t_t[:, 0:n], in0=s_t[:, 0:n], in1=pps[:, 0:n], op=ALU.mult),
                         reads=[s_b, ppb], writes=[t_b])
                    if b < 3:
                        P.op("pool", lambda h, ac=ac, t_t=t_t, n=n: h.tensor_tensor(out=ac[:, 0:n], in0=ac[:, 0:n], in1=t_t[:, 0:n], op=ALU.add),
                             reads=[acb, t_b], writes=[acb])
                    else:
                        P.op("pool", lambda h, ac=ac, t_t=t_t, n=n, fc=fc: h.tensor_tensor(out=yb[:, fc, 0:n], in0=ac[:, 0:n], in1=t_t[:, 0:n], op=ALU.add),
                             reads=[acb, t_b], writes=[ybb])
        for fc in range(8):
            zi = 4 + fc % 2
            for k in range(KC):
                P.op("pe", lambda h, zi=zi, k=k, fc=fc, n=n: h.matmul(ps[zi][:, 0:n], lhsT=wo[:, k, fc * 128:(fc + 1) * 128], rhs=yb[:, k, 0:n], start=(k == 0), stop=(k == KC - 1)),
                     reads=[wAb, ybb], writes=[psb[zi]])
            P.op("act", lambda h, zi=zi, fc=fc, n=n: h.activation(out=zb[:, fc, 0:n], in_=ps[zi][:, 0:n], func=AF.Copy), reads=[psb[zi]], writes=[zbb])
        rms_rstd(P, zb, zbb, n, sq, sqb, ps[6], psb[6], rstd, rstdb, ones)
        if DBG: P.dbgt.append(P.dma("sp", lambda h, t0=t0, n=n: h.dma_start(out=dbg_z[:, :, t0:t0 + n], in_=zb[:, :, 0:n]), reads=[zbb], writes=[P.buf()]))
        if DBG: P.dbgt.append(P.dma("sp", lambda h, t0=t0, n=n: h.dma_start(out=dbg_r[:, t0:t0 + n], in_=rstd[:, 0:n]), reads=[rstdb], writes=[P.buf()]))
        if DBG: P.dbgt.append(P.dma("sp", lambda h, t0=t0, n=n: h.dma_start(out=dbg_sq[:, :, t0:t0 + n], in_=sq[:, :, 0:n]), reads=[sqb], writes=[P.buf()]))
        if DBG: P.op("dve", lambda h, n=n: h.tensor_copy(out=P.sscp[:, 0:n], in_=ps[6][:, 0:n]), reads=[psb[6]], writes=[P.sscpb])
        if DBG: P.dbgt.append(P.dma("sp", lambda h, t0=t0, n=n: h.dma_start(out=dbg_ss[:, t0:t0 + n], in_=P.sscp[:, 0:n]), reads=[P.sscpb], writes=[P.buf()]))
        for k in range(KC):
            tb_, tt_ = tmpb[k % 2], tmp[k % 2]
            P.op("dve", lambda h, k=k, tt_=tt_, n=n: h.tensor_tensor(out=tt_[:, 0:n], in0=zb[:, k, 0:n], in1=rstd[:, 0:n], op=ALU.mult), reads=[zbb, rstdb], writes=[tb_])
            P.op("dve", lambda h, k=k, tt_=tt_, n=n, x_t=x_t, r=r: h.scalar_tensor_tensor(out=x_t[:, k, 0:n], in0=tt_[:, 0:n], scalar=gg_a[:, k, r:r + 1], in1=x_t[:, k, 0:n],
                                                                                    op0=ALU.mult, op1=ALU.add), reads=[tb_, P.modb, x_b], writes=[x_b])
        P.dma("sp", lambda h, x_t=x_t, t0=t0, n=n: h.dma_start(out=xoT[:, :, t0:t0 + n], in_=x_t[:, :, 0:n]), reads=[x_b], writes=[xob])
        if DBG: P.dbgt.append(P.dma("sp", lambda h, x_t=x_t, t0=t0, n=n: h.dma_start(out=dbg_xm[:, :, t0:t0 + n], in_=x_t[:, :, 0:n]), reads=[x_b], writes=[P.buf()]))
        if DBG: P.dbgt.append(P.dma("sp", lambda h, t0=t0, n=n: h.dma_start(out=dbg_h[:, :, t0:t0 + n], in_=hb[:, :, 0:n]), reads=[hbb], writes=[P.buf()]))
        if DBG: P.dbgt.append(P.dma("sp", lambda h, t0=t0, n=n: h.dma_start(out=dbg_y[:, :, t0:t0 + n], in_=yb[:, :, 0:n]), reads=[ybb], writes=[P.buf()]))
        rms_rstd(P, x_t, x_b, n, sq, sqb, ps[6], psb[6], rstd, rstdb, ones)
        norm_mod(P, x_t, x_b, n, rstd, rstdb, gm_f, sh_f, r, h2, h2b, tmp, tmpb, dst_off=t0, dst32=(h2f if moe else None), dst32b=h2fb)
        if moe:
            for tt in range(n // 128):
                for k in range(KC):
                    P.op("pe", lambda h, k=k, tt=tt: h.matmul(ps[7][:, 0:8], lhsT=h2f[:, k, tt * 128:(tt + 1) * 128], rhs=wr[:, k, :], start=(k == 0), stop=(k == KC - 1)),
                         reads=[h2fb, onesb], writes=[psb[7]])
                P.op("dve", lambda h: h.tensor_tensor(out=lg[:], in0=ps[7][:, 0:8], in1=br_t[:], op=ALU.add), reads=[psb[7], onesb], writes=[rb])
                P.op("dve", lambda h: h.max(out=mx8[:], in_=lg[:]), reads=[rb], writes=[rb])
                P.op("dve", lambda h: h.tensor_scalar(out=msk[:], in0=lg[:], scalar1=mx8[:, 1:2], scalar2=None, op0=ALU.is_ge), reads=[rb], writes=[rb])
                P.op("dve", lambda h: h.tensor_scalar(out=nmx[:], in0=mx8[:, 0:1], scalar1=-1.0, scalar2=None, op0=ALU.mult), reads=[rb], writes=[rb])
                P.op("act", lambda h: h.activation(out=ex[:], in_=lg[:], func=AF.Exp, bias=nmx[:, 0:1], scale=1.0), reads=[rb], writes=[rb])
                P.op("dve", lambda h: h.tensor_tensor(out=ex[:], in0=ex[:], in1=msk[:], op=ALU.mult), reads=[rb], writes=[rb])
                P.op("dve", lambda h: h.reduce_sum(out=den[:], in_=ex[:], axis=AX.X), reads=[rb], writes=[rb])
                P.op("dve", lambda h: h.reciprocal(out=den[:], in_=den[:]), reads=[rb], writes=[rb])
                P.op("dve", lambda h: h.tensor_scalar(out=ex[:], in0=ex[:], scalar1=den[:, 0:1], scalar2=None, op0=ALU.mult), reads=[rb], writes=[rb])
                P.op("pe", lambda h: h.transpose(ps[7][0:8, 128:256], ex[:], ident[:]), reads=[rb, onesb], writes=[psb[7]])
                P.op("act", lambda h, t0=t0, tt=tt: h.activation(out=cbT[:, t0 + tt * 128:t0 + (tt + 1) * 128], in_=ps[7][0:8, 128:256], func=AF.Copy), reads=[psb[7]], writes=[cbTb])
                if mode == 'moe_a':
                    P.op("pe", lambda h: h.transpose(ps[7][0:8, 256:384], msk[:], ident[:]), reads=[rb, onesb], writes=[psb[7]])
                    P.op("act", lambda h, t0=t0, tt=tt: h.activation(out=mkT[:, t0 + tt * 128:t0 + (tt + 1) * 128], in_=ps[7][0:8, 256:384], func=AF.Copy), reads=[psb[7]], writes=[mkTb])
    barrier(P)
    P.a_cur = markA
    if mode == 'moe_a':
        fin = [P.dma("sp", lambda h: h.dma_start(out=h2o[:, :, :], in_=h2[:, :, :]), reads=[h2b], writes=[P.buf()]),
               P.dma("sp", lambda h: h.dma_start(out=cbo[:, :], in_=cbT[:, :]), reads=[cbTb], writes=[P.buf()]),
               P.dma("sp", lambda h: h.dma_start(out=mko[:, :], in_=mkT[:, :]), reads=[mkTb], writes=[P.buf()])]
        barrier(P)
        P.finish_wait("sp", fin + P.dbgt)
        P.emit()
        return nc
    blocks = blocksB
    NSL = 4
    P.a_cur = w_lo
    acc = A(P, [128, KC, TT], F32)
    accb = [P.buf() for _ in blocks]
    hid = [A(P, [128, NSL, 512], BF16) for _ in range(2)]
    hidb = [P.buf(), P.buf()]
    ssb_t = [A(P, [128, 512], F32) for _ in range(2)]
    ssbb = [P.buf(), P.buf()]
    cbe = A(P, [128, 512], BF16)
    cbeb = P.buf()
    assert P.a_cur <= w_hi, "stage-B tiles overflow the weight region"
    P.a_cur = markA
    markB = markA
    wg_s = [A(P, [128, KC, NSL * 128], BF16) for _ in range(2)]
    wu_s = [A(P, [128, KC, NSL * 128], BF16) for _ in range(2)]
    wd_s = [A(P, [128, NSL, D], BF16) for _ in range(2)]
    wsb = [P.buf(), P.buf()]
    ntile = dff // 128
    slices = [(s0, min(NSL, ntile - s0)) for s0 in range(0, ntile, NSL)]
    si = 0
    hcnt = 0
    gcnt = 0
    work = [(e, s0, ns) for e in range(n_exp) for (s0, ns) in slices]

    def _ldW(widx):
        e_, s0_, ns_ = work[widx]
        wi_ = widx % 2
        wgv_ = dr["w_g"][e_].rearrange("(k p) f -> p k f", p=128)
        wuv_ = dr["w_u"][e_].rearrange("(k p) f -> p k f", p=128)
        wdv_ = dr["w_d"][e_].rearrange("(j p) c -> p j c", p=128)
        for k2 in range(2):
            P.dma("pool", lambda h, k2=k2: h.dma_start(out=wg_s[wi_][:, 4 * k2:4 * k2 + 4, 0:ns_ * 128], in_=wgv_[:, 4 * k2:4 * k2 + 4, s0_ * 128:(s0_ + ns_) * 128]), writes=[wsb[wi_]])
            P.dma("pool", lambda h, k2=k2: h.dma_start(out=wu_s[wi_][:, 4 * k2:4 * k2 + 4, 0:ns_ * 128], in_=wuv_[:, 4 * k2:4 * k2 + 4, s0_ * 128:(s0_ + ns_) * 128]), writes=[wsb[wi_]])
        for j in range(ns_):
            P.dma("pool", lambda h, j=j: h.dma_start(out=wd_s[wi_][:, j, :], in_=wdv_[:, s0_ + j, :]), writes=[wsb[wi_]])
    _ldW(0)
    for widx, (e, s0, ns) in enumerate(work):
        if True:
            wi = widx % 2
            if widx + 1 < len(work):
                _ldW(widx + 1)
            for bi, (t0, n, r) in enumerate(blocks):
                hi = hcnt % 2
                hcnt += 1
                if moe:
                    P.op("pe", lambda h, e=e, t0=t0, n=n: h.matmul(ps[7][:, 0:n], lhsT=sel[:, e, :], rhs=cbT[:, t0:t0 + n], start=True, stop=True), reads=[cbTb, onesb], writes=[psb[7]])
                    P.op("act", lambda h, n=n: h.activation(out=cbe[:, 0:n], in_=ps[7][:, 0:n], func=AF.Copy), reads=[psb[7]], writes=[cbeb])
                for j in range(ns):
                    gi = gcnt % 2
                    gcnt += 1
                    for k in range(KC):
                        P.op("pe", lambda h, gi=gi, wi=wi, j=j, k=k, t0=t0, n=n: h.matmul(ps[gi][:, 0:n], lhsT=wg_s[wi][:, k, j * 128:(j + 1) * 128], rhs=h2[:, k, t0:t0 + n], start=(k == 0), stop=(k == KC - 1)),
                             reads=[wsb[wi], h2b], writes=[psb[gi]])
                    for k in range(KC):
                        P.op("pe", lambda h, gi=gi, wi=wi, j=j, k=k, t0=t0, n=n: h.matmul(ps[2 + gi][:, 0:n], lhsT=wu_s[wi][:, k, j * 128:(j + 1) * 128], rhs=h2[:, k, t0:t0 + n], start=(k == 0), stop=(k == KC - 1)),
                             reads=[wsb[wi], h2b], writes=[psb[2 + gi]])
                    P.op("act", lambda h, gi=gi, n=n: h.activation(out=ssb_t[gi][:, 0:n], in_=ps[gi][:, 0:n], func=AF.Silu), reads=[psb[gi]], writes=[ssbb[gi]])
                    if moe:
                        P.op("dve", lambda h, gi=gi, n=n: h.tensor_tensor(out=ssb_t[gi][:, 0:n], in0=ssb_t[gi][:, 0:n], in1=ps[2 + gi][:, 0:n], op=ALU.mult),
                             reads=[ssbb[gi], psb[2 + gi]], writes=[ssbb[gi]])
                        P.op("pool", lambda h, gi=gi, hi=hi, j=j, n=n: h.tensor_tensor(out=hid[hi][:, j, 0:n], in0=ssb_t[gi][:, 0:n], in1=cbe[:, 0:n], op=ALU.mult),
                             reads=[ssbb[gi], cbeb], writes=[hidb[hi]])
                    else:
                        P.op("dve", lambda h, gi=gi, hi=hi, j=j, n=n: h.tensor_tensor(out=hid[hi][:, j, 0:n], in0=ssb_t[gi][:, 0:n], in1=ps[2 + gi][:, 0:n], op=ALU.mult),
                             reads=[ssbb[gi], psb[2 + gi]], writes=[hidb[hi]])
                first = (e == 0 and s0 == 0)
                for fc in range(8):
                    oi = 4 + fc % 2
                    for j in range(ns):
                        P.op("pe", lambda h, oi=oi, wi=wi, j=j, fc=fc, hi=hi, n=n, ns=ns: h.matmul(ps[oi][:, 0:n], lhsT=wd_s[wi][:, j, fc * 128:(fc + 1) * 128], rhs=hid[hi][:, j, 0:n], start=(j == 0), stop=(j == ns - 1)),
                             reads=[wsb[wi], hidb[hi]], writes=[psb[oi]])
                    if first:
                        P.op("act", lambda h, oi=oi, fc=fc, t0=t0, n=n: h.activation(out=acc[:, fc, t0:t0 + n], in_=ps[oi][:, 0:n], func=AF.Copy), reads=[psb[oi]], writes=[accb[bi]])
                    else:
                        P.op("dve", lambda h, oi=oi, fc=fc, t0=t0, n=n: h.tensor_tensor(out=acc[:, fc, t0:t0 + n], in0=acc[:, fc, t0:t0 + n], in1=ps[oi][:, 0:n], op=ALU.add),
                             reads=[psb[oi], accb[bi]], writes=[accb[bi]])
    barrier(P)
    P.a_cur = markB
    xm = [A(P, [128, KC, 512], F32) for _ in range(2)]
    xmb = [P.buf(), P.buf()]
    sqF = A(P, [128, KC, 512], BF16)
    rstdF = A(P, [128, 512], F32)
    tmpF = [A(P, [128, 512], F32) for _ in range(2)]
    outs = []
    for bi, (t0, n, r) in enumerate(blocks):
        x_t, x_b = xm[bi % 2], xmb[bi % 2]
        P.dma("sp", lambda h, x_t=x_t, t0=t0, n=n: h.dma_start(out=x_t[:, :, 0:n], in_=xoT[:, :, t0:t0 + n]), reads=[xob], writes=[x_b])
        accv = acc[:, :, t0:t0 + n]
        P.op("act", lambda h, accv=accv, n=n: h.activation(out=sqF[:, :, 0:n], in_=accv, func=AF.Square), reads=[accb[bi]], writes=[sqb])
        for k in range(KC):
            P.op("pe", lambda h, k=k, n=n: h.matmul(ps[6][:, 0:n], lhsT=ones[:], rhs=sqF[:, k, 0:n], start=(k == 0), stop=(k == KC - 1)), reads=[sqb, onesb], writes=[psb[6]])
        P.op("act", lambda h, n=n: h.activation(out=rstdF[:, 0:n], in_=ps[6][:, 0:n], func=AF.Ln, scale=1.0 / D, bias=P.eps_t[:, 0:1]), reads=[psb[6]], writes=[rstdb])
        P.op("act", lambda h, n=n: h.activation(out=rstdF[:, 0:n], in_=rstdF[:, 0:n], func=AF.Exp, scale=-0.5), reads=[rstdb], writes=[rstdb])
        for k in range(KC):
            tb_, tt_ = tmpb[k % 2], tmpF[k % 2]
            P.op("dve", lambda h, k=k, tt_=tt_, n=n, t0=t0: h.tensor_tensor(out=tt_[:, 0:n], in0=acc[:, k, t0:t0 + n], in1=rstdF[:, 0:n], op=ALU.mult), reads=[accb[bi], rstdb], writes=[tb_])
            P.op("dve", lambda h, k=k, tt_=tt_, n=n, x_t=x_t, r=r: h.scalar_tensor_tensor(out=x_t[:, k, 0:n], in0=tt_[:, 0:n], scalar=gg_f[:, k, r:r + 1], in1=x_t[:, k, 0:n],
                                                                                    op0=ALU.mult, op1=ALU.add), reads=[tb_, P.modb, x_b], writes=[x_b])
        outs.append(P.dma("sp", lambda h, x_t=x_t, t0=t0, n=n: h.dma_start(out=xoT[:, :, t0:t0 + n], in_=x_t[:, :, 0:n]), reads=[x_b], writes=[xob]))
    P.finish_wait("sp", outs + P.dbgt)
    P.emit()
    return nc


def build_E(groups=(4,) * 8, dff=3584):
    nc = bass.Bass("TRN2", target_bir_lowering=False)
    ngrp = len(groups)
    gtok = 512 * max(groups)
    NT = 512 * sum(groups)
    goff = [512 * sum(groups[:g]) for g in range(ngrp)]
    h2d = nc.dram_tensor("h2", [KC, 128, NT], BF16, kind="ExternalInput").ap().rearrange("k p t -> p k t")
    cbd = nc.dram_tensor("cbe", [128, NT], BF16, kind="ExternalInput").ap()
    wgd = nc.dram_tensor("w_g", [D, dff], F32, kind="ExternalInput").ap().rearrange("(k p) f -> p k f", p=128)
    wud = nc.dram_tensor("w_u", [D, dff], F32, kind="ExternalInput").ap().rearrange("(k p) f -> p k f", p=128)
    wdd = nc.dram_tensor("w_d", [dff, D], F32, kind="ExternalInput").ap().rearrange("(j p) c -> p j c", p=128)
    ye = nc.dram_tensor("ye", [KC, 128, NT], F32, kind="ExternalOutput").ap().rearrange("k p t -> p k t")
    P = Prog(nc)
    arena_init(P)
    ps = [P.ps("ps%d" % i, [128, 512], F32) for i in range(8)]
    psb = [P.buf("ps%d" % i) for i in range(8)]
    h2g = [A(P, [128, KC, gtok], BF16) for _ in range(2)]
    h2gb = [P.buf(), P.buf()]
    cbg = [A(P, [128, gtok], BF16) for _ in range(2)]
    acc = A(P, [128, KC, gtok], F32)
    NSL = 4
    wg_s = [A(P, [128, KC, NSL * 128], BF16) for _ in range(2)]
    wu_s = [A(P, [128, KC, NSL * 128], BF16) for _ in range(2)]
    wd_s = [A(P, [128, NSL, D], BF16) for _ in range(2)]
    wsb = [P.buf(), P.buf()]
    hid = [A(P, [128, NSL, 512], BF16) for _ in range(2)]
    hidb = [P.buf(), P.buf()]
    ssb_t = [A(P, [128, 512], F32) for _ in range(2)]
    ssbb = [P.buf(), P.buf()]
    ntile = dff // 128
    slices = [(s0, min(NSL, ntile - s0)) for s0 in range(0, ntile, NSL)]
    accb = [P.buf() for _ in range(max(groups))]
    si = hcnt = gcnt = 0
    outs = []
    work = [(g, sidx, s0, ns) for g in range(ngrp) for sidx, (s0, ns) in enumerate(slices)]

    def _ldG(g_):
        hg_, hgb_ = h2g[g_ % 2], h2gb[g_ % 2]
        gn_ = 512 * groups[g_]
        for k2 in range(2):
            _ldF(P, "sp", hg_[:, 4 * k2:4 * k2 + 4, 0:gn_], h2d[:, 4 * k2:4 * k2 + 4, goff[g_]:goff[g_] + gn_], [hgb_])
        _ldF(P, "sp", cbg[g_ % 2][:, 0:gn_], cbd[:, goff[g_]:goff[g_] + gn_], [hgb_])

    def _ldW(widx):
        _g, _sidx, s0_, ns_ = work[widx]
        wi_ = widx % 2
        for k2 in range(2):
            _ldF(P, "pool", wg_s[wi_][:, 4 * k2:4 * k2 + 4, 0:ns_ * 128], wgd[:, 4 * k2:4 * k2 + 4, s0_ * 128:(s0_ + ns_) * 128], [wsb[wi_]])
            _ldF(P, "pool", wu_s[wi_][:, 4 * k2:4 * k2 + 4, 0:ns_ * 128], wud[:, 4 * k2:4 * k2 + 4, s0_ * 128:(s0_ + ns_) * 128], [wsb[wi_]])
        for j in range(ns_):
            _ldF(P, "pool", wd_s[wi_][:, j, :], wdd[:, s0_ + j, :], [wsb[wi_]])
    _ldG(0)
    _ldW(0)
    pending = []

    def _down(u):
        (g_, sidx_, ns_, wi_, bi_, hi_, last_) = u
        t0_, n_ = bi_ * 512, 512
        for fc in range(8):
            oi = 4 + fc % 2
            for j in range(ns_):
                _mmF(P, ps[oi][:, 0:n_], wd_s[wi_][:, j, fc * 128:(fc + 1) * 128], hid[hi_][:, j, 0:n_], j == 0, j == ns_ - 1, [wsb[wi_], hidb[hi_]], [psb[oi]])
            av = acc[:, fc, t0_:t0_ + n_]
            pv = ps[oi][:, 0:n_]
            if sidx_ == 0:
                P.op("act", lambda h, av=av, pv=pv: h.activation(out=av, in_=pv, func=AF.Copy), reads=[psb[oi]], writes=[accb[bi_]])
            else:
                P.op("dve", lambda h, av=av, pv=pv: h.tensor_tensor(out=av, in0=av, in1=pv, op=ALU.add), reads=[psb[oi], accb[bi_]], writes=[accb[bi_]])
        if last_:
            outs.append(_ldF(P, "sp", ye[:, :, goff[g_] + t0_:goff[g_] + t0_ + 512], acc[:, :, t0_:t0_ + 512], [P.buf()], reads=[accb[bi_]]))

    for widx, (g, sidx, s0, ns) in enumerate(work):
        hg, hgb = h2g[g % 2], h2gb[g % 2]
        cg = cbg[g % 2]
        nblk = groups[g]
        if sidx == 0 and g + 1 < ngrp:
            _ldG(g + 1)
        wi = widx % 2
        for bi in range(nblk):
            t0, n = bi * 512, 512
            hi = hcnt % 2
            hcnt += 1
            for j in range(ns):
                gi = gcnt % 2
                gcnt += 1
                for k in range(KC):
                    _mmF(P, ps[gi][:, 0:n], wg_s[wi][:, k, j * 128:(j + 1) * 128], hg[:, k, t0:t0 + n], k == 0, k == KC - 1, [wsb[wi], hgb], [psb[gi]])
                for k in range(KC):
                    _mmF(P, ps[2 + gi][:, 0:n], wu_s[wi][:, k, j * 128:(j + 1) * 128], hg[:, k, t0:t0 + n], k == 0, k == KC - 1, [wsb[wi], hgb], [psb[2 + gi]])
                st_, stb_ = ssb_t[gi], ssbb[gi]
                P.op("act", lambda h, st_=st_, gi=gi, n=n: h.activation(out=st_[:, 0:n], in_=ps[gi][:, 0:n], func=AF.Silu), reads=[psb[gi]], writes=[stb_])
                P.op("dve", lambda h, st_=st_, gi=gi, n=n: h.tensor_tensor(out=st_[:, 0:n], in0=st_[:, 0:n], in1=ps[2 + gi][:, 0:n], op=ALU.mult), reads=[stb_, psb[2 + gi]], writes=[stb_])
                hd = hid[hi]
                P.op("pool", lambda h, st_=st_, hd=hd, j=j, n=n, cg=cg, t0=t0: h.tensor_tensor(out=hd[:, j, 0:n], in0=st_[:, 0:n], in1=cg[:, t0:t0 + n], op=ALU.mult), reads=[stb_, hgb], writes=[hidb[hi]])
            if pending:
                _down(pending.pop(0))
            if bi == 0 and widx + 1 < len(work):
                _ldW(widx + 1)
            pending.append((g, sidx, ns, wi, bi, hi, sidx == len(slices) - 1))
    while pending:
        _down(pending.pop(0))
    P.finish_wait("sp", outs)
    P.emit()
    return nc


def _ldF(P, q, out, in_, writes, reads=()):
    return P.dma(q, lambda h: h.dma_start(out=out, in_=in_), reads=reads, writes=writes)


def _mmF(P, out, lhsT, rhs, start, stop, reads, writes):
    return P.op("pe", lambda h: h.matmul(out, lhsT=lhsT, rhs=rhs, start=start, stop=stop), reads=reads, writes=writes)


def build_Fc(TT=2048, nexp=8):
    nc = bass.Bass("TRN2", target_bir_lowering=False)
    dr = {}

    def din(name, shape, dt=F32):
        dr[name] = nc.dram_tensor(name, list(shape), dt, kind="ExternalInput").ap()
    din("xm", [KC, 128, TT])
    din("yp", [nexp, KC, 128, TT])
    din("condT", [128, KC, 2])
    din("w_mod", [D, 6 * D])
    din("b_modT", [128, 48, 2])
    din("norm_gT", [128, 4, KC, 2])
    xo = nc.dram_tensor("xo", [KC, 128, TT], F32, kind="ExternalOutput").ap().rearrange("k p t -> p k t")
    xm = dr["xm"].rearrange("k p t -> p k t")
    P = Prog(nc)
    arena_init(P)
    ps = [P.ps("ps%d" % i, [128, 512], F32) for i in range(8)]
    psb = [P.buf("ps%d" % i) for i in range(8)]
    ones = A(P, [128, 128], BF16)
    onesb = P.buf()
    P.op("dve", lambda h: h.memset(ones[:], 1.0), writes=[onesb])
    P.eps_t = A(P, [128, 1], F32)
    P.op("dve", lambda h: h.memset(P.eps_t[:], EPS), writes=[onesb])
    mod_view = ps[7][:, 0:96].rearrange("p (j r) -> p j r", r=2)
    compute_mod(P, dr, [5], mod_view, psb[7])
    _, gg_f = mod_derived(P, 4, 5, 2, 3)
    acc = [A(P, [128, KC, 512], F32) for _ in range(2)]
    accb = [P.buf(), P.buf()]
    part = [A(P, [128, KC, 512], F32) for _ in range(3)]
    partb = [P.buf() for _ in range(3)]
    xt = [A(P, [128, KC, 512], F32) for _ in range(2)]
    xtb = [P.buf(), P.buf()]
    sq = A(P, [128, KC, 512], BF16)
    sqb = P.buf()
    rstd = A(P, [128, 512], F32)
    rstdb = P.buf()
    tmp = [A(P, [128, 512], F32) for _ in range(2)]
    tmpb = [P.buf(), P.buf()]
    outs = []
    pc = 0
    for bi in range(TT // 512):
        t0, n = bi * 512, 512
        a_t, a_b = acc[bi % 2], accb[bi % 2]
        x_t, x_b = xt[bi % 2], xtb[bi % 2]
        _ldF(P, "sp", x_t[:, :, :], xm[:, :, t0:t0 + n], [x_b])
        _ldF(P, "sp", a_t[:, :, :], dr["yp"][0].rearrange("k p t -> p k t")[:, :, t0:t0 + n], [a_b])
        for e in range(1, nexp):
            p_t, p_b = part[pc % 3], partb[pc % 3]
            pc += 1
            _ldF(P, "act" if e % 2 else "sp", p_t[:, :, :], dr["yp"][e].rearrange("k p t -> p k t")[:, :, t0:t0 + n], [p_b])
            eng = "dve" if e % 2 else "pool"
            P.op(eng, lambda h, a_t=a_t, p_t=p_t: h.tensor_tensor(out=a_t[:, :, :], in0=a_t[:, :, :], in1=p_t[:, :, :], op=ALU.add), reads=[a_b, p_b], writes=[a_b])
        rms_rstd(P, a_t, a_b, n, sq, sqb, ps[6], psb[6], rstd, rstdb, ones)
        for k in range(KC):
            tb_, tt_ = tmpb[k % 2], tmp[k % 2]
            P.op("dve", lambda h, k=k, tt_=tt_, a_t=a_t: h.tensor_tensor(out=tt_[:, :], in0=a_t[:, k, :], in1=rstd[:, :], op=ALU.mult), reads=[a_b, rstdb], writes=[tb_])
            P.op("dve", lambda h, k=k, tt_=tt_, x_t=x_t: h.scalar_tensor_tensor(out=x_t[:, k, :], in0=tt_[:, :], scalar=gg_f[:, k, 0:1], in1=x_t[:, k, :], op0=ALU.mult, op1=ALU.add),
                 reads=[tb_, P.modb, x_b], writes=[x_b])
        outs.append(_ldF(P, "sp", xo[:, :, t0:t0 + n], x_t[:, :, :], [P.buf()], reads=[x_b]))
    P.finish_wait("sp", outs)
    P.emit()
    return nc


import math, os
RET_STOP = int(os.environ.get('RET_STOP', '99'))
SKIP = os.environ.get('SKIP', '')

NTOK = 8448
NCH = 66
MAGIC = 12582912.0
TWO_PI = 2.0 * math.pi


def pos_of(dd):
    if dd == 0:
        return list(range(NCH))
    order = [1, 0] + list(range(65, 1, -1))
    pos = [0] * NCH
    for p_, c in enumerate(order):
        pos[c] = p_
    return pos


def range_reduce_sincos(P, ph, sn, cs, tmp, shape_ap, b):
    v = shape_ap
    _ts(P, "dve", v(tmp), v(ph), 1.0 / TWO_PI, MAGIC, ALU.mult, ALU.add, [b], [b])
    _ts(P, "dve", v(tmp), v(tmp), -MAGIC, None, ALU.add, None, [b], [b])
    _stt(P, v(ph), v(tmp), -TWO_PI, v(ph), ALU.mult, ALU.add, [b], [b])
    _ts(P, "dve", v(ph), v(ph), -math.pi, math.pi, ALU.max, ALU.min, [b], [b])
    _act(P, v(sn), v(ph), AF.Sin, [b], [b])
    _ts(P, "dve", v(tmp), v(ph), -1.0, None, ALU.mult, None, [b], [b])
    _tt(P, "dve", v(tmp), v(tmp), v(ph), ALU.max, [b], [b])
    _act(P, v(cs), v(tmp), AF.Sin, [b], [b], scale=-1.0, bias=P.halfpi[0:v(tmp).shape[0], 0:1])


def build_M(need_ctx_out, parts=("four", "s5", "ret", "na"), DBG=False):
    nc = bass.Bass("TRN2", target_bir_lowering=False)
    dr = {}

    def din(name, shape, dt=F32):
        dr[name] = nc.dram_tensor(name, list(shape), dt, kind="ExternalInput").ap()
    din("xT", [KC, 128, NTOK])
    din("condT", [128, KC, 2])
    din("w_mod", [D, 6 * D])
    din("b_modT", [128, 48, 2])
    din("norm_gT", [128, 4, KC, 2])
    din("w_fm", [D, 576])
    din("w_tm", [D, 256])
    din("f_CS", [64, 128], BF16); din("f_RP", [64, 128], BF16); din("f_RQ", [64, 128], BF16)
    din("f_CB", [128, 64, 128], BF16); din("f_SB", [128, 64, 128], BF16)
    din("f_C256", [128, 2, 256], BF16); din("f_S256", [128, 2, 256], BF16)
    din("r_cosF", [64, 8192]); din("r_sinF", [64, 8192]); din("r_cosT", [128, 64, 64]); din("r_sinT", [128, 64, 64])
    din("r_jcol", [128, 2]); din("r_dist", [128, 128]); din("r_mask", [2, 128, 128]); din("r_irow", [2, 64, 128])
    din("s_jrow", [128, 129]); din("s_jcol", [128, 1]); din("s_LT", [2, 128, 128], BF16); din("s_mrow", [64, 4]); din("s_msm", [128, 2, 4])
    din("ident_bf", [128, 128], BF16); din("ident_f", [128, 128])
    din("n_mask", [5, 128, 832]); din("n_toep", [15, 64, 64])
    din("s_sm", [128, 2, 2, 3]); din("s_row", [128, 2, 3, 256]); din("s_hs", [64, 2, 3, 64]); din("s_B", [64, 2, 2, 64])
    din("s_C", [128, 2, 2, 2, 16]); din("s_d", [64, 1])
    din("r_dec", [128, 2]); din("r_gn", [64, 1])
    out = nc.dram_tensor("brT_out", [4, 64, NTOK], BF16, kind="ExternalOutput").ap()
    hT = nc.dram_tensor("hT_scr", [KC, 128, NTOK], BF16, kind="Internal").ap().rearrange("k p t -> p k t")
    xT = dr["xT"].rearrange("k p t -> p k t")

    P = Prog(nc)
    arena_init(P)
    ps = [P.ps("ps%d" % i, [128, 512], F32) for i in range(8)]
    psb = [P.buf("ps%d" % i) for i in range(8)]
    cb = P.buf("consts")
    ones = A(P, [128, 128], BF16)
    P.op("dve", lambda h: h.memset(ones[:], 1.0), writes=[cb])
    P.eps_t = A(P, [128, 1], F32)
    P.op("dve", lambda h: h.memset(P.eps_t[:], EPS), writes=[cb])
    P.halfpi = A(P, [128, 1], F32)
    P.op("dve", lambda h: h.memset(P.halfpi[:], math.pi / 2), writes=[cb])
    P.one_t = A(P, [128, 1], F32)
    P.op("dve", lambda h: h.memset(P.one_t[:], 1.0), writes=[cb])
    ident = A(P, [128, 128], BF16)
    _ld(P, "sp", ident[:], dr["ident_bf"][:, :], [cb])
    mod_view = ps[7][:, 0:96].rearrange("p (j r) -> p j r", r=2)
    compute_mod(P, dr, [0, 1], mod_view, psb[7])
    gm_a, _ = mod_derived(P, 1, None, 0, 0)
    sh_a = P.modT[:, 0:8, :]
    wfm = A(P, [128, KC, 576], BF16)
    wtm = A(P, [128, KC, 256], BF16)
    wb = P.buf("w")
    _ld(P, "pool", wfm[:], dr["w_fm"].rearrange("(k p) c -> p k c", p=128), [wb])
    _ld(P, "pool", wtm[:], dr["w_tm"].rearrange("(k p) c -> p k c", p=128), [wb])
    blocks = [(0, 256, 1)] + [(256 + 512 * i, 512, 0) for i in range(16)]
    outs = []
    hTb = P.buf("hT")
    mark0 = P.a_cur

    def fm_proj(hb, hbb, n, g, pst, pstb):
        for k in range(KC):
            _mm(P, pst[0:64, 0:n], wfm[:, k, g * 64:(g + 1) * 64], hb[:, k, 0:n], k == 0, k == KC - 1, [wb, hbb], [pstb])

    sT = A(P, [64, NTOK], BF16)
    markS = P.a_cur
    fT = A(P, [64, NTOK], BF16)
    fTb, sTb = P.buf("fT"), P.buf("sT")
    markA = P.a_cur
    xb = [A(P, [128, KC, 512], F32) for _ in range(2)]
    xbb = [P.buf(), P.buf()]
    sq = A(P, [128, KC, 512], BF16)
    sqb = P.buf()
    rstd = A(P, [128, 512], F32)
    rstdb = P.buf()
    tmp = [A(P, [128, 512], F32) for _ in range(8)]
    tmpb = [P.buf() for _ in range(8)]
    hbs = [A(P, [128, KC, 512], BF16) for _ in range(2)]
    hbsb = [P.buf(), P.buf()]
    def _ldx(bi_):
        t0_, n_, _r = blocks[bi_]
        _ld(P, "sp", xb[bi_ % 2][:, 0:4, 0:n_], xT[:, 0:4, t0_:t0_ + n_], [xbb[bi_ % 2]])
        _ld(P, "sp", xb[bi_ % 2][:, 4:8, 0:n_], xT[:, 4:8, t0_:t0_ + n_], [xbb[bi_ % 2]])
    _ldx(0)
    for bi, (t0, n, r) in enumerate(blocks):
        x_t, x_b = xb[bi % 2], xbb[bi % 2]
        hb, hbb = hbs[bi % 2], hbsb[bi % 2]
        if bi + 1 < len(blocks):
            _ldx(bi + 1)
        rms_rstd(P, x_t, x_b, n, sq, sqb, ps[6], psb[6], rstd, rstdb, ones)
        norm_mod(P, x_t, x_b, n, rstd, rstdb, gm_a, sh_a, r, hb, hbb, tmp, tmpb)
        _ld(P, "sp", hT[:, :, t0:t0 + n], hb[:, :, 0:n], [hTb], reads=[hbb])
        for gi_, (g, dst, dstb) in enumerate(((0, fT, fTb), (1, sT, sTb))):
            pi_ = (2 * bi + gi_) % 4
            fm_proj(hb, hbb, n, g, ps[pi_], psb[pi_])
            _cp(P, "act" if gi_ == 0 else "dve", dst[:, t0:t0 + n], ps[pi_][0:64, 0:n], [psb[pi_]], [dstb])
    barrier(P)
    P.a_cur = markA

    if "four" in parts:
        markF = P.a_cur
        CS = A(P, [64, 128], BF16); RP = A(P, [64, 128], BF16); RQ = A(P, [64, 128], BF16)
        CB = A(P, [128, 64, 128], BF16); SB = A(P, [128, 64, 128], BF16)
        ftb = P.buf("ftab")
        for t_, nm in ((CS, "f_CS"), (RP, "f_RP"), (RQ, "f_RQ")):
            _ld(P, "sp", t_[:], dr[nm][:, :], [ftb])
        _ld(P, "sp", CB[:], dr["f_CB"][:, :, :], [ftb])
        _ld(P, "sp", SB[:], dr["f_SB"][:, :, :], [ftb])
        PQ = A(P, [64, 128, 128], BF16); PQb = P.buf("PQ")
        UVT = A(P, [128, 64, 128], BF16); UVTb = P.buf("UVT")
        aT = A(P, [64, NTOK], BF16); aTb = P.buf("aT")
        for g4 in range(32):
            pi_ = g4 % 2
            for jj in range(4):
                m2 = g4 * 4 + jj
                _mm(P, ps[pi_][0:64, jj * 128:(jj + 1) * 128], fT[:, 256 + m2:NTOK:128], CS[:, :], True, True, [fTb, ftb], [psb[pi_]])
            _cp(P, "act" if g4 % 2 else "dve", PQ[:, g4 * 4:(g4 + 1) * 4, :], ps[pi_][0:64, 0:512].rearrange("p (a b) -> p a b", b=128), [psb[pi_]], [PQb])
        for g4 in range(16):
            pi_ = 2 + g4 % 2
            for jj in range(4):
                d = g4 * 4 + jj
                _mm(P, ps[pi_][:, jj * 128:(jj + 1) * 128], PQ[:, :, d], RP[:, :], True, False, [PQb, ftb], [psb[pi_]])
                _mm(P, ps[pi_][:, jj * 128:(jj + 1) * 128], PQ[:, :, 64 + d], RQ[:, :], False, True, [PQb, ftb], [psb[pi_]])
            _cp(P, "act" if g4 % 2 else "dve", UVT[:, g4 * 4:(g4 + 1) * 4, :], ps[pi_][:, 0:512].rearrange("p (a b) -> p a b", b=128), [psb[pi_]], [UVTb])
        aT3 = aT[:, 256:NTOK].rearrange("p (a b) -> p a b", b=64)
        for g4 in range(16):
            pi_ = g4 % 2
            for jj in range(4):
                n1 = g4 * 4 + jj
                _mm(P, ps[pi_][0:64, jj * 128:(jj + 1) * 128], UVT[:, :, n1], CB[:, n1, :], True, False, [UVTb, ftb], [psb[pi_]])
                _mm(P, ps[pi_][0:64, jj * 128:(jj + 1) * 128], UVT[:, :, 64 + n1], SB[:, n1, :], False, True, [UVTb, ftb], [psb[pi_]])
            _cp(P, "act" if g4 % 2 else "dve", aT3[:, :, g4 * 4:(g4 + 1) * 4], ps[pi_][0:64, 0:512].rearrange("p (j n) -> p n j", n=128), [psb[pi_]], [aTb])
        if need_ctx_out:
            C256 = A(P, [128, 2, 256], BF16); S256 = A(P, [128, 2, 256], BF16)
            _ld(P, "sp", C256[:], dr["f_C256"][:, :, :], [ftb])
            _ld(P, "sp", S256[:], dr["f_S256"][:, :, :], [ftb])
            PQc = A(P, [128, 2, 128], BF16); PQcb = P.buf()
            for tt in range(2):
                _mm(P, ps[2 + tt][:, 0:128], fT[:, tt * 128:(tt + 1) * 128], CS[:, :], True, True, [fTb, ftb], [psb[2 + tt]])
                _cp(P, "dve", PQc[:, tt, :], ps[2 + tt][:, 0:128], [psb[2 + tt]], [PQcb])
            seq = [(tt, 0) for tt in range(2)] + [(tt, 1) for tt in range(2)]
            for i_, (tt, pq) in enumerate(seq):
                _mm(P, ps[4][0:64, 0:256], PQc[:, tt, pq * 64:(pq + 1) * 64], (C256 if pq == 0 else S256)[:, tt, :], i_ == 0, i_ == 3, [PQcb, ftb], [psb[4]])
            _cp(P, "dve", aT[:, 0:256], ps[4][0:64, 0:256], [psb[4]], [aTb])
        else:
            P.op("dve", lambda h: h.memset(aT[:, 0:256], 0.0), writes=[aTb])
        outs.append(_ld(P, "sp", out[0, :, :], aT[:, :], [P.buf()], reads=[aTb]))
        barrier(P)
    P.a_cur = markS

    if "s5" in parts:
        s5_part(P, dr, ps, psb, sT, sTb, out, outs, need_ctx_out, cb)
    barrier(P)
    P.a_cur = mark0

    if "ret" in parts:
        ret_part(P, dr, ps, psb, hT, hTb, wfm, wtm, wb, blocks, out, outs, need_ctx_out, cb, ones)
        barrier(P)
        P.a_cur = mark0
    if "na" in parts:
        na_part(P, dr, ps, psb, hT, hTb, wfm, wtm, wb, blocks, out, outs, need_ctx_out, cb, ident)
        barrier(P)
    P.finish_wait("sp", outs)
    P.emit()
    return nc


def load_h(P, hT, hTb, hbs, hbsb, bi, t0, n):
    hb, hbb = hbs[bi % 2], hbsb[bi % 2]
    _ld(P, "sp", hb[:, :, 0:n], hT[:, :, t0:t0 + n], [hbb], reads=[hTb])
    return hb, hbb


def na_part(P, dr, ps, psb, hT, hTb, wfm, wtm, wb, blocks, out, outs, need_ctx_out, cb, ident):
    Cn = consts()
    drs, types = Cn["n_drs"], Cn["n_types"]
    nqT = A(P, [64, NTOK], BF16); nkT = A(P, [64, NTOK], BF16); nvT = A(P, [128, NCH, 64], BF16)
    nqb, nkb, nvb = P.buf("nq"), P.buf("nk"), P.buf("nv")
    nT = A(P, [64, NTOK], BF16); nTb = P.buf("nT")
    bias = A(P, [128, 5, 832], F32); biasb = P.buf("bias")
    mask = A(P, [128, 5, 832], F32)
    P.op("pool", lambda h: h.memset(bias[:], 0.0), writes=[biasb])
    maskb = P.buf()
    _ld(P, "sp", mask[:], dr["n_mask"].rearrange("t p c -> p t c"), [maskb])
    for ti in range(5):
        for qr in range(2):
            for i in range(9):
                _ld(P, "sp" if (i % 2) else "act", bias[qr * 64:(qr + 1) * 64, ti, i * 64:(i + 1) * 64], dr["n_toep"][int(drs[ti, qr, i])], [biasb])
    _tt(P, "dve", bias[:], bias[:], mask[:], ALU.add, [biasb, maskb], [biasb])
    mark = P.a_cur
    hbs = [A(P, [128, KC, 512], BF16) for _ in range(2)]
    hbsb = [P.buf(), P.buf()]
    cnt = 0
    for bi, (t0, n, r) in enumerate(blocks):
        hb, hbb = load_h(P, hT, hTb, hbs, hbsb, bi, t0, n)
        for g, dst, dstb in ((7, nqT, nqb), (8, nkT, nkb)):
            pi_ = cnt % 4
            cnt += 1
            for k in range(KC):
                _mm(P, ps[pi_][0:64, 0:n], wfm[:, k, g * 64:(g + 1) * 64], hb[:, k, 0:n], k == 0, k == KC - 1, [wb, hbb], [psb[pi_]])
            _cp(P, "act" if g == 7 else "dve", dst[:, t0:t0 + n], ps[pi_][0:64, 0:n], [psb[pi_]], [dstb])
        for tt in range(n // 128):
            pi_ = 4 + (tt % 2)
            for k in range(KC):
                _mm(P, ps[pi_][:, 0:64], hb[:, k, tt * 128:(tt + 1) * 128], wtm[:, k, 192:256], k == 0, k == KC - 1, [wb, hbb], [psb[pi_]])
            _cp(P, "act", nvT[:, t0 // 128 + tt, :], ps[pi_][:, 0:64], [psb[pi_]], [nvb])
    barrier(P)
    P.a_cur = mark
    NB4 = 4
    s_t = [A(P, [128, 832], F32) for _ in range(NB4)]; s_b = [P.buf() for _ in range(NB4)]
    p_t = [A(P, [128, 832], BF16) for _ in range(NB4)]; p_b = [P.buf() for _ in range(NB4)]
    pT = [A(P, [128, 7, 128], BF16) for _ in range(NB4)]; pTb = [P.buf() for _ in range(NB4)]
    st_ = [A(P, [128, 4], F32) for _ in range(NB4)]; stb = [P.buf() for _ in range(NB4)]
    SC = 0.125

    def softmax_pv(qi, ncols, pv_list, o_ps, o_psb, o_cols, sbi, s4=None):
        if s4 is None:
            s4 = sbi
        s, sb_ = s_t[s4], s_b[s4]
        sm, smb = st_[s4], stb[s4]
        P.op("dve", lambda h: h.reduce_max(out=sm[:, 1:2], in_=s[:, 0:ncols], axis=AX.X, negate=True), reads=[sb_], writes=[smb])
        _act(P, s[:, 0:ncols], s[:, 0:ncols], AF.Exp, [sb_, smb], [sb_], bias=sm[:, 1:2], scale=1.0)
        P.op("dve", lambda h: h.reduce_sum(out=sm[:, 2:3], in_=s[:, 0:ncols], axis=AX.X), reads=[sb_], writes=[smb])
        P.op("dve", lambda h: h.reciprocal(out=sm[:, 3:4], in_=sm[:, 2:3]), reads=[smb], writes=[smb])
        p, pb = p_t[s4], p_b[s4]
        _ts(P, "dve", p[:, 0:ncols], s[:, 0:ncols], sm[:, 3:4], None, ALU.mult, None, [sb_, smb], [pb])
        tp = ps[4 + sbi][:, :].bitcast(BF16)
        for ci, (c0, nk, tile) in enumerate(pv_list):
            _tr(P, tp[0:nk, ci * 128:(ci + 1) * 128], p[:, c0:c0 + nk], ident[:, :], [pb, cb], [psb[4 + sbi]])
        nchk = len(pv_list)
        pt_, ptb = pT[s4], pTb[s4]
        _cp(P, "act", pt_[:, 0:nchk, :], tp[:, 0:nchk * 128].rearrange("p (a b) -> p a b", b=128), [psb[4 + sbi]], [ptb])
        for ci, (c0, nk, tile) in enumerate(pv_list):
            _mm(P, o_ps[0:64, o_cols:o_cols + 128], nvT[0:nk, tile, :], pt_[0:nk, ci, :], ci == 0, ci == nchk - 1, [nvb, ptb], [o_psb])

    def tile_geo(rp):
        ti = {0: 0, 1: 1, 62: 3, 63: 4}.get(rp, 2)
        r0 = 2 * rp
        if ti == 2:
            R0, nr = r0 - 4, 9
        else:
            R0, nr = types[ti][1], 8
        t_base = 2 + R0 // 2
        pv = [(128 * j, 128, t_base + j) for j in range(4)]
        if nr == 9:
            pv.append((512, 64, t_base + 4))
        pv += [(576, 128, 0), (704, 128, 1)]
        return ti, R0, nr, pv

    def stage_S(rp):
        ti, R0, nr, pv = tile_geo(rp)
        tq = 256 + 128 * rp
        kb_ = 256 + 64 * R0
        sbi = rp % 2
        s1, s2 = ps[sbi], ps[2 + sbi]
        _mm(P, s1[:, 0:512], nqT[:, tq:tq + 128], nkT[:, kb_:kb_ + 512], True, True, [nqb, nkb], [psb[sbi]])
        kb2 = kb_ + 512 if nr == 9 else kb_
        _mm(P, s2[:, 0:64], nqT[:, tq:tq + 128], nkT[:, kb2:kb2 + 64], True, True, [nqb, nkb], [psb[2 + sbi]])
        _mm(P, s2[:, 64:320], nqT[:, tq:tq + 128], nkT[:, 0:256], True, True, [nqb, nkb], [psb[2 + sbi]])
        s4 = rp % NB4
        s = s_t[s4]
        _stt(P, s[:, 0:512], s1[:, 0:512], SC, bias[:, ti, 0:512], ALU.mult, ALU.add, [psb[sbi], biasb], [s_b[s4]])
        _stt(P, s[:, 512:832], s2[:, 0:320], SC, bias[:, ti, 512:832], ALU.mult, ALU.add, [psb[2 + sbi], biasb], [s_b[s4]])

    def stage_M(rp, ncols=832):
        s4 = rp % NB4
        s, sb_ = s_t[s4], s_b[s4]
        sm, smb = st_[s4], stb[s4]
        P.op("dve", lambda h: h.reduce_max(out=sm[:, 1:2], in_=s[:, 0:ncols], axis=AX.X, negate=True), reads=[sb_], writes=[smb])
        _act(P, s[:, 0:ncols], s[:, 0:ncols], AF.Exp, [sb_, smb], [sb_], bias=sm[:, 1:2], scale=1.0)
        P.op("dve", lambda h: h.reduce_sum(out=sm[:, 2:3], in_=s[:, 0:ncols], axis=AX.X), reads=[sb_], writes=[smb])
        P.op("dve", lambda h: h.reciprocal(out=sm[:, 3:4], in_=sm[:, 2:3]), reads=[smb], writes=[smb])
        _ts(P, "dve", p_t[s4][:, 0:ncols], s[:, 0:ncols], sm[:, 3:4], None, ALU.mult, None, [sb_, smb], [p_b[s4]])

    def stage_TV(rp):
        ti, R0, nr, pv_list = tile_geo(rp)
        sbi = rp % 2
        s4 = rp % NB4
        p, pb = p_t[s4], p_b[s4]
        tp = ps[4 + sbi][:, :].bitcast(BF16)
        for ci, (c0, nk, tile) in enumerate(pv_list):
            _tr(P, tp[0:nk, ci * 128:(ci + 1) * 128], p[:, c0:c0 + nk], ident[:, :], [pb, cb], [psb[4 + sbi]])
        nchk = len(pv_list)
        pt_, ptb = pT[s4], pTb[s4]
        _cp(P, "act", pt_[:, 0:nchk, :], tp[:, 0:nchk * 128].rearrange("p (a b) -> p a b", b=128), [psb[4 + sbi]], [ptb])
        jj = rp % 4
        for ci, (c0, nk, tile) in enumerate(pv_list):
            _mm(P, ps[6][0:64, jj * 128:(jj + 1) * 128], nvT[0:nk, tile, :], pt_[0:nk, ci, :], ci == 0, ci == nchk - 1, [nvb, ptb], [psb[6]])
        if jj == 3:
            _cp(P, "dve", nT[:, 256 + 512 * (rp // 4):256 + 512 * (rp // 4 + 1)], ps[6][0:64, 0:512], [psb[6]], [nTb])

    stage_S(0)
    stage_S(1)
    stage_M(0)
    for rp in range(64):
        if rp + 2 < 64:
            stage_S(rp + 2)
        if rp + 1 < 64:
            stage_M(rp + 1)
        stage_TV(rp)
    if need_ctx_out:
        for qt in range(2):
            sbi = qt
            _mm(P, ps[sbi][:, 0:256], nqT[:, qt * 128:(qt + 1) * 128], nkT[:, 0:256], True, True, [nqb, nkb], [psb[sbi]])
            _ts(P, "dve", s_t[sbi][:, 0:256], ps[sbi][:, 0:256], SC, None, ALU.mult, None, [psb[sbi]], [s_b[sbi]])
            softmax_pv(qt, 256, [(0, 128, 0), (128, 128, 1)], ps[7], psb[7], qt * 128, sbi)
        _cp(P, "dve", nT[:, 0:256], ps[7][0:64, 0:256], [psb[7]], [nTb])
    else:
        P.op("dve", lambda h: h.memset(nT[:, 0:256], 0.0), writes=[nTb])
    outs.append(_ld(P, "sp", out[3, :, :], nT[:, :], [P.buf()], reads=[nTb]))


def ret_part(P, dr, ps, psb, hT, hTb, wfm, wtm, wb, blocks, out, outs, need_ctx_out, cb, ones):
    KS = 0.125
    qT = A(P, [64, NTOK], BF16); kT = A(P, [64, NTOK], BF16); gT = A(P, [64, NTOK], BF16)
    qb_, kb_, gb_ = P.buf("q"), P.buf("k"), P.buf("g")
    rvT = A(P, [128, NCH, 64], BF16); rvb = P.buf("rv")
    Sbf = [A(P, [64, NCH, 64], BF16) for _ in range(2)]
    rc = P.buf("retc")
    dec = A(P, [128, 2], F32); lg = A(P, [128, 2], F32); jcol = A(P, [128, 2], F32); kdec = A(P, [128, 2], F32); g128 = A(P, [128, 2], F32)
    dist = A(P, [128, 128], F32); msk = A(P, [128, 2, 128], F32); DT = A(P, [128, 128], F32); DT2 = A(P, [128, 128], F32)
    irow = A(P, [64, 2, 128], F32); qdec = A(P, [64, 2, 128], F32)
    gn = A(P, [64, 1], F32); o64 = A(P, [64, 64], F32)
    _ld(P, "sp", dec[:], dr["r_dec"][:, :], [rc])
    _ld(P, "sp", jcol[:], dr["r_jcol"][:, :], [rc])
    _ld(P, "sp", dist[:], dr["r_dist"][:, :], [rc])
    _ld(P, "sp", msk[:], dr["r_mask"].rearrange("d j i -> j d i"), [rc])
    _ld(P, "sp", irow[:], dr["r_irow"].rearrange("d p i -> p d i"), [rc])
    _ld(P, "sp", gn[:], dr["r_gn"][:, :], [rc])
    P.op("dve", lambda h: h.memset(o64[:], 1.0 / 64), writes=[rc])
    _act(P, lg[:], dec[:], AF.Exp, [rc], [rc], scale=-1.0)
    _act(P, lg[:], lg[:], AF.Ln, [rc], [rc], bias=P.one_t[:, 0:1], scale=1.0)
    _ts(P, "dve", lg[:], lg[:], -1.0, None, ALU.mult, None, [rc], [rc])
    for dd in range(2):
        _act(P, kdec[:, dd:dd + 1], jcol[:, dd:dd + 1], AF.Exp, [rc], [rc], scale=lg[:, dd:dd + 1])
        _act(P, g128[:, dd:dd + 1], lg[:, dd:dd + 1], AF.Exp, [rc], [rc], scale=128.0)
        _act(P, qdec[:, dd, :], irow[:, dd, :], AF.Exp, [rc], [rc], scale=lg[0:64, dd:dd + 1])
    _ts(P, "dve", kdec[:], kdec[:], KS, None, ALU.mult, None, [rc], [rc])
    _act(P, DT[:], dist[:], AF.Exp, [rc], [rc], scale=lg[:, 0:1])
    _tt(P, "dve", DT[:], DT[:], msk[:, 0, :], ALU.mult, [rc], [rc])
    _act(P, DT2[:], dist[:], AF.Exp, [rc], [rc], scale=lg[:, 1:2])
    _tt(P, "dve", DT2[:], DT2[:], msk[:, 1, :], ALU.mult, [rc], [rc])
    _tt(P, "dve", DT[:], DT[:], DT2[:], ALU.add, [rc], [rc])
    if RET_STOP <= 0:
        return
    mark_k = P.a_cur
    kd = [A(P, [128, NCH, 64], BF16) for _ in range(2)]
    kdb = [P.buf(), P.buf()]
    mark = P.a_cur
    hbs = [A(P, [128, KC, 512], BF16) for _ in range(2)]
    hbsb = [P.buf(), P.buf()]
    cF = [A(P, [64, 512], F32) for _ in range(2)]; sF = [A(P, [64, 512], F32) for _ in range(2)]
    cTt = [A(P, [128, 4, 64], F32) for _ in range(2)]; sTt = [A(P, [128, 4, 64], F32) for _ in range(2)]
    tabb = [P.buf(), P.buf()]
    t1 = [A(P, [128, 512], F32) for _ in range(2)]; t1b = [P.buf(), P.buf()]
    t2 = [A(P, [128, 512], F32) for _ in range(2)]; t2b = [P.buf(), P.buf()]
    cnt = 0
    for bi, (t0, n, r) in enumerate(blocks):
        hb, hbb = load_h(P, hT, hTb, hbs, hbsb, bi, t0, n)
        lat = (r == 0)
        tb_ = tabb[bi % 2]
        if lat:
            m0 = t0 - 256
            _ld(P, "sp", cF[bi % 2][:, :], dr["r_cosF"][:, m0:m0 + 512], [tb_])
            _ld(P, "sp", sF[bi % 2][:, :], dr["r_sinF"][:, m0:m0 + 512], [tb_])
            _ld(P, "sp", cTt[bi % 2][:, :, :], dr["r_cosT"][:, m0 // 128:m0 // 128 + 4, :], [tb_])
            _ld(P, "sp", sTt[bi % 2][:, :, :], dr["r_sinT"][:, m0 // 128:m0 // 128 + 4, :], [tb_])

        def proj(g, pi_):
            for k in range(KC):
                _mm(P, ps[pi_][0:64, 0:n], wfm[:, k, g * 64:(g + 1) * 64], hb[:, k, 0:n], k == 0, k == KC - 1, [wb, hbb], [psb[pi_]])
        for (g, gsw, dst, dstb, scl) in ((2, 4, qT, qb_, 1.0), (3, 5, kT, kb_, KS)):
            if 'qk' in SKIP:
                continue
            proj(g, 0)
            if lat:
                proj(gsw, 1)
                i2 = cnt % 2
                cnt += 1
                _stt(P, t1[i2][0:64, 0:n], ps[0][0:64, 0:n], scl, cF[bi % 2][:, 0:n], ALU.mult, ALU.mult, [psb[0], tb_], [t1b[i2]])
                _stt(P, t2[i2][0:64, 0:n], ps[1][0:64, 0:n], scl, sF[bi % 2][:, 0:n], ALU.mult, ALU.mult, [psb[1], tb_], [t2b[i2]])
                _tt(P, "pool", dst[:, t0:t0 + n], t1[i2][0:64, 0:n], t2[i2][0:64, 0:n], ALU.add, [t1b[i2], t2b[i2]], [dstb])
            else:
                _act(P, dst[:, t0:t0 + n], ps[0][0:64, 0:n], AF.Copy, [psb[0]], [dstb], scale=scl)
        proj(6, 2)
        _cp(P, "act", gT[:, t0:t0 + n], ps[2][0:64, 0:n], [psb[2]], [gb_])
        for tt in range(n // 128):
            if 'tm' in SKIP:
                continue
            pi_ = 4 + (tt % 2)
            tile = t0 // 128 + tt
            for k in range(KC):
                _mm(P, ps[pi_][:, 0:192], hb[:, k, tt * 128:(tt + 1) * 128], wtm[:, k, 0:192], k == 0, k == KC - 1, [wb, hbb], [psb[pi_]])
            _cp(P, "act", rvT[:, tile, :], ps[pi_][:, 128:192], [psb[pi_]], [rvb])
            if 'kd' in SKIP:
                continue
            if ('kl' in SKIP and lat) or ('kc' in SKIP and not lat):
                continue
            if lat:
                i2 = cnt % 2
                cnt += 1
                _tt(P, "dve", t1[i2][:, 0:64], ps[pi_][:, 0:64], cTt[bi % 2][:, tt, :], ALU.mult, [psb[pi_], tb_], [t1b[i2]])
                _tt(P, "dve", t2[i2][:, 0:64], ps[pi_][:, 64:128], sTt[bi % 2][:, tt, :], ALU.mult, [psb[pi_], tb_], [t2b[i2]])
                if 'k1' in SKIP:
                    continue
                _tt(P, "dve", t1[i2][:, 0:64], t1[i2][:, 0:64], t2[i2][:, 0:64], ALU.add, [t1b[i2], t2b[i2]], [t1b[i2]])
                if 'k2' in SKIP:
                    continue
                for dd in range(2):
                    _act(P, kd[dd][:, tile, :], t1[i2][:, 0:64], AF.Identity, [t1b[i2], rc], [kdb[dd]], scale=kdec[:, dd:dd + 1])
            else:
                for dd in range(2):
                    _act(P, kd[dd][:, tile, :], ps[pi_][:, 0:64], AF.Identity, [psb[pi_], rc], [kdb[dd]], scale=kdec[:, dd:dd + 1])
    barrier(P)
    P.a_cur = mark
    if RET_STOP <= 1:
        return
    S32 = [A(P, [64, NCH, 64], F32) for _ in range(2)]
    Sb = [P.buf(), P.buf()]
    orders = []
    for dd in range(2):
        pos = pos_of(dd)
        orders.append(sorted(range(NCH), key=lambda c, pos=pos: pos[c]))
        c0 = orders[dd][0]
        P.op("dve", lambda h, dd=dd, c0=c0: h.memset(S32[dd][:, c0, :], 0.0), writes=[Sb[dd]])
    for idx in range(NCH - 1):
        for dd in range(2):
            c, nxt = orders[dd][idx], orders[dd][idx + 1]
            pi_ = 2 * dd + (idx // 8) % 2
            sl = idx % 8
            _mm(P, ps[pi_][0:64, sl * 64:(sl + 1) * 64], kd[dd][:, c, :], rvT[:, c, :], True, True, [kdb[dd], rvb], [psb[pi_]])
            _stt(P, S32[dd][:, nxt, :], S32[dd][:, c, :], g128[0:64, dd:dd + 1], ps[pi_][0:64, sl * 64:(sl + 1) * 64], ALU.mult, ALU.add, [Sb[dd], psb[pi_], rc], [Sb[dd]])
    for dd in range(2):
        _cp(P, "act", Sbf[dd][:], S32[dd][:], [Sb[dd]], [Sb[dd]])
    barrier(P)
    P.a_cur = mark_k
    if RET_STOP <= 2:
        return
    oT = A(P, [64, NTOK], F32); oTb = P.buf("oT")
    sc = [A(P, [128, 128], BF16) for _ in range(2)]; scb = [P.buf(), P.buf()]
    qd = [[A(P, [64, 128], BF16) for _ in range(2)] for _ in range(2)]
    qdb = [[P.buf(), P.buf()] for _ in range(2)]
    c_start = 0 if need_ctx_out else 2
    if not need_ctx_out:
        P.op("pool", lambda h: h.memset(oT[:, 0:256], 0.0), writes=[oTb])
    def ret_S(c):
        tau = 128 * c
        i2 = c % 2
        _mm(P, ps[i2][:, 0:128], kT[:, tau:tau + 128], qT[:, tau:tau + 128], True, True, [kb_, qb_], [psb[i2]])
        _tt(P, "dve", sc[i2][:, :], ps[i2][:, 0:128], DT[:, :], ALU.mult, [psb[i2], rc], [scb[i2]])
        for dd in range(2):
            _tt(P, "pool", qd[dd][i2][:, :], qT[:, tau:tau + 128], qdec[:, dd, :], ALU.mult, [qb_, rc], [qdb[dd][i2]])

    def ret_V(c):
        i2 = c % 2
        jj = c % 4
        po = ps[4 + (c // 4) % 2]
        pob = psb[4 + (c // 4) % 2]
        _mm(P, po[0:64, jj * 128:(jj + 1) * 128], rvT[:, c, :], sc[i2][:, :], True, False, [rvb, scb[i2]], [pob])
        _mm(P, po[0:64, jj * 128:(jj + 1) * 128], Sbf[0][:, c, :], qd[0][i2][:, :], False, False, [Sb[0], qdb[0][i2]], [pob])
        _mm(P, po[0:64, jj * 128:(jj + 1) * 128], Sbf[1][:, c, :], qd[1][i2][:, :], False, True, [Sb[1], qdb[1][i2]], [pob])
        if jj == 3 or c == NCH - 1:
            b0 = (c // 4) * 512
            wid = (jj + 1) * 128
            lo = 0
            if (not need_ctx_out) and c // 4 == 0:
                lo = 256
            _cp(P, "act", oT[:, b0 + lo:b0 + wid], po[0:64, lo:wid], [pob], [oTb])

    ret_S(c_start)
    for c in range(c_start, NCH):
        if c + 1 < NCH:
            ret_S(c + 1)
        ret_V(c)
    if RET_STOP <= 3:
        return
    rT = A(P, [64, NTOK], BF16); rTb = P.buf("rT")
    o64b = A(P, [64, 64], BF16)
    P.op("dve", lambda h: h.memset(o64b[:], 1.0 / 64), writes=[rc])
    obf = [A(P, [64, 512], BF16) for _ in range(2)]; obfb = [P.buf(), P.buf()]
    cen = [A(P, [64, 512], F32) for _ in range(2)]; cenb = [P.buf(), P.buf()]
    sq_ = [A(P, [64, 512], BF16) for _ in range(2)]; sqb_ = [P.buf(), P.buf()]
    rs_ = [A(P, [64, 512], F32) for _ in range(2)]; rsb_ = [P.buf(), P.buf()]
    sg_ = [A(P, [64, 512], F32) for _ in range(2)]; sgb_ = [P.buf(), P.buf()]
    nblk = (NTOK + 511) // 512
    for bi in range(nblk):
        t0 = bi * 512
        n = min(512, NTOK - t0)
        i2 = bi % 2
        _cp(P, "act", obf[i2][:, 0:n], oT[:, t0:t0 + n], [oTb], [obfb[i2]])
        _mm(P, ps[2 + i2][0:64, 0:n], o64b[:, :], obf[i2][:, 0:n], True, True, [obfb[i2], rc], [psb[2 + i2]])
        _tt(P, "dve", cen[i2][:, 0:n], oT[:, t0:t0 + n], ps[2 + i2][0:64, 0:n], ALU.subtract, [oTb, psb[2 + i2]], [cenb[i2]])
        _act(P, sq_[i2][:, 0:n], cen[i2][:, 0:n], AF.Square, [cenb[i2]], [sqb_[i2]])
        _mm(P, ps[6 + i2][0:64, 0:n], o64b[:, :], sq_[i2][:, 0:n], True, True, [sqb_[i2], rc], [psb[6 + i2]])
        _act(P, rs_[i2][:, 0:n], ps[6 + i2][0:64, 0:n], AF.Ln, [psb[6 + i2]], [rsb_[i2]], bias=P.eps_t[0:64, 0:1], scale=1.0)
        _act(P, rs_[i2][:, 0:n], rs_[i2][:, 0:n], AF.Exp, [rsb_[i2]], [rsb_[i2]], scale=-0.5)
        _tt(P, "dve", cen[i2][:, 0:n], cen[i2][:, 0:n], rs_[i2][:, 0:n], ALU.mult, [cenb[i2], rsb_[i2]], [cenb[i2]])
        _act(P, sg_[i2][:, 0:n], gT[:, t0:t0 + n], AF.Silu, [gb_], [sgb_[i2]])
        _stt(P, rT[:, t0:t0 + n], cen[i2][:, 0:n], gn[:, 0:1], sg_[i2][:, 0:n], ALU.mult, ALU.mult, [cenb[i2], sgb_[i2], rc], [rTb])
    outs.append(_ld(P, "sp", out[2, :, :], rT[:, :], [P.buf()], reads=[rTb]))


def s5_part(P, dr, ps, psb, sT, sTb, out, outs, need_ctx_out, cb):
    pb = P.buf("s5param")

    def cplx_prep(are, aim, ldt, mk):
        T = {k: mk() for k in ("dt", "ar", "ai", "mag", "ph", "tmp", "sn", "cs", "abr", "abi", "nr", "den", "cr", "ci", "u")}
        ident_v = lambda t: t
        _act(P, T["dt"], ldt, AF.Exp, [pb], [pb])
        _tt(P, "dve", T["ar"], are, T["dt"], ALU.mult, [pb], [pb])
        _tt(P, "dve", T["ai"], aim, T["dt"], ALU.mult, [pb], [pb])
        _act(P, T["mag"], T["ar"], AF.Exp, [pb], [pb])
        _cp(P, "dve", T["ph"], T["ai"], [pb], [pb])
        range_reduce_sincos(P, T["ph"], T["sn"], T["cs"], T["tmp"], ident_v, pb)
        _tt(P, "dve", T["abr"], T["mag"], T["cs"], ALU.mult, [pb], [pb])
        _tt(P, "dve", T["abi"], T["mag"], T["sn"], ALU.mult, [pb], [pb])
        _ts(P, "dve", T["nr"], T["abr"], -1.0, None, ALU.add, None, [pb], [pb])
        _tt(P, "dve", T["den"], are, are, ALU.mult, [pb], [pb])
        _tt(P, "dve", T["u"], aim, aim, ALU.mult, [pb], [pb])
        _tt(P, "dve", T["den"], T["den"], T["u"], ALU.add, [pb], [pb])
        P.op("dve", lambda h: h.reciprocal(out=T["den"], in_=T["den"]), reads=[pb], writes=[pb])
        _tt(P, "dve", T["cr"], T["nr"], are, ALU.mult, [pb], [pb])
        _tt(P, "dve", T["u"], T["abi"], aim, ALU.mult, [pb], [pb])
        _tt(P, "dve", T["cr"], T["cr"], T["u"], ALU.add, [pb], [pb])
        _tt(P, "dve", T["cr"], T["cr"], T["den"], ALU.mult, [pb], [pb])
        _tt(P, "dve", T["ci"], T["abi"], are, ALU.mult, [pb], [pb])
        _tt(P, "dve", T["u"], T["nr"], aim, ALU.mult, [pb], [pb])
        _tt(P, "dve", T["ci"], T["ci"], T["u"], ALU.subtract, [pb], [pb])
        _tt(P, "dve", T["ci"], T["ci"], T["den"], ALU.mult, [pb], [pb])
        return T

    p_sm = A(P, [128, 2, 2, 3], F32); p_row = A(P, [128, 2, 3, 256], F32); p_hs = A(P, [64, 2, 3, 64], F32)
    Bhs = A(P, [64, 2, 2, 64], F32); Csm = A(P, [128, 2, 2, 2, 16], F32); dvec = A(P, [64, 1], F32)
    jrow = A(P, [128, 129], F32); jcol = A(P, [128, 1], F32); njcol = A(P, [128, 1], F32)
    LT = A(P, [128, 2, 128], BF16); mrow = A(P, [64, 4], F32); msm = A(P, [128, 2, 4], F32)
    for t_, src in ((p_sm[:], dr["s_sm"][:, :, :, :]), (p_row[:], dr["s_row"][:, :, :, :]), (p_hs[:], dr["s_hs"][:, :, :, :]), (Bhs[:], dr["s_B"][:, :, :, :]),
                    (Csm[:], dr["s_C"][:, :, :, :, :]), (dvec[:], dr["s_d"][:, :]), (jrow[:], dr["s_jrow"][:, :]), (jcol[:], dr["s_jcol"][:, :]),
                    (LT[:], dr["s_LT"].rearrange("d j i -> j d i")), (mrow[:], dr["s_mrow"][:, :]), (msm[:], dr["s_msm"][:, :, :])):
        _ld(P, "sp", t_, src, [pb])
    _ts(P, "dve", njcol[:], jcol[:], -1.0, None, ALU.mult, None, [pb], [pb])
    ones_col = A(P, [128, 1], BF16)
    P.op("dve", lambda h: h.memset(ones_col[:], 1.0), writes=[pb])

    BD = [A(P, [64, 512], BF16) for _ in range(2)]
    CT = [A(P, [128, 4, 64], BF16) for _ in range(2)]
    mark_prep = P.a_cur
    for dd in range(2):
        P.a_cur = mark_prep
        Ths = cplx_prep(p_hs[:, dd, 0, :], p_hs[:, dd, 1, :], p_hs[:, dd, 2, :], lambda: A(P, [64, 64], F32)[:, :])
        bbr = A(P, [64, 64], F32); bbi = A(P, [64, 64], F32); uu = A(P, [64, 64], F32)
        _tt(P, "dve", bbr[:], Ths["cr"], Bhs[:, dd, 0, :], ALU.mult, [pb], [pb])
        _tt(P, "dve", uu[:], Ths["ci"], Bhs[:, dd, 1, :], ALU.mult, [pb], [pb])
        _tt(P, "dve", bbr[:], bbr[:], uu[:], ALU.subtract, [pb], [pb])
        _tt(P, "dve", bbi[:], Ths["cr"], Bhs[:, dd, 1, :], ALU.mult, [pb], [pb])
        _tt(P, "dve", uu[:], Ths["ci"], Bhs[:, dd, 0, :], ALU.mult, [pb], [pb])
        _tt(P, "dve", bbi[:], bbi[:], uu[:], ALU.add, [pb], [pb])
        for g in range(4):
            _ts(P, "dve", BD[dd][:, g * 64:(g + 1) * 64], bbr[:], mrow[:, g:g + 1], None, ALU.mult, None, [pb], [pb])
            _ts(P, "dve", BD[dd][:, 256 + g * 64:256 + (g + 1) * 64], bbi[:], mrow[:, g:g + 1], None, ALU.mult, None, [pb], [pb])
        for ri in range(2):
            for st in range(2):
                for g in range(4):
                    _ts(P, "dve", CT[dd][:, ri * 2 + st, g * 16:(g + 1) * 16], Csm[:, dd, st, ri, :], msm[:, st, g:g + 1], (1.0 if ri == 0 else -1.0), ALU.mult, ALU.mult, [pb], [pb])
    P.a_cur = mark_prep
    TA = [[A(P, [128, 2, 129], F32) for _ in range(2)] for _ in range(2)]
    TW = [[A(P, [128, 2, 129], F32) for _ in range(2)] for _ in range(2)]
    PRE = [[A(P, [128, 256], F32) for _ in range(2)] for _ in range(2)]
    mark_t = P.a_cur
    for dd in range(2):
        P.a_cur = mark_t
        dt_ = A(P, [128, 2], F32); ar = A(P, [128, 2], F32); ai = A(P, [128, 2], F32); nar = A(P, [128, 2], F32)
        _act(P, dt_[:], p_sm[:, dd, :, 2], AF.Exp, [pb], [pb])
        _tt(P, "dve", ar[:], p_sm[:, dd, :, 0], dt_[:], ALU.mult, [pb], [pb])
        _tt(P, "dve", ai[:], p_sm[:, dd, :, 1], dt_[:], ALU.mult, [pb], [pb])
        _ts(P, "dve", nar[:], ar[:], -1.0, None, ALU.mult, None, [pb], [pb])
        mark_st = P.a_cur
        for st in range(2):
            P.a_cur = mark_st
            ph = A(P, [128, 129], F32); tmp = A(P, [128, 129], F32); sn = A(P, [128, 129], F32); cs = A(P, [128, 129], F32)
            mp = A(P, [128, 129], F32); mn = A(P, [128, 129], F32)
            _ts(P, "dve", ph[:], jrow[:], ai[:, st:st + 1], None, ALU.mult, None, [pb], [pb])
            range_reduce_sincos(P, ph[:], sn[:], cs[:], tmp[:], (lambda t: t), pb)
            _act(P, mp[:], jrow[:], AF.Exp, [pb], [pb], scale=ar[:, st:st + 1])
            _act(P, mn[:], jrow[:], AF.Exp, [pb], [pb], scale=nar[:, st:st + 1])
            _tt(P, "dve", TA[dd][0][:, st, :], mp[:], cs[:], ALU.mult, [pb], [pb])
            _tt(P, "dve", TA[dd][1][:, st, :], mp[:], sn[:], ALU.mult, [pb], [pb])
            _tt(P, "dve", TW[dd][0][:, st, :], mn[:], cs[:], ALU.mult, [pb], [pb])
            _stt(P, TW[dd][1][:, st, :], mn[:], -1.0, sn[:], ALU.mult, ALU.mult, [pb], [pb])
        P.a_cur = mark_t
        dtr = A(P, [128, 256], F32); arr = A(P, [128, 256], F32); air = A(P, [128, 256], F32)
        ph = A(P, [128, 256], F32); tmp = A(P, [128, 256], F32); sn = A(P, [128, 256], F32); cs = A(P, [128, 256], F32); mg = A(P, [128, 256], F32)
        _act(P, dtr[:], p_row[:, dd, 2, :], AF.Exp, [pb], [pb])
        _tt(P, "dve", arr[:], p_row[:, dd, 0, :], dtr[:], ALU.mult, [pb], [pb])
        _tt(P, "dve", air[:], p_row[:, dd, 1, :], dtr[:], ALU.mult, [pb], [pb])
        _ts(P, "dve", ph[:], air[:], jcol[:, 0:1], None, ALU.mult, None, [pb], [pb])
        range_reduce_sincos(P, ph[:], sn[:], cs[:], tmp[:], (lambda t: t), pb)
        _act(P, mg[:], arr[:], AF.Exp, [pb], [pb], scale=(njcol if dd == 0 else jcol)[:, 0:1])
        _tt(P, "dve", PRE[dd][0][:], mg[:], cs[:], ALU.mult, [pb], [pb])
        _stt(P, PRE[dd][1][:], mg[:], (-1.0 if dd == 0 else 1.0), sn[:], ALU.mult, ALU.mult, [pb], [pb])
        P.a_cur = mark_t
    barrier(P)
    P.a_cur = mark_t
    Xt = A(P, [128, NCH, 512], BF16); Xtb = [P.buf() for _ in range(NCH)]
    yacc = A(P, [64, NTOK], F32); yb = P.buf("yacc")
    E = A(P, [128, 4, NCH], F32); Eb = P.buf("E")
    H = [[A(P, [128, 2, NCH], F32) for _ in range(2)] for _ in range(2)]
    Hb = P.buf("H")
    cv = [A(P, [128, 2, NCH], F32) for _ in range(2)]
    pw = A(P, [128, 2, 8], F32)
    tq = [A(P, [128, 256], F32) for _ in range(8)]; tqb = [P.buf() for _ in range(8)]
    hs_ = [A(P, [128, 4, 128], BF16) for _ in range(2)]; hsb = [P.buf(), P.buf()]
    uq = [A(P, [128, 128], F32) for _ in range(16)]; uqb = [P.buf() for _ in range(16)]
    for dd in range(2):
        pos = pos_of(dd)
        def s1_X(c):
            tau = 128 * c
            px = ps[c % 2]; pxb = psb[c % 2]
            _mm(P, px[:, 0:512], sT[:, tau:tau + 128], BD[dd][:, :], True, True, [sTb, pb], [pxb])
            i2 = (c % 2) * 4
            _tt(P, "dve", tq[i2][:, :], px[:, 0:256], PRE[dd][0][:, :], ALU.mult, [pxb, pb], [tqb[i2]])
            _tt(P, "dve", tq[i2 + 1][:, :], px[:, 256:512], PRE[dd][1][:, :], ALU.mult, [pxb, pb], [tqb[i2 + 1]])
            _tt(P, "dve", tq[i2 + 2][:, :], px[:, 0:256], PRE[dd][1][:, :], ALU.mult, [pxb, pb], [tqb[i2 + 2]])
            _tt(P, "dve", tq[i2 + 3][:, :], px[:, 256:512], PRE[dd][0][:, :], ALU.mult, [pxb, pb], [tqb[i2 + 3]])
            _tt(P, "pool", Xt[:, c, 0:256], tq[i2][:, :], tq[i2 + 1][:, :], ALU.subtract, [tqb[i2], tqb[i2 + 1]], [Xtb[c]])
            _tt(P, "pool", Xt[:, c, 256:512], tq[i2 + 2][:, :], tq[i2 + 3][:, :], ALU.add, [tqb[i2 + 2], tqb[i2 + 3]], [Xtb[c]])

        def s1_E(c):
            for tl in range(4):
                col = tl * NCH + pos[c]
                _mm(P, ps[6][:, col:col + 1], Xt[:, c, tl * 128:(tl + 1) * 128], ones_col[:, :], True, True, [Xtb[c], pb], [psb[6]])

        s1_X(0)
        for c in range(NCH):
            if c + 1 < NCH:
                s1_X(c + 1)
            s1_E(c)
        _cp(P, "dve", E[:], ps[6][:, 0:4 * NCH].rearrange("p (a b) -> p a b", b=NCH), [psb[6]], [Eb])
        H0r, H0i = H[0][0], H[0][1]
        if dd == 0:
            for st in range(2):
                a_r, a_i = TA[0][0][:, st, 127:128], TA[0][1][:, st, 127:128]
                _ts(P, "dve", uq[0][:, 0:NCH], E[:, 2 + st, :], a_i, None, ALU.mult, None, [Eb, pb], [uqb[0]])
                _stt(P, H0r[:, st, :], E[:, st, :], a_r, uq[0][:, 0:NCH], ALU.mult, ALU.subtract, [Eb, pb, uqb[0]], [Hb])
                _ts(P, "dve", uq[1][:, 0:NCH], E[:, st, :], a_i, None, ALU.mult, None, [Eb, pb], [uqb[1]])
                _stt(P, H0i[:, st, :], E[:, 2 + st, :], a_r, uq[1][:, 0:NCH], ALU.mult, ALU.add, [Eb, pb, uqb[1]], [Hb])
        else:
            _cp(P, "dve", H0r[:], E[:, 0:2, :], [Eb], [Hb])
            _cp(P, "dve", H0i[:], E[:, 2:4, :], [Eb], [Hb])
        _cp(P, "dve", pw[:, :, 0], TA[dd][0][:, :, 128], [pb], [Hb])
        _cp(P, "dve", pw[:, :, 1], TA[dd][1][:, :, 128], [pb], [Hb])
        cur = 0
        d = 1
        while d < NCH:
            _ts(P, "dve", pw[:, :, 2], pw[:, :, 1], -1.0, None, ALU.mult, None, [Hb], [Hb])
            o_, n_ = H[cur], H[1 - cur]
            for ri in range(2):
                _cp(P, "dve", n_[ri][:, :, 0:d], o_[ri][:, :, 0:d], [Hb], [Hb])
            for st in range(2):
                pr, pi, npi = pw[:, st, 0:1], pw[:, st, 1:2], pw[:, st, 2:3]
                m = NCH - d
                _stt(P, uq[0][:, 0:m], o_[0][:, st, 0:m], pr, o_[0][:, st, d:NCH], ALU.mult, ALU.add, [Hb], [uqb[0]])
                _stt(P, n_[0][:, st, d:NCH], o_[1][:, st, 0:m], npi, uq[0][:, 0:m], ALU.mult, ALU.add, [Hb, uqb[0]], [Hb])
                _stt(P, uq[1][:, 0:m], o_[1][:, st, 0:m], pr, o_[1][:, st, d:NCH], ALU.mult, ALU.add, [Hb], [uqb[1]])
                _stt(P, n_[1][:, st, d:NCH], o_[0][:, st, 0:m], pi, uq[1][:, 0:m], ALU.mult, ALU.add, [Hb, uqb[1]], [Hb])
            _tt(P, "dve", pw[:, :, 3], pw[:, :, 0], pw[:, :, 0], ALU.mult, [Hb], [Hb])
            _tt(P, "dve", pw[:, :, 4], pw[:, :, 1], pw[:, :, 1], ALU.mult, [Hb], [Hb])
            _tt(P, "dve", pw[:, :, 5], pw[:, :, 0], pw[:, :, 1], ALU.mult, [Hb], [Hb])
            _tt(P, "dve", pw[:, :, 0], pw[:, :, 3], pw[:, :, 4], ALU.subtract, [Hb], [Hb])
            _ts(P, "dve", pw[:, :, 1], pw[:, :, 5], 2.0, None, ALU.mult, None, [Hb], [Hb])
            cur = 1 - cur
            d *= 2
        Hf = H[cur]
        kidx = 1 if dd == 0 else 128
        P.op("dve", lambda h: h.memset(cv[0][:, :, 0:1], 0.0), writes=[Hb])
        P.op("dve", lambda h: h.memset(cv[1][:, :, 0:1], 0.0), writes=[Hb])
        for st in range(2):
            a_r, a_i = TA[dd][0][:, st, kidx:kidx + 1], TA[dd][1][:, st, kidx:kidx + 1]
            m = NCH - 1
            _ts(P, "dve", uq[0][:, 0:m], Hf[1][:, st, 0:m], a_i, None, ALU.mult, None, [Hb, pb], [uqb[0]])
            _stt(P, cv[0][:, st, 1:NCH], Hf[0][:, st, 0:m], a_r, uq[0][:, 0:m], ALU.mult, ALU.subtract, [Hb, pb, uqb[0]], [Hb])
            _ts(P, "dve", uq[1][:, 0:m], Hf[0][:, st, 0:m], a_i, None, ALU.mult, None, [Hb, pb], [uqb[1]])
            _stt(P, cv[1][:, st, 1:NCH], Hf[1][:, st, 0:m], a_r, uq[1][:, 0:m], ALU.mult, ALU.add, [Hb, pb, uqb[1]], [Hb])
        Tt = TA[dd] if dd == 0 else TW[dd]
        c_start = 0 if need_ctx_out else 2
        def s2_G(c):
            pg = ps[2 + c % 2]; pgb = psb[2 + c % 2]
            for tl in range(4):
                _mm(P, pg[:, tl * 128:(tl + 1) * 128], Xt[:, c, tl * 128:(tl + 1) * 128], LT[:, dd, :], True, True, [Xtb[c], pb], [pgb])
            hh, hhb = hs_[c % 2], hsb[c % 2]
            pc = pos[c]
            for st in range(2):
                gr, gi = pg[:, st * 128:(st + 1) * 128], pg[:, (2 + st) * 128:(3 + st) * 128]
                c_r, c_i = cv[0][:, st, pc:pc + 1], cv[1][:, st, pc:pc + 1]
                Tr, Ti = Tt[0][:, st, 0:128], Tt[1][:, st, 0:128]
                u0 = ((c % 2) * 2 + st) * 4
                _stt(P, uq[u0][:, :], gr, c_r, Tr, ALU.add, ALU.mult, [pgb, Hb, pb], [uqb[u0]])
                _stt(P, uq[u0 + 1][:, :], gi, c_i, Ti, ALU.add, ALU.mult, [pgb, Hb, pb], [uqb[u0 + 1]])
                _stt(P, uq[u0 + 2][:, :], gi, c_i, Tr, ALU.add, ALU.mult, [pgb, Hb, pb], [uqb[u0 + 2]])
                _stt(P, uq[u0 + 3][:, :], gr, c_r, Ti, ALU.add, ALU.mult, [pgb, Hb, pb], [uqb[u0 + 3]])
                _tt(P, "pool", hh[:, st, :], uq[u0][:, :], uq[u0 + 1][:, :], ALU.subtract, [uqb[u0], uqb[u0 + 1]], [hhb])
                _tt(P, "pool", hh[:, 2 + st, :], uq[u0 + 2][:, :], uq[u0 + 3][:, :], ALU.add, [uqb[u0 + 2], uqb[u0 + 3]], [hhb])

        def s2_Y(c):
            hh, hhb = hs_[c % 2], hsb[c % 2]
            jj = c % 4
            py = ps[4 + (c // 4) % 2]; pyb = psb[4 + (c // 4) % 2]
            for tl in range(4):
                _mm(P, py[0:64, jj * 128:(jj + 1) * 128], CT[dd][:, tl, :], hh[:, tl, :], tl == 0, tl == 3, [pb, hhb], [pyb])
            if jj == 3 or c == NCH - 1:
                b0 = (c // 4) * 512
                wid = (jj + 1) * 128
                lo = 256 if ((not need_ctx_out) and c // 4 == 0) else 0
                if dd == 0:
                    _cp(P, "act", yacc[:, b0 + lo:b0 + wid], py[0:64, lo:wid], [pyb], [yb])
                else:
                    _tt(P, "dve", yacc[:, b0 + lo:b0 + wid], yacc[:, b0 + lo:b0 + wid], py[0:64, lo:wid], ALU.add, [pyb, yb], [yb])

        s2_G(c_start)
        for c in range(c_start, NCH):
            if c + 1 < NCH:
                s2_G(c + 1)
            s2_Y(c)
    zT = A(P, [64, NTOK], BF16); zb = P.buf("zT")
    g1 = [A(P, [64, 512], F32) for _ in range(2)]; g1b = [P.buf(), P.buf()]
    g2 = [A(P, [64, 512], F32) for _ in range(2)]; g2b = [P.buf(), P.buf()]
    lo_all = 0 if need_ctx_out else 256
    if not need_ctx_out:
        P.op("pool", lambda h: h.memset(zT[:, 0:256], 0.0), writes=[zb])
    nblk = (NTOK + 511) // 512
    for bi in range(nblk):
        t0 = max(bi * 512, lo_all)
        t1_ = min((bi + 1) * 512, NTOK)
        n = t1_ - t0
        i2 = bi % 2
        y = g1[i2]; w = g2[i2]
        _stt(P, y[:, 0:n], sT[:, t0:t1_], dvec[:, 0:1], yacc[:, t0:t1_], ALU.mult, ALU.add, [sTb, pb, yb], [g1b[i2]])
        _tt(P, "dve", w[:, 0:n], y[:, 0:n], y[:, 0:n], ALU.mult, [g1b[i2]], [g2b[i2]])
        _ts(P, "dve", w[:, 0:n], w[:, 0:n], 0.044715, 1.0, ALU.mult, ALU.add, [g2b[i2]], [g2b[i2]])
        _tt(P, "dve", w[:, 0:n], w[:, 0:n], y[:, 0:n], ALU.mult, [g2b[i2], g1b[i2]], [g2b[i2]])
        _act(P, w[:, 0:n], w[:, 0:n], AF.Sigmoid, [g2b[i2]], [g2b[i2]], scale=1.5957691216057308)
        _tt(P, "dve", zT[:, t0:t1_], w[:, 0:n], y[:, 0:n], ALU.mult, [g2b[i2], g1b[i2]], [zb])
    outs.append(_ld(P, "sp", out[1, :, :], zT[:, :], [P.buf()], reads=[zb]))


import ml_dtypes

BF = ml_dtypes.bfloat16
NTOK = 8448
f32 = np.float32


def fm(a):
    return np.ascontiguousarray(a.T.reshape(8, 128, a.shape[0]))


def prep_common(inp, layer, b):
    cond = np.stack([inp['c'][b], inp['c_ctx']], 0)
    condT = np.ascontiguousarray(cond.reshape(2, 8, 128).transpose(2, 1, 0))
    bm = inp['b_mod'][layer].reshape(48, 128).T
    b_modT = np.ascontiguousarray(np.stack([bm, bm], -1))
    ng = inp['norm_g'][layer].reshape(4, 8, 128).transpose(2, 0, 1)
    norm_gT = np.ascontiguousarray(np.stack([ng, ng], -1))
    return dict(condT=condT, b_modT=b_modT, norm_gT=norm_gT, w_mod=inp['w_mod'][layer])


_const_cache = {}


def consts():
    if _const_cache:
        return _const_cache
    C = _const_cache
    c = np.arange(64)
    ang = 2 * np.pi * (np.outer(c, c) % 64) / 64
    C['f_CS'] = np.concatenate([np.cos(ang), -np.sin(ang)], 1).astype(BF)
    ca, sa = np.cos(ang), np.sin(ang)
    C['f_RP'] = np.concatenate([ca, -sa], 1).astype(BF)
    C['f_RQ'] = np.concatenate([sa, ca], 1).astype(BF)
    m2 = np.arange(128)[:, None, None]
    n1 = np.arange(64)[None, :, None]
    n2 = np.arange(128)[None, None, :]
    be = 2 * np.pi * ((m2 * (n1 + 64 * n2)) % 8192) / 8192
    nrm = 1 / np.sqrt(64 * 8192)
    C['f_CB'] = (np.cos(be) * nrm).astype(BF)
    C['f_SB'] = (np.sin(be) * nrm).astype(BF)
    m = np.arange(256)
    a256 = 2 * np.pi * (np.outer(m, m) % 256) / 256
    nrm2 = 1 / np.sqrt(64 * 256)
    C['f_C256'] = np.ascontiguousarray((np.cos(a256) * nrm2).reshape(2, 128, 256).transpose(1, 0, 2)).astype(BF)
    C['f_S256'] = np.ascontiguousarray((np.sin(a256) * nrm2).reshape(2, 128, 256).transpose(1, 0, 2)).astype(BF)
    t = np.arange(8192)
    row = (t // 64).astype(f32)
    col = (t % 64).astype(f32)
    inv = (1.0 / (f32(10000.0) ** (np.arange(16, dtype=f32) / f32(16)))).astype(f32)
    angr = np.concatenate([row[:, None] * inv, col[:, None] * inv], -1).astype(f32)
    cs, sn = np.cos(angr).astype(f32), np.sin(angr).astype(f32)
    cos64 = np.concatenate([cs, cs], 1)
    sin64 = np.concatenate([-sn, sn], 1)
    C['r_cosF'] = np.ascontiguousarray(cos64.T)
    C['r_sinF'] = np.ascontiguousarray(sin64.T)
    C['r_cosT'] = np.ascontiguousarray(cos64.reshape(64, 128, 64).transpose(1, 0, 2))
    C['r_sinT'] = np.ascontiguousarray(sin64.reshape(64, 128, 64).transpose(1, 0, 2))
    j = np.arange(128, dtype=f32)
    C['r_jcol'] = np.stack([127 - j, j], 1).astype(f32)
    ii = np.arange(128)
    dist = np.abs(ii[None, :] - ii[:, None]).astype(f32)
    C['r_dist'] = dist
    C['r_mask'] = np.stack([(ii[None, :] >= ii[:, None]), (ii[:, None] >= ii[None, :])], 0).astype(f32)
    C['r_irow'] = np.stack([np.tile(j + 1, (64, 1)), np.tile(128 - j, (64, 1))], 0).astype(f32)
    C['s_jrow'] = np.tile(np.arange(129, dtype=f32), (128, 1))
    C['s_jcol'] = np.arange(128, dtype=f32)[:, None].copy()
    C['s_LT'] = np.stack([(ii[None, :] >= ii[:, None]), (ii[:, None] >= ii[None, :])], 0).astype(BF)
    g_of_row = np.arange(64) // 16
    C['s_mrow'] = (g_of_row[:, None] == np.arange(4)[None, :]).astype(f32)
    g_of_st = (np.arange(128)[:, None] // 64) + 2 * np.arange(2)[None, :]
    C['s_msm'] = (g_of_st[:, :, None] == np.arange(4)[None, None, :]).astype(f32)
    C['ident_bf'] = np.eye(128).astype(BF)
    C['ident_f'] = np.eye(128).astype(f32)
    def start(r):
        return int(np.clip(r - 4, 0, 120))
    qc = np.arange(64)
    cst = np.clip(qc - 8, 0, 48)
    kc = np.arange(64)
    colok = (kc[None, :] >= cst[:, None]) & (kc[None, :] < cst[:, None] + 16)
    types = [(0, 0, 8), (2, 0, 8), (10, 6, 9), (124, 120, 8), (126, 120, 8)]
    mask = np.full((5, 128, 832), -30000.0, f32)
    drs = np.zeros((5, 2, 9), np.int64)
    for ti, (r0, R0, nr) in enumerate(types):
        for qr in range(2):
            r = r0 + qr
            for i in range(9):
                kr = R0 + i
                dr = int(np.clip(kr - r + 7, 0, 14))
                drs[ti, qr, i] = dr
                if i < nr and start(r) <= kr < start(r) + 8:
                    blk = np.where(colok, 0.0, -30000.0)
                    mask[ti, qr * 64:(qr + 1) * 64, i * 64:(i + 1) * 64] = blk
        mask[ti, :, 576:] = 0.0
    C['n_mask'] = mask
    C['n_drs'] = drs
    C['n_types'] = types
    return C


def prep_M(inp, layer, c, xT_full):
    b, q = c // 4, c % 4
    C = consts()
    m = prep_common(inp, layer, b)
    m['xT'] = xT_full
    w_in = inp['w_in'][layer]
    o = q * 64
    sw = np.r_[32:64, 0:32]
    cols_fm = np.concatenate([np.arange(0 + o, 0 + o + 64), np.arange(256 + o, 256 + o + 64), np.arange(512 + o, 512 + o + 64),
                              np.arange(768 + o, 768 + o + 64), 512 + o + sw, 768 + o + sw, np.arange(1280 + o, 1280 + o + 64),
                              np.arange(1536 + o, 1536 + o + 64), np.arange(1792 + o, 1792 + o + 64)])
    cols_tm = np.concatenate([np.arange(768 + o, 768 + o + 64), 768 + o + sw, np.arange(1024 + o, 1024 + o + 64), np.arange(2048 + o, 2048 + o + 64)])
    m['w_fm'] = np.ascontiguousarray(w_in[:, cols_fm])
    m['w_tm'] = np.ascontiguousarray(w_in[:, cols_tm])
    for k in ('f_CS', 'f_RP', 'f_RQ', 'f_CB', 'f_SB', 'f_C256', 'f_S256', 'r_cosF', 'r_sinF', 'r_cosT', 'r_sinT', 'r_jcol', 'r_dist', 'r_mask', 'r_irow',
              's_jrow', 's_jcol', 's_LT', 's_mrow', 's_msm', 'ident_bf', 'ident_f', 'n_mask'):
        m[k] = C[k]
    gs = slice(4 * q, 4 * q + 4)
    L = layer
    are, aim = inp['s5_a_re'][L][:, gs], inp['s5_a_im'][L][:, gs]
    ldt = inp['s5_log_dt'][L][:, gs]
    def sm(a):
        return np.ascontiguousarray(a.reshape(2, 2, 128).transpose(2, 0, 1))
    ldt_b = np.broadcast_to(ldt[:, :, None], (2, 4, 64))
    m['s_sm'] = np.ascontiguousarray(np.stack([sm(are), sm(aim), sm(ldt_b)], -1))
    row = np.stack([are.reshape(2, 256), aim.reshape(2, 256), ldt_b.reshape(2, 256)], -1)
    m['s_row'] = np.ascontiguousarray(np.broadcast_to(row[None].transpose(0, 1, 3, 2), (128, 2, 3, 256)))
    hs = np.stack([are, aim, ldt_b], 2)
    hs = np.broadcast_to(hs[:, :, None], (2, 4, 16, 3, 64))
    m['s_hs'] = np.ascontiguousarray(hs.transpose(1, 2, 0, 3, 4).reshape(64, 2, 3, 64))
    bre, bim = inp['s5_b_re'][L][:, gs], inp['s5_b_im'][L][:, gs]
    B = np.stack([bre, bim], 2)
    m['s_B'] = np.ascontiguousarray(B.transpose(1, 4, 0, 2, 3).reshape(64, 2, 2, 64))
    cre, cim = inp['s5_c_re'][L][:, gs], inp['s5_c_im'][L][:, gs]
    Cc = np.stack([cre, cim], 2)
    Cc = Cc.transpose(1, 4, 0, 2, 3)
    Cc = Cc.reshape(2, 2, 64, 2, 2, 16).transpose(1, 2, 3, 0, 4, 5).reshape(128, 2, 2, 2, 16)
    m['s_C'] = np.ascontiguousarray(Cc)
    m['s_d'] = np.ascontiguousarray(inp['s5_d'][L][256 * 0 + 64 * q:64 * q + 64][:, None])
    rd = inp['ret_decay'][L][:, q]
    m['r_dec'] = np.ascontiguousarray(np.broadcast_to(rd[None, :], (128, 2))).astype(f32)
    m['r_gn'] = np.ascontiguousarray(inp['ret_gn'][L][64 * q:64 * q + 64][:, None])
    rpb = inp['na_rpb'][L][q]
    dc = np.clip(np.arange(64)[None, :] - np.arange(64)[:, None], -15, 15) + 15
    m['n_toep'] = np.ascontiguousarray(rpb[:, dc])
    return m


def _prep_F(inp, layer, c, xa, bra_bf, moe_a=False):
    b = c // 4
    m = prep_common(inp, layer, b)
    m.update(xT=fm(xa), brT=bra_bf, w_in=inp['w_in'][layer], w_br=inp['w_branch'][layer].reshape(1024, 1024), w_o=inp['w_out'][layer],
             w_glu=inp['s5_w_glu'][layer], b_gluT=np.ascontiguousarray(inp['s5_b_glu'][layer].reshape(2, 128).T))
    i = layer // 2
    if layer % 2 == 0:
        m.update(w_g=inp['ffn_w_gate'][i:i + 1], w_u=inp['ffn_w_up'][i:i + 1], w_d=inp['ffn_w_down'][i:i + 1])
    else:
        sel = np.zeros((8, 8, 128), np.float32)
        for e in range(8):
            sel[e, e, :] = 1
        m.update(w_r=inp['moe_w_router'][i], b_r=np.ascontiguousarray(np.broadcast_to(inp['moe_b_router'][i][None], (128, 8))),
                 ident=np.eye(128, dtype=np.float32), sel=sel)
    return m


def kernel(**inputs):
    inp = {k: np.asarray(v) for k, v in inputs.items()}
    NCORE = 8
    cores = list(range(NCORE))
    x = inp['x']
    ctx = inp['ctx']
    for layer in range(2):
        last = (layer == 1)
        ncM = build_M(not last)
        xfull = [fm(np.concatenate([ctx[b], x[b]], 0)) for b in range(2)]
        maps = [prep_M(inp, layer, c, xfull[c // 4]) for c in cores]
        resM = run_bass_kernel_spmd(ncM, maps, core_ids=cores).results
        del maps
        br_full = []
        for b in range(2):
            o = np.stack([np.asarray(resM[4 * b + q]['brT_out']) for q in range(4)], 1)
            br_full.append(o.reshape(1024, NTOK))
        del resM
        if not last:
            blocksA = [(i * 256, 256, 0) for i in range(8)] + [(2048, 64, 1)]
            blocksB = [(i * 512, 512, 0) for i in range(4)] + [(2048, 64, 1)]
            ncF = build_F(blocksA, blocksB, 1, 2816, False)
        else:
            blocksA = [(i * 256, 256, 0) for i in range(8)]
            blocksB = [(i * 512, 512, 0) for i in range(4)]
            ncF = build_F(blocksA, blocksB, 8, 3584, True, mode='moe_a')
        maps = []
        for c in cores:
            b, q = c // 4, c % 4
            lat = slice(256 + q * 2048, 256 + (q + 1) * 2048)
            if not last:
                xa = np.concatenate([x[b, q * 2048:(q + 1) * 2048], ctx[b, q * 64:(q + 1) * 64]], 0)
                bra = np.concatenate([br_full[b][:, lat], br_full[b][:, q * 64:(q + 1) * 64]], 1)
            else:
                xa = x[b, q * 2048:(q + 1) * 2048]
                bra = br_full[b][:, lat]
            bra = np.ascontiguousarray(bra.reshape(8, 128, bra.shape[1]))
            maps.append(_prep_F(inp, layer, c, xa, bra))
        resF = run_bass_kernel_spmd(ncF, maps, core_ids=cores).results
        del maps
        if not last:
            xn = np.empty_like(x)
            cn = np.empty_like(ctx)
            for c in cores:
                b, q = c // 4, c % 4
                o = np.asarray(resF[c]['xo']).reshape(1024, -1).T
                xn[b, q * 2048:(q + 1) * 2048] = o[:2048]
                cn[b, q * 64:(q + 1) * 64] = o[2048:]
            x, ctx = xn, cn
            continue
        i = layer // 2
        h2_all = np.concatenate([np.asarray(resF[c]['h2o']) for c in cores], 2)
        cb_all = np.concatenate([np.asarray(resF[c]['cbo']) for c in cores], 1)
        sel = np.concatenate([np.asarray(resF[c]['mko']) for c in cores], 1).astype(bool)
        idx = [np.flatnonzero(sel[e]) for e in cores]
        nb = max(1, -(-max(len(t) for t in idx) // 512))
        ng = -(-nb // 4)
        groups = tuple(nb // ng + (1 if g < nb % ng else 0) for g in range(ng))
        C = 512 * nb
        ncE = build_E(groups)
        maps = []
        for e in cores:
            n_e = len(idx[e])
            ii = np.zeros(C, np.int64)
            ii[:n_e] = idx[e]
            cbe = np.zeros(C, cb_all.dtype)
            cbe[:n_e] = cb_all[e, idx[e]]
            maps.append(dict(h2=np.ascontiguousarray(h2_all[:, :, ii]), cbe=np.ascontiguousarray(np.broadcast_to(cbe[None, :], (128, C))),
                             w_g=inp['moe_w_gate'][i][e], w_u=inp['moe_w_up'][i][e], w_d=inp['moe_w_down'][i][e]))
        resE = run_bass_kernel_spmd(ncE, maps, core_ids=cores).results
        del maps
        slot = np.cumsum(sel, axis=0) - sel
        nslot = max(1, int(sel.sum(0).max()))
        yp_all = np.zeros((nslot, 8, 128, sel.shape[1]), np.float32)
        for e in cores:
            ye = np.asarray(resE[e]['ye'])
            t = idx[e]
            sv = slot[e, t]
            for k in range(nslot):
                mk = sv == k
                yp_all[k][:, :, t[mk]] = ye[:, :, np.flatnonzero(mk)]
        ncC = build_Fc(nexp=nslot)
        maps = []
        for c in cores:
            m = prep_common(inp, layer, c // 4)
            m['xm'] = np.asarray(resF[c]['xo'])
            m['yp'] = np.ascontiguousarray(yp_all[:, :, :, c * 2048:(c + 1) * 2048])
            maps.append(m)
        resC = run_bass_kernel_spmd(ncC, maps, core_ids=cores).results
        xn = np.empty_like(x)
        for c in cores:
            b, q = c // 4, c % 4
            xn[b, q * 2048:(q + 1) * 2048] = np.asarray(resC[c]['xo']).reshape(1024, -1).T
        x = xn
    return x.astype(np.float32)
```

```python
import numpy as np
from contextlib import ExitStack
import concourse.bass as bass
import concourse.mybir as mybir
from concourse.bass_utils import run_bass_kernel_spmd

F32 = mybir.dt.float32
BF16 = mybir.dt.bfloat16
I32 = mybir.dt.int32
ALU = mybir.AluOpType
AF = mybir.ActivationFunctionType
AX = mybir.AxisListType

ENGS = ("pe", "act", "dve", "pool", "sp")
NDSEM = 12


class Buf:
    __slots__ = ("name", "lw", "rd", "psum")

    def __init__(self, name="", psum=False):
        self.name = name
        self.psum = psum
        self.lw = None
        self.rd = {}


class Prog:
    def __init__(self, nc):
        self.nc = nc
        self.stack = ExitStack()
        self.ops = {e: [] for e in ENGS}
        self.cnt = {e: 0 for e in ENGS}
        self.seen = {e: {} for e in ENGS}
        self.sems = {}
        for e in ENGS:
            self.sems[e] = self.stack.enter_context(nc.semaphore("s_" + e))
        self.dsem_use = {}
        self.dq_next = {}
        for q in ("sp", "act", "pool"):
            for i in range(NDSEM):
                k = "d_%s%d" % (q, i)
                self.sems[k] = self.stack.enter_context(nc.semaphore(k))
                self.dsem_use[k] = 0
            self.dq_next[q] = 0
        self.nbuf = 0

    def sb(self, name, shape, dt):
        return self.stack.enter_context(self.nc.sbuf_tensor(name, list(shape), dt))

    def ps(self, name, shape, dt=F32):
        return self.stack.enter_context(self.nc.psum_tensor(name, list(shape), dt))

    def buf(self, name=None, psum=None):
        self.nbuf += 1
        name = name or "b%d" % self.nbuf
        if psum is None:
            psum = name.startswith("ps")
        return Buf(name, psum)

    def _deps(self, eng, reads, writes, is_dma):
        w = {}

        def add(t):
            if t is None:
                return
            k, v = t
            if w.get(k, 0) < v:
                w[k] = v
        for b in reads:
            add(b.lw)
            if b.psum:
                for k, v in b.rd.items():
                    if k != eng:
                        add((k, v))
        for b in writes:
            if b.lw is not None:
                if not (eng == "pe" and b.lw[0] == "pe" and not is_dma):
                    add(b.lw)
            for k, v in b.rd.items():
                add((k, v))
        seen = self.seen[eng]
        out = []
        for k, v in w.items():
            if seen.get(k, 0) < v:
                seen[k] = v
                out.append((k, v))
        return out

    def _commit(self, ticket, reads, writes):
        for b in writes:
            b.lw = ticket
            b.rd = {}
        for b in reads:
            k, v = ticket
            if b.rd.get(k, 0) < v:
                b.rd[k] = v

    def op(self, eng, fn, reads=(), writes=()):
        waits = self._deps(eng, reads, writes, False)
        self.cnt[eng] += 1
        ticket = (eng, self.cnt[eng])
        self.ops[eng].append((waits, fn, (eng, 1)))
        self._commit(ticket, reads, writes)
        return ticket

    def dma(self, q, fn, reads=(), writes=()):
        i = self.dq_next[q]
        self.dq_next[q] = (i + 1) % NDSEM
        k = "d_%s%d" % (q, i)
        waits = self._deps(q, reads, writes, True)
        prev = self.dsem_use[k]
        if prev > 0 and self.seen[q].get(k, 0) < 16 * prev:
            self.seen[q][k] = 16 * prev
            waits.append((k, 16 * prev))
        self.dsem_use[k] = prev + 1
        ticket = (k, 16 * (prev + 1))
        self.ops[q].append((waits, fn, (k, 16)))
        self._commit(ticket, reads, writes)
        return ticket

    def finish_wait(self, eng, tickets):
        waits = []
        for k, v in tickets:
            if self.seen[eng].get(k, 0) < v:
                self.seen[eng][k] = v
                waits.append((k, v))
        self.ops[eng].append((waits, None, None))

    def emit(self):
        nc = self.nc
        sems = self.sems
        ops = self.ops

        def replay(e, h):
            for waits, fn, inc in ops[e]:
                for k, v in waits:
                    h.wait_ge(sems[k], v)
                if fn is not None:
                    ins = fn(h)
                    ins.then_inc(sems[inc[0]], inc[1])

        with nc.Block() as block:
            @block.sync
            def _(h):
                replay("sp", h)

            @block.scalar
            def _(h):
                replay("act", h)

            @block.vector
            def _(h):
                replay("dve", h)

            @block.gpsimd
            def _(h):
                replay("pool", h)

            @block.tensor
            def _(h):
                replay("pe", h)
        self.stack.close()


def _mm(P, out, lhsT, rhs, start, stop, reads, writes):
    return P.op("pe", lambda h: h.matmul(out, lhsT=lhsT, rhs=rhs, start=start, stop=stop), reads=reads, writes=writes)


def _tr(P, out, in_, ident, reads, writes):
    return P.op("pe", lambda h: h.transpose(out, in_, ident), reads=reads, writes=writes)


def _act(P, out, in_, func, reads, writes, scale=None, bias=None):
    kw = {}
    if scale is not None:
        kw["scale"] = scale
    if bias is not None:
        kw["bias"] = bias
    return P.op("act", lambda h: h.activation(out=out, in_=in_, func=func, **kw), reads=reads, writes=writes)


def _tt(P, eng, out, in0, in1, op, reads, writes):
    return P.op(eng, lambda h: h.tensor_tensor(out=out, in0=in0, in1=in1, op=op), reads=reads, writes=writes)


def _ts(P, eng, out, in0, s1, s2, op0, op1, reads, writes):
    if op1 is None:
        return P.op(eng, lambda h: h.tensor_scalar(out=out, in0=in0, scalar1=s1, scalar2=None, op0=op0), reads=reads, writes=writes)
    return P.op(eng, lambda h: h.tensor_scalar(out=out, in0=in0, scalar1=s1, scalar2=s2, op0=op0, op1=op1), reads=reads, writes=writes)


def _stt(P, out, in0, scalar, in1, op0, op1, reads, writes):
    return P.op("dve", lambda h: h.scalar_tensor_tensor(out=out, in0=in0, scalar=scalar, in1=in1, op0=op0, op1=op1), reads=reads, writes=writes)


def _cp(P, eng, out, in_, reads, writes):
    if eng == "act":
        return P.op("act", lambda h: h.activation(out=out, in_=in_, func=AF.Copy), reads=reads, writes=writes)
    return P.op(eng, lambda h: h.tensor_copy(out=out, in_=in_), reads=reads, writes=writes)


def _ld(P, q, out, in_, writes, reads=()):
    return P.dma(q, lambda h: h.dma_start(out=out, in_=in_), reads=reads, writes=writes)


D = 1024
KC = 8
EPS = 1e-6


def arena_init(P, nbytes=206 * 1024):
    lo, hi = P.nc.bump_sbuf(nbytes)
    P.a_lo, P.a_hi, P.a_cur = lo, hi, lo
    P.a_n = 0


def A(P, shape, dt):
    nb = int(np.prod(shape[1:])) * (4 if dt in (F32, I32) else 2)
    off = (P.a_cur + 31) // 32 * 32
    assert off + nb <= P.a_hi, ("SBUF arena overflow", off + nb - P.a_lo)
    P.a_cur = off + nb
    P.a_n += 1
    return P.nc.alloc_sbuf_tensor_at("t%d" % P.a_n, list(shape), dt, offset=off)


def barrier(P, queues=None):
    tick = [(e, P.cnt[e]) for e in ENGS if P.cnt[e] > 0]
    tick += [(k, 16 * v) for k, v in P.dsem_use.items() if v > 0 and (queues is None or any(k.startswith("d_" + q) for q in queues))]
    for e in ENGS:
        P.finish_wait(e, tick)


def rms_rstd(P, src, srcb, n, sq, sqb, ss_ps, ssb, rstd, rstdb, ones):
    P.op("act", lambda h: h.activation(out=sq[:, :, 0:n], in_=src[:, :, 0:n], func=AF.Square), reads=[srcb], writes=[sqb])
    for k in range(KC):
        P.op("pe", lambda h, k=k: h.matmul(ss_ps[:, 0:n], lhsT=ones[:], rhs=sq[:, k, 0:n], start=(k == 0), stop=(k == KC - 1)),
             reads=[sqb], writes=[ssb])
    P.op("act", lambda h: h.activation(out=rstd[:, 0:n], in_=ss_ps[:, 0:n], func=AF.Ln, scale=1.0 / D, bias=P.eps_t[:, 0:1]), reads=[ssb], writes=[rstdb])
    P.op("act", lambda h: h.activation(out=rstd[:, 0:n], in_=rstd[:, 0:n], func=AF.Exp, scale=-0.5), reads=[rstdb], writes=[rstdb])


def norm_mod(P, src, srcb, n, rstd, rstdb, gm, sh, r, dst, dstb, tmp, tmpb, dst_off=0, dst32=None, dst32b=None):
    for k in range(KC):
        tb = tmpb[k % len(tmp)]
        tt = tmp[k % len(tmp)]
        P.op("dve", lambda h, k=k, tt=tt: h.tensor_tensor(out=tt[:, 0:n], in0=src[:, k, 0:n], in1=rstd[:, 0:n], op=ALU.mult),
             reads=[srcb, rstdb], writes=[tb])
        P.op("act", lambda h, k=k, tt=tt: h.activation(out=dst[:, k, dst_off:dst_off + n], in_=tt[:, 0:n], func=AF.Identity,
                                                     scale=gm[:, k, r:r + 1], bias=sh[:, k, r:r + 1]),
             reads=[tb, P.modb], writes=[dstb])
        if dst32 is not None:
            P.op("pool", lambda h, k=k, tt=tt: h.tensor_scalar(out=dst32[:, k, 0:n], in0=tt[:, 0:n], scalar1=gm[:, k, r:r + 1],
                                                             scalar2=sh[:, k, r:r + 1], op0=ALU.mult, op1=ALU.add),
                 reads=[tb, P.modb], writes=[dst32b])


def compute_mod(P, dr, which, mod_ps, modpb, light=False):
    nc = P.nc
    cs = A(P, [128, KC, 2], F32)
    csb = P.buf()
    P.dma("sp", lambda h: h.dma_start(out=cs[:], in_=dr["condT"][:, :, :]), writes=[csb])
    sig = A(P, [128, KC, 2], F32)
    P.op("act", lambda h: h.activation(out=sig[:], in_=cs[:], func=AF.Sigmoid), reads=[csb], writes=[csb])
    P.op("dve", lambda h: h.tensor_tensor(out=cs[:], in0=cs[:], in1=sig[:], op=ALU.mult), reads=[csb], writes=[csb])
    modT = A(P, [128, 48, 2], F32)
    P.modT = modT
    P.modb = P.buf("mod")
    bm = A(P, [128, 48, 2], F32)
    bmb = P.buf()
    P.dma("sp", lambda h: h.dma_start(out=bm[:], in_=dr["b_modT"][:, :, :]), writes=[bmb])
    ng = A(P, [128, 4, KC, 2], F32)
    P.ng = ng
    P.dma("sp", lambda h: h.dma_start(out=ng[:], in_=dr["norm_gT"][:, :, :, :]), writes=[P.modb])
    mark = P.a_cur
    wm = [A(P, [128, KC, 1024], F32) for _ in range(2)]
    wmb = [P.buf(), P.buf()]
    wsrc = dr["w_mod"].rearrange("(k p) f -> p k f", p=128)
    for i, j in enumerate(which):
        w = wm[i % 2]
        wb = wmb[i % 2]
        for k2 in range(2):
            P.dma("sp", lambda h, w=w, j=j, k2=k2: h.dma_start(out=w[:, 4 * k2:4 * k2 + 4, :], in_=wsrc[:, 4 * k2:4 * k2 + 4, j * 1024:(j + 1) * 1024]), writes=[wb])
        for fc in range(8):
            for k in range(KC):
                P.op("pe", lambda h, w=w, j=j, fc=fc, k=k: h.matmul(mod_ps[:, j * 8 + fc, :], lhsT=w[:, k, fc * 128:(fc + 1) * 128], rhs=cs[:, k, :],
                                                                   start=(k == 0), stop=(k == KC - 1)), reads=[wb, csb], writes=[modpb])
    for j in which:
        P.op("dve", lambda h, j=j: h.tensor_tensor(out=modT[:, j * 8:(j + 1) * 8, :], in0=mod_ps[:, j * 8:(j + 1) * 8, :], in1=bm[:, j * 8:(j + 1) * 8, :], op=ALU.add),
             reads=[modpb, bmb], writes=[P.modb])
    barrier(P, ("sp",) if light else None)
    P.a_cur = mark


def mod_derived(P, jsc, jg, gi_norm, gi_gate):
    gm = A(P, [128, KC, 2], F32)
    gg = A(P, [128, KC, 2], F32)
    modT, ng = P.modT, P.ng
    P.op("dve", lambda h: h.scalar_tensor_tensor(out=gm[:], in0=modT[:, jsc * 8:(jsc + 1) * 8, :], scalar=1.0, in1=ng[:, gi_norm, :, :],
                                                 op0=ALU.add, op1=ALU.mult), reads=[P.modb], writes=[P.modb])
    if jg is not None:
        P.op("dve", lambda h: h.tensor_tensor(out=gg[:], in0=modT[:, jg * 8:(jg + 1) * 8, :], in1=ng[:, gi_gate, :, :], op=ALU.mult),
             reads=[P.modb], writes=[P.modb])
    return gm, gg


def build_F(blocks, blocksB, n_exp, dff, moe, DBG=False, mode='full'):
    TT = sum(b[1] for b in blocks)
    nc = bass.Bass("TRN2", target_bir_lowering=False)
    dr = {}

    def din(name, shape, dt=F32):
        dr[name] = nc.dram_tensor(name, list(shape), dt, kind="ExternalInput").ap()
    din("xT", [KC, 128, TT])
    din("brT", [KC, 128, TT], BF16)
    din("condT", [128, KC, 2])
    din("w_mod", [D, 6 * D])
    din("b_modT", [128, 48, 2])
    din("norm_gT", [128, 4, KC, 2])
    din("w_in", [D, 6400])
    din("w_br", [KC * 128, D])
    din("w_o", [D, D])
    din("w_glu", [256, 256])
    din("b_gluT", [128, 2])
    if mode == 'full':
        din("w_g", [n_exp, D, dff])
        din("w_u", [n_exp, D, dff])
        din("w_d", [n_exp, dff, D])
    if moe:
        din("w_r", [D, 8])
        din("b_r", [128, 8])
        din("ident", [128, 128])
        din("sel", [8, 8, 128])
    if mode == 'moe_a':
        h2o = nc.dram_tensor("h2o", [KC, 128, TT], BF16, kind="ExternalOutput").ap().rearrange("k p t -> p k t")
        cbo = nc.dram_tensor("cbo", [8, TT], BF16, kind="ExternalOutput").ap()
        mko = nc.dram_tensor("mko", [8, TT], BF16, kind="ExternalOutput").ap()
    xo = nc.dram_tensor("xo", [KC, 128, TT], F32, kind="ExternalOutput").ap()
    xoT = xo.rearrange("k p t -> p k t")
    if DBG: dbg_mod = nc.dram_tensor("dbg_mod", [128, 48, 2], F32, kind="ExternalOutput").ap()
    if DBG: dbg_xm = nc.dram_tensor("dbg_xm", [KC, 128, TT], F32, kind="ExternalOutput").ap().rearrange("k p t -> p k t")
    if DBG: dbg_h = nc.dram_tensor("dbg_h", [KC, 128, TT], BF16, kind="ExternalOutput").ap().rearrange("k p t -> p k t")
    if DBG: dbg_z = nc.dram_tensor("dbg_z", [KC, 128, TT], F32, kind="ExternalOutput").ap().rearrange("k p t -> p k t")
    if DBG: dbg_r = nc.dram_tensor("dbg_r", [128, TT], F32, kind="ExternalOutput").ap()
    if DBG: dbg_sq = nc.dram_tensor("dbg_sq", [KC, 128, TT], BF16, kind="ExternalOutput").ap().rearrange("k p t -> p k t")
    if DBG: dbg_ss = nc.dram_tensor("dbg_ss", [128, TT], F32, kind="ExternalOutput").ap()
    sscp = A(P, [128, 256], F32) if False else None
    if DBG: dbg_y = nc.dram_tensor("dbg_y", [KC, 128, TT], BF16, kind="ExternalOutput").ap().rearrange("k p t -> p k t")
    xT = dr["xT"].rearrange("k p t -> p k t")
    brT = dr["brT"].rearrange("k p t -> p k t")

    P = Prog(nc)
    arena_init(P)
    ps = [P.ps("ps%d" % i, [128, 512], F32) for i in range(8)]
    psb = [P.buf("ps%d" % i) for i in range(8)]
    ones = A(P, [128, 128], BF16)
    onesb = P.buf()
    P.op("dve", lambda h: h.memset(ones[:], 1.0), writes=[onesb])
    P.eps_t = A(P, [128, 1], F32)
    P.op("dve", lambda h: h.memset(P.eps_t[:], EPS), writes=[onesb])

    w_lo = P.a_cur
    wgt = A(P, [128, KC, 4096], BF16)
    wbr = A(P, [128, KC, D], BF16)
    wo = A(P, [128, KC, D], BF16)
    wAb = P.buf("wA")
    wbufs = []

    def _wb():
        wbufs.append(P.buf())
        return wbufs[-1]
    w_in_v = dr["w_in"].rearrange("(k p) c -> p k c", p=128)
    for k in range(KC):
        for c4 in range(2):
            P.dma("pool", lambda h, k=k, c4=c4: h.dma_start(out=wgt[:, k, c4 * 2048:(c4 + 1) * 2048], in_=w_in_v[:, k, 2304 + c4 * 2048:2304 + (c4 + 1) * 2048]), writes=[_wb()])
    P.dma("pool", lambda h: h.dma_start(out=wbr[:, 0:4, :], in_=dr["w_br"].rearrange("(k p) c -> p k c", p=128)[:, 0:4, :]), writes=[_wb()])
    P.dma("pool", lambda h: h.dma_start(out=wbr[:, 4:8, :], in_=dr["w_br"].rearrange("(k p) c -> p k c", p=128)[:, 4:8, :]), writes=[_wb()])
    P.dma("pool", lambda h: h.dma_start(out=wo[:, 0:4, :], in_=dr["w_o"].rearrange("(k p) c -> p k c", p=128)[:, 0:4, :]), writes=[_wb()])
    P.dma("pool", lambda h: h.dma_start(out=wo[:, 4:8, :], in_=dr["w_o"].rearrange("(k p) c -> p k c", p=128)[:, 4:8, :]), writes=[_wb()])

    wglu = A(P, [128, 2, 256], BF16)
    bglu = A(P, [128, 2], F32)
    P.dma("pool", lambda h: h.dma_start(out=wglu[:], in_=dr["w_glu"].rearrange("(k p) c -> p k c", p=128)), writes=[_wb()])
    P.dma("sp", lambda h: h.dma_start(out=bglu[:], in_=dr["b_gluT"][:, :]), writes=[_wb()])
    w_hi = P.a_cur
    mod_ps = nc.alloc_psum_tensor
    mod_view = ps[7][:, 0:96].rearrange("p (j r) -> p j r", r=2)
    compute_mod(P, dr, [0, 1, 2, 3, 4, 5], mod_view, psb[7], light=True)
    gm_a, gg_a = mod_derived(P, 1, 2, 0, 1)
    gm_f, gg_f = mod_derived(P, 4, 5, 2, 3)
    sh_a = P.modT[:, 0:8, :]
    sh_f = P.modT[:, 24:32, :]
    P.dbgt = []
    if DBG: P.dbgt += [P.dma("sp", lambda h: h.dma_start(out=dbg_mod[:, :, :], in_=P.modT[:]), reads=[P.modb], writes=[P.buf()])]

    h2 = A(P, [128, KC, TT], BF16)
    h2b = P.buf("h2")
    if moe:
        cbT = A(P, [8, TT], BF16)
        cbTb = P.buf("cbT")
        mkT = A(P, [8, TT], BF16)
        mkTb = P.buf("mkT")
        ident = A(P, [128, 128], F32)
        P.dma("sp", lambda h: h.dma_start(out=ident[:], in_=dr["ident"][:, :]), writes=[onesb])
        wr = A(P, [128, KC, 8], F32)
        P.dma("sp", lambda h: h.dma_start(out=wr[:], in_=dr["w_r"].rearrange("(k p) e -> p k e", p=128)), writes=[onesb])
        br_t = A(P, [128, 8], F32)
        P.dma("sp", lambda h: h.dma_start(out=br_t[:], in_=dr["b_r"][:, :]), writes=[onesb])
        sel = A(P, [8, 8, 128], BF16)
        P.dma("pool", lambda h: h.dma_start(out=sel[:], in_=dr["sel"][:, :, :]), writes=[onesb])
    markA = P.a_cur
    wjoin = A(P, [128, 1], F32)
    P.op("dve", lambda h: h.memset(wjoin[:], 0.0), reads=wbufs, writes=[wAb])
    glu_t = A(P, [128, 2, 256], BF16)
    glub = P.buf("glu")
    sgl = A(P, [128, 256], F32)
    sglb = P.buf("sgl")
    xb = [A(P, [128, KC, 256], F32) for _ in range(2)]
    xbb = [P.buf(), P.buf()]
    brb_t = [A(P, [128, KC, 256], BF16) for _ in range(2)]
    brbb = [P.buf(), P.buf()]
    sq = A(P, [128, KC, 256], BF16)
    sqb = P.buf()
    rstd = A(P, [128, 256], F32)
    rstdb = P.buf()
    tmp = [A(P, [128, 256], F32) for _ in range(2)]
    tmpb = [P.buf(), P.buf()]
    hb = A(P, [128, KC, 256], BF16)
    hbb = P.buf()
    yb = A(P, [128, KC, 256], BF16)
    ybb = P.buf()
    zb = A(P, [128, KC, 256], F32)
    zbb = P.buf()
    sg = [A(P, [128, 256], F32) for _ in range(2)]
    sgb = [P.buf(), P.buf()]
    tt2 = [A(P, [128, 256], F32) for _ in range(2)]
    tt2b = [P.buf(), P.buf()]
    accA = [A(P, [128, 256], F32) for _ in range(2)]
    accAb = [P.buf(), P.buf()]
    xob = P.buf("xo")
    h2fb = P.buf("h2f")
    if moe:
        h2f = A(P, [128, KC, 256], F32)
        lg = A(P, [128, 8], F32)
        mx8 = A(P, [128, 8], F32)
        msk = A(P, [128, 8], F32)
        ex = A(P, [128, 8], F32)
        den = A(P, [128, 1], F32)
        nmx = A(P, [128, 1], F32)
        rb = P.buf("router")
    cnt = 0
    P.sscp = A(P, [128, 256], F32)
    P.sscpb = P.buf()
    def _ldA(bi_):
        t0_, n_, _r = blocks[bi_]
        xx, xxb = xb[bi_ % 2], xbb[bi_ % 2]
        bb_, bbb = brb_t[bi_ % 2], brbb[bi_ % 2]
        P.dma("sp", lambda h: h.dma_start(out=xx[:, 0:4, 0:n_], in_=xT[:, 0:4, t0_:t0_ + n_]), writes=[xxb])
        P.dma("sp", lambda h: h.dma_start(out=xx[:, 4:8, 0:n_], in_=xT[:, 4:8, t0_:t0_ + n_]), writes=[xxb])
        P.dma("sp", lambda h: h.dma_start(out=bb_[:, :, 0:n_], in_=brT[:, :, t0_:t0_ + n_]), writes=[bbb])
    _ldA(0)
    for bi, (t0, n, r) in enumerate(blocks):
        x_t, x_b = xb[bi % 2], xbb[bi % 2]
        b_t, b_b = brb_t[bi % 2], brbb[bi % 2]
        if bi + 1 < len(blocks):
            _ldA(bi + 1)
        rms_rstd(P, x_t, x_b, n, sq, sqb, ps[6], psb[6], rstd, rstdb, ones)
        norm_mod(P, x_t, x_b, n, rstd, rstdb, gm_a, sh_a, r, hb, hbb, tmp, tmpb)
        for oc in range(2):
            for kc in range(2):
                P.op("pe", lambda h, oc=oc, kc=kc, n=n, b_t=b_t: h.matmul(ps[6][:, 0:n], lhsT=wglu[:, kc, oc * 128:(oc + 1) * 128], rhs=b_t[:, 2 + kc, 0:n],
                                                                      start=(kc == 0), stop=(kc == 1)), reads=[wAb, b_b], writes=[psb[6]])
            P.op("act", lambda h, oc=oc, n=n: h.activation(out=sgl[:, 0:n], in_=ps[6][:, 0:n], func=AF.Sigmoid, bias=bglu[:, oc:oc + 1], scale=1.0), reads=[psb[6], wAb], writes=[sglb])
            P.op("dve", lambda h, oc=oc, n=n, b_t=b_t: h.tensor_tensor(out=glu_t[:, oc, 0:n], in0=sgl[:, 0:n], in1=b_t[:, 2 + oc, 0:n], op=ALU.mult), reads=[sglb, b_b], writes=[glub])
        for fc in range(8):
            ac, acb = accA[fc % 2], accAb[fc % 2]
            for b in range(4):
                gi = cnt % 2
                cnt += 1
                gps, gpb = ps[gi], psb[gi]
                pps, ppb = ps[2 + gi], psb[2 + gi]
                for k in range(KC):
                    P.op("pe", lambda h, gps=gps, k=k, b=b, fc=fc, n=n: h.matmul(gps[:, 0:n], lhsT=wgt[:, k, b * 1024 + fc * 128:b * 1024 + (fc + 1) * 128], rhs=hb[:, k, 0:n],
                                                                                 start=(k == 0), stop=(k == KC - 1)), reads=[wAb, hbb], writes=[gpb])
                for hh in range(2):
                    rhs_ap = glu_t[:, hh, 0:n] if b == 1 else b_t[:, 2 * b + hh, 0:n]
                    P.op("pe", lambda h, pps=pps, hh=hh, b=b, fc=fc, n=n, rhs_ap=rhs_ap: h.matmul(pps[:, 0:n], lhsT=wbr[:, 2 * b + hh, fc * 128:(fc + 1) * 128], rhs=rhs_ap,
                                                                                          start=(hh == 0), stop=(hh == 1)), reads=[wAb, b_b, glub], writes=[ppb])
                s_t, s_b = sg[gi], sgb[gi]
                P.op("act", lambda h, s_t=s_t, gps=gps, n=n: h.activation(out=s_t[:, 0:n], in_=gps[:, 0:n], func=AF.Sigmoid), reads=[gpb], writes=[s_b])
                if b == 0:
                    P.op("dve", lambda h, ac=ac, s_t=s_t, pps=pps, n=n: h.tensor_tensor(out=ac[:, 0:n], in0=s_t[:, 0:n], in1=pps[:, 0:n], op=ALU.mult),
                         reads=[s_b, ppb], writes=[acb])
                else:
                    t_t, t_b = tt2[gi], tt2b[gi]
                    P.op("dve", lambda h, t_t=t_t, s_t=s_t, pps=pps, n=n: h.tensor_tensor(out=t_t[:, 0:n], in0=s_t[:, 0:n], in1=pps[:, 0:n], op=ALU.mult),
                         reads=[s_b, ppb], writes=[t_b])
                    if b < 3:
                        P.op("pool", lambda h, ac=ac, t_t=t_t, n=n: h.tensor_tensor(out=ac[:, 0:n], in0=ac[:, 0:n], in1=t_t[:, 0:n], op=ALU.add),
                             reads=[acb, t_b], writes=[acb])
                    else:
                        P.op("pool", lambda h, ac=ac, t_t=t_t, n=n, fc=fc: h.tensor_tensor(out=yb[:, fc, 0:n], in0=ac[:, 0:n], in1=t_t[:, 0:n], op=ALU.add),
                             reads=[acb, t_b], writes=[ybb])
        for fc in range(8):
            zi = 4 + fc % 2
            for k in range(KC):
                P.op("pe", lambda h, zi=zi, k=k, fc=fc, n=n: h.matmul(ps[zi][:, 0:n], lhsT=wo[:, k, fc * 128:(fc + 1) * 128], rhs=yb[:, k, 0:n], start=(k == 0), stop=(k == KC - 1)),
                     reads=[wAb, ybb], writes=[psb[zi]])
            P.op("act", lambda h, zi=zi, fc=fc, n=n: h.activation(out=zb[:, fc, 0:n], in_=ps[zi][:, 0:n], func=AF.Copy), reads=[psb[zi]], writes=[zbb])
        rms_rstd(P, zb, zbb, n, sq, sqb, ps[6], psb[6], rstd, rstdb, ones)
        if DBG: P.dbgt.append(P.dma("sp", lambda h, t0=t0, n=n: h.dma_start(out=dbg_z[:, :, t0:t0 + n], in_=zb[:, :, 0:n]), reads=[zbb], writes=[P.buf()]))
        if DBG: P.dbgt.append(P.dma("sp", lambda h, t0=t0, n=n: h.dma_start(out=dbg_r[:, t0:t0 + n], in_=rstd[:, 0:n]), reads=[rstdb], writes=[P.buf()]))
        if DBG: P.dbgt.append(P.dma("sp", lambda h, t0=t0, n=n: h.dma_start(out=dbg_sq[:, :, t0:t0 + n], in_=sq[:, :, 0:n]), reads=[sqb], writes=[P.buf()]))
        if DBG: P.op("dve", lambda h, n=n: h.tensor_copy(out=P.sscp[:, 0:n], in_=ps[6][:, 0:n]), reads=[psb[6]], writes=[P.sscpb])
        if DBG: P.dbgt.append(P.dma("sp", lambda h, t0=t0, n=n: h.dma_start(out=dbg_ss[:, t0:t0 + n], in_=P.sscp[:, 0:n]), reads=[P.sscpb], writes=[P.buf()]))
        for k in range(KC):
            tb_, tt_ = tmpb[k % 2], tmp[k % 2]
            P.op("dve", lambda h, k=k, tt_=tt_, n=n: h.tensor_tensor(out=tt_[:, 0:n], in0=zb[:, k, 0:n], in1=rstd[:, 0:n], op=ALU.mult), reads=[zbb, rstdb], writes=[tb_])
            P.op("dve", lambda h, k=k, tt_=tt_, n=n, x_t=x_t, r=r: h.scalar_tensor_tensor(out=x_t[:, k, 0:n], in0=tt_[:, 0:n], scalar=gg_a[:, k, r:r + 1], in1=x_t[:, k, 0:n],
                                                                                    op0=ALU.mult, op1=ALU.add), reads=[tb_, P.modb, x_b], writes=[x_b])
        P.dma("sp", lambda h, x_t=x_t, t0=t0, n=n: h.dma_start(out=xoT[:, :, t0:t0 + n], in_=x_t[:, :, 0:n]), reads=[x_b], writes=[xob])
        if DBG: P.dbgt.append(P.dma("sp", lambda h, x_t=x_t, t0=t0, n=n: h.dma_start(out=dbg_xm[:, :, t0:t0 + n], in_=x_t[:, :, 0:n]), reads=[x_b], writes=[P.buf()]))
        if DBG: P.dbgt.append(P.dma("sp", lambda h, t0=t0, n=n: h.dma_start(out=dbg_h[:, :, t0:t0 + n], in_=hb[:, :, 0:n]), reads=[hbb], writes=[P.buf()]))
        if DBG: P.dbgt.append(P.dma("sp", lambda h, t0=t0, n=n: h.dma_start(out=dbg_y[:, :, t0:t0 + n], in_=yb[:, :, 0:n]), reads=[ybb], writes=[P.buf()]))
        rms_rstd(P, x_t, x_b, n, sq, sqb, ps[6], psb[6], rstd, rstdb, ones)
        norm_mod(P, x_t, x_b, n, rstd, rstdb, gm_f, sh_f, r, h2, h2b, tmp, tmpb, dst_off=t0, dst32=(h2f if moe else None), dst32b=h2fb)
        if moe:
            for tt in range(n // 128):
                for k in range(KC):
                    P.op("pe", lambda h, k=k, tt=tt: h.matmul(ps[7][:, 0:8], lhsT=h2f[:, k, tt * 128:(tt + 1) * 128], rhs=wr[:, k, :], start=(k == 0), stop=(k == KC - 1)),
                         reads=[h2fb, onesb], writes=[psb[7]])
                P.op("dve", lambda h: h.tensor_tensor(out=lg[:], in0=ps[7][:, 0:8], in1=br_t[:], op=ALU.add), reads=[psb[7], onesb], writes=[rb])
                P.op("dve", lambda h: h.max(out=mx8[:], in_=lg[:]), reads=[rb], writes=[rb])
                P.op("dve", lambda h: h.tensor_scalar(out=msk[:], in0=lg[:], scalar1=mx8[:, 1:2], scalar2=None, op0=ALU.is_ge), reads=[rb], writes=[rb])
                P.op("dve", lambda h: h.tensor_scalar(out=nmx[:], in0=mx8[:, 0:1], scalar1=-1.0, scalar2=None, op0=ALU.mult), reads=[rb], writes=[rb])
                P.op("act", lambda h: h.activation(out=ex[:], in_=lg[:], func=AF.Exp, bias=nmx[:, 0:1], scale=1.0), reads=[rb], writes=[rb])
                P.op("dve", lambda h: h.tensor_tensor(out=ex[:], in0=ex[:], in1=msk[:], op=ALU.mult), reads=[rb], writes=[rb])
                P.op("dve", lambda h: h.reduce_sum(out=den[:], in_=ex[:], axis=AX.X), reads=[rb], writes=[rb])
                P.op("dve", lambda h: h.reciprocal(out=den[:], in_=den[:]), reads=[rb], writes=[rb])
                P.op("dve", lambda h: h.tensor_scalar(out=ex[:], in0=ex[:], scalar1=den[:, 0:1], scalar2=None, op0=ALU.mult), reads=[rb], writes=[rb])
                P.op("pe", lambda h: h.transpose(ps[7][0:8, 128:256], ex[:], ident[:]), reads=[rb, onesb], writes=[psb[7]])
                P.op("act", lambda h, t0=t0, tt=tt: h.activation(out=cbT[:, t0 + tt * 128:t0 + (tt + 1) * 128], in_=ps[7][0:8, 128:256], func=AF.Copy), reads=[psb[7]], writes=[cbTb])
                if mode == 'moe_a':
                    P.op("pe", lambda h: h.transpose(ps[7][0:8, 256:384], msk[:], ident[:]), reads=[rb, onesb], writes=[psb[7]])
                    P.op("act", lambda h, t0=t0, tt=tt: h.activation(out=mkT[:, t0 + tt * 128:t0 + (tt + 1) * 128], in_=ps[7][0:8, 256:384], func=AF.Copy), reads=[psb[7]], writes=[mkTb])
    barrier(P)
    P.a_cur = markA
    if mode == 'moe_a':
        fin = [P.dma("sp", lambda h: h.dma_start(out=h2o[:, :, :], in_=h2[:, :, :]), reads=[h2b], writes=[P.buf()]),
               P.dma("sp", lambda h: h.dma_start(out=cbo[:, :], in_=cbT[:, :]), reads=[cbTb], writes=[P.buf()]),
               P.dma("sp", lambda h: h.dma_start(out=mko[:, :], in_=mkT[:, :]), reads=[mkTb], writes=[P.buf()])]
        barrier(P)
        P.finish_wait("sp", fin + P.dbgt)
        P.emit()
        return nc
    blocks = blocksB
    NSL = 4
    P.a_cur = w_lo
    acc = A(P, [128, KC, TT], F32)
    accb = [P.buf() for _ in blocks]
    hid = [A(P, [128, NSL, 512], BF16) for _ in range(2)]
    hidb = [P.buf(), P.buf()]
    ssb_t = [A(P, [128, 512], F32) for _ in range(2)]
    ssbb = [P.buf(), P.buf()]
    cbe = A(P, [128, 512], BF16)
    cbeb = P.buf()
    assert P.a_cur <= w_hi, "stage-B tiles overflow the weight region"
    P.a_cur = markA
    markB = markA
    wg_s = [A(P, [128, KC, NSL * 128], BF16) for _ in range(2)]
    wu_s = [A(P, [128, KC, NSL * 128], BF16) for _ in range(2)]
    wd_s = [A(P, [128, NSL, D], BF16) for _ in range(2)]
    wsb = [P.buf(), P.buf()]
    ntile = dff // 128
    slices = [(s0, min(NSL, ntile - s0)) for s0 in range(0, ntile, NSL)]
    si = 0
    hcnt = 0
    gcnt = 0
    work = [(e, s0, ns) for e in range(n_exp) for (s0, ns) in slices]

    def _ldW(widx):
        e_, s0_, ns_ = work[widx]
        wi_ = widx % 2
        wgv_ = dr["w_g"][e_].rearrange("(k p) f -> p k f", p=128)
        wuv_ = dr["w_u"][e_].rearrange("(k p) f -> p k f", p=128)
        wdv_ = dr["w_d"][e_].rearrange("(j p) c -> p j c", p=128)
        for k2 in range(2):
            P.dma("pool", lambda h, k2=k2: h.dma_start(out=wg_s[wi_][:, 4 * k2:4 * k2 + 4, 0:ns_ * 128], in_=wgv_[:, 4 * k2:4 * k2 + 4, s0_ * 128:(s0_ + ns_) * 128]), writes=[wsb[wi_]])
            P.dma("pool", lambda h, k2=k2: h.dma_start(out=wu_s[wi_][:, 4 * k2:4 * k2 + 4, 0:ns_ * 128], in_=wuv_[:, 4 * k2:4 * k2 + 4, s0_ * 128:(s0_ + ns_) * 128]), writes=[wsb[wi_]])
        for j in range(ns_):
            P.dma("pool", lambda h, j=j: h.dma_start(out=wd_s[wi_][:, j, :], in_=wdv_[:, s0_ + j, :]), writes=[wsb[wi_]])
    _ldW(0)
    for widx, (e, s0, ns) in enumerate(work):
        if True:
            wi = widx % 2
            if widx + 1 < len(work):
                _ldW(widx + 1)
            for bi, (t0, n, r) in enumerate(blocks):
                hi = hcnt % 2
                hcnt += 1
                if moe:
                    P.op("pe", lambda h, e=e, t0=t0, n=n: h.matmul(ps[7][:, 0:n], lhsT=sel[:, e, :], rhs=cbT[:, t0:t0 + n], start=True, stop=True), reads=[cbTb, onesb], writes=[psb[7]])
                    P.op("act", lambda h, n=n: h.activation(out=cbe[:, 0:n], in_=ps[7][:, 0:n], func=AF.Copy), reads=[psb[7]], writes=[cbeb])
                for j in range(ns):
                    gi = gcnt % 2
                    gcnt += 1
                    for k in range(KC):
                        P.op("pe", lambda h, gi=gi, wi=wi, j=j, k=k, t0=t0, n=n: h.matmul(ps[gi][:, 0:n], lhsT=wg_s[wi][:, k, j * 128:(j + 1) * 128], rhs=h2[:, k, t0:t0 + n], start=(k == 0), stop=(k == KC - 1)),
                             reads=[wsb[wi], h2b], writes=[psb[gi]])
                    for k in range(KC):
                        P.op("pe", lambda h, gi=gi, wi=wi, j=j, k=k, t0=t0, n=n: h.matmul(ps[2 + gi][:, 0:n], lhsT=wu_s[wi][:, k, j * 128:(j + 1) * 128], rhs=h2[:, k, t0:t0 + n], start=(k == 0), stop=(k == KC - 1)),
                             reads=[wsb[wi], h2b], writes=[psb[2 + gi]])
                    P.op("act", lambda h, gi=gi, n=n: h.activation(out=ssb_t[gi][:, 0:n], in_=ps[gi][:, 0:n], func=AF.Silu), reads=[psb[gi]], writes=[ssbb[gi]])
                    if moe:
                        P.op("dve", lambda h, gi=gi, n=n: h.tensor_tensor(out=ssb_t[gi][:, 0:n], in0=ssb_t[gi][:, 0:n], in1=ps[2 + gi][:, 0:n], op=ALU.mult),
                             reads=[ssbb[gi], psb[2 + gi]], writes=[ssbb[gi]])
                        P.op("pool", lambda h, gi=gi, hi=hi, j=j, n=n: h.tensor_tensor(out=hid[hi][:, j, 0:n], in0=ssb_t[gi][:, 0:n], in1=cbe[:, 0:n], op=ALU.mult),
                             reads=[ssbb[gi], cbeb], writes=[hidb[hi]])
                    else:
                        P.op("dve", lambda h, gi=gi, hi=hi, j=j, n=n: h.tensor_tensor(out=hid[hi][:, j, 0:n], in0=ssb_t[gi][:, 0:n], in1=ps[2 + gi][:, 0:n], op=ALU.mult),
                             reads=[ssbb[gi], psb[2 + gi]], writes=[hidb[hi]])
                first = (e == 0 and s0 == 0)
                for fc in range(8):
                    oi = 4 + fc % 2
                    for j in range(ns):
                        P.op("pe", lambda h, oi=oi, wi=wi, j=j, fc=fc, hi=hi, n=n, ns=ns: h.matmul(ps[oi][:, 0:n], lhsT=wd_s[wi][:, j, fc * 128:(fc + 1) * 128], rhs=hid[hi][:, j, 0:n], start=(j == 0), stop=(j == ns - 1)),
                             reads=[wsb[wi], hidb[hi]], writes=[psb[oi]])
                    if first:
                        P.op("act", lambda h, oi=oi, fc=fc, t0=t0, n=n: h.activation(out=acc[:, fc, t0:t0 + n], in_=ps[oi][:, 0:n], func=AF.Copy), reads=[psb[oi]], writes=[accb[bi]])
                    else:
                        P.op("dve", lambda h, oi=oi, fc=fc, t0=t0, n=n: h.tensor_tensor(out=acc[:, fc, t0:t0 + n], in0=acc[:, fc, t0:t0 + n], in1=ps[oi][:, 0:n], op=ALU.add),
                             reads=[psb[oi], accb[bi]], writes=[accb[bi]])
    barrier(P)
    P.a_cur = markB
    xm = [A(P, [128, KC, 512], F32) for _ in range(2)]
    xmb = [P.buf(), P.buf()]
    sqF = A(P, [128, KC, 512], BF16)
    rstdF = A(P, [128, 512], F32)
    tmpF = [A(P, [128, 512], F32) for _ in range(2)]
    outs = []
    for bi, (t0, n, r) in enumerate(blocks):
        x_t, x_b = xm[bi % 2], xmb[bi % 2]
        P.dma("sp", lambda h, x_t=x_t, t0=t0, n=n: h.dma_start(out=x_t[:, :, 0:n], in_=xoT[:, :, t0:t0 + n]), reads=[xob], writes=[x_b])
        accv = acc[:, :, t0:t0 + n]
        P.op("act", lambda h, accv=accv, n=n: h.activation(out=sqF[:, :, 0:n], in_=accv, func=AF.Square), reads=[accb[bi]], writes=[sqb])
        for k in range(KC):
            P.op("pe", lambda h, k=k, n=n: h.matmul(ps[6][:, 0:n], lhsT=ones[:], rhs=sqF[:, k, 0:n], start=(k == 0), stop=(k == KC - 1)), reads=[sqb, onesb], writes=[psb[6]])
        P.op("act", lambda h, n=n: h.activation(out=rstdF[:, 0:n], in_=ps[6][:, 0:n], func=AF.Ln, scale=1.0 / D, bias=P.eps_t[:, 0:1]), reads=[psb[6]], writes=[rstdb])
        P.op("act", lambda h, n=n: h.activation(out=rstdF[:, 0:n], in_=rstdF[:, 0:n], func=AF.Exp, scale=-0.5), reads=[rstdb], writes=[rstdb])
        for k in range(KC):
            tb_, tt_ = tmpb[k % 2], tmpF[k % 2]
            P.op("dve", lambda h, k=k, tt_=tt_, n=n, t0=t0: h.tensor_tensor(out=tt_[:, 0:n], in0=acc[:, k, t0:t0 + n], in1=rstdF[:, 0:n], op=ALU.mult), reads=[accb[bi], rstdb], writes=[tb_])
            P.op("dve", lambda h, k=k, tt_=tt_, n=n, x_t=x_t, r=r: h.scalar_tensor_tensor(out=x_t[:, k, 0:n], in0=tt_[:, 0:n], scalar=gg_f[:, k, r:r + 1], in1=x_t[:, k, 0:n],
                                                                                    op0=ALU.mult, op1=ALU.add), reads=[tb_, P.modb, x_b], writes=[x_b])
        outs.append(P.dma("sp", lambda h, x_t=x_t, t0=t0, n=n: h.dma_start(out=xoT[:, :, t0:t0 + n], in_=x_t[:, :, 0:n]), reads=[x_b], writes=[xob]))
    P.finish_wait("sp", outs + P.dbgt)
    P.emit()
    return nc


def build_E(groups=(4,) * 8, dff=3584):
    nc = bass.Bass("TRN2", target_bir_lowering=False)
    ngrp = len(groups)
    gtok = 512 * max(groups)
    NT = 512 * sum(groups)
    goff = [512 * sum(groups[:g]) for g in range(ngrp)]
    h2d = nc.dram_tensor("h2", [KC, 128, NT], BF16, kind="ExternalInput").ap().rearrange("k p t -> p k t")
    cbd = nc.dram_tensor("cbe", [128, NT], BF16, kind="ExternalInput").ap()
    wgd = nc.dram_tensor("w_g", [D, dff], F32, kind="ExternalInput").ap().rearrange("(k p) f -> p k f", p=128)
    wud = nc.dram_tensor("w_u", [D, dff], F32, kind="ExternalInput").ap().rearrange("(k p) f -> p k f", p=128)
    wdd = nc.dram_tensor("w_d", [dff, D], F32, kind="ExternalInput").ap().rearrange("(j p) c -> p j c", p=128)
    ye = nc.dram_tensor("ye", [KC, 128, NT], F32, kind="ExternalOutput").ap().rearrange("k p t -> p k t")
    P = Prog(nc)
    arena_init(P)
    ps = [P.ps("ps%d" % i, [128, 512], F32) for i in range(8)]
    psb = [P.buf("ps%d" % i) for i in range(8)]
    h2g = [A(P, [128, KC, gtok], BF16) for _ in range(2)]
    h2gb = [P.buf(), P.buf()]
    cbg = [A(P, [128, gtok], BF16) for _ in range(2)]
    acc = A(P, [128, KC, gtok], F32)
    NSL = 4
    wg_s = [A(P, [128, KC, NSL * 128], BF16) for _ in range(2)]
    wu_s = [A(P, [128, KC, NSL * 128], BF16) for _ in range(2)]
    wd_s = [A(P, [128, NSL, D], BF16) for _ in range(2)]
    wsb = [P.buf(), P.buf()]
    hid = [A(P, [128, NSL, 512], BF16) for _ in range(2)]
    hidb = [P.buf(), P.buf()]
    ssb_t = [A(P, [128, 512], F32) for _ in range(2)]
    ssbb = [P.buf(), P.buf()]
    ntile = dff // 128
    slices = [(s0, min(NSL, ntile - s0)) for s0 in range(0, ntile, NSL)]
    accb = [P.buf() for _ in range(max(groups))]
    si = hcnt = gcnt = 0
    outs = []
    work = [(g, sidx, s0, ns) for g in range(ngrp) for sidx, (s0, ns) in enumerate(slices)]

    def _ldG(g_):
        hg_, hgb_ = h2g[g_ % 2], h2gb[g_ % 2]
        gn_ = 512 * groups[g_]
        for k2 in range(2):
            _ldF(P, "sp", hg_[:, 4 * k2:4 * k2 + 4, 0:gn_], h2d[:, 4 * k2:4 * k2 + 4, goff[g_]:goff[g_] + gn_], [hgb_])
        _ldF(P, "sp", cbg[g_ % 2][:, 0:gn_], cbd[:, goff[g_]:goff[g_] + gn_], [hgb_])

    def _ldW(widx):
        _g, _sidx, s0_, ns_ = work[widx]
        wi_ = widx % 2
        for k2 in range(2):
            _ldF(P, "pool", wg_s[wi_][:, 4 * k2:4 * k2 + 4, 0:ns_ * 128], wgd[:, 4 * k2:4 * k2 + 4, s0_ * 128:(s0_ + ns_) * 128], [wsb[wi_]])
            _ldF(P, "pool", wu_s[wi_][:, 4 * k2:4 * k2 + 4, 0:ns_ * 128], wud[:, 4 * k2:4 * k2 + 4, s0_ * 128:(s0_ + ns_) * 128], [wsb[wi_]])
        for j in range(ns_):
            _ldF(P, "pool", wd_s[wi_][:, j, :], wdd[:, s0_ + j, :], [wsb[wi_]])
    _ldG(0)
    _ldW(0)
    pending = []

    def _down(u):
        (g_, sidx_, ns_, wi_, bi_, hi_, last_) = u
        t0_, n_ = bi_ * 512, 512
        for fc in range(8):
            oi = 4 + fc % 2
            for j in range(ns_):
                _mmF(P, ps[oi][:, 0:n_], wd_s[wi_][:, j, fc * 128:(fc + 1) * 128], hid[hi_][:, j, 0:n_], j == 0, j == ns_ - 1, [wsb[wi_], hidb[hi_]], [psb[oi]])
            av = acc[:, fc, t0_:t0_ + n_]
            pv = ps[oi][:, 0:n_]
            if sidx_ == 0:
                P.op("act", lambda h, av=av, pv=pv: h.activation(out=av, in_=pv, func=AF.Copy), reads=[psb[oi]], writes=[accb[bi_]])
            else:
                P.op("dve", lambda h, av=av, pv=pv: h.tensor_tensor(out=av, in0=av, in1=pv, op=ALU.add), reads=[psb[oi], accb[bi_]], writes=[accb[bi_]])
        if last_:
            outs.append(_ldF(P, "sp", ye[:, :, goff[g_] + t0_:goff[g_] + t0_ + 512], acc[:, :, t0_:t0_ + 512], [P.buf()], reads=[accb[bi_]]))

    for widx, (g, sidx, s0, ns) in enumerate(work):
        hg, hgb = h2g[g % 2], h2gb[g % 2]
        cg = cbg[g % 2]
        nblk = groups[g]
        if sidx == 0 and g + 1 < ngrp:
            _ldG(g + 1)
        wi = widx % 2
        for bi in range(nblk):
            t0, n = bi * 512, 512
            hi = hcnt % 2
            hcnt += 1
            for j in range(ns):
                gi = gcnt % 2
                gcnt += 1
                for k in range(KC):
                    _mmF(P, ps[gi][:, 0:n], wg_s[wi][:, k, j * 128:(j + 1) * 128], hg[:, k, t0:t0 + n], k == 0, k == KC - 1, [wsb[wi], hgb], [psb[gi]])
                for k in range(KC):
                    _mmF(P, ps[2 + gi][:, 0:n], wu_s[wi][:, k, j * 128:(j + 1) * 128], hg[:, k, t0:t0 + n], k == 0, k == KC - 1, [wsb[wi], hgb], [psb[2 + gi]])
                st_, stb_ = ssb_t[gi], ssbb[gi]
                P.op("act", lambda h, st_=st_, gi=gi, n=n: h.activation(out=st_[:, 0:n], in_=ps[gi][:, 0:n], func=AF.Silu), reads=[psb[gi]], writes=[stb_])
                P.op("dve", lambda h, st_=st_, gi=gi, n=n: h.tensor_tensor(out=st_[:, 0:n], in0=st_[:, 0:n], in1=ps[2 + gi][:, 0:n], op=ALU.mult), reads=[stb_, psb[2 + gi]], writes=[stb_])
                hd = hid[hi]
                P.op("pool", lambda h, st_=st_, hd=hd, j=j, n=n, cg=cg, t0=t0: h.tensor_tensor(out=hd[:, j, 0:n], in0=st_[:, 0:n], in1=cg[:, t0:t0 + n], op=ALU.mult), reads=[stb_, hgb], writes=[hidb[hi]])
            if pending:
                _down(pending.pop(0))
            if bi == 0 and widx + 1 < len(work):
                _ldW(widx + 1)
            pending.append((g, sidx, ns, wi, bi, hi, sidx == len(slices) - 1))
    while pending:
        _down(pending.pop(0))
    P.finish_wait("sp", outs)
    P.emit()
    return nc


def _ldF(P, q, out, in_, writes, reads=()):
    return P.dma(q, lambda h: h.dma_start(out=out, in_=in_), reads=reads, writes=writes)


def _mmF(P, out, lhsT, rhs, start, stop, reads, writes):
    return P.op("pe", lambda h: h.matmul(out, lhsT=lhsT, rhs=rhs, start=start, stop=stop), reads=reads, writes=writes)


def build_Fc(TT=2048, nexp=8):
    nc = bass.Bass("TRN2", target_bir_lowering=False)
    dr = {}

    def din(name, shape, dt=F32):
        dr[name] = nc.dram_tensor(name, list(shape), dt, kind="ExternalInput").ap()
    din("xm", [KC, 128, TT])
    din("yp", [nexp, KC, 128, TT])
    din("condT", [128, KC, 2])
    din("w_mod", [D, 6 * D])
    din("b_modT", [128, 48, 2])
    din("norm_gT", [128, 4, KC, 2])
    xo = nc.dram_tensor("xo", [KC, 128, TT], F32, kind="ExternalOutput").ap().rearrange("k p t -> p k t")
    xm = dr["xm"].rearrange("k p t -> p k t")
    P = Prog(nc)
    arena_init(P)
    ps = [P.ps("ps%d" % i, [128, 512], F32) for i in range(8)]
    psb = [P.buf("ps%d" % i) for i in range(8)]
    ones = A(P, [128, 128], BF16)
    onesb = P.buf()
    P.op("dve", lambda h: h.memset(ones[:], 1.0), writes=[onesb])
    P.eps_t = A(P, [128, 1], F32)
    P.op("dve", lambda h: h.memset(P.eps_t[:], EPS), writes=[onesb])
    mod_view = ps[7][:, 0:96].rearrange("p (j r) -> p j r", r=2)
    compute_mod(P, dr, [5], mod_view, psb[7])
    _, gg_f = mod_derived(P, 4, 5, 2, 3)
    acc = [A(P, [128, KC, 512], F32) for _ in range(2)]
    accb = [P.buf(), P.buf()]
    part = [A(P, [128, KC, 512], F32) for _ in range(3)]
    partb = [P.buf() for _ in range(3)]
    xt = [A(P, [128, KC, 512], F32) for _ in range(2)]
    xtb = [P.buf(), P.buf()]
    sq = A(P, [128, KC, 512], BF16)
    sqb = P.buf()
    rstd = A(P, [128, 512], F32)
    rstdb = P.buf()
    tmp = [A(P, [128, 512], F32) for _ in range(2)]
    tmpb = [P.buf(), P.buf()]
    outs = []
    pc = 0
    for bi in range(TT // 512):
        t0, n = bi * 512, 512
        a_t, a_b = acc[bi % 2], accb[bi % 2]
        x_t, x_b = xt[bi % 2], xtb[bi % 2]
        _ldF(P, "sp", x_t[:, :, :], xm[:, :, t0:t0 + n], [x_b])
        _ldF(P, "sp", a_t[:, :, :], dr["yp"][0].rearrange("k p t -> p k t")[:, :, t0:t0 + n], [a_b])
        for e in range(1, nexp):
            p_t, p_b = part[pc % 3], partb[pc % 3]
            pc += 1
            _ldF(P, "act" if e % 2 else "sp", p_t[:, :, :], dr["yp"][e].rearrange("k p t -> p k t")[:, :, t0:t0 + n], [p_b])
            eng = "dve" if e % 2 else "pool"
            P.op(eng, lambda h, a_t=a_t, p_t=p_t: h.tensor_tensor(out=a_t[:, :, :], in0=a_t[:, :, :], in1=p_t[:, :, :], op=ALU.add), reads=[a_b, p_b], writes=[a_b])
        rms_rstd(P, a_t, a_b, n, sq, sqb, ps[6], psb[6], rstd, rstdb, ones)
        for k in range(KC):
            tb_, tt_ = tmpb[k % 2], tmp[k % 2]
            P.op("dve", lambda h, k=k, tt_=tt_, a_t=a_t: h.tensor_tensor(out=tt_[:, :], in0=a_t[:, k, :], in1=rstd[:, :], op=ALU.mult), reads=[a_b, rstdb], writes=[tb_])
            P.op("dve", lambda h, k=k, tt_=tt_, x_t=x_t: h.scalar_tensor_tensor(out=x_t[:, k, :], in0=tt_[:, :], scalar=gg_f[:, k, 0:1], in1=x_t[:, k, :], op0=ALU.mult, op1=ALU.add),
                 reads=[tb_, P.modb, x_b], writes=[x_b])
        outs.append(_ldF(P, "sp", xo[:, :, t0:t0 + n], x_t[:, :, :], [P.buf()], reads=[x_b]))
    P.finish_wait("sp", outs)
    P.emit()
    return nc


import math, os
RET_STOP = int(os.environ.get('RET_STOP', '99'))
SKIP = os.environ.get('SKIP', '')

NTOK = 8448
NCH = 66
MAGIC = 12582912.0
TWO_PI = 2.0 * math.pi


def pos_of(dd):
    if dd == 0:
        return list(range(NCH))
    order = [1, 0] + list(range(65, 1, -1))
    pos = [0] * NCH
    for p_, c in enumerate(order):
        pos[c] = p_
    return pos


def range_reduce_sincos(P, ph, sn, cs, tmp, shape_ap, b):
    v = shape_ap
    _ts(P, "dve", v(tmp), v(ph), 1.0 / TWO_PI, MAGIC, ALU.mult, ALU.add, [b], [b])
    _ts(P, "dve", v(tmp), v(tmp), -MAGIC, None, ALU.add, None, [b], [b])
    _stt(P, v(ph), v(tmp), -TWO_PI, v(ph), ALU.mult, ALU.add, [b], [b])
    _ts(P, "dve", v(ph), v(ph), -math.pi, math.pi, ALU.max, ALU.min, [b], [b])
    _act(P, v(sn), v(ph), AF.Sin, [b], [b])
    _ts(P, "dve", v(tmp), v(ph), -1.0, None, ALU.mult, None, [b], [b])
    _tt(P, "dve", v(tmp), v(tmp), v(ph), ALU.max, [b], [b])
    _act(P, v(cs), v(tmp), AF.Sin, [b], [b], scale=-1.0, bias=P.halfpi[0:v(tmp).shape[0], 0:1])


def build_M(need_ctx_out, parts=("four", "s5", "ret", "na"), DBG=False):
    nc = bass.Bass("TRN2", target_bir_lowering=False)
    dr = {}

    def din(name, shape, dt=F32):
        dr[name] = nc.dram_tensor(name, list(shape), dt, kind="ExternalInput").ap()
    din("xT", [KC, 128, NTOK])
    din("condT", [128, KC, 2])
    din("w_mod", [D, 6 * D])
    din("b_modT", [128, 48, 2])
    din("norm_gT", [128, 4, KC, 2])
    din("w_fm", [D, 576])
    din("w_tm", [D, 256])
    din("f_CS", [64, 128], BF16); din("f_RP", [64, 128], BF16); din("f_RQ", [64, 128], BF16)
    din("f_CB", [128, 64, 128], BF16); din("f_SB", [128, 64, 128], BF16)
    din("f_C256", [128, 2, 256], BF16); din("f_S256", [128, 2, 256], BF16)
    din("r_cosF", [64, 8192]); din("r_sinF", [64, 8192]); din("r_cosT", [128, 64, 64]); din("r_sinT", [128, 64, 64])
    din("r_jcol", [128, 2]); din("r_dist", [128, 128]); din("r_mask", [2, 128, 128]); din("r_irow", [2, 64, 128])
    din("s_jrow", [128, 129]); din("s_jcol", [128, 1]); din("s_LT", [2, 128, 128], BF16); din("s_mrow", [64, 4]); din("s_msm", [128, 2, 4])
    din("ident_bf", [128, 128], BF16); din("ident_f", [128, 128])
    din("n_mask", [5, 128, 832]); din("n_toep", [15, 64, 64])
    din("s_sm", [128, 2, 2, 3]); din("s_row", [128, 2, 3, 256]); din("s_hs", [64, 2, 3, 64]); din("s_B", [64, 2, 2, 64])
    din("s_C", [128, 2, 2, 2, 16]); din("s_d", [64, 1])
    din("r_dec", [128, 2]); din("r_gn", [64, 1])
    out = nc.dram_tensor("brT_out", [4, 64, NTOK], BF16, kind="ExternalOutput").ap()
    hT = nc.dram_tensor("hT_scr", [KC, 128, NTOK], BF16, kind="Internal").ap().rearrange("k p t -> p k t")
    xT = dr["xT"].rearrange("k p t -> p k t")

    P = Prog(nc)
    arena_init(P)
    ps = [P.ps("ps%d" % i, [128, 512], F32) for i in range(8)]
    psb = [P.buf("ps%d" % i) for i in range(8)]
    cb = P.buf("consts")
    ones = A(P, [128, 128], BF16)
    P.op("dve", lambda h: h.memset(ones[:], 1.0), writes=[cb])
    P.eps_t = A(P, [128, 1], F32)
    P.op("dve", lambda h: h.memset(P.eps_t[:], EPS), writes=[cb])
    P.halfpi = A(P, [128, 1], F32)
    P.op("dve", lambda h: h.memset(P.halfpi[:], math.pi / 2), writes=[cb])
    P.one_t = A(P, [128, 1], F32)
    P.op("dve", lambda h: h.memset(P.one_t[:], 1.0), writes=[cb])
    ident = A(P, [128, 128], BF16)
    _ld(P, "sp", ident[:], dr["ident_bf"][:, :], [cb])
    mod_view = ps[7][:, 0:96].rearrange("p (j r) -> p j r", r=2)
    compute_mod(P, dr, [0, 1], mod_view, psb[7])
    gm_a, _ = mod_derived(P, 1, None, 0, 0)
    sh_a = P.modT[:, 0:8, :]
    wfm = A(P, [128, KC, 576], BF16)
    wtm = A(P, [128, KC, 256], BF16)
    wb = P.buf("w")
    _ld(P, "pool", wfm[:], dr["w_fm"].rearrange("(k p) c -> p k c", p=128), [wb])
    _ld(P, "pool", wtm[:], dr["w_tm"].rearrange("(k p) c -> p k c", p=128), [wb])
    blocks = [(0, 256, 1)] + [(256 + 512 * i, 512, 0) for i in range(16)]
    outs = []
    hTb = P.buf("hT")
    mark0 = P.a_cur

    def fm_proj(hb, hbb, n, g, pst, pstb):
        for k in range(KC):
            _mm(P, pst[0:64, 0:n], wfm[:, k, g * 64:(g + 1) * 64], hb[:, k, 0:n], k == 0, k == KC - 1, [wb, hbb], [pstb])

    sT = A(P, [64, NTOK], BF16)
    markS = P.a_cur
    fT = A(P, [64, NTOK], BF16)
    fTb, sTb = P.buf("fT"), P.buf("sT")
    markA = P.a_cur
    xb = [A(P, [128, KC, 512], F32) for _ in range(2)]
    xbb = [P.buf(), P.buf()]
    sq = A(P, [128, KC, 512], BF16)
    sqb = P.buf()
    rstd = A(P, [128, 512], F32)
    rstdb = P.buf()
    tmp = [A(P, [128, 512], F32) for _ in range(8)]
    tmpb = [P.buf() for _ in range(8)]
    hbs = [A(P, [128, KC, 512], BF16) for _ in range(2)]
    hbsb = [P.buf(), P.buf()]
    def _ldx(bi_):
        t0_, n_, _r = blocks[bi_]
        _ld(P, "sp", xb[bi_ % 2][:, 0:4, 0:n_], xT[:, 0:4, t0_:t0_ + n_], [xbb[bi_ % 2]])
        _ld(P, "sp", xb[bi_ % 2][:, 4:8, 0:n_], xT[:, 4:8, t0_:t0_ + n_], [xbb[bi_ % 2]])
    _ldx(0)
    for bi, (t0, n, r) in enumerate(blocks):
        x_t, x_b = xb[bi % 2], xbb[bi % 2]
        hb, hbb = hbs[bi % 2], hbsb[bi % 2]
        if bi + 1 < len(blocks):
            _ldx(bi + 1)
        rms_rstd(P, x_t, x_b, n, sq, sqb, ps[6], psb[6], rstd, rstdb, ones)
        norm_mod(P, x_t, x_b, n, rstd, rstdb, gm_a, sh_a, r, hb, hbb, tmp, tmpb)
        _ld(P, "sp", hT[:, :, t0:t0 + n], hb[:, :, 0:n], [hTb], reads=[hbb])
        for gi_, (g, dst, dstb) in enumerate(((0, fT, fTb), (1, sT, sTb))):
            pi_ = (2 * bi + gi_) % 4
            fm_proj(hb, hbb, n, g, ps[pi_], psb[pi_])
            _cp(P, "act" if gi_ == 0 else "dve", dst[:, t0:t0 + n], ps[pi_][0:64, 0:n], [psb[pi_]], [dstb])
    barrier(P)
    P.a_cur = markA

    if "four" in parts:
        markF = P.a_cur
        CS = A(P, [64, 128], BF16); RP = A(P, [64, 128], BF16); RQ = A(P, [64, 128], BF16)
        CB = A(P, [128, 64, 128], BF16); SB = A(P, [128, 64, 128], BF16)
        ftb = P.buf("ftab")
        for t_, nm in ((CS, "f_CS"), (RP, "f_RP"), (RQ, "f_RQ")):
            _ld(P, "sp", t_[:], dr[nm][:, :], [ftb])
        _ld(P, "sp", CB[:], dr["f_CB"][:, :, :], [ftb])
        _ld(P, "sp", SB[:], dr["f_SB"][:, :, :], [ftb])
        PQ = A(P, [64, 128, 128], BF16); PQb = P.buf("PQ")
        UVT = A(P, [128, 64, 128], BF16); UVTb = P.buf("UVT")
        aT = A(P, [64, NTOK], BF16); aTb = P.buf("aT")
        for g4 in range(32):
            pi_ = g4 % 2
            for jj in range(4):
                m2 = g4 * 4 + jj
                _mm(P, ps[pi_][0:64, jj * 128:(jj + 1) * 128], fT[:, 256 + m2:NTOK:128], CS[:, :], True, True, [fTb, ftb], [psb[pi_]])
            _cp(P, "act" if g4 % 2 else "dve", PQ[:, g4 * 4:(g4 + 1) * 4, :], ps[pi_][0:64, 0:512].rearrange("p (a b) -> p a b", b=128), [psb[pi_]], [PQb])
        for g4 in range(16):
            pi_ = 2 + g4 % 2
            for jj in range(4):
                d = g4 * 4 + jj
                _mm(P, ps[pi_][:, jj * 128:(jj + 1) * 128], PQ[:, :, d], RP[:, :], True, False, [PQb, ftb], [psb[pi_]])
                _mm(P, ps[pi_][:, jj * 128:(jj + 1) * 128], PQ[:, :, 64 + d], RQ[:, :], False, True, [PQb, ftb], [psb[pi_]])
            _cp(P, "act" if g4 % 2 else "dve", UVT[:, g4 * 4:(g4 + 1) * 4, :], ps[pi_][:, 0:512].rearrange("p (a b) -> p a b", b=128), [psb[pi_]], [UVTb])
        aT3 = aT[:, 256:NTOK].rearrange("p (a b) -> p a b", b=64)
        for g4 in range(16):
            pi_ = g4 % 2
            for jj in range(4):
                n1 = g4 * 4 + jj
                _mm(P, ps[pi_][0:64, jj * 128:(jj + 1) * 128], UVT[:, :, n1], CB[:, n1, :], True, False, [UVTb, ftb], [psb[pi_]])
                _mm(P, ps[pi_][0:64, jj * 128:(jj + 1) * 128], UVT[:, :, 64 + n1], SB[:, n1, :], False, True, [UVTb, ftb], [psb[pi_]])
            _cp(P, "act" if g4 % 2 else "dve", aT3[:, :, g4 * 4:(g4 + 1) * 4], ps[pi_][0:64, 0:512].rearrange("p (j n) -> p n j", n=128), [psb[pi_]], [aTb])
        if need_ctx_out:
            C256 = A(P, [128, 2, 256], BF16); S256 = A(P, [128, 2, 256], BF16)
            _ld(P, "sp", C256[:], dr["f_C256"][:, :, :], [ftb])
            _ld(P, "sp", S256[:], dr["f_S256"][:, :, :], [ftb])
            PQc = A(P, [128, 2, 128], BF16); PQcb = P.buf()
            for tt in range(2):
                _mm(P, ps[2 + tt][:, 0:128], fT[:, tt * 128:(tt + 1) * 128], CS[:, :], True, True, [fTb, ftb], [psb[2 + tt]])
                _cp(P, "dve", PQc[:, tt, :], ps[2 + tt][:, 0:128], [psb[2 + tt]], [PQcb])
            seq = [(tt, 0) for tt in range(2)] + [(tt, 1) for tt in range(2)]
            for i_, (tt, pq) in enumerate(seq):
                _mm(P, ps[4][0:64, 0:256], PQc[:, tt, pq * 64:(pq + 1) * 64], (C256 if pq == 0 else S256)[:, tt, :], i_ == 0, i_ == 3, [PQcb, ftb], [psb[4]])
            _cp(P, "dve", aT[:, 0:256], ps[4][0:64, 0:256], [psb[4]], [aTb])
        else:
            P.op("dve", lambda h: h.memset(aT[:, 0:256], 0.0), writes=[aTb])
        outs.append(_ld(P, "sp", out[0, :, :], aT[:, :], [P.buf()], reads=[aTb]))
        barrier(P)
    P.a_cur = markS

    if "s5" in parts:
        s5_part(P, dr, ps, psb, sT, sTb, out, outs, need_ctx_out, cb)
    barrier(P)
    P.a_cur = mark0

    if "ret" in parts:
        ret_part(P, dr, ps, psb, hT, hTb, wfm, wtm, wb, blocks, out, outs, need_ctx_out, cb, ones)
        barrier(P)
        P.a_cur = mark0
    if "na" in parts:
        na_part(P, dr, ps, psb, hT, hTb, wfm, wtm, wb, blocks, out, outs, need_ctx_out, cb, ident)
        barrier(P)
    P.finish_wait("sp", outs)
    P.emit()
    return nc


def load_h(P, hT, hTb, hbs, hbsb, bi, t0, n):
    hb, hbb = hbs[bi % 2], hbsb[bi % 2]
    _ld(P, "sp", hb[:, :, 0:n], hT[:, :, t0:t0 + n], [hbb], reads=[hTb])
    return hb, hbb


def na_part(P, dr, ps, psb, hT, hTb, wfm, wtm, wb, blocks, out, outs, need_ctx_out, cb, ident):
    Cn = consts()
    drs, types = Cn["n_drs"], Cn["n_types"]
    nqT = A(P, [64, NTOK], BF16); nkT = A(P, [64, NTOK], BF16); nvT = A(P, [128, NCH, 64], BF16)
    nqb, nkb, nvb = P.buf("nq"), P.buf("nk"), P.buf("nv")
    nT = A(P, [64, NTOK], BF16); nTb = P.buf("nT")
    bias = A(P, [128, 5, 832], F32); biasb = P.buf("bias")
    mask = A(P, [128, 5, 832], F32)
    P.op("pool", lambda h: h.memset(bias[:], 0.0), writes=[biasb])
    maskb = P.buf()
    _ld(P, "sp", mask[:], dr["n_mask"].rearrange("t p c -> p t c"), [maskb])
    for ti in range(5):
        for qr in range(2):
            for i in range(9):
                _ld(P, "sp" if (i % 2) else "act", bias[qr * 64:(qr + 1) * 64, ti, i * 64:(i + 1) * 64], dr["n_toep"][int(drs[ti, qr, i])], [biasb])
    _tt(P, "dve", bias[:], bias[:], mask[:], ALU.add, [biasb, maskb], [biasb])
    mark = P.a_cur
    hbs = [A(P, [128, KC, 512], BF16) for _ in range(2)]
    hbsb = [P.buf(), P.buf()]
    cnt = 0
    for bi, (t0, n, r) in enumerate(blocks):
        hb, hbb = load_h(P, hT, hTb, hbs, hbsb, bi, t0, n)
        for g, dst, dstb in ((7, nqT, nqb), (8, nkT, nkb)):
            pi_ = cnt % 4
            cnt += 1
            for k in range(KC):
                _mm(P, ps[pi_][0:64, 0:n], wfm[:, k, g * 64:(g + 1) * 64], hb[:, k, 0:n], k == 0, k == KC - 1, [wb, hbb], [psb[pi_]])
            _cp(P, "act" if g == 7 else "dve", dst[:, t0:t0 + n], ps[pi_][0:64, 0:n], [psb[pi_]], [dstb])
        for tt in range(n // 128):
            pi_ = 4 + (tt % 2)
            for k in range(KC):
                _mm(P, ps[pi_][:, 0:64], hb[:, k, tt * 128:(tt + 1) * 128], wtm[:, k, 192:256], k == 0, k == KC - 1, [wb, hbb], [psb[pi_]])
            _cp(P, "act", nvT[:, t0 // 128 + tt, :], ps[pi_][:, 0:64], [psb[pi_]], [nvb])
    barrier(P)
    P.a_cur = mark
    NB4 = 4
    s_t = [A(P, [128, 832], F32) for _ in range(NB4)]; s_b = [P.buf() for _ in range(NB4)]
    p_t = [A(P, [128, 832], BF16) for _ in range(NB4)]; p_b = [P.buf() for _ in range(NB4)]
    pT = [A(P, [128, 7, 128], BF16) for _ in range(NB4)]; pTb = [P.buf() for _ in range(NB4)]
    st_ = [A(P, [128, 4], F32) for _ in range(NB4)]; stb = [P.buf() for _ in range(NB4)]
    SC = 0.125

    def softmax_pv(qi, ncols, pv_list, o_ps, o_psb, o_cols, sbi, s4=None):
        if s4 is None:
            s4 = sbi
        s, sb_ = s_t[s4], s_b[s4]
        sm, smb = st_[s4], stb[s4]
        P.op("dve", lambda h: h.reduce_max(out=sm[:, 1:2], in_=s[:, 0:ncols], axis=AX.X, negate=True), reads=[sb_], writes=[smb])
        _act(P, s[:, 0:ncols], s[:, 0:ncols], AF.Exp, [sb_, smb], [sb_], bias=sm[:, 1:2], scale=1.0)
        P.op("dve", lambda h: h.reduce_sum(out=sm[:, 2:3], in_=s[:, 0:ncols], axis=AX.X), reads=[sb_], writes=[smb])
        P.op("dve", lambda h: h.reciprocal(out=sm[:, 3:4], in_=sm[:, 2:3]), reads=[smb], writes=[smb])
        p, pb = p_t[s4], p_b[s4]
        _ts(P, "dve", p[:, 0:ncols], s[:, 0:ncols], sm[:, 3:4], None, ALU.mult, None, [sb_, smb], [pb])
        tp = ps[4 + sbi][:, :].bitcast(BF16)
        for ci, (c0, nk, tile) in enumerate(pv_list):
            _tr(P, tp[0:nk, ci * 128:(ci + 1) * 128], p[:, c0:c0 + nk], ident[:, :], [pb, cb], [psb[4 + sbi]])
        nchk = len(pv_list)
        pt_, ptb = pT[s4], pTb[s4]
        _cp(P, "act", pt_[:, 0:nchk, :], tp[:, 0:nchk * 128].rearrange("p (a b) -> p a b", b=128), [psb[4 + sbi]], [ptb])
        for ci, (c0, nk, tile) in enumerate(pv_list):
            _mm(P, o_ps[0:64, o_cols:o_cols + 128], nvT[0:nk, tile, :], pt_[0:nk, ci, :], ci == 0, ci == nchk - 1, [nvb, ptb], [o_psb])

    def tile_geo(rp):
        ti = {0: 0, 1: 1, 62: 3, 63: 4}.get(rp, 2)
        r0 = 2 * rp
        if ti == 2:
            R0, nr = r0 - 4, 9
        else:
            R0, nr = types[ti][1], 8
        t_base = 2 + R0 // 2
        pv = [(128 * j, 128, t_base + j) for j in range(4)]
        if nr == 9:
            pv.append((512, 64, t_base + 4))
        pv += [(576, 128, 0), (704, 128, 1)]
        return ti, R0, nr, pv

    def stage_S(rp):
        ti, R0, nr, pv = tile_geo(rp)
        tq = 256 + 128 * rp
        kb_ = 256 + 64 * R0
        sbi = rp % 2
        s1, s2 = ps[sbi], ps[2 + sbi]
        _mm(P, s1[:, 0:512], nqT[:, tq:tq + 128], nkT[:, kb_:kb_ + 512], True, True, [nqb, nkb], [psb[sbi]])
        kb2 = kb_ + 512 if nr == 9 else kb_
        _mm(P, s2[:, 0:64], nqT[:, tq:tq + 128], nkT[:, kb2:kb2 + 64], True, True, [nqb, nkb], [psb[2 + sbi]])
        _mm(P, s2[:, 64:320], nqT[:, tq:tq + 128], nkT[:, 0:256], True, True, [nqb, nkb], [psb[2 + sbi]])
        s4 = rp % NB4
        s = s_t[s4]
        _stt(P, s[:, 0:512], s1[:, 0:512], SC, bias[:, ti, 0:512], ALU.mult, ALU.add, [psb[sbi], biasb], [s_b[s4]])
        _stt(P, s[:, 512:832], s2[:, 0:320], SC, bias[:, ti, 512:832], ALU.mult, ALU.add, [psb[2 + sbi], biasb], [s_b[s4]])

    def stage_M(rp, ncols=832):
        s4 = rp % NB4
        s, sb_ = s_t[s4], s_b[s4]
        sm, smb = st_[s4], stb[s4]
        P.op("dve", lambda h: h.reduce_max(out=sm[:, 1:2], in_=s[:, 0:ncols], axis=AX.X, negate=True), reads=[sb_], writes=[smb])
        _act(P, s[:, 0:ncols], s[:, 0:ncols], AF.Exp, [sb_, smb], [sb_], bias=sm[:, 1:2], scale=1.0)
        P.op("dve", lambda h: h.reduce_sum(out=sm[:, 2:3], in_=s[:, 0:ncols], axis=AX.X), reads=[sb_], writes=[smb])
        P.op("dve", lambda h: h.reciprocal(out=sm[:, 3:4], in_=sm[:, 2:3]), reads=[smb], writes=[smb])
        _ts(P, "dve", p_t[s4][:, 0:ncols], s[:, 0:ncols], sm[:, 3:4], None, ALU.mult, None, [sb_, smb], [p_b[s4]])

    def stage_TV(rp):
        ti, R0, nr, pv_list = tile_geo(rp)
        sbi = rp % 2
        s4 = rp % NB4
        p, pb = p_t[s4], p_b[s4]
        tp = ps[4 + sbi][:, :].bitcast(BF16)
        for ci, (c0, nk, tile) in enumerate(pv_list):
            _tr(P, tp[0:nk, ci * 128:(ci + 1) * 128], p[:, c0:c0 + nk], ident[:, :], [pb, cb], [psb[4 + sbi]])
        nchk = len(pv_list)
        pt_, ptb = pT[s4], pTb[s4]
        _cp(P, "act", pt_[:, 0:nchk, :], tp[:, 0:nchk * 128].rearrange("p (a b) -> p a b", b=128), [psb[4 + sbi]], [ptb])
        jj = rp % 4
        for ci, (c0, nk, tile) in enumerate(pv_list):
            _mm(P, ps[6][0:64, jj * 128:(jj + 1) * 128], nvT[0:nk, tile, :], pt_[0:nk, ci, :], ci == 0, ci == nchk - 1, [nvb, ptb], [psb[6]])
        if jj == 3:
            _cp(P, "dve", nT[:, 256 + 512 * (rp // 4):256 + 512 * (rp // 4 + 1)], ps[6][0:64, 0:512], [psb[6]], [nTb])

    stage_S(0)
    stage_S(1)
    stage_M(0)
    for rp in range(64):
        if rp + 2 < 64:
            stage_S(rp + 2)
        if rp + 1 < 64:
            stage_M(rp + 1)
        stage_TV(rp)
    if need_ctx_out:
        for qt in range(2):
            sbi = qt
            _mm(P, ps[sbi][:, 0:256], nqT[:, qt * 128:(qt + 1) * 128], nkT[:, 0:256], True, True, [nqb, nkb], [psb[sbi]])
            _ts(P, "dve", s_t[sbi][:, 0:256], ps[sbi][:, 0:256], SC, None, ALU.mult, None, [psb[sbi]], [s_b[sbi]])
            softmax_pv(qt, 256, [(0, 128, 0), (128, 128, 1)], ps[7], psb[7], qt * 128, sbi)
        _cp(P, "dve", nT[:, 0:256], ps[7][0:64, 0:256], [psb[7]], [nTb])
    else:
        P.op("dve", lambda h: h.memset(nT[:, 0:256], 0.0), writes=[nTb])
    outs.append(_ld(P, "sp", out[3, :, :], nT[:, :], [P.buf()], reads=[nTb]))


def ret_part(P, dr, ps, psb, hT, hTb, wfm, wtm, wb, blocks, out, outs, need_ctx_out, cb, ones):
    KS = 0.125
    qT = A(P, [64, NTOK], BF16); kT = A(P, [64, NTOK], BF16); gT = A(P, [64, NTOK], BF16)
    qb_, kb_, gb_ = P.buf("q"), P.buf("k"), P.buf("g")
    rvT = A(P, [128, NCH, 64], BF16); rvb = P.buf("rv")
    Sbf = [A(P, [64, NCH, 64], BF16) for _ in range(2)]
    rc = P.buf("retc")
    dec = A(P, [128, 2], F32); lg = A(P, [128, 2], F32); jcol = A(P, [128, 2], F32); kdec = A(P, [128, 2], F32); g128 = A(P, [128, 2], F32)
    dist = A(P, [128, 128], F32); msk = A(P, [128, 2, 128], F32); DT = A(P, [128, 128], F32); DT2 = A(P, [128, 128], F32)
    irow = A(P, [64, 2, 128], F32); qdec = A(P, [64, 2, 128], F32)
    gn = A(P, [64, 1], F32); o64 = A(P, [64, 64], F32)
    _ld(P, "sp", dec[:], dr["r_dec"][:, :], [rc])
    _ld(P, "sp", jcol[:], dr["r_jcol"][:, :], [rc])
    _ld(P, "sp", dist[:], dr["r_dist"][:, :], [rc])
    _ld(P, "sp", msk[:], dr["r_mask"].rearrange("d j i -> j d i"), [rc])
    _ld(P, "sp", irow[:], dr["r_irow"].rearrange("d p i -> p d i"), [rc])
    _ld(P, "sp", gn[:], dr["r_gn"][:, :], [rc])
    P.op("dve", lambda h: h.memset(o64[:], 1.0 / 64), writes=[rc])
    _act(P, lg[:], dec[:], AF.Exp, [rc], [rc], scale=-1.0)
    _act(P, lg[:], lg[:], AF.Ln, [rc], [rc], bias=P.one_t[:, 0:1], scale=1.0)
    _ts(P, "dve", lg[:], lg[:], -1.0, None, ALU.mult, None, [rc], [rc])
    for dd in range(2):
        _act(P, kdec[:, dd:dd + 1], jcol[:, dd:dd + 1], AF.Exp, [rc], [rc], scale=lg[:, dd:dd + 1])
        _act(P, g128[:, dd:dd + 1], lg[:, dd:dd + 1], AF.Exp, [rc], [rc], scale=128.0)
        _act(P, qdec[:, dd, :], irow[:, dd, :], AF.Exp, [rc], [rc], scale=lg[0:64, dd:dd + 1])
    _ts(P, "dve", kdec[:], kdec[:], KS, None, ALU.mult, None, [rc], [rc])
    _act(P, DT[:], dist[:], AF.Exp, [rc], [rc], scale=lg[:, 0:1])
    _tt(P, "dve", DT[:], DT[:], msk[:, 0, :], ALU.mult, [rc], [rc])
    _act(P, DT2[:], dist[:], AF.Exp, [rc], [rc], scale=lg[:, 1:2])
    _tt(P, "dve", DT2[:], DT2[:], msk[:, 1, :], ALU.mult, [rc], [rc])
    _tt(P, "dve", DT[:], DT[:], DT2[:], ALU.add, [rc], [rc])
    if RET_STOP <= 0:
        return
    mark_k = P.a_cur
    kd = [A(P, [128, NCH, 64], BF16) for _ in range(2)]
    kdb = [P.buf(), P.buf()]
    mark = P.a_cur
    hbs = [A(P, [128, KC, 512], BF16) for _ in range(2)]
    hbsb = [P.buf(), P.buf()]
    cF = [A(P, [64, 512], F32) for _ in range(2)]; sF = [A(P, [64, 512], F32) for _ in range(2)]
    cTt = [A(P, [128, 4, 64], F32) for _ in range(2)]; sTt = [A(P, [128, 4, 64], F32) for _ in range(2)]
    tabb = [P.buf(), P.buf()]
    t1 = [A(P, [128, 512], F32) for _ in range(2)]; t1b = [P.buf(), P.buf()]
    t2 = [A(P, [128, 512], F32) for _ in range(2)]; t2b = [P.buf(), P.buf()]
    cnt = 0
    for bi, (t0, n, r) in enumerate(blocks):
        hb, hbb = load_h(P, hT, hTb, hbs, hbsb, bi, t0, n)
        lat = (r == 0)
        tb_ = tabb[bi % 2]
        if lat:
            m0 = t0 - 256
            _ld(P, "sp", cF[bi % 2][:, :], dr["r_cosF"][:, m0:m0 + 512], [tb_])
            _ld(P, "sp", sF[bi % 2][:, :], dr["r_sinF"][:, m0:m0 + 512], [tb_])
            _ld(P, "sp", cTt[bi % 2][:, :, :], dr["r_cosT"][:, m0 // 128:m0 // 128 + 4, :], [tb_])
            _ld(P, "sp", sTt[bi % 2][:, :, :], dr["r_sinT"][:, m0 // 128:m0 // 128 + 4, :], [tb_])

        def proj(g, pi_):
            for k in range(KC):
                _mm(P, ps[pi_][0:64, 0:n], wfm[:, k, g * 64:(g + 1) * 64], hb[:, k, 0:n], k == 0, k == KC - 1, [wb, hbb], [psb[pi_]])
        for (g, gsw, dst, dstb, scl) in ((2, 4, qT, qb_, 1.0), (3, 5, kT, kb_, KS)):
            if 'qk' in SKIP:
                continue
            proj(g, 0)
            if lat:
                proj(gsw, 1)
                i2 = cnt % 2
                cnt += 1
                _stt(P, t1[i2][0:64, 0:n], ps[0][0:64, 0:n], scl, cF[bi % 2][:, 0:n], ALU.mult, ALU.mult, [psb[0], tb_], [t1b[i2]])
                _stt(P, t2[i2][0:64, 0:n], ps[1][0:64, 0:n], scl, sF[bi % 2][:, 0:n], ALU.mult, ALU.mult, [psb[1], tb_], [t2b[i2]])
                _tt(P, "pool", dst[:, t0:t0 + n], t1[i2][0:64, 0:n], t2[i2][0:64, 0:n], ALU.add, [t1b[i2], t2b[i2]], [dstb])
            else:
                _act(P, dst[:, t0:t0 + n], ps[0][0:64, 0:n], AF.Copy, [psb[0]], [dstb], scale=scl)
        proj(6, 2)
        _cp(P, "act", gT[:, t0:t0 + n], ps[2][0:64, 0:n], [psb[2]], [gb_])
        for tt in range(n // 128):
            if 'tm' in SKIP:
                continue
            pi_ = 4 + (tt % 2)
            tile = t0 // 128 + tt
            for k in range(KC):
                _mm(P, ps[pi_][:, 0:192], hb[:, k, tt * 128:(tt + 1) * 128], wtm[:, k, 0:192], k == 0, k == KC - 1, [wb, hbb], [psb[pi_]])
            _cp(P, "act", rvT[:, tile, :], ps[pi_][:, 128:192], [psb[pi_]], [rvb])
            if 'kd' in SKIP:
                continue
            if ('kl' in SKIP and lat) or ('kc' in SKIP and not lat):
                continue
            if lat:
                i2 = cnt % 2
                cnt += 1
                _tt(P, "dve", t1[i2][:, 0:64], ps[pi_][:, 0:64], cTt[bi % 2][:, tt, :], ALU.mult, [psb[pi_], tb_], [t1b[i2]])
                _tt(P, "dve", t2[i2][:, 0:64], ps[pi_][:, 64:128], sTt[bi % 2][:, tt, :], ALU.mult, [psb[pi_], tb_], [t2b[i2]])
                if 'k1' in SKIP:
                    continue
                _tt(P, "dve", t1[i2][:, 0:64], t1[i2][:, 0:64], t2[i2][:, 0:64], ALU.add, [t1b[i2], t2b[i2]], [t1b[i2]])
                if 'k2' in SKIP:
                    continue
                for dd in range(2):
                    _act(P, kd[dd][:, tile, :], t1[i2][:, 0:64], AF.Identity, [t1b[i2], rc], [kdb[dd]], scale=kdec[:, dd:dd + 1])
            else:
                for dd in range(2):
                    _act(P, kd[dd][:, tile, :], ps[pi_][:, 0:64], AF.Identity, [psb[pi_], rc], [kdb[dd]], scale=kdec[:, dd:dd + 1])
    barrier(P)
    P.a_cur = mark
    if RET_STOP <= 1:
        return
    S32 = [A(P, [64, NCH, 64], F32) for _ in range(2)]
    Sb = [P.buf(), P.buf()]
    orders = []
    for dd in range(2):
        pos = pos_of(dd)
        orders.append(sorted(range(NCH), key=lambda c, pos=pos: pos[c]))
        c0 = orders[dd][0]
        P.op("dve", lambda h, dd=dd, c0=c0: h.memset(S32[dd][:, c0, :], 0.0), writes=[Sb[dd]])
    for idx in range(NCH - 1):
        for dd in range(2):
            c, nxt = orders[dd][idx], orders[dd][idx + 1]
            pi_ = 2 * dd + (idx // 8) % 2
            sl = idx % 8
            _mm(P, ps[pi_][0:64, sl * 64:(sl + 1) * 64], kd[dd][:, c, :], rvT[:, c, :], True, True, [kdb[dd], rvb], [psb[pi_]])
            _stt(P, S32[dd][:, nxt, :], S32[dd][:, c, :], g128[0:64, dd:dd + 1], ps[pi_][0:64, sl * 64:(sl + 1) * 64], ALU.mult, ALU.add, [Sb[dd], psb[pi_], rc], [Sb[dd]])
    for dd in range(2):
        _cp(P, "act", Sbf[dd][:], S32[dd][:], [Sb[dd]], [Sb[dd]])
    barrier(P)
    P.a_cur = mark_k
    if RET_STOP <= 2:
        return
    oT = A(P, [64, NTOK], F32); oTb = P.buf("oT")
    sc = [A(P, [128, 128], BF16) for _ in range(2)]; scb = [P.buf(), P.buf()]
    qd = [[A(P, [64, 128], BF16) for _ in range(2)] for _ in range(2)]
    qdb = [[P.buf(), P.buf()] for _ in range(2)]
    c_start = 0 if need_ctx_out else 2
    if not need_ctx_out:
        P.op("pool", lambda h: h.memset(oT[:, 0:256], 0.0), writes=[oTb])
    def ret_S(c):
        tau = 128 * c
        i2 = c % 2
        _mm(P, ps[i2][:, 0:128], kT[:, tau:tau + 128], qT[:, tau:tau + 128], True, True, [kb_, qb_], [psb[i2]])
        _tt(P, "dve", sc[i2][:, :], ps[i2][:, 0:128], DT[:, :], ALU.mult, [psb[i2], rc], [scb[i2]])
        for dd in range(2):
            _tt(P, "pool", qd[dd][i2][:, :], qT[:, tau:tau + 128], qdec[:, dd, :], ALU.mult, [qb_, rc], [qdb[dd][i2]])

    def ret_V(c):
        i2 = c % 2
        jj = c % 4
        po = ps[4 + (c // 4) % 2]
        pob = psb[4 + (c // 4) % 2]
        _mm(P, po[0:64, jj * 128:(jj + 1) * 128], rvT[:, c, :], sc[i2][:, :], True, False, [rvb, scb[i2]], [pob])
        _mm(P, po[0:64, jj * 128:(jj + 1) * 128], Sbf[0][:, c, :], qd[0][i2][:, :], False, False, [Sb[0], qdb[0][i2]], [pob])
        _mm(P, po[0:64, jj * 128:(jj + 1) * 128], Sbf[1][:, c, :], qd[1][i2][:, :], False, True, [Sb[1], qdb[1][i2]], [pob])
        if jj == 3 or c == NCH - 1:
            b0 = (c // 4) * 512
            wid = (jj + 1) * 128
            lo = 0
            if (not need_ctx_out) and c // 4 == 0:
                lo = 256
            _cp(P, "act", oT[:, b0 + lo:b0 + wid], po[0:64, lo:wid], [pob], [oTb])

    ret_S(c_start)
    for c in range(c_start, NCH):
        if c + 1 < NCH:
            ret_S(c + 1)
        ret_V(c)
    if RET_STOP <= 3:
        return
    rT = A(P, [64, NTOK], BF16); rTb = P.buf("rT")
    o64b = A(P, [64, 64], BF16)
    P.op("dve", lambda h: h.memset(o64b[:], 1.0 / 64), writes=[rc])
    obf = [A(P, [64, 512], BF16) for _ in range(2)]; obfb = [P.buf(), P.buf()]
    cen = [A(P, [64, 512], F32) for _ in range(2)]; cenb = [P.buf(), P.buf()]
    sq_ = [A(P, [64, 512], BF16) for _ in range(2)]; sqb_ = [P.buf(), P.buf()]
    rs_ = [A(P, [64, 512], F32) for _ in range(2)]; rsb_ = [P.buf(), P.buf()]
    sg_ = [A(P, [64, 512], F32) for _ in range(2)]; sgb_ = [P.buf(), P.buf()]
    nblk = (NTOK + 511) // 512

    def hn_A(bi):
        t0 = bi * 512
        n = min(512, NTOK - t0)
        i2 = bi % 2
        _cp(P, "act", obf[i2][:, 0:n], oT[:, t0:t0 + n], [oTb], [obfb[i2]])
        _mm(P, ps[2 + i2][0:64, 0:n], o64b[:, :], obf[i2][:, 0:n], True, True, [obfb[i2], rc], [psb[2 + i2]])
        _tt(P, "dve", cen[i2][:, 0:n], oT[:, t0:t0 + n], ps[2 + i2][0:64, 0:n], ALU.subtract, [oTb, psb[2 + i2]], [cenb[i2]])
        _act(P, sq_[i2][:, 0:n], cen[i2][:, 0:n], AF.Square, [cenb[i2]], [sqb_[i2]])
        _act(P, sg_[i2][:, 0:n], gT[:, t0:t0 + n], AF.Silu, [gb_], [sgb_[i2]])

    def hn_B(bi):
        t0 = bi * 512
        n = min(512, NTOK - t0)
        i2 = bi % 2
        _mm(P, ps[6 + i2][0:64, 0:n], o64b[:, :], sq_[i2][:, 0:n], True, True, [sqb_[i2], rc], [psb[6 + i2]])
        _act(P, rs_[i2][:, 0:n], ps[6 + i2][0:64, 0:n], AF.Ln, [psb[6 + i2]], [rsb_[i2]], bias=P.eps_t[0:64, 0:1], scale=1.0)
        _act(P, rs_[i2][:, 0:n], rs_[i2][:, 0:n], AF.Exp, [rsb_[i2]], [rsb_[i2]], scale=-0.5)
        _tt(P, "dve", cen[i2][:, 0:n], cen[i2][:, 0:n], rs_[i2][:, 0:n], ALU.mult, [cenb[i2], rsb_[i2]], [cenb[i2]])
        _stt(P, rT[:, t0:t0 + n], cen[i2][:, 0:n], gn[:, 0:1], sg_[i2][:, 0:n], ALU.mult, ALU.mult, [cenb[i2], sgb_[i2], rc], [rTb])

    hn_A(0)
    for bi in range(nblk):
        if bi + 1 < nblk:
            hn_A(bi + 1)
        hn_B(bi)
    outs.append(_ld(P, "sp", out[2, :, :], rT[:, :], [P.buf()], reads=[rTb]))


def s5_part(P, dr, ps, psb, sT, sTb, out, outs, need_ctx_out, cb):
    pb = P.buf("s5param")

    def cplx_prep(are, aim, ldt, mk):
        T = {k: mk() for k in ("dt", "ar", "ai", "mag", "ph", "tmp", "sn", "cs", "abr", "abi", "nr", "den", "cr", "ci", "u")}
        ident_v = lambda t: t
        _act(P, T["dt"], ldt, AF.Exp, [pb], [pb])
        _tt(P, "dve", T["ar"], are, T["dt"], ALU.mult, [pb], [pb])
        _tt(P, "dve", T["ai"], aim, T["dt"], ALU.mult, [pb], [pb])
        _act(P, T["mag"], T["ar"], AF.Exp, [pb], [pb])
        _cp(P, "dve", T["ph"], T["ai"], [pb], [pb])
        range_reduce_sincos(P, T["ph"], T["sn"], T["cs"], T["tmp"], ident_v, pb)
        _tt(P, "dve", T["abr"], T["mag"], T["cs"], ALU.mult, [pb], [pb])
        _tt(P, "dve", T["abi"], T["mag"], T["sn"], ALU.mult, [pb], [pb])
        _ts(P, "dve", T["nr"], T["abr"], -1.0, None, ALU.add, None, [pb], [pb])
        _tt(P, "dve", T["den"], are, are, ALU.mult, [pb], [pb])
        _tt(P, "dve", T["u"], aim, aim, ALU.mult, [pb], [pb])
        _tt(P, "dve", T["den"], T["den"], T["u"], ALU.add, [pb], [pb])
        P.op("dve", lambda h: h.reciprocal(out=T["den"], in_=T["den"]), reads=[pb], writes=[pb])
        _tt(P, "dve", T["cr"], T["nr"], are, ALU.mult, [pb], [pb])
        _tt(P, "dve", T["u"], T["abi"], aim, ALU.mult, [pb], [pb])
        _tt(P, "dve", T["cr"], T["cr"], T["u"], ALU.add, [pb], [pb])
        _tt(P, "dve", T["cr"], T["cr"], T["den"], ALU.mult, [pb], [pb])
        _tt(P, "dve", T["ci"], T["abi"], are, ALU.mult, [pb], [pb])
        _tt(P, "dve", T["u"], T["nr"], aim, ALU.mult, [pb], [pb])
        _tt(P, "dve", T["ci"], T["ci"], T["u"], ALU.subtract, [pb], [pb])
        _tt(P, "dve", T["ci"], T["ci"], T["den"], ALU.mult, [pb], [pb])
        return T

    p_sm = A(P, [128, 2, 2, 3], F32); p_row = A(P, [128, 2, 3, 256], F32); p_hs = A(P, [64, 2, 3, 64], F32)
    Bhs = A(P, [64, 2, 2, 64], F32); Csm = A(P, [128, 2, 2, 2, 16], F32); dvec = A(P, [64, 1], F32)
    jrow = A(P, [128, 129], F32); jcol = A(P, [128, 1], F32); njcol = A(P, [128, 1], F32)
    LT = A(P, [128, 2, 128], BF16); mrow = A(P, [64, 4], F32); msm = A(P, [128, 2, 4], F32)
    for t_, src in ((p_sm[:], dr["s_sm"][:, :, :, :]), (p_row[:], dr["s_row"][:, :, :, :]), (p_hs[:], dr["s_hs"][:, :, :, :]), (Bhs[:], dr["s_B"][:, :, :, :]),
                    (Csm[:], dr["s_C"][:, :, :, :, :]), (dvec[:], dr["s_d"][:, :]), (jrow[:], dr["s_jrow"][:, :]), (jcol[:], dr["s_jcol"][:, :]),
                    (LT[:], dr["s_LT"].rearrange("d j i -> j d i")), (mrow[:], dr["s_mrow"][:, :]), (msm[:], dr["s_msm"][:, :, :])):
        _ld(P, "sp", t_, src, [pb])
    _ts(P, "dve", njcol[:], jcol[:], -1.0, None, ALU.mult, None, [pb], [pb])
    ones_col = A(P, [128, 1], BF16)
    P.op("dve", lambda h: h.memset(ones_col[:], 1.0), writes=[pb])

    BD = [A(P, [64, 512], BF16) for _ in range(2)]
    CT = [A(P, [128, 4, 64], BF16) for _ in range(2)]
    mark_prep = P.a_cur
    for dd in range(2):
        P.a_cur = mark_prep
        Ths = cplx_prep(p_hs[:, dd, 0, :], p_hs[:, dd, 1, :], p_hs[:, dd, 2, :], lambda: A(P, [64, 64], F32)[:, :])
        bbr = A(P, [64, 64], F32); bbi = A(P, [64, 64], F32); uu = A(P, [64, 64], F32)
        _tt(P, "dve", bbr[:], Ths["cr"], Bhs[:, dd, 0, :], ALU.mult, [pb], [pb])
        _tt(P, "dve", uu[:], Ths["ci"], Bhs[:, dd, 1, :], ALU.mult, [pb], [pb])
        _tt(P, "dve", bbr[:], bbr[:], uu[:], ALU.subtract, [pb], [pb])
        _tt(P, "dve", bbi[:], Ths["cr"], Bhs[:, dd, 1, :], ALU.mult, [pb], [pb])
        _tt(P, "dve", uu[:], Ths["ci"], Bhs[:, dd, 0, :], ALU.mult, [pb], [pb])
        _tt(P, "dve", bbi[:], bbi[:], uu[:], ALU.add, [pb], [pb])
        for g in range(4):
            _ts(P, "dve", BD[dd][:, g * 64:(g + 1) * 64], bbr[:], mrow[:, g:g + 1], None, ALU.mult, None, [pb], [pb])
            _ts(P, "dve", BD[dd][:, 256 + g * 64:256 + (g + 1) * 64], bbi[:], mrow[:, g:g + 1], None, ALU.mult, None, [pb], [pb])
        for ri in range(2):
            for st in range(2):
                for g in range(4):
                    _ts(P, "dve", CT[dd][:, ri * 2 + st, g * 16:(g + 1) * 16], Csm[:, dd, st, ri, :], msm[:, st, g:g + 1], (1.0 if ri == 0 else -1.0), ALU.mult, ALU.mult, [pb], [pb])
    P.a_cur = mark_prep
    TA = [[A(P, [128, 2, 129], F32) for _ in range(2)] for _ in range(2)]
    TW = [[A(P, [128, 2, 129], F32) for _ in range(2)] for _ in range(2)]
    PRE = [[A(P, [128, 256], F32) for _ in range(2)] for _ in range(2)]
    mark_t = P.a_cur
    for dd in range(2):
        P.a_cur = mark_t
        dt_ = A(P, [128, 2], F32); ar = A(P, [128, 2], F32); ai = A(P, [128, 2], F32); nar = A(P, [128, 2], F32)
        _act(P, dt_[:], p_sm[:, dd, :, 2], AF.Exp, [pb], [pb])
        _tt(P, "dve", ar[:], p_sm[:, dd, :, 0], dt_[:], ALU.mult, [pb], [pb])
        _tt(P, "dve", ai[:], p_sm[:, dd, :, 1], dt_[:], ALU.mult, [pb], [pb])
        _ts(P, "dve", nar[:], ar[:], -1.0, None, ALU.mult, None, [pb], [pb])
        mark_st = P.a_cur
        for st in range(2):
            P.a_cur = mark_st
            ph = A(P, [128, 129], F32); tmp = A(P, [128, 129], F32); sn = A(P, [128, 129], F32); cs = A(P, [128, 129], F32)
            mp = A(P, [128, 129], F32); mn = A(P, [128, 129], F32)
            _ts(P, "dve", ph[:], jrow[:], ai[:, st:st + 1], None, ALU.mult, None, [pb], [pb])
            range_reduce_sincos(P, ph[:], sn[:], cs[:], tmp[:], (lambda t: t), pb)
            _act(P, mp[:], jrow[:], AF.Exp, [pb], [pb], scale=ar[:, st:st + 1])
            _act(P, mn[:], jrow[:], AF.Exp, [pb], [pb], scale=nar[:, st:st + 1])
            _tt(P, "dve", TA[dd][0][:, st, :], mp[:], cs[:], ALU.mult, [pb], [pb])
            _tt(P, "dve", TA[dd][1][:, st, :], mp[:], sn[:], ALU.mult, [pb], [pb])
            _tt(P, "dve", TW[dd][0][:, st, :], mn[:], cs[:], ALU.mult, [pb], [pb])
            _stt(P, TW[dd][1][:, st, :], mn[:], -1.0, sn[:], ALU.mult, ALU.mult, [pb], [pb])
        P.a_cur = mark_t
        dtr = A(P, [128, 256], F32); arr = A(P, [128, 256], F32); air = A(P, [128, 256], F32)
        ph = A(P, [128, 256], F32); tmp = A(P, [128, 256], F32); sn = A(P, [128, 256], F32); cs = A(P, [128, 256], F32); mg = A(P, [128, 256], F32)
        _act(P, dtr[:], p_row[:, dd, 2, :], AF.Exp, [pb], [pb])
        _tt(P, "dve", arr[:], p_row[:, dd, 0, :], dtr[:], ALU.mult, [pb], [pb])
        _tt(P, "dve", air[:], p_row[:, dd, 1, :], dtr[:], ALU.mult, [pb], [pb])
        _ts(P, "dve", ph[:], air[:], jcol[:, 0:1], None, ALU.mult, None, [pb], [pb])
        range_reduce_sincos(P, ph[:], sn[:], cs[:], tmp[:], (lambda t: t), pb)
        _act(P, mg[:], arr[:], AF.Exp, [pb], [pb], scale=(njcol if dd == 0 else jcol)[:, 0:1])
        _tt(P, "dve", PRE[dd][0][:], mg[:], cs[:], ALU.mult, [pb], [pb])
        _stt(P, PRE[dd][1][:], mg[:], (-1.0 if dd == 0 else 1.0), sn[:], ALU.mult, ALU.mult, [pb], [pb])
        P.a_cur = mark_t
    barrier(P)
    P.a_cur = mark_t
    Xt = A(P, [128, NCH, 512], BF16); Xtb = [P.buf() for _ in range(NCH)]
    yacc = A(P, [64, NTOK], F32); yb = P.buf("yacc")
    E = A(P, [128, 4, NCH], F32); Eb = P.buf("E")
    H = [[A(P, [128, 2, NCH], F32) for _ in range(2)] for _ in range(2)]
    Hb = P.buf("H")
    cv = [A(P, [128, 2, NCH], F32) for _ in range(2)]
    pw = A(P, [128, 2, 8], F32)
    tq = [A(P, [128, 256], F32) for _ in range(8)]; tqb = [P.buf() for _ in range(8)]
    hs_ = [A(P, [128, 4, 128], BF16) for _ in range(2)]; hsb = [P.buf(), P.buf()]
    uq = [A(P, [128, 128], F32) for _ in range(16)]; uqb = [P.buf() for _ in range(16)]
    for dd in range(2):
        pos = pos_of(dd)
        def s1_X(c):
            tau = 128 * c
            px = ps[c % 2]; pxb = psb[c % 2]
            _mm(P, px[:, 0:512], sT[:, tau:tau + 128], BD[dd][:, :], True, True, [sTb, pb], [pxb])
            i2 = (c % 2) * 4
            _tt(P, "dve", tq[i2][:, :], px[:, 0:256], PRE[dd][0][:, :], ALU.mult, [pxb, pb], [tqb[i2]])
            _tt(P, "dve", tq[i2 + 1][:, :], px[:, 256:512], PRE[dd][1][:, :], ALU.mult, [pxb, pb], [tqb[i2 + 1]])
            _tt(P, "dve", tq[i2 + 2][:, :], px[:, 0:256], PRE[dd][1][:, :], ALU.mult, [pxb, pb], [tqb[i2 + 2]])
            _tt(P, "dve", tq[i2 + 3][:, :], px[:, 256:512], PRE[dd][0][:, :], ALU.mult, [pxb, pb], [tqb[i2 + 3]])
            _tt(P, "pool", Xt[:, c, 0:256], tq[i2][:, :], tq[i2 + 1][:, :], ALU.subtract, [tqb[i2], tqb[i2 + 1]], [Xtb[c]])
            _tt(P, "pool", Xt[:, c, 256:512], tq[i2 + 2][:, :], tq[i2 + 3][:, :], ALU.add, [tqb[i2 + 2], tqb[i2 + 3]], [Xtb[c]])

        def s1_E(c):
            for tl in range(4):
                col = tl * NCH + pos[c]
                _mm(P, ps[6][:, col:col + 1], Xt[:, c, tl * 128:(tl + 1) * 128], ones_col[:, :], True, True, [Xtb[c], pb], [psb[6]])

        s1_X(0)
        for c in range(NCH):
            if c + 1 < NCH:
                s1_X(c + 1)
            s1_E(c)
        _cp(P, "dve", E[:], ps[6][:, 0:4 * NCH].rearrange("p (a b) -> p a b", b=NCH), [psb[6]], [Eb])
        H0r, H0i = H[0][0], H[0][1]
        if dd == 0:
            for st in range(2):
                a_r, a_i = TA[0][0][:, st, 127:128], TA[0][1][:, st, 127:128]
                _ts(P, "dve", uq[0][:, 0:NCH], E[:, 2 + st, :], a_i, None, ALU.mult, None, [Eb, pb], [uqb[0]])
                _stt(P, H0r[:, st, :], E[:, st, :], a_r, uq[0][:, 0:NCH], ALU.mult, ALU.subtract, [Eb, pb, uqb[0]], [Hb])
                _ts(P, "dve", uq[1][:, 0:NCH], E[:, st, :], a_i, None, ALU.mult, None, [Eb, pb], [uqb[1]])
                _stt(P, H0i[:, st, :], E[:, 2 + st, :], a_r, uq[1][:, 0:NCH], ALU.mult, ALU.add, [Eb, pb, uqb[1]], [Hb])
        else:
            _cp(P, "dve", H0r[:], E[:, 0:2, :], [Eb], [Hb])
            _cp(P, "dve", H0i[:], E[:, 2:4, :], [Eb], [Hb])
        _cp(P, "dve", pw[:, :, 0], TA[dd][0][:, :, 128], [pb], [Hb])
        _cp(P, "dve", pw[:, :, 1], TA[dd][1][:, :, 128], [pb], [Hb])
        cur = 0
        d = 1
        while d < NCH:
            _ts(P, "dve", pw[:, :, 2], pw[:, :, 1], -1.0, None, ALU.mult, None, [Hb], [Hb])
            o_, n_ = H[cur], H[1 - cur]
            for ri in range(2):
                _cp(P, "dve", n_[ri][:, :, 0:d], o_[ri][:, :, 0:d], [Hb], [Hb])
            for st in range(2):
                pr, pi, npi = pw[:, st, 0:1], pw[:, st, 1:2], pw[:, st, 2:3]
                m = NCH - d
                _stt(P, uq[0][:, 0:m], o_[0][:, st, 0:m], pr, o_[0][:, st, d:NCH], ALU.mult, ALU.add, [Hb], [uqb[0]])
                _stt(P, n_[0][:, st, d:NCH], o_[1][:, st, 0:m], npi, uq[0][:, 0:m], ALU.mult, ALU.add, [Hb, uqb[0]], [Hb])
                _stt(P, uq[1][:, 0:m], o_[1][:, st, 0:m], pr, o_[1][:, st, d:NCH], ALU.mult, ALU.add, [Hb], [uqb[1]])
                _stt(P, n_[1][:, st, d:NCH], o_[0][:, st, 0:m], pi, uq[1][:, 0:m], ALU.mult, ALU.add, [Hb, uqb[1]], [Hb])
            _tt(P, "dve", pw[:, :, 3], pw[:, :, 0], pw[:, :, 0], ALU.mult, [Hb], [Hb])
            _tt(P, "dve", pw[:, :, 4], pw[:, :, 1], pw[:, :, 1], ALU.mult, [Hb], [Hb])
            _tt(P, "dve", pw[:, :, 5], pw[:, :, 0], pw[:, :, 1], ALU.mult, [Hb], [Hb])
            _tt(P, "dve", pw[:, :, 0], pw[:, :, 3], pw[:, :, 4], ALU.subtract, [Hb], [Hb])
            _ts(P, "dve", pw[:, :, 1], pw[:, :, 5], 2.0, None, ALU.mult, None, [Hb], [Hb])
            cur = 1 - cur
            d *= 2
        Hf = H[cur]
        kidx = 1 if dd == 0 else 128
        P.op("dve", lambda h: h.memset(cv[0][:, :, 0:1], 0.0), writes=[Hb])
        P.op("dve", lambda h: h.memset(cv[1][:, :, 0:1], 0.0), writes=[Hb])
        for st in range(2):
            a_r, a_i = TA[dd][0][:, st, kidx:kidx + 1], TA[dd][1][:, st, kidx:kidx + 1]
            m = NCH - 1
            _ts(P, "dve", uq[0][:, 0:m], Hf[1][:, st, 0:m], a_i, None, ALU.mult, None, [Hb, pb], [uqb[0]])
            _stt(P, cv[0][:, st, 1:NCH], Hf[0][:, st, 0:m], a_r, uq[0][:, 0:m], ALU.mult, ALU.subtract, [Hb, pb, uqb[0]], [Hb])
            _ts(P, "dve", uq[1][:, 0:m], Hf[0][:, st, 0:m], a_i, None, ALU.mult, None, [Hb, pb], [uqb[1]])
            _stt(P, cv[1][:, st, 1:NCH], Hf[1][:, st, 0:m], a_r, uq[1][:, 0:m], ALU.mult, ALU.add, [Hb, pb, uqb[1]], [Hb])
        Tt = TA[dd] if dd == 0 else TW[dd]
        c_start = 0 if need_ctx_out else 2
        def s2_G(c):
            pg = ps[2 + c % 2]; pgb = psb[2 + c % 2]
            for tl in range(4):
                _mm(P, pg[:, tl * 128:(tl + 1) * 128], Xt[:, c, tl * 128:(tl + 1) * 128], LT[:, dd, :], True, True, [Xtb[c], pb], [pgb])
            hh, hhb = hs_[c % 2], hsb[c % 2]
            pc = pos[c]
            for st in range(2):
                gr, gi = pg[:, st * 128:(st + 1) * 128], pg[:, (2 + st) * 128:(3 + st) * 128]
                c_r, c_i = cv[0][:, st, pc:pc + 1], cv[1][:, st, pc:pc + 1]
                Tr, Ti = Tt[0][:, st, 0:128], Tt[1][:, st, 0:128]
                u0 = ((c % 2) * 2 + st) * 4
                _stt(P, uq[u0][:, :], gr, c_r, Tr, ALU.add, ALU.mult, [pgb, Hb, pb], [uqb[u0]])
                _stt(P, uq[u0 + 1][:, :], gi, c_i, Ti, ALU.add, ALU.mult, [pgb, Hb, pb], [uqb[u0 + 1]])
                _stt(P, uq[u0 + 2][:, :], gi, c_i, Tr, ALU.add, ALU.mult, [pgb, Hb, pb], [uqb[u0 + 2]])
                _stt(P, uq[u0 + 3][:, :], gr, c_r, Ti, ALU.add, ALU.mult, [pgb, Hb, pb], [uqb[u0 + 3]])
                _tt(P, "pool", hh[:, st, :], uq[u0][:, :], uq[u0 + 1][:, :], ALU.subtract, [uqb[u0], uqb[u0 + 1]], [hhb])
                _tt(P, "pool", hh[:, 2 + st, :], uq[u0 + 2][:, :], uq[u0 + 3][:, :], ALU.add, [uqb[u0 + 2], uqb[u0 + 3]], [hhb])

        def s2_Y(c):
            hh, hhb = hs_[c % 2], hsb[c % 2]
            jj = c % 4
            py = ps[4 + (c // 4) % 2]; pyb = psb[4 + (c // 4) % 2]
            for tl in range(4):
                _mm(P, py[0:64, jj * 128:(jj + 1) * 128], CT[dd][:, tl, :], hh[:, tl, :], tl == 0, tl == 3, [pb, hhb], [pyb])
            if jj == 3 or c == NCH - 1:
                b0 = (c // 4) * 512
                wid = (jj + 1) * 128
                lo = 256 if ((not need_ctx_out) and c // 4 == 0) else 0
                if dd == 0:
                    _cp(P, "act", yacc[:, b0 + lo:b0 + wid], py[0:64, lo:wid], [pyb], [yb])
                else:
                    _tt(P, "dve", yacc[:, b0 + lo:b0 + wid], yacc[:, b0 + lo:b0 + wid], py[0:64, lo:wid], ALU.add, [pyb, yb], [yb])

        s2_G(c_start)
        for c in range(c_start, NCH):
            if c + 1 < NCH:
                s2_G(c + 1)
            s2_Y(c)
    zT = A(P, [64, NTOK], BF16); zb = P.buf("zT")
    g1 = [A(P, [64, 512], F32) for _ in range(2)]; g1b = [P.buf(), P.buf()]
    g2 = [A(P, [64, 512], F32) for _ in range(2)]; g2b = [P.buf(), P.buf()]
    lo_all = 0 if need_ctx_out else 256
    if not need_ctx_out:
        P.op("pool", lambda h: h.memset(zT[:, 0:256], 0.0), writes=[zb])
    nblk = (NTOK + 511) // 512

    def ge_rng(bi):
        t0 = max(bi * 512, lo_all)
        t1_ = min((bi + 1) * 512, NTOK)
        return t0, t1_, t1_ - t0

    def ge_A(bi):
        t0, t1_, n = ge_rng(bi)
        i2 = bi % 2
        y = g1[i2]; w = g2[i2]
        _stt(P, y[:, 0:n], sT[:, t0:t1_], dvec[:, 0:1], yacc[:, t0:t1_], ALU.mult, ALU.add, [sTb, pb, yb], [g1b[i2]])
        _tt(P, "dve", w[:, 0:n], y[:, 0:n], y[:, 0:n], ALU.mult, [g1b[i2]], [g2b[i2]])
        _ts(P, "dve", w[:, 0:n], w[:, 0:n], 0.044715, 1.0, ALU.mult, ALU.add, [g2b[i2]], [g2b[i2]])
        _tt(P, "dve", w[:, 0:n], w[:, 0:n], y[:, 0:n], ALU.mult, [g2b[i2], g1b[i2]], [g2b[i2]])
        _act(P, w[:, 0:n], w[:, 0:n], AF.Sigmoid, [g2b[i2]], [g2b[i2]], scale=1.5957691216057308)

    def ge_B(bi):
        t0, t1_, n = ge_rng(bi)
        i2 = bi % 2
        _tt(P, "dve", zT[:, t0:t1_], g2[i2][:, 0:n], g1[i2][:, 0:n], ALU.mult, [g2b[i2], g1b[i2]], [zb])

    ge_A(0)
    for bi in range(nblk):
        if bi + 1 < nblk:
            ge_A(bi + 1)
        ge_B(bi)
    outs.append(_ld(P, "sp", out[1, :, :], zT[:, :], [P.buf()], reads=[zb]))


import ml_dtypes

BF = ml_dtypes.bfloat16
NTOK = 8448
f32 = np.float32


def fm(a):
    return np.ascontiguousarray(a.T.reshape(8, 128, a.shape[0]))


def prep_common(inp, layer, b):
    cond = np.stack([inp['c'][b], inp['c_ctx']], 0)
    condT = np.ascontiguousarray(cond.reshape(2, 8, 128).transpose(2, 1, 0))
    bm = inp['b_mod'][layer].reshape(48, 128).T
    b_modT = np.ascontiguousarray(np.stack([bm, bm], -1))
    ng = inp['norm_g'][layer].reshape(4, 8, 128).transpose(2, 0, 1)
    norm_gT = np.ascontiguousarray(np.stack([ng, ng], -1))
    return dict(condT=condT, b_modT=b_modT, norm_gT=norm_gT, w_mod=inp['w_mod'][layer])


_const_cache = {}


def consts():
    if _const_cache:
        return _const_cache
    C = _const_cache
    c = np.arange(64)
    ang = 2 * np.pi * (np.outer(c, c) % 64) / 64
    C['f_CS'] = np.concatenate([np.cos(ang), -np.sin(ang)], 1).astype(BF)
    ca, sa = np.cos(ang), np.sin(ang)
    C['f_RP'] = np.concatenate([ca, -sa], 1).astype(BF)
    C['f_RQ'] = np.concatenate([sa, ca], 1).astype(BF)
    m2 = np.arange(128)[:, None, None]
    n1 = np.arange(64)[None, :, None]
    n2 = np.arange(128)[None, None, :]
    be = 2 * np.pi * ((m2 * (n1 + 64 * n2)) % 8192) / 8192
    nrm = 1 / np.sqrt(64 * 8192)
    C['f_CB'] = (np.cos(be) * nrm).astype(BF)
    C['f_SB'] = (np.sin(be) * nrm).astype(BF)
    m = np.arange(256)
    a256 = 2 * np.pi * (np.outer(m, m) % 256) / 256
    nrm2 = 1 / np.sqrt(64 * 256)
    C['f_C256'] = np.ascontiguousarray((np.cos(a256) * nrm2).reshape(2, 128, 256).transpose(1, 0, 2)).astype(BF)
    C['f_S256'] = np.ascontiguousarray((np.sin(a256) * nrm2).reshape(2, 128, 256).transpose(1, 0, 2)).astype(BF)
    t = np.arange(8192)
    row = (t // 64).astype(f32)
    col = (t % 64).astype(f32)
    inv = (1.0 / (f32(10000.0) ** (np.arange(16, dtype=f32) / f32(16)))).astype(f32)
    angr = np.concatenate([row[:, None] * inv, col[:, None] * inv], -1).astype(f32)
    cs, sn = np.cos(angr).astype(f32), np.sin(angr).astype(f32)
    cos64 = np.concatenate([cs, cs], 1)
    sin64 = np.concatenate([-sn, sn], 1)
    C['r_cosF'] = np.ascontiguousarray(cos64.T)
    C['r_sinF'] = np.ascontiguousarray(sin64.T)
    C['r_cosT'] = np.ascontiguousarray(cos64.reshape(64, 128, 64).transpose(1, 0, 2))
    C['r_sinT'] = np.ascontiguousarray(sin64.reshape(64, 128, 64).transpose(1, 0, 2))
    j = np.arange(128, dtype=f32)
    C['r_jcol'] = np.stack([127 - j, j], 1).astype(f32)
    ii = np.arange(128)
    dist = np.abs(ii[None, :] - ii[:, None]).astype(f32)
    C['r_dist'] = dist
    C['r_mask'] = np.stack([(ii[None, :] >= ii[:, None]), (ii[:, None] >= ii[None, :])], 0).astype(f32)
    C['r_irow'] = np.stack([np.tile(j + 1, (64, 1)), np.tile(128 - j, (64, 1))], 0).astype(f32)
    C['s_jrow'] = np.tile(np.arange(129, dtype=f32), (128, 1))
    C['s_jcol'] = np.arange(128, dtype=f32)[:, None].copy()
    C['s_LT'] = np.stack([(ii[None, :] >= ii[:, None]), (ii[:, None] >= ii[None, :])], 0).astype(BF)
    g_of_row = np.arange(64) // 16
    C['s_mrow'] = (g_of_row[:, None] == np.arange(4)[None, :]).astype(f32)
    g_of_st = (np.arange(128)[:, None] // 64) + 2 * np.arange(2)[None, :]
    C['s_msm'] = (g_of_st[:, :, None] == np.arange(4)[None, None, :]).astype(f32)
    C['ident_bf'] = np.eye(128).astype(BF)
    C['ident_f'] = np.eye(128).astype(f32)
    def start(r):
        return int(np.clip(r - 4, 0, 120))
    qc = np.arange(64)
    cst = np.clip(qc - 8, 0, 48)
    kc = np.arange(64)
    colok = (kc[None, :] >= cst[:, None]) & (kc[None, :] < cst[:, None] + 16)
    types = [(0, 0, 8), (2, 0, 8), (10, 6, 9), (124, 120, 8), (126, 120, 8)]
    mask = np.full((5, 128, 832), -30000.0, f32)
    drs = np.zeros((5, 2, 9), np.int64)
    for ti, (r0, R0, nr) in enumerate(types):
        for qr in range(2):
            r = r0 + qr
            for i in range(9):
                kr = R0 + i
                dr = int(np.clip(kr - r + 7, 0, 14))
                drs[ti, qr, i] = dr
                if i < nr and start(r) <= kr < start(r) + 8:
                    blk = np.where(colok, 0.0, -30000.0)
                    mask[ti, qr * 64:(qr + 1) * 64, i * 64:(i + 1) * 64] = blk
        mask[ti, :, 576:] = 0.0
    C['n_mask'] = mask
    C['n_drs'] = drs
    C['n_types'] = types
    return C


def prep_M(inp, layer, c, xT_full):
    b, q = c // 4, c % 4
    C = consts()
    m = prep_common(inp, layer, b)
    m['xT'] = xT_full
    w_in = inp['w_in'][layer]
    o = q * 64
    sw = np.r_[32:64, 0:32]
    cols_fm = np.concatenate([np.arange(0 + o, 0 + o + 64), np.arange(256 + o, 256 + o + 64), np.arange(512 + o, 512 + o + 64),
                              np.arange(768 + o, 768 + o + 64), 512 + o + sw, 768 + o + sw, np.arange(1280 + o, 1280 + o + 64),
                              np.arange(1536 + o, 1536 + o + 64), np.arange(1792 + o, 1792 + o + 64)])
    cols_tm = np.concatenate([np.arange(768 + o, 768 + o + 64), 768 + o + sw, np.arange(1024 + o, 1024 + o + 64), np.arange(2048 + o, 2048 + o + 64)])
    m['w_fm'] = np.ascontiguousarray(w_in[:, cols_fm])
    m['w_tm'] = np.ascontiguousarray(w_in[:, cols_tm])
    for k in ('f_CS', 'f_RP', 'f_RQ', 'f_CB', 'f_SB', 'f_C256', 'f_S256', 'r_cosF', 'r_sinF', 'r_cosT', 'r_sinT', 'r_jcol', 'r_dist', 'r_mask', 'r_irow',
              's_jrow', 's_jcol', 's_LT', 's_mrow', 's_msm', 'ident_bf', 'ident_f', 'n_mask'):
        m[k] = C[k]
    gs = slice(4 * q, 4 * q + 4)
    L = layer
    are, aim = inp['s5_a_re'][L][:, gs], inp['s5_a_im'][L][:, gs]
    ldt = inp['s5_log_dt'][L][:, gs]
    def sm(a):
        return np.ascontiguousarray(a.reshape(2, 2, 128).transpose(2, 0, 1))
    ldt_b = np.broadcast_to(ldt[:, :, None], (2, 4, 64))
    m['s_sm'] = np.ascontiguousarray(np.stack([sm(are), sm(aim), sm(ldt_b)], -1))
    row = np.stack([are.reshape(2, 256), aim.reshape(2, 256), ldt_b.reshape(2, 256)], -1)
    m['s_row'] = np.ascontiguousarray(np.broadcast_to(row[None].transpose(0, 1, 3, 2), (128, 2, 3, 256)))
    hs = np.stack([are, aim, ldt_b], 2)
    hs = np.broadcast_to(hs[:, :, None], (2, 4, 16, 3, 64))
    m['s_hs'] = np.ascontiguousarray(hs.transpose(1, 2, 0, 3, 4).reshape(64, 2, 3, 64))
    bre, bim = inp['s5_b_re'][L][:, gs], inp['s5_b_im'][L][:, gs]
    B = np.stack([bre, bim], 2)
    m['s_B'] = np.ascontiguousarray(B.transpose(1, 4, 0, 2, 3).reshape(64, 2, 2, 64))
    cre, cim = inp['s5_c_re'][L][:, gs], inp['s5_c_im'][L][:, gs]
    Cc = np.stack([cre, cim], 2)
    Cc = Cc.transpose(1, 4, 0, 2, 3)
    Cc = Cc.reshape(2, 2, 64, 2, 2, 16).transpose(1, 2, 3, 0, 4, 5).reshape(128, 2, 2, 2, 16)
    m['s_C'] = np.ascontiguousarray(Cc)
    m['s_d'] = np.ascontiguousarray(inp['s5_d'][L][256 * 0 + 64 * q:64 * q + 64][:, None])
    rd = inp['ret_decay'][L][:, q]
    m['r_dec'] = np.ascontiguousarray(np.broadcast_to(rd[None, :], (128, 2))).astype(f32)
    m['r_gn'] = np.ascontiguousarray(inp['ret_gn'][L][64 * q:64 * q + 64][:, None])
    rpb = inp['na_rpb'][L][q]
    dc = np.clip(np.arange(64)[None, :] - np.arange(64)[:, None], -15, 15) + 15
    m['n_toep'] = np.ascontiguousarray(rpb[:, dc])
    return m


def _prep_F(inp, layer, c, xa, bra_bf, moe_a=False):
    b = c // 4
    m = prep_common(inp, layer, b)
    m.update(xT=fm(xa), brT=bra_bf, w_in=inp['w_in'][layer], w_br=inp['w_branch'][layer].reshape(1024, 1024), w_o=inp['w_out'][layer],
             w_glu=inp['s5_w_glu'][layer], b_gluT=np.ascontiguousarray(inp['s5_b_glu'][layer].reshape(2, 128).T))
    i = layer // 2
    if layer % 2 == 0:
        m.update(w_g=inp['ffn_w_gate'][i:i + 1], w_u=inp['ffn_w_up'][i:i + 1], w_d=inp['ffn_w_down'][i:i + 1])
    else:
        sel = np.zeros((8, 8, 128), np.float32)
        for e in range(8):
            sel[e, e, :] = 1
        m.update(w_r=inp['moe_w_router'][i], b_r=np.ascontiguousarray(np.broadcast_to(inp['moe_b_router'][i][None], (128, 8))),
                 ident=np.eye(128, dtype=np.float32), sel=sel)
    return m


def kernel(**inputs):
    inp = {k: np.asarray(v) for k, v in inputs.items()}
    NCORE = 8
    cores = list(range(NCORE))
    x = inp['x']
    ctx = inp['ctx']
    for layer in range(2):
        last = (layer == 1)
        ncM = build_M(not last)
        xfull = [fm(np.concatenate([ctx[b], x[b]], 0)) for b in range(2)]
        maps = [prep_M(inp, layer, c, xfull[c // 4]) for c in cores]
        resM = run_bass_kernel_spmd(ncM, maps, core_ids=cores).results
        del maps
        br_full = []
        for b in range(2):
            o = np.stack([np.asarray(resM[4 * b + q]['brT_out']) for q in range(4)], 1)
            br_full.append(o.reshape(1024, NTOK))
        del resM
        if not last:
            blocksA = [(i * 256, 256, 0) for i in range(8)] + [(2048, 64, 1)]
            blocksB = [(i * 512, 512, 0) for i in range(4)] + [(2048, 64, 1)]
            ncF = build_F(blocksA, blocksB, 1, 2816, False)
        else:
            blocksA = [(i * 256, 256, 0) for i in range(8)]
            blocksB = [(i * 512, 512, 0) for i in range(4)]
            ncF = build_F(blocksA, blocksB, 8, 3584, True, mode='moe_a')
        maps = []
        for c in cores:
            b, q = c // 4, c % 4
            lat = slice(256 + q * 2048, 256 + (q + 1) * 2048)
            if not last:
                xa = np.concatenate([x[b, q * 2048:(q + 1) * 2048], ctx[b, q * 64:(q + 1) * 64]], 0)
                bra = np.concatenate([br_full[b][:, lat], br_full[b][:, q * 64:(q + 1) * 64]], 1)
            else:
                xa = x[b, q * 2048:(q + 1) * 2048]
                bra = br_full[b][:, lat]
            bra = np.ascontiguousarray(bra.reshape(8, 128, bra.shape[1]))
            maps.append(_prep_F(inp, layer, c, xa, bra))
        resF = run_bass_kernel_spmd(ncF, maps, core_ids=cores).results
        del maps
        if not last:
            xn = np.empty_like(x)
            cn = np.empty_like(ctx)
            for c in cores:
                b, q = c // 4, c % 4
                o = np.asarray(resF[c]['xo']).reshape(1024, -1).T
                xn[b, q * 2048:(q + 1) * 2048] = o[:2048]
                cn[b, q * 64:(q + 1) * 64] = o[2048:]
            x, ctx = xn, cn
            continue
        i = layer // 2
        h2_all = np.concatenate([np.asarray(resF[c]['h2o']) for c in cores], 2)
        cb_all = np.concatenate([np.asarray(resF[c]['cbo']) for c in cores], 1)
        sel = np.concatenate([np.asarray(resF[c]['mko']) for c in cores], 1).astype(bool)
        idx = [np.flatnonzero(sel[e]) for e in cores]
        nb = max(1, -(-max(len(t) for t in idx) // 512))
        ng = -(-nb // 4)
        groups = tuple(nb // ng + (1 if g < nb % ng else 0) for g in range(ng))
        C = 512 * nb
        ncE = build_E(groups)
        maps = []
        for e in cores:
            n_e = len(idx[e])
            ii = np.zeros(C, np.int64)
            ii[:n_e] = idx[e]
            cbe = np.zeros(C, cb_all.dtype)
            cbe[:n_e] = cb_all[e, idx[e]]
            maps.append(dict(h2=np.ascontiguousarray(h2_all[:, :, ii]), cbe=np.ascontiguousarray(np.broadcast_to(cbe[None, :], (128, C))),
                             w_g=inp['moe_w_gate'][i][e], w_u=inp['moe_w_up'][i][e], w_d=inp['moe_w_down'][i][e]))
        resE = run_bass_kernel_spmd(ncE, maps, core_ids=cores).results
        del maps
        slot = np.cumsum(sel, axis=0) - sel
        nslot = max(1, int(sel.sum(0).max()))
        yp_all = np.zeros((nslot, 8, 128, sel.shape[1]), np.float32)
        for e in cores:
            ye = np.asarray(resE[e]['ye'])
            t = idx[e]
            sv = slot[e, t]
            for k in range(nslot):
                mk = sv == k
                yp_all[k][:, :, t[mk]] = ye[:, :, np.flatnonzero(mk)]
        ncC = build_Fc(nexp=nslot)
        maps = []
        for c in cores:
            m = prep_common(inp, layer, c // 4)
            m['xm'] = np.asarray(resF[c]['xo'])
            m['yp'] = np.ascontiguousarray(yp_all[:, :, :, c * 2048:(c + 1) * 2048])
            maps.append(m)
        resC = run_bass_kernel_spmd(ncC, maps, core_ids=cores).results
        xn = np.empty_like(x)
        for c in cores:
            b, q = c // 4, c % 4
            xn[b, q * 2048:(q + 1) * 2048] = np.asarray(resC[c]['xo']).reshape(1024, -1).T
        x = xn
    return x.astype(np.float32)
```
